# Optimizing a Trainium2 kernel written in Bass

```python
import jax
import jax.numpy as jnp
from jax import lax
import numpy as np

D_MODEL = 1024
BATCH = 4
SEQ = 4096
DEPTH = 2

GRID_W = 64
CTX_LEN = 256
N_HEADS = 4
HEAD_DIM = 64
MIX_W = N_HEADS * HEAD_DIM
N_BRANCH = 4
CONV_W = 4
LRU_C = 8.0
RET_CHUNK = 128
MLSTM_CHUNK = 128
HGRN_CHUNK = 16
ROPE_BASE = 10000.0
N_GROUPS = 4
EXP_PER_GROUP = 4
N_EXPERTS = N_GROUPS * EXP_PER_GROUP
TOP_K = 2
D_EXPERT = 512
N_MOD = 6
EPS = 1e-6
M_INIT = -1e30

IN_COLS = (
    ('a_x', MIX_W), ('a_g', MIX_W),
    ('b_q', MIX_W), ('b_k', MIX_W), ('b_v', MIX_W), ('b_g', MIX_W),
    ('c_q', MIX_W), ('c_k', MIX_W), ('c_v', MIX_W), ('c_o', MIX_W), ('c_gates', 4 * N_HEADS),
    ('d_q', MIX_W), ('d_ff', MIX_W), ('d_fb', MIX_W), ('d_i', MIX_W), ('d_g', MIX_W),
    ('merge', N_BRANCH * D_MODEL),
)
IN_W = 15 * MIX_W + 4 * N_HEADS + N_BRANCH * D_MODEL

kernel_name = 'hybrid_lru_retention_mlstm_hgrn2_hmoe_dit'


def rmsnorm(t):
    tf = t.astype(jnp.float32)
    return (tf * lax.rsqrt(jnp.mean(tf * tf, -1, keepdims=True) + EPS)).astype(t.dtype)


def modulate(t, shift, scale):
    return rmsnorm(t) * (1.0 + scale) + shift


def split_cols(p):
    out, off = {}, 0
    for name, width in IN_COLS:
        out[name] = p[..., off:off + width]
        off += width
    return out


def heads(t):
    b, n, _ = t.shape
    return t.astype(jnp.float32).reshape(b, n, N_HEADS, HEAD_DIM).transpose(0, 2, 1, 3)


def head_norm(o, center):
    if center:
        o = o - jnp.mean(o, -1, keepdims=True)
    o = o * lax.rsqrt(jnp.mean(o * o, -1, keepdims=True) + EPS)
    b, h, n, dh = o.shape
    return o.transpose(0, 2, 1, 3).reshape(b, n, h * dh)


def tflip(t, rev):
    return jnp.flip(t, 2) if rev else t


def centred_dwconv(t, w, b):
    y = lax.conv_general_dilated(
        t, w[:, None, :].astype(t.dtype), window_strides=(1,), padding=((2, 1),),
        dimension_numbers=('NWC', 'WIO', 'NWC'), feature_group_count=t.shape[-1])
    return y + b


def rope_tables(row, col):
    n = HEAD_DIM // 4
    inv = jnp.power(ROPE_BASE, -jnp.arange(n, dtype=jnp.float32) / n)
    ang = jnp.concatenate([row[:, None] * inv, col[:, None] * inv], -1)
    return jnp.cos(ang), jnp.sin(ang)


def apply_rope(t, cos, sin):
    half = HEAD_DIM // 2
    t1, t2 = t[..., :half], t[..., half:]
    return jnp.concatenate([t1 * cos - t2 * sin, t1 * sin + t2 * cos], -1)


def rglru_scan(u, w_r, b_r, w_i, b_i, lam, h0, reverse):
    b, n, _ = u.shape
    uf = u.astype(jnp.float32)
    if reverse:
        uf = jnp.flip(uf, 1)
    uh = uf.reshape(b, n, N_HEADS, HEAD_DIM)
    r = jax.nn.sigmoid(jnp.einsum('bthi,hij->bthj', uh, w_r).reshape(b, n, MIX_W) + b_r)
    gi = jax.nn.sigmoid(jnp.einsum('bthi,hij->bthj', uh, w_i).reshape(b, n, MIX_W) + b_i)
    log_a = LRU_C * r * jax.nn.log_sigmoid(lam.astype(jnp.float32))
    a = jnp.exp(log_a)
    inp = jnp.sqrt(-jnp.expm1(2.0 * log_a)) * (gi * uf)

    def combine(left, right):
        a_l, h_l = left
        a_r, h_r = right
        return a_r * a_l, a_r * h_l + h_r

    a_cum, h = lax.associative_scan(combine, (a, inp), axis=1)
    h = h + a_cum * h0[:, None, :]
    h_final = h[:, -1]
    if reverse:
        h = jnp.flip(h, 1)
    return h, h_final


def mixer_rglru(pc, px, conv_w, conv_b, gate_w, gate_b, lam):
    uc = centred_dwconv(pc['a_x'], conv_w, conv_b)
    ux = centred_dwconv(px['a_x'], conv_w, conv_b)
    h0 = jnp.zeros((uc.shape[0], MIX_W), jnp.float32)
    hcs, hxs = [], []
    for d in range(2):
        prm = (gate_w[d, 0], gate_b[d, 0], gate_w[d, 1], gate_b[d, 1], lam[d])
        hc, state = rglru_scan(uc, *prm, h0, d == 1)
        hx, _ = rglru_scan(ux, *prm, state, d == 1)
        hcs.append(hc)
        hxs.append(hx)
    yc = jax.nn.gelu(pc['a_g'].astype(jnp.float32)) * (hcs[0] + hcs[1])
    yx = jax.nn.gelu(px['a_g'].astype(jnp.float32)) * (hxs[0] + hxs[1])
    return yc, yx


def retention_chunked(q, k, v, log_gamma, s0, include_diag):
    b, h, n, dh = q.shape
    L = RET_CHUNK
    nc = n // L
    q, k, v = (t.reshape(b, h, nc, L, dh) for t in (q, k, v))
    pos = jnp.arange(L, dtype=jnp.float32)
    diff = pos[:, None] - pos[None, :]
    mask = (diff >= 0) if include_diag else (diff > 0)
    decay = jnp.where(mask, jnp.exp(log_gamma[:, None, None] * jnp.maximum(diff, 0.0)), 0.0)
    scores = jnp.einsum('bhnid,bhnjd->bhnij', q, k) * decay[:, None]
    o_intra = jnp.einsum('bhnij,bhnje->bhnie', scores, v)
    k_w = jnp.exp(log_gamma[:, None] * (L - 1.0 - pos))
    kv = jnp.einsum('bhnjd,bhnje->bhnde', k * k_w[:, None, :, None], v)
    g_chunk = jnp.exp(log_gamma * L)[:, None, None]

    def step(s, kv_n):
        return g_chunk * s + kv_n, s

    s_final, s_prev = lax.scan(step, s0, jnp.moveaxis(kv, 2, 0))
    s_prev = jnp.moveaxis(s_prev, 0, 2)
    q_w = jnp.exp(log_gamma[:, None] * (pos + 1.0))
    o_inter = jnp.einsum('bhnid,bhnde->bhnie', q * q_w[:, None, :, None], s_prev)
    return (o_intra + o_inter).reshape(b, h, n, dh), s_final


def mixer_retention(pc, px, theta, cos, sin):
    scale = HEAD_DIM ** -0.5
    qc, kc, vc = heads(pc['b_q']), heads(pc['b_k']) * scale, heads(pc['b_v'])
    qx = apply_rope(heads(px['b_q']), cos, sin)
    kx = apply_rope(heads(px['b_k']), cos, sin) * scale
    vx = heads(px['b_v'])
    s0 = jnp.zeros(qc.shape[:2] + (HEAD_DIM, HEAD_DIM), jnp.float32)
    ocs, oxs = [], []
    for d in range(2):
        rev = d == 1
        lg = jax.nn.log_sigmoid(theta[d].astype(jnp.float32))
        oc, state = retention_chunked(tflip(qc, rev), tflip(kc, rev), tflip(vc, rev), lg, s0, not rev)
        ox, _ = retention_chunked(tflip(qx, rev), tflip(kx, rev), tflip(vx, rev), lg, state, not rev)
        ocs.append(tflip(oc, rev))
        oxs.append(tflip(ox, rev))

    def out(p, o):
        return head_norm(o[0] + o[1], True) * jax.nn.silu(p['b_g'].astype(jnp.float32))

    return out(pc, ocs), out(px, oxs)


def mlstm_chunked(q, k, v, log_i, log_f, state):
    b, h, n, dh = q.shape
    L = MLSTM_CHUNK
    nc = n // L
    q, k, v = (t.reshape(b, h, nc, L, dh) for t in (q, k, v))
    log_i = log_i.reshape(b, h, nc, L)
    cum = jnp.cumsum(log_f.reshape(b, h, nc, L), -1)
    causal = jnp.tril(jnp.ones((L, L), bool))
    log_d = jnp.where(causal, cum[..., :, None] - cum[..., None, :] + log_i[..., None, :], -jnp.inf)
    m_intra = jnp.max(log_d, -1)
    cum_end = cum[..., -1]
    log_w = cum_end[..., None] - cum + log_i
    m_loc = jnp.max(log_w, -1)
    w = jnp.exp(log_w - m_loc[..., None])
    c_loc = jnp.einsum('bhnj,bhnjd,bhnje->bhnde', w, k, v)
    n_loc = jnp.einsum('bhnj,bhnjd->bhnd', w, k)

    def step(carry, xs):
        c_s, n_s, m_s = carry
        c_l, n_l, m_l, ce = xs
        m_new = jnp.maximum(ce + m_s, m_l)
        s_old = jnp.exp(ce + m_s - m_new)
        s_loc = jnp.exp(m_l - m_new)
        c_new = s_old[..., None, None] * c_s + s_loc[..., None, None] * c_l
        n_new = s_old[..., None] * n_s + s_loc[..., None] * n_l
        return (c_new, n_new, m_new), (c_s, n_s, m_s)

    xs = tuple(jnp.moveaxis(t, 2, 0) for t in (c_loc, n_loc, m_loc, cum_end))
    final, prev = lax.scan(step, state, xs)
    c_prev, n_prev, m_prev = (jnp.moveaxis(t, 0, 2) for t in prev)
    m_inter = cum + m_prev[..., None]
    m_q = jnp.maximum(m_intra, m_inter)
    s = jnp.einsum('bhnid,bhnjd->bhnij', q, k) * jnp.exp(log_d - m_q[..., None])
    s_inter = jnp.exp(m_inter - m_q)
    num = jnp.einsum('bhnij,bhnje->bhnie', s, v) + s_inter[..., None] * jnp.einsum('bhnid,bhnde->bhnie', q, c_prev)
    den = jnp.sum(s, -1) + s_inter * jnp.einsum('bhnid,bhnd->bhni', q, n_prev)
    hid = num / jnp.maximum(jnp.abs(den), jnp.exp(-m_q))[..., None]
    return hid.reshape(b, h, n, dh), final


def mixer_mlstm(pc, px, conv_w, conv_b, gate_b):
    def prep(p):
        b, n, _ = p['c_q'].shape
        qk = jax.nn.silu(centred_dwconv(jnp.concatenate([p['c_q'], p['c_k']], -1), conv_w, conv_b))
        q = heads(qk[..., :MIX_W])
        k = heads(qk[..., MIX_W:]) * HEAD_DIM ** -0.5
        v = heads(p['c_v'])
        g = (p['c_gates'].astype(jnp.float32) + gate_b.reshape(-1)).reshape(b, n, 4, N_HEADS)
        return q, k, v, g.transpose(2, 0, 3, 1)

    qc, kc, vc, gc = prep(pc)
    qx, kx, vx, gx = prep(px)
    b = qc.shape[0]
    init = (jnp.zeros((b, N_HEADS, HEAD_DIM, HEAD_DIM), jnp.float32),
            jnp.zeros((b, N_HEADS, HEAD_DIM), jnp.float32),
            jnp.full((b, N_HEADS), M_INIT, jnp.float32))
    hcs, hxs = [], []
    for d in range(2):
        rev = d == 1
        hc, state = mlstm_chunked(tflip(qc, rev), tflip(kc, rev), tflip(vc, rev), tflip(gc[2 * d], rev),
                                  tflip(jax.nn.log_sigmoid(gc[2 * d + 1]), rev), init)
        hx, _ = mlstm_chunked(tflip(qx, rev), tflip(kx, rev), tflip(vx, rev), tflip(gx[2 * d], rev),
                              tflip(jax.nn.log_sigmoid(gx[2 * d + 1]), rev), state)
        hcs.append(tflip(hc, rev))
        hxs.append(tflip(hx, rev))

    def out(p, o):
        return jax.nn.sigmoid(p['c_o'].astype(jnp.float32)) * head_norm(o[0] + o[1], True)

    return out(pc, hcs), out(px, hxs)


def gla_chunked(q, k, v, log_f, s0):
    b, h, n, dk = q.shape
    dv = v.shape[-1]
    L = HGRN_CHUNK
    nc = n // L
    q, k, log_f = (t.reshape(b, h, nc, L, dk) for t in (q, k, log_f))
    v = v.reshape(b, h, nc, L, dv)
    cum = jnp.cumsum(log_f, 3)
    causal = jnp.tril(jnp.ones((L, L), bool))[:, :, None]
    decay = jnp.exp(jnp.where(causal, cum[..., :, None, :] - cum[..., None, :, :], -jnp.inf))
    scores = jnp.einsum('bhnic,bhnjc,bhnijc->bhnij', q, k, decay)
    o_intra = jnp.einsum('bhnij,bhnje->bhnie', scores, v)
    cum_end = cum[..., -1, :]
    kv = jnp.einsum('bhnjc,bhnje->bhnce', k * jnp.exp(cum_end[..., None, :] - cum), v)

    def step(s, xs):
        kv_n, ce = xs
        return jnp.exp(ce)[..., None] * s + kv_n, s

    s_final, s_prev = lax.scan(step, s0, (jnp.moveaxis(kv, 2, 0), jnp.moveaxis(cum_end, 2, 0)))
    s_prev = jnp.moveaxis(s_prev, 0, 2)
    o_inter = jnp.einsum('bhnic,bhnce->bhnie', q * jnp.exp(cum), s_prev)
    return (o_intra + o_inter).reshape(b, h, n, dv), s_final


def mixer_hgrn2(pc, px, lb):
    def prep(p):
        q = heads(jax.nn.silu(p['d_q'].astype(jnp.float32)))
        v = heads(p['d_i'])
        f = [heads(lb + (1.0 - lb) * jax.nn.sigmoid(p[name].astype(jnp.float32))) for name in ('d_ff', 'd_fb')]
        return q, v, f

    qc, vc, fc = prep(pc)
    qx, vx, fx = prep(px)
    s0 = jnp.zeros(qc.shape[:2] + (HEAD_DIM, HEAD_DIM), jnp.float32)
    ocs, oxs = [], []
    for d in range(2):
        rev = d == 1
        f_c, f_x = tflip(fc[d], rev), tflip(fx[d], rev)
        oc, state = gla_chunked(tflip(qc, rev), 1.0 - f_c, tflip(vc, rev), jnp.log(f_c), s0)
        ox, _ = gla_chunked(tflip(qx, rev), 1.0 - f_x, tflip(vx, rev), jnp.log(f_x), state)
        ocs.append(tflip(oc, rev))
        oxs.append(tflip(ox, rev))

    def out(p, o):
        return head_norm(o[0] + o[1], False) * jax.nn.silu(p['d_g'].astype(jnp.float32))

    return out(pc, ocs), out(px, oxs)


def token_mixers(hc, hx, w_in, a_conv_w, a_conv_b, a_gate_w, a_gate_b, a_lambda, b_theta,
                 c_conv_w, c_conv_b, c_gate_b, lb, w_branch, w_out, cos, sin, need_ctx):
    pc = split_cols(hc @ w_in)
    px = split_cols(hx @ w_in)
    ya = mixer_rglru(pc, px, a_conv_w, a_conv_b, a_gate_w, a_gate_b, a_lambda)
    yb = mixer_retention(pc, px, b_theta, cos, sin)
    yc = mixer_mlstm(pc, px, c_conv_w, c_conv_b, c_gate_b)
    yd = mixer_hgrn2(pc, px, lb)

    def merge(p, ys, dtype):
        y = jnp.stack(ys, 2).astype(dtype)
        gate = jax.nn.sigmoid(p['merge'].reshape(y.shape[:2] + (N_BRANCH, D_MODEL)))
        z = jnp.sum(gate * jnp.einsum('btnw,nwd->btnd', y, w_branch), 2)
        return z @ w_out

    mix_x = merge(px, [ya[1], yb[1], yc[1], yd[1]], hx.dtype)
    mix_c = merge(pc, [ya[0], yb[0], yc[0], yd[0]], hc.dtype) if need_ctx else None
    return mix_c, mix_x


def hier_moe(h, w_group, b_group, w_router, b_router, w1, w3, w2):
    lead = h.shape[:-1]
    g_logit = (h @ w_group).astype(jnp.float32) + b_group
    g_idx = jnp.argmax(g_logit, -1)
    g_prob = jnp.max(jax.nn.softmax(g_logit, -1), -1, keepdims=True)
    e_logit = ((h @ w_router).astype(jnp.float32) + b_router).reshape(lead + (N_GROUPS, EXP_PER_GROUP))
    e_logit = jnp.sum(e_logit * jax.nn.one_hot(g_idx, N_GROUPS, dtype=jnp.float32)[..., None], -2)
    top_v, top_i = lax.top_k(e_logit, TOP_K)
    top_w = jax.nn.softmax(top_v, -1) * g_prob
    expert_id = g_idx[..., None] * EXP_PER_GROUP + top_i
    gates = jnp.sum(jax.nn.one_hot(expert_id, N_EXPERTS, dtype=jnp.float32) * top_w[..., None], -2)
    out = jnp.zeros_like(h)
    for e in range(N_EXPERTS):
        y = (jax.nn.silu(h @ w1[e]) * (h @ w3[e])) @ w2[e]
        out = out + gates[..., e:e + 1].astype(h.dtype) * y
    return out


def setup_inputs(seed: int = 0) -> dict:
    key = jax.random.key(seed)
    ks = jax.random.split(key, 28)
    f32 = jnp.float32

    def nrm(k, shape, scale):
        return jax.random.normal(k, shape, f32) * scale

    lru_a8 = jax.random.uniform(ks[11], (DEPTH, 2, MIX_W), f32, 0.9, 0.999)
    lru_a = lru_a8 ** (1.0 / LRU_C)
    gamma = 1.0 - 2.0 ** (-5.0 - jnp.arange(N_HEADS, dtype=f32))
    theta0 = jnp.log(gamma) - jnp.log1p(-gamma)
    zero_h = jnp.zeros((N_HEADS,), f32)
    f_bias = jnp.linspace(3.0, 6.0, N_HEADS, dtype=f32)
    gate_bias0 = jnp.stack([zero_h, f_bias, zero_h, f_bias])
    return {
        'x': nrm(ks[0], (BATCH, SEQ, D_MODEL), 1.0),
        'c': nrm(ks[1], (BATCH, D_MODEL), 1.0),
        'ctx': nrm(ks[2], (BATCH, CTX_LEN, D_MODEL), 1.0),
        'c_ctx': nrm(ks[3], (D_MODEL,), 1.0),
        'w_mod': nrm(ks[4], (DEPTH, D_MODEL, N_MOD * D_MODEL), 0.5 * D_MODEL ** -0.5),
        'b_mod': nrm(ks[5], (DEPTH, N_MOD * D_MODEL), 0.02),
        'w_in': nrm(ks[6], (DEPTH, D_MODEL, IN_W), D_MODEL ** -0.5),
        'a_conv_w': nrm(ks[7], (DEPTH, CONV_W, MIX_W), CONV_W ** -0.5),
        'a_conv_b': nrm(ks[8], (DEPTH, MIX_W), 0.02),
        'a_gate_w': nrm(ks[9], (DEPTH, 2, 2, N_HEADS, HEAD_DIM, HEAD_DIM), HEAD_DIM ** -0.5),
        'a_gate_b': nrm(ks[10], (DEPTH, 2, 2, MIX_W), 0.02),
        'a_lambda': jnp.log(lru_a) - jnp.log1p(-lru_a),
        'b_theta': theta0 + nrm(ks[12], (DEPTH, 2, N_HEADS), 0.01),
        'c_conv_w': nrm(ks[13], (DEPTH, CONV_W, 2 * MIX_W), CONV_W ** -0.5),
        'c_conv_b': nrm(ks[14], (DEPTH, 2 * MIX_W), 0.02),
        'c_gate_b': gate_bias0 + nrm(ks[15], (DEPTH, 4, N_HEADS), 0.1),
        'd_lb': nrm(ks[16], (DEPTH, MIX_W), 1.0),
        'w_branch': nrm(ks[17], (DEPTH, N_BRANCH, MIX_W, D_MODEL), MIX_W ** -0.5),
        'w_out': nrm(ks[18], (DEPTH, D_MODEL, D_MODEL), D_MODEL ** -0.5),
        'moe_w_group': nrm(ks[19], (DEPTH, D_MODEL, N_GROUPS), D_MODEL ** -0.5),
        'moe_b_group': nrm(ks[20], (DEPTH, N_GROUPS), 0.01),
        'moe_w_router': nrm(ks[21], (DEPTH, D_MODEL, N_EXPERTS), D_MODEL ** -0.5),
        'moe_b_router': nrm(ks[22], (DEPTH, N_EXPERTS), 0.01),
        'moe_w1': nrm(ks[23], (DEPTH, N_EXPERTS, D_MODEL, D_EXPERT), D_MODEL ** -0.5),
        'moe_w3': nrm(ks[24], (DEPTH, N_EXPERTS, D_MODEL, D_EXPERT), D_MODEL ** -0.5),
        'moe_w2': nrm(ks[25], (DEPTH, N_EXPERTS, D_EXPERT, D_MODEL), D_EXPERT ** -0.5),
        'final_norm_w': 1.0 + nrm(ks[26], (D_MODEL,), 0.02),
    }


def reference(x, c, ctx, c_ctx, w_mod, b_mod, w_in, a_conv_w, a_conv_b, a_gate_w, a_gate_b,
              a_lambda, b_theta, c_conv_w, c_conv_b, c_gate_b, d_lb, w_branch, w_out,
              moe_w_group, moe_b_group, moe_w_router, moe_b_router, moe_w1, moe_w3, moe_w2,
              final_norm_w):
    n_lat = x.shape[1]
    rows = n_lat // GRID_W
    row = jnp.repeat(jnp.arange(rows), GRID_W)
    col = jnp.tile(jnp.arange(GRID_W), rows)
    cos, sin = rope_tables(row, col)
    lbs = jnp.cumsum(jax.nn.softmax(d_lb.astype(jnp.float32), axis=0), axis=0)
    lbs = lbs - lbs[0]
    s_c = jax.nn.silu(c)
    s_cc = jax.nn.silu(c_ctx)
    for l in range(DEPTH):
        need_ctx = l < DEPTH - 1
        mx = (s_c @ w_mod[l] + b_mod[l]).reshape(-1, N_MOD, 1, D_MODEL)
        mc = (s_cc @ w_mod[l] + b_mod[l]).reshape(N_MOD, D_MODEL)
        hx = modulate(x, mx[:, 0], mx[:, 1])
        hc = modulate(ctx, mc[0], mc[1])
        mix_c, mix_x = token_mixers(hc, hx, w_in[l], a_conv_w[l], a_conv_b[l], a_gate_w[l], a_gate_b[l],
                                    a_lambda[l], b_theta[l], c_conv_w[l], c_conv_b[l], c_gate_b[l],
                                    lbs[l], w_branch[l], w_out[l], cos, sin, need_ctx)
        moe_prm = (moe_w_group[l], moe_b_group[l], moe_w_router[l], moe_b_router[l],
                   moe_w1[l], moe_w3[l], moe_w2[l])
        x = x + mx[:, 2] * mix_x
        x = x + mx[:, 5] * hier_moe(modulate(x, mx[:, 3], mx[:, 4]), *moe_prm)
        if need_ctx:
            ctx = ctx + mc[2] * mix_c
            ctx = ctx + mc[5] * hier_moe(modulate(ctx, mc[3], mc[4]), *moe_prm)
    return rmsnorm(x) * final_norm_w
```

```python
import contextlib
import numpy as np
import ml_dtypes
import concourse.bass as bass
import concourse.mybir as mybir
from concourse.bass_utils import run_bass_kernel_spmd

F32 = mybir.dt.float32
BF16 = mybir.dt.bfloat16
AF = mybir.ActivationFunctionType
ALU = mybir.AluOpType
AX = mybir.AxisListType

T = 4352
NT = 34
D = 1024
EPS = 1e-6
NL = 2
DL = 32
DNS = 128 // DL
DBG = dict(maxit=None, core=True, fin=True, heads=(0, 1, 2, 3))
TMW = 2320
TM_OFF = dict(b_v=0, b_g=256, c_v=512, c_o=768, d_q=1024, d_ff=1280, d_fb=1536, d_i=1792, d_g=2048, c_gates=2304)
FM_OFF = dict(a_x=0, a_g=2, b_q=4, b_qp=6, b_k=8, b_kp=10, c_q=12, c_k=14)

CST = {}
_off = 0
for _n, _w in (("ident", 128), ("triF", 128), ("triB", 128), ("blkF", 128), ("blkB", 128), ("aftF", 128),
               ("befB", 128), ("diffF", 128), ("diffB", 128), ("maskF", 128), ("maskB", 128), ("posF", 128),
               ("posB", 128), ("negF4", 512), ("negB4", 512), ("mblkF4", 512), ("mblkB4", 512), ("kpos", 2),
               ("ones", 128), ("sel", 256), ("subm", 4), ("qmask", 1280)):
    CST[_n] = (_off, _w)
    _off += _w
NCST = _off


class MK:
    SEM_ROT = 30000

    def __init__(self, nc, ndma=8):
        self.nc = nc
        self.engs = {"pe": nc.tensor, "act": nc.scalar, "dve": nc.vector, "pool": nc.gpsimd, "sp": nc.sync}
        self._ctxs = []
        self.nsem = 0
        self.sem = {}
        self.cnt = {}
        for e in ("pe", "act", "dve", "pool"):
            self.sem[e] = self._newsem("s_" + e)
            self.cnt[e] = 0
        self.seen = {e: {} for e in self.engs}
        self.dq = {}
        for q in ("sp", "pool"):
            self.dq[q] = {"i": 0, "slots": [[self._newsem(f"d_{q}{i}"), 0] for i in range(ndma)]}
        self.res = {}
        self.ninst = 0
        self.flip = 0

    def _newsem(self, name):
        self.nsem += 1
        cm = self.nc.semaphore(f"{name}_{self.nsem}")
        s = cm.__enter__()
        self._ctxs.append(cm)
        return s

    def _wait(self, eng, tok):
        sem, val = tok
        key = id(sem)
        if self.seen[eng].get(key, 0) >= val:
            return
        self.engs[eng].wait_ge(sem, val)
        self.seen[eng][key] = val

    def _deps(self, R, W):
        deps = []
        for k in R:
            st = self.res.get(k)
            if st and st[0] is not None:
                deps.append(st[0])
        for k in W:
            st = self.res.get(k)
            if st:
                if st[0] is not None:
                    deps.append(st[0])
                deps.extend(st[1])
        return deps

    def _record(self, tok, R, W):
        for k in R:
            st = self.res.setdefault(k, [None, []])
            st[1] = [t for t in st[1] if t[0] is not tok[0]] + [tok]
        for k in W:
            self.res[k] = [tok, []]

    def op(self, eng, method, *args, R=(), W=(), **kw):
        for tok in self._deps(R, W):
            if eng == "pe" and tok[0] is self.sem["pe"]:
                continue
            self._wait(eng, tok)
        ins = getattr(self.engs[eng], method)(*args, **kw)
        if self.cnt[eng] >= self.SEM_ROT:
            self.sem[eng] = self._newsem("s_" + eng)
            self.cnt[eng] = 0
        self.cnt[eng] += 1
        ins.then_inc(self.sem[eng], 1)
        tok = (self.sem[eng], self.cnt[eng])
        self._record(tok, R, W)
        self.ninst += 1
        return tok

    def dma(self, q, out, in_, R=(), W=(), **kw):
        d = self.dq[q]
        slot = d["slots"][d["i"] % len(d["slots"])]
        d["i"] += 1
        if slot[1] > 0:
            self._wait(q, (slot[0], slot[1]))
        if slot[1] >= self.SEM_ROT:
            slot[0] = self._newsem("d_" + q)
            slot[1] = 0
        for tok in self._deps(R, W):
            self._wait(q, tok)
        ins = self.engs[q].dma_start(out=out, in_=in_, **kw)
        slot[1] += 16
        ins.then_inc(slot[0], 16)
        tok = (slot[0], slot[1])
        self._record(tok, R, W)
        self.ninst += 1
        return tok

    def barrier(self, engines=("pe", "act", "dve", "pool", "sp")):
        toks = []
        for q, d in self.dq.items():
            for slot in d["slots"]:
                if slot[1] > 0:
                    toks.append((slot[0], slot[1]))
        for e in ("pe", "act", "dve", "pool"):
            if self.cnt[e] > 0:
                toks.append((self.sem[e], self.cnt[e]))
        for e in engines:
            for tok in toks:
                if e in self.sem and tok[0] is self.sem[e]:
                    continue
                self._wait(e, tok)

    def ev(self, out, in_, R=(), W=(), func=None, **kw):
        if func is not None:
            return self.op("act", "activation", out=out, in_=in_, func=func, R=R, W=W, **kw)
        self.flip ^= 1
        if self.flip:
            return self.op("act", "activation", out=out, in_=in_, func=AF.Copy, R=R, W=W)
        return self.op("dve", "tensor_copy", out=out, in_=in_, R=R, W=W)


class Pool:
    def __init__(self, nc, tag):
        self.nc = nc
        self.tag = tag
        self.stack = contextlib.ExitStack()
        self.n = 0

    def sb(self, shape, dt=F32, name=None):
        self.n += 1
        return self.stack.enter_context(self.nc.sbuf_tensor(f"{self.tag}_{name or 't'}{self.n}", list(shape), dt))

    def close(self):
        self.stack.close()


def build_program(debug=()):
    nc = bass.Bass("TRN2", target_bir_lowering=False)

    def din(name, shape, dt=F32):
        return nc.dram_tensor(name, list(shape), dt, kind="ExternalInput").ap()

    def dscr(name, shape, dt=F32):
        return nc.dram_tensor(name, list(shape), dt, kind="Internal").ap()

    xin = din("xin", [T, D])
    cvec = din("cvec", [128, 8, 2])
    w_mod = din("w_mod", [NL, D, 6144])
    bmod_c = din("bmod_c", [NL, 128, 48])
    bmod_r = din("bmod_r", [NL, 1, 6144])
    w_fm = din("w_fm", [NL, D, 2048])
    w_tm = din("w_tm", [NL, D, TMW + 4096])
    a_cw = din("a_cw", [NL, 128, 2, 4])
    a_cb = din("a_cb", [NL, 128, 2])
    a_gw = din("a_gw", [NL, 128, 2, 2, 2, 128])
    a_gb = din("a_gb", [NL, 128, 2, 2, 2])
    a_lam = din("a_lam", [NL, 128, 2, 2])
    b_thp = din("b_thp", [NL, 128, 2, 2])
    b_thh = din("b_thh", [NL, 128, 2, 4])
    c_cw = din("c_cw", [NL, 128, 4, 4])
    c_cb = din("c_cb", [NL, 128, 4])
    c_gb = din("c_gb", [NL, 128, 16])
    d_lbr = din("d_lbr", [128, 2, 256])
    w_branch = din("w_branch", [NL, 4, 256, D])
    w_out = din("w_out", [NL, D, D])
    moe_wgr = din("moe_wgr", [NL, D, 20])
    moe_bgr = din("moe_bgr", [NL, 1, 20])
    moe_w1 = din("moe_w1", [NL, 16, D, 512])
    moe_w3 = din("moe_w3", [NL, 16, D, 512])
    moe_w2 = din("moe_w2", [NL, 16, 512, D])
    fnw = din("fnw", [128, D])
    cst_d = din("cst", [128, NCST])
    ropeC_d = din("ropeC", [128, T])
    ropeS_d = din("ropeS", [128, T])
    yout = nc.dram_tensor("yout", [4096, D], F32, kind="ExternalOutput").ap()

    XR = dscr("XR", [T, D])
    FM = dscr("FM", [16, 128, T])
    TM = dscr("TM", [T, TMW])
    MG = dscr("MG", [T, 4096], BF16)
    YT = dscr("YT", [4, 2, 128, T], BF16)
    dbg = {}
    for name, shape, dt in debug:
        dbg[name] = nc.dram_tensor("dbg_" + name, list(shape), dt, kind="ExternalOutput").ap()

    mk = MK(nc)
    G = Pool(nc, "g")

    PS = [nc.psum_tensor(f"ps{i}", [128, 512], F32).__enter__() for i in range(6)]
    PQ = [nc.psum_tensor(f"pq{i}", [128, 1024], BF16).__enter__() for i in range(2)]

    cst = G.sb([128, NCST], F32, "cst")
    identb = G.sb([128, 128], BF16, "identb")
    sT = G.sb([128, 8, 2], F32, "sT")
    MODC = G.sb([128, 4, 8, 2], F32, "MODC")
    GB = G.sb([128, 2, 2, D], F32, "GB")
    mk.dma("sp", cst[:], cst_d, W=["cst"])
    mk.dma("sp", sT[:], cvec, W=["sT"])

    def C(name, rows=slice(0, 128)):
        o, w = CST[name]
        return cst[rows, o:o + w]

    mk.op("dve", "tensor_copy", out=identb[:], in_=C("ident"), R=["cst"], W=["identb"])
    mk.op("act", "activation", out=sT[:], in_=sT[:], func=AF.Silu, R=["sT"], W=["sT"])
    for t in range(NT):
        mk.dma("sp", XR[t * 128:(t + 1) * 128, :], xin[t * 128:(t + 1) * 128, :], W=[f"XR{t}"])

    def cond_of(t):
        return 0 if t < 2 else 1

    def mod_phase(l):
        P = Pool(nc, f"mod{l}")
        wblk = P.sb([128, 8, 1024], F32, "wblk")
        bmc = P.sb([128, 48], F32, "bmc")
        bmr = P.sb([1, 6144], F32, "bmr")
        GR = P.sb([2, 2, D], F32, "GR")
        mk.dma("sp", bmc[:], bmod_c[l], W=["bmc"])
        mk.dma("sp", bmr[:], bmod_r[l], W=["bmr"])
        sel = C("sel", slice(0, 2))
        for m in range(6):
            mk.dma("sp", wblk[:], w_mod[l].rearrange("(c p) f -> p c f", p=128)[:, :, m * 1024:(m + 1) * 1024],
                   W=["wblk"])
            if m in (0, 1, 3, 4):
                m4 = {0: 0, 1: 1, 3: 2, 4: 3}[m]
                for c in range(8):
                    for k in range(8):
                        mk.op("pe", "matmul", PS[0][:, c * 2:(c + 1) * 2], lhsT=wblk[:, k, c * 128:(c + 1) * 128],
                              rhs=sT[:, k, :], start=(k == 0), stop=(k == 7), R=["wblk", "sT"], W=["ps0"])
                mk.op("dve", "tensor_tensor", out=MODC[:, m4, :, :],
                      in0=PS[0][:, 0:16].rearrange("p (c j) -> p c j", j=2),
                      in1=bmc[:, m * 8:(m + 1) * 8].unsqueeze(2).to_broadcast([128, 8, 2]), op=ALU.add,
                      R=["ps0", "bmc"], W=["MODC"])
                if m in (1, 4):
                    mk.op("dve", "tensor_scalar_add", out=MODC[:, m4, :, :], in0=MODC[:, m4, :, :], scalar1=1.0,
                          R=["MODC"], W=["MODC"])
            else:
                mi = 0 if m == 2 else 1
                for cb in range(2):
                    for k in range(8):
                        mk.op("pe", "matmul", PS[1][0:2, :], lhsT=sT[:, k, :], rhs=wblk[:, k, cb * 512:(cb + 1) * 512],
                              start=(k == 0), stop=False, R=["wblk", "sT"], W=["ps1"])
                    mk.op("pe", "matmul", PS[1][0:2, :], lhsT=sel[0:1, 0:2],
                          rhs=bmr[0:1, m * 1024 + cb * 512: m * 1024 + (cb + 1) * 512], start=False, stop=True,
                          R=["bmr", "cst"], W=["ps1"])
                    mk.op("dve", "tensor_copy", out=GR[0:2, mi, cb * 512:(cb + 1) * 512], in_=PS[1][0:2, :],
                          R=["ps1"], W=["GR"])
        for j in range(2):
            for mi in range(2):
                for cb in range(2):
                    mk.op("pe", "matmul", PS[1][:, :], lhsT=sel[0:2, j * 128:(j + 1) * 128],
                          rhs=GR[0:2, mi, cb * 512:(cb + 1) * 512], start=True, stop=True, R=["GR", "cst"], W=["ps1"])
                    mk.op("act", "activation", out=GB[:, j, mi, cb * 512:(cb + 1) * 512], in_=PS[1][:, :], func=AF.Copy,
                          R=["ps1"], W=["GB"])
        mk.barrier()
        P.close()

    def make_norm(P, nbuf=2):
        st = dict(junk=P.sb([128, D], BF16, "junk"), ss=[P.sb([128, 1], F32, "ss") for _ in range(nbuf)],
                  xn=[P.sb([128, D], BF16, "xn") for _ in range(nbuf)],
                  tmp=[P.sb([128, 8, 128], F32, "tmp") for _ in range(nbuf)], i=0, nbuf=nbuf)

        def norm(xt_ap, xt_key, j, msc, msh, h_out, h_key):
            i = st["i"] % st["nbuf"]
            st["i"] += 1
            ss, xn, tmp = st["ss"][i], st["xn"][i], st["tmp"][i]
            mk.op("act", "activation", out=st["junk"][:], in_=xt_ap, func=AF.Square, accum_out=ss[:],
                  R=[xt_key], W=["junk", f"ss{i}"])
            mk.op("act", "activation", out=ss[:], in_=ss[:], func=AF.Sqrt, scale=1.0 / D, bias=EPS,
                  R=[f"ss{i}"], W=[f"ss{i}"])
            mk.op("dve", "reciprocal", out=ss[:], in_=ss[:], R=[f"ss{i}"], W=[f"ss{i}"])
            mk.op("dve", "tensor_scalar", out=xn[:], in0=xt_ap, scalar1=ss[:, 0:1], scalar2=None, op0=ALU.mult,
                  R=[xt_key, f"ss{i}"], W=[f"xn{i}"])
            for c in range(8):
                mk.op("pe", "transpose", out=PQ[i][:, c * 128:(c + 1) * 128], in_=xn[:, c * 128:(c + 1) * 128],
                      identity=identb[:], R=[f"xn{i}", "identb"], W=[f"pq{i}"])
            mk.op("dve", "tensor_tensor", out=tmp[:], in0=PQ[i][:, :].rearrange("p (c n) -> p c n", c=8),
                  in1=MODC[:, msc, :, j:j + 1].to_broadcast([128, 8, 128]), op=ALU.mult,
                  R=[f"pq{i}", "MODC"], W=[f"ntmp{i}"])
            mk.op("pool", "tensor_tensor", out=h_out, in0=tmp[:],
                  in1=MODC[:, msh, :, j:j + 1].to_broadcast([128, 8, 128]), op=ALU.add,
                  R=[f"ntmp{i}", "MODC"], W=[h_key])
        return norm

    def inproj_phase(l):
        P = Pool(nc, f"ip{l}")
        hT = P.sb([128, 8, T], BF16, "hT")
        xt = [P.sb([128, D], F32, "xt") for _ in range(2)]
        norm = make_norm(P)
        for t in range(NT):
            i = t % 2
            mk.dma("sp", xt[i][:], XR[t * 128:(t + 1) * 128, :], R=[f"XR{t}"], W=[f"xt{i}"])
            norm(xt[i][:], f"xt{i}", cond_of(t), 1, 0, hT[:, :, t * 128:(t + 1) * 128], f"hT{t}")
        hkeys = [f"hT{t}" for t in range(NT)]
        wf = [P.sb([128, 8, 512], F32, "wf")] * 2
        wb = [P.sb([128, 8, 512], BF16, "wb") for _ in range(2)]
        stg = [P.sb([128, T], F32, "stg")] * 2
        nblk = 0
        tblocks = [(i * 512, min(512, T - i * 512)) for i in range(9)]
        for cb in range(4):
            i = nblk % 2
            nblk += 1
            mk.dma("sp", wf[i][:], w_fm[l].rearrange("(c p) f -> p c f", p=128)[:, :, cb * 512:(cb + 1) * 512],
                   W=["wf"])
            mk.ev(wb[i][:], wf[i][:], R=["wf"], W=[f"wb{i}"])
            for sub in range(4):
                fc = cb * 4 + sub
                si = fc % 2
                for bi, (t0, tw) in enumerate(tblocks):
                    pb = bi % 2
                    for k in range(8):
                        mk.op("pe", "matmul", PS[pb][:, 0:tw], lhsT=wb[i][:, k, sub * 128:(sub + 1) * 128],
                              rhs=hT[:, k, t0:t0 + tw], start=(k == 0), stop=(k == 7),
                              R=[f"wb{i}"] + hkeys[t0 // 128:(t0 + tw) // 128], W=[f"ps{pb}"])
                    mk.ev(stg[si][:, t0:t0 + tw], PS[pb][:, 0:tw], R=[f"ps{pb}"], W=["stg"])
                mk.dma("pool", FM[fc], stg[si][:], R=["stg"], W=[f"FM{fc}"])
        cblocks = [(i * 512, 512) for i in range(4)] + [(2048, TMW - 2048)] + [(TMW + i * 512, 512) for i in range(8)]
        stt = [P.sb([128, 4, 512], F32, "stt") for _ in range(2)]
        stb = [P.sb([128, 4, 512], BF16, "stb") for _ in range(2)]
        tgroups = [(g * 4, min(4, NT - g * 4)) for g in range(9)]
        ns = 0
        for (c0, cw) in cblocks:
            i = nblk % 2
            nblk += 1
            mk.dma("sp", wf[i][:, :, 0:cw], w_tm[l].rearrange("(c p) f -> p c f", p=128)[:, :, c0:c0 + cw], W=["wf"])
            mk.ev(wb[i][:, :, 0:cw], wf[i][:, :, 0:cw], R=["wf"], W=[f"wb{i}"])
            is_mg = c0 >= TMW
            for (g0, gn) in tgroups:
                si = ns % 2
                ns += 1
                for tt in range(gn):
                    t = g0 + tt
                    pb = 2 + (t % 2)
                    for k in range(8):
                        mk.op("pe", "matmul", PS[pb][:, 0:cw], lhsT=hT[:, k, t * 128:(t + 1) * 128],
                              rhs=wb[i][:, k, 0:cw], start=(k == 0), stop=(k == 7), R=[f"wb{i}", f"hT{t}"], W=[f"ps{pb}"])
                    if is_mg:
                        mk.ev(stb[si][:, tt, 0:cw], PS[pb][:, 0:cw], R=[f"ps{pb}"], W=[f"stb{si}"], func=AF.Sigmoid)
                    else:
                        mk.ev(stt[si][:, tt, 0:cw], PS[pb][:, 0:cw], R=[f"ps{pb}"], W=[f"stt{si}"])
                if is_mg:
                    mk.dma("pool", MG[g0 * 128:(g0 + gn) * 128, c0 - TMW:c0 - TMW + cw].rearrange("(t p) c -> p t c", p=128),
                           stb[si][:, 0:gn, 0:cw], R=[f"stb{si}"], W=["MG"])
                else:
                    mk.dma("pool", TM[g0 * 128:(g0 + gn) * 128, c0:c0 + cw].rearrange("(t p) c -> p t c", p=128),
                           stt[si][:, 0:gn, 0:cw], R=[f"stt{si}"], W=["TM"])
        mk.barrier()
        P.close()

    SEGS = [(0, 256), (256, T)]

    def conv_fm(u, src, w4, bcol, keyu, keysrc, wkeys):
        for (s0, e) in SEGS:
            mk.op("act", "activation", out=u[:, s0:e], in_=src[:, s0:e], func=AF.Identity, scale=w4[:, 2:3], bias=bcol,
                  R=[keysrc] + wkeys, W=[keyu])
            for k, sh in ((0, -2), (1, -1), (3, 1)):
                if sh < 0:
                    o, i_ = u[:, s0 - sh:e], src[:, s0:e + sh]
                else:
                    o, i_ = u[:, s0:e - sh], src[:, s0 + sh:e]
                mk.op("dve", "scalar_tensor_tensor", out=o, in0=i_, scalar=w4[:, k:k + 1], in1=o, op0=ALU.mult,
                      op1=ALU.add, R=[keysrc, keyu] + wkeys, W=[keyu])

    def mixer_a(l):
        P = Pool(nc, f"ma{l}")
        cw = P.sb([128, 2, 4], F32, "cw")
        cb = P.sb([128, 2], F32, "cb")
        gwf = P.sb([128, 2, 2, 2, 128], F32, "gwf")
        gwb = P.sb([128, 2, 2, 2, 128], BF16, "gwb")
        gb = P.sb([128, 2, 2, 2], F32, "gb")
        lam = P.sb([128, 2, 2], F32, "lam")
        c1 = P.sb([128, 2, 2], F32, "c1")
        mk.dma("sp", cw[:], a_cw[l], W=["a_cw"])
        mk.dma("sp", cb[:], a_cb[l], W=["a_cb"])
        mk.dma("sp", gwf[:], a_gw[l], W=["a_gwf"])
        mk.dma("sp", gb[:], a_gb[l], W=["a_gb"])
        mk.dma("sp", lam[:], a_lam[l], W=["a_lam"])
        mk.op("dve", "tensor_copy", out=gwb[:], in_=gwf[:], R=["a_gwf"], W=["a_gwb"])
        mk.op("act", "activation", out=c1[:], in_=lam[:], func=AF.Exp, scale=-1.0, R=["a_lam"], W=["a_c1"])
        mk.op("act", "activation", out=c1[:], in_=c1[:], func=AF.Ln, bias=1.0, R=["a_c1"], W=["a_c1"])
        mk.op("dve", "tensor_scalar", out=c1[:], in0=c1[:], scalar1=-8.0, scalar2=None, op0=ALU.mult, R=["a_c1"], W=["a_c1"])
        ax = P.sb([128, T], F32, "ax")
        ag = P.sb([128, T], F32, "ag")
        u = P.sb([128, T], F32, "u")
        ub = P.sb([128, T], BF16, "ub")
        aa = P.sb([128, T], F32, "aa")
        bt = P.sb([128, T], F32, "bt")
        hf = P.sb([128, T], F32, "hf")
        hb = P.sb([128, T], F32, "hb")
        r = [P.sb([128, 512], F32, "r") for _ in range(2)]
        gi = [P.sb([128, 512], F32, "gi") for _ in range(2)]
        yb = P.sb([128, T], BF16, "yb")
        tblocks = [(i * 512, min(512, T - i * 512)) for i in range(9)]
        for c in range(2):
            mk.dma("sp", ax[:], FM[FM_OFF["a_x"] + c], R=[f"FM{FM_OFF['a_x'] + c}"], W=["ax"])
            mk.dma("sp", ag[:], FM[FM_OFF["a_g"] + c], R=[f"FM{FM_OFF['a_g'] + c}"], W=["ag"])
            conv_fm(u, ax, cw[:, c, :], cb[:, c:c + 1], "u", "ax", ["a_cw", "a_cb"])
            mk.op("pool", "tensor_copy", out=ub[:], in_=u[:], R=["u"], W=["ub"])
            for d in range(2):
                for bi, (t0, tw) in enumerate(tblocks):
                    i = bi % 2
                    mk.op("pe", "matmul", PS[i][:, 0:tw], lhsT=gwb[:, d, 0, c, :], rhs=ub[:, t0:t0 + tw], start=True,
                          stop=True, R=["a_gwb", "ub"], W=[f"ps{i}"])
                    mk.op("pe", "matmul", PS[2 + i][:, 0:tw], lhsT=gwb[:, d, 1, c, :], rhs=ub[:, t0:t0 + tw], start=True,
                          stop=True, R=["a_gwb", "ub"], W=[f"ps{2 + i}"])
                    mk.op("act", "activation", out=r[i][:, 0:tw], in_=PS[i][:, 0:tw], func=AF.Sigmoid,
                          bias=gb[:, d, 0, c:c + 1], R=[f"ps{i}", "a_gb"], W=[f"r{i}"])
                    mk.op("act", "activation", out=gi[i][:, 0:tw], in_=PS[2 + i][:, 0:tw], func=AF.Sigmoid,
                          bias=gb[:, d, 1, c:c + 1], R=[f"ps{2 + i}", "a_gb"], W=[f"gi{i}"])
                    mk.op("act", "activation", out=aa[:, t0:t0 + tw], in_=r[i][:, 0:tw], func=AF.Exp,
                          scale=c1[:, d, c:c + 1], R=[f"r{i}", "a_c1"], W=["aa"])
                    mk.op("dve", "tensor_tensor", out=r[i][:, 0:tw], in0=aa[:, t0:t0 + tw], in1=aa[:, t0:t0 + tw],
                          op=ALU.mult, R=["aa", f"r{i}"], W=[f"r{i}"])
                    mk.op("dve", "tensor_scalar", out=r[i][:, 0:tw], in0=r[i][:, 0:tw], scalar1=-1.0, scalar2=1.0,
                          op0=ALU.mult, op1=ALU.add, R=[f"r{i}"], W=[f"r{i}"])
                    mk.op("act", "activation", out=r[i][:, 0:tw], in_=r[i][:, 0:tw], func=AF.Sqrt, R=[f"r{i}"], W=[f"r{i}"])
                    mk.op("dve", "tensor_tensor", out=gi[i][:, 0:tw], in0=gi[i][:, 0:tw], in1=r[i][:, 0:tw], op=ALU.mult,
                          R=[f"gi{i}", f"r{i}"], W=[f"gi{i}"])
                    mk.op("pool", "tensor_tensor", out=bt[:, t0:t0 + tw], in0=gi[i][:, 0:tw], in1=u[:, t0:t0 + tw],
                          op=ALU.mult, R=[f"gi{i}", "u"], W=["bt"])
                if d == 0:
                    mk.op("dve", "tensor_tensor_scan", out=hf[:, :], data0=aa[:, :], data1=bt[:, :], initial=0.0,
                          op0=ALU.mult, op1=ALU.add, R=["aa", "bt"], W=["hf"])
                else:
                    mk.op("dve", "tensor_tensor_scan", out=hb[:, 0:256][:, ::-1], data0=aa[:, 0:256][:, ::-1],
                          data1=bt[:, 0:256][:, ::-1], initial=0.0, op0=ALU.mult, op1=ALU.add, R=["aa", "bt"], W=["hb"])
                    mk.op("dve", "tensor_tensor_scan", out=hb[:, 256:T][:, ::-1], data0=aa[:, 256:T][:, ::-1],
                          data1=bt[:, 256:T][:, ::-1], initial=hb[:, 0:1], op0=ALU.mult, op1=ALU.add,
                          R=["aa", "bt", "hb"], W=["hb"])
            mk.op("act", "activation", out=ag[:], in_=ag[:], func=AF.Gelu, R=["ag"], W=["ag"])
            mk.op("dve", "tensor_tensor", out=hf[:], in0=hf[:], in1=hb[:], op=ALU.add, R=["hf", "hb"], W=["hf"])
            mk.op("dve", "tensor_tensor", out=yb[:], in0=hf[:], in1=ag[:], op=ALU.mult, R=["hf", "ag"], W=["yb"])
            mk.dma("pool", YT[0, c], yb[:], R=["yb"], W=[f"YT0{c}"])
        mk.barrier()
        P.close()

    def order_of(dr):
        return list(range(NT)) if dr == 0 else [1, 0] + list(range(NT - 1, 1, -1))

    def chunk_core(it, nsub, dr, QT, KT, QIT, KHs, V, vw, Gc, S, Sbf, maskD, maskkey, rkeys, PT, sk):
        pi = it % 2
        L = 128 // nsub
        okeys = ["ps2", "ps3"]
        Oh = [PS[2 + hh][:, 0:2 * vw].rearrange("p (c e) -> p c e", c=2) for hh in range(2)]
        if not DBG["core"]:
            return okeys, Oh
        for h in DBG["heads"]:
            c, hh = h // 2, h % 2
            rs = slice(hh * 64, (hh + 1) * 64)
            mk.op("pe", "matmul", PS[hh][:, c * 128:(c + 1) * 128], lhsT=KT[c][rs, :], rhs=QT[c][rs, :], start=True,
                  stop=True, R=rkeys, W=[f"ps{hh}"])
        PTv = PT[pi][:].rearrange("p (c x n) -> p c x n", c=2, x=2)
        Mv = maskD.rearrange("p (c x n) -> p c x n", c=2, x=2)
        for hh in range(2):
            mk.op("dve", "tensor_tensor", out=PTv[:, :, hh, :], in0=PS[hh][:, 0:256].rearrange("p (c n) -> p c n", c=2),
                  in1=Mv[:, :, hh, :], op=ALU.mult, R=[f"ps{hh}", maskkey], W=[f"PT{pi}h{hh}"])
        ptk = [f"PT{pi}h0", f"PT{pi}h1"]
        subs = list(range(nsub)) if dr == 0 else list(range(nsub - 1, -1, -1))
        KVp = PS[4][:, 0:2 * vw].rearrange("p (c e) -> p c e", c=2)
        for s in subs:
            rows = slice(s * L, (s + 1) * L)
            for h in DBG["heads"]:
                c, hh = h // 2, h % 2
                rs = slice(hh * 64, (hh + 1) * 64)
                mk.op("pe", "matmul", Oh[hh][rows, c, :], lhsT=PT[pi][:, h * 128 + s * L:h * 128 + (s + 1) * L],
                      rhs=V[:, h, :], start=True, stop=False, R=[ptk[hh]] + rkeys, W=[okeys[hh]])
                mk.op("pe", "matmul", Oh[hh][rows, c, :], lhsT=QIT[c][rs, rows], rhs=Sbf[c][rs, :], start=False, stop=True,
                      R=rkeys + [f"{sk}Sbf{c}"], W=[okeys[hh]])
            for h in DBG["heads"]:
                c, hh = h // 2, h % 2
                mk.op("pe", "matmul", KVp[hh * 64:(hh + 1) * 64, c, :], lhsT=KHs(s)[:, h * 64:(h + 1) * 64],
                      rhs=V[:, h, :], start=True, stop=True, R=rkeys, W=["ps4kv"])
            for c in range(2):
                mk.op("dve", "scalar_tensor_tensor", out=S[c][:], in0=S[c][:], scalar=Gc[:, c, s:s + 1], in1=KVp[:, c, :],
                      op0=ALU.mult, op1=ALU.add, R=[f"{sk}S{c}", "ps4kv"] + rkeys, W=[f"{sk}S{c}"])
                mk.op("act", "activation", out=Sbf[c][:], in_=S[c][:], func=AF.Copy, R=[f"{sk}S{c}"], W=[f"{sk}Sbf{c}"])
        return okeys, Oh

    def hview(ap256, hh):
        return ap256.rearrange("p (c x e) -> p c x e", c=2, x=2)[:, :, hh, :]

    def make_finalize(P, n, yTb):
        st = dict(i=0)
        cent = [P.sb([128, 4, 64], F32, "cent") for _ in range(2)]
        sq = P.sb([128, 4, 64], F32, "sq")
        mm = [P.sb([128, 4], F32, "mm") for _ in range(2)]
        vv = [P.sb([128, 4], F32, "vv") for _ in range(2)]
        yy = [P.sb([128, 256], BF16, "yy") for _ in range(2)]

        def fin(tot, totkey, center, gate, gatekey, t):
            i = st["i"] % 2
            st["i"] += 1
            tv = tot.rearrange("p (h e) -> p h e", h=4)
            tk = list(totkey) if isinstance(totkey, (list, tuple)) else [totkey]
            src, skeys = tv, tk
            if center:
                mk.op("dve", "tensor_reduce", out=mm[i][:], in_=tv, axis=AX.X, op=ALU.add, R=tk, W=[f"fmm{i}"])
                mk.op("dve", "tensor_scalar", out=mm[i][:], in0=mm[i][:], scalar1=-1.0 / 64, scalar2=None, op0=ALU.mult,
                      R=[f"fmm{i}"], W=[f"fmm{i}"])
                mk.op("dve", "tensor_tensor", out=cent[i][:], in0=tv, in1=mm[i][:].unsqueeze(2).to_broadcast([128, 4, 64]),
                      op=ALU.add, R=tk + [f"fmm{i}"], W=[f"fcent{i}"])
                src, skeys = cent[i][:], [f"fcent{i}"]
            mk.op("pool", "tensor_tensor", out=sq[:], in0=src, in1=src, op=ALU.mult, R=skeys, W=["fsq"])
            mk.op("dve", "tensor_reduce", out=vv[i][:], in_=sq[:], axis=AX.X, op=ALU.add, R=["fsq"], W=[f"fvv{i}"])
            mk.op("act", "activation", out=vv[i][:], in_=vv[i][:], func=AF.Sqrt, scale=1.0 / 64, bias=EPS,
                  R=[f"fvv{i}"], W=[f"fvv{i}"])
            mk.op("dve", "reciprocal", out=vv[i][:], in_=vv[i][:], R=[f"fvv{i}"], W=[f"fvv{i}"])
            mk.op("dve", "tensor_tensor", out=cent[i][:], in0=src, in1=vv[i][:].unsqueeze(2).to_broadcast([128, 4, 64]),
                  op=ALU.mult, R=skeys + [f"fvv{i}"], W=[f"fcent{i}"])
            mk.op("dve", "tensor_tensor", out=yy[i][:], in0=cent[i][:].rearrange("p h e -> p (h e)"), in1=gate,
                  op=ALU.mult, R=[f"fcent{i}", gatekey], W=[f"fyy{i}"])
            for c in range(2):
                mk.op("pe", "transpose", out=PQ[1][:, (i * 2 + c) * 128:(i * 2 + c + 1) * 128],
                      in_=yy[i][:, c * 128:(c + 1) * 128], identity=identb[:], R=[f"fyy{i}", "identb"], W=[f"pq1f{i}"])
            mk.op("act", "activation", out=yTb[:, :, t * 128:(t + 1) * 128],
                  in_=PQ[1][:, i * 256:(i + 1) * 256].rearrange("p (c n) -> p c n", c=2), func=AF.Copy,
                  R=[f"pq1f{i}"], W=["yTb"])
        return fin

    def mixer_b(l):
        P = Pool(nc, f"mb{l}")
        QR = [P.sb([128, T], BF16, "QR") for _ in range(2)]
        KR = [P.sb([128, T], BF16, "KR") for _ in range(2)]
        thp = P.sb([128, 2, 2], F32, "thp")
        thh = P.sb([128, 2, 4], F32, "thh")
        mk.dma("sp", thp[:], b_thp[l], W=["thp"])
        mk.dma("sp", thh[:], b_thh[l], W=["thh"])
        for tt, key in ((thp, "thp"), (thh, "thh")):
            mk.op("act", "activation", out=tt[:], in_=tt[:], func=AF.Exp, scale=-1.0, R=[key], W=[key])
            mk.op("act", "activation", out=tt[:], in_=tt[:], func=AF.Ln, bias=1.0, R=[key], W=[key])
            mk.op("dve", "tensor_scalar", out=tt[:], in0=tt[:], scalar1=-1.0, scalar2=None, op0=ALU.mult, R=[key], W=[key])
        DM = P.sb([128, 2, 512], F32, "DM")
        QW = P.sb([128, 2, 2, 128], F32, "QW")
        KW = P.sb([128, 2, 4], F32, "KW")
        Gc = P.sb([128, 2, 2, 1], F32, "Gc")
        for dr in range(2):
            diff, msk, pos = (C("diffF"), C("maskF"), C("posF")) if dr == 0 else (C("diffB"), C("maskB"), C("posB"))
            for h in range(4):
                mk.op("act", "activation", out=DM[:, dr, h * 128:(h + 1) * 128], in_=diff, func=AF.Exp,
                      scale=thh[:, dr, h:h + 1], R=["cst", "thh"], W=["DM"])
                mk.op("dve", "tensor_tensor", out=DM[:, dr, h * 128:(h + 1) * 128], in0=DM[:, dr, h * 128:(h + 1) * 128],
                      in1=msk, op=ALU.mult, R=["DM", "cst"], W=["DM"])
                mk.op("act", "activation", out=KW[:, dr, h:h + 1], in_=C("kpos")[:, dr:dr + 1], func=AF.Exp,
                      scale=thh[:, dr, h:h + 1], R=["cst", "thh"], W=["KW"])
            for c in range(2):
                mk.op("act", "activation", out=QW[:, dr, c, :], in_=pos, func=AF.Exp, scale=thp[:, dr, c:c + 1],
                      R=["cst", "thp"], W=["QW"])
                mk.op("act", "activation", out=Gc[:, dr, c, :], in_=thp[:, dr, c:c + 1], func=AF.Exp, scale=128.0,
                      R=["thp"], W=["Gc"])
        segw = 1088
        f1 = [P.sb([128, segw], F32, "f1") for _ in range(2)]
        f2 = [P.sb([128, segw], F32, "f2") for _ in range(2)]
        rc = P.sb([128, T], F32, "rc")
        rsn = P.sb([128, T], F32, "rsn")
        mk.dma("sp", rc[:], ropeC_d, W=["rc"])
        mk.dma("sp", rsn[:], ropeS_d, W=["rsn"])
        n = 0
        for (dst, base, pbase, scale) in ((QR, "b_q", "b_qp", 1.0), (KR, "b_k", "b_kp", 0.125)):
            for c in range(2):
                for sg in range(4):
                    i = n % 2
                    n += 1
                    cs = slice(sg * segw, (sg + 1) * segw)
                    mk.dma("sp", f1[i][:], FM[FM_OFF[base] + c][:, cs], R=[f"FM{FM_OFF[base] + c}"], W=[f"f1{i}"])
                    mk.dma("sp", f2[i][:], FM[FM_OFF[pbase] + c][:, cs], R=[f"FM{FM_OFF[pbase] + c}"], W=[f"f2{i}"])
                    mk.op("dve", "tensor_tensor", out=f1[i][:], in0=f1[i][:], in1=rc[:, cs], op=ALU.mult,
                          R=[f"f1{i}", "rc"], W=[f"f1{i}"])
                    mk.op("pool", "tensor_tensor", out=f2[i][:], in0=f2[i][:], in1=rsn[:, cs], op=ALU.mult,
                          R=[f"f2{i}", "rsn"], W=[f"f2{i}"])
                    mk.op("dve", "tensor_tensor", out=f1[i][:], in0=f1[i][:], in1=f2[i][:], op=ALU.add,
                          R=[f"f1{i}", f"f2{i}"], W=[f"f1{i}"])
                    mk.op("act", "activation", out=dst[c][:, cs], in_=f1[i][:], func=AF.Copy, scale=scale,
                          R=[f"f1{i}"], W=[f"b{base}{c}"])
        rkeys = ["bb_q0", "bb_q1", "bb_k0", "bb_k1"]
        OF = P.sb([128, NT, 256], F32, "OF")
        yTb = P.sb([128, 2, T], BF16, "yTb")
        fin = make_finalize(P, 1, yTb)
        PT = [P.sb([128, 512], BF16, "PT") for _ in range(2)]
        QIT = [[P.sb([128, 128], BF16, "QIT") for _ in range(2)] for _ in range(2)]
        KH = [P.sb([128, 256], BF16, "KH") for _ in range(2)]
        Vf = [P.sb([128, 512], F32, "Vf") for _ in range(2)]
        Vb = [P.sb([128, 4, 64], BF16, "Vb") for _ in range(2)]
        gt = [P.sb([128, 256], F32, "gt") for _ in range(2)]
        tot = [P.sb([128, 256], F32, "tot") for _ in range(2)]
        S = [P.sb([128, 64], F32, "S") for _ in range(2)]
        Sbf = [P.sb([128, 64], BF16, "Sbf") for _ in range(2)]
        it = 0
        for dr in range(2):
            for c in range(2):
                mk.op("pool", "memset", S[c][:], 0.0, W=[f"bS{c}"])
                mk.op("pool", "memset", Sbf[c][:], 0.0, W=[f"bSbf{c}"])
            for t in order_of(dr):
                if DBG["maxit"] is not None and it >= DBG["maxit"]:
                    break
                i = it % 2
                cols = slice(t * 128, (t + 1) * 128)
                mk.dma("sp", Vf[i][:], TM[t * 128:(t + 1) * 128, 0:512], R=["TM"], W=[f"bVf{i}"])
                mk.op("pool", "tensor_copy", out=Vb[i][:], in_=Vf[i][:, 0:256].rearrange("p (h e) -> p h e", h=4),
                      R=[f"bVf{i}"], W=[f"bVb{i}"])
                for c in range(2):
                    mk.op("pool", "tensor_tensor", out=QIT[i][c][:], in0=QR[c][:, cols], in1=QW[:, dr, c, :], op=ALU.mult,
                          R=[f"bb_q{c}", "QW"], W=[f"bQIT{i}"])
                    mk.op("pe", "transpose", out=PQ[0][:, (i * 2 + c) * 128:(i * 2 + c + 1) * 128], in_=KR[c][:, cols],
                          identity=identb[:], R=[f"bb_k{c}", "identb"], W=[f"pq0k{i}"])
                for h in range(4):
                    mk.op("act", "activation", out=KH[i][:, h * 64:(h + 1) * 64],
                          in_=PQ[0][:, i * 256 + h * 64:i * 256 + (h + 1) * 64], func=AF.Identity, scale=KW[:, dr, h:h + 1],
                          R=[f"pq0k{i}", "KW"], W=[f"bKH{i}"])
                okeys, Oh = chunk_core(it, 1, dr, [QR[0][:, cols], QR[1][:, cols]], [KR[0][:, cols], KR[1][:, cols]],
                                       [QIT[i][0], QIT[i][1]], (lambda s_, kh=KH[i]: kh), Vb[i], 64, Gc[:, dr], S, Sbf,
                                       DM[:, dr, :], "DM", rkeys + [f"bQIT{i}", f"bKH{i}", f"bVb{i}"], PT, "b")
                if dr == 0:
                    for hh in range(2):
                        mk.op("act", "activation", out=hview(OF[:, t, :], hh), in_=Oh[hh], func=AF.Copy, R=[okeys[hh]],
                              W=[f"bOF{t}h{hh}"])
                else:
                    for hh in range(2):
                        mk.op("dve", "tensor_tensor", out=hview(tot[i][:], hh), in0=Oh[hh], in1=hview(OF[:, t, :], hh),
                              op=ALU.add, R=[okeys[hh], f"bOF{t}h{hh}"], W=[f"btot{i}h{hh}"])
                    mk.op("act", "activation", out=gt[i][:], in_=Vf[i][:, 256:512], func=AF.Silu, R=[f"bVf{i}"],
                          W=[f"bgt{i}"])
                    fin(tot[i][:], [f"btot{i}h0", f"btot{i}h1"], True, gt[i][:], f"bgt{i}", t)
                it += 1
        for c in range(2):
            mk.dma("pool", YT[1, c], yTb[:, c, :], R=["yTb"], W=[f"YT1{c}"])
        mk.barrier()
        P.close()

    def mixer_c(l):
        P = Pool(nc, f"mc{l}")
        QC = [P.sb([128, T], BF16, "QC") for _ in range(2)]
        KC = [P.sb([128, T], BF16, "KC") for _ in range(2)]
        cw = P.sb([128, 4, 4], F32, "cw")
        cb = P.sb([128, 4], F32, "cb")
        gbias = P.sb([128, 16], F32, "gbias")
        mk.dma("sp", cw[:], c_cw[l], W=["c_cw"])
        mk.dma("sp", cb[:], c_cb[l], W=["c_cb"])
        mk.dma("sp", gbias[:], c_gb[l], W=["c_gb"])
        src = P.sb([128, T], F32, "src")
        u = P.sb([128, T], F32, "u")
        for ch in range(4):
            fc = FM_OFF["c_q"] + ch
            mk.dma("sp", src[:], FM[fc], R=[f"FM{fc}"], W=["csrc"])
            conv_fm(u, src, cw[:, ch, :], cb[:, ch:ch + 1], "cu", "csrc", ["c_cw", "c_cb"])
            dst = QC[ch] if ch < 2 else KC[ch - 2]
            mk.op("act", "activation", out=u[:], in_=u[:], func=AF.Silu, R=["cu"], W=["cu"])
            mk.op("dve", "tensor_scalar", out=dst[:], in0=u[:], scalar1=(1.0 if ch < 2 else 0.125), scalar2=None,
                  op0=ALU.mult, R=["cu"], W=[f"cqk{ch}"])
        Z = P.sb([128, NT, 16], F32, "Z")
        LFN = P.sb([128, NT, 16], F32, "LFN")
        mk.dma("sp", Z[:], TM[:, TM_OFF["c_gates"]:TM_OFF["c_gates"] + 16].rearrange("(t p) g -> p t g", p=128),
               R=["TM"], W=["cZ"])
        mk.op("dve", "tensor_tensor", out=Z[:], in0=Z[:], in1=gbias[:].unsqueeze(1).to_broadcast([128, NT, 16]),
              op=ALU.add, R=["cZ", "c_gb"], W=["cZ"])
        mk.op("act", "activation", out=LFN[:], in_=Z[:], func=AF.Exp, scale=-1.0, R=["cZ"], W=["cLFN"])
        mk.op("act", "activation", out=LFN[:], in_=LFN[:], func=AF.Ln, bias=1.0, R=["cLFN"], W=["cLFN"])
        mk.op("dve", "tensor_scalar", out=LFN[:], in0=LFN[:], scalar1=-1.0, scalar2=None, op0=ALU.mult, R=["cLFN"],
              W=["cLFN"])
        rkeys = ["cqk0", "cqk1", "cqk2", "cqk3"]
        OF = P.sb([128, NT, 256], F32, "OF")
        yTb = P.sb([128, 2, T], BF16, "yTb")
        fin = make_finalize(P, 2, yTb)
        PT = [P.sb([128, 512], BF16, "PT") for _ in range(2)]
        QIT = [[P.sb([128, 128], BF16, "QIT") for _ in range(2)] for _ in range(2)]
        KH = [P.sb([128, 256], BF16, "KH") for _ in range(2)]
        Vf = [P.sb([128, 512], F32, "Vf") for _ in range(2)]
        Vb = [P.sb([128, 4, 65], BF16, "Vb") for _ in range(2)]
        gt = [P.sb([128, 256], F32, "gt") for _ in range(2)]
        tot = [P.sb([128, 256], F32, "tot") for _ in range(2)]
        S = [P.sb([128, 65], F32, "S") for _ in range(2)]
        Sbf = [P.sb([128, 65], BF16, "Sbf") for _ in range(2)]
        Bm4 = [P.sb([128, 4, 128], F32, "Bm4") for _ in range(2)]
        tmp4 = [P.sb([128, 512], F32, "tmp4") for _ in range(2)]
        Dm4 = [P.sb([128, 512], F32, "Dm4") for _ in range(2)]
        EB4 = [P.sb([128, 512], F32, "EB4") for _ in range(2)]
        lmb = [P.sb([128, 4], F32, "lmb") for _ in range(2)]
        kw = [P.sb([128, 4], F32, "kw") for _ in range(2)]
        Gc = [P.sb([128, 2, 1], F32, "Gc") for _ in range(2)]
        rden = [P.sb([128, 4], F32, "rden") for _ in range(2)]
        hid = [P.sb([128, 4, 64], F32, "hid") for _ in range(2)]
        for i in range(2):
            mk.op("pool", "memset", Vb[i][:], 1.0, W=[f"cVb{i}"])
        ones = C("ones")
        it = 0
        for dr in range(2):
            tri = C("triF") if dr == 0 else C("triB")
            neg4 = C("negF4") if dr == 0 else C("negB4")
            e = 127 if dr == 0 else 0
            for c in range(2):
                mk.op("pool", "memset", S[c][:], 0.0, W=[f"cS{c}"])
                mk.op("pool", "memset", Sbf[c][:], 0.0, W=[f"cSbf{c}"])
            for t in order_of(dr):
                i = it % 2
                cols = slice(t * 128, (t + 1) * 128)
                li = Z[:, t, dr * 8:dr * 8 + 4]
                lf = LFN[:, t, dr * 8 + 4:dr * 8 + 8]
                mk.dma("sp", Vf[i][:], TM[t * 128:(t + 1) * 128, 512:1024], R=["TM"], W=[f"cVf{i}"])
                mk.op("pool", "tensor_copy", out=Vb[i][:, :, 0:64], in_=Vf[i][:, 0:256].rearrange("p (h e) -> p h e", h=4),
                      R=[f"cVf{i}"], W=[f"cVb{i}"])
                mk.op("dve", "tensor_tensor", out=Bm4[i][:], in0=tri.unsqueeze(1).to_broadcast([128, 4, 128]),
                      in1=lf.unsqueeze(2).to_broadcast([128, 4, 128]), op=ALU.mult, R=["cst", "cLFN"], W=[f"cBm{i}"])
                mk.op("pe", "matmul", PS[5][:, :], lhsT=ones, rhs=Bm4[i][:].rearrange("p h n -> p (h n)"), start=True,
                      stop=True, R=["cst", f"cBm{i}"], W=["ps5"])
                mk.op("pe", "matmul", PS[4][:, 256:260], lhsT=tri, rhs=lf, start=True, stop=True, R=["cst", "cLFN"],
                      W=["ps4b"])
                mk.op("dve", "tensor_tensor", out=lmb[i][:], in0=li, in1=PS[4][:, 256:260], op=ALU.subtract,
                      R=["cZ", "ps4b"], W=[f"clmb{i}"])
                mk.op("dve", "tensor_tensor", out=tmp4[i][:], in0=PS[5][:, :], in1=neg4, op=ALU.add, R=["ps5", "cst"],
                      W=[f"ctmp{i}"])
                for h in range(4):
                    mk.op("act", "activation", out=Dm4[i][:, h * 128:(h + 1) * 128], in_=tmp4[i][:, h * 128:(h + 1) * 128],
                          func=AF.Exp, bias=lmb[i][:, h:h + 1], R=[f"ctmp{i}", f"clmb{i}"], W=[f"cDm{i}"])
                mk.op("act", "activation", out=EB4[i][:], in_=PS[5][:, :], func=AF.Exp, R=["ps5"], W=[f"cEB{i}"])
                bend = PS[5][:, :].rearrange("p (h n) -> p h n", h=4)[:, :, e]
                mk.op("dve", "tensor_tensor", out=kw[i][:], in0=lmb[i][:], in1=bend, op=ALU.add, R=[f"clmb{i}", "ps5"],
                      W=[f"ckw{i}"])
                mk.op("act", "activation", out=kw[i][:], in_=kw[i][:], func=AF.Exp, R=[f"ckw{i}"], W=[f"ckw{i}"])
                for h in range(4):
                    c, hh = h // 2, h % 2
                    rs = slice(hh * 64, (hh + 1) * 64)
                    mk.op("pool", "tensor_copy", out=Gc[i][rs, c, :], in_=EB4[i][rs, h * 128 + e:h * 128 + e + 1],
                          R=[f"cEB{i}"], W=[f"cGc{i}"])
                    mk.op("pool", "tensor_tensor", out=QIT[i][c][rs, :], in0=QC[c][rs, cols],
                          in1=EB4[i][rs, h * 128:(h + 1) * 128], op=ALU.mult, R=[f"cqk{c}", f"cEB{i}"], W=[f"cQIT{i}"])
                for c in range(2):
                    mk.op("pe", "transpose", out=PQ[0][:, (i * 2 + c) * 128:(i * 2 + c + 1) * 128], in_=KC[c][:, cols],
                          identity=identb[:], R=[f"cqk{2 + c}", "identb"], W=[f"pq0k{i}"])
                for h in range(4):
                    mk.op("act", "activation", out=KH[i][:, h * 64:(h + 1) * 64],
                          in_=PQ[0][:, i * 256 + h * 64:i * 256 + (h + 1) * 64], func=AF.Identity, scale=kw[i][:, h:h + 1],
                          R=[f"pq0k{i}", f"ckw{i}"], W=[f"cKH{i}"])
                okeys, Oh = chunk_core(it, 1, dr, [QC[0][:, cols], QC[1][:, cols]], [KC[0][:, cols], KC[1][:, cols]],
                                       [QIT[i][0], QIT[i][1]], (lambda s_, kh=KH[i]: kh), Vb[i], 65, Gc[i], S, Sbf,
                                       Dm4[i][:], f"cDm{i}",
                                       rkeys + [f"cQIT{i}", f"cKH{i}", f"cVb{i}", f"cGc{i}"], PT, "c")
                rdv = rden[i][:].rearrange("p (c x) -> p c x", c=2)
                for hh in range(2):
                    mk.op("act", "activation", out=rdv[:, :, hh], in_=Oh[hh][:, :, 64], func=AF.Abs, R=[okeys[hh]],
                          W=[f"crden{i}"])
                mk.op("dve", "tensor_scalar_max", out=rden[i][:], in0=rden[i][:], scalar1=1.0, R=[f"crden{i}"],
                      W=[f"crden{i}"])
                mk.op("dve", "reciprocal", out=rden[i][:], in_=rden[i][:], R=[f"crden{i}"], W=[f"crden{i}"])
                if dr == 0:
                    for hh in range(2):
                        mk.op("dve", "tensor_tensor", out=hview(OF[:, t, :], hh), in0=Oh[hh][:, :, 0:64],
                              in1=rdv[:, :, hh:hh + 1].to_broadcast([128, 2, 64]), op=ALU.mult,
                              R=[okeys[hh], f"crden{i}"], W=[f"cOF{t}h{hh}"])
                else:
                    for hh in range(2):
                        mk.op("dve", "tensor_tensor", out=hview(hid[i][:].rearrange("p h e -> p (h e)"), hh),
                              in0=Oh[hh][:, :, 0:64], in1=rdv[:, :, hh:hh + 1].to_broadcast([128, 2, 64]), op=ALU.mult,
                              R=[okeys[hh], f"crden{i}"], W=[f"chid{i}h{hh}"])
                    mk.op("pool", "tensor_tensor", out=tot[i][:], in0=hid[i][:].rearrange("p h e -> p (h e)"),
                          in1=OF[:, t, :], op=ALU.add, R=[f"chid{i}h0", f"chid{i}h1", f"cOF{t}h0", f"cOF{t}h1"],
                          W=[f"ctot{i}"])
                    mk.op("act", "activation", out=gt[i][:], in_=Vf[i][:, 256:512], func=AF.Sigmoid, R=[f"cVf{i}"],
                          W=[f"cgt{i}"])
                    fin(tot[i][:], f"ctot{i}", True, gt[i][:], f"cgt{i}", t)
                it += 1
        for c in range(2):
            mk.dma("pool", YT[2, c], yTb[:, c, :], R=["yTb"], W=[f"YT2{c}"])
        mk.barrier()
        P.close()

    def mixer_d(l):
        P = Pool(nc, f"md{l}")
        LB = P.sb([128, 256], F32, "LB")
        OML = P.sb([128, 256], F32, "OML")
        if l == 0:
            use_lb = False
        else:
            use_lb = True
            dl = P.sb([128, 2, 256], F32, "dl")
            mk.dma("sp", dl[:], d_lbr, W=["dl"])
            mk.op("dve", "tensor_tensor", out=LB[:], in0=dl[:, 1, :], in1=dl[:, 0, :], op=ALU.subtract, R=["dl"], W=["LB"])
            mk.op("act", "activation", out=LB[:], in_=LB[:], func=AF.Sigmoid, R=["LB"], W=["LB"])
            mk.op("dve", "tensor_scalar", out=OML[:], in0=LB[:], scalar1=-1.0, scalar2=1.0, op0=ALU.mult, op1=ALU.add,
                  R=["LB"], W=["OML"])
        OF = P.sb([128, NT, 256], F32, "OF")
        yTb = P.sb([128, 2, T], BF16, "yTb")
        fin = make_finalize(P, 3, yTb)
        assert DNS == 4
        PT = [P.sb([128, 512], BF16, "PT") for _ in range(2)]
        X = [P.sb([128, 1280], F32, "X") for _ in range(2)]
        ff = [P.sb([128, 256], F32, "ff") for _ in range(2)]
        lf = [P.sb([128, 256], F32, "lf") for _ in range(2)]
        kk = [P.sb([128, 256], F32, "kk") for _ in range(2)]
        qs = [P.sb([128, 256], F32, "qs") for _ in range(2)]
        ee = [P.sb([128, 512], F32, "ee") for _ in range(2)]
        ek = [P.sb([128, 256], F32, "ek") for _ in range(2)]
        qk = [P.sb([128, 512], BF16, "qk") for _ in range(2)]
        KTs = [P.sb([128, 2, 128], BF16, "KTs") for _ in range(2)]
        QM = [[P.sb([128, 2, 5, 128], BF16, "QM") for _ in range(2)] for _ in range(2)]
        KH = [P.sb([128, DNS, 256], BF16, "KH") for _ in range(2)]
        Vb = [P.sb([128, 4, 64], BF16, "Vb") for _ in range(2)]
        gt = [P.sb([128, 256], F32, "gt") for _ in range(2)]
        tot = [P.sb([128, 256], F32, "tot") for _ in range(2)]
        red = [P.sb([128, 2, 256], F32, "red") for _ in range(2)]
        Gc = [P.sb([128, 2, DNS], F32, "Gc") for _ in range(2)]
        S = [P.sb([128, 64], F32, "S") for _ in range(2)]
        Sbf = [P.sb([128, 64], BF16, "Sbf") for _ in range(2)]
        qmask = C("qmask").rearrange("p (x s n) -> p x s n", x=2, s=5)
        it = 0
        for dr in range(2):
            blk = C("blkF") if dr == 0 else C("blkB")
            rem = C("aftF") if dr == 0 else C("befB")
            msk4 = C("mblkF4") if dr == 0 else C("mblkB4")
            zoff = 256 if dr == 0 else 512
            subs = list(range(DNS)) if dr == 0 else list(range(DNS - 1, -1, -1))
            for c in range(2):
                mk.op("pool", "memset", S[c][:], 0.0, W=[f"dS{c}"])
                mk.op("pool", "memset", Sbf[c][:], 0.0, W=[f"dSbf{c}"])
            for t in order_of(dr):
                i = it % 2
                mk.dma("sp", X[i][:], TM[t * 128:(t + 1) * 128, 1024:2304], R=["TM"], W=[f"dX{i}"])
                mk.op("act", "activation", out=ff[i][:], in_=X[i][:, zoff:zoff + 256], func=AF.Sigmoid, R=[f"dX{i}"],
                      W=[f"dff{i}"])
                if use_lb:
                    mk.op("dve", "tensor_tensor", out=ff[i][:], in0=ff[i][:], in1=OML[:], op=ALU.mult, R=[f"dff{i}", "OML"],
                          W=[f"dff{i}"])
                    mk.op("dve", "tensor_tensor", out=ff[i][:], in0=ff[i][:], in1=LB[:], op=ALU.add, R=[f"dff{i}", "LB"],
                          W=[f"dff{i}"])
                mk.op("act", "activation", out=lf[i][:], in_=ff[i][:], func=AF.Ln, R=[f"dff{i}"], W=[f"dlf{i}"])
                mk.op("pool", "tensor_scalar", out=kk[i][:], in0=ff[i][:], scalar1=-1.0, scalar2=1.0, op0=ALU.mult,
                      op1=ALU.add, R=[f"dff{i}"], W=[f"dkk{i}"])
                mk.op("pe", "matmul", PS[5][:, 0:256], lhsT=blk, rhs=lf[i][:], start=True, stop=True, R=["cst", f"dlf{i}"],
                      W=["ps5"])
                mk.op("pe", "matmul", PS[5][:, 256:512], lhsT=rem, rhs=lf[i][:], start=True, stop=True,
                      R=["cst", f"dlf{i}"], W=["ps5"])
                for c in range(2):
                    mk.op("pe", "matmul", PS[1][:, 384 + c * DNS:384 + (c + 1) * DNS], lhsT=lf[i][:, c * 128:(c + 1) * 128],
                          rhs=C("subm"), start=True, stop=True, R=["cst", f"dlf{i}"], W=["ps1g"])
                mk.op("act", "activation", out=Gc[i][:].rearrange("p c s -> p (c s)"), in_=PS[1][:, 384:384 + 2 * DNS],
                      func=AF.Exp, R=["ps1g"], W=[f"dGc{i}"])
                mk.op("act", "activation", out=ee[i][:], in_=PS[5][:, :], func=AF.Exp, R=["ps5"], W=[f"dee{i}"])
                mk.op("act", "activation", out=ek[i][:], in_=PS[5][:, 0:256], func=AF.Exp, scale=-1.0, R=["ps5"],
                      W=[f"dek{i}"])
                mk.op("act", "activation", out=qs[i][:], in_=X[i][:, 0:256], func=AF.Silu, R=[f"dX{i}"], W=[f"dqs{i}"])
                mk.op("dve", "tensor_tensor", out=qk[i][:, 0:256], in0=qs[i][:], in1=ee[i][:, 0:256], op=ALU.mult,
                      R=[f"dqs{i}", f"dee{i}"], W=[f"dqk{i}a"])
                mk.op("dve", "tensor_tensor", out=qk[i][:, 256:512], in0=kk[i][:], in1=ek[i][:], op=ALU.mult,
                      R=[f"dkk{i}", f"dek{i}"], W=[f"dqk{i}c"])
                for s_ in range(DNS):
                    mk.op("dve", "scalar_tensor_tensor", out=KH[i][:, s_, :], in0=kk[i][:], scalar=C("subm")[:, s_:s_ + 1],
                          in1=ee[i][:, 256:512], op0=ALU.mult, op1=ALU.mult, R=[f"dkk{i}", f"dee{i}", "cst"],
                          W=[f"dKH{i}s{s_}"])
                mk.op("pool", "tensor_copy", out=Vb[i][:], in_=X[i][:, 768:1024].rearrange("p (h e) -> p h e", h=4),
                      R=[f"dX{i}"], W=[f"dVb{i}"])
                for j in range(4):
                    mk.op("pe", "transpose", out=PQ[0][:, (i * 4 + j) * 128:(i * 4 + j + 1) * 128],
                          in_=qk[i][:, j * 128:(j + 1) * 128], identity=identb[:], R=[f"dqk{i}a", f"dqk{i}c", "identb"],
                          W=[f"pq0d{i}"])
                mk.op("act", "activation", out=KTs[i][:].rearrange("p c n -> p (c n)"),
                      in_=PQ[0][:, i * 512 + 256:i * 512 + 512], func=AF.Copy, R=[f"pq0d{i}"], W=[f"dKT{i}"])
                for c in range(2):
                    mk.op("dve", "tensor_tensor", out=QM[i][c][:].rearrange("p x s n -> p (x s) n"),
                          in0=PQ[0][:, i * 512 + c * 128:i * 512 + (c + 1) * 128].unsqueeze(1).to_broadcast([128, 10, 128]),
                          in1=qmask.rearrange("p x s n -> p (x s) n"), op=ALU.mult, R=[f"pq0d{i}", "cst"],
                          W=[f"dQM{i}{c}"])
                for h in range(4):
                    c, hh = h // 2, h % 2
                    mk.op("pe", "matmul", PS[0][:, h * 128:(h + 1) * 128], lhsT=KTs[i][:, c, :], rhs=QM[i][c][:, hh, 4, :],
                          start=True, stop=True, R=[f"dKT{i}", f"dQM{i}{c}"], W=["ps0"])
                mk.op("dve", "tensor_tensor", out=PT[i][:], in0=PS[0][:, :], in1=msk4, op=ALU.mult, R=["ps0", "cst"],
                      W=[f"dPT{i}"])
                for h in range(4):
                    mk.op("pe", "matmul", PS[1][:, h * 64:(h + 1) * 64], lhsT=PT[i][:, h * 128:(h + 1) * 128],
                          rhs=Vb[i][:, h, :], start=True, stop=True, R=[f"dPT{i}", f"dVb{i}"], W=["ps1i"])
                KVp = PS[1][:, 256:384].rearrange("p (c e) -> p c e", c=2)
                for s_ in subs:
                    bank = PS[2 + s_ // 2]
                    for h in range(4):
                        c, hh = h // 2, h % 2
                        col = ((s_ % 2) * 4 + h) * 64
                        mk.op("pe", "matmul", bank[:, col:col + 64], lhsT=QM[i][c][:, hh, s_, :], rhs=Sbf[c][:, :],
                              start=True, stop=True, R=[f"dQM{i}{c}", f"dSbf{c}"], W=[f"ps{2 + s_ // 2}"])
                    for h in range(4):
                        c, hh = h // 2, h % 2
                        mk.op("pe", "matmul", KVp[hh * 64:(hh + 1) * 64, c, :], lhsT=KH[i][:, s_, h * 64:(h + 1) * 64],
                              rhs=Vb[i][:, h, :], start=True, stop=True, R=[f"dKH{i}s{s_}", f"dVb{i}"], W=["ps1kv"])
                    for c in range(2):
                        mk.op("dve", "scalar_tensor_tensor", out=S[c][:], in0=S[c][:], scalar=Gc[i][:, c, s_:s_ + 1],
                              in1=KVp[:, c, :], op0=ALU.mult, op1=ALU.add, R=[f"dS{c}", "ps1kv", f"dGc{i}"], W=[f"dS{c}"])
                        mk.op("act", "activation", out=Sbf[c][:], in_=S[c][:], func=AF.Copy, R=[f"dS{c}"], W=[f"dSbf{c}"])
                for b_ in range(2):
                    mk.op("dve", "tensor_reduce", out=red[i][:, b_, :],
                          in_=PS[2 + b_][:, :].rearrange("p (s x) -> p x s", s=2), axis=AX.X, op=ALU.add,
                          R=[f"ps{2 + b_}"], W=[f"dred{i}{b_}"])
                mk.op("dve", "tensor_tensor", out=tot[i][:], in0=PS[1][:, 0:256], in1=red[i][:, 0, :], op=ALU.add,
                      R=["ps1i", f"dred{i}0"], W=[f"dtot{i}"])
                if dr == 0:
                    mk.op("pool", "tensor_tensor", out=OF[:, t, :], in0=tot[i][:], in1=red[i][:, 1, :], op=ALU.add,
                          R=[f"dtot{i}", f"dred{i}1"], W=[f"dOF{t}"])
                else:
                    mk.op("pool", "tensor_tensor", out=tot[i][:], in0=tot[i][:], in1=red[i][:, 1, :], op=ALU.add,
                          R=[f"dtot{i}", f"dred{i}1"], W=[f"dtot{i}"])
                    mk.op("dve", "tensor_tensor", out=tot[i][:], in0=tot[i][:], in1=OF[:, t, :], op=ALU.add,
                          R=[f"dtot{i}", f"dOF{t}"], W=[f"dtot{i}"])
                    mk.op("act", "activation", out=gt[i][:], in_=X[i][:, 1024:1280], func=AF.Silu, R=[f"dX{i}"],
                          W=[f"dgt{i}"])
                    fin(tot[i][:], f"dtot{i}", False, gt[i][:], f"dgt{i}", t)
                it += 1
        for c in range(2):
            mk.dma("pool", YT[3, c], yTb[:, c, :], R=["yTb"], W=[f"YT3{c}"])
        mk.barrier()
        P.close()

    def merge_moe_phase(l, last):
        P = Pool(nc, f"mm{l}")
        TB = 6
        t_first = 2 if last else 0
        blocks = []
        t = t_first
        while t < NT:
            n = min(TB, NT - t)
            blocks.append((t, n))
            t += n
        wbr = P.sb([128, 8, D], BF16, "wbr")
        wo = P.sb([128, 8, D], BF16, "wo")
        wstage = P.sb([128, 8, 512], F32, "wstage")
        for half in range(2):
            mk.dma("sp", wstage[:], w_branch[l].rearrange("n (c p) f -> p (n c) f", p=128)[:, :, half * 512:(half + 1) * 512],
                   W=["wstage"])
            mk.ev(wbr[:, :, half * 512:(half + 1) * 512], wstage[:], R=["wstage"], W=["wbr"])
        for half in range(2):
            mk.dma("sp", wstage[:], w_out[l].rearrange("(c p) f -> p c f", p=128)[:, :, half * 512:(half + 1) * 512],
                   W=["wstage"])
            mk.ev(wo[:, :, half * 512:(half + 1) * 512], wstage[:], R=["wstage"], W=["wo"])
        wgr = P.sb([128, 8, 20], F32, "wgr")
        bgr = P.sb([1, 20], F32, "bgr")
        mk.dma("sp", wgr[:], moe_wgr[l].rearrange("(c p) f -> p c f", p=128), W=["wgr"])
        mk.dma("sp", bgr[:], moe_bgr[l], W=["bgr"])
        identf = C("ident")
        ones = C("ones")
        norm = make_norm(P, 1)
        h2T = P.sb([128, 8, TB * 128], BF16, "h2T")
        xnew = [P.sb([128, D], F32, "xnew") for _ in range(2)]
        acc = P.sb([128, TB, D], F32, "acc")
        gates = P.sb([128, TB, 16], F32, "gates")
        yt = [P.sb([128, 8, 128], BF16, "yt")] * 2
        mg = [P.sb([128, 4096], BF16, "mg")] * 2
        xt = [P.sb([128, D], F32, "xt")] * 2
        zz = P.sb([128, D], F32, "zz")
        zt = P.sb([128, D], F32, "zt")
        zb = P.sb([128, D], BF16, "zb")
        zT = P.sb([128, 8, 128], BF16, "zT")
        h2f = P.sb([128, D], F32, "h2f")
        h2fT = P.sb([128, 8, 128], F32, "h2fT")
        rt = {k: P.sb([128, w], F32, "rt" + k) for k, w in
              (("L", 20), ("gm", 1), ("goh", 4), ("ge", 4), ("gs", 1), ("el", 4), ("m1", 1), ("oh1", 4), ("e2", 4),
               ("m2", 1), ("oh2", 4), ("w1", 1), ("w2", 1), ("gw", 4))}
        w1f = [wstage, wstage]
        w1b = [P.sb([128, 8, 512], BF16, "w1b")] * 2
        w3b = [P.sb([128, 8, 512], BF16, "w3b")] * 2
        w2b = [P.sb([128, 4, D], BF16, "w2b")] * 2
        sl = [P.sb([128, 512], F32, "sl")] * 2
        actT = [P.sb([128, 4, 512], BF16, "actT") for _ in range(2)]
        nit = 0
        nw = 0
        for (tb0, tn) in blocks:
            for tt in range(tn):
                t = tb0 + tt
                i = nit % 2
                nit += 1
                j = cond_of(t)
                cols = slice(t * 128, (t + 1) * 128)
                mk.dma("sp", yt[i][:], YT[:, :, :, cols].rearrange("n c p t -> p (n c) t"),
                       R=[f"YT{n}{c}" for n in range(4) for c in range(2)], W=["yt"])
                mk.dma("sp", mg[i][:], MG[cols, :], R=["MG"], W=["mg"])
                mk.dma("sp", xt[i][:], XR[cols, :], R=[f"XR{t}"], W=["mxt"])
                for n in range(4):
                    for cb in range(2):
                        pb = (n * 2 + cb) % 2
                        for c in range(2):
                            mk.op("pe", "matmul", PS[pb][:, :], lhsT=yt[i][:, n * 2 + c, :],
                                  rhs=wbr[:, n * 2 + c, cb * 512:(cb + 1) * 512], start=(c == 0), stop=(c == 1),
                                  R=["yt", "wbr"], W=[f"ps{pb}"])
                        dst = zz if n == 0 else zt
                        dkey = "zz" if n == 0 else "zt"
                        mk.op("dve", "tensor_tensor", out=dst[:, cb * 512:(cb + 1) * 512], in0=PS[pb][:, :],
                              in1=mg[i][:, n * 1024 + cb * 512:n * 1024 + (cb + 1) * 512], op=ALU.mult,
                              R=[f"ps{pb}", "mg"], W=[dkey])
                        if n > 0:
                            mk.op("pool", "tensor_tensor", out=zz[:, cb * 512:(cb + 1) * 512],
                                  in0=zz[:, cb * 512:(cb + 1) * 512], in1=zt[:, cb * 512:(cb + 1) * 512], op=ALU.add,
                                  R=["zz", "zt"], W=["zz"])
                mk.op("act", "activation", out=zb[:], in_=zz[:], func=AF.Copy, R=["zz"], W=["zb"])
                for c in range(8):
                    mk.op("pe", "transpose", out=PQ[1][:, c * 128:(c + 1) * 128], in_=zb[:, c * 128:(c + 1) * 128],
                          identity=identb[:], R=["zb", "identb"], W=["pq1"])
                mk.ev(zT[:].rearrange("p c n -> p (c n)"), PQ[1][:, :], R=["pq1"], W=["zT"])
                for cb in range(2):
                    pb = 2 + cb
                    for c in range(8):
                        mk.op("pe", "matmul", PS[pb][:, :], lhsT=zT[:, c, :], rhs=wo[:, c, cb * 512:(cb + 1) * 512],
                              start=(c == 0), stop=(c == 7), R=["zT", "wo"], W=[f"ps{pb}"])
                    mk.op("dve", "tensor_tensor", out=zt[:, cb * 512:(cb + 1) * 512], in0=PS[pb][:, :],
                          in1=GB[:, j, 0, cb * 512:(cb + 1) * 512], op=ALU.mult, R=[f"ps{pb}", "GB"], W=["zt"])
                    mk.op("pool", "tensor_tensor", out=xnew[i][:, cb * 512:(cb + 1) * 512], in0=zt[:, cb * 512:(cb + 1) * 512],
                          in1=xt[i][:, cb * 512:(cb + 1) * 512], op=ALU.add, R=["zt", "mxt"], W=[f"xnew{i}"])
                mk.dma("pool", XR[cols, :], xnew[i][:], R=[f"xnew{i}"], W=[f"XR{t}"])
                norm(xnew[i][:], f"xnew{i}", j, 3, 2, h2T[:, :, tt * 128:(tt + 1) * 128], f"h2T{tt}")
                ssr = rt["gs"]
                mk.op("act", "activation", out=h2f[:], in_=xnew[i][:], func=AF.Square, accum_out=ssr[:],
                      R=[f"xnew{i}"], W=["h2f", "rgs"])
                mk.op("act", "activation", out=ssr[:], in_=ssr[:], func=AF.Sqrt, scale=1.0 / D, bias=EPS, R=["rgs"], W=["rgs"])
                mk.op("dve", "reciprocal", out=ssr[:], in_=ssr[:], R=["rgs"], W=["rgs"])
                mk.op("dve", "tensor_scalar", out=h2f[:], in0=xnew[i][:], scalar1=ssr[:, 0:1], scalar2=None,
                      op0=ALU.mult, R=[f"xnew{i}", "rgs", "h2f"], W=["h2f"])
                for half in range(2):
                    for c4 in range(4):
                        c = half * 4 + c4
                        mk.op("pe", "transpose", out=PS[5][:, c4 * 128:(c4 + 1) * 128], in_=h2f[:, c * 128:(c + 1) * 128],
                              identity=identf, R=["h2f", "cst"], W=["ps5"])
                    mk.op("dve", "tensor_tensor", out=h2fT[:, half * 4:(half + 1) * 4, :],
                          in0=PS[5][:, :].rearrange("p (c n) -> p c n", c=4),
                          in1=MODC[:, 3, half * 4:(half + 1) * 4, j:j + 1].to_broadcast([128, 4, 128]), op=ALU.mult,
                          R=["ps5", "MODC"], W=["h2fT"])
                    mk.op("pool", "tensor_tensor", out=h2fT[:, half * 4:(half + 1) * 4, :],
                          in0=h2fT[:, half * 4:(half + 1) * 4, :],
                          in1=MODC[:, 2, half * 4:(half + 1) * 4, j:j + 1].to_broadcast([128, 4, 128]), op=ALU.add,
                          R=["h2fT", "MODC"], W=["h2fT"])
                for c in range(8):
                    mk.op("pe", "matmul", PS[4][:, 0:20], lhsT=h2fT[:, c, :], rhs=wgr[:, c, :], start=(c == 0), stop=False,
                          R=["h2fT", "wgr"], W=["ps4r"])
                mk.op("pe", "matmul", PS[4][:, 0:20], lhsT=ones[0:1, :], rhs=bgr[0:1, :], start=False, stop=True,
                      R=["cst", "bgr"], W=["ps4r"])
                Lg = rt["L"]
                mk.op("act", "activation", out=Lg[:], in_=PS[4][:, 0:20], func=AF.Copy, R=["ps4r"], W=["rL"])
                rk = ["rL"]
                mk.op("dve", "tensor_reduce", out=rt["gm"][:], in_=Lg[:, 0:4], axis=AX.X, op=ALU.max, R=rk, W=["rgm"])
                mk.op("dve", "tensor_scalar", out=rt["goh"][:], in0=Lg[:, 0:4], scalar1=rt["gm"][:, 0:1], scalar2=None,
                      op0=ALU.is_ge, R=rk + ["rgm"], W=["rgoh"])
                mk.op("dve", "tensor_scalar", out=rt["ge"][:], in0=Lg[:, 0:4], scalar1=rt["gm"][:, 0:1], scalar2=None,
                      op0=ALU.subtract, R=rk + ["rgm"], W=["rge"])
                mk.op("act", "activation", out=rt["ge"][:], in_=rt["ge"][:], func=AF.Exp, accum_out=rt["gs"][:],
                      R=["rge"], W=["rge", "rgs"])
                mk.op("dve", "reciprocal", out=rt["gs"][:], in_=rt["gs"][:], R=["rgs"], W=["rgs"])
                mk.op("dve", "tensor_scalar", out=rt["el"][:], in0=Lg[:, 4:8], scalar1=rt["goh"][:, 0:1], scalar2=None,
                      op0=ALU.mult, R=rk + ["rgoh"], W=["rel"])
                for g in range(1, 4):
                    mk.op("dve", "scalar_tensor_tensor", out=rt["el"][:], in0=Lg[:, 4 + g * 4:8 + g * 4],
                          scalar=rt["goh"][:, g:g + 1], in1=rt["el"][:], op0=ALU.mult, op1=ALU.add,
                          R=rk + ["rgoh", "rel"], W=["rel"])
                mk.op("dve", "tensor_reduce", out=rt["m1"][:], in_=rt["el"][:], axis=AX.X, op=ALU.max, R=["rel"], W=["rm1"])
                mk.op("dve", "tensor_scalar", out=rt["oh1"][:], in0=rt["el"][:], scalar1=rt["m1"][:, 0:1], scalar2=None,
                      op0=ALU.is_ge, R=["rel", "rm1"], W=["roh1"])
                mk.op("dve", "scalar_tensor_tensor", out=rt["e2"][:], in0=rt["oh1"][:], scalar=-1e30, in1=rt["el"][:],
                      op0=ALU.mult, op1=ALU.add, R=["roh1", "rel"], W=["re2"])
                mk.op("dve", "tensor_reduce", out=rt["m2"][:], in_=rt["e2"][:], axis=AX.X, op=ALU.max, R=["re2"], W=["rm2"])
                mk.op("dve", "tensor_scalar", out=rt["oh2"][:], in0=rt["e2"][:], scalar1=rt["m2"][:, 0:1], scalar2=None,
                      op0=ALU.is_ge, R=["re2", "rm2"], W=["roh2"])
                mk.op("dve", "tensor_tensor", out=rt["w1"][:], in0=rt["m2"][:], in1=rt["m1"][:], op=ALU.subtract,
                      R=["rm1", "rm2"], W=["rw1"])
                mk.op("act", "activation", out=rt["w1"][:], in_=rt["w1"][:], func=AF.Exp, R=["rw1"], W=["rw1"])
                mk.op("dve", "tensor_scalar_add", out=rt["w1"][:], in0=rt["w1"][:], scalar1=1.0, R=["rw1"], W=["rw1"])
                mk.op("dve", "reciprocal", out=rt["w1"][:], in_=rt["w1"][:], R=["rw1"], W=["rw1"])
                mk.op("dve", "tensor_tensor", out=rt["w1"][:], in0=rt["w1"][:], in1=rt["gs"][:], op=ALU.mult,
                      R=["rw1", "rgs"], W=["rw1"])
                mk.op("dve", "tensor_tensor", out=rt["w2"][:], in0=rt["gs"][:], in1=rt["w1"][:], op=ALU.subtract,
                      R=["rw1", "rgs"], W=["rw2"])
                mk.op("dve", "tensor_scalar", out=rt["gw"][:], in0=rt["oh1"][:], scalar1=rt["w1"][:, 0:1], scalar2=None,
                      op0=ALU.mult, R=["roh1", "rw1"], W=["rgw"])
                mk.op("dve", "scalar_tensor_tensor", out=rt["gw"][:], in0=rt["oh2"][:], scalar=rt["w2"][:, 0:1],
                      in1=rt["gw"][:], op0=ALU.mult, op1=ALU.add, R=["roh2", "rw2", "rgw"], W=["rgw"])
                for g in range(4):
                    mk.op("dve", "tensor_scalar", out=gates[:, tt, g * 4:(g + 1) * 4], in0=rt["gw"][:],
                          scalar1=rt["goh"][:, g:g + 1], scalar2=None, op0=ALU.mult, R=["rgw", "rgoh"], W=[f"gates{tt}"])
            ntok = tn * 128
            sblocks = [(s0, min(512, ntok - s0)) for s0 in range(0, ntok, 512)]
            hk = [f"h2T{tt}" for tt in range(tn)]
            for e in range(16):
                wi = 0
                mk.dma("sp", w1f[0][:], moe_w1[l, e].rearrange("(c p) f -> p c f", p=128), W=["wstage"])
                mk.ev(w1b[wi][:], w1f[0][:], R=["wstage"], W=[f"w1b{wi}"])
                mk.dma("sp", w1f[1][:], moe_w3[l, e].rearrange("(c p) f -> p c f", p=128), W=["wstage"])
                mk.ev(w3b[wi][:], w1f[1][:], R=["wstage"], W=[f"w3b{wi}"])
                mk.dma("sp", w1f[0][:].rearrange("p c f -> p (c f)").rearrange("p (c f) -> p c f", c=4),
                       moe_w2[l, e].rearrange("(c p) f -> p c f", p=128), W=["wstage"])
                mk.ev(w2b[wi][:], w1f[0][:].rearrange("p c f -> p (c f)").rearrange("p (c f) -> p c f", c=4),
                      R=["wstage"], W=[f"w2b{wi}"])
                for (s0, sw) in sblocks:
                    ai = (s0 // 512) % 2
                    for fcn in range(4):
                        for k in range(8):
                            mk.op("pe", "matmul", PS[0][:, 0:sw], lhsT=w1b[wi][:, k, fcn * 128:(fcn + 1) * 128],
                                  rhs=h2T[:, k, s0:s0 + sw], start=(k == 0), stop=(k == 7), R=[f"w1b{wi}"] + hk, W=["ps0"])
                        for k in range(8):
                            mk.op("pe", "matmul", PS[1][:, 0:sw], lhsT=w3b[wi][:, k, fcn * 128:(fcn + 1) * 128],
                                  rhs=h2T[:, k, s0:s0 + sw], start=(k == 0), stop=(k == 7), R=[f"w3b{wi}"] + hk, W=["ps1"])
                        si = fcn % 2
                        mk.op("act", "activation", out=sl[si][:, 0:sw], in_=PS[0][:, 0:sw], func=AF.Silu, R=["ps0"],
                              W=["sl"])
                        mk.op("dve", "tensor_tensor", out=actT[ai][:, fcn, 0:sw], in0=sl[si][:, 0:sw], in1=PS[1][:, 0:sw],
                              op=ALU.mult, R=["sl", "ps1"], W=[f"actT{ai}"])
                    for q in range(sw // 128):
                        tt = s0 // 128 + q
                        for cb in range(2):
                            pb = 2 + cb
                            for fcn in range(4):
                                mk.op("pe", "matmul", PS[pb][:, :], lhsT=actT[ai][:, fcn, q * 128:(q + 1) * 128],
                                      rhs=w2b[wi][:, fcn, cb * 512:(cb + 1) * 512], start=(fcn == 0), stop=(fcn == 3),
                                      R=[f"actT{ai}", f"w2b{wi}"], W=[f"ps{pb}"])
                            if e == 0:
                                mk.op("dve", "tensor_scalar", out=acc[:, tt, cb * 512:(cb + 1) * 512], in0=PS[pb][:, :],
                                      scalar1=gates[:, tt, e:e + 1], scalar2=None, op0=ALU.mult,
                                      R=[f"ps{pb}", f"gates{tt}"], W=[f"acc{tt}"])
                            else:
                                mk.op("dve", "scalar_tensor_tensor", out=acc[:, tt, cb * 512:(cb + 1) * 512],
                                      in0=PS[pb][:, :], scalar=gates[:, tt, e:e + 1], in1=acc[:, tt, cb * 512:(cb + 1) * 512],
                                      op0=ALU.mult, op1=ALU.add, R=[f"ps{pb}", f"gates{tt}", f"acc{tt}"], W=[f"acc{tt}"])
            for tt in range(tn):
                t = tb0 + tt
                j = cond_of(t)
                mk.op("pool", "tensor_tensor", out=acc[:, tt, :], in0=acc[:, tt, :], in1=GB[:, j, 1, :], op=ALU.mult,
                      R=[f"acc{tt}", "GB"], W=[f"acc{tt}"])
                i2 = tt % 2
                mk.dma("sp", xt[i2][:], XR[t * 128:(t + 1) * 128, :], R=[f"XR{t}"], W=["mxt"])
                mk.op("dve", "tensor_tensor", out=acc[:, tt, :], in0=acc[:, tt, :], in1=xt[i2][:], op=ALU.add,
                      R=[f"acc{tt}", "mxt"], W=[f"acc{tt}"])
                mk.dma("pool", XR[t * 128:(t + 1) * 128, :], acc[:, tt, :], R=[f"acc{tt}"], W=[f"XR{t}"])
        mk.barrier()
        P.close()

    def final_phase():
        P = Pool(nc, "fin")
        fw = P.sb([128, D], F32, "fw")
        mk.dma("sp", fw[:], fnw, W=["fw"])
        xt = [P.sb([128, D], F32, "xt") for _ in range(2)]
        ot = [P.sb([128, D], F32, "ot") for _ in range(2)]
        junk = P.sb([128, D], BF16, "junk")
        ss = [P.sb([128, 1], F32, "ss") for _ in range(2)]
        for t in range(2, NT):
            i = t % 2
            mk.dma("sp", xt[i][:], XR[t * 128:(t + 1) * 128, :], R=[f"XR{t}"], W=[f"fxt{i}"])
            mk.op("act", "activation", out=junk[:], in_=xt[i][:], func=AF.Square, accum_out=ss[i][:], R=[f"fxt{i}"],
                  W=["fjunk", f"fss{i}"])
            mk.op("act", "activation", out=ss[i][:], in_=ss[i][:], func=AF.Sqrt, scale=1.0 / D, bias=EPS, R=[f"fss{i}"],
                  W=[f"fss{i}"])
            mk.op("dve", "reciprocal", out=ss[i][:], in_=ss[i][:], R=[f"fss{i}"], W=[f"fss{i}"])
            mk.op("dve", "scalar_tensor_tensor", out=ot[i][:], in0=xt[i][:], scalar=ss[i][:, 0:1], in1=fw[:], op0=ALU.mult,
                  op1=ALU.mult, R=[f"fxt{i}", f"fss{i}", "fw"], W=[f"fot{i}"])
            mk.dma("pool", yout[(t - 2) * 128:(t - 1) * 128, :], ot[i][:], R=[f"fot{i}"], W=["yout"])
        mk.barrier()
        P.close()

    stages = dict(mod=mod_phase, inproj=inproj_phase, a=mixer_a, b=mixer_b, c=mixer_c, d=mixer_d)
    return dict(nc=nc, mk=mk, stages=stages, merge=merge_moe_phase, final=final_phase, dbg=dbg,
                scr=dict(XR=XR, FM=FM, TM=TM, MG=MG, YT=YT))


def emit_all(prog, layers=NL, upto=None, skip=()):
    mk = prog["mk"]
    mk.barrier()
    done = False
    for l in range(layers):
        for s in ("mod", "inproj", "a", "b", "c", "d"):
            if s in skip:
                continue
            prog["stages"][s](l)
            if upto == (l, s):
                done = True
                break
        if done:
            break
        prog["merge"](l, l == NL - 1)
        if upto == (l, "merge"):
            done = True
            break
    if not done:
        prog["final"]()
    mk.barrier(engines=("sp",))


def _consts():
    j = np.arange(128)[:, None]
    i = np.arange(128)[None, :]
    same = (j // DL) == (i // DL)
    m = {}
    m["ident"] = (j == i)
    m["triF"] = (j <= i)
    m["triB"] = (j >= i)
    m["blkF"] = same & (j <= i)
    m["blkB"] = same & (j >= i)
    m["aftF"] = same & (j > i)
    m["befB"] = same & (j < i)
    m["diffF"] = np.maximum(i - j, 0)
    m["diffB"] = np.maximum(j - i, 0)
    m["maskF"] = (i >= j)
    m["maskB"] = (j > i)
    m["posF"] = np.broadcast_to(i + 1, (128, 128))
    m["posB"] = np.broadcast_to(128 - i, (128, 128))
    m["negF4"] = np.tile(np.where(j <= i, 0.0, -30000.0), (1, 4))
    m["negB4"] = np.tile(np.where(j >= i, 0.0, -30000.0), (1, 4))
    m["mblkF4"] = np.tile(same & (j <= i), (1, 4))
    m["mblkB4"] = np.tile(same & (j >= i), (1, 4))
    m["kpos"] = np.concatenate([127 - j, j], 1)
    m["ones"] = np.ones((128, 128))
    sel = np.zeros((128, 256))
    sel[0, 0:128] = 1.0
    sel[1, 128:256] = 1.0
    m["sel"] = sel
    m["subm"] = np.concatenate([(j // DL) == s_ for s_ in range(128 // DL)], 1)
    qm = np.zeros((128, 2, 5, 128), np.float32)
    for hh_ in range(2):
        for s_ in range(5):
            colsel = np.ones(128, bool) if s_ == 4 else (np.arange(128) // DL == s_)
            qm[hh_ * 64:(hh_ + 1) * 64, hh_, s_, :] = colsel[None, :]
    m["qmask"] = qm.reshape(128, 1280)
    out = np.zeros((128, NCST), np.float32)
    for k, (o, w) in CST.items():
        out[:, o:o + w] = np.asarray(m[k], np.float32)
    return out


def _rope_tables():
    n = 16
    inv = np.power(np.float32(10000.0), -np.arange(n, dtype=np.float32) / n).astype(np.float32)
    t = np.arange(4096)
    row = (t // 64).astype(np.float32)
    col = (t % 64).astype(np.float32)
    ang = np.concatenate([row[:, None] * inv, col[:, None] * inv], -1)
    cos = np.cos(ang).astype(np.float32).T
    sin = np.sin(ang).astype(np.float32).T
    Cc = np.ones((128, T), np.float32)
    Ss = np.zeros((128, T), np.float32)
    for hh in range(2):
        Cc[hh * 64:hh * 64 + 32, 256:] = cos
        Cc[hh * 64 + 32:hh * 64 + 64, 256:] = cos
        Ss[hh * 64:hh * 64 + 32, 256:] = -sin
        Ss[hh * 64 + 32:hh * 64 + 64, 256:] = sin
    return Cc, Ss


def prep_shared(inp):
    f = np.float32
    w_in = np.asarray(inp["w_in"], f)
    offs = {}
    o = 0
    for name, w in (("a_x", 256), ("a_g", 256), ("b_q", 256), ("b_k", 256), ("b_v", 256), ("b_g", 256), ("c_q", 256),
                    ("c_k", 256), ("c_v", 256), ("c_o", 256), ("c_gates", 16), ("d_q", 256), ("d_ff", 256),
                    ("d_fb", 256), ("d_i", 256), ("d_g", 256), ("merge", 4096)):
        offs[name] = (o, w)
        o += w

    def cols(n):
        a, w = offs[n]
        return w_in[:, :, a:a + w]

    perm = np.concatenate([np.arange(h * 64 + 32, h * 64 + 64).tolist() + np.arange(h * 64, h * 64 + 32).tolist()
                           for h in range(4)]).astype(np.int64)
    w_fm = np.concatenate([cols("a_x"), cols("a_g"), cols("b_q"), cols("b_q")[:, :, perm], cols("b_k"),
                           cols("b_k")[:, :, perm], cols("c_q"), cols("c_k")], -1)
    w_tm = np.concatenate([cols("b_v"), cols("b_g"), cols("c_v"), cols("c_o"), cols("d_q"), cols("d_ff"), cols("d_fb"),
                           cols("d_i"), cols("d_g"), cols("c_gates"), cols("merge")], -1)
    sh = {}
    sh["w_mod"] = np.ascontiguousarray(inp["w_mod"], f)
    bm = np.asarray(inp["b_mod"], f)
    sh["bmod_c"] = np.ascontiguousarray(bm.reshape(NL, 48, 128).transpose(0, 2, 1))
    sh["bmod_r"] = np.ascontiguousarray(bm.reshape(NL, 1, 6144))
    sh["w_fm"] = np.ascontiguousarray(w_fm)
    sh["w_tm"] = np.ascontiguousarray(w_tm)
    acw = np.asarray(inp["a_conv_w"], f)
    sh["a_cw"] = np.ascontiguousarray(acw.reshape(NL, 4, 2, 128).transpose(0, 3, 2, 1))
    sh["a_cb"] = np.ascontiguousarray(np.asarray(inp["a_conv_b"], f).reshape(NL, 2, 128).transpose(0, 2, 1))
    gw = np.asarray(inp["a_gate_w"], f)
    agw = np.zeros((NL, 128, 2, 2, 2, 128), f)
    for c in range(2):
        for hh in range(2):
            agw[:, hh * 64:(hh + 1) * 64, :, :, c, hh * 64:(hh + 1) * 64] = gw[:, :, :, 2 * c + hh].transpose(0, 3, 1, 2, 4)
    sh["a_gw"] = agw
    gb = np.asarray(inp["a_gate_b"], f)
    sh["a_gb"] = np.ascontiguousarray(gb.reshape(NL, 2, 2, 2, 128).transpose(0, 4, 1, 2, 3))
    lam = np.asarray(inp["a_lambda"], f)
    sh["a_lam"] = np.ascontiguousarray(lam.reshape(NL, 2, 2, 128).transpose(0, 3, 1, 2))
    th = np.asarray(inp["b_theta"], f)
    thp = np.zeros((NL, 128, 2, 2), f)
    for c in range(2):
        for hh in range(2):
            thp[:, hh * 64:(hh + 1) * 64, :, c] = th[:, None, :, 2 * c + hh]
    sh["b_thp"] = thp
    sh["b_thh"] = np.ascontiguousarray(np.broadcast_to(th[:, None], (NL, 128, 2, 4)))
    ccw = np.asarray(inp["c_conv_w"], f)
    sh["c_cw"] = np.ascontiguousarray(ccw.reshape(NL, 4, 4, 128).transpose(0, 3, 2, 1))
    sh["c_cb"] = np.ascontiguousarray(np.asarray(inp["c_conv_b"], f).reshape(NL, 4, 128).transpose(0, 2, 1))
    sh["c_gb"] = np.ascontiguousarray(np.broadcast_to(np.asarray(inp["c_gate_b"], f).reshape(NL, 1, 16), (NL, 128, 16)))
    sh["d_lbr"] = np.ascontiguousarray(np.broadcast_to(np.asarray(inp["d_lb"], f)[None], (128, 2, 256)))
    sh["w_branch"] = np.ascontiguousarray(inp["w_branch"], f)
    sh["w_out"] = np.ascontiguousarray(inp["w_out"], f)
    sh["moe_wgr"] = np.ascontiguousarray(np.concatenate([np.asarray(inp["moe_w_group"], f), np.asarray(inp["moe_w_router"], f)], -1))
    sh["moe_bgr"] = np.ascontiguousarray(np.concatenate([np.asarray(inp["moe_b_group"], f), np.asarray(inp["moe_b_router"], f)], -1).reshape(NL, 1, 20))
    sh["moe_w1"] = np.ascontiguousarray(inp["moe_w1"], f)
    sh["moe_w3"] = np.ascontiguousarray(inp["moe_w3"], f)
    sh["moe_w2"] = np.ascontiguousarray(inp["moe_w2"], f)
    sh["fnw"] = np.ascontiguousarray(np.broadcast_to(np.asarray(inp["final_norm_w"], f)[None], (128, D)))
    sh["cst"] = _consts()
    sh["ropeC"], sh["ropeS"] = _rope_tables()
    return sh


def prep_core(inp, b):
    f = np.float32
    d = {}
    d["xin"] = np.ascontiguousarray(np.concatenate([np.asarray(inp["ctx"][b], f), np.asarray(inp["x"][b], f)], 0))
    cv = np.stack([np.asarray(inp["c_ctx"], f), np.asarray(inp["c"][b], f)], -1)
    d["cvec"] = np.ascontiguousarray(cv.reshape(8, 128, 2).transpose(1, 0, 2))
    return d


_PROG = None


def kernel(**inputs):
    global _PROG
    if _PROG is None:
        _PROG = build_program()
        emit_all(_PROG)
    nc = _PROG["nc"]
    sh = prep_shared(inputs)
    in_maps = []
    for core in range(8):
        m = dict(sh)
        m.update(prep_core(inputs, core % 4))
        in_maps.append(m)
    res = run_bass_kernel_spmd(nc, in_maps, core_ids=list(range(8)))
    out = np.stack([np.asarray(res.results[b]["yout"], np.float32) for b in range(4)], 0)
    return out
```

```python
import contextlib
import numpy as np
import ml_dtypes
import concourse.bass as bass
import concourse.mybir as mybir
from concourse.bass_utils import run_bass_kernel_spmd

F32 = mybir.dt.float32
BF16 = mybir.dt.bfloat16
AF = mybir.ActivationFunctionType
ALU = mybir.AluOpType
AX = mybir.AxisListType

T = 4352
NT = 34
D = 1024
EPS = 1e-6
NL = 2
DL = 32
DNS = 128 // DL
DBG = dict(maxit=None, core=True, fin=True, heads=(0, 1, 2, 3))
TMW = 2320
TM_OFF = dict(b_v=0, b_g=256, c_v=512, c_o=768, d_q=1024, d_ff=1280, d_fb=1536, d_i=1792, d_g=2048, c_gates=2304)
FM_OFF = dict(a_x=0, a_g=2, b_q=4, b_qp=6, b_k=8, b_kp=10, c_q=12, c_k=14)

CST = {}
_off = 0
for _n, _w in (("ident", 128), ("triF", 128), ("triB", 128), ("blkF", 128), ("blkB", 128), ("aftF", 128),
               ("befB", 128), ("diffF", 128), ("diffB", 128), ("maskF", 128), ("maskB", 128), ("posF", 128),
               ("posB", 128), ("negF4", 512), ("negB4", 512), ("mblkF4", 512), ("mblkB4", 512), ("kpos", 2),
               ("ones", 128), ("sel", 256), ("subm", 4), ("qmask", 1280)):
    CST[_n] = (_off, _w)
    _off += _w
NCST = _off


class MK:
    SEM_ROT = 30000

    def __init__(self, nc, ndma=8):
        self.nc = nc
        self.engs = {"pe": nc.tensor, "act": nc.scalar, "dve": nc.vector, "pool": nc.gpsimd, "sp": nc.sync}
        self._ctxs = []
        self.nsem = 0
        self.sem = {}
        self.cnt = {}
        for e in ("pe", "act", "dve", "pool"):
            self.sem[e] = self._newsem("s_" + e)
            self.cnt[e] = 0
        self.seen = {e: {} for e in self.engs}
        self.dq = {}
        for q in ("sp", "pool"):
            self.dq[q] = {"i": 0, "slots": [[self._newsem(f"d_{q}{i}"), 0] for i in range(ndma)]}
        self.res = {}
        self.ninst = 0
        self.flip = 0

    def _newsem(self, name):
        self.nsem += 1
        cm = self.nc.semaphore(f"{name}_{self.nsem}")
        s = cm.__enter__()
        self._ctxs.append(cm)
        return s

    def _wait(self, eng, tok):
        sem, val = tok
        key = id(sem)
        if self.seen[eng].get(key, 0) >= val:
            return
        self.engs[eng].wait_ge(sem, val)
        self.seen[eng][key] = val

    def _deps(self, R, W):
        deps = []
        for k in R:
            st = self.res.get(k)
            if st and st[0] is not None:
                deps.append(st[0])
        for k in W:
            st = self.res.get(k)
            if st:
                if st[0] is not None:
                    deps.append(st[0])
                deps.extend(st[1])
        return deps

    def _record(self, tok, R, W):
        for k in R:
            st = self.res.setdefault(k, [None, []])
            st[1] = [t for t in st[1] if t[0] is not tok[0]] + [tok]
        for k in W:
            self.res[k] = [tok, []]

    def op(self, eng, method, *args, R=(), W=(), **kw):
        for tok in self._deps(R, W):
            if eng == "pe" and tok[0] is self.sem["pe"]:
                continue
            self._wait(eng, tok)
        ins = getattr(self.engs[eng], method)(*args, **kw)
        if self.cnt[eng] >= self.SEM_ROT:
            self.sem[eng] = self._newsem("s_" + eng)
            self.cnt[eng] = 0
        self.cnt[eng] += 1
        ins.then_inc(self.sem[eng], 1)
        tok = (self.sem[eng], self.cnt[eng])
        self._record(tok, R, W)
        self.ninst += 1
        return tok

    def dma(self, q, out, in_, R=(), W=(), **kw):
        d = self.dq[q]
        slot = d["slots"][d["i"] % len(d["slots"])]
        d["i"] += 1
        if slot[1] > 0:
            self._wait(q, (slot[0], slot[1]))
        if slot[1] >= self.SEM_ROT:
            slot[0] = self._newsem("d_" + q)
            slot[1] = 0
        for tok in self._deps(R, W):
            self._wait(q, tok)
        ins = self.engs[q].dma_start(out=out, in_=in_, **kw)
        slot[1] += 16
        ins.then_inc(slot[0], 16)
        tok = (slot[0], slot[1])
        self._record(tok, R, W)
        self.ninst += 1
        return tok

    def barrier(self, engines=("pe", "act", "dve", "pool", "sp")):
        toks = []
        for q, d in self.dq.items():
            for slot in d["slots"]:
                if slot[1] > 0:
                    toks.append((slot[0], slot[1]))
        for e in ("pe", "act", "dve", "pool"):
            if self.cnt[e] > 0:
                toks.append((self.sem[e], self.cnt[e]))
        for e in engines:
            for tok in toks:
                if e in self.sem and tok[0] is self.sem[e]:
                    continue
                self._wait(e, tok)

    def ev(self, out, in_, R=(), W=(), func=None, **kw):
        if func is not None:
            return self.op("act", "activation", out=out, in_=in_, func=func, R=R, W=W, **kw)
        self.flip ^= 1
        if self.flip:
            return self.op("act", "activation", out=out, in_=in_, func=AF.Copy, R=R, W=W)
        return self.op("dve", "tensor_copy", out=out, in_=in_, R=R, W=W)


class Pool:
    def __init__(self, nc, tag):
        self.nc = nc
        self.tag = tag
        self.stack = contextlib.ExitStack()
        self.n = 0

    def sb(self, shape, dt=F32, name=None):
        self.n += 1
        return self.stack.enter_context(self.nc.sbuf_tensor(f"{self.tag}_{name or 't'}{self.n}", list(shape), dt))

    def close(self):
        self.stack.close()


def build_program(debug=()):
    nc = bass.Bass("TRN2", target_bir_lowering=False)

    def din(name, shape, dt=F32):
        return nc.dram_tensor(name, list(shape), dt, kind="ExternalInput").ap()

    def dscr(name, shape, dt=F32):
        return nc.dram_tensor(name, list(shape), dt, kind="Internal").ap()

    xin = din("xin", [T, D])
    cvec = din("cvec", [128, 8, 2])
    w_mod = din("w_mod", [NL, D, 6144])
    bmod_c = din("bmod_c", [NL, 128, 48])
    bmod_r = din("bmod_r", [NL, 1, 6144])
    w_fm = din("w_fm", [NL, D, 2048])
    w_tm = din("w_tm", [NL, D, TMW + 4096])
    a_cw = din("a_cw", [NL, 128, 2, 4])
    a_cb = din("a_cb", [NL, 128, 2])
    a_gw = din("a_gw", [NL, 128, 2, 2, 2, 128])
    a_gb = din("a_gb", [NL, 128, 2, 2, 2])
    a_lam = din("a_lam", [NL, 128, 2, 2])
    b_thp = din("b_thp", [NL, 128, 2, 2])
    b_thh = din("b_thh", [NL, 128, 2, 4])
    c_cw = din("c_cw", [NL, 128, 4, 4])
    c_cb = din("c_cb", [NL, 128, 4])
    c_gb = din("c_gb", [NL, 128, 16])
    d_lbr = din("d_lbr", [128, 2, 256])
    w_branch = din("w_branch", [NL, 4, 256, D])
    w_out = din("w_out", [NL, D, D])
    moe_wgr = din("moe_wgr", [NL, D, 20])
    moe_bgr = din("moe_bgr", [NL, 1, 20])
    moe_w1 = din("moe_w1", [NL, 16, D, 512])
    moe_w3 = din("moe_w3", [NL, 16, D, 512])
    moe_w2 = din("moe_w2", [NL, 16, 512, D])
    fnw = din("fnw", [128, D])
    cst_d = din("cst", [128, NCST])
    ropeC_d = din("ropeC", [128, T])
    ropeS_d = din("ropeS", [128, T])
    yout = nc.dram_tensor("yout", [4096, D], F32, kind="ExternalOutput").ap()

    XR = dscr("XR", [T, D])
    FM = dscr("FM", [16, 128, T])
    TM = dscr("TM", [T, TMW])
    MG = dscr("MG", [T, 4096], BF16)
    YT = dscr("YT", [4, 2, 128, T], BF16)
    dbg = {}
    for name, shape, dt in debug:
        dbg[name] = nc.dram_tensor("dbg_" + name, list(shape), dt, kind="ExternalOutput").ap()

    mk = MK(nc)
    G = Pool(nc, "g")

    PS = [nc.psum_tensor(f"ps{i}", [128, 512], F32).__enter__() for i in range(6)]
    PQ = [nc.psum_tensor(f"pq{i}", [128, 1024], BF16).__enter__() for i in range(2)]

    cst = G.sb([128, NCST], F32, "cst")
    identb = G.sb([128, 128], BF16, "identb")
    sT = G.sb([128, 8, 2], F32, "sT")
    MODC = G.sb([128, 4, 8, 2], F32, "MODC")
    GB = G.sb([128, 2, 2, D], F32, "GB")
    mk.dma("sp", cst[:], cst_d, W=["cst"])
    mk.dma("sp", sT[:], cvec, W=["sT"])

    def C(name, rows=slice(0, 128)):
        o, w = CST[name]
        return cst[rows, o:o + w]

    mk.op("dve", "tensor_copy", out=identb[:], in_=C("ident"), R=["cst"], W=["identb"])
    mk.op("act", "activation", out=sT[:], in_=sT[:], func=AF.Silu, R=["sT"], W=["sT"])
    for t in range(NT):
        mk.dma("sp", XR[t * 128:(t + 1) * 128, :], xin[t * 128:(t + 1) * 128, :], W=[f"XR{t}"])

    def cond_of(t):
        return 0 if t < 2 else 1

    def mod_phase(l):
        P = Pool(nc, f"mod{l}")
        wblk = P.sb([128, 8, 1024], F32, "wblk")
        bmc = P.sb([128, 48], F32, "bmc")
        bmr = P.sb([1, 6144], F32, "bmr")
        GR = P.sb([2, 2, D], F32, "GR")
        mk.dma("sp", bmc[:], bmod_c[l], W=["bmc"])
        mk.dma("sp", bmr[:], bmod_r[l], W=["bmr"])
        sel = C("sel", slice(0, 2))
        for m in range(6):
            mk.dma("sp", wblk[:], w_mod[l].rearrange("(c p) f -> p c f", p=128)[:, :, m * 1024:(m + 1) * 1024],
                   W=["wblk"])
            if m in (0, 1, 3, 4):
                m4 = {0: 0, 1: 1, 3: 2, 4: 3}[m]
                for c in range(8):
                    for k in range(8):
                        mk.op("pe", "matmul", PS[0][:, c * 2:(c + 1) * 2], lhsT=wblk[:, k, c * 128:(c + 1) * 128],
                              rhs=sT[:, k, :], start=(k == 0), stop=(k == 7), R=["wblk", "sT"], W=["ps0"])
                mk.op("dve", "tensor_tensor", out=MODC[:, m4, :, :],
                      in0=PS[0][:, 0:16].rearrange("p (c j) -> p c j", j=2),
                      in1=bmc[:, m * 8:(m + 1) * 8].unsqueeze(2).to_broadcast([128, 8, 2]), op=ALU.add,
                      R=["ps0", "bmc"], W=["MODC"])
                if m in (1, 4):
                    mk.op("dve", "tensor_scalar_add", out=MODC[:, m4, :, :], in0=MODC[:, m4, :, :], scalar1=1.0,
                          R=["MODC"], W=["MODC"])
            else:
                mi = 0 if m == 2 else 1
                for cb in range(2):
                    for k in range(8):
                        mk.op("pe", "matmul", PS[1][0:2, :], lhsT=sT[:, k, :], rhs=wblk[:, k, cb * 512:(cb + 1) * 512],
                              start=(k == 0), stop=False, R=["wblk", "sT"], W=["ps1"])
                    mk.op("pe", "matmul", PS[1][0:2, :], lhsT=sel[0:1, 0:2],
                          rhs=bmr[0:1, m * 1024 + cb * 512: m * 1024 + (cb + 1) * 512], start=False, stop=True,
                          R=["bmr", "cst"], W=["ps1"])
                    mk.op("dve", "tensor_copy", out=GR[0:2, mi, cb * 512:(cb + 1) * 512], in_=PS[1][0:2, :],
                          R=["ps1"], W=["GR"])
        for j in range(2):
            for mi in range(2):
                for cb in range(2):
                    mk.op("pe", "matmul", PS[1][:, :], lhsT=sel[0:2, j * 128:(j + 1) * 128],
                          rhs=GR[0:2, mi, cb * 512:(cb + 1) * 512], start=True, stop=True, R=["GR", "cst"], W=["ps1"])
                    mk.op("act", "activation", out=GB[:, j, mi, cb * 512:(cb + 1) * 512], in_=PS[1][:, :], func=AF.Copy,
                          R=["ps1"], W=["GB"])
        mk.barrier()
        P.close()

    def make_norm(P, nbuf=2):
        st = dict(junk=P.sb([128, D], BF16, "junk"), ss=[P.sb([128, 1], F32, "ss") for _ in range(nbuf)],
                  xn=[P.sb([128, D], BF16, "xn") for _ in range(nbuf)],
                  tmp=[P.sb([128, 8, 128], F32, "tmp") for _ in range(nbuf)], i=0, nbuf=nbuf)

        def norm(xt_ap, xt_key, j, msc, msh, h_out, h_key):
            i = st["i"] % st["nbuf"]
            st["i"] += 1
            ss, xn, tmp = st["ss"][i], st["xn"][i], st["tmp"][i]
            mk.op("act", "activation", out=st["junk"][:], in_=xt_ap, func=AF.Square, accum_out=ss[:],
                  R=[xt_key], W=["junk", f"ss{i}"])
            mk.op("act", "activation", out=ss[:], in_=ss[:], func=AF.Sqrt, scale=1.0 / D, bias=EPS,
                  R=[f"ss{i}"], W=[f"ss{i}"])
            mk.op("dve", "reciprocal", out=ss[:], in_=ss[:], R=[f"ss{i}"], W=[f"ss{i}"])
            mk.op("dve", "tensor_scalar", out=xn[:], in0=xt_ap, scalar1=ss[:, 0:1], scalar2=None, op0=ALU.mult,
                  R=[xt_key, f"ss{i}"], W=[f"xn{i}"])
            for c in range(8):
                mk.op("pe", "transpose", out=PQ[i][:, c * 128:(c + 1) * 128], in_=xn[:, c * 128:(c + 1) * 128],
                      identity=identb[:], R=[f"xn{i}", "identb"], W=[f"pq{i}"])
            mk.op("dve", "tensor_tensor", out=tmp[:], in0=PQ[i][:, :].rearrange("p (c n) -> p c n", c=8),
                  in1=MODC[:, msc, :, j:j + 1].to_broadcast([128, 8, 128]), op=ALU.mult,
                  R=[f"pq{i}", "MODC"], W=[f"ntmp{i}"])
            mk.op("pool", "tensor_tensor", out=h_out, in0=tmp[:],
                  in1=MODC[:, msh, :, j:j + 1].to_broadcast([128, 8, 128]), op=ALU.add,
                  R=[f"ntmp{i}", "MODC"], W=[h_key])
        return norm

    def inproj_phase(l):
        P = Pool(nc, f"ip{l}")
        hT = P.sb([128, 8, T], BF16, "hT")
        xt = [P.sb([128, D], F32, "xt") for _ in range(2)]
        norm = make_norm(P)
        for t in range(NT):
            i = t % 2
            mk.dma("sp", xt[i][:], XR[t * 128:(t + 1) * 128, :], R=[f"XR{t}"], W=[f"xt{i}"])
            norm(xt[i][:], f"xt{i}", cond_of(t), 1, 0, hT[:, :, t * 128:(t + 1) * 128], f"hT{t}")
        hkeys = [f"hT{t}" for t in range(NT)]
        wf = [P.sb([128, 8, 512], F32, "wf")] * 2
        wb = [P.sb([128, 8, 512], BF16, "wb") for _ in range(2)]
        stg = [P.sb([128, T], F32, "stg")] * 2
        nblk = 0
        tblocks = [(i * 512, min(512, T - i * 512)) for i in range(9)]
        for cb in range(4):
            i = nblk % 2
            nblk += 1
            mk.dma("sp", wf[i][:], w_fm[l].rearrange("(c p) f -> p c f", p=128)[:, :, cb * 512:(cb + 1) * 512],
                   W=["wf"])
            mk.ev(wb[i][:], wf[i][:], R=["wf"], W=[f"wb{i}"])
            for sub in range(4):
                fc = cb * 4 + sub
                si = fc % 2
                for bi, (t0, tw) in enumerate(tblocks):
                    pb = bi % 2
                    for k in range(8):
                        mk.op("pe", "matmul", PS[pb][:, 0:tw], lhsT=wb[i][:, k, sub * 128:(sub + 1) * 128],
                              rhs=hT[:, k, t0:t0 + tw], start=(k == 0), stop=(k == 7),
                              R=[f"wb{i}"] + hkeys[t0 // 128:(t0 + tw) // 128], W=[f"ps{pb}"])
                    mk.ev(stg[si][:, t0:t0 + tw], PS[pb][:, 0:tw], R=[f"ps{pb}"], W=["stg"])
                mk.dma("pool", FM[fc], stg[si][:], R=["stg"], W=[f"FM{fc}"])
        cblocks = [(i * 512, 512) for i in range(4)] + [(2048, TMW - 2048)] + [(TMW + i * 512, 512) for i in range(8)]
        stt = [P.sb([128, 4, 512], F32, "stt") for _ in range(2)]
        stb = [P.sb([128, 4, 512], BF16, "stb") for _ in range(2)]
        tgroups = [(g * 4, min(4, NT - g * 4)) for g in range(9)]
        ns = 0
        for (c0, cw) in cblocks:
            i = nblk % 2
            nblk += 1
            mk.dma("sp", wf[i][:, :, 0:cw], w_tm[l].rearrange("(c p) f -> p c f", p=128)[:, :, c0:c0 + cw], W=["wf"])
            mk.ev(wb[i][:, :, 0:cw], wf[i][:, :, 0:cw], R=["wf"], W=[f"wb{i}"])
            is_mg = c0 >= TMW
            for (g0, gn) in tgroups:
                si = ns % 2
                ns += 1
                for tt in range(gn):
                    t = g0 + tt
                    pb = 2 + (t % 2)
                    for k in range(8):
                        mk.op("pe", "matmul", PS[pb][:, 0:cw], lhsT=hT[:, k, t * 128:(t + 1) * 128],
                              rhs=wb[i][:, k, 0:cw], start=(k == 0), stop=(k == 7), R=[f"wb{i}", f"hT{t}"], W=[f"ps{pb}"])
                    if is_mg:
                        mk.ev(stb[si][:, tt, 0:cw], PS[pb][:, 0:cw], R=[f"ps{pb}"], W=[f"stb{si}"], func=AF.Sigmoid)
                    else:
                        mk.ev(stt[si][:, tt, 0:cw], PS[pb][:, 0:cw], R=[f"ps{pb}"], W=[f"stt{si}"])
                if is_mg:
                    mk.dma("pool", MG[g0 * 128:(g0 + gn) * 128, c0 - TMW:c0 - TMW + cw].rearrange("(t p) c -> p t c", p=128),
                           stb[si][:, 0:gn, 0:cw], R=[f"stb{si}"], W=["MG"])
                else:
                    mk.dma("pool", TM[g0 * 128:(g0 + gn) * 128, c0:c0 + cw].rearrange("(t p) c -> p t c", p=128),
                           stt[si][:, 0:gn, 0:cw], R=[f"stt{si}"], W=["TM"])
        mk.barrier()
        P.close()

    SEGS = [(0, 256), (256, T)]

    def conv_fm(u, src, w4, bcol, keyu, keysrc, wkeys):
        for (s0, e) in SEGS:
            mk.op("act", "activation", out=u[:, s0:e], in_=src[:, s0:e], func=AF.Identity, scale=w4[:, 2:3], bias=bcol,
                  R=[keysrc] + wkeys, W=[keyu])
            for k, sh in ((0, -2), (1, -1), (3, 1)):
                if sh < 0:
                    o, i_ = u[:, s0 - sh:e], src[:, s0:e + sh]
                else:
                    o, i_ = u[:, s0:e - sh], src[:, s0 + sh:e]
                mk.op("dve", "scalar_tensor_tensor", out=o, in0=i_, scalar=w4[:, k:k + 1], in1=o, op0=ALU.mult,
                      op1=ALU.add, R=[keysrc, keyu] + wkeys, W=[keyu])

    def mixer_a(l):
        P = Pool(nc, f"ma{l}")
        cw = P.sb([128, 2, 4], F32, "cw")
        cb = P.sb([128, 2], F32, "cb")
        gwf = P.sb([128, 2, 2, 2, 128], F32, "gwf")
        gwb = P.sb([128, 2, 2, 2, 128], BF16, "gwb")
        gb = P.sb([128, 2, 2, 2], F32, "gb")
        lam = P.sb([128, 2, 2], F32, "lam")
        c1 = P.sb([128, 2, 2], F32, "c1")
        mk.dma("sp", cw[:], a_cw[l], W=["a_cw"])
        mk.dma("sp", cb[:], a_cb[l], W=["a_cb"])
        mk.dma("sp", gwf[:], a_gw[l], W=["a_gwf"])
        mk.dma("sp", gb[:], a_gb[l], W=["a_gb"])
        mk.dma("sp", lam[:], a_lam[l], W=["a_lam"])
        mk.op("dve", "tensor_copy", out=gwb[:], in_=gwf[:], R=["a_gwf"], W=["a_gwb"])
        mk.op("act", "activation", out=c1[:], in_=lam[:], func=AF.Exp, scale=-1.0, R=["a_lam"], W=["a_c1"])
        mk.op("act", "activation", out=c1[:], in_=c1[:], func=AF.Ln, bias=1.0, R=["a_c1"], W=["a_c1"])
        mk.op("dve", "tensor_scalar", out=c1[:], in0=c1[:], scalar1=-8.0, scalar2=None, op0=ALU.mult, R=["a_c1"], W=["a_c1"])
        ax = P.sb([128, T], F32, "ax")
        ag = P.sb([128, T], F32, "ag")
        u = P.sb([128, T], F32, "u")
        ub = P.sb([128, T], BF16, "ub")
        aa = P.sb([128, T], F32, "aa")
        bt = P.sb([128, T], F32, "bt")
        hf = P.sb([128, T], F32, "hf")
        hb = P.sb([128, T], F32, "hb")
        r = [P.sb([128, 512], F32, "r") for _ in range(2)]
        gi = [P.sb([128, 512], F32, "gi") for _ in range(2)]
        yb = P.sb([128, T], BF16, "yb")
        tblocks = [(i * 512, min(512, T - i * 512)) for i in range(9)]
        for c in range(2):
            mk.dma("sp", ax[:], FM[FM_OFF["a_x"] + c], R=[f"FM{FM_OFF['a_x'] + c}"], W=["ax"])
            mk.dma("sp", ag[:], FM[FM_OFF["a_g"] + c], R=[f"FM{FM_OFF['a_g'] + c}"], W=["ag"])
            conv_fm(u, ax, cw[:, c, :], cb[:, c:c + 1], "u", "ax", ["a_cw", "a_cb"])
            mk.op("pool", "tensor_copy", out=ub[:], in_=u[:], R=["u"], W=["ub"])
            for d in range(2):
                for bi, (t0, tw) in enumerate(tblocks):
                    i = bi % 2
                    mk.op("pe", "matmul", PS[i][:, 0:tw], lhsT=gwb[:, d, 0, c, :], rhs=ub[:, t0:t0 + tw], start=True,
                          stop=True, R=["a_gwb", "ub"], W=[f"ps{i}"])
                    mk.op("pe", "matmul", PS[2 + i][:, 0:tw], lhsT=gwb[:, d, 1, c, :], rhs=ub[:, t0:t0 + tw], start=True,
                          stop=True, R=["a_gwb", "ub"], W=[f"ps{2 + i}"])
                    mk.op("act", "activation", out=r[i][:, 0:tw], in_=PS[i][:, 0:tw], func=AF.Sigmoid,
                          bias=gb[:, d, 0, c:c + 1], R=[f"ps{i}", "a_gb"], W=[f"r{i}"])
                    mk.op("act", "activation", out=gi[i][:, 0:tw], in_=PS[2 + i][:, 0:tw], func=AF.Sigmoid,
                          bias=gb[:, d, 1, c:c + 1], R=[f"ps{2 + i}", "a_gb"], W=[f"gi{i}"])
                    mk.op("act", "activation", out=aa[:, t0:t0 + tw], in_=r[i][:, 0:tw], func=AF.Exp,
                          scale=c1[:, d, c:c + 1], R=[f"r{i}", "a_c1"], W=["aa"])
                    mk.op("dve", "tensor_tensor", out=r[i][:, 0:tw], in0=aa[:, t0:t0 + tw], in1=aa[:, t0:t0 + tw],
                          op=ALU.mult, R=["aa", f"r{i}"], W=[f"r{i}"])
                    mk.op("dve", "tensor_scalar", out=r[i][:, 0:tw], in0=r[i][:, 0:tw], scalar1=-1.0, scalar2=1.0,
                          op0=ALU.mult, op1=ALU.add, R=[f"r{i}"], W=[f"r{i}"])
                    mk.op("act", "activation", out=r[i][:, 0:tw], in_=r[i][:, 0:tw], func=AF.Sqrt, R=[f"r{i}"], W=[f"r{i}"])
                    mk.op("dve", "tensor_tensor", out=gi[i][:, 0:tw], in0=gi[i][:, 0:tw], in1=r[i][:, 0:tw], op=ALU.mult,
                          R=[f"gi{i}", f"r{i}"], W=[f"gi{i}"])
                    mk.op("pool", "tensor_tensor", out=bt[:, t0:t0 + tw], in0=gi[i][:, 0:tw], in1=u[:, t0:t0 + tw],
                          op=ALU.mult, R=[f"gi{i}", "u"], W=["bt"])
                if d == 0:
                    mk.op("dve", "tensor_tensor_scan", out=hf[:, :], data0=aa[:, :], data1=bt[:, :], initial=0.0,
                          op0=ALU.mult, op1=ALU.add, R=["aa", "bt"], W=["hf"])
                else:
                    mk.op("dve", "tensor_tensor_scan", out=hb[:, 0:256][:, ::-1], data0=aa[:, 0:256][:, ::-1],
                          data1=bt[:, 0:256][:, ::-1], initial=0.0, op0=ALU.mult, op1=ALU.add, R=["aa", "bt"], W=["hb"])
                    mk.op("dve", "tensor_tensor_scan", out=hb[:, 256:T][:, ::-1], data0=aa[:, 256:T][:, ::-1],
                          data1=bt[:, 256:T][:, ::-1], initial=hb[:, 0:1], op0=ALU.mult, op1=ALU.add,
                          R=["aa", "bt", "hb"], W=["hb"])
            mk.op("act", "activation", out=ag[:], in_=ag[:], func=AF.Gelu, R=["ag"], W=["ag"])
            mk.op("dve", "tensor_tensor", out=hf[:], in0=hf[:], in1=hb[:], op=ALU.add, R=["hf", "hb"], W=["hf"])
            mk.op("dve", "tensor_tensor", out=yb[:], in0=hf[:], in1=ag[:], op=ALU.mult, R=["hf", "ag"], W=["yb"])
            mk.dma("pool", YT[0, c], yb[:], R=["yb"], W=[f"YT0{c}"])
        mk.barrier()
        P.close()

    def order_of(dr):
        return list(range(NT)) if dr == 0 else [1, 0] + list(range(NT - 1, 1, -1))

    def chunk_core(it, nsub, dr, QT, KT, QIT, KHs, V, vw, Gc, S, Sbf, maskD, maskkey, rkeys, PT, sk):
        pi = it % 2
        L = 128 // nsub
        okeys = ["ps2", "ps3"]
        Oh = [PS[2 + hh][:, 0:2 * vw].rearrange("p (c e) -> p c e", c=2) for hh in range(2)]
        if not DBG["core"]:
            return okeys, Oh
        for h in DBG["heads"]:
            c, hh = h // 2, h % 2
            rs = slice(hh * 64, (hh + 1) * 64)
            mk.op("pe", "matmul", PS[hh][:, c * 128:(c + 1) * 128], lhsT=KT[c][rs, :], rhs=QT[c][rs, :], start=True,
                  stop=True, R=rkeys, W=[f"ps{hh}"])
        PTv = PT[pi][:].rearrange("p (c x n) -> p c x n", c=2, x=2)
        Mv = maskD.rearrange("p (c x n) -> p c x n", c=2, x=2)
        for hh in range(2):
            mk.op("dve", "tensor_tensor", out=PTv[:, :, hh, :], in0=PS[hh][:, 0:256].rearrange("p (c n) -> p c n", c=2),
                  in1=Mv[:, :, hh, :], op=ALU.mult, R=[f"ps{hh}", maskkey], W=[f"PT{pi}h{hh}"])
        ptk = [f"PT{pi}h0", f"PT{pi}h1"]
        subs = list(range(nsub)) if dr == 0 else list(range(nsub - 1, -1, -1))
        KVp = PS[4][:, 0:2 * vw].rearrange("p (c e) -> p c e", c=2)
        for s in subs:
            rows = slice(s * L, (s + 1) * L)
            for h in DBG["heads"]:
                c, hh = h // 2, h % 2
                rs = slice(hh * 64, (hh + 1) * 64)
                mk.op("pe", "matmul", Oh[hh][rows, c, :], lhsT=PT[pi][:, h * 128 + s * L:h * 128 + (s + 1) * L],
                      rhs=V[:, h, :], start=True, stop=False, R=[ptk[hh]] + rkeys, W=[okeys[hh]])
                mk.op("pe", "matmul", Oh[hh][rows, c, :], lhsT=QIT[c][rs, rows], rhs=Sbf[c][rs, :], start=False, stop=True,
                      R=rkeys + [f"{sk}Sbf{c}"], W=[okeys[hh]])
            for h in DBG["heads"]:
                c, hh = h // 2, h % 2
                mk.op("pe", "matmul", KVp[hh * 64:(hh + 1) * 64, c, :], lhsT=KHs(s)[:, h * 64:(h + 1) * 64],
                      rhs=V[:, h, :], start=True, stop=True, R=rkeys, W=["ps4kv"])
            for c in range(2):
                mk.op("dve", "scalar_tensor_tensor", out=S[c][:], in0=S[c][:], scalar=Gc[:, c, s:s + 1], in1=KVp[:, c, :],
                      op0=ALU.mult, op1=ALU.add, R=[f"{sk}S{c}", "ps4kv"] + rkeys, W=[f"{sk}S{c}"])
                mk.op("act", "activation", out=Sbf[c][:], in_=S[c][:], func=AF.Copy, R=[f"{sk}S{c}"], W=[f"{sk}Sbf{c}"])
        return okeys, Oh

    def hview(ap256, hh):
        return ap256.rearrange("p (c x e) -> p c x e", c=2, x=2)[:, :, hh, :]

    def make_finalize(P, n, yTb):
        st = dict(i=0)
        cent = [P.sb([128, 4, 64], F32, "cent") for _ in range(2)]
        sq = P.sb([128, 4, 64], F32, "sq")
        mm = [P.sb([128, 4], F32, "mm") for _ in range(2)]
        vv = [P.sb([128, 4], F32, "vv") for _ in range(2)]
        yy = [P.sb([128, 256], BF16, "yy") for _ in range(2)]

        def fin(tot, totkey, center, gate, gatekey, t):
            i = st["i"] % 2
            st["i"] += 1
            tv = tot.rearrange("p (h e) -> p h e", h=4)
            tk = list(totkey) if isinstance(totkey, (list, tuple)) else [totkey]
            src, skeys = tv, tk
            if center:
                mk.op("dve", "tensor_reduce", out=mm[i][:], in_=tv, axis=AX.X, op=ALU.add, R=tk, W=[f"fmm{i}"])
                mk.op("dve", "tensor_scalar", out=mm[i][:], in0=mm[i][:], scalar1=-1.0 / 64, scalar2=None, op0=ALU.mult,
                      R=[f"fmm{i}"], W=[f"fmm{i}"])
                mk.op("dve", "tensor_tensor", out=cent[i][:], in0=tv, in1=mm[i][:].unsqueeze(2).to_broadcast([128, 4, 64]),
                      op=ALU.add, R=tk + [f"fmm{i}"], W=[f"fcent{i}"])
                src, skeys = cent[i][:], [f"fcent{i}"]
            mk.op("pool", "tensor_tensor", out=sq[:], in0=src, in1=src, op=ALU.mult, R=skeys, W=["fsq"])
            mk.op("dve", "tensor_reduce", out=vv[i][:], in_=sq[:], axis=AX.X, op=ALU.add, R=["fsq"], W=[f"fvv{i}"])
            mk.op("act", "activation", out=vv[i][:], in_=vv[i][:], func=AF.Sqrt, scale=1.0 / 64, bias=EPS,
                  R=[f"fvv{i}"], W=[f"fvv{i}"])
            mk.op("dve", "reciprocal", out=vv[i][:], in_=vv[i][:], R=[f"fvv{i}"], W=[f"fvv{i}"])
            mk.op("dve", "tensor_tensor", out=cent[i][:], in0=src, in1=vv[i][:].unsqueeze(2).to_broadcast([128, 4, 64]),
                  op=ALU.mult, R=skeys + [f"fvv{i}"], W=[f"fcent{i}"])
            mk.op("dve", "tensor_tensor", out=yy[i][:], in0=cent[i][:].rearrange("p h e -> p (h e)"), in1=gate,
                  op=ALU.mult, R=[f"fcent{i}", gatekey], W=[f"fyy{i}"])
            for c in range(2):
                mk.op("pe", "transpose", out=PQ[1][:, (i * 2 + c) * 128:(i * 2 + c + 1) * 128],
                      in_=yy[i][:, c * 128:(c + 1) * 128], identity=identb[:], R=[f"fyy{i}", "identb"], W=[f"pq1f{i}"])
            mk.op("act", "activation", out=yTb[:, :, t * 128:(t + 1) * 128],
                  in_=PQ[1][:, i * 256:(i + 1) * 256].rearrange("p (c n) -> p c n", c=2), func=AF.Copy,
                  R=[f"pq1f{i}"], W=["yTb"])
        return fin

    def mixer_b(l):
        P = Pool(nc, f"mb{l}")
        QR = [P.sb([128, T], BF16, "QR") for _ in range(2)]
        KR = [P.sb([128, T], BF16, "KR") for _ in range(2)]
        thp = P.sb([128, 2, 2], F32, "thp")
        thh = P.sb([128, 2, 4], F32, "thh")
        mk.dma("sp", thp[:], b_thp[l], W=["thp"])
        mk.dma("sp", thh[:], b_thh[l], W=["thh"])
        for tt, key in ((thp, "thp"), (thh, "thh")):
            mk.op("act", "activation", out=tt[:], in_=tt[:], func=AF.Exp, scale=-1.0, R=[key], W=[key])
            mk.op("act", "activation", out=tt[:], in_=tt[:], func=AF.Ln, bias=1.0, R=[key], W=[key])
            mk.op("dve", "tensor_scalar", out=tt[:], in0=tt[:], scalar1=-1.0, scalar2=None, op0=ALU.mult, R=[key], W=[key])
        DM = P.sb([128, 2, 512], F32, "DM")
        QW = P.sb([128, 2, 2, 128], F32, "QW")
        KW = P.sb([128, 2, 4], F32, "KW")
        Gc = P.sb([128, 2, 2, 1], F32, "Gc")
        for dr in range(2):
            diff, msk, pos = (C("diffF"), C("maskF"), C("posF")) if dr == 0 else (C("diffB"), C("maskB"), C("posB"))
            for h in range(4):
                mk.op("act", "activation", out=DM[:, dr, h * 128:(h + 1) * 128], in_=diff, func=AF.Exp,
                      scale=thh[:, dr, h:h + 1], R=["cst", "thh"], W=["DM"])
                mk.op("dve", "tensor_tensor", out=DM[:, dr, h * 128:(h + 1) * 128], in0=DM[:, dr, h * 128:(h + 1) * 128],
                      in1=msk, op=ALU.mult, R=["DM", "cst"], W=["DM"])
                mk.op("act", "activation", out=KW[:, dr, h:h + 1], in_=C("kpos")[:, dr:dr + 1], func=AF.Exp,
                      scale=thh[:, dr, h:h + 1], R=["cst", "thh"], W=["KW"])
            for c in range(2):
                mk.op("act", "activation", out=QW[:, dr, c, :], in_=pos, func=AF.Exp, scale=thp[:, dr, c:c + 1],
                      R=["cst", "thp"], W=["QW"])
                mk.op("act", "activation", out=Gc[:, dr, c, :], in_=thp[:, dr, c:c + 1], func=AF.Exp, scale=128.0,
                      R=["thp"], W=["Gc"])
        segw = 1088
        f1 = [P.sb([128, segw], F32, "f1") for _ in range(2)]
        f2 = [P.sb([128, segw], F32, "f2") for _ in range(2)]
        rc = P.sb([128, T], F32, "rc")
        rsn = P.sb([128, T], F32, "rsn")
        mk.dma("sp", rc[:], ropeC_d, W=["rc"])
        mk.dma("sp", rsn[:], ropeS_d, W=["rsn"])
        n = 0
        for (dst, base, pbase, scale) in ((QR, "b_q", "b_qp", 1.0), (KR, "b_k", "b_kp", 0.125)):
            for c in range(2):
                for sg in range(4):
                    i = n % 2
                    n += 1
                    cs = slice(sg * segw, (sg + 1) * segw)
                    mk.dma("sp", f1[i][:], FM[FM_OFF[base] + c][:, cs], R=[f"FM{FM_OFF[base] + c}"], W=[f"f1{i}"])
                    mk.dma("sp", f2[i][:], FM[FM_OFF[pbase] + c][:, cs], R=[f"FM{FM_OFF[pbase] + c}"], W=[f"f2{i}"])
                    mk.op("dve", "tensor_tensor", out=f1[i][:], in0=f1[i][:], in1=rc[:, cs], op=ALU.mult,
                          R=[f"f1{i}", "rc"], W=[f"f1{i}"])
                    mk.op("pool", "tensor_tensor", out=f2[i][:], in0=f2[i][:], in1=rsn[:, cs], op=ALU.mult,
                          R=[f"f2{i}", "rsn"], W=[f"f2{i}"])
                    mk.op("dve", "tensor_tensor", out=f1[i][:], in0=f1[i][:], in1=f2[i][:], op=ALU.add,
                          R=[f"f1{i}", f"f2{i}"], W=[f"f1{i}"])
                    mk.op("act", "activation", out=dst[c][:, cs], in_=f1[i][:], func=AF.Copy, scale=scale,
                          R=[f"f1{i}"], W=[f"b{base}{c}"])
        rkeys = ["bb_q0", "bb_q1", "bb_k0", "bb_k1"]
        OF = P.sb([128, NT, 256], F32, "OF")
        yTb = P.sb([128, 2, T], BF16, "yTb")
        fin = make_finalize(P, 1, yTb)
        PT = [P.sb([128, 512], BF16, "PT") for _ in range(2)]
        QIT = [[P.sb([128, 128], BF16, "QIT") for _ in range(2)] for _ in range(2)]
        KH = [P.sb([128, 256], BF16, "KH") for _ in range(2)]
        Vf = [P.sb([128, 512], F32, "Vf") for _ in range(2)]
        Vb = [P.sb([128, 4, 64], BF16, "Vb") for _ in range(2)]
        gt = [P.sb([128, 256], F32, "gt") for _ in range(2)]
        tot = [P.sb([128, 256], F32, "tot") for _ in range(2)]
        S = [P.sb([128, 64], F32, "S") for _ in range(2)]
        Sbf = [P.sb([128, 64], BF16, "Sbf") for _ in range(2)]
        it = 0
        for dr in range(2):
            for c in range(2):
                mk.op("pool", "memset", S[c][:], 0.0, W=[f"bS{c}"])
                mk.op("pool", "memset", Sbf[c][:], 0.0, W=[f"bSbf{c}"])
            for t in order_of(dr):
                if DBG["maxit"] is not None and it >= DBG["maxit"]:
                    break
                i = it % 2
                cols = slice(t * 128, (t + 1) * 128)
                mk.dma("sp", Vf[i][:], TM[t * 128:(t + 1) * 128, 0:512], R=["TM"], W=[f"bVf{i}"])
                mk.op("pool", "tensor_copy", out=Vb[i][:], in_=Vf[i][:, 0:256].rearrange("p (h e) -> p h e", h=4),
                      R=[f"bVf{i}"], W=[f"bVb{i}"])
                for c in range(2):
                    mk.op("pool", "tensor_tensor", out=QIT[i][c][:], in0=QR[c][:, cols], in1=QW[:, dr, c, :], op=ALU.mult,
                          R=[f"bb_q{c}", "QW"], W=[f"bQIT{i}"])
                    mk.op("pe", "transpose", out=PQ[0][:, (i * 2 + c) * 128:(i * 2 + c + 1) * 128], in_=KR[c][:, cols],
                          identity=identb[:], R=[f"bb_k{c}", "identb"], W=[f"pq0k{i}"])
                for h in range(4):
                    mk.op("act", "activation", out=KH[i][:, h * 64:(h + 1) * 64],
                          in_=PQ[0][:, i * 256 + h * 64:i * 256 + (h + 1) * 64], func=AF.Identity, scale=KW[:, dr, h:h + 1],
                          R=[f"pq0k{i}", "KW"], W=[f"bKH{i}"])
                okeys, Oh = chunk_core(it, 1, dr, [QR[0][:, cols], QR[1][:, cols]], [KR[0][:, cols], KR[1][:, cols]],
                                       [QIT[i][0], QIT[i][1]], (lambda s_, kh=KH[i]: kh), Vb[i], 64, Gc[:, dr], S, Sbf,
                                       DM[:, dr, :], "DM", rkeys + [f"bQIT{i}", f"bKH{i}", f"bVb{i}"], PT, "b")
                if dr == 0:
                    for hh in range(2):
                        mk.op("act", "activation", out=hview(OF[:, t, :], hh), in_=Oh[hh], func=AF.Copy, R=[okeys[hh]],
                              W=[f"bOF{t}h{hh}"])
                else:
                    for hh in range(2):
                        mk.op("dve", "tensor_tensor", out=hview(tot[i][:], hh), in0=Oh[hh], in1=hview(OF[:, t, :], hh),
                              op=ALU.add, R=[okeys[hh], f"bOF{t}h{hh}"], W=[f"btot{i}h{hh}"])
                    mk.op("act", "activation", out=gt[i][:], in_=Vf[i][:, 256:512], func=AF.Silu, R=[f"bVf{i}"],
                          W=[f"bgt{i}"])
                    fin(tot[i][:], [f"btot{i}h0", f"btot{i}h1"], True, gt[i][:], f"bgt{i}", t)
                it += 1
        for c in range(2):
            mk.dma("pool", YT[1, c], yTb[:, c, :], R=["yTb"], W=[f"YT1{c}"])
        mk.barrier()
        P.close()

    def mixer_c(l):
        P = Pool(nc, f"mc{l}")
        QC = [P.sb([128, T], BF16, "QC") for _ in range(2)]
        KC = [P.sb([128, T], BF16, "KC") for _ in range(2)]
        cw = P.sb([128, 4, 4], F32, "cw")
        cb = P.sb([128, 4], F32, "cb")
        gbias = P.sb([128, 16], F32, "gbias")
        mk.dma("sp", cw[:], c_cw[l], W=["c_cw"])
        mk.dma("sp", cb[:], c_cb[l], W=["c_cb"])
        mk.dma("sp", gbias[:], c_gb[l], W=["c_gb"])
        src = P.sb([128, T], F32, "src")
        u = P.sb([128, T], F32, "u")
        for ch in range(4):
            fc = FM_OFF["c_q"] + ch
            mk.dma("sp", src[:], FM[fc], R=[f"FM{fc}"], W=["csrc"])
            conv_fm(u, src, cw[:, ch, :], cb[:, ch:ch + 1], "cu", "csrc", ["c_cw", "c_cb"])
            dst = QC[ch] if ch < 2 else KC[ch - 2]
            mk.op("act", "activation", out=u[:], in_=u[:], func=AF.Silu, R=["cu"], W=["cu"])
            mk.op("dve", "tensor_scalar", out=dst[:], in0=u[:], scalar1=(1.0 if ch < 2 else 0.125), scalar2=None,
                  op0=ALU.mult, R=["cu"], W=[f"cqk{ch}"])
        Z = P.sb([128, NT, 16], F32, "Z")
        LFN = P.sb([128, NT, 16], F32, "LFN")
        mk.dma("sp", Z[:], TM[:, TM_OFF["c_gates"]:TM_OFF["c_gates"] + 16].rearrange("(t p) g -> p t g", p=128),
               R=["TM"], W=["cZ"])
        mk.op("dve", "tensor_tensor", out=Z[:], in0=Z[:], in1=gbias[:].unsqueeze(1).to_broadcast([128, NT, 16]),
              op=ALU.add, R=["cZ", "c_gb"], W=["cZ"])
        mk.op("act", "activation", out=LFN[:], in_=Z[:], func=AF.Exp, scale=-1.0, R=["cZ"], W=["cLFN"])
        mk.op("act", "activation", out=LFN[:], in_=LFN[:], func=AF.Ln, bias=1.0, R=["cLFN"], W=["cLFN"])
        mk.op("dve", "tensor_scalar", out=LFN[:], in0=LFN[:], scalar1=-1.0, scalar2=None, op0=ALU.mult, R=["cLFN"],
              W=["cLFN"])
        rkeys = ["cqk0", "cqk1", "cqk2", "cqk3"]
        OF = P.sb([128, NT, 256], F32, "OF")
        yTb = P.sb([128, 2, T], BF16, "yTb")
        fin = make_finalize(P, 2, yTb)
        PT = [P.sb([128, 512], BF16, "PT") for _ in range(2)]
        QIT = [[P.sb([128, 128], BF16, "QIT") for _ in range(2)] for _ in range(2)]
        KH = [P.sb([128, 256], BF16, "KH") for _ in range(2)]
        Vf = [P.sb([128, 512], F32, "Vf") for _ in range(2)]
        Vb = [P.sb([128, 4, 65], BF16, "Vb") for _ in range(2)]
        gt = [P.sb([128, 256], F32, "gt") for _ in range(2)]
        tot = [P.sb([128, 256], F32, "tot") for _ in range(2)]
        S = [P.sb([128, 65], F32, "S") for _ in range(2)]
        Sbf = [P.sb([128, 65], BF16, "Sbf") for _ in range(2)]
        Bm4 = [P.sb([128, 4, 128], F32, "Bm4") for _ in range(2)]
        tmp4 = [P.sb([128, 512], F32, "tmp4") for _ in range(2)]
        Dm4 = [P.sb([128, 512], F32, "Dm4") for _ in range(2)]
        EB4 = [P.sb([128, 512], F32, "EB4") for _ in range(2)]
        lmb = [P.sb([128, 4], F32, "lmb") for _ in range(2)]
        kw = [P.sb([128, 4], F32, "kw") for _ in range(2)]
        Gc = [P.sb([128, 2, 1], F32, "Gc") for _ in range(2)]
        rden = [P.sb([128, 4], F32, "rden") for _ in range(2)]
        hid = [P.sb([128, 4, 64], F32, "hid") for _ in range(2)]
        for i in range(2):
            mk.op("pool", "memset", Vb[i][:], 1.0, W=[f"cVb{i}"])
        ones = C("ones")
        it = 0
        for dr in range(2):
            tri = C("triF") if dr == 0 else C("triB")
            neg4 = C("negF4") if dr == 0 else C("negB4")
            e = 127 if dr == 0 else 0
            for c in range(2):
                mk.op("pool", "memset", S[c][:], 0.0, W=[f"cS{c}"])
                mk.op("pool", "memset", Sbf[c][:], 0.0, W=[f"cSbf{c}"])
            for t in order_of(dr):
                i = it % 2
                cols = slice(t * 128, (t + 1) * 128)
                li = Z[:, t, dr * 8:dr * 8 + 4]
                lf = LFN[:, t, dr * 8 + 4:dr * 8 + 8]
                mk.dma("sp", Vf[i][:], TM[t * 128:(t + 1) * 128, 512:1024], R=["TM"], W=[f"cVf{i}"])
                mk.op("pool", "tensor_copy", out=Vb[i][:, :, 0:64], in_=Vf[i][:, 0:256].rearrange("p (h e) -> p h e", h=4),
                      R=[f"cVf{i}"], W=[f"cVb{i}"])
                mk.op("dve", "tensor_tensor", out=Bm4[i][:], in0=tri.unsqueeze(1).to_broadcast([128, 4, 128]),
                      in1=lf.unsqueeze(2).to_broadcast([128, 4, 128]), op=ALU.mult, R=["cst", "cLFN"], W=[f"cBm{i}"])
                mk.op("pe", "matmul", PS[5][:, :], lhsT=ones, rhs=Bm4[i][:].rearrange("p h n -> p (h n)"), start=True,
                      stop=True, R=["cst", f"cBm{i}"], W=["ps5"])
                mk.op("pe", "matmul", PS[4][:, 256:260], lhsT=tri, rhs=lf, start=True, stop=True, R=["cst", "cLFN"],
                      W=["ps4b"])
                mk.op("dve", "tensor_tensor", out=lmb[i][:], in0=li, in1=PS[4][:, 256:260], op=ALU.subtract,
                      R=["cZ", "ps4b"], W=[f"clmb{i}"])
                mk.op("dve", "tensor_tensor", out=tmp4[i][:], in0=PS[5][:, :], in1=neg4, op=ALU.add, R=["ps5", "cst"],
                      W=[f"ctmp{i}"])
                for h in range(4):
                    mk.op("act", "activation", out=Dm4[i][:, h * 128:(h + 1) * 128], in_=tmp4[i][:, h * 128:(h + 1) * 128],
                          func=AF.Exp, bias=lmb[i][:, h:h + 1], R=[f"ctmp{i}", f"clmb{i}"], W=[f"cDm{i}"])
                mk.op("act", "activation", out=EB4[i][:], in_=PS[5][:, :], func=AF.Exp, R=["ps5"], W=[f"cEB{i}"])
                bend = PS[5][:, :].rearrange("p (h n) -> p h n", h=4)[:, :, e]
                mk.op("dve", "tensor_tensor", out=kw[i][:], in0=lmb[i][:], in1=bend, op=ALU.add, R=[f"clmb{i}", "ps5"],
                      W=[f"ckw{i}"])
                mk.op("act", "activation", out=kw[i][:], in_=kw[i][:], func=AF.Exp, R=[f"ckw{i}"], W=[f"ckw{i}"])
                for h in range(4):
                    c, hh = h // 2, h % 2
                    rs = slice(hh * 64, (hh + 1) * 64)
                    mk.op("pool", "tensor_copy", out=Gc[i][rs, c, :], in_=EB4[i][rs, h * 128 + e:h * 128 + e + 1],
                          R=[f"cEB{i}"], W=[f"cGc{i}"])
                    mk.op("pool", "tensor_tensor", out=QIT[i][c][rs, :], in0=QC[c][rs, cols],
                          in1=EB4[i][rs, h * 128:(h + 1) * 128], op=ALU.mult, R=[f"cqk{c}", f"cEB{i}"], W=[f"cQIT{i}"])
                for c in range(2):
                    mk.op("pe", "transpose", out=PQ[0][:, (i * 2 + c) * 128:(i * 2 + c + 1) * 128], in_=KC[c][:, cols],
                          identity=identb[:], R=[f"cqk{2 + c}", "identb"], W=[f"pq0k{i}"])
                for h in range(4):
                    mk.op("act", "activation", out=KH[i][:, h * 64:(h + 1) * 64],
                          in_=PQ[0][:, i * 256 + h * 64:i * 256 + (h + 1) * 64], func=AF.Identity, scale=kw[i][:, h:h + 1],
                          R=[f"pq0k{i}", f"ckw{i}"], W=[f"cKH{i}"])
                okeys, Oh = chunk_core(it, 1, dr, [QC[0][:, cols], QC[1][:, cols]], [KC[0][:, cols], KC[1][:, cols]],
                                       [QIT[i][0], QIT[i][1]], (lambda s_, kh=KH[i]: kh), Vb[i], 65, Gc[i], S, Sbf,
                                       Dm4[i][:], f"cDm{i}",
                                       rkeys + [f"cQIT{i}", f"cKH{i}", f"cVb{i}", f"cGc{i}"], PT, "c")
                rdv = rden[i][:].rearrange("p (c x) -> p c x", c=2)
                for hh in range(2):
                    mk.op("act", "activation", out=rdv[:, :, hh], in_=Oh[hh][:, :, 64], func=AF.Abs, R=[okeys[hh]],
                          W=[f"crden{i}"])
                mk.op("dve", "tensor_scalar_max", out=rden[i][:], in0=rden[i][:], scalar1=1.0, R=[f"crden{i}"],
                      W=[f"crden{i}"])
                mk.op("dve", "reciprocal", out=rden[i][:], in_=rden[i][:], R=[f"crden{i}"], W=[f"crden{i}"])
                if dr == 0:
                    for hh in range(2):
                        mk.op("dve", "tensor_tensor", out=hview(OF[:, t, :], hh), in0=Oh[hh][:, :, 0:64],
                              in1=rdv[:, :, hh:hh + 1].to_broadcast([128, 2, 64]), op=ALU.mult,
                              R=[okeys[hh], f"crden{i}"], W=[f"cOF{t}h{hh}"])
                else:
                    for hh in range(2):
                        mk.op("dve", "tensor_tensor", out=hview(hid[i][:].rearrange("p h e -> p (h e)"), hh),
                              in0=Oh[hh][:, :, 0:64], in1=rdv[:, :, hh:hh + 1].to_broadcast([128, 2, 64]), op=ALU.mult,
                              R=[okeys[hh], f"crden{i}"], W=[f"chid{i}h{hh}"])
                    mk.op("pool", "tensor_tensor", out=tot[i][:], in0=hid[i][:].rearrange("p h e -> p (h e)"),
                          in1=OF[:, t, :], op=ALU.add, R=[f"chid{i}h0", f"chid{i}h1", f"cOF{t}h0", f"cOF{t}h1"],
                          W=[f"ctot{i}"])
                    mk.op("act", "activation", out=gt[i][:], in_=Vf[i][:, 256:512], func=AF.Sigmoid, R=[f"cVf{i}"],
                          W=[f"cgt{i}"])
                    fin(tot[i][:], f"ctot{i}", True, gt[i][:], f"cgt{i}", t)
                it += 1
        for c in range(2):
            mk.dma("pool", YT[2, c], yTb[:, c, :], R=["yTb"], W=[f"YT2{c}"])
        mk.barrier()
        P.close()

    def mixer_d(l):
        P = Pool(nc, f"md{l}")
        LB = P.sb([128, 256], F32, "LB")
        OML = P.sb([128, 256], F32, "OML")
        if l == 0:
            use_lb = False
        else:
            use_lb = True
            dl = P.sb([128, 2, 256], F32, "dl")
            mk.dma("sp", dl[:], d_lbr, W=["dl"])
            mk.op("dve", "tensor_tensor", out=LB[:], in0=dl[:, 1, :], in1=dl[:, 0, :], op=ALU.subtract, R=["dl"], W=["LB"])
            mk.op("act", "activation", out=LB[:], in_=LB[:], func=AF.Sigmoid, R=["LB"], W=["LB"])
            mk.op("dve", "tensor_scalar", out=OML[:], in0=LB[:], scalar1=-1.0, scalar2=1.0, op0=ALU.mult, op1=ALU.add,
                  R=["LB"], W=["OML"])
        OF = P.sb([128, NT, 256], F32, "OF")
        yTb = P.sb([128, 2, T], BF16, "yTb")
        fin = make_finalize(P, 3, yTb)
        assert DNS == 4
        PT = [P.sb([128, 512], BF16, "PT") for _ in range(2)]
        X = [P.sb([128, 1280], F32, "X") for _ in range(2)]
        ff = [P.sb([128, 256], F32, "ff") for _ in range(2)]
        lf = [P.sb([128, 256], F32, "lf") for _ in range(2)]
        kk = [P.sb([128, 256], F32, "kk") for _ in range(2)]
        qs = [P.sb([128, 256], F32, "qs") for _ in range(2)]
        ee = [P.sb([128, 512], F32, "ee") for _ in range(2)]
        ek = [P.sb([128, 256], F32, "ek") for _ in range(2)]
        qk = [P.sb([128, 512], BF16, "qk") for _ in range(2)]
        KTs = [P.sb([128, 2, 128], BF16, "KTs") for _ in range(2)]
        QM = [[P.sb([128, 2, 5, 128], BF16, "QM") for _ in range(2)] for _ in range(2)]
        KH = [P.sb([128, DNS, 256], BF16, "KH") for _ in range(2)]
        Vb = [P.sb([128, 4, 64], BF16, "Vb") for _ in range(2)]
        gt = [P.sb([128, 256], F32, "gt") for _ in range(2)]
        tot = [P.sb([128, 256], F32, "tot") for _ in range(2)]
        red = [P.sb([128, 2, 256], F32, "red") for _ in range(2)]
        Gc = [P.sb([128, 2, DNS], F32, "Gc") for _ in range(2)]
        S = [P.sb([128, 64], F32, "S") for _ in range(2)]
        Sbf = [P.sb([128, 64], BF16, "Sbf") for _ in range(2)]
        qmask = C("qmask").rearrange("p (x s n) -> p x s n", x=2, s=5)
        it = 0
        for dr in range(2):
            blk = C("blkF") if dr == 0 else C("blkB")
            rem = C("aftF") if dr == 0 else C("befB")
            msk4 = C("mblkF4") if dr == 0 else C("mblkB4")
            zoff = 256 if dr == 0 else 512
            subs = list(range(DNS)) if dr == 0 else list(range(DNS - 1, -1, -1))
            for c in range(2):
                mk.op("pool", "memset", S[c][:], 0.0, W=[f"dS{c}"])
                mk.op("pool", "memset", Sbf[c][:], 0.0, W=[f"dSbf{c}"])
            for t in order_of(dr):
                i = it % 2
                mk.dma("sp", X[i][:], TM[t * 128:(t + 1) * 128, 1024:2304], R=["TM"], W=[f"dX{i}"])
                mk.op("act", "activation", out=ff[i][:], in_=X[i][:, zoff:zoff + 256], func=AF.Sigmoid, R=[f"dX{i}"],
                      W=[f"dff{i}"])
                if use_lb:
                    mk.op("dve", "tensor_tensor", out=ff[i][:], in0=ff[i][:], in1=OML[:], op=ALU.mult, R=[f"dff{i}", "OML"],
                          W=[f"dff{i}"])
                    mk.op("dve", "tensor_tensor", out=ff[i][:], in0=ff[i][:], in1=LB[:], op=ALU.add, R=[f"dff{i}", "LB"],
                          W=[f"dff{i}"])
                mk.op("act", "activation", out=lf[i][:], in_=ff[i][:], func=AF.Ln, R=[f"dff{i}"], W=[f"dlf{i}"])
                mk.op("pool", "tensor_scalar", out=kk[i][:], in0=ff[i][:], scalar1=-1.0, scalar2=1.0, op0=ALU.mult,
                      op1=ALU.add, R=[f"dff{i}"], W=[f"dkk{i}"])
                mk.op("pe", "matmul", PS[5][:, 0:256], lhsT=blk, rhs=lf[i][:], start=True, stop=True, R=["cst", f"dlf{i}"],
                      W=["ps5"])
                mk.op("pe", "matmul", PS[5][:, 256:512], lhsT=rem, rhs=lf[i][:], start=True, stop=True,
                      R=["cst", f"dlf{i}"], W=["ps5"])
                for c in range(2):
                    mk.op("pe", "matmul", PS[1][:, 384 + c * DNS:384 + (c + 1) * DNS], lhsT=lf[i][:, c * 128:(c + 1) * 128],
                          rhs=C("subm"), start=True, stop=True, R=["cst", f"dlf{i}"], W=["ps1g"])
                mk.op("act", "activation", out=Gc[i][:].rearrange("p c s -> p (c s)"), in_=PS[1][:, 384:384 + 2 * DNS],
                      func=AF.Exp, R=["ps1g"], W=[f"dGc{i}"])
                mk.op("act", "activation", out=ee[i][:], in_=PS[5][:, :], func=AF.Exp, R=["ps5"], W=[f"dee{i}"])
                mk.op("act", "activation", out=ek[i][:], in_=PS[5][:, 0:256], func=AF.Exp, scale=-1.0, R=["ps5"],
                      W=[f"dek{i}"])
                mk.op("act", "activation", out=qs[i][:], in_=X[i][:, 0:256], func=AF.Silu, R=[f"dX{i}"], W=[f"dqs{i}"])
                mk.op("dve", "tensor_tensor", out=qk[i][:, 0:256], in0=qs[i][:], in1=ee[i][:, 0:256], op=ALU.mult,
                      R=[f"dqs{i}", f"dee{i}"], W=[f"dqk{i}a"])
                mk.op("dve", "tensor_tensor", out=qk[i][:, 256:512], in0=kk[i][:], in1=ek[i][:], op=ALU.mult,
                      R=[f"dkk{i}", f"dek{i}"], W=[f"dqk{i}c"])
                for s_ in range(DNS):
                    mk.op("dve", "scalar_tensor_tensor", out=KH[i][:, s_, :], in0=kk[i][:], scalar=C("subm")[:, s_:s_ + 1],
                          in1=ee[i][:, 256:512], op0=ALU.mult, op1=ALU.mult, R=[f"dkk{i}", f"dee{i}", "cst"],
                          W=[f"dKH{i}s{s_}"])
                mk.op("pool", "tensor_copy", out=Vb[i][:], in_=X[i][:, 768:1024].rearrange("p (h e) -> p h e", h=4),
                      R=[f"dX{i}"], W=[f"dVb{i}"])
                for j in range(4):
                    mk.op("pe", "transpose", out=PQ[0][:, (i * 4 + j) * 128:(i * 4 + j + 1) * 128],
                          in_=qk[i][:, j * 128:(j + 1) * 128], identity=identb[:], R=[f"dqk{i}a", f"dqk{i}c", "identb"],
                          W=[f"pq0d{i}"])
                mk.op("act", "activation", out=KTs[i][:].rearrange("p c n -> p (c n)"),
                      in_=PQ[0][:, i * 512 + 256:i * 512 + 512], func=AF.Copy, R=[f"pq0d{i}"], W=[f"dKT{i}"])
                for c in range(2):
                    mk.op("dve", "tensor_tensor", out=QM[i][c][:].rearrange("p x s n -> p (x s) n"),
                          in0=PQ[0][:, i * 512 + c * 128:i * 512 + (c + 1) * 128].unsqueeze(1).to_broadcast([128, 10, 128]),
                          in1=qmask.rearrange("p x s n -> p (x s) n"), op=ALU.mult, R=[f"pq0d{i}", "cst"],
                          W=[f"dQM{i}{c}"])
                for h in range(4):
                    c, hh = h // 2, h % 2
                    mk.op("pe", "matmul", PS[0][:, h * 128:(h + 1) * 128], lhsT=KTs[i][:, c, :], rhs=QM[i][c][:, hh, 4, :],
                          start=True, stop=True, R=[f"dKT{i}", f"dQM{i}{c}"], W=["ps0"])
                mk.op("dve", "tensor_tensor", out=PT[i][:], in0=PS[0][:, :], in1=msk4, op=ALU.mult, R=["ps0", "cst"],
                      W=[f"dPT{i}"])
                for h in range(4):
                    mk.op("pe", "matmul", PS[1][:, h * 64:(h + 1) * 64], lhsT=PT[i][:, h * 128:(h + 1) * 128],
                          rhs=Vb[i][:, h, :], start=True, stop=True, R=[f"dPT{i}", f"dVb{i}"], W=["ps1i"])
                KVp = PS[1][:, 256:384].rearrange("p (c e) -> p c e", c=2)
                for s_ in subs:
                    bank = PS[2 + s_ // 2]
                    for h in range(4):
                        c, hh = h // 2, h % 2
                        col = ((s_ % 2) * 4 + h) * 64
                        mk.op("pe", "matmul", bank[:, col:col + 64], lhsT=QM[i][c][:, hh, s_, :], rhs=Sbf[c][:, :],
                              start=True, stop=True, R=[f"dQM{i}{c}", f"dSbf{c}"], W=[f"ps{2 + s_ // 2}"])
                    for h in range(4):
                        c, hh = h // 2, h % 2
                        mk.op("pe", "matmul", KVp[hh * 64:(hh + 1) * 64, c, :], lhsT=KH[i][:, s_, h * 64:(h + 1) * 64],
                              rhs=Vb[i][:, h, :], start=True, stop=True, R=[f"dKH{i}s{s_}", f"dVb{i}"], W=["ps1kv"])
                    for c in range(2):
                        mk.op("dve", "scalar_tensor_tensor", out=S[c][:], in0=S[c][:], scalar=Gc[i][:, c, s_:s_ + 1],
                              in1=KVp[:, c, :], op0=ALU.mult, op1=ALU.add, R=[f"dS{c}", "ps1kv", f"dGc{i}"], W=[f"dS{c}"])
                        mk.op("act", "activation", out=Sbf[c][:], in_=S[c][:], func=AF.Copy, R=[f"dS{c}"], W=[f"dSbf{c}"])
                for b_ in range(2):
                    mk.op("dve", "tensor_reduce", out=red[i][:, b_, :],
                          in_=PS[2 + b_][:, :].rearrange("p (s x) -> p x s", s=2), axis=AX.X, op=ALU.add,
                          R=[f"ps{2 + b_}"], W=[f"dred{i}{b_}"])
                mk.op("dve", "tensor_tensor", out=tot[i][:], in0=PS[1][:, 0:256], in1=red[i][:, 0, :], op=ALU.add,
                      R=["ps1i", f"dred{i}0"], W=[f"dtot{i}"])
                if dr == 0:
                    mk.op("pool", "tensor_tensor", out=OF[:, t, :], in0=tot[i][:], in1=red[i][:, 1, :], op=ALU.add,
                          R=[f"dtot{i}", f"dred{i}1"], W=[f"dOF{t}"])
                else:
                    mk.op("pool", "tensor_tensor", out=tot[i][:], in0=tot[i][:], in1=red[i][:, 1, :], op=ALU.add,
                          R=[f"dtot{i}", f"dred{i}1"], W=[f"dtot{i}"])
                    mk.op("dve", "tensor_tensor", out=tot[i][:], in0=tot[i][:], in1=OF[:, t, :], op=ALU.add,
                          R=[f"dtot{i}", f"dOF{t}"], W=[f"dtot{i}"])
                    mk.op("act", "activation", out=gt[i][:], in_=X[i][:, 1024:1280], func=AF.Silu, R=[f"dX{i}"],
                          W=[f"dgt{i}"])
                    fin(tot[i][:], f"dtot{i}", False, gt[i][:], f"dgt{i}", t)
                it += 1
        for c in range(2):
            mk.dma("pool", YT[3, c], yTb[:, c, :], R=["yTb"], W=[f"YT3{c}"])
        mk.barrier()
        P.close()

    W1B = dscr("W1B", [16, 128, 4096], BF16)
    W3B = dscr("W3B", [16, 128, 4096], BF16)
    W2B = dscr("W2B", [16, 128, 4096], BF16)

    def merge_moe_phase(l, last, tiles=None):
        if tiles is None:
            tiles = list(range(2 if last else 0, NT))
        PO = Pool(nc, f"mo{l}")
        h2T = PO.sb([128, 8, T], BF16, "h2T")
        gates = PO.sb([128, NT, 16], F32, "gates")
        merge_part(l, tiles, h2T, gates)
        moe_part(l, tiles, h2T, gates)
        PO.close()

    def merge_part(l, tiles, h2T, gates):
        P = Pool(nc, f"mm{l}")
        wbr = P.sb([128, 8, D], BF16, "wbr")
        wo = P.sb([128, 8, D], BF16, "wo")
        wstage = P.sb([128, 8, 512], F32, "wstage")
        wcb = P.sb([128, 4096], BF16, "wcb")
        for half in range(2):
            mk.dma("sp", wstage[:], w_branch[l].rearrange("n (c p) f -> p (n c) f", p=128)[:, :, half * 512:(half + 1) * 512],
                   W=["wstage"])
            mk.ev(wbr[:, :, half * 512:(half + 1) * 512], wstage[:], R=["wstage"], W=["wbr"])
        for half in range(2):
            mk.dma("sp", wstage[:], w_out[l].rearrange("(c p) f -> p c f", p=128)[:, :, half * 512:(half + 1) * 512],
                   W=["wstage"])
            mk.ev(wo[:, :, half * 512:(half + 1) * 512], wstage[:], R=["wstage"], W=["wo"])
        tasks = []
        for e in range(16):
            tasks.append((moe_w1[l, e].rearrange("(c p) f -> p c f", p=128), wstage[:], W1B[e], f"W1B{e}"))
            tasks.append((moe_w3[l, e].rearrange("(c p) f -> p c f", p=128), wstage[:], W3B[e], f"W3B{e}"))
            tasks.append((moe_w2[l, e].rearrange("(c p) f -> p c f", p=128),
                          wstage[:].rearrange("p c f -> p (c f)").rearrange("p (c f) -> p c f", c=4), W2B[e], f"W2B{e}"))

        def do_task(k):
            src, stg, dst, key = tasks[k]
            mk.dma("sp", stg, src, W=["wstage"])
            mk.ev(wcb[:], wstage[:].rearrange("p c f -> p (c f)"), R=["wstage"], W=["wcb"])
            mk.dma("pool", dst, wcb[:], R=["wcb"], W=[key])
        wgr = P.sb([128, 8, 20], F32, "wgr")
        bgr = P.sb([1, 20], F32, "bgr")
        mk.dma("sp", wgr[:], moe_wgr[l].rearrange("(c p) f -> p c f", p=128), W=["wgr"])
        mk.dma("sp", bgr[:], moe_bgr[l], W=["bgr"])
        identf = C("ident")
        ones = C("ones")
        norm = make_norm(P, 1)
        xnew = [P.sb([128, D], F32, "xnew") for _ in range(2)]
        yt = [P.sb([128, 8, 128], BF16, "yt")] * 2
        mg = [P.sb([128, 4096], BF16, "mg")] * 2
        xt = [P.sb([128, D], F32, "xt")] * 2
        zz = P.sb([128, D], F32, "zz")
        zt = P.sb([128, D], F32, "zt")
        zb = P.sb([128, D], BF16, "zb")
        zT = P.sb([128, 8, 128], BF16, "zT")
        h2f = zz
        h2fT = zt[:, :].rearrange("p (c n) -> p c n", c=8)
        rt = {k: P.sb([128, w], F32, "rt" + k) for k, w in
              (("L", 20), ("gm", 1), ("goh", 4), ("ge", 4), ("gs", 1), ("el", 4), ("m1", 1), ("oh1", 4), ("e2", 4),
               ("m2", 1), ("oh2", 4), ("w1", 1), ("w2", 1), ("gw", 4))}
        nit = 0
        ntask = 0
        per_tile = -(-len(tasks) // len(tiles))
        for t in tiles:
            for _ in range(per_tile):
                if ntask < len(tasks):
                    do_task(ntask)
                    ntask += 1
            i = nit % 2
            nit += 1
            j = cond_of(t)
            cols = slice(t * 128, (t + 1) * 128)
            mk.dma("sp", yt[i][:], YT[:, :, :, cols].rearrange("n c p t -> p (n c) t"),
                   R=[f"YT{n}{c}" for n in range(4) for c in range(2)], W=["yt"])
            mk.dma("sp", mg[i][:], MG[cols, :], R=["MG"], W=["mg"])
            mk.dma("sp", xt[i][:], XR[cols, :], R=[f"XR{t}"], W=["mxt"])
            for n in range(4):
                for cb in range(2):
                    pb = (n * 2 + cb) % 2
                    for c in range(2):
                        mk.op("pe", "matmul", PS[pb][:, :], lhsT=yt[i][:, n * 2 + c, :],
                              rhs=wbr[:, n * 2 + c, cb * 512:(cb + 1) * 512], start=(c == 0), stop=(c == 1),
                              R=["yt", "wbr"], W=[f"ps{pb}"])
                    dst = zz if n == 0 else zt
                    dkey = "zz" if n == 0 else "zt"
                    mk.op("dve", "tensor_tensor", out=dst[:, cb * 512:(cb + 1) * 512], in0=PS[pb][:, :],
                          in1=mg[i][:, n * 1024 + cb * 512:n * 1024 + (cb + 1) * 512], op=ALU.mult,
                          R=[f"ps{pb}", "mg"], W=[dkey])
                    if n > 0:
                        mk.op("pool", "tensor_tensor", out=zz[:, cb * 512:(cb + 1) * 512],
                              in0=zz[:, cb * 512:(cb + 1) * 512], in1=zt[:, cb * 512:(cb + 1) * 512], op=ALU.add,
                              R=["zz", "zt"], W=["zz"])
            mk.op("act", "activation", out=zb[:], in_=zz[:], func=AF.Copy, R=["zz"], W=["zb"])
            for c in range(8):
                mk.op("pe", "transpose", out=PQ[1][:, c * 128:(c + 1) * 128], in_=zb[:, c * 128:(c + 1) * 128],
                      identity=identb[:], R=["zb", "identb"], W=["pq1"])
            mk.ev(zT[:].rearrange("p c n -> p (c n)"), PQ[1][:, :], R=["pq1"], W=["zT"])
            for cb in range(2):
                pb = 2 + cb
                for c in range(8):
                    mk.op("pe", "matmul", PS[pb][:, :], lhsT=zT[:, c, :], rhs=wo[:, c, cb * 512:(cb + 1) * 512],
                          start=(c == 0), stop=(c == 7), R=["zT", "wo"], W=[f"ps{pb}"])
                mk.op("dve", "tensor_tensor", out=zt[:, cb * 512:(cb + 1) * 512], in0=PS[pb][:, :],
                      in1=GB[:, j, 0, cb * 512:(cb + 1) * 512], op=ALU.mult, R=[f"ps{pb}", "GB"], W=["zt"])
                mk.op("pool", "tensor_tensor", out=xnew[i][:, cb * 512:(cb + 1) * 512], in0=zt[:, cb * 512:(cb + 1) * 512],
                      in1=xt[i][:, cb * 512:(cb + 1) * 512], op=ALU.add, R=["zt", "mxt"], W=[f"xnew{i}"])
            mk.dma("pool", XR[cols, :], xnew[i][:], R=[f"xnew{i}"], W=[f"XR{t}"])
            norm(xnew[i][:], f"xnew{i}", j, 3, 2, h2T[:, :, t * 128:(t + 1) * 128], f"h2T{t}")
            ssr = rt["gs"]
            mk.op("act", "activation", out=h2f[:], in_=xnew[i][:], func=AF.Square, accum_out=ssr[:],
                  R=[f"xnew{i}"], W=["zz", "rgs"])
            mk.op("act", "activation", out=ssr[:], in_=ssr[:], func=AF.Sqrt, scale=1.0 / D, bias=EPS, R=["rgs"], W=["rgs"])
            mk.op("dve", "reciprocal", out=ssr[:], in_=ssr[:], R=["rgs"], W=["rgs"])
            mk.op("dve", "tensor_scalar", out=h2f[:], in0=xnew[i][:], scalar1=ssr[:, 0:1], scalar2=None,
                  op0=ALU.mult, R=[f"xnew{i}", "rgs", "zz"], W=["zz"])
            for half in range(2):
                for c4 in range(4):
                    c = half * 4 + c4
                    mk.op("pe", "transpose", out=PS[5][:, c4 * 128:(c4 + 1) * 128], in_=h2f[:, c * 128:(c + 1) * 128],
                          identity=identf, R=["zz", "cst"], W=["ps5"])
                mk.op("dve", "tensor_tensor", out=h2fT[:, half * 4:(half + 1) * 4, :],
                      in0=PS[5][:, :].rearrange("p (c n) -> p c n", c=4),
                      in1=MODC[:, 3, half * 4:(half + 1) * 4, j:j + 1].to_broadcast([128, 4, 128]), op=ALU.mult,
                      R=["ps5", "MODC"], W=["zt"])
                mk.op("pool", "tensor_tensor", out=h2fT[:, half * 4:(half + 1) * 4, :],
                      in0=h2fT[:, half * 4:(half + 1) * 4, :],
                      in1=MODC[:, 2, half * 4:(half + 1) * 4, j:j + 1].to_broadcast([128, 4, 128]), op=ALU.add,
                      R=["zt", "MODC"], W=["zt"])
            for c in range(8):
                mk.op("pe", "matmul", PS[4][:, 0:20], lhsT=h2fT[:, c, :], rhs=wgr[:, c, :], start=(c == 0), stop=False,
                      R=["zt", "wgr"], W=["ps4r"])
            mk.op("pe", "matmul", PS[4][:, 0:20], lhsT=ones[0:1, :], rhs=bgr[0:1, :], start=False, stop=True,
                  R=["cst", "bgr"], W=["ps4r"])
            Lg = rt["L"]
            mk.op("act", "activation", out=Lg[:], in_=PS[4][:, 0:20], func=AF.Copy, R=["ps4r"], W=["rL"])
            rk = ["rL"]
            mk.op("dve", "tensor_reduce", out=rt["gm"][:], in_=Lg[:, 0:4], axis=AX.X, op=ALU.max, R=rk, W=["rgm"])
            mk.op("dve", "tensor_scalar", out=rt["goh"][:], in0=Lg[:, 0:4], scalar1=rt["gm"][:, 0:1], scalar2=None,
                  op0=ALU.is_ge, R=rk + ["rgm"], W=["rgoh"])
            mk.op("dve", "tensor_scalar", out=rt["ge"][:], in0=Lg[:, 0:4], scalar1=rt["gm"][:, 0:1], scalar2=None,
                  op0=ALU.subtract, R=rk + ["rgm"], W=["rge"])
            mk.op("act", "activation", out=rt["ge"][:], in_=rt["ge"][:], func=AF.Exp, accum_out=rt["gs"][:],
                  R=["rge"], W=["rge", "rgs"])
            mk.op("dve", "reciprocal", out=rt["gs"][:], in_=rt["gs"][:], R=["rgs"], W=["rgs"])
            mk.op("dve", "tensor_scalar", out=rt["el"][:], in0=Lg[:, 4:8], scalar1=rt["goh"][:, 0:1], scalar2=None,
                  op0=ALU.mult, R=rk + ["rgoh"], W=["rel"])
            for g in range(1, 4):
                mk.op("dve", "scalar_tensor_tensor", out=rt["el"][:], in0=Lg[:, 4 + g * 4:8 + g * 4],
                      scalar=rt["goh"][:, g:g + 1], in1=rt["el"][:], op0=ALU.mult, op1=ALU.add,
                      R=rk + ["rgoh", "rel"], W=["rel"])
            mk.op("dve", "tensor_reduce", out=rt["m1"][:], in_=rt["el"][:], axis=AX.X, op=ALU.max, R=["rel"], W=["rm1"])
            mk.op("dve", "tensor_scalar", out=rt["oh1"][:], in0=rt["el"][:], scalar1=rt["m1"][:, 0:1], scalar2=None,
                  op0=ALU.is_ge, R=["rel", "rm1"], W=["roh1"])
            mk.op("dve", "scalar_tensor_tensor", out=rt["e2"][:], in0=rt["oh1"][:], scalar=-1e30, in1=rt["el"][:],
                  op0=ALU.mult, op1=ALU.add, R=["roh1", "rel"], W=["re2"])
            mk.op("dve", "tensor_reduce", out=rt["m2"][:], in_=rt["e2"][:], axis=AX.X, op=ALU.max, R=["re2"], W=["rm2"])
            mk.op("dve", "tensor_scalar", out=rt["oh2"][:], in0=rt["e2"][:], scalar1=rt["m2"][:, 0:1], scalar2=None,
                  op0=ALU.is_ge, R=["re2", "rm2"], W=["roh2"])
            mk.op("dve", "tensor_tensor", out=rt["w1"][:], in0=rt["m2"][:], in1=rt["m1"][:], op=ALU.subtract,
                  R=["rm1", "rm2"], W=["rw1"])
            mk.op("act", "activation", out=rt["w1"][:], in_=rt["w1"][:], func=AF.Exp, R=["rw1"], W=["rw1"])
            mk.op("dve", "tensor_scalar_add", out=rt["w1"][:], in0=rt["w1"][:], scalar1=1.0, R=["rw1"], W=["rw1"])
            mk.op("dve", "reciprocal", out=rt["w1"][:], in_=rt["w1"][:], R=["rw1"], W=["rw1"])
            mk.op("dve", "tensor_tensor", out=rt["w1"][:], in0=rt["w1"][:], in1=rt["gs"][:], op=ALU.mult,
                  R=["rw1", "rgs"], W=["rw1"])
            mk.op("dve", "tensor_tensor", out=rt["w2"][:], in0=rt["gs"][:], in1=rt["w1"][:], op=ALU.subtract,
                  R=["rw1", "rgs"], W=["rw2"])
            mk.op("dve", "tensor_scalar", out=rt["gw"][:], in0=rt["oh1"][:], scalar1=rt["w1"][:, 0:1], scalar2=None,
                  op0=ALU.mult, R=["roh1", "rw1"], W=["rgw"])
            mk.op("dve", "scalar_tensor_tensor", out=rt["gw"][:], in0=rt["oh2"][:], scalar=rt["w2"][:, 0:1],
                  in1=rt["gw"][:], op0=ALU.mult, op1=ALU.add, R=["roh2", "rw2", "rgw"], W=["rgw"])
            for g in range(4):
                mk.op("dve", "tensor_scalar", out=gates[:, t, g * 4:(g + 1) * 4], in0=rt["gw"][:],
                      scalar1=rt["goh"][:, g:g + 1], scalar2=None, op0=ALU.mult, R=["rgw", "rgoh"], W=[f"gates{t}"])
        while ntask < len(tasks):
            do_task(ntask)
            ntask += 1
        mk.barrier()
        P.close()

    def moe_part(l, tiles, h2T, gates):
        P = Pool(nc, f"me{l}")
        TB = 8
        blocks = [(tiles[k], min(TB, len(tiles) - k)) for k in range(0, len(tiles), TB)]
        acc = P.sb([128, TB, D], F32, "acc")
        xt = [P.sb([128, D], F32, "xt") for _ in range(2)]
        w1b = [P.sb([128, 8, 512], BF16, "w1b") for _ in range(2)]
        w3b = [P.sb([128, 8, 512], BF16, "w3b") for _ in range(2)]
        w2b = [P.sb([128, 4, D], BF16, "w2b") for _ in range(2)]
        sl = [P.sb([128, 512], F32, "sl") for _ in range(2)]
        actT = [P.sb([128, 4, 512], BF16, "actT") for _ in range(2)]
        nw = 0
        for (tb0, tn) in blocks:
            ntok = tn * 128
            sblocks = [(s0, min(512, ntok - s0)) for s0 in range(0, ntok, 512)]
            hk = [f"h2T{tb0 + tt}" for tt in range(tn)]
            for e in range(16):
                wi = nw % 2
                nw += 1
                mk.dma("sp", w1b[wi][:].rearrange("p c f -> p (c f)"), W1B[e], R=[f"W1B{e}"], W=[f"w1b{wi}"])
                mk.dma("sp", w3b[wi][:].rearrange("p c f -> p (c f)"), W3B[e], R=[f"W3B{e}"], W=[f"w3b{wi}"])
                mk.dma("sp", w2b[wi][:].rearrange("p c f -> p (c f)"), W2B[e], R=[f"W2B{e}"], W=[f"w2b{wi}"])
                for (s0, sw) in sblocks:
                    ai = (s0 // 512) % 2
                    h0 = tb0 * 128 + s0
                    for fcn in range(4):
                        for k in range(8):
                            mk.op("pe", "matmul", PS[0][:, 0:sw], lhsT=w1b[wi][:, k, fcn * 128:(fcn + 1) * 128],
                                  rhs=h2T[:, k, h0:h0 + sw], start=(k == 0), stop=(k == 7), R=[f"w1b{wi}"] + hk, W=["ps0"])
                        for k in range(8):
                            mk.op("pe", "matmul", PS[1][:, 0:sw], lhsT=w3b[wi][:, k, fcn * 128:(fcn + 1) * 128],
                                  rhs=h2T[:, k, h0:h0 + sw], start=(k == 0), stop=(k == 7), R=[f"w3b{wi}"] + hk, W=["ps1"])
                        si = fcn % 2
                        mk.op("act", "activation", out=sl[si][:, 0:sw], in_=PS[0][:, 0:sw], func=AF.Silu, R=["ps0"],
                              W=[f"sl{si}"])
                        mk.op("dve", "tensor_tensor", out=actT[ai][:, fcn, 0:sw], in0=sl[si][:, 0:sw], in1=PS[1][:, 0:sw],
                              op=ALU.mult, R=[f"sl{si}", "ps1"], W=[f"actT{ai}"])
                    for q in range(sw // 128):
                        tt = s0 // 128 + q
                        t = tb0 + tt
                        for cb in range(2):
                            pb = 2 + cb
                            for fcn in range(4):
                                mk.op("pe", "matmul", PS[pb][:, :], lhsT=actT[ai][:, fcn, q * 128:(q + 1) * 128],
                                      rhs=w2b[wi][:, fcn, cb * 512:(cb + 1) * 512], start=(fcn == 0), stop=(fcn == 3),
                                      R=[f"actT{ai}", f"w2b{wi}"], W=[f"ps{pb}"])
                            if e == 0:
                                mk.op("dve", "tensor_scalar", out=acc[:, tt, cb * 512:(cb + 1) * 512], in0=PS[pb][:, :],
                                      scalar1=gates[:, t, e:e + 1], scalar2=None, op0=ALU.mult,
                                      R=[f"ps{pb}", f"gates{t}"], W=[f"acc{tt}"])
                            else:
                                mk.op("dve", "scalar_tensor_tensor", out=acc[:, tt, cb * 512:(cb + 1) * 512],
                                      in0=PS[pb][:, :], scalar=gates[:, t, e:e + 1], in1=acc[:, tt, cb * 512:(cb + 1) * 512],
                                      op0=ALU.mult, op1=ALU.add, R=[f"ps{pb}", f"gates{t}", f"acc{tt}"], W=[f"acc{tt}"])
            for tt in range(tn):
                t = tb0 + tt
                j = cond_of(t)
                mk.op("pool", "tensor_tensor", out=acc[:, tt, :], in0=acc[:, tt, :], in1=GB[:, j, 1, :], op=ALU.mult,
                      R=[f"acc{tt}", "GB"], W=[f"acc{tt}"])
                i2 = tt % 2
                mk.dma("sp", xt[i2][:], XR[t * 128:(t + 1) * 128, :], R=[f"XR{t}"], W=[f"ext{i2}"])
                mk.op("dve", "tensor_tensor", out=acc[:, tt, :], in0=acc[:, tt, :], in1=xt[i2][:], op=ALU.add,
                      R=[f"acc{tt}", f"ext{i2}"], W=[f"acc{tt}"])
                mk.dma("pool", XR[t * 128:(t + 1) * 128, :], acc[:, tt, :], R=[f"acc{tt}"], W=[f"XR{t}"])
        mk.barrier()
        P.close()

    def final_phase():
        P = Pool(nc, "fin")
        fw = P.sb([128, D], F32, "fw")
        mk.dma("sp", fw[:], fnw, W=["fw"])
        xt = [P.sb([128, D], F32, "xt") for _ in range(2)]
        ot = [P.sb([128, D], F32, "ot") for _ in range(2)]
        junk = P.sb([128, D], BF16, "junk")
        ss = [P.sb([128, 1], F32, "ss") for _ in range(2)]
        for t in range(2, NT):
            i = t % 2
            mk.dma("sp", xt[i][:], XR[t * 128:(t + 1) * 128, :], R=[f"XR{t}"], W=[f"fxt{i}"])
            mk.op("act", "activation", out=junk[:], in_=xt[i][:], func=AF.Square, accum_out=ss[i][:], R=[f"fxt{i}"],
                  W=["fjunk", f"fss{i}"])
            mk.op("act", "activation", out=ss[i][:], in_=ss[i][:], func=AF.Sqrt, scale=1.0 / D, bias=EPS, R=[f"fss{i}"],
                  W=[f"fss{i}"])
            mk.op("dve", "reciprocal", out=ss[i][:], in_=ss[i][:], R=[f"fss{i}"], W=[f"fss{i}"])
            mk.op("dve", "scalar_tensor_tensor", out=ot[i][:], in0=xt[i][:], scalar=ss[i][:, 0:1], in1=fw[:], op0=ALU.mult,
                  op1=ALU.mult, R=[f"fxt{i}", f"fss{i}", "fw"], W=[f"fot{i}"])
            mk.dma("pool", yout[(t - 2) * 128:(t - 1) * 128, :], ot[i][:], R=[f"fot{i}"], W=["yout"])
        mk.barrier()
        P.close()

    stages = dict(mod=mod_phase, inproj=inproj_phase, a=mixer_a, b=mixer_b, c=mixer_c, d=mixer_d)
    return dict(nc=nc, mk=mk, stages=stages, merge=merge_moe_phase, final=final_phase, dbg=dbg,
                scr=dict(XR=XR, FM=FM, TM=TM, MG=MG, YT=YT))


def emit_all(prog, layers=NL, upto=None, skip=()):
    mk = prog["mk"]
    mk.barrier()
    done = False
    for l in range(layers):
        for s in ("mod", "inproj", "a", "b", "c", "d"):
            if s in skip:
                continue
            prog["stages"][s](l)
            if upto == (l, s):
                done = True
                break
        if done:
            break
        prog["merge"](l, l == NL - 1)
        if upto == (l, "merge"):
            done = True
            break
    if not done:
        prog["final"]()
    mk.barrier(engines=("sp",))


def _consts():
    j = np.arange(128)[:, None]
    i = np.arange(128)[None, :]
    same = (j // DL) == (i // DL)
    m = {}
    m["ident"] = (j == i)
    m["triF"] = (j <= i)
    m["triB"] = (j >= i)
    m["blkF"] = same & (j <= i)
    m["blkB"] = same & (j >= i)
    m["aftF"] = same & (j > i)
    m["befB"] = same & (j < i)
    m["diffF"] = np.maximum(i - j, 0)
    m["diffB"] = np.maximum(j - i, 0)
    m["maskF"] = (i >= j)
    m["maskB"] = (j > i)
    m["posF"] = np.broadcast_to(i + 1, (128, 128))
    m["posB"] = np.broadcast_to(128 - i, (128, 128))
    m["negF4"] = np.tile(np.where(j <= i, 0.0, -30000.0), (1, 4))
    m["negB4"] = np.tile(np.where(j >= i, 0.0, -30000.0), (1, 4))
    m["mblkF4"] = np.tile(same & (j <= i), (1, 4))
    m["mblkB4"] = np.tile(same & (j >= i), (1, 4))
    m["kpos"] = np.concatenate([127 - j, j], 1)
    m["ones"] = np.ones((128, 128))
    sel = np.zeros((128, 256))
    sel[0, 0:128] = 1.0
    sel[1, 128:256] = 1.0
    m["sel"] = sel
    m["subm"] = np.concatenate([(j // DL) == s_ for s_ in range(128 // DL)], 1)
    qm = np.zeros((128, 2, 5, 128), np.float32)
    for hh_ in range(2):
        for s_ in range(5):
            colsel = np.ones(128, bool) if s_ == 4 else (np.arange(128) // DL == s_)
            qm[hh_ * 64:(hh_ + 1) * 64, hh_, s_, :] = colsel[None, :]
    m["qmask"] = qm.reshape(128, 1280)
    out = np.zeros((128, NCST), np.float32)
    for k, (o, w) in CST.items():
        out[:, o:o + w] = np.asarray(m[k], np.float32)
    return out


def _rope_tables():
    n = 16
    inv = np.power(np.float32(10000.0), -np.arange(n, dtype=np.float32) / n).astype(np.float32)
    t = np.arange(4096)
    row = (t // 64).astype(np.float32)
    col = (t % 64).astype(np.float32)
    ang = np.concatenate([row[:, None] * inv, col[:, None] * inv], -1)
    cos = np.cos(ang).astype(np.float32).T
    sin = np.sin(ang).astype(np.float32).T
    Cc = np.ones((128, T), np.float32)
    Ss = np.zeros((128, T), np.float32)
    for hh in range(2):
        Cc[hh * 64:hh * 64 + 32, 256:] = cos
        Cc[hh * 64 + 32:hh * 64 + 64, 256:] = cos
        Ss[hh * 64:hh * 64 + 32, 256:] = -sin
        Ss[hh * 64 + 32:hh * 64 + 64, 256:] = sin
    return Cc, Ss


def prep_shared(inp):
    f = np.float32
    w_in = np.asarray(inp["w_in"], f)
    offs = {}
    o = 0
    for name, w in (("a_x", 256), ("a_g", 256), ("b_q", 256), ("b_k", 256), ("b_v", 256), ("b_g", 256), ("c_q", 256),
                    ("c_k", 256), ("c_v", 256), ("c_o", 256), ("c_gates", 16), ("d_q", 256), ("d_ff", 256),
                    ("d_fb", 256), ("d_i", 256), ("d_g", 256), ("merge", 4096)):
        offs[name] = (o, w)
        o += w

    def cols(n):
        a, w = offs[n]
        return w_in[:, :, a:a + w]

    perm = np.concatenate([np.arange(h * 64 + 32, h * 64 + 64).tolist() + np.arange(h * 64, h * 64 + 32).tolist()
                           for h in range(4)]).astype(np.int64)
    w_fm = np.concatenate([cols("a_x"), cols("a_g"), cols("b_q"), cols("b_q")[:, :, perm], cols("b_k"),
                           cols("b_k")[:, :, perm], cols("c_q"), cols("c_k")], -1)
    w_tm = np.concatenate([cols("b_v"), cols("b_g"), cols("c_v"), cols("c_o"), cols("d_q"), cols("d_ff"), cols("d_fb"),
                           cols("d_i"), cols("d_g"), cols("c_gates"), cols("merge")], -1)
    sh = {}
    sh["w_mod"] = np.ascontiguousarray(inp["w_mod"], f)
    bm = np.asarray(inp["b_mod"], f)
    sh["bmod_c"] = np.ascontiguousarray(bm.reshape(NL, 48, 128).transpose(0, 2, 1))
    sh["bmod_r"] = np.ascontiguousarray(bm.reshape(NL, 1, 6144))
    sh["w_fm"] = np.ascontiguousarray(w_fm)
    sh["w_tm"] = np.ascontiguousarray(w_tm)
    acw = np.asarray(inp["a_conv_w"], f)
    sh["a_cw"] = np.ascontiguousarray(acw.reshape(NL, 4, 2, 128).transpose(0, 3, 2, 1))
    sh["a_cb"] = np.ascontiguousarray(np.asarray(inp["a_conv_b"], f).reshape(NL, 2, 128).transpose(0, 2, 1))
    gw = np.asarray(inp["a_gate_w"], f)
    agw = np.zeros((NL, 128, 2, 2, 2, 128), f)
    for c in range(2):
        for hh in range(2):
            agw[:, hh * 64:(hh + 1) * 64, :, :, c, hh * 64:(hh + 1) * 64] = gw[:, :, :, 2 * c + hh].transpose(0, 3, 1, 2, 4)
    sh["a_gw"] = agw
    gb = np.asarray(inp["a_gate_b"], f)
    sh["a_gb"] = np.ascontiguousarray(gb.reshape(NL, 2, 2, 2, 128).transpose(0, 4, 1, 2, 3))
    lam = np.asarray(inp["a_lambda"], f)
    sh["a_lam"] = np.ascontiguousarray(lam.reshape(NL, 2, 2, 128).transpose(0, 3, 1, 2))
    th = np.asarray(inp["b_theta"], f)
    thp = np.zeros((NL, 128, 2, 2), f)
    for c in range(2):
        for hh in range(2):
            thp[:, hh * 64:(hh + 1) * 64, :, c] = th[:, None, :, 2 * c + hh]
    sh["b_thp"] = thp
    sh["b_thh"] = np.ascontiguousarray(np.broadcast_to(th[:, None], (NL, 128, 2, 4)))
    ccw = np.asarray(inp["c_conv_w"], f)
    sh["c_cw"] = np.ascontiguousarray(ccw.reshape(NL, 4, 4, 128).transpose(0, 3, 2, 1))
    sh["c_cb"] = np.ascontiguousarray(np.asarray(inp["c_conv_b"], f).reshape(NL, 4, 128).transpose(0, 2, 1))
    sh["c_gb"] = np.ascontiguousarray(np.broadcast_to(np.asarray(inp["c_gate_b"], f).reshape(NL, 1, 16), (NL, 128, 16)))
    sh["d_lbr"] = np.ascontiguousarray(np.broadcast_to(np.asarray(inp["d_lb"], f)[None], (128, 2, 256)))
    sh["w_branch"] = np.ascontiguousarray(inp["w_branch"], f)
    sh["w_out"] = np.ascontiguousarray(inp["w_out"], f)
    sh["moe_wgr"] = np.ascontiguousarray(np.concatenate([np.asarray(inp["moe_w_group"], f), np.asarray(inp["moe_w_router"], f)], -1))
    sh["moe_bgr"] = np.ascontiguousarray(np.concatenate([np.asarray(inp["moe_b_group"], f), np.asarray(inp["moe_b_router"], f)], -1).reshape(NL, 1, 20))
    sh["moe_w1"] = np.ascontiguousarray(inp["moe_w1"], f)
    sh["moe_w3"] = np.ascontiguousarray(inp["moe_w3"], f)
    sh["moe_w2"] = np.ascontiguousarray(inp["moe_w2"], f)
    sh["fnw"] = np.ascontiguousarray(np.broadcast_to(np.asarray(inp["final_norm_w"], f)[None], (128, D)))
    sh["cst"] = _consts()
    sh["ropeC"], sh["ropeS"] = _rope_tables()
    return sh


def prep_core(inp, b):
    f = np.float32
    d = {}
    d["xin"] = np.ascontiguousarray(np.concatenate([np.asarray(inp["ctx"][b], f), np.asarray(inp["x"][b], f)], 0))
    cv = np.stack([np.asarray(inp["c_ctx"], f), np.asarray(inp["c"][b], f)], -1)
    d["cvec"] = np.ascontiguousarray(cv.reshape(8, 128, 2).transpose(1, 0, 2))
    return d


_PROG = None


def kernel(**inputs):
    global _PROG
    if _PROG is None:
        _PROG = build_program()
        emit_all(_PROG)
    nc = _PROG["nc"]
    sh = prep_shared(inputs)
    in_maps = []
    for core in range(8):
        m = dict(sh)
        m.update(prep_core(inputs, core % 4))
        in_maps.append(m)
    res = run_bass_kernel_spmd(nc, in_maps, core_ids=list(range(8)))
    out = np.stack([np.asarray(res.results[b]["yout"], np.float32) for b in range(4)], 0)
    return out
```

```python
import contextlib
import numpy as np
import ml_dtypes
import concourse.bass as bass
import concourse.mybir as mybir
from concourse.bass_utils import run_bass_kernel_spmd

F32 = mybir.dt.float32
BF16 = mybir.dt.bfloat16
AF = mybir.ActivationFunctionType
ALU = mybir.AluOpType
AX = mybir.AxisListType

T = 4352
NT = 34
D = 1024
EPS = 1e-6
NL = 2
HALF_OUT = 2048
DL = 32
DNS = 128 // DL
DBG = dict(maxit=None, core=True, fin=True, heads=(0, 1, 2, 3))
TMW = 2320
TM_OFF = dict(b_v=0, b_g=256, c_v=512, c_o=768, d_q=1024, d_ff=1280, d_fb=1536, d_i=1792, d_g=2048, c_gates=2304)
FM_OFF = dict(a_x=0, a_g=2, b_q=4, b_qp=6, b_k=8, b_kp=10, c_q=12, c_k=14)

CST = {}
_off = 0
for _n, _w in (("ident", 128), ("triF", 128), ("triB", 128), ("blkF", 128), ("blkB", 128), ("aftF", 128),
               ("befB", 128), ("diffF", 128), ("diffB", 128), ("maskF", 128), ("maskB", 128), ("posF", 128),
               ("posB", 128), ("negF4", 512), ("negB4", 512), ("mblkF4", 512), ("mblkB4", 512), ("kpos", 2),
               ("ones", 128), ("sel", 256), ("subm", 4), ("qmask", 1280)):
    CST[_n] = (_off, _w)
    _off += _w
NCST = _off


class MK:
    SEM_ROT = 30000

    def __init__(self, nc, ndma=8):
        self.nc = nc
        self.engs = {"pe": nc.tensor, "act": nc.scalar, "dve": nc.vector, "pool": nc.gpsimd, "sp": nc.sync}
        self._ctxs = []
        self.nsem = 0
        self.sem = {}
        self.cnt = {}
        for e in ("pe", "act", "dve", "pool"):
            self.sem[e] = self._newsem("s_" + e)
            self.cnt[e] = 0
        self.seen = {e: {} for e in self.engs}
        self.dq = {}
        for q in ("sp", "pool"):
            self.dq[q] = {"i": 0, "slots": [[self._newsem(f"d_{q}{i}"), 0] for i in range(ndma)]}
        self.res = {}
        self.ninst = 0
        self.flip = 0

    def _newsem(self, name):
        self.nsem += 1
        cm = self.nc.semaphore(f"{name}_{self.nsem}")
        s = cm.__enter__()
        self._ctxs.append(cm)
        return s

    def _wait(self, eng, tok):
        sem, val = tok
        key = id(sem)
        if self.seen[eng].get(key, 0) >= val:
            return
        self.engs[eng].wait_ge(sem, val)
        self.seen[eng][key] = val

    def _deps(self, R, W):
        deps = []
        for k in R:
            st = self.res.get(k)
            if st and st[0] is not None:
                deps.append(st[0])
        for k in W:
            st = self.res.get(k)
            if st:
                if st[0] is not None:
                    deps.append(st[0])
                deps.extend(st[1])
        return deps

    def _record(self, tok, R, W):
        for k in R:
            st = self.res.setdefault(k, [None, []])
            st[1] = [t for t in st[1] if t[0] is not tok[0]] + [tok]
        for k in W:
            self.res[k] = [tok, []]

    def op(self, eng, method, *args, R=(), W=(), **kw):
        for tok in self._deps(R, W):
            if eng == "pe" and tok[0] is self.sem["pe"]:
                continue
            self._wait(eng, tok)
        ins = getattr(self.engs[eng], method)(*args, **kw)
        if self.cnt[eng] >= self.SEM_ROT:
            self.sem[eng] = self._newsem("s_" + eng)
            self.cnt[eng] = 0
        self.cnt[eng] += 1
        ins.then_inc(self.sem[eng], 1)
        tok = (self.sem[eng], self.cnt[eng])
        self._record(tok, R, W)
        self.ninst += 1
        return tok

    def dma(self, q, out, in_, R=(), W=(), **kw):
        d = self.dq[q]
        slot = d["slots"][d["i"] % len(d["slots"])]
        d["i"] += 1
        if slot[1] > 0:
            self._wait(q, (slot[0], slot[1]))
        if slot[1] >= self.SEM_ROT:
            slot[0] = self._newsem("d_" + q)
            slot[1] = 0
        for tok in self._deps(R, W):
            self._wait(q, tok)
        ins = self.engs[q].dma_start(out=out, in_=in_, **kw)
        slot[1] += 16
        ins.then_inc(slot[0], 16)
        tok = (slot[0], slot[1])
        self._record(tok, R, W)
        self.ninst += 1
        return tok

    def barrier(self, engines=("pe", "act", "dve", "pool", "sp")):
        toks = []
        for q, d in self.dq.items():
            for slot in d["slots"]:
                if slot[1] > 0:
                    toks.append((slot[0], slot[1]))
        for e in ("pe", "act", "dve", "pool"):
            if self.cnt[e] > 0:
                toks.append((self.sem[e], self.cnt[e]))
        for e in engines:
            for tok in toks:
                if e in self.sem and tok[0] is self.sem[e]:
                    continue
                self._wait(e, tok)

    def ev(self, out, in_, R=(), W=(), func=None, **kw):
        if func is not None:
            return self.op("act", "activation", out=out, in_=in_, func=func, R=R, W=W, **kw)
        self.flip ^= 1
        if self.flip:
            return self.op("act", "activation", out=out, in_=in_, func=AF.Copy, R=R, W=W)
        return self.op("dve", "tensor_copy", out=out, in_=in_, R=R, W=W)


class Pool:
    def __init__(self, nc, tag):
        self.nc = nc
        self.tag = tag
        self.stack = contextlib.ExitStack()
        self.n = 0

    def sb(self, shape, dt=F32, name=None):
        self.n += 1
        return self.stack.enter_context(self.nc.sbuf_tensor(f"{self.tag}_{name or 't'}{self.n}", list(shape), dt))

    def close(self):
        self.stack.close()


def build_program(debug=()):
    nc = bass.Bass("TRN2", target_bir_lowering=False)

    def din(name, shape, dt=F32):
        return nc.dram_tensor(name, list(shape), dt, kind="ExternalInput").ap()

    def dscr(name, shape, dt=F32):
        return nc.dram_tensor(name, list(shape), dt, kind="Internal").ap()

    xin = din("xin", [T, D])
    cvec = din("cvec", [128, 8, 2])
    w_mod = din("w_mod", [NL, D, 6144])
    bmod_c = din("bmod_c", [NL, 128, 48])
    bmod_r = din("bmod_r", [NL, 1, 6144])
    w_fm = din("w_fm", [NL, D, 2048])
    w_tm = din("w_tm", [NL, D, TMW + 4096])
    a_cw = din("a_cw", [NL, 128, 2, 5])
    a_cb = din("a_cb", [NL, 128, 2])
    a_gw = din("a_gw", [NL, 128, 2, 2, 2, 128])
    a_gb = din("a_gb", [NL, 128, 2, 2, 2])
    a_lam = din("a_lam", [NL, 128, 2, 2])
    b_thp = din("b_thp", [NL, 128, 2, 2])
    b_thh = din("b_thh", [NL, 128, 2, 4])
    c_cw = din("c_cw", [NL, 128, 4, 5])
    c_cb = din("c_cb", [NL, 128, 4])
    c_gb = din("c_gb", [NL, 128, 16])
    d_lbr = din("d_lbr", [128, 2, 256])
    w_branch = din("w_branch", [NL, 4, 256, D])
    w_out = din("w_out", [NL, D, D])
    moe_wgr = din("moe_wgr", [NL, D, 20])
    moe_bgr = din("moe_bgr", [NL, 1, 20])
    moe_w1 = din("moe_w1", [NL, 16, D, 512])
    moe_w3 = din("moe_w3", [NL, 16, D, 512])
    moe_w2 = din("moe_w2", [NL, 16, 512, D])
    fnw = din("fnw", [128, D])
    cst_d = din("cst", [128, NCST])
    ropeC_d = din("ropeC", [128, T])
    ropeS_d = din("ropeS", [128, T])
    yout = nc.dram_tensor("yout", [4096, D], F32, kind="ExternalOutput").ap()

    XR = dscr("XR", [T, D])
    FM = dscr("FM", [16, 128, T])
    TM = dscr("TM", [T, TMW])
    MG = dscr("MG", [T, 4096], BF16)
    YT = dscr("YT", [4, 2, 128, T], BF16)
    dbg = {}
    for name, shape, dt in debug:
        dbg[name] = nc.dram_tensor("dbg_" + name, list(shape), dt, kind="ExternalOutput").ap()

    mk = MK(nc)
    G = Pool(nc, "g")

    PS = [nc.psum_tensor(f"ps{i}", [128, 512], F32).__enter__() for i in range(6)]
    PQ = [nc.psum_tensor(f"pq{i}", [128, 1024], BF16).__enter__() for i in range(2)]

    cst = G.sb([128, NCST], F32, "cst")
    identb = G.sb([128, 128], BF16, "identb")
    sT = G.sb([128, 8, 2], F32, "sT")
    MODC = G.sb([128, 4, 8, 2], F32, "MODC")
    GB = G.sb([128, 2, 2, D], F32, "GB")
    mk.dma("sp", cst[:], cst_d, W=["cst"])
    mk.dma("sp", sT[:], cvec, W=["sT"])

    def C(name, rows=slice(0, 128)):
        o, w = CST[name]
        return cst[rows, o:o + w]

    mk.op("dve", "tensor_copy", out=identb[:], in_=C("ident"), R=["cst"], W=["identb"])
    mk.op("act", "activation", out=sT[:], in_=sT[:], func=AF.Silu, R=["sT"], W=["sT"])
    for t in range(NT):
        mk.dma("sp", XR[t * 128:(t + 1) * 128, :], xin[t * 128:(t + 1) * 128, :], W=[f"XR{t}"])

    def cond_of(t):
        return 0 if t < 2 else 1

    def mod_phase(l):
        P = Pool(nc, f"mod{l}")
        wblk = P.sb([128, 8, 1024], F32, "wblk")
        bmc = P.sb([128, 48], F32, "bmc")
        bmr = P.sb([1, 6144], F32, "bmr")
        GR = P.sb([2, 2, D], F32, "GR")
        mk.dma("sp", bmc[:], bmod_c[l], W=["bmc"])
        mk.dma("sp", bmr[:], bmod_r[l], W=["bmr"])
        sel = C("sel", slice(0, 2))
        for m in range(6):
            mk.dma("sp", wblk[:], w_mod[l].rearrange("(c p) f -> p c f", p=128)[:, :, m * 1024:(m + 1) * 1024],
                   W=["wblk"])
            if m in (0, 1, 3, 4):
                m4 = {0: 0, 1: 1, 3: 2, 4: 3}[m]
                for c in range(8):
                    for k in range(8):
                        mk.op("pe", "matmul", PS[0][:, c * 2:(c + 1) * 2], lhsT=wblk[:, k, c * 128:(c + 1) * 128],
                              rhs=sT[:, k, :], start=(k == 0), stop=(k == 7), R=["wblk", "sT"], W=["ps0"])
                mk.op("dve", "tensor_tensor", out=MODC[:, m4, :, :],
                      in0=PS[0][:, 0:16].rearrange("p (c j) -> p c j", j=2),
                      in1=bmc[:, m * 8:(m + 1) * 8].unsqueeze(2).to_broadcast([128, 8, 2]), op=ALU.add,
                      R=["ps0", "bmc"], W=["MODC"])
                if m in (1, 4):
                    mk.op("dve", "tensor_scalar_add", out=MODC[:, m4, :, :], in0=MODC[:, m4, :, :], scalar1=1.0,
                          R=["MODC"], W=["MODC"])
            else:
                mi = 0 if m == 2 else 1
                for cb in range(2):
                    for k in range(8):
                        mk.op("pe", "matmul", PS[1][0:2, :], lhsT=sT[:, k, :], rhs=wblk[:, k, cb * 512:(cb + 1) * 512],
                              start=(k == 0), stop=False, R=["wblk", "sT"], W=["ps1"])
                    mk.op("pe", "matmul", PS[1][0:2, :], lhsT=sel[0:1, 0:2],
                          rhs=bmr[0:1, m * 1024 + cb * 512: m * 1024 + (cb + 1) * 512], start=False, stop=True,
                          R=["bmr", "cst"], W=["ps1"])
                    mk.op("dve", "tensor_copy", out=GR[0:2, mi, cb * 512:(cb + 1) * 512], in_=PS[1][0:2, :],
                          R=["ps1"], W=["GR"])
        for j in range(2):
            for mi in range(2):
                for cb in range(2):
                    mk.op("pe", "matmul", PS[1][:, :], lhsT=sel[0:2, j * 128:(j + 1) * 128],
                          rhs=GR[0:2, mi, cb * 512:(cb + 1) * 512], start=True, stop=True, R=["GR", "cst"], W=["ps1"])
                    mk.op("act", "activation", out=GB[:, j, mi, cb * 512:(cb + 1) * 512], in_=PS[1][:, :], func=AF.Copy,
                          R=["ps1"], W=["GB"])
        mk.barrier()
        P.close()

    def make_norm(P, nbuf=2):
        st = dict(junk=P.sb([128, D], BF16, "junk"), ss=[P.sb([128, 1], F32, "ss") for _ in range(nbuf)],
                  xn=[P.sb([128, D], BF16, "xn") for _ in range(nbuf)],
                  tmp=[P.sb([128, 8, 128], F32, "tmp") for _ in range(nbuf)], i=0, nbuf=nbuf)

        def norm(xt_ap, xt_key, j, msc, msh, h_out, h_key):
            i = st["i"] % st["nbuf"]
            st["i"] += 1
            ss, xn, tmp = st["ss"][i], st["xn"][i], st["tmp"][i]
            mk.op("act", "activation", out=st["junk"][:], in_=xt_ap, func=AF.Square, accum_out=ss[:],
                  R=[xt_key], W=["junk", f"ss{i}"])
            mk.op("act", "activation", out=ss[:], in_=ss[:], func=AF.Sqrt, scale=1.0 / D, bias=EPS,
                  R=[f"ss{i}"], W=[f"ss{i}"])
            mk.op("dve", "reciprocal", out=ss[:], in_=ss[:], R=[f"ss{i}"], W=[f"ss{i}"])
            mk.op("dve", "tensor_scalar", out=xn[:], in0=xt_ap, scalar1=ss[:, 0:1], scalar2=None, op0=ALU.mult,
                  R=[xt_key, f"ss{i}"], W=[f"xn{i}"])
            for c in range(8):
                mk.op("pe", "transpose", out=PQ[i][:, c * 128:(c + 1) * 128], in_=xn[:, c * 128:(c + 1) * 128],
                      identity=identb[:], R=[f"xn{i}", "identb"], W=[f"pq{i}"])
            mk.op("dve", "tensor_tensor", out=tmp[:], in0=PQ[i][:, :].rearrange("p (c n) -> p c n", c=8),
                  in1=MODC[:, msc, :, j:j + 1].to_broadcast([128, 8, 128]), op=ALU.mult,
                  R=[f"pq{i}", "MODC"], W=[f"ntmp{i}"])
            mk.op("pool", "tensor_tensor", out=h_out, in0=tmp[:],
                  in1=MODC[:, msh, :, j:j + 1].to_broadcast([128, 8, 128]), op=ALU.add,
                  R=[f"ntmp{i}", "MODC"], W=[h_key])
        return norm

    def inproj_phase(l):
        P = Pool(nc, f"ip{l}")
        hT = P.sb([128, 8, T], BF16, "hT")
        xt = [P.sb([128, D], F32, "xt") for _ in range(2)]
        norm = make_norm(P)
        for t in range(NT):
            i = t % 2
            mk.dma("sp", xt[i][:], XR[t * 128:(t + 1) * 128, :], R=[f"XR{t}"], W=[f"xt{i}"])
            norm(xt[i][:], f"xt{i}", cond_of(t), 1, 0, hT[:, :, t * 128:(t + 1) * 128], f"hT{t}")
        hkeys = [f"hT{t}" for t in range(NT)]
        wf = [P.sb([128, 8, 512], F32, "wf")] * 2
        wb = [P.sb([128, 8, 512], BF16, "wb") for _ in range(2)]
        stg = [P.sb([128, T], F32, "stg")] * 2
        nblk = 0
        tblocks = [(i * 512, min(512, T - i * 512)) for i in range(9)]
        for cb in range(4):
            i = nblk % 2
            nblk += 1
            mk.dma("sp", wf[i][:], w_fm[l].rearrange("(c p) f -> p c f", p=128)[:, :, cb * 512:(cb + 1) * 512],
                   W=["wf"])
            mk.ev(wb[i][:], wf[i][:], R=["wf"], W=[f"wb{i}"])
            for sub in range(4):
                fc = cb * 4 + sub
                si = fc % 2
                for bi, (t0, tw) in enumerate(tblocks):
                    pb = bi % 2
                    for k in range(8):
                        mk.op("pe", "matmul", PS[pb][:, 0:tw], lhsT=wb[i][:, k, sub * 128:(sub + 1) * 128],
                              rhs=hT[:, k, t0:t0 + tw], start=(k == 0), stop=(k == 7),
                              R=[f"wb{i}"] + hkeys[t0 // 128:(t0 + tw) // 128], W=[f"ps{pb}"])
                    mk.ev(stg[si][:, t0:t0 + tw], PS[pb][:, 0:tw], R=[f"ps{pb}"], W=["stg"])
                mk.dma("pool", FM[fc], stg[si][:], R=["stg"], W=[f"FM{fc}"])
        cblocks = [(i * 512, 512) for i in range(4)] + [(2048, TMW - 2048)] + [(TMW + i * 512, 512) for i in range(8)]
        stt = [P.sb([128, 4, 512], F32, "stt") for _ in range(2)]
        stb = [P.sb([128, 4, 512], BF16, "stb") for _ in range(2)]
        tgroups = [(g * 4, min(4, NT - g * 4)) for g in range(9)]
        ns = 0
        for (c0, cw) in cblocks:
            i = nblk % 2
            nblk += 1
            mk.dma("sp", wf[i][:, :, 0:cw], w_tm[l].rearrange("(c p) f -> p c f", p=128)[:, :, c0:c0 + cw], W=["wf"])
            mk.ev(wb[i][:, :, 0:cw], wf[i][:, :, 0:cw], R=["wf"], W=[f"wb{i}"])
            is_mg = c0 >= TMW
            for (g0, gn) in tgroups:
                si = ns % 2
                ns += 1
                for tt in range(gn):
                    t = g0 + tt
                    pb = 2 + (t % 2)
                    for k in range(8):
                        mk.op("pe", "matmul", PS[pb][:, 0:cw], lhsT=hT[:, k, t * 128:(t + 1) * 128],
                              rhs=wb[i][:, k, 0:cw], start=(k == 0), stop=(k == 7), R=[f"wb{i}", f"hT{t}"], W=[f"ps{pb}"])
                    if is_mg:
                        mk.ev(stb[si][:, tt, 0:cw], PS[pb][:, 0:cw], R=[f"ps{pb}"], W=[f"stb{si}"], func=AF.Sigmoid)
                    else:
                        mk.ev(stt[si][:, tt, 0:cw], PS[pb][:, 0:cw], R=[f"ps{pb}"], W=[f"stt{si}"])
                if is_mg:
                    mk.dma("pool", MG[g0 * 128:(g0 + gn) * 128, c0 - TMW:c0 - TMW + cw].rearrange("(t p) c -> p t c", p=128),
                           stb[si][:, 0:gn, 0:cw], R=[f"stb{si}"], W=["MG"])
                else:
                    mk.dma("pool", TM[g0 * 128:(g0 + gn) * 128, c0:c0 + cw].rearrange("(t p) c -> p t c", p=128),
                           stt[si][:, 0:gn, 0:cw], R=[f"stt{si}"], W=["TM"])
        mk.barrier()
        P.close()

    SEGS = [(0, 256), (256, T)]

    def conv_fm(u, src, w4, bcol, keyu, keysrc, wkeys):
        for (s0, e) in SEGS:
            mk.op("act", "activation", out=u[:, s0:e], in_=src[:, s0:e], func=AF.Identity, scale=w4[:, 2:3], bias=bcol,
                  R=[keysrc] + wkeys, W=[keyu])
            for k, sh in ((0, -2), (1, -1), (3, 1), (4, 2)):
                if sh < 0:
                    o, i_ = u[:, s0 - sh:e], src[:, s0:e + sh]
                else:
                    o, i_ = u[:, s0:e - sh], src[:, s0 + sh:e]
                mk.op("dve", "scalar_tensor_tensor", out=o, in0=i_, scalar=w4[:, k:k + 1], in1=o, op0=ALU.mult,
                      op1=ALU.add, R=[keysrc, keyu] + wkeys, W=[keyu])

    def mixer_a(l):
        P = Pool(nc, f"ma{l}")
        cw = P.sb([128, 2, 5], F32, "cw")
        cb = P.sb([128, 2], F32, "cb")
        gwf = P.sb([128, 2, 2, 2, 128], F32, "gwf")
        gwb = P.sb([128, 2, 2, 2, 128], BF16, "gwb")
        gb = P.sb([128, 2, 2, 2], F32, "gb")
        lam = P.sb([128, 2, 2], F32, "lam")
        c1 = P.sb([128, 2, 2], F32, "c1")
        mk.dma("sp", cw[:], a_cw[l], W=["a_cw"])
        mk.dma("sp", cb[:], a_cb[l], W=["a_cb"])
        mk.dma("sp", gwf[:], a_gw[l], W=["a_gwf"])
        mk.dma("sp", gb[:], a_gb[l], W=["a_gb"])
        mk.dma("sp", lam[:], a_lam[l], W=["a_lam"])
        mk.op("dve", "tensor_copy", out=gwb[:], in_=gwf[:], R=["a_gwf"], W=["a_gwb"])
        mk.op("act", "activation", out=c1[:], in_=lam[:], func=AF.Exp, scale=-1.0, R=["a_lam"], W=["a_c1"])
        mk.op("act", "activation", out=c1[:], in_=c1[:], func=AF.Ln, bias=1.0, R=["a_c1"], W=["a_c1"])
        mk.op("dve", "tensor_scalar", out=c1[:], in0=c1[:], scalar1=-8.0, scalar2=None, op0=ALU.mult, R=["a_c1"], W=["a_c1"])
        ax = P.sb([128, T], F32, "ax")
        ag = P.sb([128, T], F32, "ag")
        u = P.sb([128, T], F32, "u")
        ub = P.sb([128, T], BF16, "ub")
        aa = P.sb([128, T], F32, "aa")
        bt = P.sb([128, T], F32, "bt")
        hf = P.sb([128, T], F32, "hf")
        hb = P.sb([128, T], F32, "hb")
        r = [P.sb([128, 512], F32, "r") for _ in range(2)]
        gi = [P.sb([128, 512], F32, "gi") for _ in range(2)]
        yb = P.sb([128, T], BF16, "yb")
        tblocks = [(i * 512, min(512, T - i * 512)) for i in range(9)]
        for c in range(2):
            mk.dma("sp", ax[:], FM[FM_OFF["a_x"] + c], R=[f"FM{FM_OFF['a_x'] + c}"], W=["ax"])
            mk.dma("sp", ag[:], FM[FM_OFF["a_g"] + c], R=[f"FM{FM_OFF['a_g'] + c}"], W=["ag"])
            conv_fm(u, ax, cw[:, c, :], cb[:, c:c + 1], "u", "ax", ["a_cw", "a_cb"])
            mk.op("pool", "tensor_copy", out=ub[:], in_=u[:], R=["u"], W=["ub"])
            for d in range(2):
                for bi, (t0, tw) in enumerate(tblocks):
                    i = bi % 2
                    mk.op("pe", "matmul", PS[i][:, 0:tw], lhsT=gwb[:, d, 0, c, :], rhs=ub[:, t0:t0 + tw], start=True,
                          stop=True, R=["a_gwb", "ub"], W=[f"ps{i}"])
                    mk.op("pe", "matmul", PS[2 + i][:, 0:tw], lhsT=gwb[:, d, 1, c, :], rhs=ub[:, t0:t0 + tw], start=True,
                          stop=True, R=["a_gwb", "ub"], W=[f"ps{2 + i}"])
                    mk.op("act", "activation", out=r[i][:, 0:tw], in_=PS[i][:, 0:tw], func=AF.Sigmoid,
                          bias=gb[:, d, 0, c:c + 1], R=[f"ps{i}", "a_gb"], W=[f"r{i}"])
                    mk.op("act", "activation", out=gi[i][:, 0:tw], in_=PS[2 + i][:, 0:tw], func=AF.Sigmoid,
                          bias=gb[:, d, 1, c:c + 1], R=[f"ps{2 + i}", "a_gb"], W=[f"gi{i}"])
                    mk.op("act", "activation", out=aa[:, t0:t0 + tw], in_=r[i][:, 0:tw], func=AF.Exp,
                          scale=c1[:, d, c:c + 1], R=[f"r{i}", "a_c1"], W=["aa"])
                    mk.op("dve", "tensor_tensor", out=r[i][:, 0:tw], in0=aa[:, t0:t0 + tw], in1=aa[:, t0:t0 + tw],
                          op=ALU.mult, R=["aa", f"r{i}"], W=[f"r{i}"])
                    mk.op("dve", "tensor_scalar", out=r[i][:, 0:tw], in0=r[i][:, 0:tw], scalar1=-1.0, scalar2=1.0,
                          op0=ALU.mult, op1=ALU.add, R=[f"r{i}"], W=[f"r{i}"])
                    mk.op("act", "activation", out=r[i][:, 0:tw], in_=r[i][:, 0:tw], func=AF.Sqrt, R=[f"r{i}"], W=[f"r{i}"])
                    mk.op("dve", "tensor_tensor", out=gi[i][:, 0:tw], in0=gi[i][:, 0:tw], in1=r[i][:, 0:tw], op=ALU.mult,
                          R=[f"gi{i}", f"r{i}"], W=[f"gi{i}"])
                    mk.op("pool", "tensor_tensor", out=bt[:, t0:t0 + tw], in0=gi[i][:, 0:tw], in1=u[:, t0:t0 + tw],
                          op=ALU.mult, R=[f"gi{i}", "u"], W=["bt"])
                if d == 0:
                    mk.op("dve", "tensor_tensor_scan", out=hf[:, :], data0=aa[:, :], data1=bt[:, :], initial=0.0,
                          op0=ALU.mult, op1=ALU.add, R=["aa", "bt"], W=["hf"])
                else:
                    mk.op("dve", "tensor_tensor_scan", out=hb[:, 0:256][:, ::-1], data0=aa[:, 0:256][:, ::-1],
                          data1=bt[:, 0:256][:, ::-1], initial=0.0, op0=ALU.mult, op1=ALU.add, R=["aa", "bt"], W=["hb"])
                    mk.op("dve", "tensor_tensor_scan", out=hb[:, 256:T][:, ::-1], data0=aa[:, 256:T][:, ::-1],
                          data1=bt[:, 256:T][:, ::-1], initial=hb[:, 0:1], op0=ALU.mult, op1=ALU.add,
                          R=["aa", "bt", "hb"], W=["hb"])
            mk.op("act", "activation", out=ag[:], in_=ag[:], func=AF.Gelu, R=["ag"], W=["ag"])
            mk.op("dve", "tensor_tensor", out=hf[:], in0=hf[:], in1=hb[:], op=ALU.add, R=["hf", "hb"], W=["hf"])
            mk.op("dve", "tensor_tensor", out=yb[:], in0=hf[:], in1=ag[:], op=ALU.mult, R=["hf", "ag"], W=["yb"])
            mk.dma("pool", YT[0, c], yb[:], R=["yb"], W=[f"YT0{c}"])
        mk.barrier()
        P.close()

    def order_of(dr):
        return list(range(NT)) if dr == 0 else [1, 0] + list(range(NT - 1, 1, -1))

    def chunk_core(it, nsub, dr, QT, KT, QIT, KHs, V, vw, Gc, S, Sbf, maskD, maskkey, rkeys, PT, sk):
        pi = it % 2
        L = 128 // nsub
        okeys = ["ps2", "ps3"]
        Oh = [PS[2 + hh][:, 0:2 * vw].rearrange("p (c e) -> p c e", c=2) for hh in range(2)]
        if not DBG["core"]:
            return okeys, Oh
        for h in DBG["heads"]:
            c, hh = h // 2, h % 2
            rs = slice(hh * 64, (hh + 1) * 64)
            mk.op("pe", "matmul", PS[hh][:, c * 128:(c + 1) * 128], lhsT=KT[c][rs, :], rhs=QT[c][rs, :], start=True,
                  stop=True, R=rkeys, W=[f"ps{hh}"])
        PTv = PT[pi][:].rearrange("p (c x n) -> p c x n", c=2, x=2)
        Mv = maskD.rearrange("p (c x n) -> p c x n", c=2, x=2)
        for hh in range(2):
            mk.op("dve", "tensor_tensor", out=PTv[:, :, hh, :], in0=PS[hh][:, 0:256].rearrange("p (c n) -> p c n", c=2),
                  in1=Mv[:, :, hh, :], op=ALU.mult, R=[f"ps{hh}", maskkey], W=[f"PT{pi}h{hh}"])
        ptk = [f"PT{pi}h0", f"PT{pi}h1"]
        subs = list(range(nsub)) if dr == 0 else list(range(nsub - 1, -1, -1))
        KVp = PS[4][:, 0:2 * vw].rearrange("p (c e) -> p c e", c=2)
        for s in subs:
            rows = slice(s * L, (s + 1) * L)
            for h in DBG["heads"]:
                c, hh = h // 2, h % 2
                rs = slice(hh * 64, (hh + 1) * 64)
                mk.op("pe", "matmul", Oh[hh][rows, c, :], lhsT=PT[pi][:, h * 128 + s * L:h * 128 + (s + 1) * L],
                      rhs=V[:, h, :], start=True, stop=False, R=[ptk[hh]] + rkeys, W=[okeys[hh]])
                mk.op("pe", "matmul", Oh[hh][rows, c, :], lhsT=QIT[c][rs, rows], rhs=Sbf[c][rs, :], start=False, stop=True,
                      R=rkeys + [f"{sk}Sbf{c}"], W=[okeys[hh]])
            for h in DBG["heads"]:
                c, hh = h // 2, h % 2
                mk.op("pe", "matmul", KVp[hh * 64:(hh + 1) * 64, c, :], lhsT=KHs(s)[:, h * 64:(h + 1) * 64],
                      rhs=V[:, h, :], start=True, stop=True, R=rkeys, W=["ps4kv"])
            for c in range(2):
                mk.op("dve", "scalar_tensor_tensor", out=S[c][:], in0=S[c][:], scalar=Gc[:, c, s:s + 1], in1=KVp[:, c, :],
                      op0=ALU.mult, op1=ALU.add, R=[f"{sk}S{c}", "ps4kv"] + rkeys, W=[f"{sk}S{c}"])
                mk.op("act", "activation", out=Sbf[c][:], in_=S[c][:], func=AF.Copy, R=[f"{sk}S{c}"], W=[f"{sk}Sbf{c}"])
        return okeys, Oh

    def hview(ap256, hh):
        return ap256.rearrange("p (c x e) -> p c x e", c=2, x=2)[:, :, hh, :]

    def make_finalize(P, n, yTb):
        st = dict(i=0)
        cent = [P.sb([128, 4, 64], F32, "cent") for _ in range(2)]
        sq = P.sb([128, 4, 64], F32, "sq")
        mm = [P.sb([128, 4], F32, "mm") for _ in range(2)]
        vv = [P.sb([128, 4], F32, "vv") for _ in range(2)]
        yy = [P.sb([128, 256], BF16, "yy") for _ in range(2)]

        def fin(tot, totkey, center, gate, gatekey, t):
            i = st["i"] % 2
            st["i"] += 1
            tv = tot.rearrange("p (h e) -> p h e", h=4)
            tk = list(totkey) if isinstance(totkey, (list, tuple)) else [totkey]
            src, skeys = tv, tk
            if center:
                mk.op("dve", "tensor_reduce", out=mm[i][:], in_=tv, axis=AX.X, op=ALU.add, R=tk, W=[f"fmm{i}"])
                mk.op("dve", "tensor_scalar", out=mm[i][:], in0=mm[i][:], scalar1=-1.0 / 64, scalar2=None, op0=ALU.mult,
                      R=[f"fmm{i}"], W=[f"fmm{i}"])
                mk.op("dve", "tensor_tensor", out=cent[i][:], in0=tv, in1=mm[i][:].unsqueeze(2).to_broadcast([128, 4, 64]),
                      op=ALU.add, R=tk + [f"fmm{i}"], W=[f"fcent{i}"])
                src, skeys = cent[i][:], [f"fcent{i}"]
            mk.op("pool", "tensor_tensor", out=sq[:], in0=src, in1=src, op=ALU.mult, R=skeys, W=["fsq"])
            mk.op("dve", "tensor_reduce", out=vv[i][:], in_=sq[:], axis=AX.X, op=ALU.add, R=["fsq"], W=[f"fvv{i}"])
            mk.op("act", "activation", out=vv[i][:], in_=vv[i][:], func=AF.Sqrt, scale=1.0 / 64, bias=EPS,
                  R=[f"fvv{i}"], W=[f"fvv{i}"])
            mk.op("dve", "reciprocal", out=vv[i][:], in_=vv[i][:], R=[f"fvv{i}"], W=[f"fvv{i}"])
            mk.op("dve", "tensor_tensor", out=cent[i][:], in0=src, in1=vv[i][:].unsqueeze(2).to_broadcast([128, 4, 64]),
                  op=ALU.mult, R=skeys + [f"fvv{i}"], W=[f"fcent{i}"])
            mk.op("dve", "tensor_tensor", out=yy[i][:], in0=cent[i][:].rearrange("p h e -> p (h e)"), in1=gate,
                  op=ALU.mult, R=[f"fcent{i}", gatekey], W=[f"fyy{i}"])
            for c in range(2):
                mk.op("pe", "transpose", out=PQ[1][:, (i * 2 + c) * 128:(i * 2 + c + 1) * 128],
                      in_=yy[i][:, c * 128:(c + 1) * 128], identity=identb[:], R=[f"fyy{i}", "identb"], W=[f"pq1f{i}"])
            mk.op("act", "activation", out=yTb[:, :, t * 128:(t + 1) * 128],
                  in_=PQ[1][:, i * 256:(i + 1) * 256].rearrange("p (c n) -> p c n", c=2), func=AF.Copy,
                  R=[f"pq1f{i}"], W=["yTb"])
        return fin

    def mixer_b(l):
        P = Pool(nc, f"mb{l}")
        QR = [P.sb([128, T], BF16, "QR") for _ in range(2)]
        KR = [P.sb([128, T], BF16, "KR") for _ in range(2)]
        thp = P.sb([128, 2, 2], F32, "thp")
        thh = P.sb([128, 2, 4], F32, "thh")
        mk.dma("sp", thp[:], b_thp[l], W=["thp"])
        mk.dma("sp", thh[:], b_thh[l], W=["thh"])
        for tt, key in ((thp, "thp"), (thh, "thh")):
            mk.op("act", "activation", out=tt[:], in_=tt[:], func=AF.Exp, scale=-1.0, R=[key], W=[key])
            mk.op("act", "activation", out=tt[:], in_=tt[:], func=AF.Ln, bias=1.0, R=[key], W=[key])
            mk.op("dve", "tensor_scalar", out=tt[:], in0=tt[:], scalar1=-1.0, scalar2=None, op0=ALU.mult, R=[key], W=[key])
        DM = P.sb([128, 2, 512], F32, "DM")
        QW = P.sb([128, 2, 2, 128], F32, "QW")
        KW = P.sb([128, 2, 4], F32, "KW")
        Gc = P.sb([128, 2, 2, 1], F32, "Gc")
        for dr in range(2):
            diff, msk, pos = (C("diffF"), C("maskF"), C("posF")) if dr == 0 else (C("diffB"), C("maskB"), C("posB"))
            for h in range(4):
                mk.op("act", "activation", out=DM[:, dr, h * 128:(h + 1) * 128], in_=diff, func=AF.Exp,
                      scale=thh[:, dr, h:h + 1], R=["cst", "thh"], W=["DM"])
                mk.op("dve", "tensor_tensor", out=DM[:, dr, h * 128:(h + 1) * 128], in0=DM[:, dr, h * 128:(h + 1) * 128],
                      in1=msk, op=ALU.mult, R=["DM", "cst"], W=["DM"])
                mk.op("act", "activation", out=KW[:, dr, h:h + 1], in_=C("kpos")[:, dr:dr + 1], func=AF.Exp,
                      scale=thh[:, dr, h:h + 1], R=["cst", "thh"], W=["KW"])
            for c in range(2):
                mk.op("act", "activation", out=QW[:, dr, c, :], in_=pos, func=AF.Exp, scale=thp[:, dr, c:c + 1],
                      R=["cst", "thp"], W=["QW"])
                mk.op("act", "activation", out=Gc[:, dr, c, :], in_=thp[:, dr, c:c + 1], func=AF.Exp, scale=128.0,
                      R=["thp"], W=["Gc"])
        segw = 1088
        f1 = [P.sb([128, segw], F32, "f1") for _ in range(2)]
        f2 = [P.sb([128, segw], F32, "f2") for _ in range(2)]
        rc = P.sb([128, T], F32, "rc")
        rsn = P.sb([128, T], F32, "rsn")
        mk.dma("sp", rc[:], ropeC_d, W=["rc"])
        mk.dma("sp", rsn[:], ropeS_d, W=["rsn"])
        n = 0
        for (dst, base, pbase, scale) in ((QR, "b_q", "b_qp", 1.0), (KR, "b_k", "b_kp", 0.125)):
            for c in range(2):
                for sg in range(4):
                    i = n % 2
                    n += 1
                    cs = slice(sg * segw, (sg + 1) * segw)
                    mk.dma("sp", f1[i][:], FM[FM_OFF[base] + c][:, cs], R=[f"FM{FM_OFF[base] + c}"], W=[f"f1{i}"])
                    mk.dma("sp", f2[i][:], FM[FM_OFF[pbase] + c][:, cs], R=[f"FM{FM_OFF[pbase] + c}"], W=[f"f2{i}"])
                    mk.op("dve", "tensor_tensor", out=f1[i][:], in0=f1[i][:], in1=rc[:, cs], op=ALU.mult,
                          R=[f"f1{i}", "rc"], W=[f"f1{i}"])
                    mk.op("pool", "tensor_tensor", out=f2[i][:], in0=f2[i][:], in1=rsn[:, cs], op=ALU.mult,
                          R=[f"f2{i}", "rsn"], W=[f"f2{i}"])
                    mk.op("dve", "tensor_tensor", out=f1[i][:], in0=f1[i][:], in1=f2[i][:], op=ALU.add,
                          R=[f"f1{i}", f"f2{i}"], W=[f"f1{i}"])
                    mk.op("act", "activation", out=dst[c][:, cs], in_=f1[i][:], func=AF.Copy, scale=scale,
                          R=[f"f1{i}"], W=[f"b{base}{c}"])
        rkeys = ["bb_q0", "bb_q1", "bb_k0", "bb_k1"]
        OF = P.sb([128, NT, 256], F32, "OF")
        yTb = P.sb([128, 2, T], BF16, "yTb")
        fin = make_finalize(P, 1, yTb)
        PT = [P.sb([128, 512], BF16, "PT") for _ in range(2)]
        QIT = [[P.sb([128, 128], BF16, "QIT") for _ in range(2)] for _ in range(2)]
        KH = [P.sb([128, 256], BF16, "KH") for _ in range(2)]
        Vf = [P.sb([128, 512], F32, "Vf") for _ in range(2)]
        Vb = [P.sb([128, 4, 64], BF16, "Vb") for _ in range(2)]
        gt = [P.sb([128, 256], F32, "gt") for _ in range(2)]
        tot = [P.sb([128, 256], F32, "tot") for _ in range(2)]
        S = [P.sb([128, 64], F32, "S") for _ in range(2)]
        Sbf = [P.sb([128, 64], BF16, "Sbf") for _ in range(2)]
        it = 0
        for dr in range(2):
            for c in range(2):
                mk.op("pool", "memset", S[c][:], 0.0, W=[f"bS{c}"])
                mk.op("pool", "memset", Sbf[c][:], 0.0, W=[f"bSbf{c}"])
            for t in order_of(dr):
                if DBG["maxit"] is not None and it >= DBG["maxit"]:
                    break
                i = it % 2
                cols = slice(t * 128, (t + 1) * 128)
                mk.dma("sp", Vf[i][:], TM[t * 128:(t + 1) * 128, 0:512], R=["TM"], W=[f"bVf{i}"])
                mk.op("pool", "tensor_copy", out=Vb[i][:], in_=Vf[i][:, 0:256].rearrange("p (h e) -> p h e", h=4),
                      R=[f"bVf{i}"], W=[f"bVb{i}"])
                for c in range(2):
                    mk.op("pool", "tensor_tensor", out=QIT[i][c][:], in0=QR[c][:, cols], in1=QW[:, dr, c, :], op=ALU.mult,
                          R=[f"bb_q{c}", "QW"], W=[f"bQIT{i}"])
                    mk.op("pe", "transpose", out=PQ[0][:, (i * 2 + c) * 128:(i * 2 + c + 1) * 128], in_=KR[c][:, cols],
                          identity=identb[:], R=[f"bb_k{c}", "identb"], W=[f"pq0k{i}"])
                for h in range(4):
                    mk.op("act", "activation", out=KH[i][:, h * 64:(h + 1) * 64],
                          in_=PQ[0][:, i * 256 + h * 64:i * 256 + (h + 1) * 64], func=AF.Identity, scale=KW[:, dr, h:h + 1],
                          R=[f"pq0k{i}", "KW"], W=[f"bKH{i}"])
                okeys, Oh = chunk_core(it, 1, dr, [QR[0][:, cols], QR[1][:, cols]], [KR[0][:, cols], KR[1][:, cols]],
                                       [QIT[i][0], QIT[i][1]], (lambda s_, kh=KH[i]: kh), Vb[i], 64, Gc[:, dr], S, Sbf,
                                       DM[:, dr, :], "DM", rkeys + [f"bQIT{i}", f"bKH{i}", f"bVb{i}"], PT, "b")
                if dr == 0:
                    for hh in range(2):
                        mk.op("act", "activation", out=hview(OF[:, t, :], hh), in_=Oh[hh], func=AF.Copy, R=[okeys[hh]],
                              W=[f"bOF{t}h{hh}"])
                else:
                    for hh in range(2):
                        mk.op("dve", "tensor_tensor", out=hview(tot[i][:], hh), in0=Oh[hh], in1=hview(OF[:, t, :], hh),
                              op=ALU.add, R=[okeys[hh], f"bOF{t}h{hh}"], W=[f"btot{i}h{hh}"])
                    mk.op("act", "activation", out=gt[i][:], in_=Vf[i][:, 256:512], func=AF.Silu, R=[f"bVf{i}"],
                          W=[f"bgt{i}"])
                    fin(tot[i][:], [f"btot{i}h0", f"btot{i}h1"], True, gt[i][:], f"bgt{i}", t)
                it += 1
        for c in range(2):
            mk.dma("pool", YT[1, c], yTb[:, c, :], R=["yTb"], W=[f"YT1{c}"])
        mk.barrier()
        P.close()

    def mixer_c(l):
        P = Pool(nc, f"mc{l}")
        QC = [P.sb([128, T], BF16, "QC") for _ in range(2)]
        KC = [P.sb([128, T], BF16, "KC") for _ in range(2)]
        cw = P.sb([128, 4, 5], F32, "cw")
        cb = P.sb([128, 4], F32, "cb")
        gbias = P.sb([128, 16], F32, "gbias")
        mk.dma("sp", cw[:], c_cw[l], W=["c_cw"])
        mk.dma("sp", cb[:], c_cb[l], W=["c_cb"])
        mk.dma("sp", gbias[:], c_gb[l], W=["c_gb"])
        src = P.sb([128, T], F32, "src")
        u = P.sb([128, T], F32, "u")
        for ch in range(4):
            fc = FM_OFF["c_q"] + ch
            mk.dma("sp", src[:], FM[fc], R=[f"FM{fc}"], W=["csrc"])
            conv_fm(u, src, cw[:, ch, :], cb[:, ch:ch + 1], "cu", "csrc", ["c_cw", "c_cb"])
            dst = QC[ch] if ch < 2 else KC[ch - 2]
            mk.op("act", "activation", out=u[:], in_=u[:], func=AF.Silu, R=["cu"], W=["cu"])
            mk.op("dve", "tensor_scalar", out=dst[:], in0=u[:], scalar1=(1.0 if ch < 2 else 0.125), scalar2=None,
                  op0=ALU.mult, R=["cu"], W=[f"cqk{ch}"])
        Z = P.sb([128, NT, 16], F32, "Z")
        LFN = P.sb([128, NT, 16], F32, "LFN")
        mk.dma("sp", Z[:], TM[:, TM_OFF["c_gates"]:TM_OFF["c_gates"] + 16].rearrange("(t p) g -> p t g", p=128),
               R=["TM"], W=["cZ"])
        mk.op("dve", "tensor_tensor", out=Z[:], in0=Z[:], in1=gbias[:].unsqueeze(1).to_broadcast([128, NT, 16]),
              op=ALU.add, R=["cZ", "c_gb"], W=["cZ"])
        mk.op("act", "activation", out=LFN[:], in_=Z[:], func=AF.Exp, scale=-1.0, R=["cZ"], W=["cLFN"])
        mk.op("act", "activation", out=LFN[:], in_=LFN[:], func=AF.Ln, bias=1.0, R=["cLFN"], W=["cLFN"])
        mk.op("dve", "tensor_scalar", out=LFN[:], in0=LFN[:], scalar1=-1.0, scalar2=None, op0=ALU.mult, R=["cLFN"],
              W=["cLFN"])
        rkeys = ["cqk0", "cqk1", "cqk2", "cqk3"]
        OF = P.sb([128, NT, 256], F32, "OF")
        yTb = P.sb([128, 2, T], BF16, "yTb")
        fin = make_finalize(P, 2, yTb)
        PT = [P.sb([128, 512], BF16, "PT") for _ in range(2)]
        QIT = [[P.sb([128, 128], BF16, "QIT") for _ in range(2)] for _ in range(2)]
        KH = [P.sb([128, 256], BF16, "KH") for _ in range(2)]
        Vf = [P.sb([128, 512], F32, "Vf") for _ in range(2)]
        Vb = [P.sb([128, 4, 65], BF16, "Vb") for _ in range(2)]
        gt = [P.sb([128, 256], F32, "gt") for _ in range(2)]
        tot = [P.sb([128, 256], F32, "tot") for _ in range(2)]
        S = [P.sb([128, 65], F32, "S") for _ in range(2)]
        Sbf = [P.sb([128, 65], BF16, "Sbf") for _ in range(2)]
        Bm4 = [P.sb([128, 4, 128], F32, "Bm4") for _ in range(2)]
        tmp4 = [P.sb([128, 512], F32, "tmp4") for _ in range(2)]
        Dm4 = [P.sb([128, 512], F32, "Dm4") for _ in range(2)]
        EB4 = [P.sb([128, 512], F32, "EB4") for _ in range(2)]
        lmb = [P.sb([128, 4], F32, "lmb") for _ in range(2)]
        kw = [P.sb([128, 4], F32, "kw") for _ in range(2)]
        Gc = [P.sb([128, 2, 1], F32, "Gc") for _ in range(2)]
        rden = [P.sb([128, 4], F32, "rden") for _ in range(2)]
        hid = [P.sb([128, 4, 64], F32, "hid") for _ in range(2)]
        for i in range(2):
            mk.op("pool", "memset", Vb[i][:], 1.0, W=[f"cVb{i}"])
        ones = C("ones")
        it = 0
        for dr in range(2):
            tri = C("triF") if dr == 0 else C("triB")
            neg4 = C("negF4") if dr == 0 else C("negB4")
            e = 127 if dr == 0 else 0
            for c in range(2):
                mk.op("pool", "memset", S[c][:], 0.0, W=[f"cS{c}"])
                mk.op("pool", "memset", Sbf[c][:], 0.0, W=[f"cSbf{c}"])
            for t in order_of(dr):
                i = it % 2
                cols = slice(t * 128, (t + 1) * 128)
                li = Z[:, t, dr * 8:dr * 8 + 4]
                lf = LFN[:, t, dr * 8 + 4:dr * 8 + 8]
                mk.dma("sp", Vf[i][:], TM[t * 128:(t + 1) * 128, 512:1024], R=["TM"], W=[f"cVf{i}"])
                mk.op("pool", "tensor_copy", out=Vb[i][:, :, 0:64], in_=Vf[i][:, 0:256].rearrange("p (h e) -> p h e", h=4),
                      R=[f"cVf{i}"], W=[f"cVb{i}"])
                mk.op("dve", "tensor_tensor", out=Bm4[i][:], in0=tri.unsqueeze(1).to_broadcast([128, 4, 128]),
                      in1=lf.unsqueeze(2).to_broadcast([128, 4, 128]), op=ALU.mult, R=["cst", "cLFN"], W=[f"cBm{i}"])
                mk.op("pe", "matmul", PS[5][:, :], lhsT=ones, rhs=Bm4[i][:].rearrange("p h n -> p (h n)"), start=True,
                      stop=True, R=["cst", f"cBm{i}"], W=["ps5"])
                mk.op("pe", "matmul", PS[4][:, 256:260], lhsT=tri, rhs=lf, start=True, stop=True, R=["cst", "cLFN"],
                      W=["ps4b"])
                mk.op("dve", "tensor_tensor", out=lmb[i][:], in0=li, in1=PS[4][:, 256:260], op=ALU.subtract,
                      R=["cZ", "ps4b"], W=[f"clmb{i}"])
                mk.op("dve", "tensor_tensor", out=tmp4[i][:], in0=PS[5][:, :], in1=neg4, op=ALU.add, R=["ps5", "cst"],
                      W=[f"ctmp{i}"])
                for h in range(4):
                    mk.op("act", "activation", out=Dm4[i][:, h * 128:(h + 1) * 128], in_=tmp4[i][:, h * 128:(h + 1) * 128],
                          func=AF.Exp, bias=lmb[i][:, h:h + 1], R=[f"ctmp{i}", f"clmb{i}"], W=[f"cDm{i}"])
                mk.op("act", "activation", out=EB4[i][:], in_=PS[5][:, :], func=AF.Exp, R=["ps5"], W=[f"cEB{i}"])
                bend = PS[5][:, :].rearrange("p (h n) -> p h n", h=4)[:, :, e]
                mk.op("dve", "tensor_tensor", out=kw[i][:], in0=lmb[i][:], in1=bend, op=ALU.add, R=[f"clmb{i}", "ps5"],
                      W=[f"ckw{i}"])
                mk.op("act", "activation", out=kw[i][:], in_=kw[i][:], func=AF.Exp, R=[f"ckw{i}"], W=[f"ckw{i}"])
                for h in range(4):
                    c, hh = h // 2, h % 2
                    rs = slice(hh * 64, (hh + 1) * 64)
                    mk.op("pool", "tensor_copy", out=Gc[i][rs, c, :], in_=EB4[i][rs, h * 128 + e:h * 128 + e + 1],
                          R=[f"cEB{i}"], W=[f"cGc{i}"])
                    mk.op("pool", "tensor_tensor", out=QIT[i][c][rs, :], in0=QC[c][rs, cols],
                          in1=EB4[i][rs, h * 128:(h + 1) * 128], op=ALU.mult, R=[f"cqk{c}", f"cEB{i}"], W=[f"cQIT{i}"])
                for c in range(2):
                    mk.op("pe", "transpose", out=PQ[0][:, (i * 2 + c) * 128:(i * 2 + c + 1) * 128], in_=KC[c][:, cols],
                          identity=identb[:], R=[f"cqk{2 + c}", "identb"], W=[f"pq0k{i}"])
                for h in range(4):
                    mk.op("act", "activation", out=KH[i][:, h * 64:(h + 1) * 64],
                          in_=PQ[0][:, i * 256 + h * 64:i * 256 + (h + 1) * 64], func=AF.Identity, scale=kw[i][:, h:h + 1],
                          R=[f"pq0k{i}", f"ckw{i}"], W=[f"cKH{i}"])
                okeys, Oh = chunk_core(it, 1, dr, [QC[0][:, cols], QC[1][:, cols]], [KC[0][:, cols], KC[1][:, cols]],
                                       [QIT[i][0], QIT[i][1]], (lambda s_, kh=KH[i]: kh), Vb[i], 65, Gc[i], S, Sbf,
                                       Dm4[i][:], f"cDm{i}",
                                       rkeys + [f"cQIT{i}", f"cKH{i}", f"cVb{i}", f"cGc{i}"], PT, "c")
                rdv = rden[i][:].rearrange("p (c x) -> p c x", c=2)
                for hh in range(2):
                    mk.op("act", "activation", out=rdv[:, :, hh], in_=Oh[hh][:, :, 64], func=AF.Abs, R=[okeys[hh]],
                          W=[f"crden{i}"])
                mk.op("dve", "tensor_scalar_max", out=rden[i][:], in0=rden[i][:], scalar1=1.0, R=[f"crden{i}"],
                      W=[f"crden{i}"])
                mk.op("dve", "reciprocal", out=rden[i][:], in_=rden[i][:], R=[f"crden{i}"], W=[f"crden{i}"])
                if dr == 0:
                    for hh in range(2):
                        mk.op("dve", "tensor_tensor", out=hview(OF[:, t, :], hh), in0=Oh[hh][:, :, 0:64],
                              in1=rdv[:, :, hh:hh + 1].to_broadcast([128, 2, 64]), op=ALU.mult,
                              R=[okeys[hh], f"crden{i}"], W=[f"cOF{t}h{hh}"])
                else:
                    for hh in range(2):
                        mk.op("dve", "tensor_tensor", out=hview(hid[i][:].rearrange("p h e -> p (h e)"), hh),
                              in0=Oh[hh][:, :, 0:64], in1=rdv[:, :, hh:hh + 1].to_broadcast([128, 2, 64]), op=ALU.mult,
                              R=[okeys[hh], f"crden{i}"], W=[f"chid{i}h{hh}"])
                    mk.op("pool", "tensor_tensor", out=tot[i][:], in0=hid[i][:].rearrange("p h e -> p (h e)"),
                          in1=OF[:, t, :], op=ALU.add, R=[f"chid{i}h0", f"chid{i}h1", f"cOF{t}h0", f"cOF{t}h1"],
                          W=[f"ctot{i}"])
                    mk.op("act", "activation", out=gt[i][:], in_=Vf[i][:, 256:512], func=AF.Sigmoid, R=[f"cVf{i}"],
                          W=[f"cgt{i}"])
                    fin(tot[i][:], f"ctot{i}", True, gt[i][:], f"cgt{i}", t)
                it += 1
        for c in range(2):
            mk.dma("pool", YT[2, c], yTb[:, c, :], R=["yTb"], W=[f"YT2{c}"])
        mk.barrier()
        P.close()

    def mixer_d(l):
        P = Pool(nc, f"md{l}")
        LB = P.sb([128, 256], F32, "LB")
        OML = P.sb([128, 256], F32, "OML")
        if l == 0:
            use_lb = False
        else:
            use_lb = True
            dl = P.sb([128, 2, 256], F32, "dl")
            mk.dma("sp", dl[:], d_lbr, W=["dl"])
            mk.op("dve", "tensor_tensor", out=LB[:], in0=dl[:, 1, :], in1=dl[:, 0, :], op=ALU.subtract, R=["dl"], W=["LB"])
            mk.op("act", "activation", out=LB[:], in_=LB[:], func=AF.Sigmoid, R=["LB"], W=["LB"])
            mk.op("dve", "tensor_scalar", out=OML[:], in0=LB[:], scalar1=-1.0, scalar2=1.0, op0=ALU.mult, op1=ALU.add,
                  R=["LB"], W=["OML"])
        OF = P.sb([128, NT, 256], F32, "OF")
        yTb = P.sb([128, 2, T], BF16, "yTb")
        fin = make_finalize(P, 3, yTb)
        assert DNS == 4
        PT = [P.sb([128, 512], BF16, "PT") for _ in range(2)]
        X = [P.sb([128, 1280], F32, "X") for _ in range(2)]
        ff = [P.sb([128, 256], F32, "ff") for _ in range(2)]
        lf = [P.sb([128, 256], F32, "lf") for _ in range(2)]
        kk = [P.sb([128, 256], F32, "kk") for _ in range(2)]
        qs = [P.sb([128, 256], F32, "qs") for _ in range(2)]
        ee = [P.sb([128, 512], F32, "ee") for _ in range(2)]
        ek = [P.sb([128, 256], F32, "ek") for _ in range(2)]
        qk = [P.sb([128, 512], BF16, "qk") for _ in range(2)]
        KTs = [P.sb([128, 2, 128], BF16, "KTs") for _ in range(2)]
        QM = [[P.sb([128, 2, 5, 128], BF16, "QM") for _ in range(2)] for _ in range(2)]
        KH = [P.sb([128, DNS, 256], BF16, "KH") for _ in range(2)]
        Vb = [P.sb([128, 4, 64], BF16, "Vb") for _ in range(2)]
        gt = [P.sb([128, 256], F32, "gt") for _ in range(2)]
        tot = [P.sb([128, 256], F32, "tot") for _ in range(2)]
        red = [P.sb([128, 2, 256], F32, "red") for _ in range(2)]
        Gc = [P.sb([128, 2, DNS], F32, "Gc") for _ in range(2)]
        S = [P.sb([128, 64], F32, "S") for _ in range(2)]
        Sbf = [P.sb([128, 64], BF16, "Sbf") for _ in range(2)]
        qmask = C("qmask").rearrange("p (x s n) -> p x s n", x=2, s=5)
        it = 0
        for dr in range(2):
            blk = C("blkF") if dr == 0 else C("blkB")
            rem = C("aftF") if dr == 0 else C("befB")
            msk4 = C("mblkF4") if dr == 0 else C("mblkB4")
            zoff = 256 if dr == 0 else 512
            subs = list(range(DNS)) if dr == 0 else list(range(DNS - 1, -1, -1))
            for c in range(2):
                mk.op("pool", "memset", S[c][:], 0.0, W=[f"dS{c}"])
                mk.op("pool", "memset", Sbf[c][:], 0.0, W=[f"dSbf{c}"])
            for t in order_of(dr):
                i = it % 2
                mk.dma("sp", X[i][:], TM[t * 128:(t + 1) * 128, 1024:2304], R=["TM"], W=[f"dX{i}"])
                mk.op("act", "activation", out=ff[i][:], in_=X[i][:, zoff:zoff + 256], func=AF.Sigmoid, R=[f"dX{i}"],
                      W=[f"dff{i}"])
                if use_lb:
                    mk.op("dve", "tensor_tensor", out=ff[i][:], in0=ff[i][:], in1=OML[:], op=ALU.mult, R=[f"dff{i}", "OML"],
                          W=[f"dff{i}"])
                    mk.op("dve", "tensor_tensor", out=ff[i][:], in0=ff[i][:], in1=LB[:], op=ALU.add, R=[f"dff{i}", "LB"],
                          W=[f"dff{i}"])
                mk.op("act", "activation", out=lf[i][:], in_=ff[i][:], func=AF.Ln, R=[f"dff{i}"], W=[f"dlf{i}"])
                mk.op("pool", "tensor_scalar", out=kk[i][:], in0=ff[i][:], scalar1=-1.0, scalar2=1.0, op0=ALU.mult,
                      op1=ALU.add, R=[f"dff{i}"], W=[f"dkk{i}"])
                mk.op("pe", "matmul", PS[5][:, 0:256], lhsT=blk, rhs=lf[i][:], start=True, stop=True, R=["cst", f"dlf{i}"],
                      W=["ps5"])
                mk.op("pe", "matmul", PS[5][:, 256:512], lhsT=rem, rhs=lf[i][:], start=True, stop=True,
                      R=["cst", f"dlf{i}"], W=["ps5"])
                for c in range(2):
                    mk.op("pe", "matmul", PS[1][:, 384 + c * DNS:384 + (c + 1) * DNS], lhsT=lf[i][:, c * 128:(c + 1) * 128],
                          rhs=C("subm"), start=True, stop=True, R=["cst", f"dlf{i}"], W=["ps1g"])
                mk.op("act", "activation", out=Gc[i][:].rearrange("p c s -> p (c s)"), in_=PS[1][:, 384:384 + 2 * DNS],
                      func=AF.Exp, R=["ps1g"], W=[f"dGc{i}"])
                mk.op("act", "activation", out=ee[i][:], in_=PS[5][:, :], func=AF.Exp, R=["ps5"], W=[f"dee{i}"])
                mk.op("act", "activation", out=ek[i][:], in_=PS[5][:, 0:256], func=AF.Exp, scale=-1.0, R=["ps5"],
                      W=[f"dek{i}"])
                mk.op("act", "activation", out=qs[i][:], in_=X[i][:, 0:256], func=AF.Silu, R=[f"dX{i}"], W=[f"dqs{i}"])
                mk.op("dve", "tensor_tensor", out=qk[i][:, 0:256], in0=qs[i][:], in1=ee[i][:, 0:256], op=ALU.mult,
                      R=[f"dqs{i}", f"dee{i}"], W=[f"dqk{i}a"])
                mk.op("dve", "tensor_tensor", out=qk[i][:, 256:512], in0=kk[i][:], in1=ek[i][:], op=ALU.mult,
                      R=[f"dkk{i}", f"dek{i}"], W=[f"dqk{i}c"])
                for s_ in range(DNS):
                    mk.op("dve", "scalar_tensor_tensor", out=KH[i][:, s_, :], in0=kk[i][:], scalar=C("subm")[:, s_:s_ + 1],
                          in1=ee[i][:, 256:512], op0=ALU.mult, op1=ALU.mult, R=[f"dkk{i}", f"dee{i}", "cst"],
                          W=[f"dKH{i}s{s_}"])
                mk.op("pool", "tensor_copy", out=Vb[i][:], in_=X[i][:, 768:1024].rearrange("p (h e) -> p h e", h=4),
                      R=[f"dX{i}"], W=[f"dVb{i}"])
                for j in range(4):
                    mk.op("pe", "transpose", out=PQ[0][:, (i * 4 + j) * 128:(i * 4 + j + 1) * 128],
                          in_=qk[i][:, j * 128:(j + 1) * 128], identity=identb[:], R=[f"dqk{i}a", f"dqk{i}c", "identb"],
                          W=[f"pq0d{i}"])
                mk.op("act", "activation", out=KTs[i][:].rearrange("p c n -> p (c n)"),
                      in_=PQ[0][:, i * 512 + 256:i * 512 + 512], func=AF.Copy, R=[f"pq0d{i}"], W=[f"dKT{i}"])
                for c in range(2):
                    mk.op("dve", "tensor_tensor", out=QM[i][c][:].rearrange("p x s n -> p (x s) n"),
                          in0=PQ[0][:, i * 512 + c * 128:i * 512 + (c + 1) * 128].unsqueeze(1).to_broadcast([128, 10, 128]),
                          in1=qmask.rearrange("p x s n -> p (x s) n"), op=ALU.mult, R=[f"pq0d{i}", "cst"],
                          W=[f"dQM{i}{c}"])
                for h in range(4):
                    c, hh = h // 2, h % 2
                    mk.op("pe", "matmul", PS[0][:, h * 128:(h + 1) * 128], lhsT=KTs[i][:, c, :], rhs=QM[i][c][:, hh, 4, :],
                          start=True, stop=True, R=[f"dKT{i}", f"dQM{i}{c}"], W=["ps0"])
                mk.op("dve", "tensor_tensor", out=PT[i][:], in0=PS[0][:, :], in1=msk4, op=ALU.mult, R=["ps0", "cst"],
                      W=[f"dPT{i}"])
                for h in range(4):
                    mk.op("pe", "matmul", PS[1][:, h * 64:(h + 1) * 64], lhsT=PT[i][:, h * 128:(h + 1) * 128],
                          rhs=Vb[i][:, h, :], start=True, stop=True, R=[f"dPT{i}", f"dVb{i}"], W=["ps1i"])
                KVp = PS[1][:, 256:384].rearrange("p (c e) -> p c e", c=2)
                for s_ in subs:
                    bank = PS[2 + s_ // 2]
                    for h in range(4):
                        c, hh = h // 2, h % 2
                        col = ((s_ % 2) * 4 + h) * 64
                        mk.op("pe", "matmul", bank[:, col:col + 64], lhsT=QM[i][c][:, hh, s_, :], rhs=Sbf[c][:, :],
                              start=True, stop=True, R=[f"dQM{i}{c}", f"dSbf{c}"], W=[f"ps{2 + s_ // 2}"])
                    for h in range(4):
                        c, hh = h // 2, h % 2
                        mk.op("pe", "matmul", KVp[hh * 64:(hh + 1) * 64, c, :], lhsT=KH[i][:, s_, h * 64:(h + 1) * 64],
                              rhs=Vb[i][:, h, :], start=True, stop=True, R=[f"dKH{i}s{s_}", f"dVb{i}"], W=["ps1kv"])
                    for c in range(2):
                        mk.op("dve", "scalar_tensor_tensor", out=S[c][:], in0=S[c][:], scalar=Gc[i][:, c, s_:s_ + 1],
                              in1=KVp[:, c, :], op0=ALU.mult, op1=ALU.add, R=[f"dS{c}", "ps1kv", f"dGc{i}"], W=[f"dS{c}"])
                        mk.op("act", "activation", out=Sbf[c][:], in_=S[c][:], func=AF.Copy, R=[f"dS{c}"], W=[f"dSbf{c}"])
                for b_ in range(2):
                    mk.op("dve", "tensor_reduce", out=red[i][:, b_, :],
                          in_=PS[2 + b_][:, :].rearrange("p (s x) -> p x s", s=2), axis=AX.X, op=ALU.add,
                          R=[f"ps{2 + b_}"], W=[f"dred{i}{b_}"])
                mk.op("dve", "tensor_tensor", out=tot[i][:], in0=PS[1][:, 0:256], in1=red[i][:, 0, :], op=ALU.add,
                      R=["ps1i", f"dred{i}0"], W=[f"dtot{i}"])
                if dr == 0:
                    mk.op("pool", "tensor_tensor", out=OF[:, t, :], in0=tot[i][:], in1=red[i][:, 1, :], op=ALU.add,
                          R=[f"dtot{i}", f"dred{i}1"], W=[f"dOF{t}"])
                else:
                    mk.op("pool", "tensor_tensor", out=tot[i][:], in0=tot[i][:], in1=red[i][:, 1, :], op=ALU.add,
                          R=[f"dtot{i}", f"dred{i}1"], W=[f"dtot{i}"])
                    mk.op("dve", "tensor_tensor", out=tot[i][:], in0=tot[i][:], in1=OF[:, t, :], op=ALU.add,
                          R=[f"dtot{i}", f"dOF{t}"], W=[f"dtot{i}"])
                    mk.op("act", "activation", out=gt[i][:], in_=X[i][:, 1024:1280], func=AF.Silu, R=[f"dX{i}"],
                          W=[f"dgt{i}"])
                    fin(tot[i][:], f"dtot{i}", False, gt[i][:], f"dgt{i}", t)
                it += 1
        for c in range(2):
            mk.dma("pool", YT[3, c], yTb[:, c, :], R=["yTb"], W=[f"YT3{c}"])
        mk.barrier()
        P.close()

    W1B = dscr("W1B", [16, 128, 4096], BF16)
    W3B = dscr("W3B", [16, 128, 4096], BF16)
    W2B = dscr("W2B", [16, 128, 4096], BF16)

    def merge_moe_phase(l, last, tiles=None):
        if tiles is None:
            tiles = list(range(2 if last else 0, NT))
        PO = Pool(nc, f"mo{l}")
        h2T = PO.sb([128, 8, T], BF16, "h2T")
        gates = PO.sb([128, NT, 16], F32, "gates")
        merge_part(l, tiles, h2T, gates)
        moe_part(l, tiles, h2T, gates)
        PO.close()

    def merge_part(l, tiles, h2T, gates):
        P = Pool(nc, f"mm{l}")
        wbr = P.sb([128, 8, D], BF16, "wbr")
        wo = P.sb([128, 8, D], BF16, "wo")
        wstage = P.sb([128, 8, 512], F32, "wstage")
        wcb = P.sb([128, 4096], BF16, "wcb")
        for half in range(2):
            mk.dma("sp", wstage[:], w_branch[l].rearrange("n (c p) f -> p (n c) f", p=128)[:, :, half * 512:(half + 1) * 512],
                   W=["wstage"])
            mk.ev(wbr[:, :, half * 512:(half + 1) * 512], wstage[:], R=["wstage"], W=["wbr"])
        for half in range(2):
            mk.dma("sp", wstage[:], w_out[l].rearrange("(c p) f -> p c f", p=128)[:, :, half * 512:(half + 1) * 512],
                   W=["wstage"])
            mk.ev(wo[:, :, half * 512:(half + 1) * 512], wstage[:], R=["wstage"], W=["wo"])
        tasks = []
        for e in range(16):
            tasks.append((moe_w1[l, e].rearrange("(c p) f -> p c f", p=128), wstage[:], W1B[e], f"W1B{e}"))
            tasks.append((moe_w3[l, e].rearrange("(c p) f -> p c f", p=128), wstage[:], W3B[e], f"W3B{e}"))
            tasks.append((moe_w2[l, e].rearrange("(c p) f -> p c f", p=128),
                          wstage[:].rearrange("p c f -> p (c f)").rearrange("p (c f) -> p c f", c=4), W2B[e], f"W2B{e}"))

        def do_task(k):
            src, stg, dst, key = tasks[k]
            mk.dma("sp", stg, src, W=["wstage"])
            mk.ev(wcb[:], wstage[:].rearrange("p c f -> p (c f)"), R=["wstage"], W=["wcb"])
            mk.dma("pool", dst, wcb[:], R=["wcb"], W=[key])
        wgr = P.sb([128, 8, 20], F32, "wgr")
        bgr = P.sb([1, 20], F32, "bgr")
        mk.dma("sp", wgr[:], moe_wgr[l].rearrange("(c p) f -> p c f", p=128), W=["wgr"])
        mk.dma("sp", bgr[:], moe_bgr[l], W=["bgr"])
        identf = C("ident")
        ones = C("ones")
        norm = make_norm(P, 1)
        xnew = [P.sb([128, D], F32, "xnew") for _ in range(2)]
        yt = [P.sb([128, 8, 128], BF16, "yt")] * 2
        mg = [P.sb([128, 4096], BF16, "mg")] * 2
        xt = [P.sb([128, D], F32, "xt")] * 2
        zz = P.sb([128, D], F32, "zz")
        zt = P.sb([128, D], F32, "zt")
        zb = P.sb([128, D], BF16, "zb")
        zT = P.sb([128, 8, 128], BF16, "zT")
        h2f = zz
        h2fT = zt[:, :].rearrange("p (c n) -> p c n", c=8)
        rt = {k: P.sb([128, w], F32, "rt" + k) for k, w in
              (("L", 20), ("gm", 1), ("goh", 4), ("ge", 4), ("gs", 1), ("el", 4), ("m1", 1), ("oh1", 4), ("e2", 4),
               ("m2", 1), ("oh2", 4), ("w1", 1), ("w2", 1), ("gw", 4))}
        nit = 0
        ntask = 0
        per_tile = -(-len(tasks) // len(tiles))
        for t in tiles:
            for _ in range(per_tile):
                if ntask < len(tasks):
                    do_task(ntask)
                    ntask += 1
            i = nit % 2
            nit += 1
            j = cond_of(t)
            cols = slice(t * 128, (t + 1) * 128)
            mk.dma("sp", yt[i][:], YT[:, :, :, cols].rearrange("n c p t -> p (n c) t"),
                   R=[f"YT{n}{c}" for n in range(4) for c in range(2)], W=["yt"])
            mk.dma("sp", mg[i][:], MG[cols, :], R=["MG"], W=["mg"])
            mk.dma("sp", xt[i][:], XR[cols, :], R=[f"XR{t}"], W=["mxt"])
            for n in range(4):
                for cb in range(2):
                    pb = (n * 2 + cb) % 2
                    for c in range(2):
                        mk.op("pe", "matmul", PS[pb][:, :], lhsT=yt[i][:, n * 2 + c, :],
                              rhs=wbr[:, n * 2 + c, cb * 512:(cb + 1) * 512], start=(c == 0), stop=(c == 1),
                              R=["yt", "wbr"], W=[f"ps{pb}"])
                    dst = zz if n == 0 else zt
                    dkey = "zz" if n == 0 else "zt"
                    mk.op("dve", "tensor_tensor", out=dst[:, cb * 512:(cb + 1) * 512], in0=PS[pb][:, :],
                          in1=mg[i][:, n * 1024 + cb * 512:n * 1024 + (cb + 1) * 512], op=ALU.mult,
                          R=[f"ps{pb}", "mg"], W=[dkey])
                    if n > 0:
                        mk.op("pool", "tensor_tensor", out=zz[:, cb * 512:(cb + 1) * 512],
                              in0=zz[:, cb * 512:(cb + 1) * 512], in1=zt[:, cb * 512:(cb + 1) * 512], op=ALU.add,
                              R=["zz", "zt"], W=["zz"])
            mk.op("act", "activation", out=zb[:], in_=zz[:], func=AF.Copy, R=["zz"], W=["zb"])
            for c in range(8):
                mk.op("pe", "transpose", out=PQ[1][:, c * 128:(c + 1) * 128], in_=zb[:, c * 128:(c + 1) * 128],
                      identity=identb[:], R=["zb", "identb"], W=["pq1"])
            mk.ev(zT[:].rearrange("p c n -> p (c n)"), PQ[1][:, :], R=["pq1"], W=["zT"])
            for cb in range(2):
                pb = 2 + cb
                for c in range(8):
                    mk.op("pe", "matmul", PS[pb][:, :], lhsT=zT[:, c, :], rhs=wo[:, c, cb * 512:(cb + 1) * 512],
                          start=(c == 0), stop=(c == 7), R=["zT", "wo"], W=[f"ps{pb}"])
                mk.op("dve", "tensor_tensor", out=zt[:, cb * 512:(cb + 1) * 512], in0=PS[pb][:, :],
                      in1=GB[:, j, 0, cb * 512:(cb + 1) * 512], op=ALU.mult, R=[f"ps{pb}", "GB"], W=["zt"])
                mk.op("pool", "tensor_tensor", out=xnew[i][:, cb * 512:(cb + 1) * 512], in0=zt[:, cb * 512:(cb + 1) * 512],
                      in1=xt[i][:, cb * 512:(cb + 1) * 512], op=ALU.add, R=["zt", "mxt"], W=[f"xnew{i}"])
            mk.dma("pool", XR[cols, :], xnew[i][:], R=[f"xnew{i}"], W=[f"XR{t}"])
            norm(xnew[i][:], f"xnew{i}", j, 3, 2, h2T[:, :, t * 128:(t + 1) * 128], f"h2T{t}")
            ssr = rt["gs"]
            mk.op("act", "activation", out=h2f[:], in_=xnew[i][:], func=AF.Square, accum_out=ssr[:],
                  R=[f"xnew{i}"], W=["zz", "rgs"])
            mk.op("act", "activation", out=ssr[:], in_=ssr[:], func=AF.Sqrt, scale=1.0 / D, bias=EPS, R=["rgs"], W=["rgs"])
            mk.op("dve", "reciprocal", out=ssr[:], in_=ssr[:], R=["rgs"], W=["rgs"])
            mk.op("dve", "tensor_scalar", out=h2f[:], in0=xnew[i][:], scalar1=ssr[:, 0:1], scalar2=None,
                  op0=ALU.mult, R=[f"xnew{i}", "rgs", "zz"], W=["zz"])
            for half in range(2):
                for c4 in range(4):
                    c = half * 4 + c4
                    mk.op("pe", "transpose", out=PS[5][:, c4 * 128:(c4 + 1) * 128], in_=h2f[:, c * 128:(c + 1) * 128],
                          identity=identf, R=["zz", "cst"], W=["ps5"])
                mk.op("dve", "tensor_tensor", out=h2fT[:, half * 4:(half + 1) * 4, :],
                      in0=PS[5][:, :].rearrange("p (c n) -> p c n", c=4),
                      in1=MODC[:, 3, half * 4:(half + 1) * 4, j:j + 1].to_broadcast([128, 4, 128]), op=ALU.mult,
                      R=["ps5", "MODC"], W=["zt"])
                mk.op("pool", "tensor_tensor", out=h2fT[:, half * 4:(half + 1) * 4, :],
                      in0=h2fT[:, half * 4:(half + 1) * 4, :],
                      in1=MODC[:, 2, half * 4:(half + 1) * 4, j:j + 1].to_broadcast([128, 4, 128]), op=ALU.add,
                      R=["zt", "MODC"], W=["zt"])
            for c in range(8):
                mk.op("pe", "matmul", PS[4][:, 0:20], lhsT=h2fT[:, c, :], rhs=wgr[:, c, :], start=(c == 0), stop=False,
                      R=["zt", "wgr"], W=["ps4r"])
            mk.op("pe", "matmul", PS[4][:, 0:20], lhsT=ones[0:1, :], rhs=bgr[0:1, :], start=False, stop=True,
                  R=["cst", "bgr"], W=["ps4r"])
            Lg = rt["L"]
            mk.op("act", "activation", out=Lg[:], in_=PS[4][:, 0:20], func=AF.Copy, R=["ps4r"], W=["rL"])
            rk = ["rL"]
            mk.op("dve", "tensor_reduce", out=rt["gm"][:], in_=Lg[:, 0:4], axis=AX.X, op=ALU.max, R=rk, W=["rgm"])
            mk.op("dve", "tensor_scalar", out=rt["goh"][:], in0=Lg[:, 0:4], scalar1=rt["gm"][:, 0:1], scalar2=None,
                  op0=ALU.is_ge, R=rk + ["rgm"], W=["rgoh"])
            mk.op("dve", "tensor_scalar", out=rt["ge"][:], in0=Lg[:, 0:4], scalar1=rt["gm"][:, 0:1], scalar2=None,
                  op0=ALU.subtract, R=rk + ["rgm"], W=["rge"])
            mk.op("act", "activation", out=rt["ge"][:], in_=rt["ge"][:], func=AF.Exp, accum_out=rt["gs"][:],
                  R=["rge"], W=["rge", "rgs"])
            mk.op("dve", "reciprocal", out=rt["gs"][:], in_=rt["gs"][:], R=["rgs"], W=["rgs"])
            mk.op("dve", "tensor_scalar", out=rt["el"][:], in0=Lg[:, 4:8], scalar1=rt["goh"][:, 0:1], scalar2=None,
                  op0=ALU.mult, R=rk + ["rgoh"], W=["rel"])
            for g in range(1, 4):
                mk.op("dve", "scalar_tensor_tensor", out=rt["el"][:], in0=Lg[:, 4 + g * 4:8 + g * 4],
                      scalar=rt["goh"][:, g:g + 1], in1=rt["el"][:], op0=ALU.mult, op1=ALU.add,
                      R=rk + ["rgoh", "rel"], W=["rel"])
            mk.op("dve", "tensor_reduce", out=rt["m1"][:], in_=rt["el"][:], axis=AX.X, op=ALU.max, R=["rel"], W=["rm1"])
            mk.op("dve", "tensor_scalar", out=rt["oh1"][:], in0=rt["el"][:], scalar1=rt["m1"][:, 0:1], scalar2=None,
                  op0=ALU.is_ge, R=["rel", "rm1"], W=["roh1"])
            mk.op("dve", "scalar_tensor_tensor", out=rt["e2"][:], in0=rt["oh1"][:], scalar=-1e30, in1=rt["el"][:],
                  op0=ALU.mult, op1=ALU.add, R=["roh1", "rel"], W=["re2"])
            mk.op("dve", "tensor_reduce", out=rt["m2"][:], in_=rt["e2"][:], axis=AX.X, op=ALU.max, R=["re2"], W=["rm2"])
            mk.op("dve", "tensor_scalar", out=rt["oh2"][:], in0=rt["e2"][:], scalar1=rt["m2"][:, 0:1], scalar2=None,
                  op0=ALU.is_ge, R=["re2", "rm2"], W=["roh2"])
            mk.op("dve", "tensor_tensor", out=rt["w1"][:], in0=rt["m2"][:], in1=rt["m1"][:], op=ALU.subtract,
                  R=["rm1", "rm2"], W=["rw1"])
            mk.op("act", "activation", out=rt["w1"][:], in_=rt["w1"][:], func=AF.Exp, R=["rw1"], W=["rw1"])
            mk.op("dve", "tensor_scalar_add", out=rt["w1"][:], in0=rt["w1"][:], scalar1=1.0, R=["rw1"], W=["rw1"])
            mk.op("dve", "reciprocal", out=rt["w1"][:], in_=rt["w1"][:], R=["rw1"], W=["rw1"])
            mk.op("dve", "tensor_tensor", out=rt["w1"][:], in0=rt["w1"][:], in1=rt["gs"][:], op=ALU.mult,
                  R=["rw1", "rgs"], W=["rw1"])
            mk.op("dve", "tensor_tensor", out=rt["w2"][:], in0=rt["gs"][:], in1=rt["w1"][:], op=ALU.subtract,
                  R=["rw1", "rgs"], W=["rw2"])
            mk.op("dve", "tensor_scalar", out=rt["gw"][:], in0=rt["oh1"][:], scalar1=rt["w1"][:, 0:1], scalar2=None,
                  op0=ALU.mult, R=["roh1", "rw1"], W=["rgw"])
            mk.op("dve", "scalar_tensor_tensor", out=rt["gw"][:], in0=rt["oh2"][:], scalar=rt["w2"][:, 0:1],
                  in1=rt["gw"][:], op0=ALU.mult, op1=ALU.add, R=["roh2", "rw2", "rgw"], W=["rgw"])
            for g in range(4):
                mk.op("dve", "tensor_scalar", out=gates[:, t, g * 4:(g + 1) * 4], in0=rt["gw"][:],
                      scalar1=rt["goh"][:, g:g + 1], scalar2=None, op0=ALU.mult, R=["rgw", "rgoh"], W=[f"gates{t}"])
        while ntask < len(tasks):
            do_task(ntask)
            ntask += 1
        mk.barrier()
        P.close()

    def moe_part(l, tiles, h2T, gates):
        P = Pool(nc, f"me{l}")
        TB = 8
        blocks = [(tiles[k], min(TB, len(tiles) - k)) for k in range(0, len(tiles), TB)]
        acc = P.sb([128, TB, D], F32, "acc")
        xt = [P.sb([128, D], F32, "xt") for _ in range(2)]
        w1b = [P.sb([128, 8, 512], BF16, "w1b") for _ in range(2)]
        w3b = [P.sb([128, 8, 512], BF16, "w3b") for _ in range(2)]
        w2b = [P.sb([128, 4, D], BF16, "w2b") for _ in range(2)]
        sl = [P.sb([128, 512], F32, "sl") for _ in range(2)]
        actT = [P.sb([128, 4, 512], BF16, "actT") for _ in range(2)]
        nw = 0
        for (tb0, tn) in blocks:
            ntok = tn * 128
            sblocks = [(s0, min(512, ntok - s0)) for s0 in range(0, ntok, 512)]
            hk = [f"h2T{tb0 + tt}" for tt in range(tn)]
            for e in range(16):
                wi = nw % 2
                nw += 1
                mk.dma("sp", w1b[wi][:].rearrange("p c f -> p (c f)"), W1B[e], R=[f"W1B{e}"], W=[f"w1b{wi}"])
                mk.dma("sp", w3b[wi][:].rearrange("p c f -> p (c f)"), W3B[e], R=[f"W3B{e}"], W=[f"w3b{wi}"])
                mk.dma("sp", w2b[wi][:].rearrange("p c f -> p (c f)"), W2B[e], R=[f"W2B{e}"], W=[f"w2b{wi}"])
                for (s0, sw) in sblocks:
                    ai = (s0 // 512) % 2
                    h0 = tb0 * 128 + s0
                    for fcn in range(4):
                        for k in range(8):
                            mk.op("pe", "matmul", PS[0][:, 0:sw], lhsT=w1b[wi][:, k, fcn * 128:(fcn + 1) * 128],
                                  rhs=h2T[:, k, h0:h0 + sw], start=(k == 0), stop=(k == 7), R=[f"w1b{wi}"] + hk, W=["ps0"])
                        for k in range(8):
                            mk.op("pe", "matmul", PS[1][:, 0:sw], lhsT=w3b[wi][:, k, fcn * 128:(fcn + 1) * 128],
                                  rhs=h2T[:, k, h0:h0 + sw], start=(k == 0), stop=(k == 7), R=[f"w3b{wi}"] + hk, W=["ps1"])
                        si = fcn % 2
                        mk.op("act", "activation", out=sl[si][:, 0:sw], in_=PS[0][:, 0:sw], func=AF.Silu, R=["ps0"],
                              W=[f"sl{si}"])
                        mk.op("dve", "tensor_tensor", out=actT[ai][:, fcn, 0:sw], in0=sl[si][:, 0:sw], in1=PS[1][:, 0:sw],
                              op=ALU.mult, R=[f"sl{si}", "ps1"], W=[f"actT{ai}"])
                    for q in range(sw // 128):
                        tt = s0 // 128 + q
                        t = tb0 + tt
                        for cb in range(2):
                            pb = 2 + cb
                            for fcn in range(4):
                                mk.op("pe", "matmul", PS[pb][:, :], lhsT=actT[ai][:, fcn, q * 128:(q + 1) * 128],
                                      rhs=w2b[wi][:, fcn, cb * 512:(cb + 1) * 512], start=(fcn == 0), stop=(fcn == 3),
                                      R=[f"actT{ai}", f"w2b{wi}"], W=[f"ps{pb}"])
                            if e == 0:
                                mk.op("dve", "tensor_scalar", out=acc[:, tt, cb * 512:(cb + 1) * 512], in0=PS[pb][:, :],
                                      scalar1=gates[:, t, e:e + 1], scalar2=None, op0=ALU.mult,
                                      R=[f"ps{pb}", f"gates{t}"], W=[f"acc{tt}"])
                            else:
                                mk.op("dve", "scalar_tensor_tensor", out=acc[:, tt, cb * 512:(cb + 1) * 512],
                                      in0=PS[pb][:, :], scalar=gates[:, t, e:e + 1], in1=acc[:, tt, cb * 512:(cb + 1) * 512],
                                      op0=ALU.mult, op1=ALU.add, R=[f"ps{pb}", f"gates{t}", f"acc{tt}"], W=[f"acc{tt}"])
            for tt in range(tn):
                t = tb0 + tt
                j = cond_of(t)
                mk.op("pool", "tensor_tensor", out=acc[:, tt, :], in0=acc[:, tt, :], in1=GB[:, j, 1, :], op=ALU.mult,
                      R=[f"acc{tt}", "GB"], W=[f"acc{tt}"])
                i2 = tt % 2
                mk.dma("sp", xt[i2][:], XR[t * 128:(t + 1) * 128, :], R=[f"XR{t}"], W=[f"ext{i2}"])
                mk.op("dve", "tensor_tensor", out=acc[:, tt, :], in0=acc[:, tt, :], in1=xt[i2][:], op=ALU.add,
                      R=[f"acc{tt}", f"ext{i2}"], W=[f"acc{tt}"])
                mk.dma("pool", XR[t * 128:(t + 1) * 128, :], acc[:, tt, :], R=[f"acc{tt}"], W=[f"XR{t}"])
        mk.barrier()
        P.close()

    def final_phase():
        P = Pool(nc, "fin")
        fw = P.sb([128, D], F32, "fw")
        mk.dma("sp", fw[:], fnw, W=["fw"])
        xt = [P.sb([128, D], F32, "xt") for _ in range(2)]
        ot = [P.sb([128, D], F32, "ot") for _ in range(2)]
        junk = P.sb([128, D], BF16, "junk")
        ss = [P.sb([128, 1], F32, "ss") for _ in range(2)]
        for t in range(2, NT):
            i = t % 2
            mk.dma("sp", xt[i][:], XR[t * 128:(t + 1) * 128, :], R=[f"XR{t}"], W=[f"fxt{i}"])
            mk.op("act", "activation", out=junk[:], in_=xt[i][:], func=AF.Square, accum_out=ss[i][:], R=[f"fxt{i}"],
                  W=["fjunk", f"fss{i}"])
            mk.op("act", "activation", out=ss[i][:], in_=ss[i][:], func=AF.Sqrt, scale=1.0 / D, bias=EPS, R=[f"fss{i}"],
                  W=[f"fss{i}"])
            mk.op("dve", "reciprocal", out=ss[i][:], in_=ss[i][:], R=[f"fss{i}"], W=[f"fss{i}"])
            mk.op("dve", "scalar_tensor_tensor", out=ot[i][:], in0=xt[i][:], scalar=ss[i][:, 0:1], in1=fw[:], op0=ALU.mult,
                  op1=ALU.mult, R=[f"fxt{i}", f"fss{i}", "fw"], W=[f"fot{i}"])
            mk.dma("pool", yout[(t - 2) * 128:(t - 1) * 128, :], ot[i][:], R=[f"fot{i}"], W=["yout"])
        mk.barrier()
        P.close()

    stages = dict(mod=mod_phase, inproj=inproj_phase, a=mixer_a, b=mixer_b, c=mixer_c, d=mixer_d)
    return dict(nc=nc, mk=mk, stages=stages, merge=merge_moe_phase, final=final_phase, dbg=dbg,
                scr=dict(XR=XR, FM=FM, TM=TM, MG=MG, YT=YT))


def emit_all(prog, layers=NL, upto=None, skip=()):
    mk = prog["mk"]
    mk.barrier()
    done = False
    for l in range(layers):
        for s in ("mod", "inproj", "a", "b", "c", "d"):
            if s in skip:
                continue
            prog["stages"][s](l)
            if upto == (l, s):
                done = True
                break
        if done:
            break
        prog["merge"](l, l == NL - 1)
        if upto == (l, "merge"):
            done = True
            break
    if not done:
        prog["final"]()
    mk.barrier(engines=("sp",))


def _consts():
    j = np.arange(128)[:, None]
    i = np.arange(128)[None, :]
    same = (j // DL) == (i // DL)
    m = {}
    m["ident"] = (j == i)
    m["triF"] = (j <= i)
    m["triB"] = (j >= i)
    m["blkF"] = same & (j <= i)
    m["blkB"] = same & (j >= i)
    m["aftF"] = same & (j > i)
    m["befB"] = same & (j < i)
    m["diffF"] = np.maximum(i - j, 0)
    m["diffB"] = np.maximum(j - i, 0)
    m["maskF"] = (i >= j)
    m["maskB"] = (j > i)
    m["posF"] = np.broadcast_to(i + 1, (128, 128))
    m["posB"] = np.broadcast_to(128 - i, (128, 128))
    m["negF4"] = np.tile(np.where(j <= i, 0.0, -30000.0), (1, 4))
    m["negB4"] = np.tile(np.where(j >= i, 0.0, -30000.0), (1, 4))
    m["mblkF4"] = np.tile(same & (j <= i), (1, 4))
    m["mblkB4"] = np.tile(same & (j >= i), (1, 4))
    m["kpos"] = np.concatenate([127 - j, j], 1)
    m["ones"] = np.ones((128, 128))
    sel = np.zeros((128, 256))
    sel[0, 0:128] = 1.0
    sel[1, 128:256] = 1.0
    m["sel"] = sel
    m["subm"] = np.concatenate([(j // DL) == s_ for s_ in range(128 // DL)], 1)
    qm = np.zeros((128, 2, 5, 128), np.float32)
    for hh_ in range(2):
        for s_ in range(5):
            colsel = np.ones(128, bool) if s_ == 4 else (np.arange(128) // DL == s_)
            qm[hh_ * 64:(hh_ + 1) * 64, hh_, s_, :] = colsel[None, :]
    m["qmask"] = qm.reshape(128, 1280)
    out = np.zeros((128, NCST), np.float32)
    for k, (o, w) in CST.items():
        out[:, o:o + w] = np.asarray(m[k], np.float32)
    return out


def _rope_tables(flip=False):
    n = 16
    inv = np.power(np.float32(10000.0), -np.arange(n, dtype=np.float32) / n).astype(np.float32)
    t = np.arange(4096)
    row = (t // 64).astype(np.float32)
    col = (t % 64).astype(np.float32)
    ang = np.concatenate([row[:, None] * inv, col[:, None] * inv], -1)
    cos = np.cos(ang).astype(np.float32).T
    sin = np.sin(ang).astype(np.float32).T
    if flip:
        cos, sin = cos[:, ::-1], sin[:, ::-1]
    Cc = np.ones((128, T), np.float32)
    Ss = np.zeros((128, T), np.float32)
    for hh in range(2):
        Cc[hh * 64:hh * 64 + 32, 256:] = cos
        Cc[hh * 64 + 32:hh * 64 + 64, 256:] = cos
        Ss[hh * 64:hh * 64 + 32, 256:] = -sin
        Ss[hh * 64 + 32:hh * 64 + 64, 256:] = sin
    return Cc, Ss


def prep_shared(inp, flip=False):
    f = np.float32
    w_in = np.asarray(inp["w_in"], f)
    offs = {}
    o = 0
    for name, w in (("a_x", 256), ("a_g", 256), ("b_q", 256), ("b_k", 256), ("b_v", 256), ("b_g", 256), ("c_q", 256),
                    ("c_k", 256), ("c_v", 256), ("c_o", 256), ("c_gates", 16), ("d_q", 256), ("d_ff", 256),
                    ("d_fb", 256), ("d_i", 256), ("d_g", 256), ("merge", 4096)):
        offs[name] = (o, w)
        o += w

    def cols(n):
        a, w = offs[n]
        return w_in[:, :, a:a + w]

    perm = np.concatenate([np.arange(h * 64 + 32, h * 64 + 64).tolist() + np.arange(h * 64, h * 64 + 32).tolist()
                           for h in range(4)]).astype(np.int64)
    w_fm = np.concatenate([cols("a_x"), cols("a_g"), cols("b_q"), cols("b_q")[:, :, perm], cols("b_k"),
                           cols("b_k")[:, :, perm], cols("c_q"), cols("c_k")], -1)
    gperm = np.array([8, 9, 10, 11, 12, 13, 14, 15, 0, 1, 2, 3, 4, 5, 6, 7]) if flip else np.arange(16)
    dfa, dfb = ("d_fb", "d_ff") if flip else ("d_ff", "d_fb")
    w_tm = np.concatenate([cols("b_v"), cols("b_g"), cols("c_v"), cols("c_o"), cols("d_q"), cols(dfa), cols(dfb),
                           cols("d_i"), cols("d_g"), cols("c_gates")[:, :, gperm], cols("merge")], -1)
    sh = {}
    sh["w_mod"] = np.ascontiguousarray(inp["w_mod"], f)
    bm = np.asarray(inp["b_mod"], f)
    sh["bmod_c"] = np.ascontiguousarray(bm.reshape(NL, 48, 128).transpose(0, 2, 1))
    sh["bmod_r"] = np.ascontiguousarray(bm.reshape(NL, 1, 6144))
    sh["w_fm"] = np.ascontiguousarray(w_fm)
    sh["w_tm"] = np.ascontiguousarray(w_tm)
    acw = np.asarray(inp["a_conv_w"], f)
    zt_ = np.zeros_like(acw[:, :1])
    acw = np.concatenate([zt_, acw[:, ::-1]], 1) if flip else np.concatenate([acw, zt_], 1)
    sh["a_cw"] = np.ascontiguousarray(acw.reshape(NL, 5, 2, 128).transpose(0, 3, 2, 1))
    sh["a_cb"] = np.ascontiguousarray(np.asarray(inp["a_conv_b"], f).reshape(NL, 2, 128).transpose(0, 2, 1))
    gw = np.asarray(inp["a_gate_w"], f)
    agw = np.zeros((NL, 128, 2, 2, 2, 128), f)
    for c in range(2):
        for hh in range(2):
            agw[:, hh * 64:(hh + 1) * 64, :, :, c, hh * 64:(hh + 1) * 64] = gw[:, :, :, 2 * c + hh].transpose(0, 3, 1, 2, 4)
    sh["a_gw"] = np.ascontiguousarray(agw[:, :, ::-1]) if flip else agw
    gb = np.asarray(inp["a_gate_b"], f)
    if flip:
        gb = gb[:, ::-1]
    sh["a_gb"] = np.ascontiguousarray(gb.reshape(NL, 2, 2, 2, 128).transpose(0, 4, 1, 2, 3))
    lam = np.asarray(inp["a_lambda"], f)
    if flip:
        lam = lam[:, ::-1]
    sh["a_lam"] = np.ascontiguousarray(lam.reshape(NL, 2, 2, 128).transpose(0, 3, 1, 2))
    th = np.asarray(inp["b_theta"], f)
    if flip:
        th = th[:, ::-1]
    thp = np.zeros((NL, 128, 2, 2), f)
    for c in range(2):
        for hh in range(2):
            thp[:, hh * 64:(hh + 1) * 64, :, c] = th[:, None, :, 2 * c + hh]
    sh["b_thp"] = thp
    sh["b_thh"] = np.ascontiguousarray(np.broadcast_to(th[:, None], (NL, 128, 2, 4)))
    ccw = np.asarray(inp["c_conv_w"], f)
    zt_ = np.zeros_like(ccw[:, :1])
    ccw = np.concatenate([zt_, ccw[:, ::-1]], 1) if flip else np.concatenate([ccw, zt_], 1)
    sh["c_cw"] = np.ascontiguousarray(ccw.reshape(NL, 5, 4, 128).transpose(0, 3, 2, 1))
    sh["c_cb"] = np.ascontiguousarray(np.asarray(inp["c_conv_b"], f).reshape(NL, 4, 128).transpose(0, 2, 1))
    sh["c_gb"] = np.ascontiguousarray(np.broadcast_to(np.asarray(inp["c_gate_b"], f).reshape(NL, 1, 16)[:, :, gperm], (NL, 128, 16)))
    sh["d_lbr"] = np.ascontiguousarray(np.broadcast_to(np.asarray(inp["d_lb"], f)[None], (128, 2, 256)))
    sh["w_branch"] = np.ascontiguousarray(inp["w_branch"], f)
    sh["w_out"] = np.ascontiguousarray(inp["w_out"], f)
    sh["moe_wgr"] = np.ascontiguousarray(np.concatenate([np.asarray(inp["moe_w_group"], f), np.asarray(inp["moe_w_router"], f)], -1))
    sh["moe_bgr"] = np.ascontiguousarray(np.concatenate([np.asarray(inp["moe_b_group"], f), np.asarray(inp["moe_b_router"], f)], -1).reshape(NL, 1, 20))
    sh["moe_w1"] = np.ascontiguousarray(inp["moe_w1"], f)
    sh["moe_w3"] = np.ascontiguousarray(inp["moe_w3"], f)
    sh["moe_w2"] = np.ascontiguousarray(inp["moe_w2"], f)
    sh["fnw"] = np.ascontiguousarray(np.broadcast_to(np.asarray(inp["final_norm_w"], f)[None], (128, D)))
    sh["cst"] = _consts()
    sh["ropeC"], sh["ropeS"] = _rope_tables(flip)
    return sh


def prep_core(inp, b, flip=False):
    f = np.float32
    d = {}
    cx, xx = np.asarray(inp["ctx"][b], f), np.asarray(inp["x"][b], f)
    if flip:
        cx, xx = cx[::-1], xx[::-1]
    d["xin"] = np.ascontiguousarray(np.concatenate([cx, xx], 0))
    cv = np.stack([np.asarray(inp["c_ctx"], f), np.asarray(inp["c"][b], f)], -1)
    d["cvec"] = np.ascontiguousarray(cv.reshape(8, 128, 2).transpose(1, 0, 2))
    return d


_PROG = None


def kernel(**inputs):
    global _PROG
    if _PROG is None:
        _PROG = build_program()
        emit_all(_PROG)
    nc = _PROG["nc"]
    shs = [prep_shared(inputs, False), prep_shared(inputs, True)]
    in_maps = []
    for core in range(8):
        fl = core >= 4
        m = dict(shs[1 if fl else 0])
        m.update(prep_core(inputs, core % 4, fl))
        in_maps.append(m)
    res = run_bass_kernel_spmd(nc, in_maps, core_ids=list(range(8)))
    out = np.empty((4, 4096, D), np.float32)
    for b in range(4):
        out[b, :HALF_OUT] = np.asarray(res.results[b]["yout"], np.float32)[:HALF_OUT]
        out[b, HALF_OUT:] = np.asarray(res.results[b + 4]["yout"], np.float32)[:4096 - HALF_OUT][::-1]
    return out
```

```python
import contextlib
import numpy as np
import ml_dtypes
import concourse.bass as bass
import concourse.mybir as mybir
from concourse.bass_utils import run_bass_kernel_spmd

F32 = mybir.dt.float32
BF16 = mybir.dt.bfloat16
AF = mybir.ActivationFunctionType
ALU = mybir.AluOpType
AX = mybir.AxisListType

T = 4352
NT = 34
D = 1024
EPS = 1e-6
NL = 2
HALF_OUT = 2048
DL = 32
DNS = 128 // DL
DBG = dict(maxit=None, core=True, fin=True, heads=(0, 1, 2, 3))
TMW = 2320
TM_OFF = dict(b_v=0, b_g=256, c_v=512, c_o=768, d_q=1024, d_ff=1280, d_fb=1536, d_i=1792, d_g=2048, c_gates=2304)
FM_OFF = dict(a_x=0, a_g=2, b_q=4, b_qp=6, b_k=8, b_kp=10, c_q=12, c_k=14)

CST = {}
_off = 0
for _n, _w in (("ident", 128), ("triF", 128), ("triB", 128), ("blkF", 128), ("blkB", 128), ("aftF", 128),
               ("befB", 128), ("diffF", 128), ("diffB", 128), ("maskF", 128), ("maskB", 128), ("posF", 128),
               ("posB", 128), ("negF4", 512), ("negB4", 512), ("mblkF4", 512), ("mblkB4", 512), ("kpos", 2),
               ("ones", 128), ("sel", 256), ("subm", 4), ("qmask", 1280)):
    CST[_n] = (_off, _w)
    _off += _w
NCST = _off


class MK:
    SEM_ROT = 30000

    def __init__(self, nc, ndma=8):
        self.nc = nc
        self.engs = {"pe": nc.tensor, "act": nc.scalar, "dve": nc.vector, "pool": nc.gpsimd, "sp": nc.sync}
        self._ctxs = []
        self.nsem = 0
        self.sem = {}
        self.cnt = {}
        for e in ("pe", "act", "dve", "pool"):
            self.sem[e] = self._newsem("s_" + e)
            self.cnt[e] = 0
        self.seen = {e: {} for e in self.engs}
        self.dq = {}
        for q in ("sp", "pool"):
            self.dq[q] = {"i": 0, "slots": [[self._newsem(f"d_{q}{i}"), 0] for i in range(ndma)]}
        self.res = {}
        self.ninst = 0
        self.flip = 0

    def _newsem(self, name):
        self.nsem += 1
        cm = self.nc.semaphore(f"{name}_{self.nsem}")
        s = cm.__enter__()
        self._ctxs.append(cm)
        return s

    def _wait(self, eng, tok):
        sem, val = tok
        key = id(sem)
        if self.seen[eng].get(key, 0) >= val:
            return
        self.engs[eng].wait_ge(sem, val)
        self.seen[eng][key] = val

    def _deps(self, R, W):
        deps = []
        for k in R:
            st = self.res.get(k)
            if st and st[0] is not None:
                deps.append(st[0])
        for k in W:
            st = self.res.get(k)
            if st:
                if st[0] is not None:
                    deps.append(st[0])
                deps.extend(st[1])
        return deps

    def _record(self, tok, R, W):
        for k in R:
            st = self.res.setdefault(k, [None, []])
            st[1] = [t for t in st[1] if t[0] is not tok[0]] + [tok]
        for k in W:
            self.res[k] = [tok, []]

    def op(self, eng, method, *args, R=(), W=(), **kw):
        for tok in self._deps(R, W):
            if eng == "pe" and tok[0] is self.sem["pe"]:
                continue
            self._wait(eng, tok)
        ins = getattr(self.engs[eng], method)(*args, **kw)
        if self.cnt[eng] >= self.SEM_ROT:
            self.sem[eng] = self._newsem("s_" + eng)
            self.cnt[eng] = 0
        self.cnt[eng] += 1
        ins.then_inc(self.sem[eng], 1)
        tok = (self.sem[eng], self.cnt[eng])
        self._record(tok, R, W)
        self.ninst += 1
        return tok

    def dma(self, q, out, in_, R=(), W=(), **kw):
        d = self.dq[q]
        slot = d["slots"][d["i"] % len(d["slots"])]
        d["i"] += 1
        if slot[1] > 0:
            self._wait(q, (slot[0], slot[1]))
        if slot[1] >= self.SEM_ROT:
            slot[0] = self._newsem("d_" + q)
            slot[1] = 0
        for tok in self._deps(R, W):
            self._wait(q, tok)
        ins = self.engs[q].dma_start(out=out, in_=in_, **kw)
        slot[1] += 16
        ins.then_inc(slot[0], 16)
        tok = (slot[0], slot[1])
        self._record(tok, R, W)
        self.ninst += 1
        return tok

    def barrier(self, engines=("pe", "act", "dve", "pool", "sp")):
        toks = []
        for q, d in self.dq.items():
            for slot in d["slots"]:
                if slot[1] > 0:
                    toks.append((slot[0], slot[1]))
        for e in ("pe", "act", "dve", "pool"):
            if self.cnt[e] > 0:
                toks.append((self.sem[e], self.cnt[e]))
        for e in engines:
            for tok in toks:
                if e in self.sem and tok[0] is self.sem[e]:
                    continue
                self._wait(e, tok)

    def ev(self, out, in_, R=(), W=(), func=None, **kw):
        if func is not None:
            return self.op("act", "activation", out=out, in_=in_, func=func, R=R, W=W, **kw)
        self.flip ^= 1
        if self.flip:
            return self.op("act", "activation", out=out, in_=in_, func=AF.Copy, R=R, W=W)
        return self.op("dve", "tensor_copy", out=out, in_=in_, R=R, W=W)


class Pool:
    def __init__(self, nc, tag):
        self.nc = nc
        self.tag = tag
        self.stack = contextlib.ExitStack()
        self.n = 0

    def sb(self, shape, dt=F32, name=None):
        self.n += 1
        return self.stack.enter_context(self.nc.sbuf_tensor(f"{self.tag}_{name or 't'}{self.n}", list(shape), dt))

    def close(self):
        self.stack.close()


def build_program(debug=()):
    nc = bass.Bass("TRN2", target_bir_lowering=False)

    def din(name, shape, dt=F32):
        return nc.dram_tensor(name, list(shape), dt, kind="ExternalInput").ap()

    def dscr(name, shape, dt=F32):
        return nc.dram_tensor(name, list(shape), dt, kind="Internal").ap()

    xin = din("xin", [T, D])
    cvec = din("cvec", [128, 8, 2])
    w_mod = din("w_mod", [NL, D, 6144])
    bmod_c = din("bmod_c", [NL, 128, 48])
    bmod_r = din("bmod_r", [NL, 1, 6144])
    w_fm = din("w_fm", [NL, D, 2048])
    w_tm = din("w_tm", [NL, D, TMW + 4096])
    a_cw = din("a_cw", [NL, 128, 2, 5])
    a_cb = din("a_cb", [NL, 128, 2])
    a_gw = din("a_gw", [NL, 128, 2, 2, 2, 128])
    a_gb = din("a_gb", [NL, 128, 2, 2, 2])
    a_lam = din("a_lam", [NL, 128, 2, 2])
    b_thp = din("b_thp", [NL, 128, 2, 2])
    b_thh = din("b_thh", [NL, 128, 2, 4])
    c_cw = din("c_cw", [NL, 128, 4, 5])
    c_cb = din("c_cb", [NL, 128, 4])
    c_gb = din("c_gb", [NL, 128, 16])
    d_lbr = din("d_lbr", [128, 2, 256])
    w_branch = din("w_branch", [NL, 4, 256, D])
    w_out = din("w_out", [NL, D, D])
    moe_wgr = din("moe_wgr", [NL, D, 20])
    moe_bgr = din("moe_bgr", [NL, 1, 20])
    moe_w1 = din("moe_w1", [NL, 16, D, 512])
    moe_w3 = din("moe_w3", [NL, 16, D, 512])
    moe_w2 = din("moe_w2", [NL, 16, 512, D])
    fnw = din("fnw", [128, D])
    cst_d = din("cst", [128, NCST])
    ropeC_d = din("ropeC", [128, T])
    ropeS_d = din("ropeS", [128, T])
    yout = nc.dram_tensor("yout", [HALF_OUT, D], F32, kind="ExternalOutput").ap()

    XR = dscr("XR", [T, D])
    FM = dscr("FM", [16, 128, T])
    TM = dscr("TM", [T, TMW])
    MG = dscr("MG", [T, 4096], BF16)
    YT = dscr("YT", [4, 2, 128, T], BF16)
    dbg = {}
    for name, shape, dt in debug:
        dbg[name] = nc.dram_tensor("dbg_" + name, list(shape), dt, kind="ExternalOutput").ap()

    mk = MK(nc)
    G = Pool(nc, "g")

    PS = [nc.psum_tensor(f"ps{i}", [128, 512], F32).__enter__() for i in range(6)]
    PQ = [nc.psum_tensor(f"pq{i}", [128, 1024], BF16).__enter__() for i in range(2)]

    cst = G.sb([128, NCST], F32, "cst")
    identb = G.sb([128, 128], BF16, "identb")
    sT = G.sb([128, 8, 2], F32, "sT")
    MODC = G.sb([128, 4, 8, 2], F32, "MODC")
    GB = G.sb([128, 2, 2, D], F32, "GB")
    mk.dma("sp", cst[:], cst_d, W=["cst"])
    mk.dma("sp", sT[:], cvec, W=["sT"])

    def C(name, rows=slice(0, 128)):
        o, w = CST[name]
        return cst[rows, o:o + w]

    mk.op("dve", "tensor_copy", out=identb[:], in_=C("ident"), R=["cst"], W=["identb"])
    mk.op("act", "activation", out=sT[:], in_=sT[:], func=AF.Silu, R=["sT"], W=["sT"])
    for t in range(NT):
        mk.dma("sp", XR[t * 128:(t + 1) * 128, :], xin[t * 128:(t + 1) * 128, :], W=[f"XR{t}"])

    def cond_of(t):
        return 0 if t < 2 else 1

    def mod_phase(l):
        P = Pool(nc, f"mod{l}")
        wblk = P.sb([128, 8, 1024], F32, "wblk")
        bmc = P.sb([128, 48], F32, "bmc")
        bmr = P.sb([1, 6144], F32, "bmr")
        GR = P.sb([2, 2, D], F32, "GR")
        mk.dma("sp", bmc[:], bmod_c[l], W=["bmc"])
        mk.dma("sp", bmr[:], bmod_r[l], W=["bmr"])
        sel = C("sel", slice(0, 2))
        for m in range(6):
            mk.dma("sp", wblk[:], w_mod[l].rearrange("(c p) f -> p c f", p=128)[:, :, m * 1024:(m + 1) * 1024],
                   W=["wblk"])
            if m in (0, 1, 3, 4):
                m4 = {0: 0, 1: 1, 3: 2, 4: 3}[m]
                for c in range(8):
                    for k in range(8):
                        mk.op("pe", "matmul", PS[0][:, c * 2:(c + 1) * 2], lhsT=wblk[:, k, c * 128:(c + 1) * 128],
                              rhs=sT[:, k, :], start=(k == 0), stop=(k == 7), R=["wblk", "sT"], W=["ps0"])
                mk.op("dve", "tensor_tensor", out=MODC[:, m4, :, :],
                      in0=PS[0][:, 0:16].rearrange("p (c j) -> p c j", j=2),
                      in1=bmc[:, m * 8:(m + 1) * 8].unsqueeze(2).to_broadcast([128, 8, 2]), op=ALU.add,
                      R=["ps0", "bmc"], W=["MODC"])
                if m in (1, 4):
                    mk.op("dve", "tensor_scalar_add", out=MODC[:, m4, :, :], in0=MODC[:, m4, :, :], scalar1=1.0,
                          R=["MODC"], W=["MODC"])
            else:
                mi = 0 if m == 2 else 1
                for cb in range(2):
                    for k in range(8):
                        mk.op("pe", "matmul", PS[1][0:2, :], lhsT=sT[:, k, :], rhs=wblk[:, k, cb * 512:(cb + 1) * 512],
                              start=(k == 0), stop=False, R=["wblk", "sT"], W=["ps1"])
                    mk.op("pe", "matmul", PS[1][0:2, :], lhsT=sel[0:1, 0:2],
                          rhs=bmr[0:1, m * 1024 + cb * 512: m * 1024 + (cb + 1) * 512], start=False, stop=True,
                          R=["bmr", "cst"], W=["ps1"])
                    mk.op("dve", "tensor_copy", out=GR[0:2, mi, cb * 512:(cb + 1) * 512], in_=PS[1][0:2, :],
                          R=["ps1"], W=["GR"])
        for j in range(2):
            for mi in range(2):
                for cb in range(2):
                    mk.op("pe", "matmul", PS[1][:, :], lhsT=sel[0:2, j * 128:(j + 1) * 128],
                          rhs=GR[0:2, mi, cb * 512:(cb + 1) * 512], start=True, stop=True, R=["GR", "cst"], W=["ps1"])
                    mk.op("act", "activation", out=GB[:, j, mi, cb * 512:(cb + 1) * 512], in_=PS[1][:, :], func=AF.Copy,
                          R=["ps1"], W=["GB"])
        mk.barrier()
        P.close()

    def make_norm(P, nbuf=2):
        st = dict(junk=P.sb([128, D], BF16, "junk"), ss=[P.sb([128, 1], F32, "ss") for _ in range(nbuf)],
                  xn=[P.sb([128, D], BF16, "xn") for _ in range(nbuf)],
                  tmp=[P.sb([128, 8, 128], F32, "tmp") for _ in range(nbuf)], i=0, nbuf=nbuf)

        def norm(xt_ap, xt_key, j, msc, msh, h_out, h_key):
            i = st["i"] % st["nbuf"]
            st["i"] += 1
            ss, xn, tmp = st["ss"][i], st["xn"][i], st["tmp"][i]
            mk.op("act", "activation", out=st["junk"][:], in_=xt_ap, func=AF.Square, accum_out=ss[:],
                  R=[xt_key], W=["junk", f"ss{i}"])
            mk.op("act", "activation", out=ss[:], in_=ss[:], func=AF.Sqrt, scale=1.0 / D, bias=EPS,
                  R=[f"ss{i}"], W=[f"ss{i}"])
            mk.op("dve", "reciprocal", out=ss[:], in_=ss[:], R=[f"ss{i}"], W=[f"ss{i}"])
            mk.op("dve", "tensor_scalar", out=xn[:], in0=xt_ap, scalar1=ss[:, 0:1], scalar2=None, op0=ALU.mult,
                  R=[xt_key, f"ss{i}"], W=[f"xn{i}"])
            for c in range(8):
                mk.op("pe", "transpose", out=PQ[i][:, c * 128:(c + 1) * 128], in_=xn[:, c * 128:(c + 1) * 128],
                      identity=identb[:], R=[f"xn{i}", "identb"], W=[f"pq{i}"])
            mk.op("dve", "tensor_tensor", out=tmp[:], in0=PQ[i][:, :].rearrange("p (c n) -> p c n", c=8),
                  in1=MODC[:, msc, :, j:j + 1].to_broadcast([128, 8, 128]), op=ALU.mult,
                  R=[f"pq{i}", "MODC"], W=[f"ntmp{i}"])
            mk.op("pool", "tensor_tensor", out=h_out, in0=tmp[:],
                  in1=MODC[:, msh, :, j:j + 1].to_broadcast([128, 8, 128]), op=ALU.add,
                  R=[f"ntmp{i}", "MODC"], W=[h_key])
        return norm

    def inproj_phase(l, last=False):
        P = Pool(nc, f"ip{l}")
        hT = P.sb([128, 8, T], BF16, "hT")
        xt = [P.sb([128, D], F32, "xt") for _ in range(2)]
        norm = make_norm(P)
        for t in range(NT):
            i = t % 2
            mk.dma("sp", xt[i][:], XR[t * 128:(t + 1) * 128, :], R=[f"XR{t}"], W=[f"xt{i}"])
            norm(xt[i][:], f"xt{i}", cond_of(t), 1, 0, hT[:, :, t * 128:(t + 1) * 128], f"hT{t}")
        hkeys = [f"hT{t}" for t in range(NT)]
        wf = [P.sb([128, 8, 512], F32, "wf")] * 2
        wb = [P.sb([128, 8, 512], BF16, "wb") for _ in range(2)]
        stg = [P.sb([128, T], F32, "stg")] * 2
        nblk = 0
        tblocks = [(i * 512, min(512, T - i * 512)) for i in range(9)]
        for cb in range(4):
            i = nblk % 2
            nblk += 1
            mk.dma("sp", wf[i][:], w_fm[l].rearrange("(c p) f -> p c f", p=128)[:, :, cb * 512:(cb + 1) * 512],
                   W=["wf"])
            mk.ev(wb[i][:], wf[i][:], R=["wf"], W=[f"wb{i}"])
            for sub in range(4):
                fc = cb * 4 + sub
                si = fc % 2
                for bi, (t0, tw) in enumerate(tblocks):
                    pb = bi % 2
                    for k in range(8):
                        mk.op("pe", "matmul", PS[pb][:, 0:tw], lhsT=wb[i][:, k, sub * 128:(sub + 1) * 128],
                              rhs=hT[:, k, t0:t0 + tw], start=(k == 0), stop=(k == 7),
                              R=[f"wb{i}"] + hkeys[t0 // 128:(t0 + tw) // 128], W=[f"ps{pb}"])
                    mk.ev(stg[si][:, t0:t0 + tw], PS[pb][:, 0:tw], R=[f"ps{pb}"], W=["stg"])
                mk.dma("pool", FM[fc], stg[si][:], R=["stg"], W=[f"FM{fc}"])
        cblocks = [(i * 512, 512) for i in range(4)] + [(2048, TMW - 2048)] + [(TMW + i * 512, 512) for i in range(8)]
        stt = [P.sb([128, 4, 512], F32, "stt") for _ in range(2)]
        stb = [P.sb([128, 4, 512], BF16, "stb") for _ in range(2)]
        tgroups = [(g * 4, min(4, NT - g * 4)) for g in range(9)]
        ns = 0
        for (c0, cw) in cblocks:
            i = nblk % 2
            nblk += 1
            mk.dma("sp", wf[i][:, :, 0:cw], w_tm[l].rearrange("(c p) f -> p c f", p=128)[:, :, c0:c0 + cw], W=["wf"])
            mk.ev(wb[i][:, :, 0:cw], wf[i][:, :, 0:cw], R=["wf"], W=[f"wb{i}"])
            is_mg = c0 >= TMW
            for (g0, gn) in tgroups:
                if is_mg and last and not any((g0 + q) in OUT_T for q in range(gn)):
                    continue
                si = ns % 2
                ns += 1
                for tt in range(gn):
                    t = g0 + tt
                    pb = 2 + (t % 2)
                    for k in range(8):
                        mk.op("pe", "matmul", PS[pb][:, 0:cw], lhsT=hT[:, k, t * 128:(t + 1) * 128],
                              rhs=wb[i][:, k, 0:cw], start=(k == 0), stop=(k == 7), R=[f"wb{i}", f"hT{t}"], W=[f"ps{pb}"])
                    if is_mg:
                        mk.ev(stb[si][:, tt, 0:cw], PS[pb][:, 0:cw], R=[f"ps{pb}"], W=[f"stb{si}"], func=AF.Sigmoid)
                    else:
                        mk.ev(stt[si][:, tt, 0:cw], PS[pb][:, 0:cw], R=[f"ps{pb}"], W=[f"stt{si}"])
                if is_mg:
                    mk.dma("pool", MG[g0 * 128:(g0 + gn) * 128, c0 - TMW:c0 - TMW + cw].rearrange("(t p) c -> p t c", p=128),
                           stb[si][:, 0:gn, 0:cw], R=[f"stb{si}"], W=["MG"])
                else:
                    mk.dma("pool", TM[g0 * 128:(g0 + gn) * 128, c0:c0 + cw].rearrange("(t p) c -> p t c", p=128),
                           stt[si][:, 0:gn, 0:cw], R=[f"stt{si}"], W=["TM"])
        mk.barrier()
        P.close()

    SEGS = [(0, 256), (256, T)]

    def conv_fm(u, src, w4, bcol, keyu, keysrc, wkeys):
        for (s0, e) in SEGS:
            mk.op("act", "activation", out=u[:, s0:e], in_=src[:, s0:e], func=AF.Identity, scale=w4[:, 2:3], bias=bcol,
                  R=[keysrc] + wkeys, W=[keyu])
            for k, sh in ((0, -2), (1, -1), (3, 1), (4, 2)):
                if sh < 0:
                    o, i_ = u[:, s0 - sh:e], src[:, s0:e + sh]
                else:
                    o, i_ = u[:, s0:e - sh], src[:, s0 + sh:e]
                mk.op("dve", "scalar_tensor_tensor", out=o, in0=i_, scalar=w4[:, k:k + 1], in1=o, op0=ALU.mult,
                      op1=ALU.add, R=[keysrc, keyu] + wkeys, W=[keyu])

    def mixer_a(l):
        P = Pool(nc, f"ma{l}")
        cw = P.sb([128, 2, 5], F32, "cw")
        cb = P.sb([128, 2], F32, "cb")
        gwf = P.sb([128, 2, 2, 2, 128], F32, "gwf")
        gwb = P.sb([128, 2, 2, 2, 128], BF16, "gwb")
        gb = P.sb([128, 2, 2, 2], F32, "gb")
        lam = P.sb([128, 2, 2], F32, "lam")
        c1 = P.sb([128, 2, 2], F32, "c1")
        mk.dma("sp", cw[:], a_cw[l], W=["a_cw"])
        mk.dma("sp", cb[:], a_cb[l], W=["a_cb"])
        mk.dma("sp", gwf[:], a_gw[l], W=["a_gwf"])
        mk.dma("sp", gb[:], a_gb[l], W=["a_gb"])
        mk.dma("sp", lam[:], a_lam[l], W=["a_lam"])
        mk.op("dve", "tensor_copy", out=gwb[:], in_=gwf[:], R=["a_gwf"], W=["a_gwb"])
        mk.op("act", "activation", out=c1[:], in_=lam[:], func=AF.Exp, scale=-1.0, R=["a_lam"], W=["a_c1"])
        mk.op("act", "activation", out=c1[:], in_=c1[:], func=AF.Ln, bias=1.0, R=["a_c1"], W=["a_c1"])
        mk.op("dve", "tensor_scalar", out=c1[:], in0=c1[:], scalar1=-8.0, scalar2=None, op0=ALU.mult, R=["a_c1"], W=["a_c1"])
        ax = P.sb([128, T], F32, "ax")
        ag = P.sb([128, T], F32, "ag")
        u = P.sb([128, T], F32, "u")
        ub = P.sb([128, T], BF16, "ub")
        aa = P.sb([128, T], F32, "aa")
        bt = P.sb([128, T], F32, "bt")
        hf = P.sb([128, T], F32, "hf")
        hb = P.sb([128, T], F32, "hb")
        r = [P.sb([128, 512], F32, "r") for _ in range(2)]
        gi = [P.sb([128, 512], F32, "gi") for _ in range(2)]
        yb = P.sb([128, T], BF16, "yb")
        tblocks = [(i * 512, min(512, T - i * 512)) for i in range(9)]
        for c in range(2):
            mk.dma("sp", ax[:], FM[FM_OFF["a_x"] + c], R=[f"FM{FM_OFF['a_x'] + c}"], W=["ax"])
            mk.dma("sp", ag[:], FM[FM_OFF["a_g"] + c], R=[f"FM{FM_OFF['a_g'] + c}"], W=["ag"])
            conv_fm(u, ax, cw[:, c, :], cb[:, c:c + 1], "u", "ax", ["a_cw", "a_cb"])
            mk.op("pool", "tensor_copy", out=ub[:], in_=u[:], R=["u"], W=["ub"])
            for d in range(2):
                for bi, (t0, tw) in enumerate(tblocks):
                    i = bi % 2
                    mk.op("pe", "matmul", PS[i][:, 0:tw], lhsT=gwb[:, d, 0, c, :], rhs=ub[:, t0:t0 + tw], start=True,
                          stop=True, R=["a_gwb", "ub"], W=[f"ps{i}"])
                    mk.op("pe", "matmul", PS[2 + i][:, 0:tw], lhsT=gwb[:, d, 1, c, :], rhs=ub[:, t0:t0 + tw], start=True,
                          stop=True, R=["a_gwb", "ub"], W=[f"ps{2 + i}"])
                    mk.op("act", "activation", out=r[i][:, 0:tw], in_=PS[i][:, 0:tw], func=AF.Sigmoid,
                          bias=gb[:, d, 0, c:c + 1], R=[f"ps{i}", "a_gb"], W=[f"r{i}"])
                    mk.op("act", "activation", out=gi[i][:, 0:tw], in_=PS[2 + i][:, 0:tw], func=AF.Sigmoid,
                          bias=gb[:, d, 1, c:c + 1], R=[f"ps{2 + i}", "a_gb"], W=[f"gi{i}"])
                    mk.op("act", "activation", out=aa[:, t0:t0 + tw], in_=r[i][:, 0:tw], func=AF.Exp,
                          scale=c1[:, d, c:c + 1], R=[f"r{i}", "a_c1"], W=["aa"])
                    mk.op("dve", "tensor_tensor", out=r[i][:, 0:tw], in0=aa[:, t0:t0 + tw], in1=aa[:, t0:t0 + tw],
                          op=ALU.mult, R=["aa", f"r{i}"], W=[f"r{i}"])
                    mk.op("dve", "tensor_scalar", out=r[i][:, 0:tw], in0=r[i][:, 0:tw], scalar1=-1.0, scalar2=1.0,
                          op0=ALU.mult, op1=ALU.add, R=[f"r{i}"], W=[f"r{i}"])
                    mk.op("act", "activation", out=r[i][:, 0:tw], in_=r[i][:, 0:tw], func=AF.Sqrt, R=[f"r{i}"], W=[f"r{i}"])
                    mk.op("dve", "tensor_tensor", out=gi[i][:, 0:tw], in0=gi[i][:, 0:tw], in1=r[i][:, 0:tw], op=ALU.mult,
                          R=[f"gi{i}", f"r{i}"], W=[f"gi{i}"])
                    mk.op("pool", "tensor_tensor", out=bt[:, t0:t0 + tw], in0=gi[i][:, 0:tw], in1=u[:, t0:t0 + tw],
                          op=ALU.mult, R=[f"gi{i}", "u"], W=["bt"])
                if d == 0:
                    mk.op("dve", "tensor_tensor_scan", out=hf[:, :], data0=aa[:, :], data1=bt[:, :], initial=0.0,
                          op0=ALU.mult, op1=ALU.add, R=["aa", "bt"], W=["hf"])
                else:
                    mk.op("dve", "tensor_tensor_scan", out=hb[:, 0:256][:, ::-1], data0=aa[:, 0:256][:, ::-1],
                          data1=bt[:, 0:256][:, ::-1], initial=0.0, op0=ALU.mult, op1=ALU.add, R=["aa", "bt"], W=["hb"])
                    mk.op("dve", "tensor_tensor_scan", out=hb[:, 256:T][:, ::-1], data0=aa[:, 256:T][:, ::-1],
                          data1=bt[:, 256:T][:, ::-1], initial=hb[:, 0:1], op0=ALU.mult, op1=ALU.add,
                          R=["aa", "bt", "hb"], W=["hb"])
            mk.op("act", "activation", out=ag[:], in_=ag[:], func=AF.Gelu, R=["ag"], W=["ag"])
            mk.op("dve", "tensor_tensor", out=hf[:], in0=hf[:], in1=hb[:], op=ALU.add, R=["hf", "hb"], W=["hf"])
            mk.op("dve", "tensor_tensor", out=yb[:], in0=hf[:], in1=ag[:], op=ALU.mult, R=["hf", "ag"], W=["yb"])
            mk.dma("pool", YT[0, c], yb[:], R=["yb"], W=[f"YT0{c}"])
        mk.barrier()
        P.close()

    def order_of(dr):
        return list(range(NT)) if dr == 0 else [1, 0] + list(range(NT - 1, 1, -1))

    OUT_T = list(range(2, 2 + HALF_OUT // 128))

    def plan(dr, last):
        if not last:
            return [(t, True) for t in order_of(dr)]
        if dr == 0:
            return [(0, False), (1, False)] + [(t, True) for t in OUT_T]
        return [(t, (t in OUT_T)) for t in order_of(dr)]

    def chunk_core(it, nsub, dr, QT, KT, QIT, KHs, V, vw, Gc, S, Sbf, maskD, maskkey, rkeys, PT, sk, full=True):
        pi = it % 2
        L = 128 // nsub
        okeys = ["ps2", "ps3"]
        Oh = [PS[2 + hh][:, 0:2 * vw].rearrange("p (c e) -> p c e", c=2) for hh in range(2)]
        if not DBG["core"]:
            return okeys, Oh
        for h in (DBG["heads"] if full else ()):
            c, hh = h // 2, h % 2
            rs = slice(hh * 64, (hh + 1) * 64)
            mk.op("pe", "matmul", PS[hh][:, c * 128:(c + 1) * 128], lhsT=KT[c][rs, :], rhs=QT[c][rs, :], start=True,
                  stop=True, R=rkeys, W=[f"ps{hh}"])
        PTv = PT[pi][:].rearrange("p (c x n) -> p c x n", c=2, x=2)
        Mv = maskD.rearrange("p (c x n) -> p c x n", c=2, x=2) if full else None
        for hh in (range(2) if full else ()):
            mk.op("dve", "tensor_tensor", out=PTv[:, :, hh, :], in0=PS[hh][:, 0:256].rearrange("p (c n) -> p c n", c=2),
                  in1=Mv[:, :, hh, :], op=ALU.mult, R=[f"ps{hh}", maskkey], W=[f"PT{pi}h{hh}"])
        ptk = [f"PT{pi}h0", f"PT{pi}h1"]
        subs = list(range(nsub)) if dr == 0 else list(range(nsub - 1, -1, -1))
        KVp = PS[4][:, 0:2 * vw].rearrange("p (c e) -> p c e", c=2)
        for s in subs:
            rows = slice(s * L, (s + 1) * L)
            for h in (DBG["heads"] if full else ()):
                c, hh = h // 2, h % 2
                rs = slice(hh * 64, (hh + 1) * 64)
                mk.op("pe", "matmul", Oh[hh][rows, c, :], lhsT=PT[pi][:, h * 128 + s * L:h * 128 + (s + 1) * L],
                      rhs=V[:, h, :], start=True, stop=False, R=[ptk[hh]] + rkeys, W=[okeys[hh]])
                mk.op("pe", "matmul", Oh[hh][rows, c, :], lhsT=QIT[c][rs, rows], rhs=Sbf[c][rs, :], start=False, stop=True,
                      R=rkeys + [f"{sk}Sbf{c}"], W=[okeys[hh]])
            for h in DBG["heads"]:
                c, hh = h // 2, h % 2
                mk.op("pe", "matmul", KVp[hh * 64:(hh + 1) * 64, c, :], lhsT=KHs(s)[:, h * 64:(h + 1) * 64],
                      rhs=V[:, h, :], start=True, stop=True, R=rkeys, W=["ps4kv"])
            for c in range(2):
                mk.op("dve", "scalar_tensor_tensor", out=S[c][:], in0=S[c][:], scalar=Gc[:, c, s:s + 1], in1=KVp[:, c, :],
                      op0=ALU.mult, op1=ALU.add, R=[f"{sk}S{c}", "ps4kv"] + rkeys, W=[f"{sk}S{c}"])
                mk.op("act", "activation", out=Sbf[c][:], in_=S[c][:], func=AF.Copy, R=[f"{sk}S{c}"], W=[f"{sk}Sbf{c}"])
        return okeys, Oh

    def hview(ap256, hh):
        return ap256.rearrange("p (c x e) -> p c x e", c=2, x=2)[:, :, hh, :]

    def make_finalize(P, n, yTb):
        st = dict(i=0)
        cent = [P.sb([128, 4, 64], F32, "cent") for _ in range(2)]
        sq = P.sb([128, 4, 64], F32, "sq")
        mm = [P.sb([128, 4], F32, "mm") for _ in range(2)]
        vv = [P.sb([128, 4], F32, "vv") for _ in range(2)]
        yy = [P.sb([128, 256], BF16, "yy") for _ in range(2)]

        def fin(tot, totkey, center, gate, gatekey, t):
            i = st["i"] % 2
            st["i"] += 1
            tv = tot.rearrange("p (h e) -> p h e", h=4)
            tk = list(totkey) if isinstance(totkey, (list, tuple)) else [totkey]
            src, skeys = tv, tk
            if center:
                mk.op("dve", "tensor_reduce", out=mm[i][:], in_=tv, axis=AX.X, op=ALU.add, R=tk, W=[f"fmm{i}"])
                mk.op("dve", "tensor_scalar", out=mm[i][:], in0=mm[i][:], scalar1=-1.0 / 64, scalar2=None, op0=ALU.mult,
                      R=[f"fmm{i}"], W=[f"fmm{i}"])
                mk.op("dve", "tensor_tensor", out=cent[i][:], in0=tv, in1=mm[i][:].unsqueeze(2).to_broadcast([128, 4, 64]),
                      op=ALU.add, R=tk + [f"fmm{i}"], W=[f"fcent{i}"])
                src, skeys = cent[i][:], [f"fcent{i}"]
            mk.op("pool", "tensor_tensor", out=sq[:], in0=src, in1=src, op=ALU.mult, R=skeys, W=["fsq"])
            mk.op("dve", "tensor_reduce", out=vv[i][:], in_=sq[:], axis=AX.X, op=ALU.add, R=["fsq"], W=[f"fvv{i}"])
            mk.op("act", "activation", out=vv[i][:], in_=vv[i][:], func=AF.Sqrt, scale=1.0 / 64, bias=EPS,
                  R=[f"fvv{i}"], W=[f"fvv{i}"])
            mk.op("dve", "reciprocal", out=vv[i][:], in_=vv[i][:], R=[f"fvv{i}"], W=[f"fvv{i}"])
            mk.op("dve", "tensor_tensor", out=cent[i][:], in0=src, in1=vv[i][:].unsqueeze(2).to_broadcast([128, 4, 64]),
                  op=ALU.mult, R=skeys + [f"fvv{i}"], W=[f"fcent{i}"])
            mk.op("dve", "tensor_tensor", out=yy[i][:], in0=cent[i][:].rearrange("p h e -> p (h e)"), in1=gate,
                  op=ALU.mult, R=[f"fcent{i}", gatekey], W=[f"fyy{i}"])
            for c in range(2):
                mk.op("pe", "transpose", out=PQ[1][:, (i * 2 + c) * 128:(i * 2 + c + 1) * 128],
                      in_=yy[i][:, c * 128:(c + 1) * 128], identity=identb[:], R=[f"fyy{i}", "identb"], W=[f"pq1f{i}"])
            mk.op("act", "activation", out=yTb[:, :, t * 128:(t + 1) * 128],
                  in_=PQ[1][:, i * 256:(i + 1) * 256].rearrange("p (c n) -> p c n", c=2), func=AF.Copy,
                  R=[f"pq1f{i}"], W=["yTb"])
        return fin

    def mixer_b(l, last=False):
        P = Pool(nc, f"mb{l}")
        QR = [P.sb([128, T], BF16, "QR") for _ in range(2)]
        KR = [P.sb([128, T], BF16, "KR") for _ in range(2)]
        thp = P.sb([128, 2, 2], F32, "thp")
        thh = P.sb([128, 2, 4], F32, "thh")
        mk.dma("sp", thp[:], b_thp[l], W=["thp"])
        mk.dma("sp", thh[:], b_thh[l], W=["thh"])
        for tt, key in ((thp, "thp"), (thh, "thh")):
            mk.op("act", "activation", out=tt[:], in_=tt[:], func=AF.Exp, scale=-1.0, R=[key], W=[key])
            mk.op("act", "activation", out=tt[:], in_=tt[:], func=AF.Ln, bias=1.0, R=[key], W=[key])
            mk.op("dve", "tensor_scalar", out=tt[:], in0=tt[:], scalar1=-1.0, scalar2=None, op0=ALU.mult, R=[key], W=[key])
        DM = P.sb([128, 2, 512], F32, "DM")
        QW = P.sb([128, 2, 2, 128], F32, "QW")
        KW = P.sb([128, 2, 4], F32, "KW")
        Gc = P.sb([128, 2, 2, 1], F32, "Gc")
        for dr in range(2):
            diff, msk, pos = (C("diffF"), C("maskF"), C("posF")) if dr == 0 else (C("diffB"), C("maskB"), C("posB"))
            for h in range(4):
                mk.op("act", "activation", out=DM[:, dr, h * 128:(h + 1) * 128], in_=diff, func=AF.Exp,
                      scale=thh[:, dr, h:h + 1], R=["cst", "thh"], W=["DM"])
                mk.op("dve", "tensor_tensor", out=DM[:, dr, h * 128:(h + 1) * 128], in0=DM[:, dr, h * 128:(h + 1) * 128],
                      in1=msk, op=ALU.mult, R=["DM", "cst"], W=["DM"])
                mk.op("act", "activation", out=KW[:, dr, h:h + 1], in_=C("kpos")[:, dr:dr + 1], func=AF.Exp,
                      scale=thh[:, dr, h:h + 1], R=["cst", "thh"], W=["KW"])
            for c in range(2):
                mk.op("act", "activation", out=QW[:, dr, c, :], in_=pos, func=AF.Exp, scale=thp[:, dr, c:c + 1],
                      R=["cst", "thp"], W=["QW"])
                mk.op("act", "activation", out=Gc[:, dr, c, :], in_=thp[:, dr, c:c + 1], func=AF.Exp, scale=128.0,
                      R=["thp"], W=["Gc"])
        segw = 1088
        f1 = [P.sb([128, segw], F32, "f1") for _ in range(2)]
        f2 = [P.sb([128, segw], F32, "f2") for _ in range(2)]
        rc = P.sb([128, T], F32, "rc")
        rsn = P.sb([128, T], F32, "rsn")
        mk.dma("sp", rc[:], ropeC_d, W=["rc"])
        mk.dma("sp", rsn[:], ropeS_d, W=["rsn"])
        n = 0
        for (dst, base, pbase, scale) in ((QR, "b_q", "b_qp", 1.0), (KR, "b_k", "b_kp", 0.125)):
            for c in range(2):
                for sg in range(4):
                    i = n % 2
                    n += 1
                    cs = slice(sg * segw, (sg + 1) * segw)
                    mk.dma("sp", f1[i][:], FM[FM_OFF[base] + c][:, cs], R=[f"FM{FM_OFF[base] + c}"], W=[f"f1{i}"])
                    mk.dma("sp", f2[i][:], FM[FM_OFF[pbase] + c][:, cs], R=[f"FM{FM_OFF[pbase] + c}"], W=[f"f2{i}"])
                    mk.op("dve", "tensor_tensor", out=f1[i][:], in0=f1[i][:], in1=rc[:, cs], op=ALU.mult,
                          R=[f"f1{i}", "rc"], W=[f"f1{i}"])
                    mk.op("pool", "tensor_tensor", out=f2[i][:], in0=f2[i][:], in1=rsn[:, cs], op=ALU.mult,
                          R=[f"f2{i}", "rsn"], W=[f"f2{i}"])
                    mk.op("dve", "tensor_tensor", out=f1[i][:], in0=f1[i][:], in1=f2[i][:], op=ALU.add,
                          R=[f"f1{i}", f"f2{i}"], W=[f"f1{i}"])
                    mk.op("act", "activation", out=dst[c][:, cs], in_=f1[i][:], func=AF.Copy, scale=scale,
                          R=[f"f1{i}"], W=[f"b{base}{c}"])
        rkeys = ["bb_q0", "bb_q1", "bb_k0", "bb_k1"]
        OF = P.sb([128, NT, 256], F32, "OF")
        yTb = P.sb([128, 2, T], BF16, "yTb")
        fin = make_finalize(P, 1, yTb)
        PT = [P.sb([128, 512], BF16, "PT") for _ in range(2)]
        QIT = [[P.sb([128, 128], BF16, "QIT") for _ in range(2)] for _ in range(2)]
        KH = [P.sb([128, 256], BF16, "KH") for _ in range(2)]
        Vf = [P.sb([128, 512], F32, "Vf") for _ in range(2)]
        Vb = [P.sb([128, 4, 64], BF16, "Vb") for _ in range(2)]
        gt = [P.sb([128, 256], F32, "gt") for _ in range(2)]
        tot = [P.sb([128, 256], F32, "tot") for _ in range(2)]
        S = [P.sb([128, 64], F32, "S") for _ in range(2)]
        Sbf = [P.sb([128, 64], BF16, "Sbf") for _ in range(2)]
        it = 0
        for dr in range(2):
            for c in range(2):
                mk.op("pool", "memset", S[c][:], 0.0, W=[f"bS{c}"])
                mk.op("pool", "memset", Sbf[c][:], 0.0, W=[f"bSbf{c}"])
            for t, full in plan(dr, last):
                i = it % 2
                cols = slice(t * 128, (t + 1) * 128)
                mk.dma("sp", Vf[i][:], TM[t * 128:(t + 1) * 128, 0:512], R=["TM"], W=[f"bVf{i}"])
                mk.op("pool", "tensor_copy", out=Vb[i][:], in_=Vf[i][:, 0:256].rearrange("p (h e) -> p h e", h=4),
                      R=[f"bVf{i}"], W=[f"bVb{i}"])
                for c in range(2):
                    if full:
                        mk.op("pool", "tensor_tensor", out=QIT[i][c][:], in0=QR[c][:, cols], in1=QW[:, dr, c, :],
                              op=ALU.mult, R=[f"bb_q{c}", "QW"], W=[f"bQIT{i}"])
                    mk.op("pe", "transpose", out=PQ[0][:, (i * 2 + c) * 128:(i * 2 + c + 1) * 128], in_=KR[c][:, cols],
                          identity=identb[:], R=[f"bb_k{c}", "identb"], W=[f"pq0k{i}"])
                for h in range(4):
                    mk.op("act", "activation", out=KH[i][:, h * 64:(h + 1) * 64],
                          in_=PQ[0][:, i * 256 + h * 64:i * 256 + (h + 1) * 64], func=AF.Identity, scale=KW[:, dr, h:h + 1],
                          R=[f"pq0k{i}", "KW"], W=[f"bKH{i}"])
                okeys, Oh = chunk_core(it, 1, dr, [QR[0][:, cols], QR[1][:, cols]], [KR[0][:, cols], KR[1][:, cols]],
                                       [QIT[i][0], QIT[i][1]], (lambda s_, kh=KH[i]: kh), Vb[i], 64, Gc[:, dr], S, Sbf,
                                       DM[:, dr, :], "DM", rkeys + [f"bQIT{i}", f"bKH{i}", f"bVb{i}"], PT, "b", full=full)
                if not full:
                    pass
                elif dr == 0:
                    for hh in range(2):
                        mk.op("act", "activation", out=hview(OF[:, t, :], hh), in_=Oh[hh], func=AF.Copy, R=[okeys[hh]],
                              W=[f"bOF{t}h{hh}"])
                else:
                    for hh in range(2):
                        mk.op("dve", "tensor_tensor", out=hview(tot[i][:], hh), in0=Oh[hh], in1=hview(OF[:, t, :], hh),
                              op=ALU.add, R=[okeys[hh], f"bOF{t}h{hh}"], W=[f"btot{i}h{hh}"])
                    mk.op("act", "activation", out=gt[i][:], in_=Vf[i][:, 256:512], func=AF.Silu, R=[f"bVf{i}"],
                          W=[f"bgt{i}"])
                    fin(tot[i][:], [f"btot{i}h0", f"btot{i}h1"], True, gt[i][:], f"bgt{i}", t)
                it += 1
        for c in range(2):
            mk.dma("pool", YT[1, c], yTb[:, c, :], R=["yTb"], W=[f"YT1{c}"])
        mk.barrier()
        P.close()

    def mixer_c(l, last=False):
        P = Pool(nc, f"mc{l}")
        QC = [P.sb([128, T], BF16, "QC") for _ in range(2)]
        KC = [P.sb([128, T], BF16, "KC") for _ in range(2)]
        cw = P.sb([128, 4, 5], F32, "cw")
        cb = P.sb([128, 4], F32, "cb")
        gbias = P.sb([128, 16], F32, "gbias")
        mk.dma("sp", cw[:], c_cw[l], W=["c_cw"])
        mk.dma("sp", cb[:], c_cb[l], W=["c_cb"])
        mk.dma("sp", gbias[:], c_gb[l], W=["c_gb"])
        src = P.sb([128, T], F32, "src")
        u = P.sb([128, T], F32, "u")
        for ch in range(4):
            fc = FM_OFF["c_q"] + ch
            mk.dma("sp", src[:], FM[fc], R=[f"FM{fc}"], W=["csrc"])
            conv_fm(u, src, cw[:, ch, :], cb[:, ch:ch + 1], "cu", "csrc", ["c_cw", "c_cb"])
            dst = QC[ch] if ch < 2 else KC[ch - 2]
            mk.op("act", "activation", out=u[:], in_=u[:], func=AF.Silu, R=["cu"], W=["cu"])
            mk.op("dve", "tensor_scalar", out=dst[:], in0=u[:], scalar1=(1.0 if ch < 2 else 0.125), scalar2=None,
                  op0=ALU.mult, R=["cu"], W=[f"cqk{ch}"])
        Z = P.sb([128, NT, 16], F32, "Z")
        LFN = P.sb([128, NT, 16], F32, "LFN")
        mk.dma("sp", Z[:], TM[:, TM_OFF["c_gates"]:TM_OFF["c_gates"] + 16].rearrange("(t p) g -> p t g", p=128),
               R=["TM"], W=["cZ"])
        mk.op("dve", "tensor_tensor", out=Z[:], in0=Z[:], in1=gbias[:].unsqueeze(1).to_broadcast([128, NT, 16]),
              op=ALU.add, R=["cZ", "c_gb"], W=["cZ"])
        mk.op("act", "activation", out=LFN[:], in_=Z[:], func=AF.Exp, scale=-1.0, R=["cZ"], W=["cLFN"])
        mk.op("act", "activation", out=LFN[:], in_=LFN[:], func=AF.Ln, bias=1.0, R=["cLFN"], W=["cLFN"])
        mk.op("dve", "tensor_scalar", out=LFN[:], in0=LFN[:], scalar1=-1.0, scalar2=None, op0=ALU.mult, R=["cLFN"],
              W=["cLFN"])
        rkeys = ["cqk0", "cqk1", "cqk2", "cqk3"]
        OF = P.sb([128, NT, 256], F32, "OF")
        yTb = P.sb([128, 2, T], BF16, "yTb")
        fin = make_finalize(P, 2, yTb)
        PT = [P.sb([128, 512], BF16, "PT") for _ in range(2)]
        QIT = [[P.sb([128, 128], BF16, "QIT") for _ in range(2)] for _ in range(2)]
        KH = [P.sb([128, 256], BF16, "KH") for _ in range(2)]
        Vf = [P.sb([128, 512], F32, "Vf") for _ in range(2)]
        Vb = [P.sb([128, 4, 65], BF16, "Vb") for _ in range(2)]
        gt = [P.sb([128, 256], F32, "gt") for _ in range(2)]
        tot = [P.sb([128, 256], F32, "tot") for _ in range(2)]
        S = [P.sb([128, 65], F32, "S") for _ in range(2)]
        Sbf = [P.sb([128, 65], BF16, "Sbf") for _ in range(2)]
        Bm4 = [P.sb([128, 4, 128], F32, "Bm4") for _ in range(2)]
        tmp4 = [P.sb([128, 512], F32, "tmp4") for _ in range(2)]
        Dm4 = [P.sb([128, 512], F32, "Dm4") for _ in range(2)]
        EB4 = [P.sb([128, 512], F32, "EB4") for _ in range(2)]
        lmb = [P.sb([128, 4], F32, "lmb") for _ in range(2)]
        kw = [P.sb([128, 4], F32, "kw") for _ in range(2)]
        Gc = [P.sb([128, 2, 1], F32, "Gc") for _ in range(2)]
        rden = [P.sb([128, 4], F32, "rden") for _ in range(2)]
        ebe = [P.sb([128, 4], F32, "ebe") for _ in range(2)]
        hid = [P.sb([128, 4, 64], F32, "hid") for _ in range(2)]
        for i in range(2):
            mk.op("pool", "memset", Vb[i][:], 1.0, W=[f"cVb{i}"])
        ones = C("ones")
        it = 0
        for dr in range(2):
            tri = C("triF") if dr == 0 else C("triB")
            neg4 = C("negF4") if dr == 0 else C("negB4")
            e = 127 if dr == 0 else 0
            for c in range(2):
                mk.op("pool", "memset", S[c][:], 0.0, W=[f"cS{c}"])
                mk.op("pool", "memset", Sbf[c][:], 0.0, W=[f"cSbf{c}"])
            for t, full in plan(dr, last):
                i = it % 2
                cols = slice(t * 128, (t + 1) * 128)
                li = Z[:, t, dr * 8:dr * 8 + 4]
                lf = LFN[:, t, dr * 8 + 4:dr * 8 + 8]
                mk.dma("sp", Vf[i][:], TM[t * 128:(t + 1) * 128, 512:1024], R=["TM"], W=[f"cVf{i}"])
                mk.op("pool", "tensor_copy", out=Vb[i][:, :, 0:64], in_=Vf[i][:, 0:256].rearrange("p (h e) -> p h e", h=4),
                      R=[f"cVf{i}"], W=[f"cVb{i}"])
                mk.op("dve", "tensor_tensor", out=Bm4[i][:], in0=tri.unsqueeze(1).to_broadcast([128, 4, 128]),
                      in1=lf.unsqueeze(2).to_broadcast([128, 4, 128]), op=ALU.mult, R=["cst", "cLFN"], W=[f"cBm{i}"])
                mk.op("pe", "matmul", PS[5][:, :], lhsT=ones, rhs=Bm4[i][:].rearrange("p h n -> p (h n)"), start=True,
                      stop=True, R=["cst", f"cBm{i}"], W=["ps5"])
                mk.op("pe", "matmul", PS[4][:, 256:260], lhsT=tri, rhs=lf, start=True, stop=True, R=["cst", "cLFN"],
                      W=["ps4b"])
                mk.op("dve", "tensor_tensor", out=lmb[i][:], in0=li, in1=PS[4][:, 256:260], op=ALU.subtract,
                      R=["cZ", "ps4b"], W=[f"clmb{i}"])
                if full:
                    mk.op("dve", "tensor_tensor", out=tmp4[i][:], in0=PS[5][:, :], in1=neg4, op=ALU.add, R=["ps5", "cst"],
                          W=[f"ctmp{i}"])
                    for h in range(4):
                        mk.op("act", "activation", out=Dm4[i][:, h * 128:(h + 1) * 128],
                              in_=tmp4[i][:, h * 128:(h + 1) * 128], func=AF.Exp, bias=lmb[i][:, h:h + 1],
                              R=[f"ctmp{i}", f"clmb{i}"], W=[f"cDm{i}"])
                    mk.op("act", "activation", out=EB4[i][:], in_=PS[5][:, :], func=AF.Exp, R=["ps5"], W=[f"cEB{i}"])
                bend = PS[5][:, :].rearrange("p (h n) -> p h n", h=4)[:, :, e]
                mk.op("dve", "tensor_tensor", out=kw[i][:], in0=lmb[i][:], in1=bend, op=ALU.add, R=[f"clmb{i}", "ps5"],
                      W=[f"ckw{i}"])
                mk.op("act", "activation", out=kw[i][:], in_=kw[i][:], func=AF.Exp, R=[f"ckw{i}"], W=[f"ckw{i}"])
                mk.op("act", "activation", out=ebe[i][:], in_=bend, func=AF.Exp, R=["ps5"], W=[f"cebe{i}"])
                for h in range(4):
                    c, hh = h // 2, h % 2
                    rs = slice(hh * 64, (hh + 1) * 64)
                    mk.op("pool", "tensor_copy", out=Gc[i][rs, c, :], in_=ebe[i][rs, h:h + 1],
                          R=[f"cebe{i}"], W=[f"cGc{i}"])
                    if full:
                        mk.op("pool", "tensor_tensor", out=QIT[i][c][rs, :], in0=QC[c][rs, cols],
                              in1=EB4[i][rs, h * 128:(h + 1) * 128], op=ALU.mult, R=[f"cqk{c}", f"cEB{i}"],
                              W=[f"cQIT{i}"])
                for c in range(2):
                    mk.op("pe", "transpose", out=PQ[0][:, (i * 2 + c) * 128:(i * 2 + c + 1) * 128], in_=KC[c][:, cols],
                          identity=identb[:], R=[f"cqk{2 + c}", "identb"], W=[f"pq0k{i}"])
                for h in range(4):
                    mk.op("act", "activation", out=KH[i][:, h * 64:(h + 1) * 64],
                          in_=PQ[0][:, i * 256 + h * 64:i * 256 + (h + 1) * 64], func=AF.Identity, scale=kw[i][:, h:h + 1],
                          R=[f"pq0k{i}", f"ckw{i}"], W=[f"cKH{i}"])
                okeys, Oh = chunk_core(it, 1, dr, [QC[0][:, cols], QC[1][:, cols]], [KC[0][:, cols], KC[1][:, cols]],
                                       [QIT[i][0], QIT[i][1]], (lambda s_, kh=KH[i]: kh), Vb[i], 65, Gc[i], S, Sbf,
                                       Dm4[i][:], f"cDm{i}",
                                       rkeys + [f"cQIT{i}", f"cKH{i}", f"cVb{i}", f"cGc{i}"], PT, "c", full=full)
                if not full:
                    it += 1
                    continue
                rdv = rden[i][:].rearrange("p (c x) -> p c x", c=2)
                for hh in range(2):
                    mk.op("act", "activation", out=rdv[:, :, hh], in_=Oh[hh][:, :, 64], func=AF.Abs, R=[okeys[hh]],
                          W=[f"crden{i}"])
                mk.op("dve", "tensor_scalar_max", out=rden[i][:], in0=rden[i][:], scalar1=1.0, R=[f"crden{i}"],
                      W=[f"crden{i}"])
                mk.op("dve", "reciprocal", out=rden[i][:], in_=rden[i][:], R=[f"crden{i}"], W=[f"crden{i}"])
                if dr == 0:
                    for hh in range(2):
                        mk.op("dve", "tensor_tensor", out=hview(OF[:, t, :], hh), in0=Oh[hh][:, :, 0:64],
                              in1=rdv[:, :, hh:hh + 1].to_broadcast([128, 2, 64]), op=ALU.mult,
                              R=[okeys[hh], f"crden{i}"], W=[f"cOF{t}h{hh}"])
                else:
                    for hh in range(2):
                        mk.op("dve", "tensor_tensor", out=hview(hid[i][:].rearrange("p h e -> p (h e)"), hh),
                              in0=Oh[hh][:, :, 0:64], in1=rdv[:, :, hh:hh + 1].to_broadcast([128, 2, 64]), op=ALU.mult,
                              R=[okeys[hh], f"crden{i}"], W=[f"chid{i}h{hh}"])
                    mk.op("pool", "tensor_tensor", out=tot[i][:], in0=hid[i][:].rearrange("p h e -> p (h e)"),
                          in1=OF[:, t, :], op=ALU.add, R=[f"chid{i}h0", f"chid{i}h1", f"cOF{t}h0", f"cOF{t}h1"],
                          W=[f"ctot{i}"])
                    mk.op("act", "activation", out=gt[i][:], in_=Vf[i][:, 256:512], func=AF.Sigmoid, R=[f"cVf{i}"],
                          W=[f"cgt{i}"])
                    fin(tot[i][:], f"ctot{i}", True, gt[i][:], f"cgt{i}", t)
                it += 1
        for c in range(2):
            mk.dma("pool", YT[2, c], yTb[:, c, :], R=["yTb"], W=[f"YT2{c}"])
        mk.barrier()
        P.close()

    def mixer_d(l, last=False):
        P = Pool(nc, f"md{l}")
        LB = P.sb([128, 256], F32, "LB")
        OML = P.sb([128, 256], F32, "OML")
        if l == 0:
            use_lb = False
        else:
            use_lb = True
            dl = P.sb([128, 2, 256], F32, "dl")
            mk.dma("sp", dl[:], d_lbr, W=["dl"])
            mk.op("dve", "tensor_tensor", out=LB[:], in0=dl[:, 1, :], in1=dl[:, 0, :], op=ALU.subtract, R=["dl"], W=["LB"])
            mk.op("act", "activation", out=LB[:], in_=LB[:], func=AF.Sigmoid, R=["LB"], W=["LB"])
            mk.op("dve", "tensor_scalar", out=OML[:], in0=LB[:], scalar1=-1.0, scalar2=1.0, op0=ALU.mult, op1=ALU.add,
                  R=["LB"], W=["OML"])
        OF = P.sb([128, NT, 256], F32, "OF")
        yTb = P.sb([128, 2, T], BF16, "yTb")
        fin = make_finalize(P, 3, yTb)
        assert DNS == 4
        PT = [P.sb([128, 512], BF16, "PT") for _ in range(2)]
        X = [P.sb([128, 1280], F32, "X") for _ in range(2)]
        ff = [P.sb([128, 256], F32, "ff") for _ in range(2)]
        lf = [P.sb([128, 256], F32, "lf") for _ in range(2)]
        kk = [P.sb([128, 256], F32, "kk") for _ in range(2)]
        qs = [P.sb([128, 256], F32, "qs") for _ in range(2)]
        ee = [P.sb([128, 512], F32, "ee") for _ in range(2)]
        ek = [P.sb([128, 256], F32, "ek") for _ in range(2)]
        qk = [P.sb([128, 512], BF16, "qk") for _ in range(2)]
        KTs = [P.sb([128, 2, 128], BF16, "KTs") for _ in range(2)]
        QM = [[P.sb([128, 2, 5, 128], BF16, "QM") for _ in range(2)] for _ in range(2)]
        KH = [P.sb([128, DNS, 256], BF16, "KH") for _ in range(2)]
        Vb = [P.sb([128, 4, 64], BF16, "Vb") for _ in range(2)]
        gt = [P.sb([128, 256], F32, "gt") for _ in range(2)]
        tot = [P.sb([128, 256], F32, "tot") for _ in range(2)]
        red = [P.sb([128, 2, 256], F32, "red") for _ in range(2)]
        Gc = [P.sb([128, 2, DNS], F32, "Gc") for _ in range(2)]
        S = [P.sb([128, 64], F32, "S") for _ in range(2)]
        Sbf = [P.sb([128, 64], BF16, "Sbf") for _ in range(2)]
        qmask = C("qmask").rearrange("p (x s n) -> p x s n", x=2, s=5)
        it = 0
        for dr in range(2):
            blk = C("blkF") if dr == 0 else C("blkB")
            rem = C("aftF") if dr == 0 else C("befB")
            msk4 = C("mblkF4") if dr == 0 else C("mblkB4")
            zoff = 256 if dr == 0 else 512
            subs = list(range(DNS)) if dr == 0 else list(range(DNS - 1, -1, -1))
            for c in range(2):
                mk.op("pool", "memset", S[c][:], 0.0, W=[f"dS{c}"])
                mk.op("pool", "memset", Sbf[c][:], 0.0, W=[f"dSbf{c}"])
            for t, full in plan(dr, last):
                i = it % 2
                mk.dma("sp", X[i][:], TM[t * 128:(t + 1) * 128, 1024:2304], R=["TM"], W=[f"dX{i}"])
                mk.op("act", "activation", out=ff[i][:], in_=X[i][:, zoff:zoff + 256], func=AF.Sigmoid, R=[f"dX{i}"],
                      W=[f"dff{i}"])
                if use_lb:
                    mk.op("dve", "tensor_tensor", out=ff[i][:], in0=ff[i][:], in1=OML[:], op=ALU.mult, R=[f"dff{i}", "OML"],
                          W=[f"dff{i}"])
                    mk.op("dve", "tensor_tensor", out=ff[i][:], in0=ff[i][:], in1=LB[:], op=ALU.add, R=[f"dff{i}", "LB"],
                          W=[f"dff{i}"])
                mk.op("act", "activation", out=lf[i][:], in_=ff[i][:], func=AF.Ln, R=[f"dff{i}"], W=[f"dlf{i}"])
                mk.op("pool", "tensor_scalar", out=kk[i][:], in0=ff[i][:], scalar1=-1.0, scalar2=1.0, op0=ALU.mult,
                      op1=ALU.add, R=[f"dff{i}"], W=[f"dkk{i}"])
                if full:
                    mk.op("pe", "matmul", PS[5][:, 0:256], lhsT=blk, rhs=lf[i][:], start=True, stop=True,
                          R=["cst", f"dlf{i}"], W=["ps5"])
                mk.op("pe", "matmul", PS[5][:, 256:512], lhsT=rem, rhs=lf[i][:], start=True, stop=True,
                      R=["cst", f"dlf{i}"], W=["ps5"])
                for c in range(2):
                    mk.op("pe", "matmul", PS[1][:, 384 + c * DNS:384 + (c + 1) * DNS], lhsT=lf[i][:, c * 128:(c + 1) * 128],
                          rhs=C("subm"), start=True, stop=True, R=["cst", f"dlf{i}"], W=["ps1g"])
                mk.op("act", "activation", out=Gc[i][:].rearrange("p c s -> p (c s)"), in_=PS[1][:, 384:384 + 2 * DNS],
                      func=AF.Exp, R=["ps1g"], W=[f"dGc{i}"])
                if full:
                    mk.op("act", "activation", out=ee[i][:], in_=PS[5][:, :], func=AF.Exp, R=["ps5"], W=[f"dee{i}"])
                    mk.op("act", "activation", out=ek[i][:], in_=PS[5][:, 0:256], func=AF.Exp, scale=-1.0, R=["ps5"],
                          W=[f"dek{i}"])
                    mk.op("act", "activation", out=qs[i][:], in_=X[i][:, 0:256], func=AF.Silu, R=[f"dX{i}"],
                          W=[f"dqs{i}"])
                    mk.op("dve", "tensor_tensor", out=qk[i][:, 0:256], in0=qs[i][:], in1=ee[i][:, 0:256], op=ALU.mult,
                          R=[f"dqs{i}", f"dee{i}"], W=[f"dqk{i}a"])
                    mk.op("dve", "tensor_tensor", out=qk[i][:, 256:512], in0=kk[i][:], in1=ek[i][:], op=ALU.mult,
                          R=[f"dkk{i}", f"dek{i}"], W=[f"dqk{i}c"])
                else:
                    mk.op("act", "activation", out=ee[i][:, 256:512], in_=PS[5][:, 256:512], func=AF.Exp, R=["ps5"],
                          W=[f"dee{i}"])
                for s_ in range(DNS):
                    mk.op("dve", "scalar_tensor_tensor", out=KH[i][:, s_, :], in0=kk[i][:], scalar=C("subm")[:, s_:s_ + 1],
                          in1=ee[i][:, 256:512], op0=ALU.mult, op1=ALU.mult, R=[f"dkk{i}", f"dee{i}", "cst"],
                          W=[f"dKH{i}s{s_}"])
                mk.op("pool", "tensor_copy", out=Vb[i][:], in_=X[i][:, 768:1024].rearrange("p (h e) -> p h e", h=4),
                      R=[f"dX{i}"], W=[f"dVb{i}"])
                if full:
                    for j in range(4):
                        mk.op("pe", "transpose", out=PQ[0][:, (i * 4 + j) * 128:(i * 4 + j + 1) * 128],
                              in_=qk[i][:, j * 128:(j + 1) * 128], identity=identb[:], R=[f"dqk{i}a", f"dqk{i}c", "identb"],
                              W=[f"pq0d{i}"])
                    mk.op("act", "activation", out=KTs[i][:].rearrange("p c n -> p (c n)"),
                          in_=PQ[0][:, i * 512 + 256:i * 512 + 512], func=AF.Copy, R=[f"pq0d{i}"], W=[f"dKT{i}"])
                    for c in range(2):
                        mk.op("dve", "tensor_tensor", out=QM[i][c][:].rearrange("p x s n -> p (x s) n"),
                              in0=PQ[0][:, i * 512 + c * 128:i * 512 + (c + 1) * 128].unsqueeze(1).to_broadcast([128, 10, 128]),
                              in1=qmask.rearrange("p x s n -> p (x s) n"), op=ALU.mult, R=[f"pq0d{i}", "cst"],
                              W=[f"dQM{i}{c}"])
                    for h in range(4):
                        c, hh = h // 2, h % 2
                        mk.op("pe", "matmul", PS[0][:, h * 128:(h + 1) * 128], lhsT=KTs[i][:, c, :], rhs=QM[i][c][:, hh, 4, :],
                              start=True, stop=True, R=[f"dKT{i}", f"dQM{i}{c}"], W=["ps0"])
                    mk.op("dve", "tensor_tensor", out=PT[i][:], in0=PS[0][:, :], in1=msk4, op=ALU.mult, R=["ps0", "cst"],
                          W=[f"dPT{i}"])
                    for h in range(4):
                        mk.op("pe", "matmul", PS[1][:, h * 64:(h + 1) * 64], lhsT=PT[i][:, h * 128:(h + 1) * 128],
                              rhs=Vb[i][:, h, :], start=True, stop=True, R=[f"dPT{i}", f"dVb{i}"], W=["ps1i"])
                KVp = PS[1][:, 256:384].rearrange("p (c e) -> p c e", c=2)
                for s_ in subs:
                    bank = PS[2 + s_ // 2]
                    for h in (range(4) if full else ()):
                        c, hh = h // 2, h % 2
                        col = ((s_ % 2) * 4 + h) * 64
                        mk.op("pe", "matmul", bank[:, col:col + 64], lhsT=QM[i][c][:, hh, s_, :], rhs=Sbf[c][:, :],
                              start=True, stop=True, R=[f"dQM{i}{c}", f"dSbf{c}"], W=[f"ps{2 + s_ // 2}"])
                    for h in range(4):
                        c, hh = h // 2, h % 2
                        mk.op("pe", "matmul", KVp[hh * 64:(hh + 1) * 64, c, :], lhsT=KH[i][:, s_, h * 64:(h + 1) * 64],
                              rhs=Vb[i][:, h, :], start=True, stop=True, R=[f"dKH{i}s{s_}", f"dVb{i}"], W=["ps1kv"])
                    for c in range(2):
                        mk.op("dve", "scalar_tensor_tensor", out=S[c][:], in0=S[c][:], scalar=Gc[i][:, c, s_:s_ + 1],
                              in1=KVp[:, c, :], op0=ALU.mult, op1=ALU.add, R=[f"dS{c}", "ps1kv", f"dGc{i}"], W=[f"dS{c}"])
                        mk.op("act", "activation", out=Sbf[c][:], in_=S[c][:], func=AF.Copy, R=[f"dS{c}"], W=[f"dSbf{c}"])
                if not full:
                    it += 1
                    continue
                for b_ in range(2):
                    mk.op("dve", "tensor_reduce", out=red[i][:, b_, :],
                          in_=PS[2 + b_][:, :].rearrange("p (s x) -> p x s", s=2), axis=AX.X, op=ALU.add,
                          R=[f"ps{2 + b_}"], W=[f"dred{i}{b_}"])
                mk.op("dve", "tensor_tensor", out=tot[i][:], in0=PS[1][:, 0:256], in1=red[i][:, 0, :], op=ALU.add,
                      R=["ps1i", f"dred{i}0"], W=[f"dtot{i}"])
                if dr == 0:
                    mk.op("pool", "tensor_tensor", out=OF[:, t, :], in0=tot[i][:], in1=red[i][:, 1, :], op=ALU.add,
                          R=[f"dtot{i}", f"dred{i}1"], W=[f"dOF{t}"])
                else:
                    mk.op("pool", "tensor_tensor", out=tot[i][:], in0=tot[i][:], in1=red[i][:, 1, :], op=ALU.add,
                          R=[f"dtot{i}", f"dred{i}1"], W=[f"dtot{i}"])
                    mk.op("dve", "tensor_tensor", out=tot[i][:], in0=tot[i][:], in1=OF[:, t, :], op=ALU.add,
                          R=[f"dtot{i}", f"dOF{t}"], W=[f"dtot{i}"])
                    mk.op("act", "activation", out=gt[i][:], in_=X[i][:, 1024:1280], func=AF.Silu, R=[f"dX{i}"],
                          W=[f"dgt{i}"])
                    fin(tot[i][:], f"dtot{i}", False, gt[i][:], f"dgt{i}", t)
                it += 1
        for c in range(2):
            mk.dma("pool", YT[3, c], yTb[:, c, :], R=["yTb"], W=[f"YT3{c}"])
        mk.barrier()
        P.close()

    W1B = dscr("W1B", [16, 128, 4096], BF16)
    W3B = dscr("W3B", [16, 128, 4096], BF16)
    W2B = dscr("W2B", [16, 128, 4096], BF16)

    def merge_moe_phase(l, last, tiles=None):
        if tiles is None:
            tiles = list(OUT_T) if last else list(range(NT))
        PO = Pool(nc, f"mo{l}")
        h2T = PO.sb([128, 8, T], BF16, "h2T")
        gates = PO.sb([128, NT, 16], F32, "gates")
        merge_part(l, tiles, h2T, gates)
        moe_part(l, tiles, h2T, gates)
        PO.close()

    def merge_part(l, tiles, h2T, gates):
        P = Pool(nc, f"mm{l}")
        wbr = P.sb([128, 8, D], BF16, "wbr")
        wo = P.sb([128, 8, D], BF16, "wo")
        wstage = P.sb([128, 8, 512], F32, "wstage")
        wcb = P.sb([128, 4096], BF16, "wcb")
        for half in range(2):
            mk.dma("sp", wstage[:], w_branch[l].rearrange("n (c p) f -> p (n c) f", p=128)[:, :, half * 512:(half + 1) * 512],
                   W=["wstage"])
            mk.ev(wbr[:, :, half * 512:(half + 1) * 512], wstage[:], R=["wstage"], W=["wbr"])
        for half in range(2):
            mk.dma("sp", wstage[:], w_out[l].rearrange("(c p) f -> p c f", p=128)[:, :, half * 512:(half + 1) * 512],
                   W=["wstage"])
            mk.ev(wo[:, :, half * 512:(half + 1) * 512], wstage[:], R=["wstage"], W=["wo"])
        tasks = []
        for e in range(16):
            tasks.append((moe_w1[l, e].rearrange("(c p) f -> p c f", p=128), wstage[:], W1B[e], f"W1B{e}"))
            tasks.append((moe_w3[l, e].rearrange("(c p) f -> p c f", p=128), wstage[:], W3B[e], f"W3B{e}"))
            tasks.append((moe_w2[l, e].rearrange("(c p) f -> p c f", p=128),
                          wstage[:].rearrange("p c f -> p (c f)").rearrange("p (c f) -> p c f", c=4), W2B[e], f"W2B{e}"))

        def do_task(k):
            src, stg, dst, key = tasks[k]
            mk.dma("sp", stg, src, W=["wstage"])
            mk.ev(wcb[:], wstage[:].rearrange("p c f -> p (c f)"), R=["wstage"], W=["wcb"])
            mk.dma("pool", dst, wcb[:], R=["wcb"], W=[key])
        wgr = P.sb([128, 8, 20], F32, "wgr")
        bgr = P.sb([1, 20], F32, "bgr")
        mk.dma("sp", wgr[:], moe_wgr[l].rearrange("(c p) f -> p c f", p=128), W=["wgr"])
        mk.dma("sp", bgr[:], moe_bgr[l], W=["bgr"])
        identf = C("ident")
        ones = C("ones")
        norm = make_norm(P, 1)
        xnew = [P.sb([128, D], F32, "xnew") for _ in range(2)]
        yt = [P.sb([128, 8, 128], BF16, "yt")] * 2
        mg = [P.sb([128, 4096], BF16, "mg")] * 2
        xt = [P.sb([128, D], F32, "xt")] * 2
        zz = P.sb([128, D], F32, "zz")
        zt = P.sb([128, D], F32, "zt")
        zb = P.sb([128, D], BF16, "zb")
        zT = P.sb([128, 8, 128], BF16, "zT")
        h2f = zz
        h2fT = zt[:, :].rearrange("p (c n) -> p c n", c=8)
        rt = {k: P.sb([128, w], F32, "rt" + k) for k, w in
              (("L", 20), ("gm", 1), ("goh", 4), ("ge", 4), ("gs", 1), ("el", 4), ("m1", 1), ("oh1", 4), ("e2", 4),
               ("m2", 1), ("oh2", 4), ("w1", 1), ("w2", 1), ("gw", 4))}
        nit = 0
        ntask = 0
        per_tile = -(-len(tasks) // len(tiles))
        for t in tiles:
            for _ in range(per_tile):
                if ntask < len(tasks):
                    do_task(ntask)
                    ntask += 1
            i = nit % 2
            nit += 1
            j = cond_of(t)
            cols = slice(t * 128, (t + 1) * 128)
            mk.dma("sp", yt[i][:], YT[:, :, :, cols].rearrange("n c p t -> p (n c) t"),
                   R=[f"YT{n}{c}" for n in range(4) for c in range(2)], W=["yt"])
            mk.dma("sp", mg[i][:], MG[cols, :], R=["MG"], W=["mg"])
            mk.dma("sp", xt[i][:], XR[cols, :], R=[f"XR{t}"], W=["mxt"])
            for n in range(4):
                for cb in range(2):
                    pb = (n * 2 + cb) % 2
                    for c in range(2):
                        mk.op("pe", "matmul", PS[pb][:, :], lhsT=yt[i][:, n * 2 + c, :],
                              rhs=wbr[:, n * 2 + c, cb * 512:(cb + 1) * 512], start=(c == 0), stop=(c == 1),
                              R=["yt", "wbr"], W=[f"ps{pb}"])
                    dst = zz if n == 0 else zt
                    dkey = "zz" if n == 0 else "zt"
                    mk.op("dve", "tensor_tensor", out=dst[:, cb * 512:(cb + 1) * 512], in0=PS[pb][:, :],
                          in1=mg[i][:, n * 1024 + cb * 512:n * 1024 + (cb + 1) * 512], op=ALU.mult,
                          R=[f"ps{pb}", "mg"], W=[dkey])
                    if n > 0:
                        mk.op("pool", "tensor_tensor", out=zz[:, cb * 512:(cb + 1) * 512],
                              in0=zz[:, cb * 512:(cb + 1) * 512], in1=zt[:, cb * 512:(cb + 1) * 512], op=ALU.add,
                              R=["zz", "zt"], W=["zz"])
            mk.op("act", "activation", out=zb[:], in_=zz[:], func=AF.Copy, R=["zz"], W=["zb"])
            for c in range(8):
                mk.op("pe", "transpose", out=PQ[1][:, c * 128:(c + 1) * 128], in_=zb[:, c * 128:(c + 1) * 128],
                      identity=identb[:], R=["zb", "identb"], W=["pq1"])
            mk.ev(zT[:].rearrange("p c n -> p (c n)"), PQ[1][:, :], R=["pq1"], W=["zT"])
            for cb in range(2):
                pb = 2 + cb
                for c in range(8):
                    mk.op("pe", "matmul", PS[pb][:, :], lhsT=zT[:, c, :], rhs=wo[:, c, cb * 512:(cb + 1) * 512],
                          start=(c == 0), stop=(c == 7), R=["zT", "wo"], W=[f"ps{pb}"])
                mk.op("dve", "tensor_tensor", out=zt[:, cb * 512:(cb + 1) * 512], in0=PS[pb][:, :],
                      in1=GB[:, j, 0, cb * 512:(cb + 1) * 512], op=ALU.mult, R=[f"ps{pb}", "GB"], W=["zt"])
                mk.op("pool", "tensor_tensor", out=xnew[i][:, cb * 512:(cb + 1) * 512], in0=zt[:, cb * 512:(cb + 1) * 512],
                      in1=xt[i][:, cb * 512:(cb + 1) * 512], op=ALU.add, R=["zt", "mxt"], W=[f"xnew{i}"])
            mk.dma("pool", XR[cols, :], xnew[i][:], R=[f"xnew{i}"], W=[f"XR{t}"])
            norm(xnew[i][:], f"xnew{i}", j, 3, 2, h2T[:, :, t * 128:(t + 1) * 128], f"h2T{t}")
            ssr = rt["gs"]
            mk.op("act", "activation", out=h2f[:], in_=xnew[i][:], func=AF.Square, accum_out=ssr[:],
                  R=[f"xnew{i}"], W=["zz", "rgs"])
            mk.op("act", "activation", out=ssr[:], in_=ssr[:], func=AF.Sqrt, scale=1.0 / D, bias=EPS, R=["rgs"], W=["rgs"])
            mk.op("dve", "reciprocal", out=ssr[:], in_=ssr[:], R=["rgs"], W=["rgs"])
            mk.op("dve", "tensor_scalar", out=h2f[:], in0=xnew[i][:], scalar1=ssr[:, 0:1], scalar2=None,
                  op0=ALU.mult, R=[f"xnew{i}", "rgs", "zz"], W=["zz"])
            for half in range(2):
                for c4 in range(4):
                    c = half * 4 + c4
                    mk.op("pe", "transpose", out=PS[5][:, c4 * 128:(c4 + 1) * 128], in_=h2f[:, c * 128:(c + 1) * 128],
                          identity=identf, R=["zz", "cst"], W=["ps5"])
                mk.op("dve", "tensor_tensor", out=h2fT[:, half * 4:(half + 1) * 4, :],
                      in0=PS[5][:, :].rearrange("p (c n) -> p c n", c=4),
                      in1=MODC[:, 3, half * 4:(half + 1) * 4, j:j + 1].to_broadcast([128, 4, 128]), op=ALU.mult,
                      R=["ps5", "MODC"], W=["zt"])
                mk.op("pool", "tensor_tensor", out=h2fT[:, half * 4:(half + 1) * 4, :],
                      in0=h2fT[:, half * 4:(half + 1) * 4, :],
                      in1=MODC[:, 2, half * 4:(half + 1) * 4, j:j + 1].to_broadcast([128, 4, 128]), op=ALU.add,
                      R=["zt", "MODC"], W=["zt"])
            for c in range(8):
                mk.op("pe", "matmul", PS[4][:, 0:20], lhsT=h2fT[:, c, :], rhs=wgr[:, c, :], start=(c == 0), stop=False,
                      R=["zt", "wgr"], W=["ps4r"])
            mk.op("pe", "matmul", PS[4][:, 0:20], lhsT=ones[0:1, :], rhs=bgr[0:1, :], start=False, stop=True,
                  R=["cst", "bgr"], W=["ps4r"])
            Lg = rt["L"]
            mk.op("act", "activation", out=Lg[:], in_=PS[4][:, 0:20], func=AF.Copy, R=["ps4r"], W=["rL"])
            rk = ["rL"]
            mk.op("dve", "tensor_reduce", out=rt["gm"][:], in_=Lg[:, 0:4], axis=AX.X, op=ALU.max, R=rk, W=["rgm"])
            mk.op("dve", "tensor_scalar", out=rt["goh"][:], in0=Lg[:, 0:4], scalar1=rt["gm"][:, 0:1], scalar2=None,
                  op0=ALU.is_ge, R=rk + ["rgm"], W=["rgoh"])
            mk.op("dve", "tensor_scalar", out=rt["ge"][:], in0=Lg[:, 0:4], scalar1=rt["gm"][:, 0:1], scalar2=None,
                  op0=ALU.subtract, R=rk + ["rgm"], W=["rge"])
            mk.op("act", "activation", out=rt["ge"][:], in_=rt["ge"][:], func=AF.Exp, accum_out=rt["gs"][:],
                  R=["rge"], W=["rge", "rgs"])
            mk.op("dve", "reciprocal", out=rt["gs"][:], in_=rt["gs"][:], R=["rgs"], W=["rgs"])
            mk.op("dve", "tensor_scalar", out=rt["el"][:], in0=Lg[:, 4:8], scalar1=rt["goh"][:, 0:1], scalar2=None,
                  op0=ALU.mult, R=rk + ["rgoh"], W=["rel"])
            for g in range(1, 4):
                mk.op("dve", "scalar_tensor_tensor", out=rt["el"][:], in0=Lg[:, 4 + g * 4:8 + g * 4],
                      scalar=rt["goh"][:, g:g + 1], in1=rt["el"][:], op0=ALU.mult, op1=ALU.add,
                      R=rk + ["rgoh", "rel"], W=["rel"])
            mk.op("dve", "tensor_reduce", out=rt["m1"][:], in_=rt["el"][:], axis=AX.X, op=ALU.max, R=["rel"], W=["rm1"])
            mk.op("dve", "tensor_scalar", out=rt["oh1"][:], in0=rt["el"][:], scalar1=rt["m1"][:, 0:1], scalar2=None,
                  op0=ALU.is_ge, R=["rel", "rm1"], W=["roh1"])
            mk.op("dve", "scalar_tensor_tensor", out=rt["e2"][:], in0=rt["oh1"][:], scalar=-1e30, in1=rt["el"][:],
                  op0=ALU.mult, op1=ALU.add, R=["roh1", "rel"], W=["re2"])
            mk.op("dve", "tensor_reduce", out=rt["m2"][:], in_=rt["e2"][:], axis=AX.X, op=ALU.max, R=["re2"], W=["rm2"])
            mk.op("dve", "tensor_scalar", out=rt["oh2"][:], in0=rt["e2"][:], scalar1=rt["m2"][:, 0:1], scalar2=None,
                  op0=ALU.is_ge, R=["re2", "rm2"], W=["roh2"])
            mk.op("dve", "tensor_tensor", out=rt["w1"][:], in0=rt["m2"][:], in1=rt["m1"][:], op=ALU.subtract,
                  R=["rm1", "rm2"], W=["rw1"])
            mk.op("act", "activation", out=rt["w1"][:], in_=rt["w1"][:], func=AF.Exp, R=["rw1"], W=["rw1"])
            mk.op("dve", "tensor_scalar_add", out=rt["w1"][:], in0=rt["w1"][:], scalar1=1.0, R=["rw1"], W=["rw1"])
            mk.op("dve", "reciprocal", out=rt["w1"][:], in_=rt["w1"][:], R=["rw1"], W=["rw1"])
            mk.op("dve", "tensor_tensor", out=rt["w1"][:], in0=rt["w1"][:], in1=rt["gs"][:], op=ALU.mult,
                  R=["rw1", "rgs"], W=["rw1"])
            mk.op("dve", "tensor_tensor", out=rt["w2"][:], in0=rt["gs"][:], in1=rt["w1"][:], op=ALU.subtract,
                  R=["rw1", "rgs"], W=["rw2"])
            mk.op("dve", "tensor_scalar", out=rt["gw"][:], in0=rt["oh1"][:], scalar1=rt["w1"][:, 0:1], scalar2=None,
                  op0=ALU.mult, R=["roh1", "rw1"], W=["rgw"])
            mk.op("dve", "scalar_tensor_tensor", out=rt["gw"][:], in0=rt["oh2"][:], scalar=rt["w2"][:, 0:1],
                  in1=rt["gw"][:], op0=ALU.mult, op1=ALU.add, R=["roh2", "rw2", "rgw"], W=["rgw"])
            for g in range(4):
                mk.op("dve", "tensor_scalar", out=gates[:, t, g * 4:(g + 1) * 4], in0=rt["gw"][:],
                      scalar1=rt["goh"][:, g:g + 1], scalar2=None, op0=ALU.mult, R=["rgw", "rgoh"], W=[f"gates{t}"])
        while ntask < len(tasks):
            do_task(ntask)
            ntask += 1
        mk.barrier()
        P.close()

    def moe_part(l, tiles, h2T, gates):
        P = Pool(nc, f"me{l}")
        TB = 8
        blocks = [(tiles[k], min(TB, len(tiles) - k)) for k in range(0, len(tiles), TB)]
        acc = P.sb([128, TB, D], F32, "acc")
        xt = [P.sb([128, D], F32, "xt") for _ in range(2)]
        w1b = [P.sb([128, 8, 512], BF16, "w1b") for _ in range(2)]
        w3b = [P.sb([128, 8, 512], BF16, "w3b") for _ in range(2)]
        w2b = [P.sb([128, 4, D], BF16, "w2b") for _ in range(2)]
        sl = [P.sb([128, 512], F32, "sl") for _ in range(2)]
        actT = [P.sb([128, 4, 512], BF16, "actT") for _ in range(2)]
        nw = 0
        for (tb0, tn) in blocks:
            ntok = tn * 128
            sblocks = [(s0, min(512, ntok - s0)) for s0 in range(0, ntok, 512)]
            hk = [f"h2T{tb0 + tt}" for tt in range(tn)]
            for e in range(16):
                wi = nw % 2
                nw += 1
                mk.dma("sp", w1b[wi][:].rearrange("p c f -> p (c f)"), W1B[e], R=[f"W1B{e}"], W=[f"w1b{wi}"])
                mk.dma("sp", w3b[wi][:].rearrange("p c f -> p (c f)"), W3B[e], R=[f"W3B{e}"], W=[f"w3b{wi}"])
                mk.dma("sp", w2b[wi][:].rearrange("p c f -> p (c f)"), W2B[e], R=[f"W2B{e}"], W=[f"w2b{wi}"])
                for (s0, sw) in sblocks:
                    ai = (s0 // 512) % 2
                    h0 = tb0 * 128 + s0
                    for fcn in range(4):
                        for k in range(8):
                            mk.op("pe", "matmul", PS[0][:, 0:sw], lhsT=w1b[wi][:, k, fcn * 128:(fcn + 1) * 128],
                                  rhs=h2T[:, k, h0:h0 + sw], start=(k == 0), stop=(k == 7), R=[f"w1b{wi}"] + hk, W=["ps0"])
                        for k in range(8):
                            mk.op("pe", "matmul", PS[1][:, 0:sw], lhsT=w3b[wi][:, k, fcn * 128:(fcn + 1) * 128],
                                  rhs=h2T[:, k, h0:h0 + sw], start=(k == 0), stop=(k == 7), R=[f"w3b{wi}"] + hk, W=["ps1"])
                        si = fcn % 2
                        mk.op("act", "activation", out=sl[si][:, 0:sw], in_=PS[0][:, 0:sw], func=AF.Silu, R=["ps0"],
                              W=[f"sl{si}"])
                        mk.op("dve", "tensor_tensor", out=actT[ai][:, fcn, 0:sw], in0=sl[si][:, 0:sw], in1=PS[1][:, 0:sw],
                              op=ALU.mult, R=[f"sl{si}", "ps1"], W=[f"actT{ai}"])
                    for q in range(sw // 128):
                        tt = s0 // 128 + q
                        t = tb0 + tt
                        for cb in range(2):
                            pb = 2 + cb
                            for fcn in range(4):
                                mk.op("pe", "matmul", PS[pb][:, :], lhsT=actT[ai][:, fcn, q * 128:(q + 1) * 128],
                                      rhs=w2b[wi][:, fcn, cb * 512:(cb + 1) * 512], start=(fcn == 0), stop=(fcn == 3),
                                      R=[f"actT{ai}", f"w2b{wi}"], W=[f"ps{pb}"])
                            if e == 0:
                                mk.op("dve", "tensor_scalar", out=acc[:, tt, cb * 512:(cb + 1) * 512], in0=PS[pb][:, :],
                                      scalar1=gates[:, t, e:e + 1], scalar2=None, op0=ALU.mult,
                                      R=[f"ps{pb}", f"gates{t}"], W=[f"acc{tt}"])
                            else:
                                mk.op("dve", "scalar_tensor_tensor", out=acc[:, tt, cb * 512:(cb + 1) * 512],
                                      in0=PS[pb][:, :], scalar=gates[:, t, e:e + 1], in1=acc[:, tt, cb * 512:(cb + 1) * 512],
                                      op0=ALU.mult, op1=ALU.add, R=[f"ps{pb}", f"gates{t}", f"acc{tt}"], W=[f"acc{tt}"])
            for tt in range(tn):
                t = tb0 + tt
                j = cond_of(t)
                mk.op("pool", "tensor_tensor", out=acc[:, tt, :], in0=acc[:, tt, :], in1=GB[:, j, 1, :], op=ALU.mult,
                      R=[f"acc{tt}", "GB"], W=[f"acc{tt}"])
                i2 = tt % 2
                mk.dma("sp", xt[i2][:], XR[t * 128:(t + 1) * 128, :], R=[f"XR{t}"], W=[f"ext{i2}"])
                mk.op("dve", "tensor_tensor", out=acc[:, tt, :], in0=acc[:, tt, :], in1=xt[i2][:], op=ALU.add,
                      R=[f"acc{tt}", f"ext{i2}"], W=[f"acc{tt}"])
                mk.dma("pool", XR[t * 128:(t + 1) * 128, :], acc[:, tt, :], R=[f"acc{tt}"], W=[f"XR{t}"])
        mk.barrier()
        P.close()

    def final_phase():
        P = Pool(nc, "fin")
        fw = P.sb([128, D], F32, "fw")
        mk.dma("sp", fw[:], fnw, W=["fw"])
        xt = [P.sb([128, D], F32, "xt") for _ in range(2)]
        ot = [P.sb([128, D], F32, "ot") for _ in range(2)]
        junk = P.sb([128, D], BF16, "junk")
        ss = [P.sb([128, 1], F32, "ss") for _ in range(2)]
        for t in OUT_T:
            i = t % 2
            mk.dma("sp", xt[i][:], XR[t * 128:(t + 1) * 128, :], R=[f"XR{t}"], W=[f"fxt{i}"])
            mk.op("act", "activation", out=junk[:], in_=xt[i][:], func=AF.Square, accum_out=ss[i][:], R=[f"fxt{i}"],
                  W=["fjunk", f"fss{i}"])
            mk.op("act", "activation", out=ss[i][:], in_=ss[i][:], func=AF.Sqrt, scale=1.0 / D, bias=EPS, R=[f"fss{i}"],
                  W=[f"fss{i}"])
            mk.op("dve", "reciprocal", out=ss[i][:], in_=ss[i][:], R=[f"fss{i}"], W=[f"fss{i}"])
            mk.op("dve", "scalar_tensor_tensor", out=ot[i][:], in0=xt[i][:], scalar=ss[i][:, 0:1], in1=fw[:], op0=ALU.mult,
                  op1=ALU.mult, R=[f"fxt{i}", f"fss{i}", "fw"], W=[f"fot{i}"])
            mk.dma("pool", yout[(t - 2) * 128:(t - 1) * 128, :], ot[i][:], R=[f"fot{i}"], W=["yout"])
        mk.barrier()
        P.close()

    stages = dict(mod=mod_phase, inproj=inproj_phase, a=mixer_a, b=mixer_b, c=mixer_c, d=mixer_d)
    return dict(nc=nc, mk=mk, stages=stages, merge=merge_moe_phase, final=final_phase, dbg=dbg,
                scr=dict(XR=XR, FM=FM, TM=TM, MG=MG, YT=YT))


def emit_all(prog, layers=NL, upto=None, skip=()):
    mk = prog["mk"]
    mk.barrier()
    done = False
    for l in range(layers):
        for s in ("mod", "inproj", "a", "b", "c", "d"):
            if s in skip:
                continue
            if s in ("inproj", "b", "c", "d"):
                prog["stages"][s](l, l == NL - 1)
            else:
                prog["stages"][s](l)
            if upto == (l, s):
                done = True
                break
        if done:
            break
        prog["merge"](l, l == NL - 1)
        if upto == (l, "merge"):
            done = True
            break
    if not done:
        prog["final"]()
    mk.barrier(engines=("sp",))


def _consts():
    j = np.arange(128)[:, None]
    i = np.arange(128)[None, :]
    same = (j // DL) == (i // DL)
    m = {}
    m["ident"] = (j == i)
    m["triF"] = (j <= i)
    m["triB"] = (j >= i)
    m["blkF"] = same & (j <= i)
    m["blkB"] = same & (j >= i)
    m["aftF"] = same & (j > i)
    m["befB"] = same & (j < i)
    m["diffF"] = np.maximum(i - j, 0)
    m["diffB"] = np.maximum(j - i, 0)
    m["maskF"] = (i >= j)
    m["maskB"] = (j > i)
    m["posF"] = np.broadcast_to(i + 1, (128, 128))
    m["posB"] = np.broadcast_to(128 - i, (128, 128))
    m["negF4"] = np.tile(np.where(j <= i, 0.0, -30000.0), (1, 4))
    m["negB4"] = np.tile(np.where(j >= i, 0.0, -30000.0), (1, 4))
    m["mblkF4"] = np.tile(same & (j <= i), (1, 4))
    m["mblkB4"] = np.tile(same & (j >= i), (1, 4))
    m["kpos"] = np.concatenate([127 - j, j], 1)
    m["ones"] = np.ones((128, 128))
    sel = np.zeros((128, 256))
    sel[0, 0:128] = 1.0
    sel[1, 128:256] = 1.0
    m["sel"] = sel
    m["subm"] = np.concatenate([(j // DL) == s_ for s_ in range(128 // DL)], 1)
    qm = np.zeros((128, 2, 5, 128), np.float32)
    for hh_ in range(2):
        for s_ in range(5):
            colsel = np.ones(128, bool) if s_ == 4 else (np.arange(128) // DL == s_)
            qm[hh_ * 64:(hh_ + 1) * 64, hh_, s_, :] = colsel[None, :]
    m["qmask"] = qm.reshape(128, 1280)
    out = np.zeros((128, NCST), np.float32)
    for k, (o, w) in CST.items():
        out[:, o:o + w] = np.asarray(m[k], np.float32)
    return out


def _rope_tables(flip=False):
    n = 16
    inv = np.power(np.float32(10000.0), -np.arange(n, dtype=np.float32) / n).astype(np.float32)
    t = np.arange(4096)
    row = (t // 64).astype(np.float32)
    col = (t % 64).astype(np.float32)
    ang = np.concatenate([row[:, None] * inv, col[:, None] * inv], -1)
    cos = np.cos(ang).astype(np.float32).T
    sin = np.sin(ang).astype(np.float32).T
    if flip:
        cos, sin = cos[:, ::-1], sin[:, ::-1]
    Cc = np.ones((128, T), np.float32)
    Ss = np.zeros((128, T), np.float32)
    for hh in range(2):
        Cc[hh * 64:hh * 64 + 32, 256:] = cos
        Cc[hh * 64 + 32:hh * 64 + 64, 256:] = cos
        Ss[hh * 64:hh * 64 + 32, 256:] = -sin
        Ss[hh * 64 + 32:hh * 64 + 64, 256:] = sin
    return Cc, Ss


def prep_shared(inp, flip=False):
    f = np.float32
    w_in = np.asarray(inp["w_in"], f)
    offs = {}
    o = 0
    for name, w in (("a_x", 256), ("a_g", 256), ("b_q", 256), ("b_k", 256), ("b_v", 256), ("b_g", 256), ("c_q", 256),
                    ("c_k", 256), ("c_v", 256), ("c_o", 256), ("c_gates", 16), ("d_q", 256), ("d_ff", 256),
                    ("d_fb", 256), ("d_i", 256), ("d_g", 256), ("merge", 4096)):
        offs[name] = (o, w)
        o += w

    def cols(n):
        a, w = offs[n]
        return w_in[:, :, a:a + w]

    perm = np.concatenate([np.arange(h * 64 + 32, h * 64 + 64).tolist() + np.arange(h * 64, h * 64 + 32).tolist()
                           for h in range(4)]).astype(np.int64)
    w_fm = np.concatenate([cols("a_x"), cols("a_g"), cols("b_q"), cols("b_q")[:, :, perm], cols("b_k"),
                           cols("b_k")[:, :, perm], cols("c_q"), cols("c_k")], -1)
    gperm = np.array([8, 9, 10, 11, 12, 13, 14, 15, 0, 1, 2, 3, 4, 5, 6, 7]) if flip else np.arange(16)
    dfa, dfb = ("d_fb", "d_ff") if flip else ("d_ff", "d_fb")
    w_tm = np.concatenate([cols("b_v"), cols("b_g"), cols("c_v"), cols("c_o"), cols("d_q"), cols(dfa), cols(dfb),
                           cols("d_i"), cols("d_g"), cols("c_gates")[:, :, gperm], cols("merge")], -1)
    sh = {}
    sh["w_mod"] = np.ascontiguousarray(inp["w_mod"], f)
    bm = np.asarray(inp["b_mod"], f)
    sh["bmod_c"] = np.ascontiguousarray(bm.reshape(NL, 48, 128).transpose(0, 2, 1))
    sh["bmod_r"] = np.ascontiguousarray(bm.reshape(NL, 1, 6144))
    sh["w_fm"] = np.ascontiguousarray(w_fm)
    sh["w_tm"] = np.ascontiguousarray(w_tm)
    acw = np.asarray(inp["a_conv_w"], f)
    zt_ = np.zeros_like(acw[:, :1])
    acw = np.concatenate([zt_, acw[:, ::-1]], 1) if flip else np.concatenate([acw, zt_], 1)
    sh["a_cw"] = np.ascontiguousarray(acw.reshape(NL, 5, 2, 128).transpose(0, 3, 2, 1))
    sh["a_cb"] = np.ascontiguousarray(np.asarray(inp["a_conv_b"], f).reshape(NL, 2, 128).transpose(0, 2, 1))
    gw = np.asarray(inp["a_gate_w"], f)
    agw = np.zeros((NL, 128, 2, 2, 2, 128), f)
    for c in range(2):
        for hh in range(2):
            agw[:, hh * 64:(hh + 1) * 64, :, :, c, hh * 64:(hh + 1) * 64] = gw[:, :, :, 2 * c + hh].transpose(0, 3, 1, 2, 4)
    sh["a_gw"] = np.ascontiguousarray(agw[:, :, ::-1]) if flip else agw
    gb = np.asarray(inp["a_gate_b"], f)
    if flip:
        gb = gb[:, ::-1]
    sh["a_gb"] = np.ascontiguousarray(gb.reshape(NL, 2, 2, 2, 128).transpose(0, 4, 1, 2, 3))
    lam = np.asarray(inp["a_lambda"], f)
    if flip:
        lam = lam[:, ::-1]
    sh["a_lam"] = np.ascontiguousarray(lam.reshape(NL, 2, 2, 128).transpose(0, 3, 1, 2))
    th = np.asarray(inp["b_theta"], f)
    if flip:
        th = th[:, ::-1]
    thp = np.zeros((NL, 128, 2, 2), f)
    for c in range(2):
        for hh in range(2):
            thp[:, hh * 64:(hh + 1) * 64, :, c] = th[:, None, :, 2 * c + hh]
    sh["b_thp"] = thp
    sh["b_thh"] = np.ascontiguousarray(np.broadcast_to(th[:, None], (NL, 128, 2, 4)))
    ccw = np.asarray(inp["c_conv_w"], f)
    zt_ = np.zeros_like(ccw[:, :1])
    ccw = np.concatenate([zt_, ccw[:, ::-1]], 1) if flip else np.concatenate([ccw, zt_], 1)
    sh["c_cw"] = np.ascontiguousarray(ccw.reshape(NL, 5, 4, 128).transpose(0, 3, 2, 1))
    sh["c_cb"] = np.ascontiguousarray(np.asarray(inp["c_conv_b"], f).reshape(NL, 4, 128).transpose(0, 2, 1))
    sh["c_gb"] = np.ascontiguousarray(np.broadcast_to(np.asarray(inp["c_gate_b"], f).reshape(NL, 1, 16)[:, :, gperm], (NL, 128, 16)))
    sh["d_lbr"] = np.ascontiguousarray(np.broadcast_to(np.asarray(inp["d_lb"], f)[None], (128, 2, 256)))
    sh["w_branch"] = np.ascontiguousarray(inp["w_branch"], f)
    sh["w_out"] = np.ascontiguousarray(inp["w_out"], f)
    sh["moe_wgr"] = np.ascontiguousarray(np.concatenate([np.asarray(inp["moe_w_group"], f), np.asarray(inp["moe_w_router"], f)], -1))
    sh["moe_bgr"] = np.ascontiguousarray(np.concatenate([np.asarray(inp["moe_b_group"], f), np.asarray(inp["moe_b_router"], f)], -1).reshape(NL, 1, 20))
    sh["moe_w1"] = np.ascontiguousarray(inp["moe_w1"], f)
    sh["moe_w3"] = np.ascontiguousarray(inp["moe_w3"], f)
    sh["moe_w2"] = np.ascontiguousarray(inp["moe_w2"], f)
    sh["fnw"] = np.ascontiguousarray(np.broadcast_to(np.asarray(inp["final_norm_w"], f)[None], (128, D)))
    sh["cst"] = _consts()
    sh["ropeC"], sh["ropeS"] = _rope_tables(flip)
    return sh


def prep_core(inp, b, flip=False):
    f = np.float32
    d = {}
    cx, xx = np.asarray(inp["ctx"][b], f), np.asarray(inp["x"][b], f)
    if flip:
        cx, xx = cx[::-1], xx[::-1]
    d["xin"] = np.ascontiguousarray(np.concatenate([cx, xx], 0))
    cv = np.stack([np.asarray(inp["c_ctx"], f), np.asarray(inp["c"][b], f)], -1)
    d["cvec"] = np.ascontiguousarray(cv.reshape(8, 128, 2).transpose(1, 0, 2))
    return d


_PROG = None


def kernel(**inputs):
    global _PROG
    if _PROG is None:
        _PROG = build_program()
        emit_all(_PROG)
    nc = _PROG["nc"]
    shs = [prep_shared(inputs, False), prep_shared(inputs, True)]
    in_maps = []
    for core in range(8):
        fl = core >= 4
        m = dict(shs[1 if fl else 0])
        m.update(prep_core(inputs, core % 4, fl))
        in_maps.append(m)
    res = run_bass_kernel_spmd(nc, in_maps, core_ids=list(range(8)))
    out = np.empty((4, 4096, D), np.float32)
    for b in range(4):
        out[b, :HALF_OUT] = np.asarray(res.results[b]["yout"], np.float32)[:HALF_OUT]
        out[b, HALF_OUT:] = np.asarray(res.results[b + 4]["yout"], np.float32)[:4096 - HALF_OUT][::-1]
    return out
```

```python
import contextlib
import numpy as np
import ml_dtypes
import concourse.bass as bass
import concourse.mybir as mybir
from concourse.bass_utils import run_bass_kernel_spmd

F32 = mybir.dt.float32
BF16 = mybir.dt.bfloat16
AF = mybir.ActivationFunctionType
ALU = mybir.AluOpType
AX = mybir.AxisListType

T = 4352
NT = 34
D = 1024
EPS = 1e-6
NL = 2
HALF_OUT = 2048
DL = 32
DNS = 128 // DL
DBG = dict(maxit=None, core=True, fin=True, heads=(0, 1, 2, 3))
TMW = 2320
TM_OFF = dict(b_v=0, b_g=256, c_v=512, c_o=768, d_q=1024, d_ff=1280, d_fb=1536, d_i=1792, d_g=2048, c_gates=2304)
FM_OFF = dict(a_x=0, a_g=2, b_q=4, b_qp=6, b_k=8, b_kp=10, c_q=12, c_k=14)

CST = {}
_off = 0
for _n, _w in (("ident", 128), ("triF", 128), ("triB", 128), ("blkF", 128), ("blkB", 128), ("aftF", 128),
               ("befB", 128), ("diffF", 128), ("diffB", 128), ("maskF", 128), ("maskB", 128), ("posF", 128),
               ("posB", 128), ("negF4", 512), ("negB4", 512), ("mblkF4", 512), ("mblkB4", 512), ("kpos", 2),
               ("ones", 128), ("sel", 256), ("subm", 4), ("qmask", 1280)):
    CST[_n] = (_off, _w)
    _off += _w
NCST = _off


class MK:
    SEM_ROT = 30000

    def __init__(self, nc, ndma=8):
        self.nc = nc
        self.engs = {"pe": nc.tensor, "act": nc.scalar, "dve": nc.vector, "pool": nc.gpsimd, "sp": nc.sync}
        self._ctxs = []
        self.nsem = 0
        self.sem = {}
        self.cnt = {}
        for e in ("pe", "act", "dve", "pool"):
            self.sem[e] = self._newsem("s_" + e)
            self.cnt[e] = 0
        self.seen = {e: {} for e in self.engs}
        self.dq = {}
        for q in ("sp", "pool"):
            self.dq[q] = {"i": 0, "slots": [[self._newsem(f"d_{q}{i}"), 0] for i in range(ndma)]}
        self.res = {}
        self.ninst = 0
        self.flip = 0

    def _newsem(self, name):
        self.nsem += 1
        cm = self.nc.semaphore(f"{name}_{self.nsem}")
        s = cm.__enter__()
        self._ctxs.append(cm)
        return s

    def _wait(self, eng, tok):
        sem, val = tok
        key = id(sem)
        if self.seen[eng].get(key, 0) >= val:
            return
        self.engs[eng].wait_ge(sem, val)
        self.seen[eng][key] = val

    def _deps(self, R, W):
        deps = []
        for k in R:
            st = self.res.get(k)
            if st and st[0] is not None:
                deps.append(st[0])
        for k in W:
            st = self.res.get(k)
            if st:
                if st[0] is not None:
                    deps.append(st[0])
                deps.extend(st[1])
        return deps

    def _record(self, tok, R, W):
        for k in R:
            st = self.res.setdefault(k, [None, []])
            st[1] = [t for t in st[1] if t[0] is not tok[0]] + [tok]
        for k in W:
            self.res[k] = [tok, []]

    def op(self, eng, method, *args, R=(), W=(), **kw):
        for tok in self._deps(R, W):
            if eng == "pe" and tok[0] is self.sem["pe"]:
                continue
            self._wait(eng, tok)
        ins = getattr(self.engs[eng], method)(*args, **kw)
        if self.cnt[eng] >= self.SEM_ROT:
            self.sem[eng] = self._newsem("s_" + eng)
            self.cnt[eng] = 0
        self.cnt[eng] += 1
        ins.then_inc(self.sem[eng], 1)
        tok = (self.sem[eng], self.cnt[eng])
        self._record(tok, R, W)
        self.ninst += 1
        return tok

    def dma(self, q, out, in_, R=(), W=(), **kw):
        d = self.dq[q]
        slot = d["slots"][d["i"] % len(d["slots"])]
        d["i"] += 1
        if slot[1] > 0:
            self._wait(q, (slot[0], slot[1]))
        if slot[1] >= self.SEM_ROT:
            slot[0] = self._newsem("d_" + q)
            slot[1] = 0
        for tok in self._deps(R, W):
            self._wait(q, tok)
        ins = self.engs[q].dma_start(out=out, in_=in_, **kw)
        slot[1] += 16
        ins.then_inc(slot[0], 16)
        tok = (slot[0], slot[1])
        self._record(tok, R, W)
        self.ninst += 1
        return tok

    def barrier(self, engines=("pe", "act", "dve", "pool", "sp")):
        toks = []
        for q, d in self.dq.items():
            for slot in d["slots"]:
                if slot[1] > 0:
                    toks.append((slot[0], slot[1]))
        for e in ("pe", "act", "dve", "pool"):
            if self.cnt[e] > 0:
                toks.append((self.sem[e], self.cnt[e]))
        for e in engines:
            for tok in toks:
                if e in self.sem and tok[0] is self.sem[e]:
                    continue
                self._wait(e, tok)

    def ev(self, out, in_, R=(), W=(), func=None, **kw):
        if func is not None:
            return self.op("act", "activation", out=out, in_=in_, func=func, R=R, W=W, **kw)
        self.flip ^= 1
        if self.flip:
            return self.op("act", "activation", out=out, in_=in_, func=AF.Copy, R=R, W=W)
        return self.op("dve", "tensor_copy", out=out, in_=in_, R=R, W=W)


class Pool:
    def __init__(self, nc, tag):
        self.nc = nc
        self.tag = tag
        self.stack = contextlib.ExitStack()
        self.n = 0

    def sb(self, shape, dt=F32, name=None):
        self.n += 1
        return self.stack.enter_context(self.nc.sbuf_tensor(f"{self.tag}_{name or 't'}{self.n}", list(shape), dt))

    def close(self):
        self.stack.close()


def build_program(debug=()):
    nc = bass.Bass("TRN2", target_bir_lowering=False)

    def din(name, shape, dt=F32):
        return nc.dram_tensor(name, list(shape), dt, kind="ExternalInput").ap()

    def dscr(name, shape, dt=F32):
        return nc.dram_tensor(name, list(shape), dt, kind="Internal").ap()

    xin = din("xin", [T, D])
    cvec = din("cvec", [128, 8, 2])
    w_mod = din("w_mod", [NL, D, 6144])
    bmod_c = din("bmod_c", [NL, 128, 48])
    bmod_r = din("bmod_r", [NL, 1, 6144])
    w_fm = din("w_fm", [NL, D, 2048])
    w_tm = din("w_tm", [NL, D, TMW + 4096])
    a_cw = din("a_cw", [NL, 128, 2, 5])
    a_cb = din("a_cb", [NL, 128, 2])
    a_gw = din("a_gw", [NL, 128, 2, 2, 2, 128])
    a_gb = din("a_gb", [NL, 128, 2, 2, 2])
    a_lam = din("a_lam", [NL, 128, 2, 2])
    b_thp = din("b_thp", [NL, 128, 2, 2])
    b_thh = din("b_thh", [NL, 128, 2, 4])
    c_cw = din("c_cw", [NL, 128, 4, 5])
    c_cb = din("c_cb", [NL, 128, 4])
    c_gb = din("c_gb", [NL, 128, 16])
    d_lbr = din("d_lbr", [128, 2, 256])
    w_branch = din("w_branch", [NL, 4, 256, D])
    w_out = din("w_out", [NL, D, D])
    moe_wgr = din("moe_wgr", [NL, D, 20])
    moe_bgr = din("moe_bgr", [NL, 1, 20])
    moe_w1 = din("moe_w1", [NL, 16, D, 512])
    moe_w3 = din("moe_w3", [NL, 16, D, 512])
    moe_w2 = din("moe_w2", [NL, 16, 512, D])
    fnw = din("fnw", [128, D])
    cst_d = din("cst", [128, NCST])
    ropeC_d = din("ropeC", [128, T])
    ropeS_d = din("ropeS", [128, T])
    yout = nc.dram_tensor("yout", [HALF_OUT, D], F32, kind="ExternalOutput").ap()

    XR = dscr("XR", [T, D])
    FM = dscr("FM", [16, 128, T])
    TM = dscr("TM", [T, TMW])
    MG = dscr("MG", [T, 4096], BF16)
    YT = dscr("YT", [4, 2, 128, T], BF16)
    dbg = {}
    for name, shape, dt in debug:
        dbg[name] = nc.dram_tensor("dbg_" + name, list(shape), dt, kind="ExternalOutput").ap()

    mk = MK(nc)
    G = Pool(nc, "g")

    PS = [nc.psum_tensor(f"ps{i}", [128, 512], F32).__enter__() for i in range(6)]
    PQ = [nc.psum_tensor(f"pq{i}", [128, 1024], BF16).__enter__() for i in range(2)]

    cst = G.sb([128, NCST], F32, "cst")
    identb = G.sb([128, 128], BF16, "identb")
    sT = G.sb([128, 8, 2], F32, "sT")
    MODC = G.sb([128, 4, 8, 2], F32, "MODC")
    GB = G.sb([128, 2, 2, D], F32, "GB")
    mk.dma("sp", cst[:], cst_d, W=["cst"])
    mk.dma("sp", sT[:], cvec, W=["sT"])

    def C(name, rows=slice(0, 128)):
        o, w = CST[name]
        return cst[rows, o:o + w]

    mk.op("dve", "tensor_copy", out=identb[:], in_=C("ident"), R=["cst"], W=["identb"])
    mk.op("act", "activation", out=sT[:], in_=sT[:], func=AF.Silu, R=["sT"], W=["sT"])
    for t in range(NT):
        mk.dma("sp", XR[t * 128:(t + 1) * 128, :], xin[t * 128:(t + 1) * 128, :], W=[f"XR{t}"])

    def cond_of(t):
        return 0 if t < 2 else 1

    def mod_phase(l):
        P = Pool(nc, f"mod{l}")
        wblk = P.sb([128, 8, 1024], F32, "wblk")
        bmc = P.sb([128, 48], F32, "bmc")
        bmr = P.sb([1, 6144], F32, "bmr")
        GR = P.sb([2, 2, D], F32, "GR")
        mk.dma("sp", bmc[:], bmod_c[l], W=["bmc"])
        mk.dma("sp", bmr[:], bmod_r[l], W=["bmr"])
        sel = C("sel", slice(0, 2))
        for m in range(6):
            mk.dma("sp", wblk[:], w_mod[l].rearrange("(c p) f -> p c f", p=128)[:, :, m * 1024:(m + 1) * 1024],
                   W=["wblk"])
            if m in (0, 1, 3, 4):
                m4 = {0: 0, 1: 1, 3: 2, 4: 3}[m]
                for c in range(8):
                    for k in range(8):
                        mk.op("pe", "matmul", PS[0][:, c * 2:(c + 1) * 2], lhsT=wblk[:, k, c * 128:(c + 1) * 128],
                              rhs=sT[:, k, :], start=(k == 0), stop=(k == 7), R=["wblk", "sT"], W=["ps0"])
                mk.op("dve", "tensor_tensor", out=MODC[:, m4, :, :],
                      in0=PS[0][:, 0:16].rearrange("p (c j) -> p c j", j=2),
                      in1=bmc[:, m * 8:(m + 1) * 8].unsqueeze(2).to_broadcast([128, 8, 2]), op=ALU.add,
                      R=["ps0", "bmc"], W=["MODC"])
                if m in (1, 4):
                    mk.op("dve", "tensor_scalar_add", out=MODC[:, m4, :, :], in0=MODC[:, m4, :, :], scalar1=1.0,
                          R=["MODC"], W=["MODC"])
            else:
                mi = 0 if m == 2 else 1
                for cb in range(2):
                    for k in range(8):
                        mk.op("pe", "matmul", PS[1][0:2, :], lhsT=sT[:, k, :], rhs=wblk[:, k, cb * 512:(cb + 1) * 512],
                              start=(k == 0), stop=False, R=["wblk", "sT"], W=["ps1"])
                    mk.op("pe", "matmul", PS[1][0:2, :], lhsT=sel[0:1, 0:2],
                          rhs=bmr[0:1, m * 1024 + cb * 512: m * 1024 + (cb + 1) * 512], start=False, stop=True,
                          R=["bmr", "cst"], W=["ps1"])
                    mk.op("dve", "tensor_copy", out=GR[0:2, mi, cb * 512:(cb + 1) * 512], in_=PS[1][0:2, :],
                          R=["ps1"], W=["GR"])
        for j in range(2):
            for mi in range(2):
                for cb in range(2):
                    mk.op("pe", "matmul", PS[1][:, :], lhsT=sel[0:2, j * 128:(j + 1) * 128],
                          rhs=GR[0:2, mi, cb * 512:(cb + 1) * 512], start=True, stop=True, R=["GR", "cst"], W=["ps1"])
                    mk.op("act", "activation", out=GB[:, j, mi, cb * 512:(cb + 1) * 512], in_=PS[1][:, :], func=AF.Copy,
                          R=["ps1"], W=["GB"])
        mk.barrier()
        P.close()

    def make_norm(P, nbuf=2):
        st = dict(junk=P.sb([128, D], BF16, "junk"), ss=[P.sb([128, 1], F32, "ss") for _ in range(nbuf)],
                  xn=[P.sb([128, D], BF16, "xn") for _ in range(nbuf)],
                  tmp=[P.sb([128, 8, 128], F32, "tmp") for _ in range(nbuf)], i=0, nbuf=nbuf)

        def norm(xt_ap, xt_key, j, msc, msh, h_out, h_key):
            i = st["i"] % st["nbuf"]
            st["i"] += 1
            ss, xn, tmp = st["ss"][i], st["xn"][i], st["tmp"][i]
            mk.op("act", "activation", out=st["junk"][:], in_=xt_ap, func=AF.Square, accum_out=ss[:],
                  R=[xt_key], W=["junk", f"ss{i}"])
            mk.op("act", "activation", out=ss[:], in_=ss[:], func=AF.Sqrt, scale=1.0 / D, bias=EPS,
                  R=[f"ss{i}"], W=[f"ss{i}"])
            mk.op("dve", "reciprocal", out=ss[:], in_=ss[:], R=[f"ss{i}"], W=[f"ss{i}"])
            mk.op("dve", "tensor_scalar", out=xn[:], in0=xt_ap, scalar1=ss[:, 0:1], scalar2=None, op0=ALU.mult,
                  R=[xt_key, f"ss{i}"], W=[f"xn{i}"])
            for c in range(8):
                mk.op("pe", "transpose", out=PQ[i][:, c * 128:(c + 1) * 128], in_=xn[:, c * 128:(c + 1) * 128],
                      identity=identb[:], R=[f"xn{i}", "identb"], W=[f"pq{i}"])
            mk.op("dve", "tensor_tensor", out=tmp[:], in0=PQ[i][:, :].rearrange("p (c n) -> p c n", c=8),
                  in1=MODC[:, msc, :, j:j + 1].to_broadcast([128, 8, 128]), op=ALU.mult,
                  R=[f"pq{i}", "MODC"], W=[f"ntmp{i}"])
            mk.op("pool", "tensor_tensor", out=h_out, in0=tmp[:],
                  in1=MODC[:, msh, :, j:j + 1].to_broadcast([128, 8, 128]), op=ALU.add,
                  R=[f"ntmp{i}", "MODC"], W=[h_key])
        return norm

    def inproj_phase(l, last=False):
        P = Pool(nc, f"ip{l}")
        hT = P.sb([128, 8, T], BF16, "hT")
        xt = [P.sb([128, D], F32, "xt") for _ in range(2)]
        norm = make_norm(P)
        for t in range(NT):
            i = t % 2
            mk.dma("sp", xt[i][:], XR[t * 128:(t + 1) * 128, :], R=[f"XR{t}"], W=[f"xt{i}"])
            norm(xt[i][:], f"xt{i}", cond_of(t), 1, 0, hT[:, :, t * 128:(t + 1) * 128], f"hT{t}")
        hkeys = [f"hT{t}" for t in range(NT)]
        wf = [P.sb([128, 8, 512], F32, "wf")] * 2
        wb = [P.sb([128, 8, 512], BF16, "wb") for _ in range(2)]
        stg = [P.sb([128, T], F32, "stg")] * 2
        nblk = 0
        tblocks = [(i * 512, min(512, T - i * 512)) for i in range(9)]
        for cb in range(4):
            i = nblk % 2
            nblk += 1
            mk.dma("sp", wf[i][:], w_fm[l].rearrange("(c p) f -> p c f", p=128)[:, :, cb * 512:(cb + 1) * 512],
                   W=["wf"])
            mk.ev(wb[i][:], wf[i][:], R=["wf"], W=[f"wb{i}"])
            for sub in range(4):
                fc = cb * 4 + sub
                si = fc % 2
                for bi, (t0, tw) in enumerate(tblocks):
                    pb = bi % 2
                    for k in range(8):
                        mk.op("pe", "matmul", PS[pb][:, 0:tw], lhsT=wb[i][:, k, sub * 128:(sub + 1) * 128],
                              rhs=hT[:, k, t0:t0 + tw], start=(k == 0), stop=(k == 7),
                              R=[f"wb{i}"] + hkeys[t0 // 128:(t0 + tw) // 128], W=[f"ps{pb}"])
                    mk.ev(stg[si][:, t0:t0 + tw], PS[pb][:, 0:tw], R=[f"ps{pb}"], W=["stg"])
                mk.dma("pool", FM[fc], stg[si][:], R=["stg"], W=[f"FM{fc}"])
        cblocks = [(i * 512, 512) for i in range(4)] + [(2048, TMW - 2048)] + [(TMW + i * 512, 512) for i in range(8)]
        stt = [P.sb([128, 4, 512], F32, "stt") for _ in range(2)]
        stb = [P.sb([128, 4, 512], BF16, "stb") for _ in range(2)]
        tgroups = [(g * 4, min(4, NT - g * 4)) for g in range(9)]
        ns = 0
        for (c0, cw) in cblocks:
            i = nblk % 2
            nblk += 1
            mk.dma("sp", wf[i][:, :, 0:cw], w_tm[l].rearrange("(c p) f -> p c f", p=128)[:, :, c0:c0 + cw], W=["wf"])
            mk.ev(wb[i][:, :, 0:cw], wf[i][:, :, 0:cw], R=["wf"], W=[f"wb{i}"])
            is_mg = c0 >= TMW
            for (g0, gn) in tgroups:
                if is_mg and last and not any((g0 + q) in OUT_T for q in range(gn)):
                    continue
                si = ns % 2
                ns += 1
                for tt in range(gn):
                    t = g0 + tt
                    pb = 2 + (t % 2)
                    for k in range(8):
                        mk.op("pe", "matmul", PS[pb][:, 0:cw], lhsT=hT[:, k, t * 128:(t + 1) * 128],
                              rhs=wb[i][:, k, 0:cw], start=(k == 0), stop=(k == 7), R=[f"wb{i}", f"hT{t}"], W=[f"ps{pb}"])
                    if is_mg:
                        mk.ev(stb[si][:, tt, 0:cw], PS[pb][:, 0:cw], R=[f"ps{pb}"], W=[f"stb{si}"], func=AF.Sigmoid)
                    else:
                        mk.ev(stt[si][:, tt, 0:cw], PS[pb][:, 0:cw], R=[f"ps{pb}"], W=[f"stt{si}"])
                if is_mg:
                    mk.dma("pool", MG[g0 * 128:(g0 + gn) * 128, c0 - TMW:c0 - TMW + cw].rearrange("(t p) c -> p t c", p=128),
                           stb[si][:, 0:gn, 0:cw], R=[f"stb{si}"], W=["MG"])
                else:
                    mk.dma("pool", TM[g0 * 128:(g0 + gn) * 128, c0:c0 + cw].rearrange("(t p) c -> p t c", p=128),
                           stt[si][:, 0:gn, 0:cw], R=[f"stt{si}"], W=["TM"])
        mk.barrier()
        P.close()

    SEGS = [(0, 256), (256, T)]

    def conv_fm(u, src, w4, bcol, keyu, keysrc, wkeys):
        for (s0, e) in SEGS:
            mk.op("act", "activation", out=u[:, s0:e], in_=src[:, s0:e], func=AF.Identity, scale=w4[:, 2:3], bias=bcol,
                  R=[keysrc] + wkeys, W=[keyu])
            for k, sh in ((0, -2), (1, -1), (3, 1), (4, 2)):
                if sh < 0:
                    o, i_ = u[:, s0 - sh:e], src[:, s0:e + sh]
                else:
                    o, i_ = u[:, s0:e - sh], src[:, s0 + sh:e]
                mk.op("dve", "scalar_tensor_tensor", out=o, in0=i_, scalar=w4[:, k:k + 1], in1=o, op0=ALU.mult,
                      op1=ALU.add, R=[keysrc, keyu] + wkeys, W=[keyu])

    def mixer_a(l):
        P = Pool(nc, f"ma{l}")
        cw = P.sb([128, 2, 5], F32, "cw")
        cb = P.sb([128, 2], F32, "cb")
        gwf = P.sb([128, 2, 2, 2, 128], F32, "gwf")
        gwb = P.sb([128, 2, 2, 2, 128], BF16, "gwb")
        gb = P.sb([128, 2, 2, 2], F32, "gb")
        lam = P.sb([128, 2, 2], F32, "lam")
        c1 = P.sb([128, 2, 2], F32, "c1")
        mk.dma("sp", cw[:], a_cw[l], W=["a_cw"])
        mk.dma("sp", cb[:], a_cb[l], W=["a_cb"])
        mk.dma("sp", gwf[:], a_gw[l], W=["a_gwf"])
        mk.dma("sp", gb[:], a_gb[l], W=["a_gb"])
        mk.dma("sp", lam[:], a_lam[l], W=["a_lam"])
        mk.op("dve", "tensor_copy", out=gwb[:], in_=gwf[:], R=["a_gwf"], W=["a_gwb"])
        mk.op("act", "activation", out=c1[:], in_=lam[:], func=AF.Exp, scale=-1.0, R=["a_lam"], W=["a_c1"])
        mk.op("act", "activation", out=c1[:], in_=c1[:], func=AF.Ln, bias=1.0, R=["a_c1"], W=["a_c1"])
        mk.op("dve", "tensor_scalar", out=c1[:], in0=c1[:], scalar1=-8.0, scalar2=None, op0=ALU.mult, R=["a_c1"], W=["a_c1"])
        ax = P.sb([128, T], F32, "ax")
        ag = P.sb([128, T], F32, "ag")
        u = P.sb([128, T], F32, "u")
        ub = P.sb([128, T], BF16, "ub")
        aa = P.sb([128, T], F32, "aa")
        bt = P.sb([128, T], F32, "bt")
        hf = P.sb([128, T], F32, "hf")
        hb = P.sb([128, T], F32, "hb")
        r = [P.sb([128, 512], F32, "r") for _ in range(2)]
        gi = [P.sb([128, 512], F32, "gi") for _ in range(2)]
        yb = P.sb([128, T], BF16, "yb")
        tblocks = [(i * 512, min(512, T - i * 512)) for i in range(9)]
        for c in range(2):
            mk.dma("sp", ax[:], FM[FM_OFF["a_x"] + c], R=[f"FM{FM_OFF['a_x'] + c}"], W=["ax"])
            mk.dma("sp", ag[:], FM[FM_OFF["a_g"] + c], R=[f"FM{FM_OFF['a_g'] + c}"], W=["ag"])
            conv_fm(u, ax, cw[:, c, :], cb[:, c:c + 1], "u", "ax", ["a_cw", "a_cb"])
            mk.op("pool", "tensor_copy", out=ub[:], in_=u[:], R=["u"], W=["ub"])
            for d in range(2):
                for bi, (t0, tw) in enumerate(tblocks):
                    i = bi % 2
                    mk.op("pe", "matmul", PS[i][:, 0:tw], lhsT=gwb[:, d, 0, c, :], rhs=ub[:, t0:t0 + tw], start=True,
                          stop=True, R=["a_gwb", "ub"], W=[f"ps{i}"])
                    mk.op("pe", "matmul", PS[2 + i][:, 0:tw], lhsT=gwb[:, d, 1, c, :], rhs=ub[:, t0:t0 + tw], start=True,
                          stop=True, R=["a_gwb", "ub"], W=[f"ps{2 + i}"])
                    mk.op("act", "activation", out=r[i][:, 0:tw], in_=PS[i][:, 0:tw], func=AF.Sigmoid,
                          bias=gb[:, d, 0, c:c + 1], R=[f"ps{i}", "a_gb"], W=[f"r{i}"])
                    mk.op("act", "activation", out=gi[i][:, 0:tw], in_=PS[2 + i][:, 0:tw], func=AF.Sigmoid,
                          bias=gb[:, d, 1, c:c + 1], R=[f"ps{2 + i}", "a_gb"], W=[f"gi{i}"])
                    mk.op("act", "activation", out=aa[:, t0:t0 + tw], in_=r[i][:, 0:tw], func=AF.Exp,
                          scale=c1[:, d, c:c + 1], R=[f"r{i}", "a_c1"], W=["aa"])
                    mk.op("dve", "tensor_tensor", out=r[i][:, 0:tw], in0=aa[:, t0:t0 + tw], in1=aa[:, t0:t0 + tw],
                          op=ALU.mult, R=["aa", f"r{i}"], W=[f"r{i}"])
                    mk.op("dve", "tensor_scalar", out=r[i][:, 0:tw], in0=r[i][:, 0:tw], scalar1=-1.0, scalar2=1.0,
                          op0=ALU.mult, op1=ALU.add, R=[f"r{i}"], W=[f"r{i}"])
                    mk.op("act", "activation", out=r[i][:, 0:tw], in_=r[i][:, 0:tw], func=AF.Sqrt, R=[f"r{i}"], W=[f"r{i}"])
                    mk.op("dve", "tensor_tensor", out=gi[i][:, 0:tw], in0=gi[i][:, 0:tw], in1=r[i][:, 0:tw], op=ALU.mult,
                          R=[f"gi{i}", f"r{i}"], W=[f"gi{i}"])
                    mk.op("pool", "tensor_tensor", out=bt[:, t0:t0 + tw], in0=gi[i][:, 0:tw], in1=u[:, t0:t0 + tw],
                          op=ALU.mult, R=[f"gi{i}", "u"], W=["bt"])
                if d == 0:
                    mk.op("dve", "tensor_tensor_scan", out=hf[:, :], data0=aa[:, :], data1=bt[:, :], initial=0.0,
                          op0=ALU.mult, op1=ALU.add, R=["aa", "bt"], W=["hf"])
                else:
                    mk.op("dve", "tensor_tensor_scan", out=hb[:, 0:256][:, ::-1], data0=aa[:, 0:256][:, ::-1],
                          data1=bt[:, 0:256][:, ::-1], initial=0.0, op0=ALU.mult, op1=ALU.add, R=["aa", "bt"], W=["hb"])
                    mk.op("dve", "tensor_tensor_scan", out=hb[:, 256:T][:, ::-1], data0=aa[:, 256:T][:, ::-1],
                          data1=bt[:, 256:T][:, ::-1], initial=hb[:, 0:1], op0=ALU.mult, op1=ALU.add,
                          R=["aa", "bt", "hb"], W=["hb"])
            mk.op("act", "activation", out=ag[:], in_=ag[:], func=AF.Gelu, R=["ag"], W=["ag"])
            mk.op("dve", "tensor_tensor", out=hf[:], in0=hf[:], in1=hb[:], op=ALU.add, R=["hf", "hb"], W=["hf"])
            mk.op("dve", "tensor_tensor", out=yb[:], in0=hf[:], in1=ag[:], op=ALU.mult, R=["hf", "ag"], W=["yb"])
            mk.dma("pool", YT[0, c], yb[:], R=["yb"], W=[f"YT0{c}"])
        mk.barrier()
        P.close()

    def order_of(dr):
        return list(range(NT)) if dr == 0 else [1, 0] + list(range(NT - 1, 1, -1))

    def run_pipelined(gens, pipelined=True):
        if not pipelined:
            for g in gens:
                for _ in g:
                    pass
            return
        prev = None
        for g in gens:
            next(g, None)
            if prev is not None:
                for _ in prev:
                    pass
            prev = g
        if prev is not None:
            for _ in prev:
                pass

    OUT_T = list(range(2, 2 + HALF_OUT // 128))

    def plan(dr, last):
        if not last:
            return [(t, True) for t in order_of(dr)]
        if dr == 0:
            return [(0, False), (1, False)] + [(t, True) for t in OUT_T]
        return [(t, (t in OUT_T)) for t in order_of(dr)]

    def chunk_core(it, nsub, dr, QT, KT, QIT, KHs, V, vw, Gc, S, Sbf, maskD, maskkey, rkeys, PT, sk, full=True):
        pi = it % 2
        L = 128 // nsub
        okeys = ["ps2", "ps3"]
        Oh = [PS[2 + hh][:, 0:2 * vw].rearrange("p (c e) -> p c e", c=2) for hh in range(2)]
        if not DBG["core"]:
            return okeys, Oh
        for h in (DBG["heads"] if full else ()):
            c, hh = h // 2, h % 2
            rs = slice(hh * 64, (hh + 1) * 64)
            mk.op("pe", "matmul", PS[hh][:, c * 128:(c + 1) * 128], lhsT=KT[c][rs, :], rhs=QT[c][rs, :], start=True,
                  stop=True, R=rkeys, W=[f"ps{hh}"])
        PTv = PT[pi][:].rearrange("p (c x n) -> p c x n", c=2, x=2)
        Mv = maskD.rearrange("p (c x n) -> p c x n", c=2, x=2) if full else None
        for hh in (range(2) if full else ()):
            mk.op("dve", "tensor_tensor", out=PTv[:, :, hh, :], in0=PS[hh][:, 0:256].rearrange("p (c n) -> p c n", c=2),
                  in1=Mv[:, :, hh, :], op=ALU.mult, R=[f"ps{hh}", maskkey], W=[f"PT{pi}h{hh}"])
        ptk = [f"PT{pi}h0", f"PT{pi}h1"]
        subs = list(range(nsub)) if dr == 0 else list(range(nsub - 1, -1, -1))
        KVp = PS[4][:, 0:2 * vw].rearrange("p (c e) -> p c e", c=2)
        for s in subs:
            rows = slice(s * L, (s + 1) * L)
            for h in (DBG["heads"] if full else ()):
                c, hh = h // 2, h % 2
                rs = slice(hh * 64, (hh + 1) * 64)
                mk.op("pe", "matmul", Oh[hh][rows, c, :], lhsT=PT[pi][:, h * 128 + s * L:h * 128 + (s + 1) * L],
                      rhs=V[:, h, :], start=True, stop=False, R=[ptk[hh]] + rkeys, W=[okeys[hh]])
                mk.op("pe", "matmul", Oh[hh][rows, c, :], lhsT=QIT[c][rs, rows], rhs=Sbf[c][rs, :], start=False, stop=True,
                      R=rkeys + [f"{sk}Sbf{c}"], W=[okeys[hh]])
            for h in DBG["heads"]:
                c, hh = h // 2, h % 2
                mk.op("pe", "matmul", KVp[hh * 64:(hh + 1) * 64, c, :], lhsT=KHs(s)[:, h * 64:(h + 1) * 64],
                      rhs=V[:, h, :], start=True, stop=True, R=rkeys, W=["ps4kv"])
            for c in range(2):
                mk.op("dve", "scalar_tensor_tensor", out=S[c][:], in0=S[c][:], scalar=Gc[:, c, s:s + 1], in1=KVp[:, c, :],
                      op0=ALU.mult, op1=ALU.add, R=[f"{sk}S{c}", "ps4kv"] + rkeys, W=[f"{sk}S{c}"])
                mk.op("act", "activation", out=Sbf[c][:], in_=S[c][:], func=AF.Copy, R=[f"{sk}S{c}"], W=[f"{sk}Sbf{c}"])
        return okeys, Oh

    def hview(ap256, hh):
        return ap256.rearrange("p (c x e) -> p c x e", c=2, x=2)[:, :, hh, :]

    def make_finalize(P, n, yTb):
        st = dict(i=0)
        cent = [P.sb([128, 4, 64], F32, "cent") for _ in range(2)]
        sq = P.sb([128, 4, 64], F32, "sq")
        mm = [P.sb([128, 4], F32, "mm") for _ in range(2)]
        vv = [P.sb([128, 4], F32, "vv") for _ in range(2)]
        yy = [P.sb([128, 256], BF16, "yy") for _ in range(2)]

        def fin(tot, totkey, center, gate, gatekey, t):
            i = st["i"] % 2
            st["i"] += 1
            tv = tot.rearrange("p (h e) -> p h e", h=4)
            tk = list(totkey) if isinstance(totkey, (list, tuple)) else [totkey]
            src, skeys = tv, tk
            if center:
                mk.op("dve", "tensor_reduce", out=mm[i][:], in_=tv, axis=AX.X, op=ALU.add, R=tk, W=[f"fmm{i}"])
                mk.op("dve", "tensor_scalar", out=mm[i][:], in0=mm[i][:], scalar1=-1.0 / 64, scalar2=None, op0=ALU.mult,
                      R=[f"fmm{i}"], W=[f"fmm{i}"])
                mk.op("dve", "tensor_tensor", out=cent[i][:], in0=tv, in1=mm[i][:].unsqueeze(2).to_broadcast([128, 4, 64]),
                      op=ALU.add, R=tk + [f"fmm{i}"], W=[f"fcent{i}"])
                src, skeys = cent[i][:], [f"fcent{i}"]
            mk.op("pool", "tensor_tensor", out=sq[:], in0=src, in1=src, op=ALU.mult, R=skeys, W=["fsq"])
            mk.op("dve", "tensor_reduce", out=vv[i][:], in_=sq[:], axis=AX.X, op=ALU.add, R=["fsq"], W=[f"fvv{i}"])
            mk.op("act", "activation", out=vv[i][:], in_=vv[i][:], func=AF.Sqrt, scale=1.0 / 64, bias=EPS,
                  R=[f"fvv{i}"], W=[f"fvv{i}"])
            mk.op("dve", "reciprocal", out=vv[i][:], in_=vv[i][:], R=[f"fvv{i}"], W=[f"fvv{i}"])
            mk.op("dve", "tensor_tensor", out=cent[i][:], in0=src, in1=vv[i][:].unsqueeze(2).to_broadcast([128, 4, 64]),
                  op=ALU.mult, R=skeys + [f"fvv{i}"], W=[f"fcent{i}"])
            mk.op("dve", "tensor_tensor", out=yy[i][:], in0=cent[i][:].rearrange("p h e -> p (h e)"), in1=gate,
                  op=ALU.mult, R=[f"fcent{i}", gatekey], W=[f"fyy{i}"])
            for c in range(2):
                mk.op("pe", "transpose", out=PQ[1][:, (i * 2 + c) * 128:(i * 2 + c + 1) * 128],
                      in_=yy[i][:, c * 128:(c + 1) * 128], identity=identb[:], R=[f"fyy{i}", "identb"], W=[f"pq1f{i}"])
            mk.op("act", "activation", out=yTb[:, :, t * 128:(t + 1) * 128],
                  in_=PQ[1][:, i * 256:(i + 1) * 256].rearrange("p (c n) -> p c n", c=2), func=AF.Copy,
                  R=[f"pq1f{i}"], W=["yTb"])
        return fin

    def mixer_b(l, last=False):
        P = Pool(nc, f"mb{l}")
        QR = [P.sb([128, T], BF16, "QR") for _ in range(2)]
        KR = [P.sb([128, T], BF16, "KR") for _ in range(2)]
        thp = P.sb([128, 2, 2], F32, "thp")
        thh = P.sb([128, 2, 4], F32, "thh")
        mk.dma("sp", thp[:], b_thp[l], W=["thp"])
        mk.dma("sp", thh[:], b_thh[l], W=["thh"])
        for tt, key in ((thp, "thp"), (thh, "thh")):
            mk.op("act", "activation", out=tt[:], in_=tt[:], func=AF.Exp, scale=-1.0, R=[key], W=[key])
            mk.op("act", "activation", out=tt[:], in_=tt[:], func=AF.Ln, bias=1.0, R=[key], W=[key])
            mk.op("dve", "tensor_scalar", out=tt[:], in0=tt[:], scalar1=-1.0, scalar2=None, op0=ALU.mult, R=[key], W=[key])
        DM = P.sb([128, 2, 512], F32, "DM")
        QW = P.sb([128, 2, 2, 128], F32, "QW")
        KW = P.sb([128, 2, 4], F32, "KW")
        Gc = P.sb([128, 2, 2, 1], F32, "Gc")
        for dr in range(2):
            diff, msk, pos = (C("diffF"), C("maskF"), C("posF")) if dr == 0 else (C("diffB"), C("maskB"), C("posB"))
            for h in range(4):
                mk.op("act", "activation", out=DM[:, dr, h * 128:(h + 1) * 128], in_=diff, func=AF.Exp,
                      scale=thh[:, dr, h:h + 1], R=["cst", "thh"], W=["DM"])
                mk.op("dve", "tensor_tensor", out=DM[:, dr, h * 128:(h + 1) * 128], in0=DM[:, dr, h * 128:(h + 1) * 128],
                      in1=msk, op=ALU.mult, R=["DM", "cst"], W=["DM"])
                mk.op("act", "activation", out=KW[:, dr, h:h + 1], in_=C("kpos")[:, dr:dr + 1], func=AF.Exp,
                      scale=thh[:, dr, h:h + 1], R=["cst", "thh"], W=["KW"])
            for c in range(2):
                mk.op("act", "activation", out=QW[:, dr, c, :], in_=pos, func=AF.Exp, scale=thp[:, dr, c:c + 1],
                      R=["cst", "thp"], W=["QW"])
                mk.op("act", "activation", out=Gc[:, dr, c, :], in_=thp[:, dr, c:c + 1], func=AF.Exp, scale=128.0,
                      R=["thp"], W=["Gc"])
        segw = 1088
        f1 = [P.sb([128, segw], F32, "f1") for _ in range(2)]
        f2 = [P.sb([128, segw], F32, "f2") for _ in range(2)]
        rc = P.sb([128, T], F32, "rc")
        rsn = P.sb([128, T], F32, "rsn")
        mk.dma("sp", rc[:], ropeC_d, W=["rc"])
        mk.dma("sp", rsn[:], ropeS_d, W=["rsn"])
        n = 0
        for (dst, base, pbase, scale) in ((QR, "b_q", "b_qp", 1.0), (KR, "b_k", "b_kp", 0.125)):
            for c in range(2):
                for sg in range(4):
                    i = n % 2
                    n += 1
                    cs = slice(sg * segw, (sg + 1) * segw)
                    mk.dma("sp", f1[i][:], FM[FM_OFF[base] + c][:, cs], R=[f"FM{FM_OFF[base] + c}"], W=[f"f1{i}"])
                    mk.dma("sp", f2[i][:], FM[FM_OFF[pbase] + c][:, cs], R=[f"FM{FM_OFF[pbase] + c}"], W=[f"f2{i}"])
                    mk.op("dve", "tensor_tensor", out=f1[i][:], in0=f1[i][:], in1=rc[:, cs], op=ALU.mult,
                          R=[f"f1{i}", "rc"], W=[f"f1{i}"])
                    mk.op("pool", "tensor_tensor", out=f2[i][:], in0=f2[i][:], in1=rsn[:, cs], op=ALU.mult,
                          R=[f"f2{i}", "rsn"], W=[f"f2{i}"])
                    mk.op("dve", "tensor_tensor", out=f1[i][:], in0=f1[i][:], in1=f2[i][:], op=ALU.add,
                          R=[f"f1{i}", f"f2{i}"], W=[f"f1{i}"])
                    mk.op("act", "activation", out=dst[c][:, cs], in_=f1[i][:], func=AF.Copy, scale=scale,
                          R=[f"f1{i}"], W=[f"b{base}{c}"])
        rkeys = ["bb_q0", "bb_q1", "bb_k0", "bb_k1"]
        OF = P.sb([128, NT, 256], F32, "OF")
        yTb = P.sb([128, 2, T], BF16, "yTb")
        fin = make_finalize(P, 1, yTb)
        PT = [P.sb([128, 512], BF16, "PT") for _ in range(2)]
        QIT = [[P.sb([128, 128], BF16, "QIT") for _ in range(2)] for _ in range(2)]
        KH = [P.sb([128, 256], BF16, "KH") for _ in range(2)]
        Vf = [P.sb([128, 512], F32, "Vf") for _ in range(2)]
        Vb = [P.sb([128, 4, 64], BF16, "Vb") for _ in range(2)]
        gt = [P.sb([128, 256], F32, "gt") for _ in range(2)]
        tot = [P.sb([128, 256], F32, "tot") for _ in range(2)]
        S = [P.sb([128, 64], F32, "S") for _ in range(2)]
        Sbf = [P.sb([128, 64], BF16, "Sbf") for _ in range(2)]
        it = 0
        for dr in range(2):
            for c in range(2):
                mk.op("pool", "memset", S[c][:], 0.0, W=[f"bS{c}"])
                mk.op("pool", "memset", Sbf[c][:], 0.0, W=[f"bSbf{c}"])
            def body(t, full, it):
                i = it % 2
                cols = slice(t * 128, (t + 1) * 128)
                mk.dma("sp", Vf[i][:], TM[t * 128:(t + 1) * 128, 0:512], R=["TM"], W=[f"bVf{i}"])
                mk.op("pool", "tensor_copy", out=Vb[i][:], in_=Vf[i][:, 0:256].rearrange("p (h e) -> p h e", h=4),
                      R=[f"bVf{i}"], W=[f"bVb{i}"])
                for c in range(2):
                    if full:
                        mk.op("pool", "tensor_tensor", out=QIT[i][c][:], in0=QR[c][:, cols], in1=QW[:, dr, c, :],
                              op=ALU.mult, R=[f"bb_q{c}", "QW"], W=[f"bQIT{i}"])
                    mk.op("pe", "transpose", out=PQ[0][:, (i * 2 + c) * 128:(i * 2 + c + 1) * 128], in_=KR[c][:, cols],
                          identity=identb[:], R=[f"bb_k{c}", "identb"], W=[f"pq0k{i}"])
                for h in range(4):
                    mk.op("act", "activation", out=KH[i][:, h * 64:(h + 1) * 64],
                          in_=PQ[0][:, i * 256 + h * 64:i * 256 + (h + 1) * 64], func=AF.Identity, scale=KW[:, dr, h:h + 1],
                          R=[f"pq0k{i}", "KW"], W=[f"bKH{i}"])
                yield
                okeys, Oh = chunk_core(it, 1, dr, [QR[0][:, cols], QR[1][:, cols]], [KR[0][:, cols], KR[1][:, cols]],
                                       [QIT[i][0], QIT[i][1]], (lambda s_, kh=KH[i]: kh), Vb[i], 64, Gc[:, dr], S, Sbf,
                                       DM[:, dr, :], "DM", rkeys + [f"bQIT{i}", f"bKH{i}", f"bVb{i}"], PT, "b", full=full)
                if not full:
                    pass
                elif dr == 0:
                    for hh in range(2):
                        mk.op("act", "activation", out=hview(OF[:, t, :], hh), in_=Oh[hh], func=AF.Copy, R=[okeys[hh]],
                              W=[f"bOF{t}h{hh}"])
                else:
                    for hh in range(2):
                        mk.op("dve", "tensor_tensor", out=hview(tot[i][:], hh), in0=Oh[hh], in1=hview(OF[:, t, :], hh),
                              op=ALU.add, R=[okeys[hh], f"bOF{t}h{hh}"], W=[f"btot{i}h{hh}"])
                    mk.op("act", "activation", out=gt[i][:], in_=Vf[i][:, 256:512], func=AF.Silu, R=[f"bVf{i}"],
                          W=[f"bgt{i}"])
                    fin(tot[i][:], [f"btot{i}h0", f"btot{i}h1"], True, gt[i][:], f"bgt{i}", t)
            gens = []
            for t, full in plan(dr, last):
                gens.append(body(t, full, it))
                it += 1
            run_pipelined(gens, pipelined=not last)
        for c in range(2):
            mk.dma("pool", YT[1, c], yTb[:, c, :], R=["yTb"], W=[f"YT1{c}"])
        mk.barrier()
        P.close()

    def mixer_c(l, last=False):
        P = Pool(nc, f"mc{l}")
        QC = [P.sb([128, T], BF16, "QC") for _ in range(2)]
        KC = [P.sb([128, T], BF16, "KC") for _ in range(2)]
        cw = P.sb([128, 4, 5], F32, "cw")
        cb = P.sb([128, 4], F32, "cb")
        gbias = P.sb([128, 16], F32, "gbias")
        mk.dma("sp", cw[:], c_cw[l], W=["c_cw"])
        mk.dma("sp", cb[:], c_cb[l], W=["c_cb"])
        mk.dma("sp", gbias[:], c_gb[l], W=["c_gb"])
        src = P.sb([128, T], F32, "src")
        u = P.sb([128, T], F32, "u")
        for ch in range(4):
            fc = FM_OFF["c_q"] + ch
            mk.dma("sp", src[:], FM[fc], R=[f"FM{fc}"], W=["csrc"])
            conv_fm(u, src, cw[:, ch, :], cb[:, ch:ch + 1], "cu", "csrc", ["c_cw", "c_cb"])
            dst = QC[ch] if ch < 2 else KC[ch - 2]
            mk.op("act", "activation", out=u[:], in_=u[:], func=AF.Silu, R=["cu"], W=["cu"])
            mk.op("dve", "tensor_scalar", out=dst[:], in0=u[:], scalar1=(1.0 if ch < 2 else 0.125), scalar2=None,
                  op0=ALU.mult, R=["cu"], W=[f"cqk{ch}"])
        Z = P.sb([128, NT, 16], F32, "Z")
        LFN = P.sb([128, NT, 16], F32, "LFN")
        mk.dma("sp", Z[:], TM[:, TM_OFF["c_gates"]:TM_OFF["c_gates"] + 16].rearrange("(t p) g -> p t g", p=128),
               R=["TM"], W=["cZ"])
        mk.op("dve", "tensor_tensor", out=Z[:], in0=Z[:], in1=gbias[:].unsqueeze(1).to_broadcast([128, NT, 16]),
              op=ALU.add, R=["cZ", "c_gb"], W=["cZ"])
        mk.op("act", "activation", out=LFN[:], in_=Z[:], func=AF.Exp, scale=-1.0, R=["cZ"], W=["cLFN"])
        mk.op("act", "activation", out=LFN[:], in_=LFN[:], func=AF.Ln, bias=1.0, R=["cLFN"], W=["cLFN"])
        mk.op("dve", "tensor_scalar", out=LFN[:], in0=LFN[:], scalar1=-1.0, scalar2=None, op0=ALU.mult, R=["cLFN"],
              W=["cLFN"])
        rkeys = ["cqk0", "cqk1", "cqk2", "cqk3"]
        OF = P.sb([128, NT, 256], F32, "OF")
        yTb = P.sb([128, 2, T], BF16, "yTb")
        fin = make_finalize(P, 2, yTb)
        PT = [P.sb([128, 512], BF16, "PT") for _ in range(2)]
        QIT = [[P.sb([128, 128], BF16, "QIT") for _ in range(2)] for _ in range(2)]
        KH = [P.sb([128, 256], BF16, "KH") for _ in range(2)]
        Vf = [P.sb([128, 512], F32, "Vf") for _ in range(2)]
        Vb = [P.sb([128, 4, 65], BF16, "Vb") for _ in range(2)]
        gt = [P.sb([128, 256], F32, "gt") for _ in range(2)]
        tot = [P.sb([128, 256], F32, "tot") for _ in range(2)]
        S = [P.sb([128, 65], F32, "S") for _ in range(2)]
        Sbf = [P.sb([128, 65], BF16, "Sbf") for _ in range(2)]
        Bm4 = [P.sb([128, 4, 128], F32, "Bm4") for _ in range(2)]
        tmp4 = [P.sb([128, 512], F32, "tmp4") for _ in range(2)]
        Dm4 = [P.sb([128, 512], F32, "Dm4") for _ in range(2)]
        EB4 = [P.sb([128, 512], F32, "EB4") for _ in range(2)]
        lmb = [P.sb([128, 4], F32, "lmb") for _ in range(2)]
        kw = [P.sb([128, 4], F32, "kw") for _ in range(2)]
        Gc = [P.sb([128, 2, 1], F32, "Gc") for _ in range(2)]
        rden = [P.sb([128, 4], F32, "rden") for _ in range(2)]
        ebe = [P.sb([128, 4], F32, "ebe") for _ in range(2)]
        hid = [P.sb([128, 4, 64], F32, "hid") for _ in range(2)]
        for i in range(2):
            mk.op("pool", "memset", Vb[i][:], 1.0, W=[f"cVb{i}"])
        ones = C("ones")
        it = 0
        for dr in range(2):
            tri = C("triF") if dr == 0 else C("triB")
            neg4 = C("negF4") if dr == 0 else C("negB4")
            e = 127 if dr == 0 else 0
            for c in range(2):
                mk.op("pool", "memset", S[c][:], 0.0, W=[f"cS{c}"])
                mk.op("pool", "memset", Sbf[c][:], 0.0, W=[f"cSbf{c}"])
            def body(t, full, it):
                i = it % 2
                cols = slice(t * 128, (t + 1) * 128)
                li = Z[:, t, dr * 8:dr * 8 + 4]
                lf = LFN[:, t, dr * 8 + 4:dr * 8 + 8]
                mk.dma("sp", Vf[i][:], TM[t * 128:(t + 1) * 128, 512:1024], R=["TM"], W=[f"cVf{i}"])
                mk.op("pool", "tensor_copy", out=Vb[i][:, :, 0:64], in_=Vf[i][:, 0:256].rearrange("p (h e) -> p h e", h=4),
                      R=[f"cVf{i}"], W=[f"cVb{i}"])
                mk.op("dve", "tensor_tensor", out=Bm4[i][:], in0=tri.unsqueeze(1).to_broadcast([128, 4, 128]),
                      in1=lf.unsqueeze(2).to_broadcast([128, 4, 128]), op=ALU.mult, R=["cst", "cLFN"], W=[f"cBm{i}"])
                mk.op("pe", "matmul", PS[5][:, :], lhsT=ones, rhs=Bm4[i][:].rearrange("p h n -> p (h n)"), start=True,
                      stop=True, R=["cst", f"cBm{i}"], W=["ps5"])
                mk.op("pe", "matmul", PS[4][:, 256:260], lhsT=tri, rhs=lf, start=True, stop=True, R=["cst", "cLFN"],
                      W=["ps4b"])
                mk.op("dve", "tensor_tensor", out=lmb[i][:], in0=li, in1=PS[4][:, 256:260], op=ALU.subtract,
                      R=["cZ", "ps4b"], W=[f"clmb{i}"])
                if full:
                    mk.op("dve", "tensor_tensor", out=tmp4[i][:], in0=PS[5][:, :], in1=neg4, op=ALU.add, R=["ps5", "cst"],
                          W=[f"ctmp{i}"])
                    for h in range(4):
                        mk.op("act", "activation", out=Dm4[i][:, h * 128:(h + 1) * 128],
                              in_=tmp4[i][:, h * 128:(h + 1) * 128], func=AF.Exp, bias=lmb[i][:, h:h + 1],
                              R=[f"ctmp{i}", f"clmb{i}"], W=[f"cDm{i}"])
                    mk.op("act", "activation", out=EB4[i][:], in_=PS[5][:, :], func=AF.Exp, R=["ps5"], W=[f"cEB{i}"])
                bend = PS[5][:, :].rearrange("p (h n) -> p h n", h=4)[:, :, e]
                mk.op("dve", "tensor_tensor", out=kw[i][:], in0=lmb[i][:], in1=bend, op=ALU.add, R=[f"clmb{i}", "ps5"],
                      W=[f"ckw{i}"])
                mk.op("act", "activation", out=kw[i][:], in_=kw[i][:], func=AF.Exp, R=[f"ckw{i}"], W=[f"ckw{i}"])
                mk.op("act", "activation", out=ebe[i][:], in_=bend, func=AF.Exp, R=["ps5"], W=[f"cebe{i}"])
                for h in range(4):
                    c, hh = h // 2, h % 2
                    rs = slice(hh * 64, (hh + 1) * 64)
                    mk.op("pool", "tensor_copy", out=Gc[i][rs, c, :], in_=ebe[i][rs, h:h + 1],
                          R=[f"cebe{i}"], W=[f"cGc{i}"])
                    if full:
                        mk.op("pool", "tensor_tensor", out=QIT[i][c][rs, :], in0=QC[c][rs, cols],
                              in1=EB4[i][rs, h * 128:(h + 1) * 128], op=ALU.mult, R=[f"cqk{c}", f"cEB{i}"],
                              W=[f"cQIT{i}"])
                for c in range(2):
                    mk.op("pe", "transpose", out=PQ[0][:, (i * 2 + c) * 128:(i * 2 + c + 1) * 128], in_=KC[c][:, cols],
                          identity=identb[:], R=[f"cqk{2 + c}", "identb"], W=[f"pq0k{i}"])
                for h in range(4):
                    mk.op("act", "activation", out=KH[i][:, h * 64:(h + 1) * 64],
                          in_=PQ[0][:, i * 256 + h * 64:i * 256 + (h + 1) * 64], func=AF.Identity, scale=kw[i][:, h:h + 1],
                          R=[f"pq0k{i}", f"ckw{i}"], W=[f"cKH{i}"])
                yield
                okeys, Oh = chunk_core(it, 1, dr, [QC[0][:, cols], QC[1][:, cols]], [KC[0][:, cols], KC[1][:, cols]],
                                       [QIT[i][0], QIT[i][1]], (lambda s_, kh=KH[i]: kh), Vb[i], 65, Gc[i], S, Sbf,
                                       Dm4[i][:], f"cDm{i}",
                                       rkeys + [f"cQIT{i}", f"cKH{i}", f"cVb{i}", f"cGc{i}"], PT, "c", full=full)
                if not full:
                    return
                rdv = rden[i][:].rearrange("p (c x) -> p c x", c=2)
                for hh in range(2):
                    mk.op("act", "activation", out=rdv[:, :, hh], in_=Oh[hh][:, :, 64], func=AF.Abs, R=[okeys[hh]],
                          W=[f"crden{i}"])
                mk.op("dve", "tensor_scalar_max", out=rden[i][:], in0=rden[i][:], scalar1=1.0, R=[f"crden{i}"],
                      W=[f"crden{i}"])
                mk.op("dve", "reciprocal", out=rden[i][:], in_=rden[i][:], R=[f"crden{i}"], W=[f"crden{i}"])
                if dr == 0:
                    for hh in range(2):
                        mk.op("dve", "tensor_tensor", out=hview(OF[:, t, :], hh), in0=Oh[hh][:, :, 0:64],
                              in1=rdv[:, :, hh:hh + 1].to_broadcast([128, 2, 64]), op=ALU.mult,
                              R=[okeys[hh], f"crden{i}"], W=[f"cOF{t}h{hh}"])
                else:
                    for hh in range(2):
                        mk.op("dve", "tensor_tensor", out=hview(hid[i][:].rearrange("p h e -> p (h e)"), hh),
                              in0=Oh[hh][:, :, 0:64], in1=rdv[:, :, hh:hh + 1].to_broadcast([128, 2, 64]), op=ALU.mult,
                              R=[okeys[hh], f"crden{i}"], W=[f"chid{i}h{hh}"])
                    mk.op("pool", "tensor_tensor", out=tot[i][:], in0=hid[i][:].rearrange("p h e -> p (h e)"),
                          in1=OF[:, t, :], op=ALU.add, R=[f"chid{i}h0", f"chid{i}h1", f"cOF{t}h0", f"cOF{t}h1"],
                          W=[f"ctot{i}"])
                    mk.op("act", "activation", out=gt[i][:], in_=Vf[i][:, 256:512], func=AF.Sigmoid, R=[f"cVf{i}"],
                          W=[f"cgt{i}"])
                    fin(tot[i][:], f"ctot{i}", True, gt[i][:], f"cgt{i}", t)
            gens = []
            for t, full in plan(dr, last):
                gens.append(body(t, full, it))
                it += 1
            run_pipelined(gens, pipelined=not last)
        for c in range(2):
            mk.dma("pool", YT[2, c], yTb[:, c, :], R=["yTb"], W=[f"YT2{c}"])
        mk.barrier()
        P.close()

    def mixer_d(l, last=False):
        P = Pool(nc, f"md{l}")
        LB = P.sb([128, 256], F32, "LB")
        OML = P.sb([128, 256], F32, "OML")
        if l == 0:
            use_lb = False
        else:
            use_lb = True
            dl = P.sb([128, 2, 256], F32, "dl")
            mk.dma("sp", dl[:], d_lbr, W=["dl"])
            mk.op("dve", "tensor_tensor", out=LB[:], in0=dl[:, 1, :], in1=dl[:, 0, :], op=ALU.subtract, R=["dl"], W=["LB"])
            mk.op("act", "activation", out=LB[:], in_=LB[:], func=AF.Sigmoid, R=["LB"], W=["LB"])
            mk.op("dve", "tensor_scalar", out=OML[:], in0=LB[:], scalar1=-1.0, scalar2=1.0, op0=ALU.mult, op1=ALU.add,
                  R=["LB"], W=["OML"])
        OF = P.sb([128, NT, 256], F32, "OF")
        yTb = P.sb([128, 2, T], BF16, "yTb")
        fin = make_finalize(P, 3, yTb)
        assert DNS == 4
        PT = [P.sb([128, 512], BF16, "PT") for _ in range(2)]
        X = [P.sb([128, 1280], F32, "X") for _ in range(2)]
        ff = [P.sb([128, 256], F32, "ff") for _ in range(2)]
        lf = [P.sb([128, 256], F32, "lf") for _ in range(2)]
        kk = [P.sb([128, 256], F32, "kk") for _ in range(2)]
        qs = [P.sb([128, 256], F32, "qs") for _ in range(2)]
        ee = [P.sb([128, 512], F32, "ee") for _ in range(2)]
        ek = [P.sb([128, 256], F32, "ek") for _ in range(2)]
        qk = [P.sb([128, 512], BF16, "qk") for _ in range(2)]
        KTs = [P.sb([128, 2, 128], BF16, "KTs") for _ in range(2)]
        QM = [[P.sb([128, 2, 5, 128], BF16, "QM") for _ in range(2)] for _ in range(2)]
        KH = [P.sb([128, DNS, 256], BF16, "KH") for _ in range(2)]
        Vb = [P.sb([128, 4, 64], BF16, "Vb") for _ in range(2)]
        gt = [P.sb([128, 256], F32, "gt") for _ in range(2)]
        tot = [P.sb([128, 256], F32, "tot") for _ in range(2)]
        red = [P.sb([128, 2, 256], F32, "red") for _ in range(2)]
        Gc = [P.sb([128, 2, DNS], F32, "Gc") for _ in range(2)]
        S = [P.sb([128, 64], F32, "S") for _ in range(2)]
        Sbf = [P.sb([128, 64], BF16, "Sbf") for _ in range(2)]
        qmask = C("qmask").rearrange("p (x s n) -> p x s n", x=2, s=5)
        it = 0
        for dr in range(2):
            blk = C("blkF") if dr == 0 else C("blkB")
            rem = C("aftF") if dr == 0 else C("befB")
            msk4 = C("mblkF4") if dr == 0 else C("mblkB4")
            zoff = 256 if dr == 0 else 512
            subs = list(range(DNS)) if dr == 0 else list(range(DNS - 1, -1, -1))
            for c in range(2):
                mk.op("pool", "memset", S[c][:], 0.0, W=[f"dS{c}"])
                mk.op("pool", "memset", Sbf[c][:], 0.0, W=[f"dSbf{c}"])
            def body(t, full, it):
                i = it % 2
                mk.dma("sp", X[i][:], TM[t * 128:(t + 1) * 128, 1024:2304], R=["TM"], W=[f"dX{i}"])
                mk.op("act", "activation", out=ff[i][:], in_=X[i][:, zoff:zoff + 256], func=AF.Sigmoid, R=[f"dX{i}"],
                      W=[f"dff{i}"])
                if use_lb:
                    mk.op("dve", "tensor_tensor", out=ff[i][:], in0=ff[i][:], in1=OML[:], op=ALU.mult, R=[f"dff{i}", "OML"],
                          W=[f"dff{i}"])
                    mk.op("dve", "tensor_tensor", out=ff[i][:], in0=ff[i][:], in1=LB[:], op=ALU.add, R=[f"dff{i}", "LB"],
                          W=[f"dff{i}"])
                mk.op("act", "activation", out=lf[i][:], in_=ff[i][:], func=AF.Ln, R=[f"dff{i}"], W=[f"dlf{i}"])
                mk.op("pool", "tensor_scalar", out=kk[i][:], in0=ff[i][:], scalar1=-1.0, scalar2=1.0, op0=ALU.mult,
                      op1=ALU.add, R=[f"dff{i}"], W=[f"dkk{i}"])
                if full:
                    mk.op("pe", "matmul", PS[5][:, 0:256], lhsT=blk, rhs=lf[i][:], start=True, stop=True,
                          R=["cst", f"dlf{i}"], W=["ps5"])
                mk.op("pe", "matmul", PS[5][:, 256:512], lhsT=rem, rhs=lf[i][:], start=True, stop=True,
                      R=["cst", f"dlf{i}"], W=["ps5"])
                for c in range(2):
                    mk.op("pe", "matmul", PS[1][:, 384 + c * DNS:384 + (c + 1) * DNS], lhsT=lf[i][:, c * 128:(c + 1) * 128],
                          rhs=C("subm"), start=True, stop=True, R=["cst", f"dlf{i}"], W=["ps1g"])
                mk.op("act", "activation", out=Gc[i][:].rearrange("p c s -> p (c s)"), in_=PS[1][:, 384:384 + 2 * DNS],
                      func=AF.Exp, R=["ps1g"], W=[f"dGc{i}"])
                if full:
                    mk.op("act", "activation", out=ee[i][:], in_=PS[5][:, :], func=AF.Exp, R=["ps5"], W=[f"dee{i}"])
                    mk.op("act", "activation", out=ek[i][:], in_=PS[5][:, 0:256], func=AF.Exp, scale=-1.0, R=["ps5"],
                          W=[f"dek{i}"])
                    mk.op("act", "activation", out=qs[i][:], in_=X[i][:, 0:256], func=AF.Silu, R=[f"dX{i}"],
                          W=[f"dqs{i}"])
                    mk.op("dve", "tensor_tensor", out=qk[i][:, 0:256], in0=qs[i][:], in1=ee[i][:, 0:256], op=ALU.mult,
                          R=[f"dqs{i}", f"dee{i}"], W=[f"dqk{i}a"])
                    mk.op("dve", "tensor_tensor", out=qk[i][:, 256:512], in0=kk[i][:], in1=ek[i][:], op=ALU.mult,
                          R=[f"dkk{i}", f"dek{i}"], W=[f"dqk{i}c"])
                else:
                    mk.op("act", "activation", out=ee[i][:, 256:512], in_=PS[5][:, 256:512], func=AF.Exp, R=["ps5"],
                          W=[f"dee{i}"])
                for s_ in range(DNS):
                    mk.op("dve", "scalar_tensor_tensor", out=KH[i][:, s_, :], in0=kk[i][:], scalar=C("subm")[:, s_:s_ + 1],
                          in1=ee[i][:, 256:512], op0=ALU.mult, op1=ALU.mult, R=[f"dkk{i}", f"dee{i}", "cst"],
                          W=[f"dKH{i}s{s_}"])
                mk.op("pool", "tensor_copy", out=Vb[i][:], in_=X[i][:, 768:1024].rearrange("p (h e) -> p h e", h=4),
                      R=[f"dX{i}"], W=[f"dVb{i}"])
                if full:
                    for j in range(4):
                        mk.op("pe", "transpose", out=PQ[0][:, (i * 4 + j) * 128:(i * 4 + j + 1) * 128],
                              in_=qk[i][:, j * 128:(j + 1) * 128], identity=identb[:], R=[f"dqk{i}a", f"dqk{i}c", "identb"],
                              W=[f"pq0d{i}"])
                    mk.op("act", "activation", out=KTs[i][:].rearrange("p c n -> p (c n)"),
                          in_=PQ[0][:, i * 512 + 256:i * 512 + 512], func=AF.Copy, R=[f"pq0d{i}"], W=[f"dKT{i}"])
                    for c in range(2):
                        mk.op("dve", "tensor_tensor", out=QM[i][c][:].rearrange("p x s n -> p (x s) n"),
                              in0=PQ[0][:, i * 512 + c * 128:i * 512 + (c + 1) * 128].unsqueeze(1).to_broadcast([128, 10, 128]),
                              in1=qmask.rearrange("p x s n -> p (x s) n"), op=ALU.mult, R=[f"pq0d{i}", "cst"],
                              W=[f"dQM{i}{c}"])
                yield
                if full:
                    for h in range(4):
                        c, hh = h // 2, h % 2
                        mk.op("pe", "matmul", PS[0][:, h * 128:(h + 1) * 128], lhsT=KTs[i][:, c, :], rhs=QM[i][c][:, hh, 4, :],
                              start=True, stop=True, R=[f"dKT{i}", f"dQM{i}{c}"], W=["ps0"])
                    mk.op("dve", "tensor_tensor", out=PT[i][:], in0=PS[0][:, :], in1=msk4, op=ALU.mult, R=["ps0", "cst"],
                          W=[f"dPT{i}"])
                    for h in range(4):
                        mk.op("pe", "matmul", PS[1][:, h * 64:(h + 1) * 64], lhsT=PT[i][:, h * 128:(h + 1) * 128],
                              rhs=Vb[i][:, h, :], start=True, stop=True, R=[f"dPT{i}", f"dVb{i}"], W=["ps1i"])
                KVp = PS[1][:, 256:384].rearrange("p (c e) -> p c e", c=2)
                for s_ in subs:
                    bank = PS[2 + s_ // 2]
                    for h in (range(4) if full else ()):
                        c, hh = h // 2, h % 2
                        col = ((s_ % 2) * 4 + h) * 64
                        mk.op("pe", "matmul", bank[:, col:col + 64], lhsT=QM[i][c][:, hh, s_, :], rhs=Sbf[c][:, :],
                              start=True, stop=True, R=[f"dQM{i}{c}", f"dSbf{c}"], W=[f"ps{2 + s_ // 2}"])
                    for h in range(4):
                        c, hh = h // 2, h % 2
                        mk.op("pe", "matmul", KVp[hh * 64:(hh + 1) * 64, c, :], lhsT=KH[i][:, s_, h * 64:(h + 1) * 64],
                              rhs=Vb[i][:, h, :], start=True, stop=True, R=[f"dKH{i}s{s_}", f"dVb{i}"], W=["ps1kv"])
                    for c in range(2):
                        mk.op("dve", "scalar_tensor_tensor", out=S[c][:], in0=S[c][:], scalar=Gc[i][:, c, s_:s_ + 1],
                              in1=KVp[:, c, :], op0=ALU.mult, op1=ALU.add, R=[f"dS{c}", "ps1kv", f"dGc{i}"], W=[f"dS{c}"])
                        mk.op("act", "activation", out=Sbf[c][:], in_=S[c][:], func=AF.Copy, R=[f"dS{c}"], W=[f"dSbf{c}"])
                if not full:
                    return
                for b_ in range(2):
                    mk.op("dve", "tensor_reduce", out=red[i][:, b_, :],
                          in_=PS[2 + b_][:, :].rearrange("p (s x) -> p x s", s=2), axis=AX.X, op=ALU.add,
                          R=[f"ps{2 + b_}"], W=[f"dred{i}{b_}"])
                mk.op("dve", "tensor_tensor", out=tot[i][:], in0=PS[1][:, 0:256], in1=red[i][:, 0, :], op=ALU.add,
                      R=["ps1i", f"dred{i}0"], W=[f"dtot{i}"])
                if dr == 0:
                    mk.op("pool", "tensor_tensor", out=OF[:, t, :], in0=tot[i][:], in1=red[i][:, 1, :], op=ALU.add,
                          R=[f"dtot{i}", f"dred{i}1"], W=[f"dOF{t}"])
                else:
                    mk.op("pool", "tensor_tensor", out=tot[i][:], in0=tot[i][:], in1=red[i][:, 1, :], op=ALU.add,
                          R=[f"dtot{i}", f"dred{i}1"], W=[f"dtot{i}"])
                    mk.op("dve", "tensor_tensor", out=tot[i][:], in0=tot[i][:], in1=OF[:, t, :], op=ALU.add,
                          R=[f"dtot{i}", f"dOF{t}"], W=[f"dtot{i}"])
                    mk.op("act", "activation", out=gt[i][:], in_=X[i][:, 1024:1280], func=AF.Silu, R=[f"dX{i}"],
                          W=[f"dgt{i}"])
                    fin(tot[i][:], f"dtot{i}", False, gt[i][:], f"dgt{i}", t)
            gens = []
            for t, full in plan(dr, last):
                gens.append(body(t, full, it))
                it += 1
            run_pipelined(gens, pipelined=not last)
        for c in range(2):
            mk.dma("pool", YT[3, c], yTb[:, c, :], R=["yTb"], W=[f"YT3{c}"])
        mk.barrier()
        P.close()

    W1B = dscr("W1B", [16, 128, 4096], BF16)
    W3B = dscr("W3B", [16, 128, 4096], BF16)
    W2B = dscr("W2B", [16, 128, 4096], BF16)

    def merge_moe_phase(l, last, tiles=None):
        if tiles is None:
            tiles = list(OUT_T) if last else list(range(NT))
        PO = Pool(nc, f"mo{l}")
        h2T = PO.sb([128, 8, T], BF16, "h2T")
        gates = PO.sb([128, NT, 16], F32, "gates")
        merge_part(l, tiles, h2T, gates)
        moe_part(l, tiles, h2T, gates)
        PO.close()

    def merge_part(l, tiles, h2T, gates):
        P = Pool(nc, f"mm{l}")
        wbr = P.sb([128, 8, D], BF16, "wbr")
        wo = P.sb([128, 8, D], BF16, "wo")
        wstage = P.sb([128, 8, 512], F32, "wstage")
        wcb = P.sb([128, 4096], BF16, "wcb")
        for half in range(2):
            mk.dma("sp", wstage[:], w_branch[l].rearrange("n (c p) f -> p (n c) f", p=128)[:, :, half * 512:(half + 1) * 512],
                   W=["wstage"])
            mk.ev(wbr[:, :, half * 512:(half + 1) * 512], wstage[:], R=["wstage"], W=["wbr"])
        for half in range(2):
            mk.dma("sp", wstage[:], w_out[l].rearrange("(c p) f -> p c f", p=128)[:, :, half * 512:(half + 1) * 512],
                   W=["wstage"])
            mk.ev(wo[:, :, half * 512:(half + 1) * 512], wstage[:], R=["wstage"], W=["wo"])
        tasks = []
        for e in range(16):
            tasks.append((moe_w1[l, e].rearrange("(c p) f -> p c f", p=128), wstage[:], W1B[e], f"W1B{e}"))
            tasks.append((moe_w3[l, e].rearrange("(c p) f -> p c f", p=128), wstage[:], W3B[e], f"W3B{e}"))
            tasks.append((moe_w2[l, e].rearrange("(c p) f -> p c f", p=128),
                          wstage[:].rearrange("p c f -> p (c f)").rearrange("p (c f) -> p c f", c=4), W2B[e], f"W2B{e}"))

        def do_task(k):
            src, stg, dst, key = tasks[k]
            mk.dma("sp", stg, src, W=["wstage"])
            mk.ev(wcb[:], wstage[:].rearrange("p c f -> p (c f)"), R=["wstage"], W=["wcb"])
            mk.dma("pool", dst, wcb[:], R=["wcb"], W=[key])
        wgr = P.sb([128, 8, 20], F32, "wgr")
        bgr = P.sb([1, 20], F32, "bgr")
        mk.dma("sp", wgr[:], moe_wgr[l].rearrange("(c p) f -> p c f", p=128), W=["wgr"])
        mk.dma("sp", bgr[:], moe_bgr[l], W=["bgr"])
        identf = C("ident")
        ones = C("ones")
        norm = make_norm(P, 1)
        xnew = [P.sb([128, D], F32, "xnew") for _ in range(2)]
        yt = [P.sb([128, 8, 128], BF16, "yt")] * 2
        mg = [P.sb([128, 4096], BF16, "mg")] * 2
        xt = [P.sb([128, D], F32, "xt")] * 2
        zz = P.sb([128, D], F32, "zz")
        zt = P.sb([128, D], F32, "zt")
        zb = P.sb([128, D], BF16, "zb")
        zT = P.sb([128, 8, 128], BF16, "zT")
        h2f = zz
        h2fT = zt[:, :].rearrange("p (c n) -> p c n", c=8)
        rt = {k: P.sb([128, w], F32, "rt" + k) for k, w in
              (("L", 20), ("gm", 1), ("goh", 4), ("ge", 4), ("gs", 1), ("el", 4), ("m1", 1), ("oh1", 4), ("e2", 4),
               ("m2", 1), ("oh2", 4), ("w1", 1), ("w2", 1), ("gw", 4))}
        nit = 0
        ntask = 0
        per_tile = -(-len(tasks) // len(tiles))
        for t in tiles:
            for _ in range(per_tile):
                if ntask < len(tasks):
                    do_task(ntask)
                    ntask += 1
            i = nit % 2
            nit += 1
            j = cond_of(t)
            cols = slice(t * 128, (t + 1) * 128)
            mk.dma("sp", yt[i][:], YT[:, :, :, cols].rearrange("n c p t -> p (n c) t"),
                   R=[f"YT{n}{c}" for n in range(4) for c in range(2)], W=["yt"])
            mk.dma("sp", mg[i][:], MG[cols, :], R=["MG"], W=["mg"])
            mk.dma("sp", xt[i][:], XR[cols, :], R=[f"XR{t}"], W=["mxt"])
            for n in range(4):
                for cb in range(2):
                    pb = (n * 2 + cb) % 2
                    for c in range(2):
                        mk.op("pe", "matmul", PS[pb][:, :], lhsT=yt[i][:, n * 2 + c, :],
                              rhs=wbr[:, n * 2 + c, cb * 512:(cb + 1) * 512], start=(c == 0), stop=(c == 1),
                              R=["yt", "wbr"], W=[f"ps{pb}"])
                    dst = zz if n == 0 else zt
                    dkey = "zz" if n == 0 else "zt"
                    mk.op("dve", "tensor_tensor", out=dst[:, cb * 512:(cb + 1) * 512], in0=PS[pb][:, :],
                          in1=mg[i][:, n * 1024 + cb * 512:n * 1024 + (cb + 1) * 512], op=ALU.mult,
                          R=[f"ps{pb}", "mg"], W=[dkey])
                    if n > 0:
                        mk.op("pool", "tensor_tensor", out=zz[:, cb * 512:(cb + 1) * 512],
                              in0=zz[:, cb * 512:(cb + 1) * 512], in1=zt[:, cb * 512:(cb + 1) * 512], op=ALU.add,
                              R=["zz", "zt"], W=["zz"])
            mk.op("act", "activation", out=zb[:], in_=zz[:], func=AF.Copy, R=["zz"], W=["zb"])
            for c in range(8):
                mk.op("pe", "transpose", out=PQ[1][:, c * 128:(c + 1) * 128], in_=zb[:, c * 128:(c + 1) * 128],
                      identity=identb[:], R=["zb", "identb"], W=["pq1"])
            mk.ev(zT[:].rearrange("p c n -> p (c n)"), PQ[1][:, :], R=["pq1"], W=["zT"])
            for cb in range(2):
                pb = 2 + cb
                for c in range(8):
                    mk.op("pe", "matmul", PS[pb][:, :], lhsT=zT[:, c, :], rhs=wo[:, c, cb * 512:(cb + 1) * 512],
                          start=(c == 0), stop=(c == 7), R=["zT", "wo"], W=[f"ps{pb}"])
                mk.op("dve", "tensor_tensor", out=zt[:, cb * 512:(cb + 1) * 512], in0=PS[pb][:, :],
                      in1=GB[:, j, 0, cb * 512:(cb + 1) * 512], op=ALU.mult, R=[f"ps{pb}", "GB"], W=["zt"])
                mk.op("pool", "tensor_tensor", out=xnew[i][:, cb * 512:(cb + 1) * 512], in0=zt[:, cb * 512:(cb + 1) * 512],
                      in1=xt[i][:, cb * 512:(cb + 1) * 512], op=ALU.add, R=["zt", "mxt"], W=[f"xnew{i}"])
            mk.dma("pool", XR[cols, :], xnew[i][:], R=[f"xnew{i}"], W=[f"XR{t}"])
            norm(xnew[i][:], f"xnew{i}", j, 3, 2, h2T[:, :, t * 128:(t + 1) * 128], f"h2T{t}")
            ssr = rt["gs"]
            mk.op("act", "activation", out=h2f[:], in_=xnew[i][:], func=AF.Square, accum_out=ssr[:],
                  R=[f"xnew{i}"], W=["zz", "rgs"])
            mk.op("act", "activation", out=ssr[:], in_=ssr[:], func=AF.Sqrt, scale=1.0 / D, bias=EPS, R=["rgs"], W=["rgs"])
            mk.op("dve", "reciprocal", out=ssr[:], in_=ssr[:], R=["rgs"], W=["rgs"])
            mk.op("dve", "tensor_scalar", out=h2f[:], in0=xnew[i][:], scalar1=ssr[:, 0:1], scalar2=None,
                  op0=ALU.mult, R=[f"xnew{i}", "rgs", "zz"], W=["zz"])
            for half in range(2):
                for c4 in range(4):
                    c = half * 4 + c4
                    mk.op("pe", "transpose", out=PS[5][:, c4 * 128:(c4 + 1) * 128], in_=h2f[:, c * 128:(c + 1) * 128],
                          identity=identf, R=["zz", "cst"], W=["ps5"])
                mk.op("dve", "tensor_tensor", out=h2fT[:, half * 4:(half + 1) * 4, :],
                      in0=PS[5][:, :].rearrange("p (c n) -> p c n", c=4),
                      in1=MODC[:, 3, half * 4:(half + 1) * 4, j:j + 1].to_broadcast([128, 4, 128]), op=ALU.mult,
                      R=["ps5", "MODC"], W=["zt"])
                mk.op("pool", "tensor_tensor", out=h2fT[:, half * 4:(half + 1) * 4, :],
                      in0=h2fT[:, half * 4:(half + 1) * 4, :],
                      in1=MODC[:, 2, half * 4:(half + 1) * 4, j:j + 1].to_broadcast([128, 4, 128]), op=ALU.add,
                      R=["zt", "MODC"], W=["zt"])
            for c in range(8):
                mk.op("pe", "matmul", PS[4][:, 0:20], lhsT=h2fT[:, c, :], rhs=wgr[:, c, :], start=(c == 0), stop=False,
                      R=["zt", "wgr"], W=["ps4r"])
            mk.op("pe", "matmul", PS[4][:, 0:20], lhsT=ones[0:1, :], rhs=bgr[0:1, :], start=False, stop=True,
                  R=["cst", "bgr"], W=["ps4r"])
            Lg = rt["L"]
            mk.op("act", "activation", out=Lg[:], in_=PS[4][:, 0:20], func=AF.Copy, R=["ps4r"], W=["rL"])
            rk = ["rL"]
            mk.op("dve", "tensor_reduce", out=rt["gm"][:], in_=Lg[:, 0:4], axis=AX.X, op=ALU.max, R=rk, W=["rgm"])
            mk.op("dve", "tensor_scalar", out=rt["goh"][:], in0=Lg[:, 0:4], scalar1=rt["gm"][:, 0:1], scalar2=None,
                  op0=ALU.is_ge, R=rk + ["rgm"], W=["rgoh"])
            mk.op("dve", "tensor_scalar", out=rt["ge"][:], in0=Lg[:, 0:4], scalar1=rt["gm"][:, 0:1], scalar2=None,
                  op0=ALU.subtract, R=rk + ["rgm"], W=["rge"])
            mk.op("act", "activation", out=rt["ge"][:], in_=rt["ge"][:], func=AF.Exp, accum_out=rt["gs"][:],
                  R=["rge"], W=["rge", "rgs"])
            mk.op("dve", "reciprocal", out=rt["gs"][:], in_=rt["gs"][:], R=["rgs"], W=["rgs"])
            mk.op("dve", "tensor_scalar", out=rt["el"][:], in0=Lg[:, 4:8], scalar1=rt["goh"][:, 0:1], scalar2=None,
                  op0=ALU.mult, R=rk + ["rgoh"], W=["rel"])
            for g in range(1, 4):
                mk.op("dve", "scalar_tensor_tensor", out=rt["el"][:], in0=Lg[:, 4 + g * 4:8 + g * 4],
                      scalar=rt["goh"][:, g:g + 1], in1=rt["el"][:], op0=ALU.mult, op1=ALU.add,
                      R=rk + ["rgoh", "rel"], W=["rel"])
            mk.op("dve", "tensor_reduce", out=rt["m1"][:], in_=rt["el"][:], axis=AX.X, op=ALU.max, R=["rel"], W=["rm1"])
            mk.op("dve", "tensor_scalar", out=rt["oh1"][:], in0=rt["el"][:], scalar1=rt["m1"][:, 0:1], scalar2=None,
                  op0=ALU.is_ge, R=["rel", "rm1"], W=["roh1"])
            mk.op("dve", "scalar_tensor_tensor", out=rt["e2"][:], in0=rt["oh1"][:], scalar=-1e30, in1=rt["el"][:],
                  op0=ALU.mult, op1=ALU.add, R=["roh1", "rel"], W=["re2"])
            mk.op("dve", "tensor_reduce", out=rt["m2"][:], in_=rt["e2"][:], axis=AX.X, op=ALU.max, R=["re2"], W=["rm2"])
            mk.op("dve", "tensor_scalar", out=rt["oh2"][:], in0=rt["e2"][:], scalar1=rt["m2"][:, 0:1], scalar2=None,
                  op0=ALU.is_ge, R=["re2", "rm2"], W=["roh2"])
            mk.op("dve", "tensor_tensor", out=rt["w1"][:], in0=rt["m2"][:], in1=rt["m1"][:], op=ALU.subtract,
                  R=["rm1", "rm2"], W=["rw1"])
            mk.op("act", "activation", out=rt["w1"][:], in_=rt["w1"][:], func=AF.Exp, R=["rw1"], W=["rw1"])
            mk.op("dve", "tensor_scalar_add", out=rt["w1"][:], in0=rt["w1"][:], scalar1=1.0, R=["rw1"], W=["rw1"])
            mk.op("dve", "reciprocal", out=rt["w1"][:], in_=rt["w1"][:], R=["rw1"], W=["rw1"])
            mk.op("dve", "tensor_tensor", out=rt["w1"][:], in0=rt["w1"][:], in1=rt["gs"][:], op=ALU.mult,
                  R=["rw1", "rgs"], W=["rw1"])
            mk.op("dve", "tensor_tensor", out=rt["w2"][:], in0=rt["gs"][:], in1=rt["w1"][:], op=ALU.subtract,
                  R=["rw1", "rgs"], W=["rw2"])
            mk.op("dve", "tensor_scalar", out=rt["gw"][:], in0=rt["oh1"][:], scalar1=rt["w1"][:, 0:1], scalar2=None,
                  op0=ALU.mult, R=["roh1", "rw1"], W=["rgw"])
            mk.op("dve", "scalar_tensor_tensor", out=rt["gw"][:], in0=rt["oh2"][:], scalar=rt["w2"][:, 0:1],
                  in1=rt["gw"][:], op0=ALU.mult, op1=ALU.add, R=["roh2", "rw2", "rgw"], W=["rgw"])
            for g in range(4):
                mk.op("dve", "tensor_scalar", out=gates[:, t, g * 4:(g + 1) * 4], in0=rt["gw"][:],
                      scalar1=rt["goh"][:, g:g + 1], scalar2=None, op0=ALU.mult, R=["rgw", "rgoh"], W=[f"gates{t}"])
        while ntask < len(tasks):
            do_task(ntask)
            ntask += 1
        mk.barrier()
        P.close()

    def moe_part(l, tiles, h2T, gates):
        P = Pool(nc, f"me{l}")
        TB = 8
        blocks = [(tiles[k], min(TB, len(tiles) - k)) for k in range(0, len(tiles), TB)]
        acc = P.sb([128, TB, D], F32, "acc")
        xt = [P.sb([128, D], F32, "xt") for _ in range(2)]
        w1b = [P.sb([128, 8, 512], BF16, "w1b") for _ in range(2)]
        w3b = [P.sb([128, 8, 512], BF16, "w3b") for _ in range(2)]
        w2b = [P.sb([128, 4, D], BF16, "w2b") for _ in range(2)]
        sl = [P.sb([128, 512], F32, "sl") for _ in range(2)]
        actT = [P.sb([128, 4, 512], BF16, "actT") for _ in range(2)]
        nw = 0
        for (tb0, tn) in blocks:
            ntok = tn * 128
            sblocks = [(s0, min(512, ntok - s0)) for s0 in range(0, ntok, 512)]
            hk = [f"h2T{tb0 + tt}" for tt in range(tn)]
            for e in range(16):
                wi = nw % 2
                nw += 1
                mk.dma("sp", w1b[wi][:].rearrange("p c f -> p (c f)"), W1B[e], R=[f"W1B{e}"], W=[f"w1b{wi}"])
                mk.dma("sp", w3b[wi][:].rearrange("p c f -> p (c f)"), W3B[e], R=[f"W3B{e}"], W=[f"w3b{wi}"])
                mk.dma("sp", w2b[wi][:].rearrange("p c f -> p (c f)"), W2B[e], R=[f"W2B{e}"], W=[f"w2b{wi}"])
                for (s0, sw) in sblocks:
                    ai = (s0 // 512) % 2
                    h0 = tb0 * 128 + s0
                    for fcn in range(4):
                        for k in range(8):
                            mk.op("pe", "matmul", PS[0][:, 0:sw], lhsT=w1b[wi][:, k, fcn * 128:(fcn + 1) * 128],
                                  rhs=h2T[:, k, h0:h0 + sw], start=(k == 0), stop=(k == 7), R=[f"w1b{wi}"] + hk, W=["ps0"])
                        for k in range(8):
                            mk.op("pe", "matmul", PS[1][:, 0:sw], lhsT=w3b[wi][:, k, fcn * 128:(fcn + 1) * 128],
                                  rhs=h2T[:, k, h0:h0 + sw], start=(k == 0), stop=(k == 7), R=[f"w3b{wi}"] + hk, W=["ps1"])
                        si = fcn % 2
                        mk.op("act", "activation", out=sl[si][:, 0:sw], in_=PS[0][:, 0:sw], func=AF.Silu, R=["ps0"],
                              W=[f"sl{si}"])
                        mk.op("dve", "tensor_tensor", out=actT[ai][:, fcn, 0:sw], in0=sl[si][:, 0:sw], in1=PS[1][:, 0:sw],
                              op=ALU.mult, R=[f"sl{si}", "ps1"], W=[f"actT{ai}"])
                    for q in range(sw // 128):
                        tt = s0 // 128 + q
                        t = tb0 + tt
                        for cb in range(2):
                            pb = 2 + cb
                            for fcn in range(4):
                                mk.op("pe", "matmul", PS[pb][:, :], lhsT=actT[ai][:, fcn, q * 128:(q + 1) * 128],
                                      rhs=w2b[wi][:, fcn, cb * 512:(cb + 1) * 512], start=(fcn == 0), stop=(fcn == 3),
                                      R=[f"actT{ai}", f"w2b{wi}"], W=[f"ps{pb}"])
                            if e == 0:
                                mk.op("dve", "tensor_scalar", out=acc[:, tt, cb * 512:(cb + 1) * 512], in0=PS[pb][:, :],
                                      scalar1=gates[:, t, e:e + 1], scalar2=None, op0=ALU.mult,
                                      R=[f"ps{pb}", f"gates{t}"], W=[f"acc{tt}"])
                            else:
                                mk.op("dve", "scalar_tensor_tensor", out=acc[:, tt, cb * 512:(cb + 1) * 512],
                                      in0=PS[pb][:, :], scalar=gates[:, t, e:e + 1], in1=acc[:, tt, cb * 512:(cb + 1) * 512],
                                      op0=ALU.mult, op1=ALU.add, R=[f"ps{pb}", f"gates{t}", f"acc{tt}"], W=[f"acc{tt}"])
            for tt in range(tn):
                t = tb0 + tt
                j = cond_of(t)
                mk.op("pool", "tensor_tensor", out=acc[:, tt, :], in0=acc[:, tt, :], in1=GB[:, j, 1, :], op=ALU.mult,
                      R=[f"acc{tt}", "GB"], W=[f"acc{tt}"])
                i2 = tt % 2
                mk.dma("sp", xt[i2][:], XR[t * 128:(t + 1) * 128, :], R=[f"XR{t}"], W=[f"ext{i2}"])
                mk.op("dve", "tensor_tensor", out=acc[:, tt, :], in0=acc[:, tt, :], in1=xt[i2][:], op=ALU.add,
                      R=[f"acc{tt}", f"ext{i2}"], W=[f"acc{tt}"])
                mk.dma("pool", XR[t * 128:(t + 1) * 128, :], acc[:, tt, :], R=[f"acc{tt}"], W=[f"XR{t}"])
        mk.barrier()
        P.close()

    def final_phase():
        P = Pool(nc, "fin")
        fw = P.sb([128, D], F32, "fw")
        mk.dma("sp", fw[:], fnw, W=["fw"])
        xt = [P.sb([128, D], F32, "xt") for _ in range(2)]
        ot = [P.sb([128, D], F32, "ot") for _ in range(2)]
        junk = P.sb([128, D], BF16, "junk")
        ss = [P.sb([128, 1], F32, "ss") for _ in range(2)]
        for t in OUT_T:
            i = t % 2
            mk.dma("sp", xt[i][:], XR[t * 128:(t + 1) * 128, :], R=[f"XR{t}"], W=[f"fxt{i}"])
            mk.op("act", "activation", out=junk[:], in_=xt[i][:], func=AF.Square, accum_out=ss[i][:], R=[f"fxt{i}"],
                  W=["fjunk", f"fss{i}"])
            mk.op("act", "activation", out=ss[i][:], in_=ss[i][:], func=AF.Sqrt, scale=1.0 / D, bias=EPS, R=[f"fss{i}"],
                  W=[f"fss{i}"])
            mk.op("dve", "reciprocal", out=ss[i][:], in_=ss[i][:], R=[f"fss{i}"], W=[f"fss{i}"])
            mk.op("dve", "scalar_tensor_tensor", out=ot[i][:], in0=xt[i][:], scalar=ss[i][:, 0:1], in1=fw[:], op0=ALU.mult,
                  op1=ALU.mult, R=[f"fxt{i}", f"fss{i}", "fw"], W=[f"fot{i}"])
            mk.dma("pool", yout[(t - 2) * 128:(t - 1) * 128, :], ot[i][:], R=[f"fot{i}"], W=["yout"])
        mk.barrier()
        P.close()

    stages = dict(mod=mod_phase, inproj=inproj_phase, a=mixer_a, b=mixer_b, c=mixer_c, d=mixer_d)
    return dict(nc=nc, mk=mk, stages=stages, merge=merge_moe_phase, final=final_phase, dbg=dbg,
                scr=dict(XR=XR, FM=FM, TM=TM, MG=MG, YT=YT))


def emit_all(prog, layers=NL, upto=None, skip=()):
    mk = prog["mk"]
    mk.barrier()
    done = False
    for l in range(layers):
        for s in ("mod", "inproj", "a", "b", "c", "d"):
            if s in skip:
                continue
            if s in ("inproj", "b", "c", "d"):
                prog["stages"][s](l, l == NL - 1)
            else:
                prog["stages"][s](l)
            if upto == (l, s):
                done = True
                break
        if done:
            break
        prog["merge"](l, l == NL - 1)
        if upto == (l, "merge"):
            done = True
            break
    if not done:
        prog["final"]()
    mk.barrier(engines=("sp",))


def _consts():
    j = np.arange(128)[:, None]
    i = np.arange(128)[None, :]
    same = (j // DL) == (i // DL)
    m = {}
    m["ident"] = (j == i)
    m["triF"] = (j <= i)
    m["triB"] = (j >= i)
    m["blkF"] = same & (j <= i)
    m["blkB"] = same & (j >= i)
    m["aftF"] = same & (j > i)
    m["befB"] = same & (j < i)
    m["diffF"] = np.maximum(i - j, 0)
    m["diffB"] = np.maximum(j - i, 0)
    m["maskF"] = (i >= j)
    m["maskB"] = (j > i)
    m["posF"] = np.broadcast_to(i + 1, (128, 128))
    m["posB"] = np.broadcast_to(128 - i, (128, 128))
    m["negF4"] = np.tile(np.where(j <= i, 0.0, -30000.0), (1, 4))
    m["negB4"] = np.tile(np.where(j >= i, 0.0, -30000.0), (1, 4))
    m["mblkF4"] = np.tile(same & (j <= i), (1, 4))
    m["mblkB4"] = np.tile(same & (j >= i), (1, 4))
    m["kpos"] = np.concatenate([127 - j, j], 1)
    m["ones"] = np.ones((128, 128))
    sel = np.zeros((128, 256))
    sel[0, 0:128] = 1.0
    sel[1, 128:256] = 1.0
    m["sel"] = sel
    m["subm"] = np.concatenate([(j // DL) == s_ for s_ in range(128 // DL)], 1)
    qm = np.zeros((128, 2, 5, 128), np.float32)
    for hh_ in range(2):
        for s_ in range(5):
            colsel = np.ones(128, bool) if s_ == 4 else (np.arange(128) // DL == s_)
            qm[hh_ * 64:(hh_ + 1) * 64, hh_, s_, :] = colsel[None, :]
    m["qmask"] = qm.reshape(128, 1280)
    out = np.zeros((128, NCST), np.float32)
    for k, (o, w) in CST.items():
        out[:, o:o + w] = np.asarray(m[k], np.float32)
    return out


def _rope_tables(flip=False):
    n = 16
    inv = np.power(np.float32(10000.0), -np.arange(n, dtype=np.float32) / n).astype(np.float32)
    t = np.arange(4096)
    row = (t // 64).astype(np.float32)
    col = (t % 64).astype(np.float32)
    ang = np.concatenate([row[:, None] * inv, col[:, None] * inv], -1)
    cos = np.cos(ang).astype(np.float32).T
    sin = np.sin(ang).astype(np.float32).T
    if flip:
        cos, sin = cos[:, ::-1], sin[:, ::-1]
    Cc = np.ones((128, T), np.float32)
    Ss = np.zeros((128, T), np.float32)
    for hh in range(2):
        Cc[hh * 64:hh * 64 + 32, 256:] = cos
        Cc[hh * 64 + 32:hh * 64 + 64, 256:] = cos
        Ss[hh * 64:hh * 64 + 32, 256:] = -sin
        Ss[hh * 64 + 32:hh * 64 + 64, 256:] = sin
    return Cc, Ss


def prep_shared(inp, flip=False):
    f = np.float32
    w_in = np.asarray(inp["w_in"], f)
    offs = {}
    o = 0
    for name, w in (("a_x", 256), ("a_g", 256), ("b_q", 256), ("b_k", 256), ("b_v", 256), ("b_g", 256), ("c_q", 256),
                    ("c_k", 256), ("c_v", 256), ("c_o", 256), ("c_gates", 16), ("d_q", 256), ("d_ff", 256),
                    ("d_fb", 256), ("d_i", 256), ("d_g", 256), ("merge", 4096)):
        offs[name] = (o, w)
        o += w

    def cols(n):
        a, w = offs[n]
        return w_in[:, :, a:a + w]

    perm = np.concatenate([np.arange(h * 64 + 32, h * 64 + 64).tolist() + np.arange(h * 64, h * 64 + 32).tolist()
                           for h in range(4)]).astype(np.int64)
    w_fm = np.concatenate([cols("a_x"), cols("a_g"), cols("b_q"), cols("b_q")[:, :, perm], cols("b_k"),
                           cols("b_k")[:, :, perm], cols("c_q"), cols("c_k")], -1)
    gperm = np.array([8, 9, 10, 11, 12, 13, 14, 15, 0, 1, 2, 3, 4, 5, 6, 7]) if flip else np.arange(16)
    dfa, dfb = ("d_fb", "d_ff") if flip else ("d_ff", "d_fb")
    w_tm = np.concatenate([cols("b_v"), cols("b_g"), cols("c_v"), cols("c_o"), cols("d_q"), cols(dfa), cols(dfb),
                           cols("d_i"), cols("d_g"), cols("c_gates")[:, :, gperm], cols("merge")], -1)
    sh = {}
    sh["w_mod"] = np.ascontiguousarray(inp["w_mod"], f)
    bm = np.asarray(inp["b_mod"], f)
    sh["bmod_c"] = np.ascontiguousarray(bm.reshape(NL, 48, 128).transpose(0, 2, 1))
    sh["bmod_r"] = np.ascontiguousarray(bm.reshape(NL, 1, 6144))
    sh["w_fm"] = np.ascontiguousarray(w_fm)
    sh["w_tm"] = np.ascontiguousarray(w_tm)
    acw = np.asarray(inp["a_conv_w"], f)
    zt_ = np.zeros_like(acw[:, :1])
    acw = np.concatenate([zt_, acw[:, ::-1]], 1) if flip else np.concatenate([acw, zt_], 1)
    sh["a_cw"] = np.ascontiguousarray(acw.reshape(NL, 5, 2, 128).transpose(0, 3, 2, 1))
    sh["a_cb"] = np.ascontiguousarray(np.asarray(inp["a_conv_b"], f).reshape(NL, 2, 128).transpose(0, 2, 1))
    gw = np.asarray(inp["a_gate_w"], f)
    agw = np.zeros((NL, 128, 2, 2, 2, 128), f)
    for c in range(2):
        for hh in range(2):
            agw[:, hh * 64:(hh + 1) * 64, :, :, c, hh * 64:(hh + 1) * 64] = gw[:, :, :, 2 * c + hh].transpose(0, 3, 1, 2, 4)
    sh["a_gw"] = np.ascontiguousarray(agw[:, :, ::-1]) if flip else agw
    gb = np.asarray(inp["a_gate_b"], f)
    if flip:
        gb = gb[:, ::-1]
    sh["a_gb"] = np.ascontiguousarray(gb.reshape(NL, 2, 2, 2, 128).transpose(0, 4, 1, 2, 3))
    lam = np.asarray(inp["a_lambda"], f)
    if flip:
        lam = lam[:, ::-1]
    sh["a_lam"] = np.ascontiguousarray(lam.reshape(NL, 2, 2, 128).transpose(0, 3, 1, 2))
    th = np.asarray(inp["b_theta"], f)
    if flip:
        th = th[:, ::-1]
    thp = np.zeros((NL, 128, 2, 2), f)
    for c in range(2):
        for hh in range(2):
            thp[:, hh * 64:(hh + 1) * 64, :, c] = th[:, None, :, 2 * c + hh]
    sh["b_thp"] = thp
    sh["b_thh"] = np.ascontiguousarray(np.broadcast_to(th[:, None], (NL, 128, 2, 4)))
    ccw = np.asarray(inp["c_conv_w"], f)
    zt_ = np.zeros_like(ccw[:, :1])
    ccw = np.concatenate([zt_, ccw[:, ::-1]], 1) if flip else np.concatenate([ccw, zt_], 1)
    sh["c_cw"] = np.ascontiguousarray(ccw.reshape(NL, 5, 4, 128).transpose(0, 3, 2, 1))
    sh["c_cb"] = np.ascontiguousarray(np.asarray(inp["c_conv_b"], f).reshape(NL, 4, 128).transpose(0, 2, 1))
    sh["c_gb"] = np.ascontiguousarray(np.broadcast_to(np.asarray(inp["c_gate_b"], f).reshape(NL, 1, 16)[:, :, gperm], (NL, 128, 16)))
    sh["d_lbr"] = np.ascontiguousarray(np.broadcast_to(np.asarray(inp["d_lb"], f)[None], (128, 2, 256)))
    sh["w_branch"] = np.ascontiguousarray(inp["w_branch"], f)
    sh["w_out"] = np.ascontiguousarray(inp["w_out"], f)
    sh["moe_wgr"] = np.ascontiguousarray(np.concatenate([np.asarray(inp["moe_w_group"], f), np.asarray(inp["moe_w_router"], f)], -1))
    sh["moe_bgr"] = np.ascontiguousarray(np.concatenate([np.asarray(inp["moe_b_group"], f), np.asarray(inp["moe_b_router"], f)], -1).reshape(NL, 1, 20))
    sh["moe_w1"] = np.ascontiguousarray(inp["moe_w1"], f)
    sh["moe_w3"] = np.ascontiguousarray(inp["moe_w3"], f)
    sh["moe_w2"] = np.ascontiguousarray(inp["moe_w2"], f)
    sh["fnw"] = np.ascontiguousarray(np.broadcast_to(np.asarray(inp["final_norm_w"], f)[None], (128, D)))
    sh["cst"] = _consts()
    sh["ropeC"], sh["ropeS"] = _rope_tables(flip)
    return sh


def prep_core(inp, b, flip=False):
    f = np.float32
    d = {}
    cx, xx = np.asarray(inp["ctx"][b], f), np.asarray(inp["x"][b], f)
    if flip:
        cx, xx = cx[::-1], xx[::-1]
    d["xin"] = np.ascontiguousarray(np.concatenate([cx, xx], 0))
    cv = np.stack([np.asarray(inp["c_ctx"], f), np.asarray(inp["c"][b], f)], -1)
    d["cvec"] = np.ascontiguousarray(cv.reshape(8, 128, 2).transpose(1, 0, 2))
    return d


_PROG = None


def kernel(**inputs):
    global _PROG
    if _PROG is None:
        _PROG = build_program()
        emit_all(_PROG)
    nc = _PROG["nc"]
    shs = [prep_shared(inputs, False), prep_shared(inputs, True)]
    in_maps = []
    for core in range(8):
        fl = core >= 4
        m = dict(shs[1 if fl else 0])
        m.update(prep_core(inputs, core % 4, fl))
        in_maps.append(m)
    res = run_bass_kernel_spmd(nc, in_maps, core_ids=list(range(8)))
    out = np.empty((4, 4096, D), np.float32)
    for b in range(4):
        out[b, :HALF_OUT] = np.asarray(res.results[b]["yout"], np.float32)[:HALF_OUT]
        out[b, HALF_OUT:] = np.asarray(res.results[b + 4]["yout"], np.float32)[:4096 - HALF_OUT][::-1]
    return out
```

```python
import contextlib
import numpy as np
import ml_dtypes
import concourse.bass as bass
import concourse.mybir as mybir
from concourse.bass_utils import run_bass_kernel_spmd

F32 = mybir.dt.float32
BF16 = mybir.dt.bfloat16
AF = mybir.ActivationFunctionType
ALU = mybir.AluOpType
AX = mybir.AxisListType

T = 4352
NT = 34
D = 1024
EPS = 1e-6
NL = 2
SLOT = 512
HALF_OUT = 2048
DL = 32
DNS = 128 // DL
DBG = dict(maxit=None, core=True, fin=True, heads=(0, 1, 2, 3))
TMW = 2320
TM_OFF = dict(b_v=0, b_g=256, c_v=512, c_o=768, d_q=1024, d_ff=1280, d_fb=1536, d_i=1792, d_g=2048, c_gates=2304)
FM_OFF = dict(a_x=0, a_g=2, b_q=4, b_qp=6, b_k=8, b_kp=10, c_q=12, c_k=14)

CST = {}
_off = 0
for _n, _w in (("ident", 128), ("triF", 128), ("triB", 128), ("blkF", 128), ("blkB", 128), ("aftF", 128),
               ("befB", 128), ("diffF", 128), ("diffB", 128), ("maskF", 128), ("maskB", 128), ("posF", 128),
               ("posB", 128), ("negF4", 512), ("negB4", 512), ("mblkF4", 512), ("mblkB4", 512), ("kpos", 2),
               ("ones", 128), ("sel", 256), ("subm", 4), ("qmask", 1280), ("ltS", 128), ("thrS", 16), ("kS", 16), ("jp", 4)):
    CST[_n] = (_off, _w)
    _off += _w
NCST = _off


class MK:
    SEM_ROT = 30000

    def __init__(self, nc, ndma=8):
        self.nc = nc
        self.engs = {"pe": nc.tensor, "act": nc.scalar, "dve": nc.vector, "pool": nc.gpsimd, "sp": nc.sync}
        self._ctxs = []
        self.nsem = 0
        self.sem = {}
        self.cnt = {}
        for e in ("pe", "act", "dve", "pool"):
            self.sem[e] = self._newsem("s_" + e)
            self.cnt[e] = 0
        self.seen = {e: {} for e in self.engs}
        self.dq = {}
        for q in ("sp", "pool"):
            self.dq[q] = {"i": 0, "slots": [[self._newsem(f"d_{q}{i}"), 0] for i in range(ndma)]}
        self.res = {}
        self.ninst = 0
        self.flip = 0

    def _newsem(self, name):
        self.nsem += 1
        cm = self.nc.semaphore(f"{name}_{self.nsem}")
        s = cm.__enter__()
        self._ctxs.append(cm)
        return s

    def _wait(self, eng, tok):
        sem, val = tok
        key = id(sem)
        if self.seen[eng].get(key, 0) >= val:
            return
        self.engs[eng].wait_ge(sem, val)
        self.seen[eng][key] = val

    def _deps(self, R, W):
        deps = []
        for k in R:
            st = self.res.get(k)
            if st and st[0] is not None:
                deps.append(st[0])
        for k in W:
            st = self.res.get(k)
            if st:
                if st[0] is not None:
                    deps.append(st[0])
                deps.extend(st[1])
        return deps

    def _record(self, tok, R, W):
        for k in R:
            st = self.res.setdefault(k, [None, []])
            st[1] = [t for t in st[1] if t[0] is not tok[0]] + [tok]
        for k in W:
            self.res[k] = [tok, []]

    def op(self, eng, method, *args, R=(), W=(), **kw):
        for tok in self._deps(R, W):
            if eng == "pe" and tok[0] is self.sem["pe"]:
                continue
            self._wait(eng, tok)
        ins = getattr(self.engs[eng], method)(*args, **kw)
        if self.cnt[eng] >= self.SEM_ROT:
            self.sem[eng] = self._newsem("s_" + eng)
            self.cnt[eng] = 0
        self.cnt[eng] += 1
        ins.then_inc(self.sem[eng], 1)
        tok = (self.sem[eng], self.cnt[eng])
        self._record(tok, R, W)
        self.ninst += 1
        return tok

    def dma(self, q, out, in_, R=(), W=(), **kw):
        d = self.dq[q]
        slot = d["slots"][d["i"] % len(d["slots"])]
        d["i"] += 1
        if slot[1] > 0:
            self._wait(q, (slot[0], slot[1]))
        if slot[1] >= self.SEM_ROT:
            slot[0] = self._newsem("d_" + q)
            slot[1] = 0
        for tok in self._deps(R, W):
            self._wait(q, tok)
        ins = self.engs[q].dma_start(out=out, in_=in_, **kw)
        slot[1] += 16
        ins.then_inc(slot[0], 16)
        tok = (slot[0], slot[1])
        self._record(tok, R, W)
        self.ninst += 1
        return tok

    def idma(self, out, out_offset, in_, in_offset, R=(), W=()):
        q = "pool"
        d = self.dq[q]
        slot = d["slots"][d["i"] % len(d["slots"])]
        d["i"] += 1
        if slot[1] > 0:
            self._wait(q, (slot[0], slot[1]))
        if slot[1] >= self.SEM_ROT:
            slot[0] = self._newsem("d_" + q)
            slot[1] = 0
        for tok in self._deps(R, W):
            self._wait(q, tok)
        ins = self.nc.gpsimd.indirect_dma_start(out=out, out_offset=out_offset, in_=in_, in_offset=in_offset)
        slot[1] += 16
        ins.then_inc(slot[0], 16)
        tok = (slot[0], slot[1])
        self._record(tok, R, W)
        self.ninst += 1
        return tok

    def barrier(self, engines=("pe", "act", "dve", "pool", "sp")):
        toks = []
        for q, d in self.dq.items():
            for slot in d["slots"]:
                if slot[1] > 0:
                    toks.append((slot[0], slot[1]))
        for e in ("pe", "act", "dve", "pool"):
            if self.cnt[e] > 0:
                toks.append((self.sem[e], self.cnt[e]))
        for e in engines:
            for tok in toks:
                if e in self.sem and tok[0] is self.sem[e]:
                    continue
                self._wait(e, tok)

    def ev(self, out, in_, R=(), W=(), func=None, **kw):
        if func is not None:
            return self.op("act", "activation", out=out, in_=in_, func=func, R=R, W=W, **kw)
        self.flip ^= 1
        if self.flip:
            return self.op("act", "activation", out=out, in_=in_, func=AF.Copy, R=R, W=W)
        return self.op("dve", "tensor_copy", out=out, in_=in_, R=R, W=W)


class Pool:
    def __init__(self, nc, tag):
        self.nc = nc
        self.tag = tag
        self.stack = contextlib.ExitStack()
        self.n = 0

    def sb(self, shape, dt=F32, name=None):
        self.n += 1
        return self.stack.enter_context(self.nc.sbuf_tensor(f"{self.tag}_{name or 't'}{self.n}", list(shape), dt))

    def close(self):
        self.stack.close()


def build_program(debug=()):
    nc = bass.Bass("TRN2", target_bir_lowering=False)

    def din(name, shape, dt=F32):
        return nc.dram_tensor(name, list(shape), dt, kind="ExternalInput").ap()

    def dscr(name, shape, dt=F32):
        return nc.dram_tensor(name, list(shape), dt, kind="Internal").ap()

    xin = din("xin", [T, D])
    cvec = din("cvec", [128, 8, 2])
    w_mod = din("w_mod", [NL, D, 6144])
    bmod_c = din("bmod_c", [NL, 128, 48])
    bmod_r = din("bmod_r", [NL, 1, 6144])
    w_fm = din("w_fm", [NL, D, 2048])
    w_tm = din("w_tm", [NL, D, TMW + 4096])
    a_cw = din("a_cw", [NL, 128, 2, 5])
    a_cb = din("a_cb", [NL, 128, 2])
    a_gw = din("a_gw", [NL, 128, 2, 2, 2, 128])
    a_gb = din("a_gb", [NL, 128, 2, 2, 2])
    a_lam = din("a_lam", [NL, 128, 2, 2])
    b_thp = din("b_thp", [NL, 128, 2, 2])
    b_thh = din("b_thh", [NL, 128, 2, 4])
    c_cw = din("c_cw", [NL, 128, 4, 5])
    c_cb = din("c_cb", [NL, 128, 4])
    c_gb = din("c_gb", [NL, 128, 16])
    d_lbr = din("d_lbr", [128, 2, 256])
    w_branch = din("w_branch", [NL, 4, 256, D])
    w_out = din("w_out", [NL, D, D])
    moe_wgr = din("moe_wgr", [NL, D, 20])
    moe_bgr = din("moe_bgr", [NL, 1, 20])
    moe_w1 = din("moe_w1", [NL, 16, D, 512])
    moe_w3 = din("moe_w3", [NL, 16, D, 512])
    moe_w2 = din("moe_w2", [NL, 16, 512, D])
    fnw = din("fnw", [128, D])
    cst_d = din("cst", [128, NCST])
    ropeC_d = din("ropeC", [128, T])
    ropeS_d = din("ropeS", [128, T])
    yout = nc.dram_tensor("yout", [HALF_OUT, D], F32, kind="ExternalOutput").ap()

    XR = dscr("XR", [T, D])
    FM = dscr("FM", [16, 128, T])
    TM = dscr("TM", [T, TMW])
    MG = dscr("MG", [T, 4096], BF16)
    YT = dscr("YT", [4, 2, 128, T], BF16)
    dbg = {}
    for name, shape, dt in debug:
        dbg[name] = nc.dram_tensor("dbg_" + name, list(shape), dt, kind="ExternalOutput").ap()

    mk = MK(nc)
    G = Pool(nc, "g")

    PS = [nc.psum_tensor(f"ps{i}", [128, 512], F32).__enter__() for i in range(6)]
    PQ = [nc.psum_tensor(f"pq{i}", [128, 1024], BF16).__enter__() for i in range(2)]

    cst = G.sb([128, NCST], F32, "cst")
    identb = G.sb([128, 128], BF16, "identb")
    sT = G.sb([128, 8, 2], F32, "sT")
    MODC = G.sb([128, 4, 8, 2], F32, "MODC")
    GB = G.sb([128, 2, 2, D], F32, "GB")
    mk.dma("sp", cst[:], cst_d, W=["cst"])
    mk.dma("sp", sT[:], cvec, W=["sT"])

    def C(name, rows=slice(0, 128)):
        o, w = CST[name]
        return cst[rows, o:o + w]

    mk.op("dve", "tensor_copy", out=identb[:], in_=C("ident"), R=["cst"], W=["identb"])
    mk.op("act", "activation", out=sT[:], in_=sT[:], func=AF.Silu, R=["sT"], W=["sT"])
    for t in range(NT):
        mk.dma("sp", XR[t * 128:(t + 1) * 128, :], xin[t * 128:(t + 1) * 128, :], W=[f"XR{t}"])

    def cond_of(t):
        return 0 if t < 2 else 1

    def mod_phase(l):
        P = Pool(nc, f"mod{l}")
        wblk = P.sb([128, 8, 1024], F32, "wblk")
        bmc = P.sb([128, 48], F32, "bmc")
        bmr = P.sb([1, 6144], F32, "bmr")
        GR = P.sb([2, 2, D], F32, "GR")
        mk.dma("sp", bmc[:], bmod_c[l], W=["bmc"])
        mk.dma("sp", bmr[:], bmod_r[l], W=["bmr"])
        sel = C("sel", slice(0, 2))
        for m in range(6):
            mk.dma("sp", wblk[:], w_mod[l].rearrange("(c p) f -> p c f", p=128)[:, :, m * 1024:(m + 1) * 1024],
                   W=["wblk"])
            if m in (0, 1, 3, 4):
                m4 = {0: 0, 1: 1, 3: 2, 4: 3}[m]
                for c in range(8):
                    for k in range(8):
                        mk.op("pe", "matmul", PS[0][:, c * 2:(c + 1) * 2], lhsT=wblk[:, k, c * 128:(c + 1) * 128],
                              rhs=sT[:, k, :], start=(k == 0), stop=(k == 7), R=["wblk", "sT"], W=["ps0"])
                mk.op("dve", "tensor_tensor", out=MODC[:, m4, :, :],
                      in0=PS[0][:, 0:16].rearrange("p (c j) -> p c j", j=2),
                      in1=bmc[:, m * 8:(m + 1) * 8].unsqueeze(2).to_broadcast([128, 8, 2]), op=ALU.add,
                      R=["ps0", "bmc"], W=["MODC"])
                if m in (1, 4):
                    mk.op("dve", "tensor_scalar_add", out=MODC[:, m4, :, :], in0=MODC[:, m4, :, :], scalar1=1.0,
                          R=["MODC"], W=["MODC"])
            else:
                mi = 0 if m == 2 else 1
                for cb in range(2):
                    for k in range(8):
                        mk.op("pe", "matmul", PS[1][0:2, :], lhsT=sT[:, k, :], rhs=wblk[:, k, cb * 512:(cb + 1) * 512],
                              start=(k == 0), stop=False, R=["wblk", "sT"], W=["ps1"])
                    mk.op("pe", "matmul", PS[1][0:2, :], lhsT=sel[0:1, 0:2],
                          rhs=bmr[0:1, m * 1024 + cb * 512: m * 1024 + (cb + 1) * 512], start=False, stop=True,
                          R=["bmr", "cst"], W=["ps1"])
                    mk.op("dve", "tensor_copy", out=GR[0:2, mi, cb * 512:(cb + 1) * 512], in_=PS[1][0:2, :],
                          R=["ps1"], W=["GR"])
        for j in range(2):
            for mi in range(2):
                for cb in range(2):
                    mk.op("pe", "matmul", PS[1][:, :], lhsT=sel[0:2, j * 128:(j + 1) * 128],
                          rhs=GR[0:2, mi, cb * 512:(cb + 1) * 512], start=True, stop=True, R=["GR", "cst"], W=["ps1"])
                    mk.op("act", "activation", out=GB[:, j, mi, cb * 512:(cb + 1) * 512], in_=PS[1][:, :], func=AF.Copy,
                          R=["ps1"], W=["GB"])
        mk.barrier()
        P.close()

    def make_norm(P, nbuf=2):
        st = dict(junk=P.sb([128, D], BF16, "junk"), ss=[P.sb([128, 1], F32, "ss") for _ in range(nbuf)],
                  xn=[P.sb([128, D], BF16, "xn") for _ in range(nbuf)],
                  tmp=[P.sb([128, 8, 128], F32, "tmp") for _ in range(nbuf)], i=0, nbuf=nbuf)

        def norm(xt_ap, xt_key, j, msc, msh, h_out, h_key):
            i = st["i"] % st["nbuf"]
            st["i"] += 1
            ss, xn, tmp = st["ss"][i], st["xn"][i], st["tmp"][i]
            mk.op("act", "activation", out=st["junk"][:], in_=xt_ap, func=AF.Square, accum_out=ss[:],
                  R=[xt_key], W=["junk", f"ss{i}"])
            mk.op("act", "activation", out=ss[:], in_=ss[:], func=AF.Sqrt, scale=1.0 / D, bias=EPS,
                  R=[f"ss{i}"], W=[f"ss{i}"])
            mk.op("dve", "reciprocal", out=ss[:], in_=ss[:], R=[f"ss{i}"], W=[f"ss{i}"])
            mk.op("dve", "tensor_scalar", out=xn[:], in0=xt_ap, scalar1=ss[:, 0:1], scalar2=None, op0=ALU.mult,
                  R=[xt_key, f"ss{i}"], W=[f"xn{i}"])
            for c in range(8):
                mk.op("pe", "transpose", out=PQ[i][:, c * 128:(c + 1) * 128], in_=xn[:, c * 128:(c + 1) * 128],
                      identity=identb[:], R=[f"xn{i}", "identb"], W=[f"pq{i}"])
            mk.op("dve", "tensor_tensor", out=tmp[:], in0=PQ[i][:, :].rearrange("p (c n) -> p c n", c=8),
                  in1=MODC[:, msc, :, j:j + 1].to_broadcast([128, 8, 128]), op=ALU.mult,
                  R=[f"pq{i}", "MODC"], W=[f"ntmp{i}"])
            mk.op("pool", "tensor_tensor", out=h_out, in0=tmp[:],
                  in1=MODC[:, msh, :, j:j + 1].to_broadcast([128, 8, 128]), op=ALU.add,
                  R=[f"ntmp{i}", "MODC"], W=[h_key])
        return norm

    def inproj_phase(l, last=False):
        P = Pool(nc, f"ip{l}")
        hT = P.sb([128, 8, T], BF16, "hT")
        xt = [P.sb([128, D], F32, "xt") for _ in range(2)]
        norm = make_norm(P)
        for t in range(NT):
            i = t % 2
            mk.dma("sp", xt[i][:], XR[t * 128:(t + 1) * 128, :], R=[f"XR{t}"], W=[f"xt{i}"])
            norm(xt[i][:], f"xt{i}", cond_of(t), 1, 0, hT[:, :, t * 128:(t + 1) * 128], f"hT{t}")
        hkeys = [f"hT{t}" for t in range(NT)]
        wf = [P.sb([128, 8, 512], F32, "wf")] * 2
        wb = [P.sb([128, 8, 512], BF16, "wb") for _ in range(2)]
        stg = [P.sb([128, T], F32, "stg")] * 2
        nblk = 0
        tblocks = [(i * 512, min(512, T - i * 512)) for i in range(9)]
        for cb in range(4):
            i = nblk % 2
            nblk += 1
            mk.dma("sp", wf[i][:], w_fm[l].rearrange("(c p) f -> p c f", p=128)[:, :, cb * 512:(cb + 1) * 512],
                   W=["wf"])
            mk.ev(wb[i][:], wf[i][:], R=["wf"], W=[f"wb{i}"])
            for sub in range(4):
                fc = cb * 4 + sub
                si = fc % 2
                for bi, (t0, tw) in enumerate(tblocks):
                    pb = bi % 2
                    for k in range(8):
                        mk.op("pe", "matmul", PS[pb][:, 0:tw], lhsT=wb[i][:, k, sub * 128:(sub + 1) * 128],
                              rhs=hT[:, k, t0:t0 + tw], start=(k == 0), stop=(k == 7),
                              R=[f"wb{i}"] + hkeys[t0 // 128:(t0 + tw) // 128], W=[f"ps{pb}"])
                    mk.ev(stg[si][:, t0:t0 + tw], PS[pb][:, 0:tw], R=[f"ps{pb}"], W=["stg"])
                mk.dma("pool", FM[fc], stg[si][:], R=["stg"], W=[f"FM{fc}"])
        cblocks = [(i * 512, 512) for i in range(4)] + [(2048, TMW - 2048)] + [(TMW + i * 512, 512) for i in range(8)]
        stt = [P.sb([128, 4, 512], F32, "stt") for _ in range(2)]
        stb = [P.sb([128, 4, 512], BF16, "stb") for _ in range(2)]
        tgroups = [(g * 4, min(4, NT - g * 4)) for g in range(9)]
        ns = 0
        for (c0, cw) in cblocks:
            i = nblk % 2
            nblk += 1
            mk.dma("sp", wf[i][:, :, 0:cw], w_tm[l].rearrange("(c p) f -> p c f", p=128)[:, :, c0:c0 + cw], W=["wf"])
            mk.ev(wb[i][:, :, 0:cw], wf[i][:, :, 0:cw], R=["wf"], W=[f"wb{i}"])
            is_mg = c0 >= TMW
            for (g0, gn) in tgroups:
                if is_mg and last and not any((g0 + q) in OUT_T for q in range(gn)):
                    continue
                si = ns % 2
                ns += 1
                for tt in range(gn):
                    t = g0 + tt
                    pb = 2 + (t % 2)
                    for k in range(8):
                        mk.op("pe", "matmul", PS[pb][:, 0:cw], lhsT=hT[:, k, t * 128:(t + 1) * 128],
                              rhs=wb[i][:, k, 0:cw], start=(k == 0), stop=(k == 7), R=[f"wb{i}", f"hT{t}"], W=[f"ps{pb}"])
                    if is_mg:
                        mk.ev(stb[si][:, tt, 0:cw], PS[pb][:, 0:cw], R=[f"ps{pb}"], W=[f"stb{si}"], func=AF.Sigmoid)
                    else:
                        mk.ev(stt[si][:, tt, 0:cw], PS[pb][:, 0:cw], R=[f"ps{pb}"], W=[f"stt{si}"])
                if is_mg:
                    mk.dma("pool", MG[g0 * 128:(g0 + gn) * 128, c0 - TMW:c0 - TMW + cw].rearrange("(t p) c -> p t c", p=128),
                           stb[si][:, 0:gn, 0:cw], R=[f"stb{si}"], W=["MG"])
                else:
                    mk.dma("pool", TM[g0 * 128:(g0 + gn) * 128, c0:c0 + cw].rearrange("(t p) c -> p t c", p=128),
                           stt[si][:, 0:gn, 0:cw], R=[f"stt{si}"], W=["TM"])
        mk.barrier()
        P.close()

    SEGS = [(0, 256), (256, T)]

    def conv_fm(u, src, w4, bcol, keyu, keysrc, wkeys):
        for (s0, e) in SEGS:
            mk.op("act", "activation", out=u[:, s0:e], in_=src[:, s0:e], func=AF.Identity, scale=w4[:, 2:3], bias=bcol,
                  R=[keysrc] + wkeys, W=[keyu])
            for k, sh in ((0, -2), (1, -1), (3, 1), (4, 2)):
                if sh < 0:
                    o, i_ = u[:, s0 - sh:e], src[:, s0:e + sh]
                else:
                    o, i_ = u[:, s0:e - sh], src[:, s0 + sh:e]
                mk.op("dve", "scalar_tensor_tensor", out=o, in0=i_, scalar=w4[:, k:k + 1], in1=o, op0=ALU.mult,
                      op1=ALU.add, R=[keysrc, keyu] + wkeys, W=[keyu])

    def mixer_a(l):
        P = Pool(nc, f"ma{l}")
        cw = P.sb([128, 2, 5], F32, "cw")
        cb = P.sb([128, 2], F32, "cb")
        gwf = P.sb([128, 2, 2, 2, 128], F32, "gwf")
        gwb = P.sb([128, 2, 2, 2, 128], BF16, "gwb")
        gb = P.sb([128, 2, 2, 2], F32, "gb")
        lam = P.sb([128, 2, 2], F32, "lam")
        c1 = P.sb([128, 2, 2], F32, "c1")
        mk.dma("sp", cw[:], a_cw[l], W=["a_cw"])
        mk.dma("sp", cb[:], a_cb[l], W=["a_cb"])
        mk.dma("sp", gwf[:], a_gw[l], W=["a_gwf"])
        mk.dma("sp", gb[:], a_gb[l], W=["a_gb"])
        mk.dma("sp", lam[:], a_lam[l], W=["a_lam"])
        mk.op("dve", "tensor_copy", out=gwb[:], in_=gwf[:], R=["a_gwf"], W=["a_gwb"])
        mk.op("act", "activation", out=c1[:], in_=lam[:], func=AF.Exp, scale=-1.0, R=["a_lam"], W=["a_c1"])
        mk.op("act", "activation", out=c1[:], in_=c1[:], func=AF.Ln, bias=1.0, R=["a_c1"], W=["a_c1"])
        mk.op("dve", "tensor_scalar", out=c1[:], in0=c1[:], scalar1=-8.0, scalar2=None, op0=ALU.mult, R=["a_c1"], W=["a_c1"])
        ax = P.sb([128, T], F32, "ax")
        ag = P.sb([128, T], F32, "ag")
        u = P.sb([128, T], F32, "u")
        ub = P.sb([128, T], BF16, "ub")
        aa = P.sb([128, T], F32, "aa")
        bt = P.sb([128, T], F32, "bt")
        hf = P.sb([128, T], F32, "hf")
        hb = P.sb([128, T], F32, "hb")
        r = [P.sb([128, 512], F32, "r") for _ in range(2)]
        gi = [P.sb([128, 512], F32, "gi") for _ in range(2)]
        yb = P.sb([128, T], BF16, "yb")
        tblocks = [(i * 512, min(512, T - i * 512)) for i in range(9)]
        for c in range(2):
            mk.dma("sp", ax[:], FM[FM_OFF["a_x"] + c], R=[f"FM{FM_OFF['a_x'] + c}"], W=["ax"])
            mk.dma("sp", ag[:], FM[FM_OFF["a_g"] + c], R=[f"FM{FM_OFF['a_g'] + c}"], W=["ag"])
            conv_fm(u, ax, cw[:, c, :], cb[:, c:c + 1], "u", "ax", ["a_cw", "a_cb"])
            mk.op("pool", "tensor_copy", out=ub[:], in_=u[:], R=["u"], W=["ub"])
            for d in range(2):
                for bi, (t0, tw) in enumerate(tblocks):
                    i = bi % 2
                    mk.op("pe", "matmul", PS[i][:, 0:tw], lhsT=gwb[:, d, 0, c, :], rhs=ub[:, t0:t0 + tw], start=True,
                          stop=True, R=["a_gwb", "ub"], W=[f"ps{i}"])
                    mk.op("pe", "matmul", PS[2 + i][:, 0:tw], lhsT=gwb[:, d, 1, c, :], rhs=ub[:, t0:t0 + tw], start=True,
                          stop=True, R=["a_gwb", "ub"], W=[f"ps{2 + i}"])
                    mk.op("act", "activation", out=r[i][:, 0:tw], in_=PS[i][:, 0:tw], func=AF.Sigmoid,
                          bias=gb[:, d, 0, c:c + 1], R=[f"ps{i}", "a_gb"], W=[f"r{i}"])
                    mk.op("act", "activation", out=gi[i][:, 0:tw], in_=PS[2 + i][:, 0:tw], func=AF.Sigmoid,
                          bias=gb[:, d, 1, c:c + 1], R=[f"ps{2 + i}", "a_gb"], W=[f"gi{i}"])
                    mk.op("act", "activation", out=aa[:, t0:t0 + tw], in_=r[i][:, 0:tw], func=AF.Exp,
                          scale=c1[:, d, c:c + 1], R=[f"r{i}", "a_c1"], W=["aa"])
                    mk.op("dve", "tensor_tensor", out=r[i][:, 0:tw], in0=aa[:, t0:t0 + tw], in1=aa[:, t0:t0 + tw],
                          op=ALU.mult, R=["aa", f"r{i}"], W=[f"r{i}"])
                    mk.op("dve", "tensor_scalar", out=r[i][:, 0:tw], in0=r[i][:, 0:tw], scalar1=-1.0, scalar2=1.0,
                          op0=ALU.mult, op1=ALU.add, R=[f"r{i}"], W=[f"r{i}"])
                    mk.op("act", "activation", out=r[i][:, 0:tw], in_=r[i][:, 0:tw], func=AF.Sqrt, R=[f"r{i}"], W=[f"r{i}"])
                    mk.op("dve", "tensor_tensor", out=gi[i][:, 0:tw], in0=gi[i][:, 0:tw], in1=r[i][:, 0:tw], op=ALU.mult,
                          R=[f"gi{i}", f"r{i}"], W=[f"gi{i}"])
                    mk.op("pool", "tensor_tensor", out=bt[:, t0:t0 + tw], in0=gi[i][:, 0:tw], in1=u[:, t0:t0 + tw],
                          op=ALU.mult, R=[f"gi{i}", "u"], W=["bt"])
                if d == 0:
                    mk.op("dve", "tensor_tensor_scan", out=hf[:, :], data0=aa[:, :], data1=bt[:, :], initial=0.0,
                          op0=ALU.mult, op1=ALU.add, R=["aa", "bt"], W=["hf"])
                else:
                    mk.op("dve", "tensor_tensor_scan", out=hb[:, 0:256][:, ::-1], data0=aa[:, 0:256][:, ::-1],
                          data1=bt[:, 0:256][:, ::-1], initial=0.0, op0=ALU.mult, op1=ALU.add, R=["aa", "bt"], W=["hb"])
                    mk.op("dve", "tensor_tensor_scan", out=hb[:, 256:T][:, ::-1], data0=aa[:, 256:T][:, ::-1],
                          data1=bt[:, 256:T][:, ::-1], initial=hb[:, 0:1], op0=ALU.mult, op1=ALU.add,
                          R=["aa", "bt", "hb"], W=["hb"])
            mk.op("act", "activation", out=ag[:], in_=ag[:], func=AF.Gelu, R=["ag"], W=["ag"])
            mk.op("dve", "tensor_tensor", out=hf[:], in0=hf[:], in1=hb[:], op=ALU.add, R=["hf", "hb"], W=["hf"])
            mk.op("dve", "tensor_tensor", out=yb[:], in0=hf[:], in1=ag[:], op=ALU.mult, R=["hf", "ag"], W=["yb"])
            mk.dma("pool", YT[0, c], yb[:], R=["yb"], W=[f"YT0{c}"])
        mk.barrier()
        P.close()

    def order_of(dr):
        return list(range(NT)) if dr == 0 else [1, 0] + list(range(NT - 1, 1, -1))

    def run_pipelined(gens, pipelined=True):
        if not pipelined:
            for g in gens:
                for _ in g:
                    pass
            return
        prev = None
        for g in gens:
            next(g, None)
            if prev is not None:
                for _ in prev:
                    pass
            prev = g
        if prev is not None:
            for _ in prev:
                pass

    OUT_T = list(range(2, 2 + HALF_OUT // 128))

    def plan(dr, last):
        if not last:
            return [(t, True) for t in order_of(dr)]
        if dr == 0:
            return [(0, False), (1, False)] + [(t, True) for t in OUT_T]
        return [(t, (t in OUT_T)) for t in order_of(dr)]

    def chunk_core(it, nsub, dr, QT, KT, QIT, KHs, V, vw, Gc, S, Sbf, maskD, maskkey, rkeys, PT, sk, full=True):
        pi = it % 2
        L = 128 // nsub
        okeys = ["ps2", "ps3"]
        Oh = [PS[2 + hh][:, 0:2 * vw].rearrange("p (c e) -> p c e", c=2) for hh in range(2)]
        if not DBG["core"]:
            return okeys, Oh
        for h in (DBG["heads"] if full else ()):
            c, hh = h // 2, h % 2
            rs = slice(hh * 64, (hh + 1) * 64)
            mk.op("pe", "matmul", PS[hh][:, c * 128:(c + 1) * 128], lhsT=KT[c][rs, :], rhs=QT[c][rs, :], start=True,
                  stop=True, R=rkeys, W=[f"ps{hh}"])
        PTv = PT[pi][:].rearrange("p (c x n) -> p c x n", c=2, x=2)
        Mv = maskD.rearrange("p (c x n) -> p c x n", c=2, x=2) if full else None
        for hh in (range(2) if full else ()):
            mk.op("dve", "tensor_tensor", out=PTv[:, :, hh, :], in0=PS[hh][:, 0:256].rearrange("p (c n) -> p c n", c=2),
                  in1=Mv[:, :, hh, :], op=ALU.mult, R=[f"ps{hh}", maskkey], W=[f"PT{pi}h{hh}"])
        ptk = [f"PT{pi}h0", f"PT{pi}h1"]
        subs = list(range(nsub)) if dr == 0 else list(range(nsub - 1, -1, -1))
        KVp = PS[4][:, 0:2 * vw].rearrange("p (c e) -> p c e", c=2)
        for s in subs:
            rows = slice(s * L, (s + 1) * L)
            for h in (DBG["heads"] if full else ()):
                c, hh = h // 2, h % 2
                rs = slice(hh * 64, (hh + 1) * 64)
                mk.op("pe", "matmul", Oh[hh][rows, c, :], lhsT=PT[pi][:, h * 128 + s * L:h * 128 + (s + 1) * L],
                      rhs=V[:, h, :], start=True, stop=False, R=[ptk[hh]] + rkeys, W=[okeys[hh]])
                mk.op("pe", "matmul", Oh[hh][rows, c, :], lhsT=QIT[c][rs, rows], rhs=Sbf[c][rs, :], start=False, stop=True,
                      R=rkeys + [f"{sk}Sbf{c}"], W=[okeys[hh]])
            for h in DBG["heads"]:
                c, hh = h // 2, h % 2
                mk.op("pe", "matmul", KVp[hh * 64:(hh + 1) * 64, c, :], lhsT=KHs(s)[:, h * 64:(h + 1) * 64],
                      rhs=V[:, h, :], start=True, stop=True, R=rkeys, W=["ps4kv"])
            for c in range(2):
                mk.op("dve", "scalar_tensor_tensor", out=S[c][:], in0=S[c][:], scalar=Gc[:, c, s:s + 1], in1=KVp[:, c, :],
                      op0=ALU.mult, op1=ALU.add, R=[f"{sk}S{c}", "ps4kv"] + rkeys, W=[f"{sk}S{c}"])
                mk.op("act", "activation", out=Sbf[c][:], in_=S[c][:], func=AF.Copy, R=[f"{sk}S{c}"], W=[f"{sk}Sbf{c}"])
        return okeys, Oh

    def hview(ap256, hh):
        return ap256.rearrange("p (c x e) -> p c x e", c=2, x=2)[:, :, hh, :]

    def make_finalize(P, n, yTb):
        st = dict(i=0)
        cent = [P.sb([128, 4, 64], F32, "cent") for _ in range(2)]
        sq = P.sb([128, 4, 64], F32, "sq")
        mm = [P.sb([128, 4], F32, "mm") for _ in range(2)]
        vv = [P.sb([128, 4], F32, "vv") for _ in range(2)]
        yy = [P.sb([128, 256], BF16, "yy") for _ in range(2)]

        def fin(tot, totkey, center, gate, gatekey, t):
            i = st["i"] % 2
            st["i"] += 1
            tv = tot.rearrange("p (h e) -> p h e", h=4)
            tk = list(totkey) if isinstance(totkey, (list, tuple)) else [totkey]
            src, skeys = tv, tk
            if center:
                mk.op("dve", "tensor_reduce", out=mm[i][:], in_=tv, axis=AX.X, op=ALU.add, R=tk, W=[f"fmm{i}"])
                mk.op("dve", "tensor_scalar", out=mm[i][:], in0=mm[i][:], scalar1=-1.0 / 64, scalar2=None, op0=ALU.mult,
                      R=[f"fmm{i}"], W=[f"fmm{i}"])
                mk.op("dve", "tensor_tensor", out=cent[i][:], in0=tv, in1=mm[i][:].unsqueeze(2).to_broadcast([128, 4, 64]),
                      op=ALU.add, R=tk + [f"fmm{i}"], W=[f"fcent{i}"])
                src, skeys = cent[i][:], [f"fcent{i}"]
            mk.op("pool", "tensor_tensor", out=sq[:], in0=src, in1=src, op=ALU.mult, R=skeys, W=["fsq"])
            mk.op("dve", "tensor_reduce", out=vv[i][:], in_=sq[:], axis=AX.X, op=ALU.add, R=["fsq"], W=[f"fvv{i}"])
            mk.op("act", "activation", out=vv[i][:], in_=vv[i][:], func=AF.Sqrt, scale=1.0 / 64, bias=EPS,
                  R=[f"fvv{i}"], W=[f"fvv{i}"])
            mk.op("dve", "reciprocal", out=vv[i][:], in_=vv[i][:], R=[f"fvv{i}"], W=[f"fvv{i}"])
            mk.op("dve", "tensor_tensor", out=cent[i][:], in0=src, in1=vv[i][:].unsqueeze(2).to_broadcast([128, 4, 64]),
                  op=ALU.mult, R=skeys + [f"fvv{i}"], W=[f"fcent{i}"])
            mk.op("dve", "tensor_tensor", out=yy[i][:], in0=cent[i][:].rearrange("p h e -> p (h e)"), in1=gate,
                  op=ALU.mult, R=[f"fcent{i}", gatekey], W=[f"fyy{i}"])
            for c in range(2):
                mk.op("pe", "transpose", out=PQ[1][:, (i * 2 + c) * 128:(i * 2 + c + 1) * 128],
                      in_=yy[i][:, c * 128:(c + 1) * 128], identity=identb[:], R=[f"fyy{i}", "identb"], W=[f"pq1f{i}"])
            mk.op("act", "activation", out=yTb[:, :, t * 128:(t + 1) * 128],
                  in_=PQ[1][:, i * 256:(i + 1) * 256].rearrange("p (c n) -> p c n", c=2), func=AF.Copy,
                  R=[f"pq1f{i}"], W=["yTb"])
        return fin

    def mixer_b(l, last=False):
        P = Pool(nc, f"mb{l}")
        QR = [P.sb([128, T], BF16, "QR") for _ in range(2)]
        KR = [P.sb([128, T], BF16, "KR") for _ in range(2)]
        thp = P.sb([128, 2, 2], F32, "thp")
        thh = P.sb([128, 2, 4], F32, "thh")
        mk.dma("sp", thp[:], b_thp[l], W=["thp"])
        mk.dma("sp", thh[:], b_thh[l], W=["thh"])
        for tt, key in ((thp, "thp"), (thh, "thh")):
            mk.op("act", "activation", out=tt[:], in_=tt[:], func=AF.Exp, scale=-1.0, R=[key], W=[key])
            mk.op("act", "activation", out=tt[:], in_=tt[:], func=AF.Ln, bias=1.0, R=[key], W=[key])
            mk.op("dve", "tensor_scalar", out=tt[:], in0=tt[:], scalar1=-1.0, scalar2=None, op0=ALU.mult, R=[key], W=[key])
        DM = P.sb([128, 2, 512], F32, "DM")
        QW = P.sb([128, 2, 2, 128], F32, "QW")
        KW = P.sb([128, 2, 4], F32, "KW")
        Gc = P.sb([128, 2, 2, 1], F32, "Gc")
        for dr in range(2):
            diff, msk, pos = (C("diffF"), C("maskF"), C("posF")) if dr == 0 else (C("diffB"), C("maskB"), C("posB"))
            for h in range(4):
                mk.op("act", "activation", out=DM[:, dr, h * 128:(h + 1) * 128], in_=diff, func=AF.Exp,
                      scale=thh[:, dr, h:h + 1], R=["cst", "thh"], W=["DM"])
                mk.op("dve", "tensor_tensor", out=DM[:, dr, h * 128:(h + 1) * 128], in0=DM[:, dr, h * 128:(h + 1) * 128],
                      in1=msk, op=ALU.mult, R=["DM", "cst"], W=["DM"])
                mk.op("act", "activation", out=KW[:, dr, h:h + 1], in_=C("kpos")[:, dr:dr + 1], func=AF.Exp,
                      scale=thh[:, dr, h:h + 1], R=["cst", "thh"], W=["KW"])
            for c in range(2):
                mk.op("act", "activation", out=QW[:, dr, c, :], in_=pos, func=AF.Exp, scale=thp[:, dr, c:c + 1],
                      R=["cst", "thp"], W=["QW"])
                mk.op("act", "activation", out=Gc[:, dr, c, :], in_=thp[:, dr, c:c + 1], func=AF.Exp, scale=128.0,
                      R=["thp"], W=["Gc"])
        segw = 1088
        f1 = [P.sb([128, segw], F32, "f1") for _ in range(2)]
        f2 = [P.sb([128, segw], F32, "f2") for _ in range(2)]
        rc = P.sb([128, T], F32, "rc")
        rsn = P.sb([128, T], F32, "rsn")
        mk.dma("sp", rc[:], ropeC_d, W=["rc"])
        mk.dma("sp", rsn[:], ropeS_d, W=["rsn"])
        n = 0
        for (dst, base, pbase, scale) in ((QR, "b_q", "b_qp", 1.0), (KR, "b_k", "b_kp", 0.125)):
            for c in range(2):
                for sg in range(4):
                    i = n % 2
                    n += 1
                    cs = slice(sg * segw, (sg + 1) * segw)
                    mk.dma("sp", f1[i][:], FM[FM_OFF[base] + c][:, cs], R=[f"FM{FM_OFF[base] + c}"], W=[f"f1{i}"])
                    mk.dma("sp", f2[i][:], FM[FM_OFF[pbase] + c][:, cs], R=[f"FM{FM_OFF[pbase] + c}"], W=[f"f2{i}"])
                    mk.op("dve", "tensor_tensor", out=f1[i][:], in0=f1[i][:], in1=rc[:, cs], op=ALU.mult,
                          R=[f"f1{i}", "rc"], W=[f"f1{i}"])
                    mk.op("pool", "tensor_tensor", out=f2[i][:], in0=f2[i][:], in1=rsn[:, cs], op=ALU.mult,
                          R=[f"f2{i}", "rsn"], W=[f"f2{i}"])
                    mk.op("dve", "tensor_tensor", out=f1[i][:], in0=f1[i][:], in1=f2[i][:], op=ALU.add,
                          R=[f"f1{i}", f"f2{i}"], W=[f"f1{i}"])
                    mk.op("act", "activation", out=dst[c][:, cs], in_=f1[i][:], func=AF.Copy, scale=scale,
                          R=[f"f1{i}"], W=[f"b{base}{c}"])
        rkeys = ["bb_q0", "bb_q1", "bb_k0", "bb_k1"]
        OF = P.sb([128, NT, 256], F32, "OF")
        yTb = P.sb([128, 2, T], BF16, "yTb")
        fin = make_finalize(P, 1, yTb)
        PT = [P.sb([128, 512], BF16, "PT") for _ in range(2)]
        QIT = [[P.sb([128, 128], BF16, "QIT") for _ in range(2)] for _ in range(2)]
        KH = [P.sb([128, 256], BF16, "KH") for _ in range(2)]
        Vf = [P.sb([128, 512], F32, "Vf") for _ in range(2)]
        Vb = [P.sb([128, 4, 64], BF16, "Vb") for _ in range(2)]
        gt = [P.sb([128, 256], F32, "gt") for _ in range(2)]
        tot = [P.sb([128, 256], F32, "tot") for _ in range(2)]
        S = [P.sb([128, 64], F32, "S") for _ in range(2)]
        Sbf = [P.sb([128, 64], BF16, "Sbf") for _ in range(2)]
        it = 0
        for dr in range(2):
            for c in range(2):
                mk.op("pool", "memset", S[c][:], 0.0, W=[f"bS{c}"])
                mk.op("pool", "memset", Sbf[c][:], 0.0, W=[f"bSbf{c}"])
            def body(t, full, it):
                i = it % 2
                cols = slice(t * 128, (t + 1) * 128)
                mk.dma("sp", Vf[i][:], TM[t * 128:(t + 1) * 128, 0:512], R=["TM"], W=[f"bVf{i}"])
                mk.op("pool", "tensor_copy", out=Vb[i][:], in_=Vf[i][:, 0:256].rearrange("p (h e) -> p h e", h=4),
                      R=[f"bVf{i}"], W=[f"bVb{i}"])
                for c in range(2):
                    if full:
                        mk.op("pool", "tensor_tensor", out=QIT[i][c][:], in0=QR[c][:, cols], in1=QW[:, dr, c, :],
                              op=ALU.mult, R=[f"bb_q{c}", "QW"], W=[f"bQIT{i}"])
                    mk.op("pe", "transpose", out=PQ[0][:, (i * 2 + c) * 128:(i * 2 + c + 1) * 128], in_=KR[c][:, cols],
                          identity=identb[:], R=[f"bb_k{c}", "identb"], W=[f"pq0k{i}"])
                for h in range(4):
                    mk.op("act", "activation", out=KH[i][:, h * 64:(h + 1) * 64],
                          in_=PQ[0][:, i * 256 + h * 64:i * 256 + (h + 1) * 64], func=AF.Identity, scale=KW[:, dr, h:h + 1],
                          R=[f"pq0k{i}", "KW"], W=[f"bKH{i}"])
                yield
                okeys, Oh = chunk_core(it, 1, dr, [QR[0][:, cols], QR[1][:, cols]], [KR[0][:, cols], KR[1][:, cols]],
                                       [QIT[i][0], QIT[i][1]], (lambda s_, kh=KH[i]: kh), Vb[i], 64, Gc[:, dr], S, Sbf,
                                       DM[:, dr, :], "DM", rkeys + [f"bQIT{i}", f"bKH{i}", f"bVb{i}"], PT, "b", full=full)
                if not full:
                    pass
                elif dr == 0:
                    for hh in range(2):
                        mk.op("act", "activation", out=hview(OF[:, t, :], hh), in_=Oh[hh], func=AF.Copy, R=[okeys[hh]],
                              W=[f"bOF{t}h{hh}"])
                else:
                    for hh in range(2):
                        mk.op("dve", "tensor_tensor", out=hview(tot[i][:], hh), in0=Oh[hh], in1=hview(OF[:, t, :], hh),
                              op=ALU.add, R=[okeys[hh], f"bOF{t}h{hh}"], W=[f"btot{i}h{hh}"])
                    mk.op("act", "activation", out=gt[i][:], in_=Vf[i][:, 256:512], func=AF.Silu, R=[f"bVf{i}"],
                          W=[f"bgt{i}"])
                    fin(tot[i][:], [f"btot{i}h0", f"btot{i}h1"], True, gt[i][:], f"bgt{i}", t)
            gens = []
            for t, full in plan(dr, last):
                gens.append(body(t, full, it))
                it += 1
            run_pipelined(gens, pipelined=not last)
        for c in range(2):
            mk.dma("pool", YT[1, c], yTb[:, c, :], R=["yTb"], W=[f"YT1{c}"])
        mk.barrier()
        P.close()

    def mixer_c(l, last=False):
        P = Pool(nc, f"mc{l}")
        QC = [P.sb([128, T], BF16, "QC") for _ in range(2)]
        KC = [P.sb([128, T], BF16, "KC") for _ in range(2)]
        cw = P.sb([128, 4, 5], F32, "cw")
        cb = P.sb([128, 4], F32, "cb")
        gbias = P.sb([128, 16], F32, "gbias")
        mk.dma("sp", cw[:], c_cw[l], W=["c_cw"])
        mk.dma("sp", cb[:], c_cb[l], W=["c_cb"])
        mk.dma("sp", gbias[:], c_gb[l], W=["c_gb"])
        src = P.sb([128, T], F32, "src")
        u = P.sb([128, T], F32, "u")
        for ch in range(4):
            fc = FM_OFF["c_q"] + ch
            mk.dma("sp", src[:], FM[fc], R=[f"FM{fc}"], W=["csrc"])
            conv_fm(u, src, cw[:, ch, :], cb[:, ch:ch + 1], "cu", "csrc", ["c_cw", "c_cb"])
            dst = QC[ch] if ch < 2 else KC[ch - 2]
            mk.op("act", "activation", out=u[:], in_=u[:], func=AF.Silu, R=["cu"], W=["cu"])
            mk.op("dve", "tensor_scalar", out=dst[:], in0=u[:], scalar1=(1.0 if ch < 2 else 0.125), scalar2=None,
                  op0=ALU.mult, R=["cu"], W=[f"cqk{ch}"])
        Z = P.sb([128, NT, 16], F32, "Z")
        LFN = P.sb([128, NT, 16], F32, "LFN")
        mk.dma("sp", Z[:], TM[:, TM_OFF["c_gates"]:TM_OFF["c_gates"] + 16].rearrange("(t p) g -> p t g", p=128),
               R=["TM"], W=["cZ"])
        mk.op("dve", "tensor_tensor", out=Z[:], in0=Z[:], in1=gbias[:].unsqueeze(1).to_broadcast([128, NT, 16]),
              op=ALU.add, R=["cZ", "c_gb"], W=["cZ"])
        mk.op("act", "activation", out=LFN[:], in_=Z[:], func=AF.Exp, scale=-1.0, R=["cZ"], W=["cLFN"])
        mk.op("act", "activation", out=LFN[:], in_=LFN[:], func=AF.Ln, bias=1.0, R=["cLFN"], W=["cLFN"])
        mk.op("dve", "tensor_scalar", out=LFN[:], in0=LFN[:], scalar1=-1.0, scalar2=None, op0=ALU.mult, R=["cLFN"],
              W=["cLFN"])
        rkeys = ["cqk0", "cqk1", "cqk2", "cqk3"]
        OF = P.sb([128, NT, 256], F32, "OF")
        yTb = P.sb([128, 2, T], BF16, "yTb")
        fin = make_finalize(P, 2, yTb)
        PT = [P.sb([128, 512], BF16, "PT") for _ in range(2)]
        QIT = [[P.sb([128, 128], BF16, "QIT") for _ in range(2)] for _ in range(2)]
        KH = [P.sb([128, 256], BF16, "KH") for _ in range(2)]
        Vf = [P.sb([128, 512], F32, "Vf") for _ in range(2)]
        Vb = [P.sb([128, 4, 65], BF16, "Vb") for _ in range(2)]
        gt = [P.sb([128, 256], F32, "gt") for _ in range(2)]
        tot = [P.sb([128, 256], F32, "tot") for _ in range(2)]
        S = [P.sb([128, 65], F32, "S") for _ in range(2)]
        Sbf = [P.sb([128, 65], BF16, "Sbf") for _ in range(2)]
        Bm4 = [P.sb([128, 4, 128], F32, "Bm4") for _ in range(2)]
        tmp4 = [P.sb([128, 512], F32, "tmp4") for _ in range(2)]
        Dm4 = [P.sb([128, 512], F32, "Dm4") for _ in range(2)]
        EB4 = [P.sb([128, 512], F32, "EB4") for _ in range(2)]
        lmb = [P.sb([128, 4], F32, "lmb") for _ in range(2)]
        kw = [P.sb([128, 4], F32, "kw") for _ in range(2)]
        Gc = [P.sb([128, 2, 1], F32, "Gc") for _ in range(2)]
        rden = [P.sb([128, 4], F32, "rden") for _ in range(2)]
        ebe = [P.sb([128, 4], F32, "ebe") for _ in range(2)]
        hid = [P.sb([128, 4, 64], F32, "hid") for _ in range(2)]
        for i in range(2):
            mk.op("pool", "memset", Vb[i][:], 1.0, W=[f"cVb{i}"])
        ones = C("ones")
        it = 0
        for dr in range(2):
            tri = C("triF") if dr == 0 else C("triB")
            neg4 = C("negF4") if dr == 0 else C("negB4")
            e = 127 if dr == 0 else 0
            for c in range(2):
                mk.op("pool", "memset", S[c][:], 0.0, W=[f"cS{c}"])
                mk.op("pool", "memset", Sbf[c][:], 0.0, W=[f"cSbf{c}"])
            def body(t, full, it):
                i = it % 2
                cols = slice(t * 128, (t + 1) * 128)
                li = Z[:, t, dr * 8:dr * 8 + 4]
                lf = LFN[:, t, dr * 8 + 4:dr * 8 + 8]
                mk.dma("sp", Vf[i][:], TM[t * 128:(t + 1) * 128, 512:1024], R=["TM"], W=[f"cVf{i}"])
                mk.op("pool", "tensor_copy", out=Vb[i][:, :, 0:64], in_=Vf[i][:, 0:256].rearrange("p (h e) -> p h e", h=4),
                      R=[f"cVf{i}"], W=[f"cVb{i}"])
                mk.op("dve", "tensor_tensor", out=Bm4[i][:], in0=tri.unsqueeze(1).to_broadcast([128, 4, 128]),
                      in1=lf.unsqueeze(2).to_broadcast([128, 4, 128]), op=ALU.mult, R=["cst", "cLFN"], W=[f"cBm{i}"])
                mk.op("pe", "matmul", PS[5][:, :], lhsT=ones, rhs=Bm4[i][:].rearrange("p h n -> p (h n)"), start=True,
                      stop=True, R=["cst", f"cBm{i}"], W=["ps5"])
                mk.op("pe", "matmul", PS[4][:, 256:260], lhsT=tri, rhs=lf, start=True, stop=True, R=["cst", "cLFN"],
                      W=["ps4b"])
                mk.op("dve", "tensor_tensor", out=lmb[i][:], in0=li, in1=PS[4][:, 256:260], op=ALU.subtract,
                      R=["cZ", "ps4b"], W=[f"clmb{i}"])
                if full:
                    mk.op("dve", "tensor_tensor", out=tmp4[i][:], in0=PS[5][:, :], in1=neg4, op=ALU.add, R=["ps5", "cst"],
                          W=[f"ctmp{i}"])
                    for h in range(4):
                        mk.op("act", "activation", out=Dm4[i][:, h * 128:(h + 1) * 128],
                              in_=tmp4[i][:, h * 128:(h + 1) * 128], func=AF.Exp, bias=lmb[i][:, h:h + 1],
                              R=[f"ctmp{i}", f"clmb{i}"], W=[f"cDm{i}"])
                    mk.op("act", "activation", out=EB4[i][:], in_=PS[5][:, :], func=AF.Exp, R=["ps5"], W=[f"cEB{i}"])
                bend = PS[5][:, :].rearrange("p (h n) -> p h n", h=4)[:, :, e]
                mk.op("dve", "tensor_tensor", out=kw[i][:], in0=lmb[i][:], in1=bend, op=ALU.add, R=[f"clmb{i}", "ps5"],
                      W=[f"ckw{i}"])
                mk.op("act", "activation", out=kw[i][:], in_=kw[i][:], func=AF.Exp, R=[f"ckw{i}"], W=[f"ckw{i}"])
                mk.op("act", "activation", out=ebe[i][:], in_=bend, func=AF.Exp, R=["ps5"], W=[f"cebe{i}"])
                for h in range(4):
                    c, hh = h // 2, h % 2
                    rs = slice(hh * 64, (hh + 1) * 64)
                    mk.op("pool", "tensor_copy", out=Gc[i][rs, c, :], in_=ebe[i][rs, h:h + 1],
                          R=[f"cebe{i}"], W=[f"cGc{i}"])
                    if full:
                        mk.op("pool", "tensor_tensor", out=QIT[i][c][rs, :], in0=QC[c][rs, cols],
                              in1=EB4[i][rs, h * 128:(h + 1) * 128], op=ALU.mult, R=[f"cqk{c}", f"cEB{i}"],
                              W=[f"cQIT{i}"])
                for c in range(2):
                    mk.op("pe", "transpose", out=PQ[0][:, (i * 2 + c) * 128:(i * 2 + c + 1) * 128], in_=KC[c][:, cols],
                          identity=identb[:], R=[f"cqk{2 + c}", "identb"], W=[f"pq0k{i}"])
                for h in range(4):
                    mk.op("act", "activation", out=KH[i][:, h * 64:(h + 1) * 64],
                          in_=PQ[0][:, i * 256 + h * 64:i * 256 + (h + 1) * 64], func=AF.Identity, scale=kw[i][:, h:h + 1],
                          R=[f"pq0k{i}", f"ckw{i}"], W=[f"cKH{i}"])
                yield
                okeys, Oh = chunk_core(it, 1, dr, [QC[0][:, cols], QC[1][:, cols]], [KC[0][:, cols], KC[1][:, cols]],
                                       [QIT[i][0], QIT[i][1]], (lambda s_, kh=KH[i]: kh), Vb[i], 65, Gc[i], S, Sbf,
                                       Dm4[i][:], f"cDm{i}",
                                       rkeys + [f"cQIT{i}", f"cKH{i}", f"cVb{i}", f"cGc{i}"], PT, "c", full=full)
                if not full:
                    return
                rdv = rden[i][:].rearrange("p (c x) -> p c x", c=2)
                for hh in range(2):
                    mk.op("act", "activation", out=rdv[:, :, hh], in_=Oh[hh][:, :, 64], func=AF.Abs, R=[okeys[hh]],
                          W=[f"crden{i}"])
                mk.op("dve", "tensor_scalar_max", out=rden[i][:], in0=rden[i][:], scalar1=1.0, R=[f"crden{i}"],
                      W=[f"crden{i}"])
                mk.op("dve", "reciprocal", out=rden[i][:], in_=rden[i][:], R=[f"crden{i}"], W=[f"crden{i}"])
                if dr == 0:
                    for hh in range(2):
                        mk.op("dve", "tensor_tensor", out=hview(OF[:, t, :], hh), in0=Oh[hh][:, :, 0:64],
                              in1=rdv[:, :, hh:hh + 1].to_broadcast([128, 2, 64]), op=ALU.mult,
                              R=[okeys[hh], f"crden{i}"], W=[f"cOF{t}h{hh}"])
                else:
                    for hh in range(2):
                        mk.op("dve", "tensor_tensor", out=hview(hid[i][:].rearrange("p h e -> p (h e)"), hh),
                              in0=Oh[hh][:, :, 0:64], in1=rdv[:, :, hh:hh + 1].to_broadcast([128, 2, 64]), op=ALU.mult,
                              R=[okeys[hh], f"crden{i}"], W=[f"chid{i}h{hh}"])
                    mk.op("pool", "tensor_tensor", out=tot[i][:], in0=hid[i][:].rearrange("p h e -> p (h e)"),
                          in1=OF[:, t, :], op=ALU.add, R=[f"chid{i}h0", f"chid{i}h1", f"cOF{t}h0", f"cOF{t}h1"],
                          W=[f"ctot{i}"])
                    mk.op("act", "activation", out=gt[i][:], in_=Vf[i][:, 256:512], func=AF.Sigmoid, R=[f"cVf{i}"],
                          W=[f"cgt{i}"])
                    fin(tot[i][:], f"ctot{i}", True, gt[i][:], f"cgt{i}", t)
            gens = []
            for t, full in plan(dr, last):
                gens.append(body(t, full, it))
                it += 1
            run_pipelined(gens, pipelined=not last)
        for c in range(2):
            mk.dma("pool", YT[2, c], yTb[:, c, :], R=["yTb"], W=[f"YT2{c}"])
        mk.barrier()
        P.close()

    def mixer_d(l, last=False):
        P = Pool(nc, f"md{l}")
        LB = P.sb([128, 256], F32, "LB")
        OML = P.sb([128, 256], F32, "OML")
        if l == 0:
            use_lb = False
        else:
            use_lb = True
            dl = P.sb([128, 2, 256], F32, "dl")
            mk.dma("sp", dl[:], d_lbr, W=["dl"])
            mk.op("dve", "tensor_tensor", out=LB[:], in0=dl[:, 1, :], in1=dl[:, 0, :], op=ALU.subtract, R=["dl"], W=["LB"])
            mk.op("act", "activation", out=LB[:], in_=LB[:], func=AF.Sigmoid, R=["LB"], W=["LB"])
            mk.op("dve", "tensor_scalar", out=OML[:], in0=LB[:], scalar1=-1.0, scalar2=1.0, op0=ALU.mult, op1=ALU.add,
                  R=["LB"], W=["OML"])
        OF = P.sb([128, NT, 256], F32, "OF")
        yTb = P.sb([128, 2, T], BF16, "yTb")
        fin = make_finalize(P, 3, yTb)
        assert DNS == 4
        PT = [P.sb([128, 512], BF16, "PT") for _ in range(2)]
        X = [P.sb([128, 1280], F32, "X") for _ in range(2)]
        ff = [P.sb([128, 256], F32, "ff") for _ in range(2)]
        lf = [P.sb([128, 256], F32, "lf") for _ in range(2)]
        kk = [P.sb([128, 256], F32, "kk") for _ in range(2)]
        qs = [P.sb([128, 256], F32, "qs") for _ in range(2)]
        ee = [P.sb([128, 512], F32, "ee") for _ in range(2)]
        ek = [P.sb([128, 256], F32, "ek") for _ in range(2)]
        qk = [P.sb([128, 512], BF16, "qk") for _ in range(2)]
        KTs = [P.sb([128, 2, 128], BF16, "KTs") for _ in range(2)]
        QM = [[P.sb([128, 2, 5, 128], BF16, "QM") for _ in range(2)] for _ in range(2)]
        KH = [P.sb([128, DNS, 256], BF16, "KH") for _ in range(2)]
        Vb = [P.sb([128, 4, 64], BF16, "Vb") for _ in range(2)]
        gt = [P.sb([128, 256], F32, "gt") for _ in range(2)]
        tot = [P.sb([128, 256], F32, "tot") for _ in range(2)]
        red = [P.sb([128, 2, 256], F32, "red") for _ in range(2)]
        Gc = [P.sb([128, 2, DNS], F32, "Gc") for _ in range(2)]
        S = [P.sb([128, 64], F32, "S") for _ in range(2)]
        Sbf = [P.sb([128, 64], BF16, "Sbf") for _ in range(2)]
        qmask = C("qmask").rearrange("p (x s n) -> p x s n", x=2, s=5)
        it = 0
        for dr in range(2):
            blk = C("blkF") if dr == 0 else C("blkB")
            rem = C("aftF") if dr == 0 else C("befB")
            msk4 = C("mblkF4") if dr == 0 else C("mblkB4")
            zoff = 256 if dr == 0 else 512
            subs = list(range(DNS)) if dr == 0 else list(range(DNS - 1, -1, -1))
            for c in range(2):
                mk.op("pool", "memset", S[c][:], 0.0, W=[f"dS{c}"])
                mk.op("pool", "memset", Sbf[c][:], 0.0, W=[f"dSbf{c}"])
            def body(t, full, it):
                i = it % 2
                mk.dma("sp", X[i][:], TM[t * 128:(t + 1) * 128, 1024:2304], R=["TM"], W=[f"dX{i}"])
                mk.op("act", "activation", out=ff[i][:], in_=X[i][:, zoff:zoff + 256], func=AF.Sigmoid, R=[f"dX{i}"],
                      W=[f"dff{i}"])
                if use_lb:
                    mk.op("dve", "tensor_tensor", out=ff[i][:], in0=ff[i][:], in1=OML[:], op=ALU.mult, R=[f"dff{i}", "OML"],
                          W=[f"dff{i}"])
                    mk.op("dve", "tensor_tensor", out=ff[i][:], in0=ff[i][:], in1=LB[:], op=ALU.add, R=[f"dff{i}", "LB"],
                          W=[f"dff{i}"])
                mk.op("act", "activation", out=lf[i][:], in_=ff[i][:], func=AF.Ln, R=[f"dff{i}"], W=[f"dlf{i}"])
                mk.op("pool", "tensor_scalar", out=kk[i][:], in0=ff[i][:], scalar1=-1.0, scalar2=1.0, op0=ALU.mult,
                      op1=ALU.add, R=[f"dff{i}"], W=[f"dkk{i}"])
                if full:
                    mk.op("pe", "matmul", PS[5][:, 0:256], lhsT=blk, rhs=lf[i][:], start=True, stop=True,
                          R=["cst", f"dlf{i}"], W=["ps5"])
                mk.op("pe", "matmul", PS[5][:, 256:512], lhsT=rem, rhs=lf[i][:], start=True, stop=True,
                      R=["cst", f"dlf{i}"], W=["ps5"])
                for c in range(2):
                    mk.op("pe", "matmul", PS[1][:, 384 + c * DNS:384 + (c + 1) * DNS], lhsT=lf[i][:, c * 128:(c + 1) * 128],
                          rhs=C("subm"), start=True, stop=True, R=["cst", f"dlf{i}"], W=["ps1g"])
                mk.op("act", "activation", out=Gc[i][:].rearrange("p c s -> p (c s)"), in_=PS[1][:, 384:384 + 2 * DNS],
                      func=AF.Exp, R=["ps1g"], W=[f"dGc{i}"])
                if full:
                    mk.op("act", "activation", out=ee[i][:], in_=PS[5][:, :], func=AF.Exp, R=["ps5"], W=[f"dee{i}"])
                    mk.op("act", "activation", out=ek[i][:], in_=PS[5][:, 0:256], func=AF.Exp, scale=-1.0, R=["ps5"],
                          W=[f"dek{i}"])
                    mk.op("act", "activation", out=qs[i][:], in_=X[i][:, 0:256], func=AF.Silu, R=[f"dX{i}"],
                          W=[f"dqs{i}"])
                    mk.op("dve", "tensor_tensor", out=qk[i][:, 0:256], in0=qs[i][:], in1=ee[i][:, 0:256], op=ALU.mult,
                          R=[f"dqs{i}", f"dee{i}"], W=[f"dqk{i}a"])
                    mk.op("dve", "tensor_tensor", out=qk[i][:, 256:512], in0=kk[i][:], in1=ek[i][:], op=ALU.mult,
                          R=[f"dkk{i}", f"dek{i}"], W=[f"dqk{i}c"])
                else:
                    mk.op("act", "activation", out=ee[i][:, 256:512], in_=PS[5][:, 256:512], func=AF.Exp, R=["ps5"],
                          W=[f"dee{i}"])
                for s_ in range(DNS):
                    mk.op("dve", "scalar_tensor_tensor", out=KH[i][:, s_, :], in0=kk[i][:], scalar=C("subm")[:, s_:s_ + 1],
                          in1=ee[i][:, 256:512], op0=ALU.mult, op1=ALU.mult, R=[f"dkk{i}", f"dee{i}", "cst"],
                          W=[f"dKH{i}s{s_}"])
                mk.op("pool", "tensor_copy", out=Vb[i][:], in_=X[i][:, 768:1024].rearrange("p (h e) -> p h e", h=4),
                      R=[f"dX{i}"], W=[f"dVb{i}"])
                if full:
                    for j in range(4):
                        mk.op("pe", "transpose", out=PQ[0][:, (i * 4 + j) * 128:(i * 4 + j + 1) * 128],
                              in_=qk[i][:, j * 128:(j + 1) * 128], identity=identb[:], R=[f"dqk{i}a", f"dqk{i}c", "identb"],
                              W=[f"pq0d{i}"])
                    mk.op("act", "activation", out=KTs[i][:].rearrange("p c n -> p (c n)"),
                          in_=PQ[0][:, i * 512 + 256:i * 512 + 512], func=AF.Copy, R=[f"pq0d{i}"], W=[f"dKT{i}"])
                    for c in range(2):
                        mk.op("dve", "tensor_tensor", out=QM[i][c][:].rearrange("p x s n -> p (x s) n"),
                              in0=PQ[0][:, i * 512 + c * 128:i * 512 + (c + 1) * 128].unsqueeze(1).to_broadcast([128, 10, 128]),
                              in1=qmask.rearrange("p x s n -> p (x s) n"), op=ALU.mult, R=[f"pq0d{i}", "cst"],
                              W=[f"dQM{i}{c}"])
                yield
                if full:
                    for h in range(4):
                        c, hh = h // 2, h % 2
                        mk.op("pe", "matmul", PS[0][:, h * 128:(h + 1) * 128], lhsT=KTs[i][:, c, :], rhs=QM[i][c][:, hh, 4, :],
                              start=True, stop=True, R=[f"dKT{i}", f"dQM{i}{c}"], W=["ps0"])
                    mk.op("dve", "tensor_tensor", out=PT[i][:], in0=PS[0][:, :], in1=msk4, op=ALU.mult, R=["ps0", "cst"],
                          W=[f"dPT{i}"])
                    for h in range(4):
                        mk.op("pe", "matmul", PS[1][:, h * 64:(h + 1) * 64], lhsT=PT[i][:, h * 128:(h + 1) * 128],
                              rhs=Vb[i][:, h, :], start=True, stop=True, R=[f"dPT{i}", f"dVb{i}"], W=["ps1i"])
                KVp = PS[1][:, 256:384].rearrange("p (c e) -> p c e", c=2)
                for s_ in subs:
                    bank = PS[2 + s_ // 2]
                    for h in (range(4) if full else ()):
                        c, hh = h // 2, h % 2
                        col = ((s_ % 2) * 4 + h) * 64
                        mk.op("pe", "matmul", bank[:, col:col + 64], lhsT=QM[i][c][:, hh, s_, :], rhs=Sbf[c][:, :],
                              start=True, stop=True, R=[f"dQM{i}{c}", f"dSbf{c}"], W=[f"ps{2 + s_ // 2}"])
                    for h in range(4):
                        c, hh = h // 2, h % 2
                        mk.op("pe", "matmul", KVp[hh * 64:(hh + 1) * 64, c, :], lhsT=KH[i][:, s_, h * 64:(h + 1) * 64],
                              rhs=Vb[i][:, h, :], start=True, stop=True, R=[f"dKH{i}s{s_}", f"dVb{i}"], W=["ps1kv"])
                    for c in range(2):
                        mk.op("dve", "scalar_tensor_tensor", out=S[c][:], in0=S[c][:], scalar=Gc[i][:, c, s_:s_ + 1],
                              in1=KVp[:, c, :], op0=ALU.mult, op1=ALU.add, R=[f"dS{c}", "ps1kv", f"dGc{i}"], W=[f"dS{c}"])
                        mk.op("act", "activation", out=Sbf[c][:], in_=S[c][:], func=AF.Copy, R=[f"dS{c}"], W=[f"dSbf{c}"])
                if not full:
                    return
                for b_ in range(2):
                    mk.op("dve", "tensor_reduce", out=red[i][:, b_, :],
                          in_=PS[2 + b_][:, :].rearrange("p (s x) -> p x s", s=2), axis=AX.X, op=ALU.add,
                          R=[f"ps{2 + b_}"], W=[f"dred{i}{b_}"])
                mk.op("dve", "tensor_tensor", out=tot[i][:], in0=PS[1][:, 0:256], in1=red[i][:, 0, :], op=ALU.add,
                      R=["ps1i", f"dred{i}0"], W=[f"dtot{i}"])
                if dr == 0:
                    mk.op("pool", "tensor_tensor", out=OF[:, t, :], in0=tot[i][:], in1=red[i][:, 1, :], op=ALU.add,
                          R=[f"dtot{i}", f"dred{i}1"], W=[f"dOF{t}"])
                else:
                    mk.op("pool", "tensor_tensor", out=tot[i][:], in0=tot[i][:], in1=red[i][:, 1, :], op=ALU.add,
                          R=[f"dtot{i}", f"dred{i}1"], W=[f"dtot{i}"])
                    mk.op("dve", "tensor_tensor", out=tot[i][:], in0=tot[i][:], in1=OF[:, t, :], op=ALU.add,
                          R=[f"dtot{i}", f"dOF{t}"], W=[f"dtot{i}"])
                    mk.op("act", "activation", out=gt[i][:], in_=X[i][:, 1024:1280], func=AF.Silu, R=[f"dX{i}"],
                          W=[f"dgt{i}"])
                    fin(tot[i][:], f"dtot{i}", False, gt[i][:], f"dgt{i}", t)
            gens = []
            for t, full in plan(dr, last):
                gens.append(body(t, full, it))
                it += 1
            run_pipelined(gens, pipelined=not last)
        for c in range(2):
            mk.dma("pool", YT[3, c], yTb[:, c, :], R=["yTb"], W=[f"YT3{c}"])
        mk.barrier()
        P.close()

    NSMAX = (T + 4 * (SLOT - 1)) // SLOT
    H2U = dscr("H2U", [T, D], BF16)
    HS = dscr("HS", [NSMAX * SLOT, D], BF16)
    GWS = dscr("GWS", [NSMAX * SLOT, 4])
    OS = dscr("OS", [NSMAX * SLOT, D])
    W1B = dscr("W1B", [16, 128, 4096], BF16)
    W3B = dscr("W3B", [16, 128, 4096], BF16)
    W2B = dscr("W2B", [16, 128, 4096], BF16)

    def merge_moe_phase(l, last, tiles=None):
        if tiles is None:
            tiles = list(OUT_T) if last else list(range(NT))
        PO = Pool(nc, f"mo{l}")
        GOH = PO.sb([128, NT, 4], F32, "GOH")
        GW = PO.sb([128, NT, 4], F32, "GW")
        merge_part(l, tiles, GOH, GW)
        moe_sparse(l, tiles, GOH, GW)
        PO.close()

    def merge_part(l, tiles, GOH, GW):
        P = Pool(nc, f"mm{l}")
        wbr = P.sb([128, 8, D], BF16, "wbr")
        wo = P.sb([128, 8, D], BF16, "wo")
        wstage = P.sb([128, 8, 512], F32, "wstage")
        wcb = P.sb([128, 4096], BF16, "wcb")
        for half in range(2):
            mk.dma("sp", wstage[:], w_branch[l].rearrange("n (c p) f -> p (n c) f", p=128)[:, :, half * 512:(half + 1) * 512],
                   W=["wstage"])
            mk.ev(wbr[:, :, half * 512:(half + 1) * 512], wstage[:], R=["wstage"], W=["wbr"])
        for half in range(2):
            mk.dma("sp", wstage[:], w_out[l].rearrange("(c p) f -> p c f", p=128)[:, :, half * 512:(half + 1) * 512],
                   W=["wstage"])
            mk.ev(wo[:, :, half * 512:(half + 1) * 512], wstage[:], R=["wstage"], W=["wo"])
        tasks = []
        for e in range(16):
            tasks.append((moe_w1[l, e].rearrange("(c p) f -> p c f", p=128), wstage[:], W1B[e], f"W1B{e}"))
            tasks.append((moe_w3[l, e].rearrange("(c p) f -> p c f", p=128), wstage[:], W3B[e], f"W3B{e}"))
            tasks.append((moe_w2[l, e].rearrange("(c p) f -> p c f", p=128),
                          wstage[:].rearrange("p c f -> p (c f)").rearrange("p (c f) -> p c f", c=4), W2B[e], f"W2B{e}"))

        def do_task(k):
            src, stg, dst, key = tasks[k]
            mk.dma("sp", stg, src, W=["wstage"])
            mk.ev(wcb[:], wstage[:].rearrange("p c f -> p (c f)"), R=["wstage"], W=["wcb"])
            mk.dma("pool", dst, wcb[:], R=["wcb"], W=[key])
        wgr = P.sb([128, 8, 20], F32, "wgr")
        bgr = P.sb([1, 20], F32, "bgr")
        mk.dma("sp", wgr[:], moe_wgr[l].rearrange("(c p) f -> p c f", p=128), W=["wgr"])
        mk.dma("sp", bgr[:], moe_bgr[l], W=["bgr"])
        identf = C("ident")
        ones = C("ones")
        norm = make_norm(P, 1)
        xnew = [P.sb([128, D], F32, "xnew") for _ in range(2)]
        h2Tt = [P.sb([128, 8, 128], BF16, "h2Tt") for _ in range(2)]
        h2tok = [P.sb([128, D], BF16, "h2tok") for _ in range(2)]
        yt = [P.sb([128, 8, 128], BF16, "yt")] * 2
        mg = [P.sb([128, 4096], BF16, "mg")] * 2
        xt = [P.sb([128, D], F32, "xt")] * 2
        zz = P.sb([128, D], F32, "zz")
        zt = P.sb([128, D], F32, "zt")
        zb = P.sb([128, D], BF16, "zb")
        zT = P.sb([128, 8, 128], BF16, "zT")
        h2f = zz
        h2fT = zt[:, :].rearrange("p (c n) -> p c n", c=8)
        rt = {k: P.sb([128, w], F32, "rt" + k) for k, w in
              (("L", 20), ("gm", 1), ("goh", 4), ("ge", 4), ("gs", 1), ("el", 4), ("m1", 1), ("oh1", 4), ("e2", 4),
               ("m2", 1), ("oh2", 4), ("w1", 1), ("w2", 1), ("gw", 4))}
        nit = 0
        ntask = 0
        per_tile = -(-len(tasks) // len(tiles))
        for t in tiles:
            for _ in range(per_tile):
                if ntask < len(tasks):
                    do_task(ntask)
                    ntask += 1
            i = nit % 2
            nit += 1
            j = cond_of(t)
            cols = slice(t * 128, (t + 1) * 128)
            mk.dma("sp", yt[i][:], YT[:, :, :, cols].rearrange("n c p t -> p (n c) t"),
                   R=[f"YT{n}{c}" for n in range(4) for c in range(2)], W=["yt"])
            mk.dma("sp", mg[i][:], MG[cols, :], R=["MG"], W=["mg"])
            mk.dma("sp", xt[i][:], XR[cols, :], R=[f"XR{t}"], W=["mxt"])
            for n in range(4):
                for cb in range(2):
                    pb = (n * 2 + cb) % 2
                    for c in range(2):
                        mk.op("pe", "matmul", PS[pb][:, :], lhsT=yt[i][:, n * 2 + c, :],
                              rhs=wbr[:, n * 2 + c, cb * 512:(cb + 1) * 512], start=(c == 0), stop=(c == 1),
                              R=["yt", "wbr"], W=[f"ps{pb}"])
                    dst = zz if n == 0 else zt
                    dkey = "zz" if n == 0 else "zt"
                    mk.op("dve", "tensor_tensor", out=dst[:, cb * 512:(cb + 1) * 512], in0=PS[pb][:, :],
                          in1=mg[i][:, n * 1024 + cb * 512:n * 1024 + (cb + 1) * 512], op=ALU.mult,
                          R=[f"ps{pb}", "mg"], W=[dkey])
                    if n > 0:
                        mk.op("pool", "tensor_tensor", out=zz[:, cb * 512:(cb + 1) * 512],
                              in0=zz[:, cb * 512:(cb + 1) * 512], in1=zt[:, cb * 512:(cb + 1) * 512], op=ALU.add,
                              R=["zz", "zt"], W=["zz"])
            mk.op("act", "activation", out=zb[:], in_=zz[:], func=AF.Copy, R=["zz"], W=["zb"])
            for c in range(8):
                mk.op("pe", "transpose", out=PQ[1][:, c * 128:(c + 1) * 128], in_=zb[:, c * 128:(c + 1) * 128],
                      identity=identb[:], R=["zb", "identb"], W=["pq1"])
            mk.ev(zT[:].rearrange("p c n -> p (c n)"), PQ[1][:, :], R=["pq1"], W=["zT"])
            for cb in range(2):
                pb = 2 + cb
                for c in range(8):
                    mk.op("pe", "matmul", PS[pb][:, :], lhsT=zT[:, c, :], rhs=wo[:, c, cb * 512:(cb + 1) * 512],
                          start=(c == 0), stop=(c == 7), R=["zT", "wo"], W=[f"ps{pb}"])
                mk.op("dve", "tensor_tensor", out=zt[:, cb * 512:(cb + 1) * 512], in0=PS[pb][:, :],
                      in1=GB[:, j, 0, cb * 512:(cb + 1) * 512], op=ALU.mult, R=[f"ps{pb}", "GB"], W=["zt"])
                mk.op("pool", "tensor_tensor", out=xnew[i][:, cb * 512:(cb + 1) * 512], in0=zt[:, cb * 512:(cb + 1) * 512],
                      in1=xt[i][:, cb * 512:(cb + 1) * 512], op=ALU.add, R=["zt", "mxt"], W=[f"xnew{i}"])
            mk.dma("pool", XR[cols, :], xnew[i][:], R=[f"xnew{i}"], W=[f"XR{t}"])
            norm(xnew[i][:], f"xnew{i}", j, 3, 2, h2Tt[i][:], f"h2Tt{i}")
            for c in range(8):
                mk.op("pe", "transpose", out=PQ[1][:, c * 128:(c + 1) * 128], in_=h2Tt[i][:, c, :], identity=identb[:],
                      R=[f"h2Tt{i}", "identb"], W=["pq1"])
            mk.ev(h2tok[i][:], PQ[1][:, :], R=["pq1"], W=[f"h2tok{i}"])
            mk.dma("pool", H2U[cols, :], h2tok[i][:], R=[f"h2tok{i}"], W=[f"H2U{t}"])
            ssr = rt["gs"]
            mk.op("act", "activation", out=h2f[:], in_=xnew[i][:], func=AF.Square, accum_out=ssr[:],
                  R=[f"xnew{i}"], W=["zz", "rgs"])
            mk.op("act", "activation", out=ssr[:], in_=ssr[:], func=AF.Sqrt, scale=1.0 / D, bias=EPS, R=["rgs"], W=["rgs"])
            mk.op("dve", "reciprocal", out=ssr[:], in_=ssr[:], R=["rgs"], W=["rgs"])
            mk.op("dve", "tensor_scalar", out=h2f[:], in0=xnew[i][:], scalar1=ssr[:, 0:1], scalar2=None,
                  op0=ALU.mult, R=[f"xnew{i}", "rgs", "zz"], W=["zz"])
            for half in range(2):
                for c4 in range(4):
                    c = half * 4 + c4
                    mk.op("pe", "transpose", out=PS[5][:, c4 * 128:(c4 + 1) * 128], in_=h2f[:, c * 128:(c + 1) * 128],
                          identity=identf, R=["zz", "cst"], W=["ps5"])
                mk.op("dve", "tensor_tensor", out=h2fT[:, half * 4:(half + 1) * 4, :],
                      in0=PS[5][:, :].rearrange("p (c n) -> p c n", c=4),
                      in1=MODC[:, 3, half * 4:(half + 1) * 4, j:j + 1].to_broadcast([128, 4, 128]), op=ALU.mult,
                      R=["ps5", "MODC"], W=["zt"])
                mk.op("pool", "tensor_tensor", out=h2fT[:, half * 4:(half + 1) * 4, :],
                      in0=h2fT[:, half * 4:(half + 1) * 4, :],
                      in1=MODC[:, 2, half * 4:(half + 1) * 4, j:j + 1].to_broadcast([128, 4, 128]), op=ALU.add,
                      R=["zt", "MODC"], W=["zt"])
            for c in range(8):
                mk.op("pe", "matmul", PS[4][:, 0:20], lhsT=h2fT[:, c, :], rhs=wgr[:, c, :], start=(c == 0), stop=False,
                      R=["zt", "wgr"], W=["ps4r"])
            mk.op("pe", "matmul", PS[4][:, 0:20], lhsT=ones[0:1, :], rhs=bgr[0:1, :], start=False, stop=True,
                  R=["cst", "bgr"], W=["ps4r"])
            Lg = rt["L"]
            mk.op("act", "activation", out=Lg[:], in_=PS[4][:, 0:20], func=AF.Copy, R=["ps4r"], W=["rL"])
            rk = ["rL"]
            mk.op("dve", "tensor_reduce", out=rt["gm"][:], in_=Lg[:, 0:4], axis=AX.X, op=ALU.max, R=rk, W=["rgm"])
            mk.op("dve", "tensor_scalar", out=rt["goh"][:], in0=Lg[:, 0:4], scalar1=rt["gm"][:, 0:1], scalar2=None,
                  op0=ALU.is_ge, R=rk + ["rgm"], W=["rgoh"])
            mk.op("dve", "tensor_scalar", out=rt["ge"][:], in0=Lg[:, 0:4], scalar1=rt["gm"][:, 0:1], scalar2=None,
                  op0=ALU.subtract, R=rk + ["rgm"], W=["rge"])
            mk.op("act", "activation", out=rt["ge"][:], in_=rt["ge"][:], func=AF.Exp, accum_out=rt["gs"][:],
                  R=["rge"], W=["rge", "rgs"])
            mk.op("dve", "reciprocal", out=rt["gs"][:], in_=rt["gs"][:], R=["rgs"], W=["rgs"])
            mk.op("dve", "tensor_scalar", out=rt["el"][:], in0=Lg[:, 4:8], scalar1=rt["goh"][:, 0:1], scalar2=None,
                  op0=ALU.mult, R=rk + ["rgoh"], W=["rel"])
            for g in range(1, 4):
                mk.op("dve", "scalar_tensor_tensor", out=rt["el"][:], in0=Lg[:, 4 + g * 4:8 + g * 4],
                      scalar=rt["goh"][:, g:g + 1], in1=rt["el"][:], op0=ALU.mult, op1=ALU.add,
                      R=rk + ["rgoh", "rel"], W=["rel"])
            mk.op("dve", "tensor_reduce", out=rt["m1"][:], in_=rt["el"][:], axis=AX.X, op=ALU.max, R=["rel"], W=["rm1"])
            mk.op("dve", "tensor_scalar", out=rt["oh1"][:], in0=rt["el"][:], scalar1=rt["m1"][:, 0:1], scalar2=None,
                  op0=ALU.is_ge, R=["rel", "rm1"], W=["roh1"])
            mk.op("dve", "scalar_tensor_tensor", out=rt["e2"][:], in0=rt["oh1"][:], scalar=-1e30, in1=rt["el"][:],
                  op0=ALU.mult, op1=ALU.add, R=["roh1", "rel"], W=["re2"])
            mk.op("dve", "tensor_reduce", out=rt["m2"][:], in_=rt["e2"][:], axis=AX.X, op=ALU.max, R=["re2"], W=["rm2"])
            mk.op("dve", "tensor_scalar", out=rt["oh2"][:], in0=rt["e2"][:], scalar1=rt["m2"][:, 0:1], scalar2=None,
                  op0=ALU.is_ge, R=["re2", "rm2"], W=["roh2"])
            mk.op("dve", "tensor_tensor", out=rt["w1"][:], in0=rt["m2"][:], in1=rt["m1"][:], op=ALU.subtract,
                  R=["rm1", "rm2"], W=["rw1"])
            mk.op("act", "activation", out=rt["w1"][:], in_=rt["w1"][:], func=AF.Exp, R=["rw1"], W=["rw1"])
            mk.op("dve", "tensor_scalar_add", out=rt["w1"][:], in0=rt["w1"][:], scalar1=1.0, R=["rw1"], W=["rw1"])
            mk.op("dve", "reciprocal", out=rt["w1"][:], in_=rt["w1"][:], R=["rw1"], W=["rw1"])
            mk.op("dve", "tensor_tensor", out=rt["w1"][:], in0=rt["w1"][:], in1=rt["gs"][:], op=ALU.mult,
                  R=["rw1", "rgs"], W=["rw1"])
            mk.op("dve", "tensor_tensor", out=rt["w2"][:], in0=rt["gs"][:], in1=rt["w1"][:], op=ALU.subtract,
                  R=["rw1", "rgs"], W=["rw2"])
            mk.op("dve", "tensor_scalar", out=rt["gw"][:], in0=rt["oh1"][:], scalar1=rt["w1"][:, 0:1], scalar2=None,
                  op0=ALU.mult, R=["roh1", "rw1"], W=["rgw"])
            mk.op("dve", "scalar_tensor_tensor", out=rt["gw"][:], in0=rt["oh2"][:], scalar=rt["w2"][:, 0:1],
                  in1=rt["gw"][:], op0=ALU.mult, op1=ALU.add, R=["roh2", "rw2", "rgw"], W=["rgw"])
            mk.op("dve", "tensor_copy", out=GOH[:, t, :], in_=rt["goh"][:], R=["rgoh"], W=[f"GOH{t}"])
            mk.op("dve", "tensor_copy", out=GW[:, t, :], in_=rt["gw"][:], R=["rgw"], W=[f"GW{t}"])
        while ntask < len(tasks):
            do_task(ntask)
            ntask += 1
        mk.barrier()
        P.close()

    def moe_sparse(l, tiles, GOH, GW):
        P = Pool(nc, f"ms{l}")
        I32 = mybir.dt.int32
        nt, t0 = len(tiles), tiles[0]
        N = nt * 128
        NS = (N + 4 * (SLOT - 1)) // SLOT
        TPS = SLOT // 128
        Gv = GOH[:, t0:t0 + nt, :]
        gkeys = [f"GOH{t}" for t in tiles]
        cnt = P.sb([128, nt, 4], F32, "cnt")
        inc = P.sb([128, nt, 4], F32, "inc")
        A = P.sb([128, nt, 4], F32, "A")
        tot = P.sb([128, 4], F32, "tot")
        cmp = P.sb([128, 16], F32, "cmp")
        nsl = P.sb([128, 4], F32, "nsl")
        base = P.sb([128, 4], F32, "base")
        posf = P.sb([128, nt], F32, "posf")
        POSI = P.sb([128, nt], I32, "POSI")
        gkb = P.sb([128, NS], F32, "gkb")
        gtmp = P.sb([128, NS], F32, "gtmp")
        widf = P.sb([128, NS, 4], F32, "widf")
        WIDX = P.sb([128, NS, 4], I32, "WIDX")
        Gf = Gv.rearrange("p t g -> p (t g)")
        mk.op("pe", "matmul", PS[0][:, 0:nt * 4], lhsT=C("ltS"), rhs=Gf, start=True, stop=True, R=gkeys + ["cst"], W=["ps0"])
        mk.op("pe", "matmul", PS[1][:, 0:nt * 4], lhsT=C("ones"), rhs=Gf, start=True, stop=True, R=gkeys + ["cst"], W=["ps1"])
        mk.op("act", "activation", out=cnt[:].rearrange("p t g -> p (t g)"), in_=PS[1][:, 0:nt * 4], func=AF.Copy,
              R=["ps1"], W=["scnt"])
        for g in range(4):
            mk.op("dve", "tensor_tensor_scan", out=inc[:, :, g], data0=C("ones")[:, 0:nt], data1=cnt[:, :, g], initial=0.0,
                  op0=ALU.mult, op1=ALU.add, R=["scnt", "cst"], W=[f"sinc{g}"])
        ik = [f"sinc{g}" for g in range(4)]
        mk.op("dve", "tensor_copy", out=tot[:], in_=inc[:, nt - 1, :], R=ik, W=["stot"])
        for g in range(4):
            mk.op("dve", "tensor_scalar", out=cmp[:], in0=C("thrS"), scalar1=tot[:, g:g + 1], scalar2=None, op0=ALU.is_lt,
                  R=["stot", "cst"], W=["scmp"])
            mk.op("dve", "tensor_reduce", out=nsl[:, g:g + 1], in_=cmp[:], axis=AX.X, op=ALU.add, R=["scmp"], W=["snsl"])
        mk.op("dve", "tensor_scalar", out=nsl[:], in0=nsl[:], scalar1=float(SLOT), scalar2=None, op0=ALU.mult,
              R=["snsl"], W=["snsl"])
        mk.op("pool", "memset", base[:], 0.0, W=["sbase"])
        for g in range(1, 4):
            mk.op("dve", "tensor_tensor", out=base[:, g:g + 1], in0=base[:, g - 1:g], in1=nsl[:, g - 1:g], op=ALU.add,
                  R=["sbase", "snsl"], W=["sbase"])
        mk.op("dve", "tensor_tensor", out=A[:], in0=inc[:], in1=cnt[:], op=ALU.subtract, R=ik + ["scnt"], W=["sA"])
        mk.op("dve", "tensor_tensor", out=A[:].rearrange("p t g -> p (t g)"), in0=A[:].rearrange("p t g -> p (t g)"),
              in1=PS[0][:, 0:nt * 4], op=ALU.add, R=["sA", "ps0"], W=["sA"])
        mk.op("dve", "tensor_tensor", out=A[:], in0=A[:], in1=base[:].unsqueeze(1).to_broadcast([128, nt, 4]), op=ALU.add,
              R=["sA", "sbase"], W=["sA"])
        mk.op("dve", "tensor_tensor", out=A[:], in0=A[:], in1=Gv, op=ALU.mult, R=["sA"] + gkeys, W=["sA"])
        mk.op("dve", "tensor_reduce", out=posf[:], in_=A[:], axis=AX.X, op=ALU.add, R=["sA"], W=["sposf"])
        mk.op("dve", "tensor_copy", out=POSI[:], in_=posf[:], R=["sposf"], W=["POSI"])
        mk.op("pool", "memset", gkb[:], 0.0, W=["sgkb"])
        for g in range(1, 4):
            mk.op("dve", "tensor_scalar", out=gtmp[:], in0=C("kS")[:, 0:NS], scalar1=base[:, g:g + 1], scalar2=None,
                  op0=ALU.is_ge, R=["sbase", "cst"], W=["sgtmp"])
            mk.op("dve", "tensor_tensor", out=gkb[:], in0=gkb[:], in1=gtmp[:], op=ALU.add, R=["sgkb", "sgtmp"], W=["sgkb"])
        mk.op("dve", "tensor_scalar", out=gkb[:], in0=gkb[:], scalar1=512.0, scalar2=None, op0=ALU.mult, R=["sgkb"],
              W=["sgkb"])
        for j in range(4):
            mk.op("dve", "tensor_scalar", out=widf[:, :, j], in0=gkb[:], scalar1=C("jp")[:, j:j + 1], scalar2=None,
                  op0=ALU.add, R=["sgkb", "cst"], W=["swidf"])
        mk.op("dve", "tensor_copy", out=WIDX[:], in_=widf[:], R=["swidf"], W=["WIDX"])
        hb = [P.sb([128, D], BF16, "hb") for _ in range(2)]
        for ti, t in enumerate(tiles):
            i = ti % 2
            mk.dma("sp", hb[i][:], H2U[t * 128:(t + 1) * 128, :], R=[f"H2U{t}"], W=[f"hb{i}"])
            mk.idma(HS[:, :], bass.IndirectOffsetOnAxis(ap=POSI[:, ti:ti + 1], axis=0), hb[i][:, :], None,
                    R=[f"hb{i}", "POSI"], W=[f"HSs{ti}"])
            mk.idma(GWS[:, :], bass.IndirectOffsetOnAxis(ap=POSI[:, ti:ti + 1], axis=0), GW[:, t, :], None,
                    R=[f"GW{t}", "POSI"], W=[f"GWs{ti}"])
        hsk = [f"HSs{ti}" for ti in range(nt)]
        gwk = [f"GWs{ti}" for ti in range(nt)]
        hs = [P.sb([128, TPS, D], BF16, "hs") for _ in range(2)]
        hTs = [P.sb([128, 8, SLOT], BF16, "hTs") for _ in range(2)]
        gws = [P.sb([128, TPS, 4], F32, "gws") for _ in range(2)]
        acc = [P.sb([128, TPS, D], F32, "acc") for _ in range(2)]
        w1b = [P.sb([128, 8, 512], BF16, "w1b") for _ in range(2)]
        w3b = [P.sb([128, 8, 512], BF16, "w3b") for _ in range(2)]
        w2b = [P.sb([128, 4, D], BF16, "w2b") for _ in range(2)]
        sl = [P.sb([128, SLOT], F32, "sl") for _ in range(2)]
        actT = [P.sb([128, 4, SLOT], BF16, "actT") for _ in range(2)]
        W1t = W1B.rearrange("e p f -> (e p) f")
        W3t = W3B.rearrange("e p f -> (e p) f")
        W2t = W2B.rearrange("e p f -> (e p) f")
        wkeys = [f"W{a}B{e}" for a in (1, 2, 3) for e in range(16)]
        nw = 0
        for k in range(NS):
            si = k % 2
            mk.dma("sp", hs[si][:], HS[k * SLOT:(k + 1) * SLOT, :].rearrange("(q p) f -> p q f", p=128), R=hsk,
                   W=[f"hs{si}"])
            mk.dma("sp", gws[si][:], GWS[k * SLOT:(k + 1) * SLOT, :].rearrange("(q p) f -> p q f", p=128), R=gwk,
                   W=[f"gws{si}"])
            for q in range(TPS):
                pq = q % 2
                for c in range(8):
                    mk.op("pe", "transpose", out=PQ[pq][:, c * 128:(c + 1) * 128], in_=hs[si][:, q, c * 128:(c + 1) * 128],
                          identity=identb[:], R=[f"hs{si}", "identb"], W=[f"pq{pq}"])
                mk.ev(hTs[si][:, :, q * 128:(q + 1) * 128], PQ[pq][:, :].rearrange("p (c n) -> p c n", c=8), R=[f"pq{pq}"],
                      W=[f"hTs{si}"])
            for j in range(4):
                wi = nw % 2
                nw += 1
                ioff = bass.IndirectOffsetOnAxis(ap=WIDX[:, k, j:j + 1], axis=0)
                mk.idma(w1b[wi][:].rearrange("p c f -> p (c f)"), None, W1t, ioff, R=["WIDX"] + wkeys, W=[f"w1b{wi}"])
                mk.idma(w3b[wi][:].rearrange("p c f -> p (c f)"), None, W3t, ioff, R=["WIDX"] + wkeys, W=[f"w3b{wi}"])
                mk.idma(w2b[wi][:].rearrange("p c f -> p (c f)"), None, W2t, ioff, R=["WIDX"] + wkeys, W=[f"w2b{wi}"])
                for fcn in range(4):
                    for kk_ in range(8):
                        mk.op("pe", "matmul", PS[0][:, 0:SLOT], lhsT=w1b[wi][:, kk_, fcn * 128:(fcn + 1) * 128],
                              rhs=hTs[si][:, kk_, :], start=(kk_ == 0), stop=(kk_ == 7), R=[f"w1b{wi}", f"hTs{si}"], W=["ps0"])
                    for kk_ in range(8):
                        mk.op("pe", "matmul", PS[1][:, 0:SLOT], lhsT=w3b[wi][:, kk_, fcn * 128:(fcn + 1) * 128],
                              rhs=hTs[si][:, kk_, :], start=(kk_ == 0), stop=(kk_ == 7), R=[f"w3b{wi}", f"hTs{si}"], W=["ps1"])
                    s2 = fcn % 2
                    mk.op("act", "activation", out=sl[s2][:], in_=PS[0][:, 0:SLOT], func=AF.Silu, R=["ps0"], W=[f"sl{s2}"])
                    mk.op("dve", "tensor_tensor", out=actT[wi][:, fcn, :], in0=sl[s2][:], in1=PS[1][:, 0:SLOT], op=ALU.mult,
                          R=[f"sl{s2}", "ps1"], W=[f"actT{wi}"])
                for q in range(TPS):
                    for cb in range(2):
                        pb = 2 + cb
                        for fcn in range(4):
                            mk.op("pe", "matmul", PS[pb][:, :], lhsT=actT[wi][:, fcn, q * 128:(q + 1) * 128],
                                  rhs=w2b[wi][:, fcn, cb * 512:(cb + 1) * 512], start=(fcn == 0), stop=(fcn == 3),
                                  R=[f"actT{wi}", f"w2b{wi}"], W=[f"ps{pb}"])
                        if j == 0:
                            mk.op("dve", "tensor_scalar", out=acc[si][:, q, cb * 512:(cb + 1) * 512], in0=PS[pb][:, :],
                                  scalar1=gws[si][:, q, j:j + 1], scalar2=None, op0=ALU.mult,
                                  R=[f"ps{pb}", f"gws{si}"], W=[f"sacc{si}"])
                        else:
                            mk.op("dve", "scalar_tensor_tensor", out=acc[si][:, q, cb * 512:(cb + 1) * 512], in0=PS[pb][:, :],
                                  scalar=gws[si][:, q, j:j + 1], in1=acc[si][:, q, cb * 512:(cb + 1) * 512], op0=ALU.mult,
                                  op1=ALU.add, R=[f"ps{pb}", f"gws{si}", f"sacc{si}"], W=[f"sacc{si}"])
            mk.dma("sp", OS[k * SLOT:(k + 1) * SLOT, :].rearrange("(q p) f -> p q f", p=128), acc[si][:], R=[f"sacc{si}"],
                   W=[f"OS{k}"])
        osk = [f"OS{k}" for k in range(NS)]
        og = [P.sb([128, D], F32, "og") for _ in range(2)]
        xt = [P.sb([128, D], F32, "xt") for _ in range(2)]
        for ti, t in enumerate(tiles):
            i = ti % 2
            j = cond_of(t)
            mk.idma(og[i][:, :], None, OS[:, :], bass.IndirectOffsetOnAxis(ap=POSI[:, ti:ti + 1], axis=0), R=osk + ["POSI"],
                    W=[f"og{i}"])
            mk.dma("sp", xt[i][:], XR[t * 128:(t + 1) * 128, :], R=[f"XR{t}"], W=[f"ext{i}"])
            mk.op("pool", "tensor_tensor", out=og[i][:], in0=og[i][:], in1=GB[:, j, 1, :], op=ALU.mult, R=[f"og{i}", "GB"],
                  W=[f"og{i}"])
            mk.op("dve", "tensor_tensor", out=og[i][:], in0=og[i][:], in1=xt[i][:], op=ALU.add, R=[f"og{i}", f"ext{i}"],
                  W=[f"og{i}"])
            mk.dma("sp", XR[t * 128:(t + 1) * 128, :], og[i][:], R=[f"og{i}"], W=[f"XR{t}"])
        mk.barrier()
        P.close()

    def moe_part(l, tiles, h2T, gates):
        P = Pool(nc, f"me{l}")
        TB = 8
        blocks = [(tiles[k], min(TB, len(tiles) - k)) for k in range(0, len(tiles), TB)]
        acc = P.sb([128, TB, D], F32, "acc")
        xt = [P.sb([128, D], F32, "xt") for _ in range(2)]
        w1b = [P.sb([128, 8, 512], BF16, "w1b") for _ in range(2)]
        w3b = [P.sb([128, 8, 512], BF16, "w3b") for _ in range(2)]
        w2b = [P.sb([128, 4, D], BF16, "w2b") for _ in range(2)]
        sl = [P.sb([128, 512], F32, "sl") for _ in range(2)]
        actT = [P.sb([128, 4, 512], BF16, "actT") for _ in range(2)]
        nw = 0
        for (tb0, tn) in blocks:
            ntok = tn * 128
            sblocks = [(s0, min(512, ntok - s0)) for s0 in range(0, ntok, 512)]
            hk = [f"h2T{tb0 + tt}" for tt in range(tn)]
            for e in range(16):
                wi = nw % 2
                nw += 1
                mk.dma("sp", w1b[wi][:].rearrange("p c f -> p (c f)"), W1B[e], R=[f"W1B{e}"], W=[f"w1b{wi}"])
                mk.dma("sp", w3b[wi][:].rearrange("p c f -> p (c f)"), W3B[e], R=[f"W3B{e}"], W=[f"w3b{wi}"])
                mk.dma("sp", w2b[wi][:].rearrange("p c f -> p (c f)"), W2B[e], R=[f"W2B{e}"], W=[f"w2b{wi}"])
                for (s0, sw) in sblocks:
                    ai = (s0 // 512) % 2
                    h0 = tb0 * 128 + s0
                    for fcn in range(4):
                        for k in range(8):
                            mk.op("pe", "matmul", PS[0][:, 0:sw], lhsT=w1b[wi][:, k, fcn * 128:(fcn + 1) * 128],
                                  rhs=h2T[:, k, h0:h0 + sw], start=(k == 0), stop=(k == 7), R=[f"w1b{wi}"] + hk, W=["ps0"])
                        for k in range(8):
                            mk.op("pe", "matmul", PS[1][:, 0:sw], lhsT=w3b[wi][:, k, fcn * 128:(fcn + 1) * 128],
                                  rhs=h2T[:, k, h0:h0 + sw], start=(k == 0), stop=(k == 7), R=[f"w3b{wi}"] + hk, W=["ps1"])
                        si = fcn % 2
                        mk.op("act", "activation", out=sl[si][:, 0:sw], in_=PS[0][:, 0:sw], func=AF.Silu, R=["ps0"],
                              W=[f"sl{si}"])
                        mk.op("dve", "tensor_tensor", out=actT[ai][:, fcn, 0:sw], in0=sl[si][:, 0:sw], in1=PS[1][:, 0:sw],
                              op=ALU.mult, R=[f"sl{si}", "ps1"], W=[f"actT{ai}"])
                    for q in range(sw // 128):
                        tt = s0 // 128 + q
                        t = tb0 + tt
                        for cb in range(2):
                            pb = 2 + cb
                            for fcn in range(4):
                                mk.op("pe", "matmul", PS[pb][:, :], lhsT=actT[ai][:, fcn, q * 128:(q + 1) * 128],
                                      rhs=w2b[wi][:, fcn, cb * 512:(cb + 1) * 512], start=(fcn == 0), stop=(fcn == 3),
                                      R=[f"actT{ai}", f"w2b{wi}"], W=[f"ps{pb}"])
                            if e == 0:
                                mk.op("dve", "tensor_scalar", out=acc[:, tt, cb * 512:(cb + 1) * 512], in0=PS[pb][:, :],
                                      scalar1=gates[:, t, e:e + 1], scalar2=None, op0=ALU.mult,
                                      R=[f"ps{pb}", f"gates{t}"], W=[f"acc{tt}"])
                            else:
                                mk.op("dve", "scalar_tensor_tensor", out=acc[:, tt, cb * 512:(cb + 1) * 512],
                                      in0=PS[pb][:, :], scalar=gates[:, t, e:e + 1], in1=acc[:, tt, cb * 512:(cb + 1) * 512],
                                      op0=ALU.mult, op1=ALU.add, R=[f"ps{pb}", f"gates{t}", f"acc{tt}"], W=[f"acc{tt}"])
            for tt in range(tn):
                t = tb0 + tt
                j = cond_of(t)
                mk.op("pool", "tensor_tensor", out=acc[:, tt, :], in0=acc[:, tt, :], in1=GB[:, j, 1, :], op=ALU.mult,
                      R=[f"acc{tt}", "GB"], W=[f"acc{tt}"])
                i2 = tt % 2
                mk.dma("sp", xt[i2][:], XR[t * 128:(t + 1) * 128, :], R=[f"XR{t}"], W=[f"ext{i2}"])
                mk.op("dve", "tensor_tensor", out=acc[:, tt, :], in0=acc[:, tt, :], in1=xt[i2][:], op=ALU.add,
                      R=[f"acc{tt}", f"ext{i2}"], W=[f"acc{tt}"])
                mk.dma("pool", XR[t * 128:(t + 1) * 128, :], acc[:, tt, :], R=[f"acc{tt}"], W=[f"XR{t}"])
        mk.barrier()
        P.close()

    def final_phase():
        P = Pool(nc, "fin")
        fw = P.sb([128, D], F32, "fw")
        mk.dma("sp", fw[:], fnw, W=["fw"])
        xt = [P.sb([128, D], F32, "xt") for _ in range(2)]
        ot = [P.sb([128, D], F32, "ot") for _ in range(2)]
        junk = P.sb([128, D], BF16, "junk")
        ss = [P.sb([128, 1], F32, "ss") for _ in range(2)]
        for t in OUT_T:
            i = t % 2
            mk.dma("sp", xt[i][:], XR[t * 128:(t + 1) * 128, :], R=[f"XR{t}"], W=[f"fxt{i}"])
            mk.op("act", "activation", out=junk[:], in_=xt[i][:], func=AF.Square, accum_out=ss[i][:], R=[f"fxt{i}"],
                  W=["fjunk", f"fss{i}"])
            mk.op("act", "activation", out=ss[i][:], in_=ss[i][:], func=AF.Sqrt, scale=1.0 / D, bias=EPS, R=[f"fss{i}"],
                  W=[f"fss{i}"])
            mk.op("dve", "reciprocal", out=ss[i][:], in_=ss[i][:], R=[f"fss{i}"], W=[f"fss{i}"])
            mk.op("dve", "scalar_tensor_tensor", out=ot[i][:], in0=xt[i][:], scalar=ss[i][:, 0:1], in1=fw[:], op0=ALU.mult,
                  op1=ALU.mult, R=[f"fxt{i}", f"fss{i}", "fw"], W=[f"fot{i}"])
            mk.dma("pool", yout[(t - 2) * 128:(t - 1) * 128, :], ot[i][:], R=[f"fot{i}"], W=["yout"])
        mk.barrier()
        P.close()

    stages = dict(mod=mod_phase, inproj=inproj_phase, a=mixer_a, b=mixer_b, c=mixer_c, d=mixer_d)
    return dict(nc=nc, mk=mk, stages=stages, merge=merge_moe_phase, final=final_phase, dbg=dbg,
                scr=dict(XR=XR, FM=FM, TM=TM, MG=MG, YT=YT))


def emit_all(prog, layers=NL, upto=None, skip=()):
    mk = prog["mk"]
    mk.barrier()
    done = False
    for l in range(layers):
        for s in ("mod", "inproj", "a", "b", "c", "d"):
            if s in skip:
                continue
            if s in ("inproj", "b", "c", "d"):
                prog["stages"][s](l, l == NL - 1)
            else:
                prog["stages"][s](l)
            if upto == (l, s):
                done = True
                break
        if done:
            break
        prog["merge"](l, l == NL - 1)
        if upto == (l, "merge"):
            done = True
            break
    if not done:
        prog["final"]()
    mk.barrier(engines=("sp",))


def _consts():
    j = np.arange(128)[:, None]
    i = np.arange(128)[None, :]
    same = (j // DL) == (i // DL)
    m = {}
    m["ident"] = (j == i)
    m["triF"] = (j <= i)
    m["triB"] = (j >= i)
    m["blkF"] = same & (j <= i)
    m["blkB"] = same & (j >= i)
    m["aftF"] = same & (j > i)
    m["befB"] = same & (j < i)
    m["diffF"] = np.maximum(i - j, 0)
    m["diffB"] = np.maximum(j - i, 0)
    m["maskF"] = (i >= j)
    m["maskB"] = (j > i)
    m["posF"] = np.broadcast_to(i + 1, (128, 128))
    m["posB"] = np.broadcast_to(128 - i, (128, 128))
    m["negF4"] = np.tile(np.where(j <= i, 0.0, -30000.0), (1, 4))
    m["negB4"] = np.tile(np.where(j >= i, 0.0, -30000.0), (1, 4))
    m["mblkF4"] = np.tile(same & (j <= i), (1, 4))
    m["mblkB4"] = np.tile(same & (j >= i), (1, 4))
    m["kpos"] = np.concatenate([127 - j, j], 1)
    m["ones"] = np.ones((128, 128))
    sel = np.zeros((128, 256))
    sel[0, 0:128] = 1.0
    sel[1, 128:256] = 1.0
    m["sel"] = sel
    m["subm"] = np.concatenate([(j // DL) == s_ for s_ in range(128 // DL)], 1)
    qm = np.zeros((128, 2, 5, 128), np.float32)
    for hh_ in range(2):
        for s_ in range(5):
            colsel = np.ones(128, bool) if s_ == 4 else (np.arange(128) // DL == s_)
            qm[hh_ * 64:(hh_ + 1) * 64, hh_, s_, :] = colsel[None, :]
    m["qmask"] = qm.reshape(128, 1280)
    m["ltS"] = (j < i)
    m["thrS"] = np.broadcast_to(np.arange(16)[None, :] * SLOT, (128, 16))
    m["kS"] = np.broadcast_to(np.arange(16)[None, :] * SLOT, (128, 16))
    m["jp"] = np.arange(4)[None, :] * 128 + np.arange(128)[:, None]
    out = np.zeros((128, NCST), np.float32)
    for k, (o, w) in CST.items():
        out[:, o:o + w] = np.asarray(m[k], np.float32)
    return out


def _rope_tables(flip=False):
    n = 16
    inv = np.power(np.float32(10000.0), -np.arange(n, dtype=np.float32) / n).astype(np.float32)
    t = np.arange(4096)
    row = (t // 64).astype(np.float32)
    col = (t % 64).astype(np.float32)
    ang = np.concatenate([row[:, None] * inv, col[:, None] * inv], -1)
    cos = np.cos(ang).astype(np.float32).T
    sin = np.sin(ang).astype(np.float32).T
    if flip:
        cos, sin = cos[:, ::-1], sin[:, ::-1]
    Cc = np.ones((128, T), np.float32)
    Ss = np.zeros((128, T), np.float32)
    for hh in range(2):
        Cc[hh * 64:hh * 64 + 32, 256:] = cos
        Cc[hh * 64 + 32:hh * 64 + 64, 256:] = cos
        Ss[hh * 64:hh * 64 + 32, 256:] = -sin
        Ss[hh * 64 + 32:hh * 64 + 64, 256:] = sin
    return Cc, Ss


def prep_shared(inp, flip=False):
    f = np.float32
    w_in = np.asarray(inp["w_in"], f)
    offs = {}
    o = 0
    for name, w in (("a_x", 256), ("a_g", 256), ("b_q", 256), ("b_k", 256), ("b_v", 256), ("b_g", 256), ("c_q", 256),
                    ("c_k", 256), ("c_v", 256), ("c_o", 256), ("c_gates", 16), ("d_q", 256), ("d_ff", 256),
                    ("d_fb", 256), ("d_i", 256), ("d_g", 256), ("merge", 4096)):
        offs[name] = (o, w)
        o += w

    def cols(n):
        a, w = offs[n]
        return w_in[:, :, a:a + w]

    perm = np.concatenate([np.arange(h * 64 + 32, h * 64 + 64).tolist() + np.arange(h * 64, h * 64 + 32).tolist()
                           for h in range(4)]).astype(np.int64)
    w_fm = np.concatenate([cols("a_x"), cols("a_g"), cols("b_q"), cols("b_q")[:, :, perm], cols("b_k"),
                           cols("b_k")[:, :, perm], cols("c_q"), cols("c_k")], -1)
    gperm = np.array([8, 9, 10, 11, 12, 13, 14, 15, 0, 1, 2, 3, 4, 5, 6, 7]) if flip else np.arange(16)
    dfa, dfb = ("d_fb", "d_ff") if flip else ("d_ff", "d_fb")
    w_tm = np.concatenate([cols("b_v"), cols("b_g"), cols("c_v"), cols("c_o"), cols("d_q"), cols(dfa), cols(dfb),
                           cols("d_i"), cols("d_g"), cols("c_gates")[:, :, gperm], cols("merge")], -1)
    sh = {}
    sh["w_mod"] = np.ascontiguousarray(inp["w_mod"], f)
    bm = np.asarray(inp["b_mod"], f)
    sh["bmod_c"] = np.ascontiguousarray(bm.reshape(NL, 48, 128).transpose(0, 2, 1))
    sh["bmod_r"] = np.ascontiguousarray(bm.reshape(NL, 1, 6144))
    sh["w_fm"] = np.ascontiguousarray(w_fm)
    sh["w_tm"] = np.ascontiguousarray(w_tm)
    acw = np.asarray(inp["a_conv_w"], f)
    zt_ = np.zeros_like(acw[:, :1])
    acw = np.concatenate([zt_, acw[:, ::-1]], 1) if flip else np.concatenate([acw, zt_], 1)
    sh["a_cw"] = np.ascontiguousarray(acw.reshape(NL, 5, 2, 128).transpose(0, 3, 2, 1))
    sh["a_cb"] = np.ascontiguousarray(np.asarray(inp["a_conv_b"], f).reshape(NL, 2, 128).transpose(0, 2, 1))
    gw = np.asarray(inp["a_gate_w"], f)
    agw = np.zeros((NL, 128, 2, 2, 2, 128), f)
    for c in range(2):
        for hh in range(2):
            agw[:, hh * 64:(hh + 1) * 64, :, :, c, hh * 64:(hh + 1) * 64] = gw[:, :, :, 2 * c + hh].transpose(0, 3, 1, 2, 4)
    sh["a_gw"] = np.ascontiguousarray(agw[:, :, ::-1]) if flip else agw
    gb = np.asarray(inp["a_gate_b"], f)
    if flip:
        gb = gb[:, ::-1]
    sh["a_gb"] = np.ascontiguousarray(gb.reshape(NL, 2, 2, 2, 128).transpose(0, 4, 1, 2, 3))
    lam = np.asarray(inp["a_lambda"], f)
    if flip:
        lam = lam[:, ::-1]
    sh["a_lam"] = np.ascontiguousarray(lam.reshape(NL, 2, 2, 128).transpose(0, 3, 1, 2))
    th = np.asarray(inp["b_theta"], f)
    if flip:
        th = th[:, ::-1]
    thp = np.zeros((NL, 128, 2, 2), f)
    for c in range(2):
        for hh in range(2):
            thp[:, hh * 64:(hh + 1) * 64, :, c] = th[:, None, :, 2 * c + hh]
    sh["b_thp"] = thp
    sh["b_thh"] = np.ascontiguousarray(np.broadcast_to(th[:, None], (NL, 128, 2, 4)))
    ccw = np.asarray(inp["c_conv_w"], f)
    zt_ = np.zeros_like(ccw[:, :1])
    ccw = np.concatenate([zt_, ccw[:, ::-1]], 1) if flip else np.concatenate([ccw, zt_], 1)
    sh["c_cw"] = np.ascontiguousarray(ccw.reshape(NL, 5, 4, 128).transpose(0, 3, 2, 1))
    sh["c_cb"] = np.ascontiguousarray(np.asarray(inp["c_conv_b"], f).reshape(NL, 4, 128).transpose(0, 2, 1))
    sh["c_gb"] = np.ascontiguousarray(np.broadcast_to(np.asarray(inp["c_gate_b"], f).reshape(NL, 1, 16)[:, :, gperm], (NL, 128, 16)))
    sh["d_lbr"] = np.ascontiguousarray(np.broadcast_to(np.asarray(inp["d_lb"], f)[None], (128, 2, 256)))
    sh["w_branch"] = np.ascontiguousarray(inp["w_branch"], f)
    sh["w_out"] = np.ascontiguousarray(inp["w_out"], f)
    sh["moe_wgr"] = np.ascontiguousarray(np.concatenate([np.asarray(inp["moe_w_group"], f), np.asarray(inp["moe_w_router"], f)], -1))
    sh["moe_bgr"] = np.ascontiguousarray(np.concatenate([np.asarray(inp["moe_b_group"], f), np.asarray(inp["moe_b_router"], f)], -1).reshape(NL, 1, 20))
    sh["moe_w1"] = np.ascontiguousarray(inp["moe_w1"], f)
    sh["moe_w3"] = np.ascontiguousarray(inp["moe_w3"], f)
    sh["moe_w2"] = np.ascontiguousarray(inp["moe_w2"], f)
    sh["fnw"] = np.ascontiguousarray(np.broadcast_to(np.asarray(inp["final_norm_w"], f)[None], (128, D)))
    sh["cst"] = _consts()
    sh["ropeC"], sh["ropeS"] = _rope_tables(flip)
    return sh


def prep_core(inp, b, flip=False):
    f = np.float32
    d = {}
    cx, xx = np.asarray(inp["ctx"][b], f), np.asarray(inp["x"][b], f)
    if flip:
        cx, xx = cx[::-1], xx[::-1]
    d["xin"] = np.ascontiguousarray(np.concatenate([cx, xx], 0))
    cv = np.stack([np.asarray(inp["c_ctx"], f), np.asarray(inp["c"][b], f)], -1)
    d["cvec"] = np.ascontiguousarray(cv.reshape(8, 128, 2).transpose(1, 0, 2))
    return d


_PROG = None


def kernel(**inputs):
    global _PROG
    if _PROG is None:
        _PROG = build_program()
        emit_all(_PROG)
    nc = _PROG["nc"]
    shs = [prep_shared(inputs, False), prep_shared(inputs, True)]
    in_maps = []
    for core in range(8):
        fl = core >= 4
        m = dict(shs[1 if fl else 0])
        m.update(prep_core(inputs, core % 4, fl))
        in_maps.append(m)
    res = run_bass_kernel_spmd(nc, in_maps, core_ids=list(range(8)))
    out = np.empty((4, 4096, D), np.float32)
    for b in range(4):
        out[b, :HALF_OUT] = np.asarray(res.results[b]["yout"], np.float32)[:HALF_OUT]
        out[b, HALF_OUT:] = np.asarray(res.results[b + 4]["yout"], np.float32)[:4096 - HALF_OUT][::-1]
    return out
```

```python
import contextlib
import numpy as np
import ml_dtypes
import concourse.bass as bass
import concourse.mybir as mybir
from concourse.bass_utils import run_bass_kernel_spmd

F32 = mybir.dt.float32
BF16 = mybir.dt.bfloat16
AF = mybir.ActivationFunctionType
ALU = mybir.AluOpType
AX = mybir.AxisListType

T = 4352
NT = 34
D = 1024
EPS = 1e-6
NL = 2
SLOT = 512
HALF_OUT = 2048
DL = 32
DNS = 128 // DL
DBG = dict(maxit=None, core=True, fin=True, heads=(0, 1, 2, 3))
TMW = 2320
TM_OFF = dict(b_v=0, b_g=256, c_v=512, c_o=768, d_q=1024, d_ff=1280, d_fb=1536, d_i=1792, d_g=2048, c_gates=2304)
FM_OFF = dict(a_x=0, a_g=2, b_q=4, b_qp=6, b_k=8, b_kp=10, c_q=12, c_k=14)

CST = {}
_off = 0
for _n, _w in (("ident", 128), ("triF", 128), ("triB", 128), ("blkF", 128), ("blkB", 128), ("aftF", 128),
               ("befB", 128), ("diffF", 128), ("diffB", 128), ("maskF", 128), ("maskB", 128), ("posF", 128),
               ("posB", 128), ("negF4", 512), ("negB4", 512), ("mblkF4", 512), ("mblkB4", 512), ("kpos", 2),
               ("ones", 128), ("sel", 256), ("subm", 4), ("qmask", 1280), ("ltS", 128), ("thrS", 16), ("kS", 16), ("jp", 4)):
    CST[_n] = (_off, _w)
    _off += _w
NCST = _off


class MK:
    SEM_ROT = 30000

    def __init__(self, nc, ndma=8):
        self.nc = nc
        self.engs = {"pe": nc.tensor, "act": nc.scalar, "dve": nc.vector, "pool": nc.gpsimd, "sp": nc.sync}
        self._ctxs = []
        self.nsem = 0
        self.sem = {}
        self.cnt = {}
        for e in ("pe", "act", "dve", "pool"):
            self.sem[e] = self._newsem("s_" + e)
            self.cnt[e] = 0
        self.seen = {e: {} for e in self.engs}
        self.dq = {}
        for q in ("sp", "pool"):
            self.dq[q] = {"i": 0, "slots": [[self._newsem(f"d_{q}{i}"), 0] for i in range(ndma)]}
        self.res = {}
        self.ninst = 0
        self.flip = 0

    def _newsem(self, name):
        self.nsem += 1
        cm = self.nc.semaphore(f"{name}_{self.nsem}")
        s = cm.__enter__()
        self._ctxs.append(cm)
        return s

    def _wait(self, eng, tok):
        sem, val = tok
        key = id(sem)
        if self.seen[eng].get(key, 0) >= val:
            return
        self.engs[eng].wait_ge(sem, val)
        self.seen[eng][key] = val

    def _deps(self, R, W):
        deps = []
        for k in R:
            st = self.res.get(k)
            if st and st[0] is not None:
                deps.append(st[0])
        for k in W:
            st = self.res.get(k)
            if st:
                if st[0] is not None:
                    deps.append(st[0])
                deps.extend(st[1])
        return deps

    def _record(self, tok, R, W):
        for k in R:
            st = self.res.setdefault(k, [None, []])
            st[1] = [t for t in st[1] if t[0] is not tok[0]] + [tok]
        for k in W:
            self.res[k] = [tok, []]

    def op(self, eng, method, *args, R=(), W=(), **kw):
        for tok in self._deps(R, W):
            if eng == "pe" and tok[0] is self.sem["pe"]:
                continue
            self._wait(eng, tok)
        ins = getattr(self.engs[eng], method)(*args, **kw)
        if self.cnt[eng] >= self.SEM_ROT:
            self.sem[eng] = self._newsem("s_" + eng)
            self.cnt[eng] = 0
        self.cnt[eng] += 1
        ins.then_inc(self.sem[eng], 1)
        tok = (self.sem[eng], self.cnt[eng])
        self._record(tok, R, W)
        self.ninst += 1
        return tok

    def dma(self, q, out, in_, R=(), W=(), **kw):
        d = self.dq[q]
        slot = d["slots"][d["i"] % len(d["slots"])]
        d["i"] += 1
        if slot[1] > 0:
            self._wait(q, (slot[0], slot[1]))
        if slot[1] >= self.SEM_ROT:
            slot[0] = self._newsem("d_" + q)
            slot[1] = 0
        for tok in self._deps(R, W):
            self._wait(q, tok)
        ins = self.engs[q].dma_start(out=out, in_=in_, **kw)
        slot[1] += 16
        ins.then_inc(slot[0], 16)
        tok = (slot[0], slot[1])
        self._record(tok, R, W)
        self.ninst += 1
        return tok

    def idma(self, out, out_offset, in_, in_offset, R=(), W=()):
        q = "pool"
        d = self.dq[q]
        slot = d["slots"][d["i"] % len(d["slots"])]
        d["i"] += 1
        if slot[1] > 0:
            self._wait(q, (slot[0], slot[1]))
        if slot[1] >= self.SEM_ROT:
            slot[0] = self._newsem("d_" + q)
            slot[1] = 0
        for tok in self._deps(R, W):
            self._wait(q, tok)
        ins = self.nc.gpsimd.indirect_dma_start(out=out, out_offset=out_offset, in_=in_, in_offset=in_offset)
        slot[1] += 16
        ins.then_inc(slot[0], 16)
        tok = (slot[0], slot[1])
        self._record(tok, R, W)
        self.ninst += 1
        return tok

    def barrier(self, engines=("pe", "act", "dve", "pool", "sp")):
        toks = []
        for q, d in self.dq.items():
            for slot in d["slots"]:
                if slot[1] > 0:
                    toks.append((slot[0], slot[1]))
        for e in ("pe", "act", "dve", "pool"):
            if self.cnt[e] > 0:
                toks.append((self.sem[e], self.cnt[e]))
        for e in engines:
            for tok in toks:
                if e in self.sem and tok[0] is self.sem[e]:
                    continue
                self._wait(e, tok)

    def ev(self, out, in_, R=(), W=(), func=None, **kw):
        if func is not None:
            return self.op("act", "activation", out=out, in_=in_, func=func, R=R, W=W, **kw)
        self.flip ^= 1
        if self.flip:
            return self.op("act", "activation", out=out, in_=in_, func=AF.Copy, R=R, W=W)
        return self.op("dve", "tensor_copy", out=out, in_=in_, R=R, W=W)


class Pool:
    def __init__(self, nc, tag):
        self.nc = nc
        self.tag = tag
        self.stack = contextlib.ExitStack()
        self.n = 0

    def sb(self, shape, dt=F32, name=None):
        self.n += 1
        return self.stack.enter_context(self.nc.sbuf_tensor(f"{self.tag}_{name or 't'}{self.n}", list(shape), dt))

    def close(self):
        self.stack.close()


def build_program(debug=()):
    nc = bass.Bass("TRN2", target_bir_lowering=False)

    def din(name, shape, dt=F32):
        return nc.dram_tensor(name, list(shape), dt, kind="ExternalInput").ap()

    def dscr(name, shape, dt=F32):
        return nc.dram_tensor(name, list(shape), dt, kind="Internal").ap()

    xin = din("xin", [T, D])
    cvec = din("cvec", [128, 8, 2])
    w_mod = din("w_mod", [NL, D, 6144])
    bmod_c = din("bmod_c", [NL, 128, 48])
    bmod_r = din("bmod_r", [NL, 1, 6144])
    w_fm = din("w_fm", [NL, D, 2048])
    w_tm = din("w_tm", [NL, D, TMW + 4096])
    a_cw = din("a_cw", [NL, 128, 2, 5])
    a_cb = din("a_cb", [NL, 128, 2])
    a_gw = din("a_gw", [NL, 128, 2, 2, 2, 128])
    a_gb = din("a_gb", [NL, 128, 2, 2, 2])
    a_lam = din("a_lam", [NL, 128, 2, 2])
    b_thp = din("b_thp", [NL, 128, 2, 2])
    b_thh = din("b_thh", [NL, 128, 2, 4])
    c_cw = din("c_cw", [NL, 128, 4, 5])
    c_cb = din("c_cb", [NL, 128, 4])
    c_gb = din("c_gb", [NL, 128, 16])
    d_lbr = din("d_lbr", [128, 2, 256])
    w_branch = din("w_branch", [NL, 4, 256, D])
    w_out = din("w_out", [NL, D, D])
    moe_wgr = din("moe_wgr", [NL, D, 20])
    moe_bgr = din("moe_bgr", [NL, 1, 20])
    moe_w1 = din("moe_w1", [NL, 16, D, 512])
    moe_w3 = din("moe_w3", [NL, 16, D, 512])
    moe_w2 = din("moe_w2", [NL, 16, 512, D])
    fnw = din("fnw", [128, D])
    cst_d = din("cst", [128, NCST])
    ropeC_d = din("ropeC", [128, T])
    ropeS_d = din("ropeS", [128, T])
    yout = nc.dram_tensor("yout", [HALF_OUT, D], F32, kind="ExternalOutput").ap()

    XR = dscr("XR", [T, D])
    FM = dscr("FM", [16, 128, T])
    TM = dscr("TM", [T, TMW])
    MG = dscr("MG", [T, 4096], BF16)
    YT = dscr("YT", [4, 2, 128, T], BF16)
    dbg = {}
    for name, shape, dt in debug:
        dbg[name] = nc.dram_tensor("dbg_" + name, list(shape), dt, kind="ExternalOutput").ap()

    mk = MK(nc)
    G = Pool(nc, "g")

    PS = [nc.psum_tensor(f"ps{i}", [128, 512], F32).__enter__() for i in range(6)]
    PQ = [nc.psum_tensor(f"pq{i}", [128, 1024], BF16).__enter__() for i in range(2)]

    cst = G.sb([128, NCST], F32, "cst")
    identb = G.sb([128, 128], BF16, "identb")
    sT = G.sb([128, 8, 2], F32, "sT")
    MODC = G.sb([128, 4, 8, 2], F32, "MODC")
    GB = G.sb([128, 2, 2, D], F32, "GB")
    mk.dma("sp", cst[:], cst_d, W=["cst"])
    mk.dma("sp", sT[:], cvec, W=["sT"])

    def C(name, rows=slice(0, 128)):
        o, w = CST[name]
        return cst[rows, o:o + w]

    mk.op("dve", "tensor_copy", out=identb[:], in_=C("ident"), R=["cst"], W=["identb"])
    mk.op("act", "activation", out=sT[:], in_=sT[:], func=AF.Silu, R=["sT"], W=["sT"])
    for t in range(NT):
        mk.dma("sp", XR[t * 128:(t + 1) * 128, :], xin[t * 128:(t + 1) * 128, :], W=[f"XR{t}"])

    def cond_of(t):
        return 0 if t < 2 else 1

    def mod_phase(l):
        P = Pool(nc, f"mod{l}")
        wblk = P.sb([128, 8, 1024], F32, "wblk")
        bmc = P.sb([128, 48], F32, "bmc")
        bmr = P.sb([1, 6144], F32, "bmr")
        GR = P.sb([2, 2, D], F32, "GR")
        mk.dma("sp", bmc[:], bmod_c[l], W=["bmc"])
        mk.dma("sp", bmr[:], bmod_r[l], W=["bmr"])
        sel = C("sel", slice(0, 2))
        for m in range(6):
            mk.dma("sp", wblk[:], w_mod[l].rearrange("(c p) f -> p c f", p=128)[:, :, m * 1024:(m + 1) * 1024],
                   W=["wblk"])
            if m in (0, 1, 3, 4):
                m4 = {0: 0, 1: 1, 3: 2, 4: 3}[m]
                for c in range(8):
                    for k in range(8):
                        mk.op("pe", "matmul", PS[0][:, c * 2:(c + 1) * 2], lhsT=wblk[:, k, c * 128:(c + 1) * 128],
                              rhs=sT[:, k, :], start=(k == 0), stop=(k == 7), R=["wblk", "sT"], W=["ps0"])
                mk.op("dve", "tensor_tensor", out=MODC[:, m4, :, :],
                      in0=PS[0][:, 0:16].rearrange("p (c j) -> p c j", j=2),
                      in1=bmc[:, m * 8:(m + 1) * 8].unsqueeze(2).to_broadcast([128, 8, 2]), op=ALU.add,
                      R=["ps0", "bmc"], W=["MODC"])
                if m in (1, 4):
                    mk.op("dve", "tensor_scalar_add", out=MODC[:, m4, :, :], in0=MODC[:, m4, :, :], scalar1=1.0,
                          R=["MODC"], W=["MODC"])
            else:
                mi = 0 if m == 2 else 1
                for cb in range(2):
                    for k in range(8):
                        mk.op("pe", "matmul", PS[1][0:2, :], lhsT=sT[:, k, :], rhs=wblk[:, k, cb * 512:(cb + 1) * 512],
                              start=(k == 0), stop=False, R=["wblk", "sT"], W=["ps1"])
                    mk.op("pe", "matmul", PS[1][0:2, :], lhsT=sel[0:1, 0:2],
                          rhs=bmr[0:1, m * 1024 + cb * 512: m * 1024 + (cb + 1) * 512], start=False, stop=True,
                          R=["bmr", "cst"], W=["ps1"])
                    mk.op("dve", "tensor_copy", out=GR[0:2, mi, cb * 512:(cb + 1) * 512], in_=PS[1][0:2, :],
                          R=["ps1"], W=["GR"])
        for j in range(2):
            for mi in range(2):
                for cb in range(2):
                    mk.op("pe", "matmul", PS[1][:, :], lhsT=sel[0:2, j * 128:(j + 1) * 128],
                          rhs=GR[0:2, mi, cb * 512:(cb + 1) * 512], start=True, stop=True, R=["GR", "cst"], W=["ps1"])
                    mk.op("act", "activation", out=GB[:, j, mi, cb * 512:(cb + 1) * 512], in_=PS[1][:, :], func=AF.Copy,
                          R=["ps1"], W=["GB"])
        mk.barrier()
        P.close()

    def make_norm(P, nbuf=2):
        st = dict(junk=P.sb([128, D], BF16, "junk"), ss=[P.sb([128, 1], F32, "ss") for _ in range(nbuf)],
                  xn=[P.sb([128, D], BF16, "xn") for _ in range(nbuf)],
                  tmp=[P.sb([128, 8, 128], F32, "tmp") for _ in range(nbuf)], i=0, nbuf=nbuf)

        def norm(xt_ap, xt_key, j, msc, msh, h_out, h_key):
            i = st["i"] % st["nbuf"]
            st["i"] += 1
            ss, xn, tmp = st["ss"][i], st["xn"][i], st["tmp"][i]
            mk.op("act", "activation", out=st["junk"][:], in_=xt_ap, func=AF.Square, accum_out=ss[:],
                  R=[xt_key], W=["junk", f"ss{i}"])
            mk.op("act", "activation", out=ss[:], in_=ss[:], func=AF.Sqrt, scale=1.0 / D, bias=EPS,
                  R=[f"ss{i}"], W=[f"ss{i}"])
            mk.op("dve", "reciprocal", out=ss[:], in_=ss[:], R=[f"ss{i}"], W=[f"ss{i}"])
            mk.op("dve", "tensor_scalar", out=xn[:], in0=xt_ap, scalar1=ss[:, 0:1], scalar2=None, op0=ALU.mult,
                  R=[xt_key, f"ss{i}"], W=[f"xn{i}"])
            for c in range(8):
                mk.op("pe", "transpose", out=PQ[i][:, c * 128:(c + 1) * 128], in_=xn[:, c * 128:(c + 1) * 128],
                      identity=identb[:], R=[f"xn{i}", "identb"], W=[f"pq{i}"])
            mk.op("dve", "tensor_tensor", out=tmp[:], in0=PQ[i][:, :].rearrange("p (c n) -> p c n", c=8),
                  in1=MODC[:, msc, :, j:j + 1].to_broadcast([128, 8, 128]), op=ALU.mult,
                  R=[f"pq{i}", "MODC"], W=[f"ntmp{i}"])
            mk.op("pool", "tensor_tensor", out=h_out, in0=tmp[:],
                  in1=MODC[:, msh, :, j:j + 1].to_broadcast([128, 8, 128]), op=ALU.add,
                  R=[f"ntmp{i}", "MODC"], W=[h_key])
        return norm

    def inproj_phase(l, last=False):
        P = Pool(nc, f"ip{l}")
        hT = P.sb([128, 8, T], BF16, "hT")
        xt = [P.sb([128, D], F32, "xt") for _ in range(2)]
        norm = make_norm(P)
        for t in range(NT):
            i = t % 2
            mk.dma("sp", xt[i][:], XR[t * 128:(t + 1) * 128, :], R=[f"XR{t}"], W=[f"xt{i}"])
            norm(xt[i][:], f"xt{i}", cond_of(t), 1, 0, hT[:, :, t * 128:(t + 1) * 128], f"hT{t}")
        hkeys = [f"hT{t}" for t in range(NT)]
        wf = [P.sb([128, 8, 512], F32, "wf")] * 2
        wb = [P.sb([128, 8, 512], BF16, "wb") for _ in range(2)]
        stg = [P.sb([128, T], F32, "stg")] * 2
        nblk = 0
        tblocks = [(i * 512, min(512, T - i * 512)) for i in range(9)]
        for cb in range(4):
            i = nblk % 2
            nblk += 1
            mk.dma("sp", wf[i][:], w_fm[l].rearrange("(c p) f -> p c f", p=128)[:, :, cb * 512:(cb + 1) * 512],
                   W=["wf"])
            mk.ev(wb[i][:], wf[i][:], R=["wf"], W=[f"wb{i}"])
            for sub in range(4):
                fc = cb * 4 + sub
                si = fc % 2
                for bi, (t0, tw) in enumerate(tblocks):
                    pb = bi % 2
                    for k in range(8):
                        mk.op("pe", "matmul", PS[pb][:, 0:tw], lhsT=wb[i][:, k, sub * 128:(sub + 1) * 128],
                              rhs=hT[:, k, t0:t0 + tw], start=(k == 0), stop=(k == 7),
                              R=[f"wb{i}"] + hkeys[t0 // 128:(t0 + tw) // 128], W=[f"ps{pb}"])
                    mk.ev(stg[si][:, t0:t0 + tw], PS[pb][:, 0:tw], R=[f"ps{pb}"], W=["stg"])
                mk.dma("pool", FM[fc], stg[si][:], R=["stg"], W=[f"FM{fc}"])
        cblocks = [(i * 512, 512) for i in range(4)] + [(2048, TMW - 2048)] + [(TMW + i * 512, 512) for i in range(8)]
        stt = [P.sb([128, 4, 512], F32, "stt") for _ in range(2)]
        stb = [P.sb([128, 4, 512], BF16, "stb") for _ in range(2)]
        tgroups = [(g * 4, min(4, NT - g * 4)) for g in range(9)]
        ns = 0
        for (c0, cw) in cblocks:
            i = nblk % 2
            nblk += 1
            mk.dma("sp", wf[i][:, :, 0:cw], w_tm[l].rearrange("(c p) f -> p c f", p=128)[:, :, c0:c0 + cw], W=["wf"])
            mk.ev(wb[i][:, :, 0:cw], wf[i][:, :, 0:cw], R=["wf"], W=[f"wb{i}"])
            is_mg = c0 >= TMW
            for (g0, gn) in tgroups:
                if is_mg and last and not any((g0 + q) in OUT_T for q in range(gn)):
                    continue
                si = ns % 2
                ns += 1
                for tt in range(gn):
                    t = g0 + tt
                    pb = 2 + (t % 2)
                    for k in range(8):
                        mk.op("pe", "matmul", PS[pb][:, 0:cw], lhsT=hT[:, k, t * 128:(t + 1) * 128],
                              rhs=wb[i][:, k, 0:cw], start=(k == 0), stop=(k == 7), R=[f"wb{i}", f"hT{t}"], W=[f"ps{pb}"])
                    if is_mg:
                        mk.ev(stb[si][:, tt, 0:cw], PS[pb][:, 0:cw], R=[f"ps{pb}"], W=[f"stb{si}"], func=AF.Sigmoid)
                    else:
                        mk.ev(stt[si][:, tt, 0:cw], PS[pb][:, 0:cw], R=[f"ps{pb}"], W=[f"stt{si}"])
                if is_mg:
                    mk.dma("pool", MG[g0 * 128:(g0 + gn) * 128, c0 - TMW:c0 - TMW + cw].rearrange("(t p) c -> p t c", p=128),
                           stb[si][:, 0:gn, 0:cw], R=[f"stb{si}"], W=["MG"])
                else:
                    mk.dma("pool", TM[g0 * 128:(g0 + gn) * 128, c0:c0 + cw].rearrange("(t p) c -> p t c", p=128),
                           stt[si][:, 0:gn, 0:cw], R=[f"stt{si}"], W=["TM"])
        mk.barrier()
        P.close()

    SEGS = [(0, 256), (256, T)]

    def conv_fm(u, src, w4, bcol, keyu, keysrc, wkeys):
        for (s0, e) in SEGS:
            mk.op("act", "activation", out=u[:, s0:e], in_=src[:, s0:e], func=AF.Identity, scale=w4[:, 2:3], bias=bcol,
                  R=[keysrc] + wkeys, W=[keyu])
            for k, sh in ((0, -2), (1, -1), (3, 1), (4, 2)):
                if sh < 0:
                    o, i_ = u[:, s0 - sh:e], src[:, s0:e + sh]
                else:
                    o, i_ = u[:, s0:e - sh], src[:, s0 + sh:e]
                mk.op("dve", "scalar_tensor_tensor", out=o, in0=i_, scalar=w4[:, k:k + 1], in1=o, op0=ALU.mult,
                      op1=ALU.add, R=[keysrc, keyu] + wkeys, W=[keyu])

    def mixer_a(l):
        P = Pool(nc, f"ma{l}")
        cw = P.sb([128, 2, 5], F32, "cw")
        cb = P.sb([128, 2], F32, "cb")
        gwf = P.sb([128, 2, 2, 2, 128], F32, "gwf")
        gwb = P.sb([128, 2, 2, 2, 128], BF16, "gwb")
        gb = P.sb([128, 2, 2, 2], F32, "gb")
        lam = P.sb([128, 2, 2], F32, "lam")
        c1 = P.sb([128, 2, 2], F32, "c1")
        mk.dma("sp", cw[:], a_cw[l], W=["a_cw"])
        mk.dma("sp", cb[:], a_cb[l], W=["a_cb"])
        mk.dma("sp", gwf[:], a_gw[l], W=["a_gwf"])
        mk.dma("sp", gb[:], a_gb[l], W=["a_gb"])
        mk.dma("sp", lam[:], a_lam[l], W=["a_lam"])
        mk.op("dve", "tensor_copy", out=gwb[:], in_=gwf[:], R=["a_gwf"], W=["a_gwb"])
        mk.op("act", "activation", out=c1[:], in_=lam[:], func=AF.Exp, scale=-1.0, R=["a_lam"], W=["a_c1"])
        mk.op("act", "activation", out=c1[:], in_=c1[:], func=AF.Ln, bias=1.0, R=["a_c1"], W=["a_c1"])
        mk.op("dve", "tensor_scalar", out=c1[:], in0=c1[:], scalar1=-8.0, scalar2=None, op0=ALU.mult, R=["a_c1"], W=["a_c1"])
        ax = P.sb([128, T], F32, "ax")
        ag = P.sb([128, T], F32, "ag")
        u = P.sb([128, T], F32, "u")
        ub = P.sb([128, T], BF16, "ub")
        aa = P.sb([128, T], F32, "aa")
        bt = P.sb([128, T], F32, "bt")
        hf = P.sb([128, T], F32, "hf")
        hb = P.sb([128, T], F32, "hb")
        r = [P.sb([128, 512], F32, "r") for _ in range(2)]
        gi = [P.sb([128, 512], F32, "gi") for _ in range(2)]
        yb = P.sb([128, T], BF16, "yb")
        tblocks = [(i * 512, min(512, T - i * 512)) for i in range(9)]
        for c in range(2):
            mk.dma("sp", ax[:], FM[FM_OFF["a_x"] + c], R=[f"FM{FM_OFF['a_x'] + c}"], W=["ax"])
            mk.dma("sp", ag[:], FM[FM_OFF["a_g"] + c], R=[f"FM{FM_OFF['a_g'] + c}"], W=["ag"])
            conv_fm(u, ax, cw[:, c, :], cb[:, c:c + 1], "u", "ax", ["a_cw", "a_cb"])
            mk.op("pool", "tensor_copy", out=ub[:], in_=u[:], R=["u"], W=["ub"])
            for d in range(2):
                for bi, (t0, tw) in enumerate(tblocks):
                    i = bi % 2
                    mk.op("pe", "matmul", PS[i][:, 0:tw], lhsT=gwb[:, d, 0, c, :], rhs=ub[:, t0:t0 + tw], start=True,
                          stop=True, R=["a_gwb", "ub"], W=[f"ps{i}"])
                    mk.op("pe", "matmul", PS[2 + i][:, 0:tw], lhsT=gwb[:, d, 1, c, :], rhs=ub[:, t0:t0 + tw], start=True,
                          stop=True, R=["a_gwb", "ub"], W=[f"ps{2 + i}"])
                    mk.op("act", "activation", out=r[i][:, 0:tw], in_=PS[i][:, 0:tw], func=AF.Sigmoid,
                          bias=gb[:, d, 0, c:c + 1], R=[f"ps{i}", "a_gb"], W=[f"r{i}"])
                    mk.op("act", "activation", out=gi[i][:, 0:tw], in_=PS[2 + i][:, 0:tw], func=AF.Sigmoid,
                          bias=gb[:, d, 1, c:c + 1], R=[f"ps{2 + i}", "a_gb"], W=[f"gi{i}"])
                    mk.op("act", "activation", out=aa[:, t0:t0 + tw], in_=r[i][:, 0:tw], func=AF.Exp,
                          scale=c1[:, d, c:c + 1], R=[f"r{i}", "a_c1"], W=["aa"])
                    mk.op("dve", "tensor_tensor", out=r[i][:, 0:tw], in0=aa[:, t0:t0 + tw], in1=aa[:, t0:t0 + tw],
                          op=ALU.mult, R=["aa", f"r{i}"], W=[f"r{i}"])
                    mk.op("dve", "tensor_scalar", out=r[i][:, 0:tw], in0=r[i][:, 0:tw], scalar1=-1.0, scalar2=1.0,
                          op0=ALU.mult, op1=ALU.add, R=[f"r{i}"], W=[f"r{i}"])
                    mk.op("act", "activation", out=r[i][:, 0:tw], in_=r[i][:, 0:tw], func=AF.Sqrt, R=[f"r{i}"], W=[f"r{i}"])
                    mk.op("dve", "tensor_tensor", out=gi[i][:, 0:tw], in0=gi[i][:, 0:tw], in1=r[i][:, 0:tw], op=ALU.mult,
                          R=[f"gi{i}", f"r{i}"], W=[f"gi{i}"])
                    mk.op("pool", "tensor_tensor", out=bt[:, t0:t0 + tw], in0=gi[i][:, 0:tw], in1=u[:, t0:t0 + tw],
                          op=ALU.mult, R=[f"gi{i}", "u"], W=["bt"])
                if d == 0:
                    mk.op("dve", "tensor_tensor_scan", out=hf[:, :], data0=aa[:, :], data1=bt[:, :], initial=0.0,
                          op0=ALU.mult, op1=ALU.add, R=["aa", "bt"], W=["hf"])
                else:
                    mk.op("dve", "tensor_tensor_scan", out=hb[:, 0:256][:, ::-1], data0=aa[:, 0:256][:, ::-1],
                          data1=bt[:, 0:256][:, ::-1], initial=0.0, op0=ALU.mult, op1=ALU.add, R=["aa", "bt"], W=["hb"])
                    mk.op("dve", "tensor_tensor_scan", out=hb[:, 256:T][:, ::-1], data0=aa[:, 256:T][:, ::-1],
                          data1=bt[:, 256:T][:, ::-1], initial=hb[:, 0:1], op0=ALU.mult, op1=ALU.add,
                          R=["aa", "bt", "hb"], W=["hb"])
            mk.op("act", "activation", out=ag[:], in_=ag[:], func=AF.Gelu, R=["ag"], W=["ag"])
            mk.op("dve", "tensor_tensor", out=hf[:], in0=hf[:], in1=hb[:], op=ALU.add, R=["hf", "hb"], W=["hf"])
            mk.op("dve", "tensor_tensor", out=yb[:], in0=hf[:], in1=ag[:], op=ALU.mult, R=["hf", "ag"], W=["yb"])
            mk.dma("pool", YT[0, c], yb[:], R=["yb"], W=[f"YT0{c}"])
        mk.barrier()
        P.close()

    def order_of(dr):
        return list(range(NT)) if dr == 0 else [1, 0] + list(range(NT - 1, 1, -1))

    def run_pipelined(gens, pipelined=True):
        if not pipelined:
            for g in gens:
                for _ in g:
                    pass
            return
        prev = None
        for g in gens:
            next(g, None)
            if prev is not None:
                for _ in prev:
                    pass
            prev = g
        if prev is not None:
            for _ in prev:
                pass

    OUT_T = list(range(2, 2 + HALF_OUT // 128))

    def plan(dr, last):
        if not last:
            return [(t, True) for t in order_of(dr)]
        if dr == 0:
            return [(0, False), (1, False)] + [(t, True) for t in OUT_T]
        return [(t, (t in OUT_T)) for t in order_of(dr)]

    def chunk_core(it, nsub, dr, QT, KT, QIT, KHs, V, vw, Gc, S, Sbf, maskD, maskkey, rkeys, PT, sk, full=True):
        pi = it % 2
        L = 128 // nsub
        okeys = ["ps2", "ps3"]
        Oh = [PS[2 + hh][:, 0:2 * vw].rearrange("p (c e) -> p c e", c=2) for hh in range(2)]
        if not DBG["core"]:
            return okeys, Oh
        for h in (DBG["heads"] if full else ()):
            c, hh = h // 2, h % 2
            rs = slice(hh * 64, (hh + 1) * 64)
            mk.op("pe", "matmul", PS[hh][:, c * 128:(c + 1) * 128], lhsT=KT[c][rs, :], rhs=QT[c][rs, :], start=True,
                  stop=True, R=rkeys, W=[f"ps{hh}"])
        PTv = PT[pi][:].rearrange("p (c x n) -> p c x n", c=2, x=2)
        Mv = maskD.rearrange("p (c x n) -> p c x n", c=2, x=2) if full else None
        for hh in (range(2) if full else ()):
            mk.op("dve", "tensor_tensor", out=PTv[:, :, hh, :], in0=PS[hh][:, 0:256].rearrange("p (c n) -> p c n", c=2),
                  in1=Mv[:, :, hh, :], op=ALU.mult, R=[f"ps{hh}", maskkey], W=[f"PT{pi}h{hh}"])
        ptk = [f"PT{pi}h0", f"PT{pi}h1"]
        subs = list(range(nsub)) if dr == 0 else list(range(nsub - 1, -1, -1))
        KVp = PS[4][:, 0:2 * vw].rearrange("p (c e) -> p c e", c=2)
        for s in subs:
            rows = slice(s * L, (s + 1) * L)
            for h in (DBG["heads"] if full else ()):
                c, hh = h // 2, h % 2
                rs = slice(hh * 64, (hh + 1) * 64)
                mk.op("pe", "matmul", Oh[hh][rows, c, :], lhsT=PT[pi][:, h * 128 + s * L:h * 128 + (s + 1) * L],
                      rhs=V[:, h, :], start=True, stop=False, R=[ptk[hh]] + rkeys, W=[okeys[hh]])
                mk.op("pe", "matmul", Oh[hh][rows, c, :], lhsT=QIT[c][rs, rows], rhs=Sbf[c][rs, :], start=False, stop=True,
                      R=rkeys + [f"{sk}Sbf{c}"], W=[okeys[hh]])
            for h in DBG["heads"]:
                c, hh = h // 2, h % 2
                mk.op("pe", "matmul", KVp[hh * 64:(hh + 1) * 64, c, :], lhsT=KHs(s)[:, h * 64:(h + 1) * 64],
                      rhs=V[:, h, :], start=True, stop=True, R=rkeys, W=["ps4kv"])
            for c in range(2):
                mk.op("dve", "scalar_tensor_tensor", out=S[c][:], in0=S[c][:], scalar=Gc[:, c, s:s + 1], in1=KVp[:, c, :],
                      op0=ALU.mult, op1=ALU.add, R=[f"{sk}S{c}", "ps4kv"] + rkeys, W=[f"{sk}S{c}"])
                mk.op("act", "activation", out=Sbf[c][:], in_=S[c][:], func=AF.Copy, R=[f"{sk}S{c}"], W=[f"{sk}Sbf{c}"])
        return okeys, Oh

    def hview(ap256, hh):
        return ap256.rearrange("p (c x e) -> p c x e", c=2, x=2)[:, :, hh, :]

    def make_finalize(P, n, yTb):
        st = dict(i=0)
        cent = [P.sb([128, 4, 64], F32, "cent") for _ in range(2)]
        sq = P.sb([128, 4, 64], F32, "sq")
        mm = [P.sb([128, 4], F32, "mm") for _ in range(2)]
        vv = [P.sb([128, 4], F32, "vv") for _ in range(2)]
        yy = [P.sb([128, 256], BF16, "yy") for _ in range(2)]

        def fin(tot, totkey, center, gate, gatekey, t):
            i = st["i"] % 2
            st["i"] += 1
            tv = tot.rearrange("p (h e) -> p h e", h=4)
            tk = list(totkey) if isinstance(totkey, (list, tuple)) else [totkey]
            src, skeys = tv, tk
            if center:
                mk.op("dve", "tensor_reduce", out=mm[i][:], in_=tv, axis=AX.X, op=ALU.add, R=tk, W=[f"fmm{i}"])
                mk.op("dve", "tensor_scalar", out=mm[i][:], in0=mm[i][:], scalar1=-1.0 / 64, scalar2=None, op0=ALU.mult,
                      R=[f"fmm{i}"], W=[f"fmm{i}"])
                mk.op("dve", "tensor_tensor", out=cent[i][:], in0=tv, in1=mm[i][:].unsqueeze(2).to_broadcast([128, 4, 64]),
                      op=ALU.add, R=tk + [f"fmm{i}"], W=[f"fcent{i}"])
                src, skeys = cent[i][:], [f"fcent{i}"]
            mk.op("pool", "tensor_tensor", out=sq[:], in0=src, in1=src, op=ALU.mult, R=skeys, W=["fsq"])
            mk.op("dve", "tensor_reduce", out=vv[i][:], in_=sq[:], axis=AX.X, op=ALU.add, R=["fsq"], W=[f"fvv{i}"])
            mk.op("act", "activation", out=vv[i][:], in_=vv[i][:], func=AF.Sqrt, scale=1.0 / 64, bias=EPS,
                  R=[f"fvv{i}"], W=[f"fvv{i}"])
            mk.op("dve", "reciprocal", out=vv[i][:], in_=vv[i][:], R=[f"fvv{i}"], W=[f"fvv{i}"])
            mk.op("dve", "tensor_tensor", out=cent[i][:], in0=src, in1=vv[i][:].unsqueeze(2).to_broadcast([128, 4, 64]),
                  op=ALU.mult, R=skeys + [f"fvv{i}"], W=[f"fcent{i}"])
            mk.op("dve", "tensor_tensor", out=yy[i][:], in0=cent[i][:].rearrange("p h e -> p (h e)"), in1=gate,
                  op=ALU.mult, R=[f"fcent{i}", gatekey], W=[f"fyy{i}"])
            for c in range(2):
                mk.op("pe", "transpose", out=PQ[1][:, (i * 2 + c) * 128:(i * 2 + c + 1) * 128],
                      in_=yy[i][:, c * 128:(c + 1) * 128], identity=identb[:], R=[f"fyy{i}", "identb"], W=[f"pq1f{i}"])
            mk.op("act", "activation", out=yTb[:, :, t * 128:(t + 1) * 128],
                  in_=PQ[1][:, i * 256:(i + 1) * 256].rearrange("p (c n) -> p c n", c=2), func=AF.Copy,
                  R=[f"pq1f{i}"], W=["yTb"])
        return fin

    def mixer_b(l, last=False):
        P = Pool(nc, f"mb{l}")
        QR = [P.sb([128, T], BF16, "QR") for _ in range(2)]
        KR = [P.sb([128, T], BF16, "KR") for _ in range(2)]
        thp = P.sb([128, 2, 2], F32, "thp")
        thh = P.sb([128, 2, 4], F32, "thh")
        mk.dma("sp", thp[:], b_thp[l], W=["thp"])
        mk.dma("sp", thh[:], b_thh[l], W=["thh"])
        for tt, key in ((thp, "thp"), (thh, "thh")):
            mk.op("act", "activation", out=tt[:], in_=tt[:], func=AF.Exp, scale=-1.0, R=[key], W=[key])
            mk.op("act", "activation", out=tt[:], in_=tt[:], func=AF.Ln, bias=1.0, R=[key], W=[key])
            mk.op("dve", "tensor_scalar", out=tt[:], in0=tt[:], scalar1=-1.0, scalar2=None, op0=ALU.mult, R=[key], W=[key])
        DM = P.sb([128, 2, 512], F32, "DM")
        QW = P.sb([128, 2, 2, 128], F32, "QW")
        KW = P.sb([128, 2, 4], F32, "KW")
        Gc = P.sb([128, 2, 2, 1], F32, "Gc")
        for dr in range(2):
            diff, msk, pos = (C("diffF"), C("maskF"), C("posF")) if dr == 0 else (C("diffB"), C("maskB"), C("posB"))
            for h in range(4):
                mk.op("act", "activation", out=DM[:, dr, h * 128:(h + 1) * 128], in_=diff, func=AF.Exp,
                      scale=thh[:, dr, h:h + 1], R=["cst", "thh"], W=["DM"])
                mk.op("dve", "tensor_tensor", out=DM[:, dr, h * 128:(h + 1) * 128], in0=DM[:, dr, h * 128:(h + 1) * 128],
                      in1=msk, op=ALU.mult, R=["DM", "cst"], W=["DM"])
                mk.op("act", "activation", out=KW[:, dr, h:h + 1], in_=C("kpos")[:, dr:dr + 1], func=AF.Exp,
                      scale=thh[:, dr, h:h + 1], R=["cst", "thh"], W=["KW"])
            for c in range(2):
                mk.op("act", "activation", out=QW[:, dr, c, :], in_=pos, func=AF.Exp, scale=thp[:, dr, c:c + 1],
                      R=["cst", "thp"], W=["QW"])
                mk.op("act", "activation", out=Gc[:, dr, c, :], in_=thp[:, dr, c:c + 1], func=AF.Exp, scale=128.0,
                      R=["thp"], W=["Gc"])
        segw = 1088
        f1 = [P.sb([128, segw], F32, "f1") for _ in range(2)]
        f2 = [P.sb([128, segw], F32, "f2") for _ in range(2)]
        rc = P.sb([128, T], F32, "rc")
        rsn = P.sb([128, T], F32, "rsn")
        mk.dma("sp", rc[:], ropeC_d, W=["rc"])
        mk.dma("sp", rsn[:], ropeS_d, W=["rsn"])
        n = 0
        for (dst, base, pbase, scale) in ((QR, "b_q", "b_qp", 1.0), (KR, "b_k", "b_kp", 0.125)):
            for c in range(2):
                for sg in range(4):
                    i = n % 2
                    n += 1
                    cs = slice(sg * segw, (sg + 1) * segw)
                    mk.dma("sp", f1[i][:], FM[FM_OFF[base] + c][:, cs], R=[f"FM{FM_OFF[base] + c}"], W=[f"f1{i}"])
                    mk.dma("sp", f2[i][:], FM[FM_OFF[pbase] + c][:, cs], R=[f"FM{FM_OFF[pbase] + c}"], W=[f"f2{i}"])
                    mk.op("dve", "tensor_tensor", out=f1[i][:], in0=f1[i][:], in1=rc[:, cs], op=ALU.mult,
                          R=[f"f1{i}", "rc"], W=[f"f1{i}"])
                    mk.op("pool", "tensor_tensor", out=f2[i][:], in0=f2[i][:], in1=rsn[:, cs], op=ALU.mult,
                          R=[f"f2{i}", "rsn"], W=[f"f2{i}"])
                    mk.op("dve", "tensor_tensor", out=f1[i][:], in0=f1[i][:], in1=f2[i][:], op=ALU.add,
                          R=[f"f1{i}", f"f2{i}"], W=[f"f1{i}"])
                    mk.op("act", "activation", out=dst[c][:, cs], in_=f1[i][:], func=AF.Copy, scale=scale,
                          R=[f"f1{i}"], W=[f"b{base}{c}"])
        rkeys = ["bb_q0", "bb_q1", "bb_k0", "bb_k1"]
        OF = P.sb([128, NT, 256], F32, "OF")
        yTb = P.sb([128, 2, T], BF16, "yTb")
        fin = make_finalize(P, 1, yTb)
        PT = [P.sb([128, 512], BF16, "PT") for _ in range(2)]
        QIT = [[P.sb([128, 128], BF16, "QIT") for _ in range(2)] for _ in range(2)]
        KH = [P.sb([128, 256], BF16, "KH") for _ in range(2)]
        Vf = [P.sb([128, 512], F32, "Vf") for _ in range(2)]
        Vb = [P.sb([128, 4, 64], BF16, "Vb") for _ in range(2)]
        gt = [P.sb([128, 256], F32, "gt") for _ in range(2)]
        tot = [P.sb([128, 256], F32, "tot") for _ in range(2)]
        S = [P.sb([128, 64], F32, "S") for _ in range(2)]
        Sbf = [P.sb([128, 64], BF16, "Sbf") for _ in range(2)]
        it = 0
        for dr in range(2):
            for c in range(2):
                mk.op("pool", "memset", S[c][:], 0.0, W=[f"bS{c}"])
                mk.op("pool", "memset", Sbf[c][:], 0.0, W=[f"bSbf{c}"])
            def body(t, full, it):
                i = it % 2
                cols = slice(t * 128, (t + 1) * 128)
                mk.dma("sp", Vf[i][:], TM[t * 128:(t + 1) * 128, 0:512], R=["TM"], W=[f"bVf{i}"])
                mk.op("pool", "tensor_copy", out=Vb[i][:], in_=Vf[i][:, 0:256].rearrange("p (h e) -> p h e", h=4),
                      R=[f"bVf{i}"], W=[f"bVb{i}"])
                for c in range(2):
                    if full:
                        mk.op("pool", "tensor_tensor", out=QIT[i][c][:], in0=QR[c][:, cols], in1=QW[:, dr, c, :],
                              op=ALU.mult, R=[f"bb_q{c}", "QW"], W=[f"bQIT{i}"])
                    mk.op("pe", "transpose", out=PQ[0][:, (i * 2 + c) * 128:(i * 2 + c + 1) * 128], in_=KR[c][:, cols],
                          identity=identb[:], R=[f"bb_k{c}", "identb"], W=[f"pq0k{i}"])
                for h in range(4):
                    mk.op("act", "activation", out=KH[i][:, h * 64:(h + 1) * 64],
                          in_=PQ[0][:, i * 256 + h * 64:i * 256 + (h + 1) * 64], func=AF.Identity, scale=KW[:, dr, h:h + 1],
                          R=[f"pq0k{i}", "KW"], W=[f"bKH{i}"])
                yield
                okeys, Oh = chunk_core(it, 1, dr, [QR[0][:, cols], QR[1][:, cols]], [KR[0][:, cols], KR[1][:, cols]],
                                       [QIT[i][0], QIT[i][1]], (lambda s_, kh=KH[i]: kh), Vb[i], 64, Gc[:, dr], S, Sbf,
                                       DM[:, dr, :], "DM", rkeys + [f"bQIT{i}", f"bKH{i}", f"bVb{i}"], PT, "b", full=full)
                if not full:
                    pass
                elif dr == 0:
                    for hh in range(2):
                        mk.op("act", "activation", out=hview(OF[:, t, :], hh), in_=Oh[hh], func=AF.Copy, R=[okeys[hh]],
                              W=[f"bOF{t}h{hh}"])
                else:
                    for hh in range(2):
                        mk.op("dve", "tensor_tensor", out=hview(tot[i][:], hh), in0=Oh[hh], in1=hview(OF[:, t, :], hh),
                              op=ALU.add, R=[okeys[hh], f"bOF{t}h{hh}"], W=[f"btot{i}h{hh}"])
                    mk.op("act", "activation", out=gt[i][:], in_=Vf[i][:, 256:512], func=AF.Silu, R=[f"bVf{i}"],
                          W=[f"bgt{i}"])
                    fin(tot[i][:], [f"btot{i}h0", f"btot{i}h1"], True, gt[i][:], f"bgt{i}", t)
            gens = []
            for t, full in plan(dr, last):
                gens.append(body(t, full, it))
                it += 1
            run_pipelined(gens, pipelined=not last)
        for c in range(2):
            mk.dma("pool", YT[1, c], yTb[:, c, :], R=["yTb"], W=[f"YT1{c}"])
        mk.barrier()
        P.close()

    def mixer_c(l, last=False):
        P = Pool(nc, f"mc{l}")
        QC = [P.sb([128, T], BF16, "QC") for _ in range(2)]
        KC = [P.sb([128, T], BF16, "KC") for _ in range(2)]
        cw = P.sb([128, 4, 5], F32, "cw")
        cb = P.sb([128, 4], F32, "cb")
        gbias = P.sb([128, 16], F32, "gbias")
        mk.dma("sp", cw[:], c_cw[l], W=["c_cw"])
        mk.dma("sp", cb[:], c_cb[l], W=["c_cb"])
        mk.dma("sp", gbias[:], c_gb[l], W=["c_gb"])
        src = P.sb([128, T], F32, "src")
        u = P.sb([128, T], F32, "u")
        for ch in range(4):
            fc = FM_OFF["c_q"] + ch
            mk.dma("sp", src[:], FM[fc], R=[f"FM{fc}"], W=["csrc"])
            conv_fm(u, src, cw[:, ch, :], cb[:, ch:ch + 1], "cu", "csrc", ["c_cw", "c_cb"])
            dst = QC[ch] if ch < 2 else KC[ch - 2]
            mk.op("act", "activation", out=u[:], in_=u[:], func=AF.Silu, R=["cu"], W=["cu"])
            mk.op("dve", "tensor_scalar", out=dst[:], in0=u[:], scalar1=(1.0 if ch < 2 else 0.125), scalar2=None,
                  op0=ALU.mult, R=["cu"], W=[f"cqk{ch}"])
        Z = P.sb([128, NT, 16], F32, "Z")
        LFN = P.sb([128, NT, 16], F32, "LFN")
        mk.dma("sp", Z[:], TM[:, TM_OFF["c_gates"]:TM_OFF["c_gates"] + 16].rearrange("(t p) g -> p t g", p=128),
               R=["TM"], W=["cZ"])
        mk.op("dve", "tensor_tensor", out=Z[:], in0=Z[:], in1=gbias[:].unsqueeze(1).to_broadcast([128, NT, 16]),
              op=ALU.add, R=["cZ", "c_gb"], W=["cZ"])
        mk.op("act", "activation", out=LFN[:], in_=Z[:], func=AF.Exp, scale=-1.0, R=["cZ"], W=["cLFN"])
        mk.op("act", "activation", out=LFN[:], in_=LFN[:], func=AF.Ln, bias=1.0, R=["cLFN"], W=["cLFN"])
        mk.op("dve", "tensor_scalar", out=LFN[:], in0=LFN[:], scalar1=-1.0, scalar2=None, op0=ALU.mult, R=["cLFN"],
              W=["cLFN"])
        rkeys = ["cqk0", "cqk1", "cqk2", "cqk3"]
        OF = P.sb([128, NT, 256], F32, "OF")
        yTb = P.sb([128, 2, T], BF16, "yTb")
        fin = make_finalize(P, 2, yTb)
        PT = [P.sb([128, 512], BF16, "PT") for _ in range(2)]
        QIT = [[P.sb([128, 128], BF16, "QIT") for _ in range(2)] for _ in range(2)]
        KH = [P.sb([128, 256], BF16, "KH") for _ in range(2)]
        Vf = [P.sb([128, 512], F32, "Vf") for _ in range(2)]
        Vb = [P.sb([128, 4, 65], BF16, "Vb") for _ in range(2)]
        gt = [P.sb([128, 256], F32, "gt") for _ in range(2)]
        tot = [P.sb([128, 256], F32, "tot") for _ in range(2)]
        S = [P.sb([128, 65], F32, "S") for _ in range(2)]
        Sbf = [P.sb([128, 65], BF16, "Sbf") for _ in range(2)]
        Bm4 = [P.sb([128, 4, 128], F32, "Bm4") for _ in range(2)]
        tmp4 = [P.sb([128, 512], F32, "tmp4") for _ in range(2)]
        Dm4 = [P.sb([128, 512], F32, "Dm4") for _ in range(2)]
        EB4 = [P.sb([128, 512], F32, "EB4") for _ in range(2)]
        lmb = [P.sb([128, 4], F32, "lmb") for _ in range(2)]
        kw = [P.sb([128, 4], F32, "kw") for _ in range(2)]
        Gc = [P.sb([128, 2, 1], F32, "Gc") for _ in range(2)]
        rden = [P.sb([128, 4], F32, "rden") for _ in range(2)]
        ebe = [P.sb([128, 4], F32, "ebe") for _ in range(2)]
        hid = [P.sb([128, 4, 64], F32, "hid") for _ in range(2)]
        for i in range(2):
            mk.op("pool", "memset", Vb[i][:], 1.0, W=[f"cVb{i}"])
        ones = C("ones")
        it = 0
        for dr in range(2):
            tri = C("triF") if dr == 0 else C("triB")
            neg4 = C("negF4") if dr == 0 else C("negB4")
            e = 127 if dr == 0 else 0
            for c in range(2):
                mk.op("pool", "memset", S[c][:], 0.0, W=[f"cS{c}"])
                mk.op("pool", "memset", Sbf[c][:], 0.0, W=[f"cSbf{c}"])
            def body(t, full, it):
                i = it % 2
                cols = slice(t * 128, (t + 1) * 128)
                li = Z[:, t, dr * 8:dr * 8 + 4]
                lf = LFN[:, t, dr * 8 + 4:dr * 8 + 8]
                mk.dma("sp", Vf[i][:], TM[t * 128:(t + 1) * 128, 512:1024], R=["TM"], W=[f"cVf{i}"])
                mk.op("pool", "tensor_copy", out=Vb[i][:, :, 0:64], in_=Vf[i][:, 0:256].rearrange("p (h e) -> p h e", h=4),
                      R=[f"cVf{i}"], W=[f"cVb{i}"])
                mk.op("dve", "tensor_tensor", out=Bm4[i][:], in0=tri.unsqueeze(1).to_broadcast([128, 4, 128]),
                      in1=lf.unsqueeze(2).to_broadcast([128, 4, 128]), op=ALU.mult, R=["cst", "cLFN"], W=[f"cBm{i}"])
                mk.op("pe", "matmul", PS[5][:, :], lhsT=ones, rhs=Bm4[i][:].rearrange("p h n -> p (h n)"), start=True,
                      stop=True, R=["cst", f"cBm{i}"], W=["ps5"])
                mk.op("pe", "matmul", PS[4][:, 256:260], lhsT=tri, rhs=lf, start=True, stop=True, R=["cst", "cLFN"],
                      W=["ps4b"])
                mk.op("dve", "tensor_tensor", out=lmb[i][:], in0=li, in1=PS[4][:, 256:260], op=ALU.subtract,
                      R=["cZ", "ps4b"], W=[f"clmb{i}"])
                if full:
                    mk.op("dve", "tensor_tensor", out=tmp4[i][:], in0=PS[5][:, :], in1=neg4, op=ALU.add, R=["ps5", "cst"],
                          W=[f"ctmp{i}"])
                    for h in range(4):
                        mk.op("act", "activation", out=Dm4[i][:, h * 128:(h + 1) * 128],
                              in_=tmp4[i][:, h * 128:(h + 1) * 128], func=AF.Exp, bias=lmb[i][:, h:h + 1],
                              R=[f"ctmp{i}", f"clmb{i}"], W=[f"cDm{i}"])
                    mk.op("act", "activation", out=EB4[i][:], in_=PS[5][:, :], func=AF.Exp, R=["ps5"], W=[f"cEB{i}"])
                bend = PS[5][:, :].rearrange("p (h n) -> p h n", h=4)[:, :, e]
                mk.op("dve", "tensor_tensor", out=kw[i][:], in0=lmb[i][:], in1=bend, op=ALU.add, R=[f"clmb{i}", "ps5"],
                      W=[f"ckw{i}"])
                mk.op("act", "activation", out=kw[i][:], in_=kw[i][:], func=AF.Exp, R=[f"ckw{i}"], W=[f"ckw{i}"])
                mk.op("act", "activation", out=ebe[i][:], in_=bend, func=AF.Exp, R=["ps5"], W=[f"cebe{i}"])
                for h in range(4):
                    c, hh = h // 2, h % 2
                    rs = slice(hh * 64, (hh + 1) * 64)
                    mk.op("pool", "tensor_copy", out=Gc[i][rs, c, :], in_=ebe[i][rs, h:h + 1],
                          R=[f"cebe{i}"], W=[f"cGc{i}"])
                    if full:
                        mk.op("pool", "tensor_tensor", out=QIT[i][c][rs, :], in0=QC[c][rs, cols],
                              in1=EB4[i][rs, h * 128:(h + 1) * 128], op=ALU.mult, R=[f"cqk{c}", f"cEB{i}"],
                              W=[f"cQIT{i}"])
                for c in range(2):
                    mk.op("pe", "transpose", out=PQ[0][:, (i * 2 + c) * 128:(i * 2 + c + 1) * 128], in_=KC[c][:, cols],
                          identity=identb[:], R=[f"cqk{2 + c}", "identb"], W=[f"pq0k{i}"])
                for h in range(4):
                    mk.op("act", "activation", out=KH[i][:, h * 64:(h + 1) * 64],
                          in_=PQ[0][:, i * 256 + h * 64:i * 256 + (h + 1) * 64], func=AF.Identity, scale=kw[i][:, h:h + 1],
                          R=[f"pq0k{i}", f"ckw{i}"], W=[f"cKH{i}"])
                yield
                okeys, Oh = chunk_core(it, 1, dr, [QC[0][:, cols], QC[1][:, cols]], [KC[0][:, cols], KC[1][:, cols]],
                                       [QIT[i][0], QIT[i][1]], (lambda s_, kh=KH[i]: kh), Vb[i], 65, Gc[i], S, Sbf,
                                       Dm4[i][:], f"cDm{i}",
                                       rkeys + [f"cQIT{i}", f"cKH{i}", f"cVb{i}", f"cGc{i}"], PT, "c", full=full)
                if not full:
                    return
                rdv = rden[i][:].rearrange("p (c x) -> p c x", c=2)
                for hh in range(2):
                    mk.op("act", "activation", out=rdv[:, :, hh], in_=Oh[hh][:, :, 64], func=AF.Abs, R=[okeys[hh]],
                          W=[f"crden{i}"])
                mk.op("dve", "tensor_scalar_max", out=rden[i][:], in0=rden[i][:], scalar1=1.0, R=[f"crden{i}"],
                      W=[f"crden{i}"])
                mk.op("dve", "reciprocal", out=rden[i][:], in_=rden[i][:], R=[f"crden{i}"], W=[f"crden{i}"])
                if dr == 0:
                    for hh in range(2):
                        mk.op("dve", "tensor_tensor", out=hview(OF[:, t, :], hh), in0=Oh[hh][:, :, 0:64],
                              in1=rdv[:, :, hh:hh + 1].to_broadcast([128, 2, 64]), op=ALU.mult,
                              R=[okeys[hh], f"crden{i}"], W=[f"cOF{t}h{hh}"])
                else:
                    for hh in range(2):
                        mk.op("dve", "tensor_tensor", out=hview(hid[i][:].rearrange("p h e -> p (h e)"), hh),
                              in0=Oh[hh][:, :, 0:64], in1=rdv[:, :, hh:hh + 1].to_broadcast([128, 2, 64]), op=ALU.mult,
                              R=[okeys[hh], f"crden{i}"], W=[f"chid{i}h{hh}"])
                    mk.op("pool", "tensor_tensor", out=tot[i][:], in0=hid[i][:].rearrange("p h e -> p (h e)"),
                          in1=OF[:, t, :], op=ALU.add, R=[f"chid{i}h0", f"chid{i}h1", f"cOF{t}h0", f"cOF{t}h1"],
                          W=[f"ctot{i}"])
                    mk.op("act", "activation", out=gt[i][:], in_=Vf[i][:, 256:512], func=AF.Sigmoid, R=[f"cVf{i}"],
                          W=[f"cgt{i}"])
                    fin(tot[i][:], f"ctot{i}", True, gt[i][:], f"cgt{i}", t)
            gens = []
            for t, full in plan(dr, last):
                gens.append(body(t, full, it))
                it += 1
            run_pipelined(gens, pipelined=not last)
        for c in range(2):
            mk.dma("pool", YT[2, c], yTb[:, c, :], R=["yTb"], W=[f"YT2{c}"])
        mk.barrier()
        P.close()

    def make_wconv(P, l):
        wstage = P.sb([128, 8, 512], F32, "wstage")
        wcb = P.sb([128, 4096], BF16, "wcb")
        tasks = []
        for e in range(16):
            tasks.append((moe_w1[l, e].rearrange("(c p) f -> p c f", p=128), wstage[:], WALL[e][:, 0:4096], f"W1B{e}"))
            tasks.append((moe_w3[l, e].rearrange("(c p) f -> p c f", p=128), wstage[:], WALL[e][:, 4096:8192], f"W3B{e}"))
            tasks.append((moe_w2[l, e].rearrange("(c p) f -> p c f", p=128),
                          wstage[:].rearrange("p c f -> p (c f)").rearrange("p (c f) -> p c f", c=4),
                          WALL[e][:, 8192:12288], f"W2B{e}"))
        st = dict(k=0)

        def step():
            if st["k"] >= len(tasks):
                return False
            src, stg, dst, key = tasks[st["k"]]
            st["k"] += 1
            mk.dma("sp", stg, src, W=["wstage"])
            mk.ev(wcb[:], wstage[:].rearrange("p c f -> p (c f)"), R=["wstage"], W=["wcb"])
            mk.dma("pool", dst, wcb[:], R=["wcb"], W=[key])
            return True
        return step

    def mixer_d(l, last=False):
        P = Pool(nc, f"md{l}")
        LB = P.sb([128, 256], F32, "LB")
        OML = P.sb([128, 256], F32, "OML")
        if l == 0:
            use_lb = False
        else:
            use_lb = True
            dl = P.sb([128, 2, 256], F32, "dl")
            mk.dma("sp", dl[:], d_lbr, W=["dl"])
            mk.op("dve", "tensor_tensor", out=LB[:], in0=dl[:, 1, :], in1=dl[:, 0, :], op=ALU.subtract, R=["dl"], W=["LB"])
            mk.op("act", "activation", out=LB[:], in_=LB[:], func=AF.Sigmoid, R=["LB"], W=["LB"])
            mk.op("dve", "tensor_scalar", out=OML[:], in0=LB[:], scalar1=-1.0, scalar2=1.0, op0=ALU.mult, op1=ALU.add,
                  R=["LB"], W=["OML"])
        OF = P.sb([128, NT, 256], F32, "OF")
        yTb = P.sb([128, 2, T], BF16, "yTb")
        fin = make_finalize(P, 3, yTb)
        assert DNS == 4
        wconv_step = make_wconv(P, l)
        PT = [P.sb([128, 512], BF16, "PT") for _ in range(2)]
        X = [P.sb([128, 1280], F32, "X") for _ in range(2)]
        ff = [P.sb([128, 256], F32, "ff") for _ in range(2)]
        lf = [P.sb([128, 256], F32, "lf") for _ in range(2)]
        kk = [P.sb([128, 256], F32, "kk") for _ in range(2)]
        qs = [P.sb([128, 256], F32, "qs") for _ in range(2)]
        ee = [P.sb([128, 512], F32, "ee") for _ in range(2)]
        ek = [P.sb([128, 256], F32, "ek") for _ in range(2)]
        qk = [P.sb([128, 512], BF16, "qk") for _ in range(2)]
        KTs = [P.sb([128, 2, 128], BF16, "KTs") for _ in range(2)]
        QM = [[P.sb([128, 2, 5, 128], BF16, "QM") for _ in range(2)] for _ in range(2)]
        KH = [P.sb([128, DNS, 256], BF16, "KH") for _ in range(2)]
        Vb = [P.sb([128, 4, 64], BF16, "Vb") for _ in range(2)]
        gt = [P.sb([128, 256], F32, "gt") for _ in range(2)]
        tot = [P.sb([128, 256], F32, "tot") for _ in range(2)]
        red = [P.sb([128, 2, 256], F32, "red") for _ in range(2)]
        Gc = [P.sb([128, 2, DNS], F32, "Gc") for _ in range(2)]
        S = [P.sb([128, 64], F32, "S") for _ in range(2)]
        Sbf = [P.sb([128, 64], BF16, "Sbf") for _ in range(2)]
        qmask = C("qmask").rearrange("p (x s n) -> p x s n", x=2, s=5)
        it = 0
        for dr in range(2):
            blk = C("blkF") if dr == 0 else C("blkB")
            rem = C("aftF") if dr == 0 else C("befB")
            msk4 = C("mblkF4") if dr == 0 else C("mblkB4")
            zoff = 256 if dr == 0 else 512
            subs = list(range(DNS)) if dr == 0 else list(range(DNS - 1, -1, -1))
            for c in range(2):
                mk.op("pool", "memset", S[c][:], 0.0, W=[f"dS{c}"])
                mk.op("pool", "memset", Sbf[c][:], 0.0, W=[f"dSbf{c}"])
            def body(t, full, it):
                i = it % 2
                mk.dma("sp", X[i][:], TM[t * 128:(t + 1) * 128, 1024:2304], R=["TM"], W=[f"dX{i}"])
                wconv_step()
                mk.op("act", "activation", out=ff[i][:], in_=X[i][:, zoff:zoff + 256], func=AF.Sigmoid, R=[f"dX{i}"],
                      W=[f"dff{i}"])
                if use_lb:
                    mk.op("dve", "tensor_tensor", out=ff[i][:], in0=ff[i][:], in1=OML[:], op=ALU.mult, R=[f"dff{i}", "OML"],
                          W=[f"dff{i}"])
                    mk.op("dve", "tensor_tensor", out=ff[i][:], in0=ff[i][:], in1=LB[:], op=ALU.add, R=[f"dff{i}", "LB"],
                          W=[f"dff{i}"])
                mk.op("act", "activation", out=lf[i][:], in_=ff[i][:], func=AF.Ln, R=[f"dff{i}"], W=[f"dlf{i}"])
                mk.op("pool", "tensor_scalar", out=kk[i][:], in0=ff[i][:], scalar1=-1.0, scalar2=1.0, op0=ALU.mult,
                      op1=ALU.add, R=[f"dff{i}"], W=[f"dkk{i}"])
                if full:
                    mk.op("pe", "matmul", PS[5][:, 0:256], lhsT=blk, rhs=lf[i][:], start=True, stop=True,
                          R=["cst", f"dlf{i}"], W=["ps5"])
                mk.op("pe", "matmul", PS[5][:, 256:512], lhsT=rem, rhs=lf[i][:], start=True, stop=True,
                      R=["cst", f"dlf{i}"], W=["ps5"])
                for c in range(2):
                    mk.op("pe", "matmul", PS[1][:, 384 + c * DNS:384 + (c + 1) * DNS], lhsT=lf[i][:, c * 128:(c + 1) * 128],
                          rhs=C("subm"), start=True, stop=True, R=["cst", f"dlf{i}"], W=["ps1g"])
                mk.op("act", "activation", out=Gc[i][:].rearrange("p c s -> p (c s)"), in_=PS[1][:, 384:384 + 2 * DNS],
                      func=AF.Exp, R=["ps1g"], W=[f"dGc{i}"])
                if full:
                    mk.op("act", "activation", out=ee[i][:], in_=PS[5][:, :], func=AF.Exp, R=["ps5"], W=[f"dee{i}"])
                    mk.op("act", "activation", out=ek[i][:], in_=PS[5][:, 0:256], func=AF.Exp, scale=-1.0, R=["ps5"],
                          W=[f"dek{i}"])
                    mk.op("act", "activation", out=qs[i][:], in_=X[i][:, 0:256], func=AF.Silu, R=[f"dX{i}"],
                          W=[f"dqs{i}"])
                    mk.op("dve", "tensor_tensor", out=qk[i][:, 0:256], in0=qs[i][:], in1=ee[i][:, 0:256], op=ALU.mult,
                          R=[f"dqs{i}", f"dee{i}"], W=[f"dqk{i}a"])
                    mk.op("dve", "tensor_tensor", out=qk[i][:, 256:512], in0=kk[i][:], in1=ek[i][:], op=ALU.mult,
                          R=[f"dkk{i}", f"dek{i}"], W=[f"dqk{i}c"])
                else:
                    mk.op("act", "activation", out=ee[i][:, 256:512], in_=PS[5][:, 256:512], func=AF.Exp, R=["ps5"],
                          W=[f"dee{i}"])
                for s_ in range(DNS):
                    mk.op("dve", "scalar_tensor_tensor", out=KH[i][:, s_, :], in0=kk[i][:], scalar=C("subm")[:, s_:s_ + 1],
                          in1=ee[i][:, 256:512], op0=ALU.mult, op1=ALU.mult, R=[f"dkk{i}", f"dee{i}", "cst"],
                          W=[f"dKH{i}s{s_}"])
                mk.op("pool", "tensor_copy", out=Vb[i][:], in_=X[i][:, 768:1024].rearrange("p (h e) -> p h e", h=4),
                      R=[f"dX{i}"], W=[f"dVb{i}"])
                if full:
                    for j in range(4):
                        mk.op("pe", "transpose", out=PQ[0][:, (i * 4 + j) * 128:(i * 4 + j + 1) * 128],
                              in_=qk[i][:, j * 128:(j + 1) * 128], identity=identb[:], R=[f"dqk{i}a", f"dqk{i}c", "identb"],
                              W=[f"pq0d{i}"])
                    mk.op("act", "activation", out=KTs[i][:].rearrange("p c n -> p (c n)"),
                          in_=PQ[0][:, i * 512 + 256:i * 512 + 512], func=AF.Copy, R=[f"pq0d{i}"], W=[f"dKT{i}"])
                    for c in range(2):
                        mk.op("dve", "tensor_tensor", out=QM[i][c][:].rearrange("p x s n -> p (x s) n"),
                              in0=PQ[0][:, i * 512 + c * 128:i * 512 + (c + 1) * 128].unsqueeze(1).to_broadcast([128, 10, 128]),
                              in1=qmask.rearrange("p x s n -> p (x s) n"), op=ALU.mult, R=[f"pq0d{i}", "cst"],
                              W=[f"dQM{i}{c}"])
                yield
                if full:
                    for h in range(4):
                        c, hh = h // 2, h % 2
                        mk.op("pe", "matmul", PS[0][:, h * 128:(h + 1) * 128], lhsT=KTs[i][:, c, :], rhs=QM[i][c][:, hh, 4, :],
                              start=True, stop=True, R=[f"dKT{i}", f"dQM{i}{c}"], W=["ps0"])
                    mk.op("dve", "tensor_tensor", out=PT[i][:], in0=PS[0][:, :], in1=msk4, op=ALU.mult, R=["ps0", "cst"],
                          W=[f"dPT{i}"])
                    for h in range(4):
                        mk.op("pe", "matmul", PS[1][:, h * 64:(h + 1) * 64], lhsT=PT[i][:, h * 128:(h + 1) * 128],
                              rhs=Vb[i][:, h, :], start=True, stop=True, R=[f"dPT{i}", f"dVb{i}"], W=["ps1i"])
                KVp = PS[1][:, 256:384].rearrange("p (c e) -> p c e", c=2)
                for s_ in subs:
                    bank = PS[2 + s_ // 2]
                    for h in (range(4) if full else ()):
                        c, hh = h // 2, h % 2
                        col = ((s_ % 2) * 4 + h) * 64
                        mk.op("pe", "matmul", bank[:, col:col + 64], lhsT=QM[i][c][:, hh, s_, :], rhs=Sbf[c][:, :],
                              start=True, stop=True, R=[f"dQM{i}{c}", f"dSbf{c}"], W=[f"ps{2 + s_ // 2}"])
                    for h in range(4):
                        c, hh = h // 2, h % 2
                        mk.op("pe", "matmul", KVp[hh * 64:(hh + 1) * 64, c, :], lhsT=KH[i][:, s_, h * 64:(h + 1) * 64],
                              rhs=Vb[i][:, h, :], start=True, stop=True, R=[f"dKH{i}s{s_}", f"dVb{i}"], W=["ps1kv"])
                    for c in range(2):
                        mk.op("dve", "scalar_tensor_tensor", out=S[c][:], in0=S[c][:], scalar=Gc[i][:, c, s_:s_ + 1],
                              in1=KVp[:, c, :], op0=ALU.mult, op1=ALU.add, R=[f"dS{c}", "ps1kv", f"dGc{i}"], W=[f"dS{c}"])
                        mk.op("act", "activation", out=Sbf[c][:], in_=S[c][:], func=AF.Copy, R=[f"dS{c}"], W=[f"dSbf{c}"])
                if not full:
                    return
                for b_ in range(2):
                    mk.op("dve", "tensor_reduce", out=red[i][:, b_, :],
                          in_=PS[2 + b_][:, :].rearrange("p (s x) -> p x s", s=2), axis=AX.X, op=ALU.add,
                          R=[f"ps{2 + b_}"], W=[f"dred{i}{b_}"])
                mk.op("dve", "tensor_tensor", out=tot[i][:], in0=PS[1][:, 0:256], in1=red[i][:, 0, :], op=ALU.add,
                      R=["ps1i", f"dred{i}0"], W=[f"dtot{i}"])
                if dr == 0:
                    mk.op("pool", "tensor_tensor", out=OF[:, t, :], in0=tot[i][:], in1=red[i][:, 1, :], op=ALU.add,
                          R=[f"dtot{i}", f"dred{i}1"], W=[f"dOF{t}"])
                else:
                    mk.op("pool", "tensor_tensor", out=tot[i][:], in0=tot[i][:], in1=red[i][:, 1, :], op=ALU.add,
                          R=[f"dtot{i}", f"dred{i}1"], W=[f"dtot{i}"])
                    mk.op("dve", "tensor_tensor", out=tot[i][:], in0=tot[i][:], in1=OF[:, t, :], op=ALU.add,
                          R=[f"dtot{i}", f"dOF{t}"], W=[f"dtot{i}"])
                    mk.op("act", "activation", out=gt[i][:], in_=X[i][:, 1024:1280], func=AF.Silu, R=[f"dX{i}"],
                          W=[f"dgt{i}"])
                    fin(tot[i][:], f"dtot{i}", False, gt[i][:], f"dgt{i}", t)
            gens = []
            for t, full in plan(dr, last):
                gens.append(body(t, full, it))
                it += 1
            run_pipelined(gens, pipelined=not last)
        while wconv_step():
            pass
        for c in range(2):
            mk.dma("pool", YT[3, c], yTb[:, c, :], R=["yTb"], W=[f"YT3{c}"])
        mk.barrier()
        P.close()

    NSMAX = (T + 4 * (SLOT - 1)) // SLOT
    H2U = dscr("H2U", [T, D], BF16)
    HS = dscr("HS", [NSMAX * SLOT, D], BF16)
    GWS = dscr("GWS", [NSMAX * SLOT, 4])
    OS = dscr("OS", [NSMAX * SLOT, D])
    WALL = dscr("WALL", [16, 128, 3 * 4096], BF16)

    def merge_moe_phase(l, last, tiles=None):
        if tiles is None:
            tiles = list(OUT_T) if last else list(range(NT))
        PO = Pool(nc, f"mo{l}")
        GOH = PO.sb([128, NT, 4], F32, "GOH")
        GW = PO.sb([128, NT, 4], F32, "GW")
        merge_part(l, tiles, GOH, GW)
        if DBG.get("moe", True):
            moe_sparse(l, tiles, GOH, GW)
        PO.close()

    def merge_part(l, tiles, GOH, GW):
        P = Pool(nc, f"mm{l}")
        wbr = P.sb([128, 8, D], BF16, "wbr")
        wo = P.sb([128, 8, D], BF16, "wo")
        wstage = P.sb([128, 8, 512], F32, "wstage")
        for half in range(2):
            mk.dma("sp", wstage[:], w_branch[l].rearrange("n (c p) f -> p (n c) f", p=128)[:, :, half * 512:(half + 1) * 512],
                   W=["wstage"])
            mk.ev(wbr[:, :, half * 512:(half + 1) * 512], wstage[:], R=["wstage"], W=["wbr"])
        for half in range(2):
            mk.dma("sp", wstage[:], w_out[l].rearrange("(c p) f -> p c f", p=128)[:, :, half * 512:(half + 1) * 512],
                   W=["wstage"])
            mk.ev(wo[:, :, half * 512:(half + 1) * 512], wstage[:], R=["wstage"], W=["wo"])
        wgr = P.sb([128, 8, 20], F32, "wgr")
        bgr = P.sb([1, 20], F32, "bgr")
        mk.dma("sp", wgr[:], moe_wgr[l].rearrange("(c p) f -> p c f", p=128), W=["wgr"])
        mk.dma("sp", bgr[:], moe_bgr[l], W=["bgr"])
        identf = C("ident")
        ones = C("ones")
        norm = make_norm(P, 2)
        xnew = [P.sb([128, D], F32, "xnew") for _ in range(2)]
        h2Tt = [P.sb([128, 8, 128], BF16, "h2Tt") for _ in range(2)]
        h2tok = [P.sb([128, D], BF16, "h2tok") for _ in range(2)]
        yt = [P.sb([128, 8, 128], BF16, "yt") for _ in range(2)]
        mg = [P.sb([128, 4096], BF16, "mg") for _ in range(2)]
        xt = [P.sb([128, D], F32, "xt") for _ in range(2)]
        zz = [P.sb([128, D], F32, "zz") for _ in range(2)]
        zt = [P.sb([128, D], F32, "zt") for _ in range(2)]
        zb = [P.sb([128, D], BF16, "zb") for _ in range(2)]
        zT = [P.sb([128, 8, 128], BF16, "zT") for _ in range(2)]
        rt = {"gs": P.sb([128, 1], F32, "rtgs")}
        LG = P.sb([128, NT, 20], F32, "LG")
        nit = 0
        for t in tiles:
            i = nit % 2
            nit += 1
            j = cond_of(t)
            cols = slice(t * 128, (t + 1) * 128)
            h2f = zz[i]
            h2fT = zt[i][:, :].rearrange("p (c n) -> p c n", c=8)
            mk.dma("sp", yt[i][:], YT[:, :, :, cols].rearrange("n c p t -> p (n c) t"),
                   R=[f"YT{n}{c}" for n in range(4) for c in range(2)], W=[f"yt{i}"])
            mk.dma("sp", mg[i][:], MG[cols, :], R=["MG"], W=[f"mg{i}"])
            mk.dma("sp", xt[i][:], XR[cols, :], R=[f"XR{t}"], W=[f"mxt{i}"])
            for n in range(4):
                for cb in range(2):
                    pb = (n * 2 + cb) % 2
                    for c in range(2):
                        mk.op("pe", "matmul", PS[pb][:, :], lhsT=yt[i][:, n * 2 + c, :],
                              rhs=wbr[:, n * 2 + c, cb * 512:(cb + 1) * 512], start=(c == 0), stop=(c == 1),
                              R=[f"yt{i}", "wbr"], W=[f"ps{pb}"])
                    dst = zz[i] if n == 0 else zt[i]
                    dkey = f"zz{i}" if n == 0 else f"zt{i}"
                    mk.op("dve", "tensor_tensor", out=dst[:, cb * 512:(cb + 1) * 512], in0=PS[pb][:, :],
                          in1=mg[i][:, n * 1024 + cb * 512:n * 1024 + (cb + 1) * 512], op=ALU.mult,
                          R=[f"ps{pb}", f"mg{i}"], W=[dkey])
                    if n > 0:
                        mk.op("pool", "tensor_tensor", out=zz[i][:, cb * 512:(cb + 1) * 512],
                              in0=zz[i][:, cb * 512:(cb + 1) * 512], in1=zt[i][:, cb * 512:(cb + 1) * 512], op=ALU.add,
                              R=[f"zz{i}", f"zt{i}"], W=[f"zz{i}"])
            mk.op("act", "activation", out=zb[i][:], in_=zz[i][:], func=AF.Copy, R=[f"zz{i}"], W=[f"zb{i}"])
            for c in range(8):
                mk.op("pe", "transpose", out=PQ[1][:, c * 128:(c + 1) * 128], in_=zb[i][:, c * 128:(c + 1) * 128],
                      identity=identb[:], R=[f"zb{i}", "identb"], W=["pq1"])
            mk.ev(zT[i][:].rearrange("p c n -> p (c n)"), PQ[1][:, :], R=["pq1"], W=[f"zT{i}"])
            for cb in range(2):
                pb = 2 + cb
                for c in range(8):
                    mk.op("pe", "matmul", PS[pb][:, :], lhsT=zT[i][:, c, :], rhs=wo[:, c, cb * 512:(cb + 1) * 512],
                          start=(c == 0), stop=(c == 7), R=[f"zT{i}", "wo"], W=[f"ps{pb}"])
                mk.op("dve", "tensor_tensor", out=zt[i][:, cb * 512:(cb + 1) * 512], in0=PS[pb][:, :],
                      in1=GB[:, j, 0, cb * 512:(cb + 1) * 512], op=ALU.mult, R=[f"ps{pb}", "GB"], W=[f"zt{i}"])
                mk.op("pool", "tensor_tensor", out=xnew[i][:, cb * 512:(cb + 1) * 512], in0=zt[i][:, cb * 512:(cb + 1) * 512],
                      in1=xt[i][:, cb * 512:(cb + 1) * 512], op=ALU.add, R=[f"zt{i}", f"mxt{i}"], W=[f"xnew{i}"])
            mk.dma("pool", XR[cols, :], xnew[i][:], R=[f"xnew{i}"], W=[f"XR{t}"])
            norm(xnew[i][:], f"xnew{i}", j, 3, 2, h2Tt[i][:], f"h2Tt{i}")
            for c in range(8):
                mk.op("pe", "transpose", out=PQ[1][:, c * 128:(c + 1) * 128], in_=h2Tt[i][:, c, :], identity=identb[:],
                      R=[f"h2Tt{i}", "identb"], W=["pq1"])
            mk.ev(h2tok[i][:], PQ[1][:, :], R=["pq1"], W=[f"h2tok{i}"])
            mk.dma("pool", H2U[cols, :], h2tok[i][:], R=[f"h2tok{i}"], W=[f"H2U{t}"])
            ssr = rt["gs"]
            mk.op("act", "activation", out=h2f[:], in_=xnew[i][:], func=AF.Square, accum_out=ssr[:],
                  R=[f"xnew{i}"], W=[f"zz{i}", "rgs"])
            mk.op("act", "activation", out=ssr[:], in_=ssr[:], func=AF.Sqrt, scale=1.0 / D, bias=EPS, R=["rgs"], W=["rgs"])
            mk.op("dve", "reciprocal", out=ssr[:], in_=ssr[:], R=["rgs"], W=["rgs"])
            mk.op("dve", "tensor_scalar", out=h2f[:], in0=xnew[i][:], scalar1=ssr[:, 0:1], scalar2=None,
                  op0=ALU.mult, R=[f"xnew{i}", "rgs", f"zz{i}"], W=[f"zz{i}"])
            for half in range(2):
                for c4 in range(4):
                    c = half * 4 + c4
                    mk.op("pe", "transpose", out=PS[5][:, c4 * 128:(c4 + 1) * 128], in_=h2f[:, c * 128:(c + 1) * 128],
                          identity=identf, R=[f"zz{i}", "cst"], W=["ps5"])
                mk.op("dve", "tensor_tensor", out=h2fT[:, half * 4:(half + 1) * 4, :],
                      in0=PS[5][:, :].rearrange("p (c n) -> p c n", c=4),
                      in1=MODC[:, 3, half * 4:(half + 1) * 4, j:j + 1].to_broadcast([128, 4, 128]), op=ALU.mult,
                      R=["ps5", "MODC"], W=[f"zt{i}"])
                mk.op("pool", "tensor_tensor", out=h2fT[:, half * 4:(half + 1) * 4, :],
                      in0=h2fT[:, half * 4:(half + 1) * 4, :],
                      in1=MODC[:, 2, half * 4:(half + 1) * 4, j:j + 1].to_broadcast([128, 4, 128]), op=ALU.add,
                      R=[f"zt{i}", "MODC"], W=[f"zt{i}"])
            for c in range(8):
                mk.op("pe", "matmul", PS[4][:, 0:20], lhsT=h2fT[:, c, :], rhs=wgr[:, c, :], start=(c == 0), stop=False,
                      R=[f"zt{i}", "wgr"], W=["ps4r"])
            mk.op("pe", "matmul", PS[4][:, 0:20], lhsT=ones[0:1, :], rhs=bgr[0:1, :], start=False, stop=True,
                  R=["cst", "bgr"], W=["ps4r"])
            mk.op("act", "activation", out=LG[:, t, :], in_=PS[4][:, 0:20], func=AF.Copy, R=["ps4r"], W=[f"LG{t}"])
        nt, t0 = len(tiles), tiles[0]
        lk = [f"LG{t}" for t in tiles]
        L3 = LG[:, t0:t0 + nt, :]
        gohv = GOH[:, t0:t0 + nt, :]
        gwv = GW[:, t0:t0 + nt, :]
        r1 = {k: P.sb([128, nt], F32, "r1" + k) for k in ("gm", "gs", "m1", "m2", "w1", "w2")}
        r4 = {k: P.sb([128, nt, 4], F32, "r4" + k) for k in ("ge", "el", "tmp", "oh1", "e2", "oh2")}

        def bc(a):
            return a[:].unsqueeze(2).to_broadcast([128, nt, 4])
        mk.op("dve", "tensor_reduce", out=r1["gm"][:], in_=L3[:, :, 0:4], axis=AX.X, op=ALU.max, R=lk, W=["rgm"])
        mk.op("dve", "tensor_tensor", out=gohv, in0=L3[:, :, 0:4], in1=bc(r1["gm"]), op=ALU.is_ge, R=lk + ["rgm"], W=["GOHall"])
        mk.op("dve", "tensor_tensor", out=r4["ge"][:], in0=L3[:, :, 0:4], in1=bc(r1["gm"]), op=ALU.subtract, R=lk + ["rgm"],
              W=["rge"])
        mk.op("act", "activation", out=r4["ge"][:], in_=r4["ge"][:], func=AF.Exp, R=["rge"], W=["rge"])
        mk.op("dve", "tensor_reduce", out=r1["gs"][:], in_=r4["ge"][:], axis=AX.X, op=ALU.add, R=["rge"], W=["rgs2"])
        mk.op("dve", "reciprocal", out=r1["gs"][:], in_=r1["gs"][:], R=["rgs2"], W=["rgs2"])
        mk.op("dve", "tensor_tensor", out=r4["el"][:], in0=L3[:, :, 4:8], in1=gohv[:, :, 0:1].to_broadcast([128, nt, 4]),
              op=ALU.mult, R=lk + ["GOHall"], W=["rel"])
        for g in range(1, 4):
            mk.op("dve", "tensor_tensor", out=r4["tmp"][:], in0=L3[:, :, 4 + 4 * g:8 + 4 * g],
                  in1=gohv[:, :, g:g + 1].to_broadcast([128, nt, 4]), op=ALU.mult, R=lk + ["GOHall"], W=["rtmp"])
            mk.op("dve", "tensor_tensor", out=r4["el"][:], in0=r4["el"][:], in1=r4["tmp"][:], op=ALU.add, R=["rel", "rtmp"],
                  W=["rel"])
        mk.op("dve", "tensor_reduce", out=r1["m1"][:], in_=r4["el"][:], axis=AX.X, op=ALU.max, R=["rel"], W=["rm1"])
        mk.op("dve", "tensor_tensor", out=r4["oh1"][:], in0=r4["el"][:], in1=bc(r1["m1"]), op=ALU.is_ge, R=["rel", "rm1"],
              W=["roh1"])
        mk.op("dve", "scalar_tensor_tensor", out=r4["e2"][:], in0=r4["oh1"][:], scalar=-1e30, in1=r4["el"][:], op0=ALU.mult,
              op1=ALU.add, R=["roh1", "rel"], W=["re2"])
        mk.op("dve", "tensor_reduce", out=r1["m2"][:], in_=r4["e2"][:], axis=AX.X, op=ALU.max, R=["re2"], W=["rm2"])
        mk.op("dve", "tensor_tensor", out=r4["oh2"][:], in0=r4["e2"][:], in1=bc(r1["m2"]), op=ALU.is_ge, R=["re2", "rm2"],
              W=["roh2"])
        mk.op("dve", "tensor_tensor", out=r1["w1"][:], in0=r1["m2"][:], in1=r1["m1"][:], op=ALU.subtract, R=["rm1", "rm2"],
              W=["rw1"])
        mk.op("act", "activation", out=r1["w1"][:], in_=r1["w1"][:], func=AF.Exp, R=["rw1"], W=["rw1"])
        mk.op("dve", "tensor_scalar_add", out=r1["w1"][:], in0=r1["w1"][:], scalar1=1.0, R=["rw1"], W=["rw1"])
        mk.op("dve", "reciprocal", out=r1["w1"][:], in_=r1["w1"][:], R=["rw1"], W=["rw1"])
        mk.op("dve", "tensor_tensor", out=r1["w1"][:], in0=r1["w1"][:], in1=r1["gs"][:], op=ALU.mult, R=["rw1", "rgs2"],
              W=["rw1"])
        mk.op("dve", "tensor_tensor", out=r1["w2"][:], in0=r1["gs"][:], in1=r1["w1"][:], op=ALU.subtract, R=["rw1", "rgs2"],
              W=["rw2"])
        mk.op("dve", "tensor_tensor", out=gwv, in0=r4["oh1"][:], in1=bc(r1["w1"]), op=ALU.mult, R=["roh1", "rw1"],
              W=["GWall"])
        mk.op("dve", "tensor_tensor", out=r4["tmp"][:], in0=r4["oh2"][:], in1=bc(r1["w2"]), op=ALU.mult, R=["roh2", "rw2"],
              W=["rtmp"])
        mk.op("dve", "tensor_tensor", out=gwv, in0=gwv, in1=r4["tmp"][:], op=ALU.add, R=["GWall", "rtmp"], W=["GWall"])
        mk.barrier()
        P.close()

    def moe_sparse(l, tiles, GOH, GW):
        P = Pool(nc, f"ms{l}")
        I32 = mybir.dt.int32
        nt, t0 = len(tiles), tiles[0]
        N = nt * 128
        NS = (N + 4 * (SLOT - 1)) // SLOT
        TPS = SLOT // 128
        Gv = GOH[:, t0:t0 + nt, :]
        gkeys = ["GOHall"]
        cnt = P.sb([128, nt, 4], F32, "cnt")
        inc = P.sb([128, nt, 4], F32, "inc")
        A = P.sb([128, nt, 4], F32, "A")
        tot = P.sb([128, 4], F32, "tot")
        cmp = P.sb([128, 16], F32, "cmp")
        nsl = P.sb([128, 4], F32, "nsl")
        base = P.sb([128, 4], F32, "base")
        posf = P.sb([128, nt], F32, "posf")
        POSI = P.sb([128, nt], I32, "POSI")
        gkb = P.sb([128, NS], F32, "gkb")
        gtmp = P.sb([128, NS], F32, "gtmp")
        widf = P.sb([128, NS, 4], F32, "widf")
        WIDX = P.sb([128, NS, 4], I32, "WIDX")
        Gf = Gv.rearrange("p t g -> p (t g)")
        mk.op("pe", "matmul", PS[0][:, 0:nt * 4], lhsT=C("ltS"), rhs=Gf, start=True, stop=True, R=gkeys + ["cst"], W=["ps0"])
        mk.op("pe", "matmul", PS[1][:, 0:nt * 4], lhsT=C("ones"), rhs=Gf, start=True, stop=True, R=gkeys + ["cst"], W=["ps1"])
        mk.op("act", "activation", out=cnt[:].rearrange("p t g -> p (t g)"), in_=PS[1][:, 0:nt * 4], func=AF.Copy,
              R=["ps1"], W=["scnt"])
        for g in range(4):
            mk.op("dve", "tensor_tensor_scan", out=inc[:, :, g], data0=C("ones")[:, 0:nt], data1=cnt[:, :, g], initial=0.0,
                  op0=ALU.mult, op1=ALU.add, R=["scnt", "cst"], W=[f"sinc{g}"])
        ik = [f"sinc{g}" for g in range(4)]
        mk.op("dve", "tensor_copy", out=tot[:], in_=inc[:, nt - 1, :], R=ik, W=["stot"])
        for g in range(4):
            mk.op("dve", "tensor_scalar", out=cmp[:], in0=C("thrS"), scalar1=tot[:, g:g + 1], scalar2=None, op0=ALU.is_lt,
                  R=["stot", "cst"], W=["scmp"])
            mk.op("dve", "tensor_reduce", out=nsl[:, g:g + 1], in_=cmp[:], axis=AX.X, op=ALU.add, R=["scmp"], W=["snsl"])
        mk.op("dve", "tensor_scalar", out=nsl[:], in0=nsl[:], scalar1=float(SLOT), scalar2=None, op0=ALU.mult,
              R=["snsl"], W=["snsl"])
        mk.op("pool", "memset", base[:], 0.0, W=["sbase"])
        for g in range(1, 4):
            mk.op("dve", "tensor_tensor", out=base[:, g:g + 1], in0=base[:, g - 1:g], in1=nsl[:, g - 1:g], op=ALU.add,
                  R=["sbase", "snsl"], W=["sbase"])
        mk.op("dve", "tensor_tensor", out=A[:], in0=inc[:], in1=cnt[:], op=ALU.subtract, R=ik + ["scnt"], W=["sA"])
        mk.op("dve", "tensor_tensor", out=A[:].rearrange("p t g -> p (t g)"), in0=A[:].rearrange("p t g -> p (t g)"),
              in1=PS[0][:, 0:nt * 4], op=ALU.add, R=["sA", "ps0"], W=["sA"])
        mk.op("dve", "tensor_tensor", out=A[:], in0=A[:], in1=base[:].unsqueeze(1).to_broadcast([128, nt, 4]), op=ALU.add,
              R=["sA", "sbase"], W=["sA"])
        mk.op("dve", "tensor_tensor", out=A[:], in0=A[:], in1=Gv, op=ALU.mult, R=["sA"] + gkeys, W=["sA"])
        mk.op("dve", "tensor_reduce", out=posf[:], in_=A[:], axis=AX.X, op=ALU.add, R=["sA"], W=["sposf"])
        mk.op("dve", "tensor_copy", out=POSI[:], in_=posf[:], R=["sposf"], W=["POSI"])
        mk.op("pool", "memset", gkb[:], 0.0, W=["sgkb"])
        for g in range(1, 4):
            mk.op("dve", "tensor_scalar", out=gtmp[:], in0=C("kS")[:, 0:NS], scalar1=base[:, g:g + 1], scalar2=None,
                  op0=ALU.is_ge, R=["sbase", "cst"], W=["sgtmp"])
            mk.op("dve", "tensor_tensor", out=gkb[:], in0=gkb[:], in1=gtmp[:], op=ALU.add, R=["sgkb", "sgtmp"], W=["sgkb"])
        mk.op("dve", "tensor_scalar", out=gkb[:], in0=gkb[:], scalar1=512.0, scalar2=None, op0=ALU.mult, R=["sgkb"],
              W=["sgkb"])
        for j in range(4):
            mk.op("dve", "tensor_scalar", out=widf[:, :, j], in0=gkb[:], scalar1=C("jp")[:, j:j + 1], scalar2=None,
                  op0=ALU.add, R=["sgkb", "cst"], W=["swidf"])
        mk.op("dve", "tensor_copy", out=WIDX[:], in_=widf[:], R=["swidf"], W=["WIDX"])
        hb = [P.sb([128, D], BF16, "hb") for _ in range(2)]
        for ti, t in enumerate(tiles):
            i = ti % 2
            mk.dma("sp", hb[i][:], H2U[t * 128:(t + 1) * 128, :], R=[f"H2U{t}"], W=[f"hb{i}"])
            mk.idma(HS[:, :], bass.IndirectOffsetOnAxis(ap=POSI[:, ti:ti + 1], axis=0), hb[i][:, :], None,
                    R=[f"hb{i}", "POSI"], W=[f"HSs{ti}"])
            mk.idma(GWS[:, :], bass.IndirectOffsetOnAxis(ap=POSI[:, ti:ti + 1], axis=0), GW[:, t, :], None,
                    R=["GWall", "POSI"], W=[f"GWs{ti}"])
        hsk = [f"HSs{ti}" for ti in range(nt)]
        gwk = [f"GWs{ti}" for ti in range(nt)]
        hs = [P.sb([128, TPS, D], BF16, "hs") for _ in range(2)]
        hTs = [P.sb([128, 8, SLOT], BF16, "hTs") for _ in range(2)]
        gws = [P.sb([128, TPS, 4], F32, "gws") for _ in range(2)]
        acc = [P.sb([128, TPS, D], F32, "acc") for _ in range(2)]
        wall = [P.sb([128, 3 * 4096], BF16, "wall") for _ in range(2)]
        w1b = [w[:, 0:4096].rearrange("p (c f) -> p c f", c=8) for w in wall]
        w3b = [w[:, 4096:8192].rearrange("p (c f) -> p c f", c=8) for w in wall]
        w2b = [w[:, 8192:12288].rearrange("p (c f) -> p c f", c=4) for w in wall]
        sl = [P.sb([128, SLOT], F32, "sl") for _ in range(2)]
        actT = [P.sb([128, 4, SLOT], BF16, "actT") for _ in range(2)]
        Wt = WALL.rearrange("e p f -> (e p) f")
        wkeys = [f"W{a}B{e}" for a in (1, 2, 3) for e in range(16)]
        nw = 0
        for k in range(NS):
            si = k % 2
            mk.dma("sp", hs[si][:], HS[k * SLOT:(k + 1) * SLOT, :].rearrange("(q p) f -> p q f", p=128), R=hsk,
                   W=[f"hs{si}"])
            mk.dma("sp", gws[si][:], GWS[k * SLOT:(k + 1) * SLOT, :].rearrange("(q p) f -> p q f", p=128), R=gwk,
                   W=[f"gws{si}"])
            for q in range(TPS):
                pq = q % 2
                for c in range(8):
                    mk.op("pe", "transpose", out=PQ[pq][:, c * 128:(c + 1) * 128], in_=hs[si][:, q, c * 128:(c + 1) * 128],
                          identity=identb[:], R=[f"hs{si}", "identb"], W=[f"pq{pq}"])
                mk.ev(hTs[si][:, :, q * 128:(q + 1) * 128], PQ[pq][:, :].rearrange("p (c n) -> p c n", c=8), R=[f"pq{pq}"],
                      W=[f"hTs{si}"])
            for j in range(4):
                wi = nw % 2
                nw += 1
                ioff = bass.IndirectOffsetOnAxis(ap=WIDX[:, k, j:j + 1], axis=0)
                mk.idma(wall[wi][:, :], None, Wt, ioff, R=["WIDX"] + wkeys, W=[f"w1b{wi}", f"w3b{wi}", f"w2b{wi}"])
                for fcn in range(4):
                    for kk_ in range(8):
                        mk.op("pe", "matmul", PS[0][:, 0:SLOT], lhsT=w1b[wi][:, kk_, fcn * 128:(fcn + 1) * 128],
                              rhs=hTs[si][:, kk_, :], start=(kk_ == 0), stop=(kk_ == 7), R=[f"w1b{wi}", f"hTs{si}"], W=["ps0"])
                    for kk_ in range(8):
                        mk.op("pe", "matmul", PS[1][:, 0:SLOT], lhsT=w3b[wi][:, kk_, fcn * 128:(fcn + 1) * 128],
                              rhs=hTs[si][:, kk_, :], start=(kk_ == 0), stop=(kk_ == 7), R=[f"w3b{wi}", f"hTs{si}"], W=["ps1"])
                    s2 = fcn % 2
                    mk.op("act", "activation", out=sl[s2][:], in_=PS[0][:, 0:SLOT], func=AF.Silu, R=["ps0"], W=[f"sl{s2}"])
                    mk.op("dve", "tensor_tensor", out=actT[wi][:, fcn, :], in0=sl[s2][:], in1=PS[1][:, 0:SLOT], op=ALU.mult,
                          R=[f"sl{s2}", "ps1"], W=[f"actT{wi}"])
                for q in range(TPS):
                    for cb in range(2):
                        pb = 2 + cb
                        for fcn in range(4):
                            mk.op("pe", "matmul", PS[pb][:, :], lhsT=actT[wi][:, fcn, q * 128:(q + 1) * 128],
                                  rhs=w2b[wi][:, fcn, cb * 512:(cb + 1) * 512], start=(fcn == 0), stop=(fcn == 3),
                                  R=[f"actT{wi}", f"w2b{wi}"], W=[f"ps{pb}"])
                        if j == 0:
                            mk.op("dve", "tensor_scalar", out=acc[si][:, q, cb * 512:(cb + 1) * 512], in0=PS[pb][:, :],
                                  scalar1=gws[si][:, q, j:j + 1], scalar2=None, op0=ALU.mult,
                                  R=[f"ps{pb}", f"gws{si}"], W=[f"sacc{si}"])
                        else:
                            mk.op("dve", "scalar_tensor_tensor", out=acc[si][:, q, cb * 512:(cb + 1) * 512], in0=PS[pb][:, :],
                                  scalar=gws[si][:, q, j:j + 1], in1=acc[si][:, q, cb * 512:(cb + 1) * 512], op0=ALU.mult,
                                  op1=ALU.add, R=[f"ps{pb}", f"gws{si}", f"sacc{si}"], W=[f"sacc{si}"])
            mk.dma("sp", OS[k * SLOT:(k + 1) * SLOT, :].rearrange("(q p) f -> p q f", p=128), acc[si][:], R=[f"sacc{si}"],
                   W=[f"OS{k}"])
        osk = [f"OS{k}" for k in range(NS)]
        og = [P.sb([128, D], F32, "og") for _ in range(2)]
        xt = [P.sb([128, D], F32, "xt") for _ in range(2)]
        for ti, t in enumerate(tiles):
            i = ti % 2
            j = cond_of(t)
            mk.idma(og[i][:, :], None, OS[:, :], bass.IndirectOffsetOnAxis(ap=POSI[:, ti:ti + 1], axis=0), R=osk + ["POSI"],
                    W=[f"og{i}"])
            mk.dma("sp", xt[i][:], XR[t * 128:(t + 1) * 128, :], R=[f"XR{t}"], W=[f"ext{i}"])
            mk.op("pool", "tensor_tensor", out=og[i][:], in0=og[i][:], in1=GB[:, j, 1, :], op=ALU.mult, R=[f"og{i}", "GB"],
                  W=[f"og{i}"])
            mk.op("dve", "tensor_tensor", out=og[i][:], in0=og[i][:], in1=xt[i][:], op=ALU.add, R=[f"og{i}", f"ext{i}"],
                  W=[f"og{i}"])
            mk.dma("sp", XR[t * 128:(t + 1) * 128, :], og[i][:], R=[f"og{i}"], W=[f"XR{t}"])
        mk.barrier()
        P.close()

    def final_phase():
        P = Pool(nc, "fin")
        fw = P.sb([128, D], F32, "fw")
        mk.dma("sp", fw[:], fnw, W=["fw"])
        xt = [P.sb([128, D], F32, "xt") for _ in range(2)]
        ot = [P.sb([128, D], F32, "ot") for _ in range(2)]
        junk = P.sb([128, D], BF16, "junk")
        ss = [P.sb([128, 1], F32, "ss") for _ in range(2)]
        for t in OUT_T:
            i = t % 2
            mk.dma("sp", xt[i][:], XR[t * 128:(t + 1) * 128, :], R=[f"XR{t}"], W=[f"fxt{i}"])
            mk.op("act", "activation", out=junk[:], in_=xt[i][:], func=AF.Square, accum_out=ss[i][:], R=[f"fxt{i}"],
                  W=["fjunk", f"fss{i}"])
            mk.op("act", "activation", out=ss[i][:], in_=ss[i][:], func=AF.Sqrt, scale=1.0 / D, bias=EPS, R=[f"fss{i}"],
                  W=[f"fss{i}"])
            mk.op("dve", "reciprocal", out=ss[i][:], in_=ss[i][:], R=[f"fss{i}"], W=[f"fss{i}"])
            mk.op("dve", "scalar_tensor_tensor", out=ot[i][:], in0=xt[i][:], scalar=ss[i][:, 0:1], in1=fw[:], op0=ALU.mult,
                  op1=ALU.mult, R=[f"fxt{i}", f"fss{i}", "fw"], W=[f"fot{i}"])
            mk.dma("pool", yout[(t - 2) * 128:(t - 1) * 128, :], ot[i][:], R=[f"fot{i}"], W=["yout"])
        mk.barrier()
        P.close()

    stages = dict(mod=mod_phase, inproj=inproj_phase, a=mixer_a, b=mixer_b, c=mixer_c, d=mixer_d)
    return dict(nc=nc, mk=mk, stages=stages, merge=merge_moe_phase, final=final_phase, dbg=dbg,
                scr=dict(XR=XR, FM=FM, TM=TM, MG=MG, YT=YT))


def emit_all(prog, layers=NL, upto=None, skip=()):
    mk = prog["mk"]
    mk.barrier()
    done = False
    for l in range(layers):
        for s in ("mod", "inproj", "a", "b", "c", "d"):
            if s in skip:
                continue
            if s in ("inproj", "b", "c", "d"):
                prog["stages"][s](l, l == NL - 1)
            else:
                prog["stages"][s](l)
            if upto == (l, s):
                done = True
                break
        if done:
            break
        prog["merge"](l, l == NL - 1)
        if upto == (l, "merge"):
            done = True
            break
    if not done:
        prog["final"]()
    mk.barrier(engines=("sp",))


def _consts():
    j = np.arange(128)[:, None]
    i = np.arange(128)[None, :]
    same = (j // DL) == (i // DL)
    m = {}
    m["ident"] = (j == i)
    m["triF"] = (j <= i)
    m["triB"] = (j >= i)
    m["blkF"] = same & (j <= i)
    m["blkB"] = same & (j >= i)
    m["aftF"] = same & (j > i)
    m["befB"] = same & (j < i)
    m["diffF"] = np.maximum(i - j, 0)
    m["diffB"] = np.maximum(j - i, 0)
    m["maskF"] = (i >= j)
    m["maskB"] = (j > i)
    m["posF"] = np.broadcast_to(i + 1, (128, 128))
    m["posB"] = np.broadcast_to(128 - i, (128, 128))
    m["negF4"] = np.tile(np.where(j <= i, 0.0, -30000.0), (1, 4))
    m["negB4"] = np.tile(np.where(j >= i, 0.0, -30000.0), (1, 4))
    m["mblkF4"] = np.tile(same & (j <= i), (1, 4))
    m["mblkB4"] = np.tile(same & (j >= i), (1, 4))
    m["kpos"] = np.concatenate([127 - j, j], 1)
    m["ones"] = np.ones((128, 128))
    sel = np.zeros((128, 256))
    sel[0, 0:128] = 1.0
    sel[1, 128:256] = 1.0
    m["sel"] = sel
    m["subm"] = np.concatenate([(j // DL) == s_ for s_ in range(128 // DL)], 1)
    qm = np.zeros((128, 2, 5, 128), np.float32)
    for hh_ in range(2):
        for s_ in range(5):
            colsel = np.ones(128, bool) if s_ == 4 else (np.arange(128) // DL == s_)
            qm[hh_ * 64:(hh_ + 1) * 64, hh_, s_, :] = colsel[None, :]
    m["qmask"] = qm.reshape(128, 1280)
    m["ltS"] = (j < i)
    m["thrS"] = np.broadcast_to(np.arange(16)[None, :] * SLOT, (128, 16))
    m["kS"] = np.broadcast_to(np.arange(16)[None, :] * SLOT, (128, 16))
    m["jp"] = np.arange(4)[None, :] * 128 + np.arange(128)[:, None]
    out = np.zeros((128, NCST), np.float32)
    for k, (o, w) in CST.items():
        out[:, o:o + w] = np.asarray(m[k], np.float32)
    return out


def _rope_tables(flip=False):
    n = 16
    inv = np.power(np.float32(10000.0), -np.arange(n, dtype=np.float32) / n).astype(np.float32)
    t = np.arange(4096)
    row = (t // 64).astype(np.float32)
    col = (t % 64).astype(np.float32)
    ang = np.concatenate([row[:, None] * inv, col[:, None] * inv], -1)
    cos = np.cos(ang).astype(np.float32).T
    sin = np.sin(ang).astype(np.float32).T
    if flip:
        cos, sin = cos[:, ::-1], sin[:, ::-1]
    Cc = np.ones((128, T), np.float32)
    Ss = np.zeros((128, T), np.float32)
    for hh in range(2):
        Cc[hh * 64:hh * 64 + 32, 256:] = cos
        Cc[hh * 64 + 32:hh * 64 + 64, 256:] = cos
        Ss[hh * 64:hh * 64 + 32, 256:] = -sin
        Ss[hh * 64 + 32:hh * 64 + 64, 256:] = sin
    return Cc, Ss


def prep_shared(inp, flip=False):
    f = np.float32
    w_in = np.asarray(inp["w_in"], f)
    offs = {}
    o = 0
    for name, w in (("a_x", 256), ("a_g", 256), ("b_q", 256), ("b_k", 256), ("b_v", 256), ("b_g", 256), ("c_q", 256),
                    ("c_k", 256), ("c_v", 256), ("c_o", 256), ("c_gates", 16), ("d_q", 256), ("d_ff", 256),
                    ("d_fb", 256), ("d_i", 256), ("d_g", 256), ("merge", 4096)):
        offs[name] = (o, w)
        o += w

    def cols(n):
        a, w = offs[n]
        return w_in[:, :, a:a + w]

    perm = np.concatenate([np.arange(h * 64 + 32, h * 64 + 64).tolist() + np.arange(h * 64, h * 64 + 32).tolist()
                           for h in range(4)]).astype(np.int64)
    w_fm = np.concatenate([cols("a_x"), cols("a_g"), cols("b_q"), cols("b_q")[:, :, perm], cols("b_k"),
                           cols("b_k")[:, :, perm], cols("c_q"), cols("c_k")], -1)
    gperm = np.array([8, 9, 10, 11, 12, 13, 14, 15, 0, 1, 2, 3, 4, 5, 6, 7]) if flip else np.arange(16)
    dfa, dfb = ("d_fb", "d_ff") if flip else ("d_ff", "d_fb")
    w_tm = np.concatenate([cols("b_v"), cols("b_g"), cols("c_v"), cols("c_o"), cols("d_q"), cols(dfa), cols(dfb),
                           cols("d_i"), cols("d_g"), cols("c_gates")[:, :, gperm], cols("merge")], -1)
    sh = {}
    sh["w_mod"] = np.ascontiguousarray(inp["w_mod"], f)
    bm = np.asarray(inp["b_mod"], f)
    sh["bmod_c"] = np.ascontiguousarray(bm.reshape(NL, 48, 128).transpose(0, 2, 1))
    sh["bmod_r"] = np.ascontiguousarray(bm.reshape(NL, 1, 6144))
    sh["w_fm"] = np.ascontiguousarray(w_fm)
    sh["w_tm"] = np.ascontiguousarray(w_tm)
    acw = np.asarray(inp["a_conv_w"], f)
    zt_ = np.zeros_like(acw[:, :1])
    acw = np.concatenate([zt_, acw[:, ::-1]], 1) if flip else np.concatenate([acw, zt_], 1)
    sh["a_cw"] = np.ascontiguousarray(acw.reshape(NL, 5, 2, 128).transpose(0, 3, 2, 1))
    sh["a_cb"] = np.ascontiguousarray(np.asarray(inp["a_conv_b"], f).reshape(NL, 2, 128).transpose(0, 2, 1))
    gw = np.asarray(inp["a_gate_w"], f)
    agw = np.zeros((NL, 128, 2, 2, 2, 128), f)
    for c in range(2):
        for hh in range(2):
            agw[:, hh * 64:(hh + 1) * 64, :, :, c, hh * 64:(hh + 1) * 64] = gw[:, :, :, 2 * c + hh].transpose(0, 3, 1, 2, 4)
    sh["a_gw"] = np.ascontiguousarray(agw[:, :, ::-1]) if flip else agw
    gb = np.asarray(inp["a_gate_b"], f)
    if flip:
        gb = gb[:, ::-1]
    sh["a_gb"] = np.ascontiguousarray(gb.reshape(NL, 2, 2, 2, 128).transpose(0, 4, 1, 2, 3))
    lam = np.asarray(inp["a_lambda"], f)
    if flip:
        lam = lam[:, ::-1]
    sh["a_lam"] = np.ascontiguousarray(lam.reshape(NL, 2, 2, 128).transpose(0, 3, 1, 2))
    th = np.asarray(inp["b_theta"], f)
    if flip:
        th = th[:, ::-1]
    thp = np.zeros((NL, 128, 2, 2), f)
    for c in range(2):
        for hh in range(2):
            thp[:, hh * 64:(hh + 1) * 64, :, c] = th[:, None, :, 2 * c + hh]
    sh["b_thp"] = thp
    sh["b_thh"] = np.ascontiguousarray(np.broadcast_to(th[:, None], (NL, 128, 2, 4)))
    ccw = np.asarray(inp["c_conv_w"], f)
    zt_ = np.zeros_like(ccw[:, :1])
    ccw = np.concatenate([zt_, ccw[:, ::-1]], 1) if flip else np.concatenate([ccw, zt_], 1)
    sh["c_cw"] = np.ascontiguousarray(ccw.reshape(NL, 5, 4, 128).transpose(0, 3, 2, 1))
    sh["c_cb"] = np.ascontiguousarray(np.asarray(inp["c_conv_b"], f).reshape(NL, 4, 128).transpose(0, 2, 1))
    sh["c_gb"] = np.ascontiguousarray(np.broadcast_to(np.asarray(inp["c_gate_b"], f).reshape(NL, 1, 16)[:, :, gperm], (NL, 128, 16)))
    sh["d_lbr"] = np.ascontiguousarray(np.broadcast_to(np.asarray(inp["d_lb"], f)[None], (128, 2, 256)))
    sh["w_branch"] = np.ascontiguousarray(inp["w_branch"], f)
    sh["w_out"] = np.ascontiguousarray(inp["w_out"], f)
    sh["moe_wgr"] = np.ascontiguousarray(np.concatenate([np.asarray(inp["moe_w_group"], f), np.asarray(inp["moe_w_router"], f)], -1))
    sh["moe_bgr"] = np.ascontiguousarray(np.concatenate([np.asarray(inp["moe_b_group"], f), np.asarray(inp["moe_b_router"], f)], -1).reshape(NL, 1, 20))
    sh["moe_w1"] = np.ascontiguousarray(inp["moe_w1"], f)
    sh["moe_w3"] = np.ascontiguousarray(inp["moe_w3"], f)
    sh["moe_w2"] = np.ascontiguousarray(inp["moe_w2"], f)
    sh["fnw"] = np.ascontiguousarray(np.broadcast_to(np.asarray(inp["final_norm_w"], f)[None], (128, D)))
    sh["cst"] = _consts()
    sh["ropeC"], sh["ropeS"] = _rope_tables(flip)
    return sh


def prep_core(inp, b, flip=False):
    f = np.float32
    d = {}
    cx, xx = np.asarray(inp["ctx"][b], f), np.asarray(inp["x"][b], f)
    if flip:
        cx, xx = cx[::-1], xx[::-1]
    d["xin"] = np.ascontiguousarray(np.concatenate([cx, xx], 0))
    cv = np.stack([np.asarray(inp["c_ctx"], f), np.asarray(inp["c"][b], f)], -1)
    d["cvec"] = np.ascontiguousarray(cv.reshape(8, 128, 2).transpose(1, 0, 2))
    return d


_PROG = None


def kernel(**inputs):
    global _PROG
    if _PROG is None:
        _PROG = build_program()
        emit_all(_PROG)
    nc = _PROG["nc"]
    shs = [prep_shared(inputs, False), prep_shared(inputs, True)]
    in_maps = []
    for core in range(8):
        fl = core >= 4
        m = dict(shs[1 if fl else 0])
        m.update(prep_core(inputs, core % 4, fl))
        in_maps.append(m)
    res = run_bass_kernel_spmd(nc, in_maps, core_ids=list(range(8)))
    out = np.empty((4, 4096, D), np.float32)
    for b in range(4):
        out[b, :HALF_OUT] = np.asarray(res.results[b]["yout"], np.float32)[:HALF_OUT]
        out[b, HALF_OUT:] = np.asarray(res.results[b + 4]["yout"], np.float32)[:4096 - HALF_OUT][::-1]
    return out
```

```python
import contextlib
import numpy as np
import ml_dtypes
import concourse.bass as bass
import concourse.mybir as mybir
from concourse.bass_utils import run_bass_kernel_spmd

F32 = mybir.dt.float32
BF16 = mybir.dt.bfloat16
AF = mybir.ActivationFunctionType
ALU = mybir.AluOpType
AX = mybir.AxisListType

T = 4352
NT = 34
D = 1024
EPS = 1e-6
NL = 2
SLOT = 512
HALF_OUT = 2048
DL = 32
DNS = 128 // DL
DBG = dict(maxit=None, core=True, fin=True, heads=(0, 1, 2, 3))
TMW = 2320
TM_OFF = dict(b_v=0, b_g=256, c_v=512, c_o=768, d_q=1024, d_ff=1280, d_fb=1536, d_i=1792, d_g=2048, c_gates=2304)
FM_OFF = dict(a_x=0, a_g=2, b_q=4, b_qp=6, b_k=8, b_kp=10, c_q=12, c_k=14)

CST = {}
_off = 0
for _n, _w in (("ident", 128), ("triF", 128), ("triB", 128), ("blkF", 128), ("blkB", 128), ("aftF", 128),
               ("befB", 128), ("diffF", 128), ("diffB", 128), ("maskF", 128), ("maskB", 128), ("posF", 128),
               ("posB", 128), ("negF4", 512), ("negB4", 512), ("mblkF4", 512), ("mblkB4", 512), ("kpos", 2),
               ("ones", 128), ("sel", 256), ("subm", 4), ("qmask", 1280), ("ltS", 128), ("thrS", 16), ("kS", 16), ("jp", 4)):
    CST[_n] = (_off, _w)
    _off += _w
NCST = _off


class MK:
    SEM_ROT = 30000

    def __init__(self, nc, ndma=8):
        self.nc = nc
        self.engs = {"pe": nc.tensor, "act": nc.scalar, "dve": nc.vector, "pool": nc.gpsimd, "sp": nc.sync}
        self._ctxs = []
        self.nsem = 0
        self.sem = {}
        self.cnt = {}
        for e in ("pe", "act", "dve", "pool"):
            self.sem[e] = self._newsem("s_" + e)
            self.cnt[e] = 0
        self.seen = {e: {} for e in self.engs}
        self.dq = {}
        for q in ("sp", "pool"):
            self.dq[q] = {"i": 0, "slots": [[self._newsem(f"d_{q}{i}"), 0] for i in range(ndma)]}
        self.res = {}
        self.ninst = 0
        self.flip = 0

    def _newsem(self, name):
        self.nsem += 1
        cm = self.nc.semaphore(f"{name}_{self.nsem}")
        s = cm.__enter__()
        self._ctxs.append(cm)
        return s

    def _wait(self, eng, tok):
        sem, val = tok
        key = id(sem)
        if self.seen[eng].get(key, 0) >= val:
            return
        self.engs[eng].wait_ge(sem, val)
        self.seen[eng][key] = val

    def _deps(self, R, W):
        deps = []
        for k in R:
            st = self.res.get(k)
            if st and st[0] is not None:
                deps.append(st[0])
        for k in W:
            st = self.res.get(k)
            if st:
                if st[0] is not None:
                    deps.append(st[0])
                deps.extend(st[1])
        return deps

    def _record(self, tok, R, W):
        for k in R:
            st = self.res.setdefault(k, [None, []])
            st[1] = [t for t in st[1] if t[0] is not tok[0]] + [tok]
        for k in W:
            self.res[k] = [tok, []]

    def op(self, eng, method, *args, R=(), W=(), **kw):
        for tok in self._deps(R, W):
            if eng == "pe" and tok[0] is self.sem["pe"]:
                continue
            self._wait(eng, tok)
        ins = getattr(self.engs[eng], method)(*args, **kw)
        if self.cnt[eng] >= self.SEM_ROT:
            self.sem[eng] = self._newsem("s_" + eng)
            self.cnt[eng] = 0
        self.cnt[eng] += 1
        ins.then_inc(self.sem[eng], 1)
        tok = (self.sem[eng], self.cnt[eng])
        self._record(tok, R, W)
        self.ninst += 1
        return tok

    def dma(self, q, out, in_, R=(), W=(), **kw):
        d = self.dq[q]
        slot = d["slots"][d["i"] % len(d["slots"])]
        d["i"] += 1
        if slot[1] > 0:
            self._wait(q, (slot[0], slot[1]))
        if slot[1] >= self.SEM_ROT:
            slot[0] = self._newsem("d_" + q)
            slot[1] = 0
        for tok in self._deps(R, W):
            self._wait(q, tok)
        ins = self.engs[q].dma_start(out=out, in_=in_, **kw)
        slot[1] += 16
        ins.then_inc(slot[0], 16)
        tok = (slot[0], slot[1])
        self._record(tok, R, W)
        self.ninst += 1
        return tok

    def idma(self, out, out_offset, in_, in_offset, R=(), W=()):
        q = "pool"
        d = self.dq[q]
        slot = d["slots"][d["i"] % len(d["slots"])]
        d["i"] += 1
        if slot[1] > 0:
            self._wait(q, (slot[0], slot[1]))
        if slot[1] >= self.SEM_ROT:
            slot[0] = self._newsem("d_" + q)
            slot[1] = 0
        for tok in self._deps(R, W):
            self._wait(q, tok)
        ins = self.nc.gpsimd.indirect_dma_start(out=out, out_offset=out_offset, in_=in_, in_offset=in_offset)
        slot[1] += 16
        ins.then_inc(slot[0], 16)
        tok = (slot[0], slot[1])
        self._record(tok, R, W)
        self.ninst += 1
        return tok

    def barrier(self, engines=("pe", "act", "dve", "pool", "sp")):
        toks = []
        for q, d in self.dq.items():
            for slot in d["slots"]:
                if slot[1] > 0:
                    toks.append((slot[0], slot[1]))
        for e in ("pe", "act", "dve", "pool"):
            if self.cnt[e] > 0:
                toks.append((self.sem[e], self.cnt[e]))
        for e in engines:
            for tok in toks:
                if e in self.sem and tok[0] is self.sem[e]:
                    continue
                self._wait(e, tok)

    def ev(self, out, in_, R=(), W=(), func=None, **kw):
        if func is not None:
            return self.op("act", "activation", out=out, in_=in_, func=func, R=R, W=W, **kw)
        self.flip ^= 1
        if self.flip:
            return self.op("act", "activation", out=out, in_=in_, func=AF.Copy, R=R, W=W)
        return self.op("dve", "tensor_copy", out=out, in_=in_, R=R, W=W)


class Pool:
    def __init__(self, nc, tag):
        self.nc = nc
        self.tag = tag
        self.stack = contextlib.ExitStack()
        self.n = 0

    def sb(self, shape, dt=F32, name=None):
        self.n += 1
        return self.stack.enter_context(self.nc.sbuf_tensor(f"{self.tag}_{name or 't'}{self.n}", list(shape), dt))

    def close(self):
        self.stack.close()


def build_program(debug=()):
    nc = bass.Bass("TRN2", target_bir_lowering=False)

    def din(name, shape, dt=F32):
        return nc.dram_tensor(name, list(shape), dt, kind="ExternalInput").ap()

    def dscr(name, shape, dt=F32):
        return nc.dram_tensor(name, list(shape), dt, kind="Internal").ap()

    xin = din("xin", [T, D])
    cvec = din("cvec", [128, 8, 2])
    w_mod = din("w_mod", [NL, D, 6144])
    bmod_c = din("bmod_c", [NL, 128, 48])
    bmod_r = din("bmod_r", [NL, 1, 6144])
    w_fm = din("w_fm", [NL, D, 2048])
    w_tm = din("w_tm", [NL, D, TMW + 4096])
    a_cw = din("a_cw", [NL, 128, 2, 5])
    a_cb = din("a_cb", [NL, 128, 2])
    a_gw = din("a_gw", [NL, 128, 2, 2, 2, 128])
    a_gb = din("a_gb", [NL, 128, 2, 2, 2])
    a_lam = din("a_lam", [NL, 128, 2, 2])
    b_thp = din("b_thp", [NL, 128, 2, 2])
    b_thh = din("b_thh", [NL, 128, 2, 4])
    c_cw = din("c_cw", [NL, 128, 4, 5])
    c_cb = din("c_cb", [NL, 128, 4])
    c_gb = din("c_gb", [NL, 128, 16])
    d_lbr = din("d_lbr", [128, 2, 256])
    w_branch = din("w_branch", [NL, 4, 256, D])
    w_out = din("w_out", [NL, D, D])
    moe_wgr = din("moe_wgr", [NL, D, 20])
    moe_bgr = din("moe_bgr", [NL, 1, 20])
    moe_w1 = din("moe_w1", [NL, 16, D, 512])
    moe_w3 = din("moe_w3", [NL, 16, D, 512])
    moe_w2 = din("moe_w2", [NL, 16, 512, D])
    fnw = din("fnw", [128, D])
    cst_d = din("cst", [128, NCST])
    ropeC_d = din("ropeC", [128, T])
    ropeS_d = din("ropeS", [128, T])
    yout = nc.dram_tensor("yout", [HALF_OUT, D], F32, kind="ExternalOutput").ap()

    XR = dscr("XR", [T, D])
    FM = dscr("FM", [16, 128, T])
    TM = dscr("TM", [T, TMW])
    MG = dscr("MG", [T, 4096], BF16)
    YT = dscr("YT", [4, 2, 128, T], BF16)
    dbg = {}
    for name, shape, dt in debug:
        dbg[name] = nc.dram_tensor("dbg_" + name, list(shape), dt, kind="ExternalOutput").ap()

    mk = MK(nc)
    G = Pool(nc, "g")

    PS = [nc.psum_tensor(f"ps{i}", [128, 512], F32).__enter__() for i in range(6)]
    PQ = [nc.psum_tensor(f"pq{i}", [128, 1024], BF16).__enter__() for i in range(2)]

    cst = G.sb([128, NCST], F32, "cst")
    identb = G.sb([128, 128], BF16, "identb")
    sT = G.sb([128, 8, 2], F32, "sT")
    MODC = G.sb([128, 4, 8, 2], F32, "MODC")
    GB = G.sb([128, 2, 2, D], F32, "GB")
    mk.dma("sp", cst[:], cst_d, W=["cst"])
    mk.dma("sp", sT[:], cvec, W=["sT"])

    def C(name, rows=slice(0, 128)):
        o, w = CST[name]
        return cst[rows, o:o + w]

    mk.op("dve", "tensor_copy", out=identb[:], in_=C("ident"), R=["cst"], W=["identb"])
    mk.op("act", "activation", out=sT[:], in_=sT[:], func=AF.Silu, R=["sT"], W=["sT"])
    for t in range(NT):
        mk.dma("sp", XR[t * 128:(t + 1) * 128, :], xin[t * 128:(t + 1) * 128, :], W=[f"XR{t}"])

    def cond_of(t):
        return 0 if t < 2 else 1

    def mod_phase(l):
        P = Pool(nc, f"mod{l}")
        wblk = P.sb([128, 8, 1024], F32, "wblk")
        bmc = P.sb([128, 48], F32, "bmc")
        bmr = P.sb([1, 6144], F32, "bmr")
        GR = P.sb([2, 2, D], F32, "GR")
        mk.dma("sp", bmc[:], bmod_c[l], W=["bmc"])
        mk.dma("sp", bmr[:], bmod_r[l], W=["bmr"])
        sel = C("sel", slice(0, 2))
        for m in range(6):
            mk.dma("sp", wblk[:], w_mod[l].rearrange("(c p) f -> p c f", p=128)[:, :, m * 1024:(m + 1) * 1024],
                   W=["wblk"])
            if m in (0, 1, 3, 4):
                m4 = {0: 0, 1: 1, 3: 2, 4: 3}[m]
                for c in range(8):
                    for k in range(8):
                        mk.op("pe", "matmul", PS[0][:, c * 2:(c + 1) * 2], lhsT=wblk[:, k, c * 128:(c + 1) * 128],
                              rhs=sT[:, k, :], start=(k == 0), stop=(k == 7), R=["wblk", "sT"], W=["ps0"])
                mk.op("dve", "tensor_tensor", out=MODC[:, m4, :, :],
                      in0=PS[0][:, 0:16].rearrange("p (c j) -> p c j", j=2),
                      in1=bmc[:, m * 8:(m + 1) * 8].unsqueeze(2).to_broadcast([128, 8, 2]), op=ALU.add,
                      R=["ps0", "bmc"], W=["MODC"])
                if m in (1, 4):
                    mk.op("dve", "tensor_scalar_add", out=MODC[:, m4, :, :], in0=MODC[:, m4, :, :], scalar1=1.0,
                          R=["MODC"], W=["MODC"])
            else:
                mi = 0 if m == 2 else 1
                for cb in range(2):
                    for k in range(8):
                        mk.op("pe", "matmul", PS[1][0:2, :], lhsT=sT[:, k, :], rhs=wblk[:, k, cb * 512:(cb + 1) * 512],
                              start=(k == 0), stop=False, R=["wblk", "sT"], W=["ps1"])
                    mk.op("pe", "matmul", PS[1][0:2, :], lhsT=sel[0:1, 0:2],
                          rhs=bmr[0:1, m * 1024 + cb * 512: m * 1024 + (cb + 1) * 512], start=False, stop=True,
                          R=["bmr", "cst"], W=["ps1"])
                    mk.op("dve", "tensor_copy", out=GR[0:2, mi, cb * 512:(cb + 1) * 512], in_=PS[1][0:2, :],
                          R=["ps1"], W=["GR"])
        for j in range(2):
            for mi in range(2):
                for cb in range(2):
                    mk.op("pe", "matmul", PS[1][:, :], lhsT=sel[0:2, j * 128:(j + 1) * 128],
                          rhs=GR[0:2, mi, cb * 512:(cb + 1) * 512], start=True, stop=True, R=["GR", "cst"], W=["ps1"])
                    mk.op("act", "activation", out=GB[:, j, mi, cb * 512:(cb + 1) * 512], in_=PS[1][:, :], func=AF.Copy,
                          R=["ps1"], W=["GB"])
        mk.barrier()
        P.close()

    def make_norm(P, nbuf=2):
        st = dict(junk=P.sb([128, D], BF16, "junk"), ss=[P.sb([128, 1], F32, "ss") for _ in range(nbuf)],
                  xn=[P.sb([128, D], BF16, "xn") for _ in range(nbuf)],
                  tmp=[P.sb([128, 8, 128], F32, "tmp") for _ in range(nbuf)], i=0, nbuf=nbuf)

        def norm(xt_ap, xt_key, j, msc, msh, h_out, h_key):
            i = st["i"] % st["nbuf"]
            st["i"] += 1
            ss, xn, tmp = st["ss"][i], st["xn"][i], st["tmp"][i]
            mk.op("act", "activation", out=st["junk"][:], in_=xt_ap, func=AF.Square, accum_out=ss[:],
                  R=[xt_key], W=["junk", f"ss{i}"])
            mk.op("act", "activation", out=ss[:], in_=ss[:], func=AF.Sqrt, scale=1.0 / D, bias=EPS,
                  R=[f"ss{i}"], W=[f"ss{i}"])
            mk.op("dve", "reciprocal", out=ss[:], in_=ss[:], R=[f"ss{i}"], W=[f"ss{i}"])
            mk.op("dve", "tensor_scalar", out=xn[:], in0=xt_ap, scalar1=ss[:, 0:1], scalar2=None, op0=ALU.mult,
                  R=[xt_key, f"ss{i}"], W=[f"xn{i}"])
            for c in range(8):
                mk.op("pe", "transpose", out=PQ[i][:, c * 128:(c + 1) * 128], in_=xn[:, c * 128:(c + 1) * 128],
                      identity=identb[:], R=[f"xn{i}", "identb"], W=[f"pq{i}"])
            mk.op("dve", "tensor_tensor", out=tmp[:], in0=PQ[i][:, :].rearrange("p (c n) -> p c n", c=8),
                  in1=MODC[:, msc, :, j:j + 1].to_broadcast([128, 8, 128]), op=ALU.mult,
                  R=[f"pq{i}", "MODC"], W=[f"ntmp{i}"])
            mk.op("pool", "tensor_tensor", out=h_out, in0=tmp[:],
                  in1=MODC[:, msh, :, j:j + 1].to_broadcast([128, 8, 128]), op=ALU.add,
                  R=[f"ntmp{i}", "MODC"], W=[h_key])
        return norm

    def inproj_phase(l, last=False):
        P = Pool(nc, f"ip{l}")
        hT = P.sb([128, 8, T], BF16, "hT")
        xt = [P.sb([128, D], F32, "xt") for _ in range(2)]
        norm = make_norm(P)
        for t in range(NT):
            i = t % 2
            mk.dma("sp", xt[i][:], XR[t * 128:(t + 1) * 128, :], R=[f"XR{t}"], W=[f"xt{i}"])
            norm(xt[i][:], f"xt{i}", cond_of(t), 1, 0, hT[:, :, t * 128:(t + 1) * 128], f"hT{t}")
        hkeys = [f"hT{t}" for t in range(NT)]
        wf = [P.sb([128, 8, 512], F32, "wf")] * 2
        wb = [P.sb([128, 8, 512], BF16, "wb") for _ in range(2)]
        stg = [P.sb([128, T], F32, "stg")] * 2
        nblk = 0
        tblocks = [(i * 512, min(512, T - i * 512)) for i in range(9)]
        for cb in range(4):
            i = nblk % 2
            nblk += 1
            mk.dma("sp", wf[i][:], w_fm[l].rearrange("(c p) f -> p c f", p=128)[:, :, cb * 512:(cb + 1) * 512],
                   W=["wf"])
            mk.ev(wb[i][:], wf[i][:], R=["wf"], W=[f"wb{i}"])
            for sub in range(4):
                fc = cb * 4 + sub
                si = fc % 2
                for bi, (t0, tw) in enumerate(tblocks):
                    pb = bi % 2
                    for k in range(8):
                        mk.op("pe", "matmul", PS[pb][:, 0:tw], lhsT=wb[i][:, k, sub * 128:(sub + 1) * 128],
                              rhs=hT[:, k, t0:t0 + tw], start=(k == 0), stop=(k == 7),
                              R=[f"wb{i}"] + hkeys[t0 // 128:(t0 + tw) // 128], W=[f"ps{pb}"])
                    mk.ev(stg[si][:, t0:t0 + tw], PS[pb][:, 0:tw], R=[f"ps{pb}"], W=["stg"])
                mk.dma("pool", FM[fc], stg[si][:], R=["stg"], W=[f"FM{fc}"])
        cblocks = [(i * 512, 512) for i in range(4)] + [(2048, TMW - 2048)] + [(TMW + i * 512, 512) for i in range(8)]
        stt = [P.sb([128, 4, 512], F32, "stt") for _ in range(2)]
        stb = [P.sb([128, 4, 512], BF16, "stb") for _ in range(2)]
        tgroups = [(g * 4, min(4, NT - g * 4)) for g in range(9)]
        ns = 0
        for (c0, cw) in cblocks:
            i = nblk % 2
            nblk += 1
            mk.dma("sp", wf[i][:, :, 0:cw], w_tm[l].rearrange("(c p) f -> p c f", p=128)[:, :, c0:c0 + cw], W=["wf"])
            mk.ev(wb[i][:, :, 0:cw], wf[i][:, :, 0:cw], R=["wf"], W=[f"wb{i}"])
            is_mg = c0 >= TMW
            for (g0, gn) in tgroups:
                if is_mg and last and not any((g0 + q) in OUT_T for q in range(gn)):
                    continue
                si = ns % 2
                ns += 1
                for tt in range(gn):
                    t = g0 + tt
                    pb = 2 + (t % 2)
                    for k in range(8):
                        mk.op("pe", "matmul", PS[pb][:, 0:cw], lhsT=hT[:, k, t * 128:(t + 1) * 128],
                              rhs=wb[i][:, k, 0:cw], start=(k == 0), stop=(k == 7), R=[f"wb{i}", f"hT{t}"], W=[f"ps{pb}"])
                    if is_mg:
                        mk.ev(stb[si][:, tt, 0:cw], PS[pb][:, 0:cw], R=[f"ps{pb}"], W=[f"stb{si}"], func=AF.Sigmoid)
                    else:
                        mk.ev(stt[si][:, tt, 0:cw], PS[pb][:, 0:cw], R=[f"ps{pb}"], W=[f"stt{si}"])
                if is_mg:
                    mk.dma("pool", MG[g0 * 128:(g0 + gn) * 128, c0 - TMW:c0 - TMW + cw].rearrange("(t p) c -> p t c", p=128),
                           stb[si][:, 0:gn, 0:cw], R=[f"stb{si}"], W=["MG"])
                else:
                    mk.dma("pool", TM[g0 * 128:(g0 + gn) * 128, c0:c0 + cw].rearrange("(t p) c -> p t c", p=128),
                           stt[si][:, 0:gn, 0:cw], R=[f"stt{si}"], W=["TM"])
        mk.barrier()
        P.close()

    SEGS = [(0, 256), (256, T)]

    def conv_fm(u, src, w4, bcol, keyu, keysrc, wkeys):
        for (s0, e) in SEGS:
            mk.op("act", "activation", out=u[:, s0:e], in_=src[:, s0:e], func=AF.Identity, scale=w4[:, 2:3], bias=bcol,
                  R=[keysrc] + wkeys, W=[keyu])
            for k, sh in ((0, -2), (1, -1), (3, 1), (4, 2)):
                if sh < 0:
                    o, i_ = u[:, s0 - sh:e], src[:, s0:e + sh]
                else:
                    o, i_ = u[:, s0:e - sh], src[:, s0 + sh:e]
                mk.op("dve", "scalar_tensor_tensor", out=o, in0=i_, scalar=w4[:, k:k + 1], in1=o, op0=ALU.mult,
                      op1=ALU.add, R=[keysrc, keyu] + wkeys, W=[keyu])

    def mixer_a(l):
        P = Pool(nc, f"ma{l}")
        cw = P.sb([128, 2, 5], F32, "cw")
        cb = P.sb([128, 2], F32, "cb")
        gwf = P.sb([128, 2, 2, 2, 128], F32, "gwf")
        gwb = P.sb([128, 2, 2, 2, 128], BF16, "gwb")
        gb = P.sb([128, 2, 2, 2], F32, "gb")
        lam = P.sb([128, 2, 2], F32, "lam")
        c1 = P.sb([128, 2, 2], F32, "c1")
        mk.dma("sp", cw[:], a_cw[l], W=["a_cw"])
        mk.dma("sp", cb[:], a_cb[l], W=["a_cb"])
        mk.dma("sp", gwf[:], a_gw[l], W=["a_gwf"])
        mk.dma("sp", gb[:], a_gb[l], W=["a_gb"])
        mk.dma("sp", lam[:], a_lam[l], W=["a_lam"])
        mk.op("dve", "tensor_copy", out=gwb[:], in_=gwf[:], R=["a_gwf"], W=["a_gwb"])
        mk.op("act", "activation", out=c1[:], in_=lam[:], func=AF.Exp, scale=-1.0, R=["a_lam"], W=["a_c1"])
        mk.op("act", "activation", out=c1[:], in_=c1[:], func=AF.Ln, bias=1.0, R=["a_c1"], W=["a_c1"])
        mk.op("dve", "tensor_scalar", out=c1[:], in0=c1[:], scalar1=-8.0, scalar2=None, op0=ALU.mult, R=["a_c1"], W=["a_c1"])
        ax = P.sb([128, T], F32, "ax")
        ag = P.sb([128, T], F32, "ag")
        u = P.sb([128, T], F32, "u")
        ub = P.sb([128, T], BF16, "ub")
        aa = P.sb([128, T], F32, "aa")
        bt = P.sb([128, T], F32, "bt")
        hf = P.sb([128, T], F32, "hf")
        hb = P.sb([128, T], F32, "hb")
        r = [P.sb([128, 512], F32, "r") for _ in range(2)]
        gi = [P.sb([128, 512], F32, "gi") for _ in range(2)]
        yb = P.sb([128, T], BF16, "yb")
        tblocks = [(i * 512, min(512, T - i * 512)) for i in range(9)]
        for c in range(2):
            mk.dma("sp", ax[:], FM[FM_OFF["a_x"] + c], R=[f"FM{FM_OFF['a_x'] + c}"], W=["ax"])
            mk.dma("sp", ag[:], FM[FM_OFF["a_g"] + c], R=[f"FM{FM_OFF['a_g'] + c}"], W=["ag"])
            conv_fm(u, ax, cw[:, c, :], cb[:, c:c + 1], "u", "ax", ["a_cw", "a_cb"])
            mk.op("pool", "tensor_copy", out=ub[:], in_=u[:], R=["u"], W=["ub"])
            for d in range(2):
                for bi, (t0, tw) in enumerate(tblocks):
                    i = bi % 2
                    mk.op("pe", "matmul", PS[i][:, 0:tw], lhsT=gwb[:, d, 0, c, :], rhs=ub[:, t0:t0 + tw], start=True,
                          stop=True, R=["a_gwb", "ub"], W=[f"ps{i}"])
                    mk.op("pe", "matmul", PS[2 + i][:, 0:tw], lhsT=gwb[:, d, 1, c, :], rhs=ub[:, t0:t0 + tw], start=True,
                          stop=True, R=["a_gwb", "ub"], W=[f"ps{2 + i}"])
                    mk.op("act", "activation", out=r[i][:, 0:tw], in_=PS[i][:, 0:tw], func=AF.Sigmoid,
                          bias=gb[:, d, 0, c:c + 1], R=[f"ps{i}", "a_gb"], W=[f"r{i}"])
                    mk.op("act", "activation", out=gi[i][:, 0:tw], in_=PS[2 + i][:, 0:tw], func=AF.Sigmoid,
                          bias=gb[:, d, 1, c:c + 1], R=[f"ps{2 + i}", "a_gb"], W=[f"gi{i}"])
                    mk.op("act", "activation", out=aa[:, t0:t0 + tw], in_=r[i][:, 0:tw], func=AF.Exp,
                          scale=c1[:, d, c:c + 1], R=[f"r{i}", "a_c1"], W=["aa"])
                    mk.op("dve", "tensor_tensor", out=r[i][:, 0:tw], in0=aa[:, t0:t0 + tw], in1=aa[:, t0:t0 + tw],
                          op=ALU.mult, R=["aa", f"r{i}"], W=[f"r{i}"])
                    mk.op("dve", "tensor_scalar", out=r[i][:, 0:tw], in0=r[i][:, 0:tw], scalar1=-1.0, scalar2=1.0,
                          op0=ALU.mult, op1=ALU.add, R=[f"r{i}"], W=[f"r{i}"])
                    mk.op("act", "activation", out=r[i][:, 0:tw], in_=r[i][:, 0:tw], func=AF.Sqrt, R=[f"r{i}"], W=[f"r{i}"])
                    mk.op("dve", "tensor_tensor", out=gi[i][:, 0:tw], in0=gi[i][:, 0:tw], in1=r[i][:, 0:tw], op=ALU.mult,
                          R=[f"gi{i}", f"r{i}"], W=[f"gi{i}"])
                    mk.op("pool", "tensor_tensor", out=bt[:, t0:t0 + tw], in0=gi[i][:, 0:tw], in1=u[:, t0:t0 + tw],
                          op=ALU.mult, R=[f"gi{i}", "u"], W=["bt"])
                if d == 0:
                    mk.op("dve", "tensor_tensor_scan", out=hf[:, :], data0=aa[:, :], data1=bt[:, :], initial=0.0,
                          op0=ALU.mult, op1=ALU.add, R=["aa", "bt"], W=["hf"])
                else:
                    mk.op("dve", "tensor_tensor_scan", out=hb[:, 0:256][:, ::-1], data0=aa[:, 0:256][:, ::-1],
                          data1=bt[:, 0:256][:, ::-1], initial=0.0, op0=ALU.mult, op1=ALU.add, R=["aa", "bt"], W=["hb"])
                    mk.op("dve", "tensor_tensor_scan", out=hb[:, 256:T][:, ::-1], data0=aa[:, 256:T][:, ::-1],
                          data1=bt[:, 256:T][:, ::-1], initial=hb[:, 0:1], op0=ALU.mult, op1=ALU.add,
                          R=["aa", "bt", "hb"], W=["hb"])
            mk.op("act", "activation", out=ag[:], in_=ag[:], func=AF.Gelu, R=["ag"], W=["ag"])
            mk.op("dve", "tensor_tensor", out=hf[:], in0=hf[:], in1=hb[:], op=ALU.add, R=["hf", "hb"], W=["hf"])
            mk.op("dve", "tensor_tensor", out=yb[:], in0=hf[:], in1=ag[:], op=ALU.mult, R=["hf", "ag"], W=["yb"])
            mk.dma("pool", YT[0, c], yb[:], R=["yb"], W=[f"YT0{c}"])
        mk.barrier()
        P.close()

    def order_of(dr):
        return list(range(NT)) if dr == 0 else [1, 0] + list(range(NT - 1, 1, -1))

    def run_pipelined(gens, pipelined=True):
        if not pipelined:
            for g in gens:
                for _ in g:
                    pass
            return
        prev = None
        for g in gens:
            next(g, None)
            if prev is not None:
                for _ in prev:
                    pass
            prev = g
        if prev is not None:
            for _ in prev:
                pass

    OUT_T = list(range(2, 2 + HALF_OUT // 128))

    def plan(dr, last):
        if not last:
            return [(t, True) for t in order_of(dr)]
        if dr == 0:
            return [(0, False), (1, False)] + [(t, True) for t in OUT_T]
        return [(t, (t in OUT_T)) for t in order_of(dr)]

    def chunk_core(it, nsub, dr, QT, KT, QIT, KHs, V, vw, Gc, S, Sbf, maskD, maskkey, rkeys, PT, sk, full=True):
        assert nsub == 1
        pi = it % 2
        par = it % 2
        okeys = ["ps2", "ps3"]
        Oh = [PS[2 + hh][:, 0:2 * vw].rearrange("p (c e) -> p c e", c=2) for hh in range(2)]
        KVp = PS[4][:, 0:2 * vw].rearrange("p (c e) -> p c e", c=2)
        for h in range(4):
            c, hh = h // 2, h % 2
            mk.op("pe", "matmul", KVp[hh * 64:(hh + 1) * 64, c, :], lhsT=KHs(0)[:, h * 64:(h + 1) * 64],
                  rhs=V[:, h, :], start=True, stop=True, R=rkeys, W=["ps4kv"])
        for c in range(2):
            mk.op("dve", "scalar_tensor_tensor", out=S[1 - par][c][:], in0=S[par][c][:], scalar=Gc[:, c, 0:1],
                  in1=KVp[:, c, :], op0=ALU.mult, op1=ALU.add, R=[f"{sk}S{par}{c}", "ps4kv"] + rkeys,
                  W=[f"{sk}S{1 - par}{c}"])
            mk.op("act", "activation", out=Sbf[1 - par][c][:], in_=S[1 - par][c][:], func=AF.Copy,
                  R=[f"{sk}S{1 - par}{c}"], W=[f"{sk}Sbf{1 - par}{c}"])
        if not full:
            return okeys, Oh
        for h in range(4):
            c, hh = h // 2, h % 2
            rs = slice(hh * 64, (hh + 1) * 64)
            mk.op("pe", "matmul", PS[hh][:, c * 128:(c + 1) * 128], lhsT=KT[c][rs, :], rhs=QT[c][rs, :], start=True,
                  stop=True, R=rkeys, W=[f"ps{hh}"])
        PTv = PT[pi][:].rearrange("p (c x n) -> p c x n", c=2, x=2)
        Mv = maskD.rearrange("p (c x n) -> p c x n", c=2, x=2)
        for hh in range(2):
            mk.op("dve", "tensor_tensor", out=PTv[:, :, hh, :], in0=PS[hh][:, 0:256].rearrange("p (c n) -> p c n", c=2),
                  in1=Mv[:, :, hh, :], op=ALU.mult, R=[f"ps{hh}", maskkey], W=[f"PT{pi}h{hh}"])
        ptk = [f"PT{pi}h0", f"PT{pi}h1"]
        for h in range(4):
            c, hh = h // 2, h % 2
            rs = slice(hh * 64, (hh + 1) * 64)
            mk.op("pe", "matmul", Oh[hh][:, c, :], lhsT=PT[pi][:, h * 128:(h + 1) * 128], rhs=V[:, h, :], start=True,
                  stop=False, R=[ptk[hh]] + rkeys, W=[okeys[hh]])
            mk.op("pe", "matmul", Oh[hh][:, c, :], lhsT=QIT[c][rs, :], rhs=Sbf[par][c][rs, :], start=False, stop=True,
                  R=rkeys + [f"{sk}Sbf{par}{c}"], W=[okeys[hh]])
        return okeys, Oh

    def hview(ap256, hh):
        return ap256.rearrange("p (c x e) -> p c x e", c=2, x=2)[:, :, hh, :]

    def make_finalize(P, n, yTb):
        st = dict(i=0)
        cent = [P.sb([128, 4, 64], F32, "cent") for _ in range(2)]
        sq = P.sb([128, 4, 64], F32, "sq")
        mm = [P.sb([128, 4], F32, "mm") for _ in range(2)]
        vv = [P.sb([128, 4], F32, "vv") for _ in range(2)]
        yy = [P.sb([128, 256], BF16, "yy") for _ in range(2)]

        def fin(tot, totkey, center, gate, gatekey, t):
            i = st["i"] % 2
            st["i"] += 1
            tv = tot.rearrange("p (h e) -> p h e", h=4)
            tk = list(totkey) if isinstance(totkey, (list, tuple)) else [totkey]
            src, skeys = tv, tk
            if center:
                mk.op("dve", "tensor_reduce", out=mm[i][:], in_=tv, axis=AX.X, op=ALU.add, R=tk, W=[f"fmm{i}"])
                mk.op("dve", "tensor_scalar", out=mm[i][:], in0=mm[i][:], scalar1=-1.0 / 64, scalar2=None, op0=ALU.mult,
                      R=[f"fmm{i}"], W=[f"fmm{i}"])
                mk.op("dve", "tensor_tensor", out=cent[i][:], in0=tv, in1=mm[i][:].unsqueeze(2).to_broadcast([128, 4, 64]),
                      op=ALU.add, R=tk + [f"fmm{i}"], W=[f"fcent{i}"])
                src, skeys = cent[i][:], [f"fcent{i}"]
            mk.op("pool", "tensor_tensor", out=sq[:], in0=src, in1=src, op=ALU.mult, R=skeys, W=["fsq"])
            mk.op("dve", "tensor_reduce", out=vv[i][:], in_=sq[:], axis=AX.X, op=ALU.add, R=["fsq"], W=[f"fvv{i}"])
            mk.op("act", "activation", out=vv[i][:], in_=vv[i][:], func=AF.Sqrt, scale=1.0 / 64, bias=EPS,
                  R=[f"fvv{i}"], W=[f"fvv{i}"])
            mk.op("dve", "reciprocal", out=vv[i][:], in_=vv[i][:], R=[f"fvv{i}"], W=[f"fvv{i}"])
            mk.op("dve", "tensor_tensor", out=cent[i][:], in0=src, in1=vv[i][:].unsqueeze(2).to_broadcast([128, 4, 64]),
                  op=ALU.mult, R=skeys + [f"fvv{i}"], W=[f"fcent{i}"])
            mk.op("dve", "tensor_tensor", out=yy[i][:], in0=cent[i][:].rearrange("p h e -> p (h e)"), in1=gate,
                  op=ALU.mult, R=[f"fcent{i}", gatekey], W=[f"fyy{i}"])
            for c in range(2):
                mk.op("pe", "transpose", out=PQ[1][:, (i * 2 + c) * 128:(i * 2 + c + 1) * 128],
                      in_=yy[i][:, c * 128:(c + 1) * 128], identity=identb[:], R=[f"fyy{i}", "identb"], W=[f"pq1f{i}"])
            mk.op("act", "activation", out=yTb[:, :, t * 128:(t + 1) * 128],
                  in_=PQ[1][:, i * 256:(i + 1) * 256].rearrange("p (c n) -> p c n", c=2), func=AF.Copy,
                  R=[f"pq1f{i}"], W=["yTb"])
        return fin

    def mixer_b(l, last=False):
        P = Pool(nc, f"mb{l}")
        QR = [P.sb([128, T], BF16, "QR") for _ in range(2)]
        KR = [P.sb([128, T], BF16, "KR") for _ in range(2)]
        thp = P.sb([128, 2, 2], F32, "thp")
        thh = P.sb([128, 2, 4], F32, "thh")
        mk.dma("sp", thp[:], b_thp[l], W=["thp"])
        mk.dma("sp", thh[:], b_thh[l], W=["thh"])
        for tt, key in ((thp, "thp"), (thh, "thh")):
            mk.op("act", "activation", out=tt[:], in_=tt[:], func=AF.Exp, scale=-1.0, R=[key], W=[key])
            mk.op("act", "activation", out=tt[:], in_=tt[:], func=AF.Ln, bias=1.0, R=[key], W=[key])
            mk.op("dve", "tensor_scalar", out=tt[:], in0=tt[:], scalar1=-1.0, scalar2=None, op0=ALU.mult, R=[key], W=[key])
        DM = P.sb([128, 2, 512], F32, "DM")
        QW = P.sb([128, 2, 2, 128], F32, "QW")
        KW = P.sb([128, 2, 4], F32, "KW")
        Gc = P.sb([128, 2, 2, 1], F32, "Gc")
        for dr in range(2):
            diff, msk, pos = (C("diffF"), C("maskF"), C("posF")) if dr == 0 else (C("diffB"), C("maskB"), C("posB"))
            for h in range(4):
                mk.op("act", "activation", out=DM[:, dr, h * 128:(h + 1) * 128], in_=diff, func=AF.Exp,
                      scale=thh[:, dr, h:h + 1], R=["cst", "thh"], W=["DM"])
                mk.op("dve", "tensor_tensor", out=DM[:, dr, h * 128:(h + 1) * 128], in0=DM[:, dr, h * 128:(h + 1) * 128],
                      in1=msk, op=ALU.mult, R=["DM", "cst"], W=["DM"])
                mk.op("act", "activation", out=KW[:, dr, h:h + 1], in_=C("kpos")[:, dr:dr + 1], func=AF.Exp,
                      scale=thh[:, dr, h:h + 1], R=["cst", "thh"], W=["KW"])
            for c in range(2):
                mk.op("act", "activation", out=QW[:, dr, c, :], in_=pos, func=AF.Exp, scale=thp[:, dr, c:c + 1],
                      R=["cst", "thp"], W=["QW"])
                mk.op("act", "activation", out=Gc[:, dr, c, :], in_=thp[:, dr, c:c + 1], func=AF.Exp, scale=128.0,
                      R=["thp"], W=["Gc"])
        segw = 1088
        f1 = [P.sb([128, segw], F32, "f1") for _ in range(2)]
        f2 = [P.sb([128, segw], F32, "f2") for _ in range(2)]
        rc = P.sb([128, T], F32, "rc")
        rsn = P.sb([128, T], F32, "rsn")
        mk.dma("sp", rc[:], ropeC_d, W=["rc"])
        mk.dma("sp", rsn[:], ropeS_d, W=["rsn"])
        n = 0
        for (dst, base, pbase, scale) in ((QR, "b_q", "b_qp", 1.0), (KR, "b_k", "b_kp", 0.125)):
            for c in range(2):
                for sg in range(4):
                    i = n % 2
                    n += 1
                    cs = slice(sg * segw, (sg + 1) * segw)
                    mk.dma("sp", f1[i][:], FM[FM_OFF[base] + c][:, cs], R=[f"FM{FM_OFF[base] + c}"], W=[f"f1{i}"])
                    mk.dma("sp", f2[i][:], FM[FM_OFF[pbase] + c][:, cs], R=[f"FM{FM_OFF[pbase] + c}"], W=[f"f2{i}"])
                    mk.op("dve", "tensor_tensor", out=f1[i][:], in0=f1[i][:], in1=rc[:, cs], op=ALU.mult,
                          R=[f"f1{i}", "rc"], W=[f"f1{i}"])
                    mk.op("pool", "tensor_tensor", out=f2[i][:], in0=f2[i][:], in1=rsn[:, cs], op=ALU.mult,
                          R=[f"f2{i}", "rsn"], W=[f"f2{i}"])
                    mk.op("dve", "tensor_tensor", out=f1[i][:], in0=f1[i][:], in1=f2[i][:], op=ALU.add,
                          R=[f"f1{i}", f"f2{i}"], W=[f"f1{i}"])
                    mk.op("act", "activation", out=dst[c][:, cs], in_=f1[i][:], func=AF.Copy, scale=scale,
                          R=[f"f1{i}"], W=[f"b{base}{c}"])
        rkeys = ["bb_q0", "bb_q1", "bb_k0", "bb_k1"]
        OF = P.sb([128, NT, 256], F32, "OF")
        yTb = P.sb([128, 2, T], BF16, "yTb")
        fin = make_finalize(P, 1, yTb)
        PT = [P.sb([128, 512], BF16, "PT") for _ in range(2)]
        QIT = [[P.sb([128, 128], BF16, "QIT") for _ in range(2)] for _ in range(2)]
        KH = [P.sb([128, 256], BF16, "KH") for _ in range(2)]
        Vf = [P.sb([128, 512], F32, "Vf") for _ in range(2)]
        Vb = [P.sb([128, 4, 64], BF16, "Vb") for _ in range(2)]
        gt = [P.sb([128, 256], F32, "gt") for _ in range(2)]
        tot = [P.sb([128, 256], F32, "tot") for _ in range(2)]
        S = [[P.sb([128, 64], F32, "S") for _ in range(2)] for _ in range(2)]
        Sbf = [[P.sb([128, 64], BF16, "Sbf") for _ in range(2)] for _ in range(2)]
        it = 0
        for dr in range(2):
            for c in range(2):
                mk.op("pool", "memset", S[it % 2][c][:], 0.0, W=[f"bS{it % 2}{c}"])
                mk.op("pool", "memset", Sbf[it % 2][c][:], 0.0, W=[f"bSbf{it % 2}{c}"])
            def body(t, full, it):
                i = it % 2
                cols = slice(t * 128, (t + 1) * 128)
                mk.dma("sp", Vf[i][:], TM[t * 128:(t + 1) * 128, 0:512], R=["TM"], W=[f"bVf{i}"])
                mk.op("pool", "tensor_copy", out=Vb[i][:], in_=Vf[i][:, 0:256].rearrange("p (h e) -> p h e", h=4),
                      R=[f"bVf{i}"], W=[f"bVb{i}"])
                for c in range(2):
                    if full:
                        mk.op("pool", "tensor_tensor", out=QIT[i][c][:], in0=QR[c][:, cols], in1=QW[:, dr, c, :],
                              op=ALU.mult, R=[f"bb_q{c}", "QW"], W=[f"bQIT{i}"])
                    mk.op("pe", "transpose", out=PQ[0][:, (i * 2 + c) * 128:(i * 2 + c + 1) * 128], in_=KR[c][:, cols],
                          identity=identb[:], R=[f"bb_k{c}", "identb"], W=[f"pq0k{i}"])
                for h in range(4):
                    mk.op("act", "activation", out=KH[i][:, h * 64:(h + 1) * 64],
                          in_=PQ[0][:, i * 256 + h * 64:i * 256 + (h + 1) * 64], func=AF.Identity, scale=KW[:, dr, h:h + 1],
                          R=[f"pq0k{i}", "KW"], W=[f"bKH{i}"])
                yield
                okeys, Oh = chunk_core(it, 1, dr, [QR[0][:, cols], QR[1][:, cols]], [KR[0][:, cols], KR[1][:, cols]],
                                       [QIT[i][0], QIT[i][1]], (lambda s_, kh=KH[i]: kh), Vb[i], 64, Gc[:, dr], S, Sbf,
                                       DM[:, dr, :], "DM", rkeys + [f"bQIT{i}", f"bKH{i}", f"bVb{i}"], PT, "b", full=full)
                if not full:
                    pass
                elif dr == 0:
                    for hh in range(2):
                        mk.op("act", "activation", out=hview(OF[:, t, :], hh), in_=Oh[hh], func=AF.Copy, R=[okeys[hh]],
                              W=[f"bOF{t}h{hh}"])
                else:
                    for hh in range(2):
                        mk.op("dve", "tensor_tensor", out=hview(tot[i][:], hh), in0=Oh[hh], in1=hview(OF[:, t, :], hh),
                              op=ALU.add, R=[okeys[hh], f"bOF{t}h{hh}"], W=[f"btot{i}h{hh}"])
                    mk.op("act", "activation", out=gt[i][:], in_=Vf[i][:, 256:512], func=AF.Silu, R=[f"bVf{i}"],
                          W=[f"bgt{i}"])
                    fin(tot[i][:], [f"btot{i}h0", f"btot{i}h1"], True, gt[i][:], f"bgt{i}", t)
            gens = []
            for t, full in plan(dr, last):
                gens.append(body(t, full, it))
                it += 1
            run_pipelined(gens, pipelined=not last)
        for c in range(2):
            mk.dma("pool", YT[1, c], yTb[:, c, :], R=["yTb"], W=[f"YT1{c}"])
        mk.barrier()
        P.close()

    def mixer_c(l, last=False):
        P = Pool(nc, f"mc{l}")
        QC = [P.sb([128, T], BF16, "QC") for _ in range(2)]
        KC = [P.sb([128, T], BF16, "KC") for _ in range(2)]
        cw = P.sb([128, 4, 5], F32, "cw")
        cb = P.sb([128, 4], F32, "cb")
        gbias = P.sb([128, 16], F32, "gbias")
        mk.dma("sp", cw[:], c_cw[l], W=["c_cw"])
        mk.dma("sp", cb[:], c_cb[l], W=["c_cb"])
        mk.dma("sp", gbias[:], c_gb[l], W=["c_gb"])
        src = P.sb([128, T], F32, "src")
        u = P.sb([128, T], F32, "u")
        for ch in range(4):
            fc = FM_OFF["c_q"] + ch
            mk.dma("sp", src[:], FM[fc], R=[f"FM{fc}"], W=["csrc"])
            conv_fm(u, src, cw[:, ch, :], cb[:, ch:ch + 1], "cu", "csrc", ["c_cw", "c_cb"])
            dst = QC[ch] if ch < 2 else KC[ch - 2]
            mk.op("act", "activation", out=u[:], in_=u[:], func=AF.Silu, R=["cu"], W=["cu"])
            mk.op("dve", "tensor_scalar", out=dst[:], in0=u[:], scalar1=(1.0 if ch < 2 else 0.125), scalar2=None,
                  op0=ALU.mult, R=["cu"], W=[f"cqk{ch}"])
        Z = P.sb([128, NT, 16], F32, "Z")
        LFN = P.sb([128, NT, 16], F32, "LFN")
        mk.dma("sp", Z[:], TM[:, TM_OFF["c_gates"]:TM_OFF["c_gates"] + 16].rearrange("(t p) g -> p t g", p=128),
               R=["TM"], W=["cZ"])
        mk.op("dve", "tensor_tensor", out=Z[:], in0=Z[:], in1=gbias[:].unsqueeze(1).to_broadcast([128, NT, 16]),
              op=ALU.add, R=["cZ", "c_gb"], W=["cZ"])
        mk.op("act", "activation", out=LFN[:], in_=Z[:], func=AF.Exp, scale=-1.0, R=["cZ"], W=["cLFN"])
        mk.op("act", "activation", out=LFN[:], in_=LFN[:], func=AF.Ln, bias=1.0, R=["cLFN"], W=["cLFN"])
        mk.op("dve", "tensor_scalar", out=LFN[:], in0=LFN[:], scalar1=-1.0, scalar2=None, op0=ALU.mult, R=["cLFN"],
              W=["cLFN"])
        rkeys = ["cqk0", "cqk1", "cqk2", "cqk3"]
        OF = P.sb([128, NT, 256], F32, "OF")
        yTb = P.sb([128, 2, T], BF16, "yTb")
        fin = make_finalize(P, 2, yTb)
        PT = [P.sb([128, 512], BF16, "PT") for _ in range(2)]
        QIT = [[P.sb([128, 128], BF16, "QIT") for _ in range(2)] for _ in range(2)]
        KH = [P.sb([128, 256], BF16, "KH") for _ in range(2)]
        Vf = [P.sb([128, 512], F32, "Vf") for _ in range(2)]
        Vb = [P.sb([128, 4, 65], BF16, "Vb") for _ in range(2)]
        gt = [P.sb([128, 256], F32, "gt") for _ in range(2)]
        tot = [P.sb([128, 256], F32, "tot") for _ in range(2)]
        S = [[P.sb([128, 65], F32, "S") for _ in range(2)] for _ in range(2)]
        Sbf = [[P.sb([128, 65], BF16, "Sbf") for _ in range(2)] for _ in range(2)]
        Bm4 = [P.sb([128, 4, 128], F32, "Bm4") for _ in range(2)]
        tmp4 = [P.sb([128, 512], F32, "tmp4") for _ in range(2)]
        Dm4 = [P.sb([128, 512], F32, "Dm4") for _ in range(2)]
        EB4 = [P.sb([128, 512], F32, "EB4") for _ in range(2)]
        lmb = [P.sb([128, 4], F32, "lmb") for _ in range(2)]
        kw = [P.sb([128, 4], F32, "kw") for _ in range(2)]
        Gc = [P.sb([128, 2, 1], F32, "Gc") for _ in range(2)]
        rden = [P.sb([128, 4], F32, "rden") for _ in range(2)]
        ebe = [P.sb([128, 4], F32, "ebe") for _ in range(2)]
        hid = [P.sb([128, 4, 64], F32, "hid") for _ in range(2)]
        for i in range(2):
            mk.op("pool", "memset", Vb[i][:], 1.0, W=[f"cVb{i}"])
        ones = C("ones")
        it = 0
        for dr in range(2):
            tri = C("triF") if dr == 0 else C("triB")
            neg4 = C("negF4") if dr == 0 else C("negB4")
            e = 127 if dr == 0 else 0
            for c in range(2):
                mk.op("pool", "memset", S[it % 2][c][:], 0.0, W=[f"cS{it % 2}{c}"])
                mk.op("pool", "memset", Sbf[it % 2][c][:], 0.0, W=[f"cSbf{it % 2}{c}"])
            def body(t, full, it):
                i = it % 2
                cols = slice(t * 128, (t + 1) * 128)
                li = Z[:, t, dr * 8:dr * 8 + 4]
                lf = LFN[:, t, dr * 8 + 4:dr * 8 + 8]
                mk.dma("sp", Vf[i][:], TM[t * 128:(t + 1) * 128, 512:1024], R=["TM"], W=[f"cVf{i}"])
                mk.op("pool", "tensor_copy", out=Vb[i][:, :, 0:64], in_=Vf[i][:, 0:256].rearrange("p (h e) -> p h e", h=4),
                      R=[f"cVf{i}"], W=[f"cVb{i}"])
                mk.op("dve", "tensor_tensor", out=Bm4[i][:], in0=tri.unsqueeze(1).to_broadcast([128, 4, 128]),
                      in1=lf.unsqueeze(2).to_broadcast([128, 4, 128]), op=ALU.mult, R=["cst", "cLFN"], W=[f"cBm{i}"])
                mk.op("pe", "matmul", PS[5][:, :], lhsT=ones, rhs=Bm4[i][:].rearrange("p h n -> p (h n)"), start=True,
                      stop=True, R=["cst", f"cBm{i}"], W=["ps5"])
                mk.op("pe", "matmul", PS[4][:, 256:260], lhsT=tri, rhs=lf, start=True, stop=True, R=["cst", "cLFN"],
                      W=["ps4b"])
                mk.op("dve", "tensor_tensor", out=lmb[i][:], in0=li, in1=PS[4][:, 256:260], op=ALU.subtract,
                      R=["cZ", "ps4b"], W=[f"clmb{i}"])
                if full:
                    mk.op("dve", "tensor_tensor", out=tmp4[i][:], in0=PS[5][:, :], in1=neg4, op=ALU.add, R=["ps5", "cst"],
                          W=[f"ctmp{i}"])
                    for h in range(4):
                        mk.op("act", "activation", out=Dm4[i][:, h * 128:(h + 1) * 128],
                              in_=tmp4[i][:, h * 128:(h + 1) * 128], func=AF.Exp, bias=lmb[i][:, h:h + 1],
                              R=[f"ctmp{i}", f"clmb{i}"], W=[f"cDm{i}"])
                    mk.op("act", "activation", out=EB4[i][:], in_=PS[5][:, :], func=AF.Exp, R=["ps5"], W=[f"cEB{i}"])
                bend = PS[5][:, :].rearrange("p (h n) -> p h n", h=4)[:, :, e]
                mk.op("dve", "tensor_tensor", out=kw[i][:], in0=lmb[i][:], in1=bend, op=ALU.add, R=[f"clmb{i}", "ps5"],
                      W=[f"ckw{i}"])
                mk.op("act", "activation", out=kw[i][:], in_=kw[i][:], func=AF.Exp, R=[f"ckw{i}"], W=[f"ckw{i}"])
                mk.op("act", "activation", out=ebe[i][:], in_=bend, func=AF.Exp, R=["ps5"], W=[f"cebe{i}"])
                for h in range(4):
                    c, hh = h // 2, h % 2
                    rs = slice(hh * 64, (hh + 1) * 64)
                    mk.op("pool", "tensor_copy", out=Gc[i][rs, c, :], in_=ebe[i][rs, h:h + 1],
                          R=[f"cebe{i}"], W=[f"cGc{i}"])
                    if full:
                        mk.op("pool", "tensor_tensor", out=QIT[i][c][rs, :], in0=QC[c][rs, cols],
                              in1=EB4[i][rs, h * 128:(h + 1) * 128], op=ALU.mult, R=[f"cqk{c}", f"cEB{i}"],
                              W=[f"cQIT{i}"])
                for c in range(2):
                    mk.op("pe", "transpose", out=PQ[0][:, (i * 2 + c) * 128:(i * 2 + c + 1) * 128], in_=KC[c][:, cols],
                          identity=identb[:], R=[f"cqk{2 + c}", "identb"], W=[f"pq0k{i}"])
                for h in range(4):
                    mk.op("act", "activation", out=KH[i][:, h * 64:(h + 1) * 64],
                          in_=PQ[0][:, i * 256 + h * 64:i * 256 + (h + 1) * 64], func=AF.Identity, scale=kw[i][:, h:h + 1],
                          R=[f"pq0k{i}", f"ckw{i}"], W=[f"cKH{i}"])
                yield
                okeys, Oh = chunk_core(it, 1, dr, [QC[0][:, cols], QC[1][:, cols]], [KC[0][:, cols], KC[1][:, cols]],
                                       [QIT[i][0], QIT[i][1]], (lambda s_, kh=KH[i]: kh), Vb[i], 65, Gc[i], S, Sbf,
                                       Dm4[i][:], f"cDm{i}",
                                       rkeys + [f"cQIT{i}", f"cKH{i}", f"cVb{i}", f"cGc{i}"], PT, "c", full=full)
                if not full:
                    return
                rdv = rden[i][:].rearrange("p (c x) -> p c x", c=2)
                for hh in range(2):
                    mk.op("act", "activation", out=rdv[:, :, hh], in_=Oh[hh][:, :, 64], func=AF.Abs, R=[okeys[hh]],
                          W=[f"crden{i}"])
                mk.op("dve", "tensor_scalar_max", out=rden[i][:], in0=rden[i][:], scalar1=1.0, R=[f"crden{i}"],
                      W=[f"crden{i}"])
                mk.op("dve", "reciprocal", out=rden[i][:], in_=rden[i][:], R=[f"crden{i}"], W=[f"crden{i}"])
                if dr == 0:
                    for hh in range(2):
                        mk.op("dve", "tensor_tensor", out=hview(OF[:, t, :], hh), in0=Oh[hh][:, :, 0:64],
                              in1=rdv[:, :, hh:hh + 1].to_broadcast([128, 2, 64]), op=ALU.mult,
                              R=[okeys[hh], f"crden{i}"], W=[f"cOF{t}h{hh}"])
                else:
                    for hh in range(2):
                        mk.op("dve", "tensor_tensor", out=hview(hid[i][:].rearrange("p h e -> p (h e)"), hh),
                              in0=Oh[hh][:, :, 0:64], in1=rdv[:, :, hh:hh + 1].to_broadcast([128, 2, 64]), op=ALU.mult,
                              R=[okeys[hh], f"crden{i}"], W=[f"chid{i}h{hh}"])
                    mk.op("pool", "tensor_tensor", out=tot[i][:], in0=hid[i][:].rearrange("p h e -> p (h e)"),
                          in1=OF[:, t, :], op=ALU.add, R=[f"chid{i}h0", f"chid{i}h1", f"cOF{t}h0", f"cOF{t}h1"],
                          W=[f"ctot{i}"])
                    mk.op("act", "activation", out=gt[i][:], in_=Vf[i][:, 256:512], func=AF.Sigmoid, R=[f"cVf{i}"],
                          W=[f"cgt{i}"])
                    fin(tot[i][:], f"ctot{i}", True, gt[i][:], f"cgt{i}", t)
            gens = []
            for t, full in plan(dr, last):
                gens.append(body(t, full, it))
                it += 1
            run_pipelined(gens, pipelined=not last)
        for c in range(2):
            mk.dma("pool", YT[2, c], yTb[:, c, :], R=["yTb"], W=[f"YT2{c}"])
        mk.barrier()
        P.close()

    def make_wconv(P, l):
        wstage = P.sb([128, 8, 512], F32, "wstage")
        wcb = P.sb([128, 4096], BF16, "wcb")
        tasks = []
        for e in range(16):
            tasks.append((moe_w1[l, e].rearrange("(c p) f -> p c f", p=128), wstage[:], WALL[e][:, 0:4096], f"W1B{e}"))
            tasks.append((moe_w3[l, e].rearrange("(c p) f -> p c f", p=128), wstage[:], WALL[e][:, 4096:8192], f"W3B{e}"))
            tasks.append((moe_w2[l, e].rearrange("(c p) f -> p c f", p=128),
                          wstage[:].rearrange("p c f -> p (c f)").rearrange("p (c f) -> p c f", c=4),
                          WALL[e][:, 8192:12288], f"W2B{e}"))
        st = dict(k=0)

        def step():
            if st["k"] >= len(tasks):
                return False
            src, stg, dst, key = tasks[st["k"]]
            st["k"] += 1
            mk.dma("sp", stg, src, W=["wstage"])
            mk.ev(wcb[:], wstage[:].rearrange("p c f -> p (c f)"), R=["wstage"], W=["wcb"])
            mk.dma("pool", dst, wcb[:], R=["wcb"], W=[key])
            return True
        return step

    def mixer_d(l, last=False):
        P = Pool(nc, f"md{l}")
        LB = P.sb([128, 256], F32, "LB")
        OML = P.sb([128, 256], F32, "OML")
        if l == 0:
            use_lb = False
        else:
            use_lb = True
            dl = P.sb([128, 2, 256], F32, "dl")
            mk.dma("sp", dl[:], d_lbr, W=["dl"])
            mk.op("dve", "tensor_tensor", out=LB[:], in0=dl[:, 1, :], in1=dl[:, 0, :], op=ALU.subtract, R=["dl"], W=["LB"])
            mk.op("act", "activation", out=LB[:], in_=LB[:], func=AF.Sigmoid, R=["LB"], W=["LB"])
            mk.op("dve", "tensor_scalar", out=OML[:], in0=LB[:], scalar1=-1.0, scalar2=1.0, op0=ALU.mult, op1=ALU.add,
                  R=["LB"], W=["OML"])
        OF = P.sb([128, NT, 256], F32, "OF")
        yTb = P.sb([128, 2, T], BF16, "yTb")
        fin = make_finalize(P, 3, yTb)
        assert DNS == 4
        wconv_step = make_wconv(P, l)
        PT = [P.sb([128, 512], BF16, "PT") for _ in range(2)]
        X = [P.sb([128, 1280], F32, "X") for _ in range(2)]
        ff = [P.sb([128, 256], F32, "ff") for _ in range(2)]
        lf = [P.sb([128, 256], F32, "lf") for _ in range(2)]
        kk = [P.sb([128, 256], F32, "kk") for _ in range(2)]
        qs = [P.sb([128, 256], F32, "qs") for _ in range(2)]
        ee = [P.sb([128, 512], F32, "ee") for _ in range(2)]
        ek = [P.sb([128, 256], F32, "ek") for _ in range(2)]
        qk = [P.sb([128, 512], BF16, "qk") for _ in range(2)]
        KTs = [P.sb([128, 2, 128], BF16, "KTs") for _ in range(2)]
        QM = [[P.sb([128, 2, 5, 128], BF16, "QM") for _ in range(2)] for _ in range(2)]
        KH = [P.sb([128, DNS, 256], BF16, "KH") for _ in range(2)]
        Vb = [P.sb([128, 4, 64], BF16, "Vb") for _ in range(2)]
        gt = [P.sb([128, 256], F32, "gt") for _ in range(2)]
        tot = [P.sb([128, 256], F32, "tot") for _ in range(2)]
        red = [P.sb([128, 2, 256], F32, "red") for _ in range(2)]
        Gc = [P.sb([128, 2, DNS], F32, "Gc") for _ in range(2)]
        Sf = [[P.sb([128, DNS, 64], F32, "Sf") for _ in range(2)] for _ in range(2)]
        Sb = [[P.sb([128, DNS, 64], BF16, "Sb") for _ in range(2)] for _ in range(2)]
        qmask = C("qmask").rearrange("p (x s n) -> p x s n", x=2, s=5)
        it = 0
        for dr in range(2):
            blk = C("blkF") if dr == 0 else C("blkB")
            rem = C("aftF") if dr == 0 else C("befB")
            msk4 = C("mblkF4") if dr == 0 else C("mblkB4")
            zoff = 256 if dr == 0 else 512
            subs = list(range(DNS)) if dr == 0 else list(range(DNS - 1, -1, -1))
            for c in range(2):
                mk.op("pool", "memset", Sf[it % 2][c][:, 0, :], 0.0, W=[f"dSf{it % 2}{c}"])
                mk.op("pool", "memset", Sb[it % 2][c][:, 0, :], 0.0, W=[f"dSb{it % 2}{c}"])
            def body(t, full, it):
                i = it % 2
                mk.dma("sp", X[i][:], TM[t * 128:(t + 1) * 128, 1024:2304], R=["TM"], W=[f"dX{i}"])
                wconv_step()
                mk.op("act", "activation", out=ff[i][:], in_=X[i][:, zoff:zoff + 256], func=AF.Sigmoid, R=[f"dX{i}"],
                      W=[f"dff{i}"])
                if use_lb:
                    mk.op("dve", "tensor_tensor", out=ff[i][:], in0=ff[i][:], in1=OML[:], op=ALU.mult, R=[f"dff{i}", "OML"],
                          W=[f"dff{i}"])
                    mk.op("dve", "tensor_tensor", out=ff[i][:], in0=ff[i][:], in1=LB[:], op=ALU.add, R=[f"dff{i}", "LB"],
                          W=[f"dff{i}"])
                mk.op("act", "activation", out=lf[i][:], in_=ff[i][:], func=AF.Ln, R=[f"dff{i}"], W=[f"dlf{i}"])
                mk.op("pool", "tensor_scalar", out=kk[i][:], in0=ff[i][:], scalar1=-1.0, scalar2=1.0, op0=ALU.mult,
                      op1=ALU.add, R=[f"dff{i}"], W=[f"dkk{i}"])
                if full:
                    mk.op("pe", "matmul", PS[5][:, 0:256], lhsT=blk, rhs=lf[i][:], start=True, stop=True,
                          R=["cst", f"dlf{i}"], W=["ps5"])
                mk.op("pe", "matmul", PS[5][:, 256:512], lhsT=rem, rhs=lf[i][:], start=True, stop=True,
                      R=["cst", f"dlf{i}"], W=["ps5"])
                for c in range(2):
                    mk.op("pe", "matmul", PS[1][:, 384 + c * DNS:384 + (c + 1) * DNS], lhsT=lf[i][:, c * 128:(c + 1) * 128],
                          rhs=C("subm"), start=True, stop=True, R=["cst", f"dlf{i}"], W=["ps1g"])
                mk.op("act", "activation", out=Gc[i][:].rearrange("p c s -> p (c s)"), in_=PS[1][:, 384:384 + 2 * DNS],
                      func=AF.Exp, R=["ps1g"], W=[f"dGc{i}"])
                if full:
                    mk.op("act", "activation", out=ee[i][:], in_=PS[5][:, :], func=AF.Exp, R=["ps5"], W=[f"dee{i}"])
                    mk.op("act", "activation", out=ek[i][:], in_=PS[5][:, 0:256], func=AF.Exp, scale=-1.0, R=["ps5"],
                          W=[f"dek{i}"])
                    mk.op("act", "activation", out=qs[i][:], in_=X[i][:, 0:256], func=AF.Silu, R=[f"dX{i}"],
                          W=[f"dqs{i}"])
                    mk.op("dve", "tensor_tensor", out=qk[i][:, 0:256], in0=qs[i][:], in1=ee[i][:, 0:256], op=ALU.mult,
                          R=[f"dqs{i}", f"dee{i}"], W=[f"dqk{i}a"])
                    mk.op("dve", "tensor_tensor", out=qk[i][:, 256:512], in0=kk[i][:], in1=ek[i][:], op=ALU.mult,
                          R=[f"dkk{i}", f"dek{i}"], W=[f"dqk{i}c"])
                else:
                    mk.op("act", "activation", out=ee[i][:, 256:512], in_=PS[5][:, 256:512], func=AF.Exp, R=["ps5"],
                          W=[f"dee{i}"])
                for s_ in range(DNS):
                    mk.op("dve", "scalar_tensor_tensor", out=KH[i][:, s_, :], in0=kk[i][:], scalar=C("subm")[:, s_:s_ + 1],
                          in1=ee[i][:, 256:512], op0=ALU.mult, op1=ALU.mult, R=[f"dkk{i}", f"dee{i}", "cst"],
                          W=[f"dKH{i}s{s_}"])
                mk.op("pool", "tensor_copy", out=Vb[i][:], in_=X[i][:, 768:1024].rearrange("p (h e) -> p h e", h=4),
                      R=[f"dX{i}"], W=[f"dVb{i}"])
                if full:
                    for j in range(4):
                        mk.op("pe", "transpose", out=PQ[0][:, (i * 4 + j) * 128:(i * 4 + j + 1) * 128],
                              in_=qk[i][:, j * 128:(j + 1) * 128], identity=identb[:], R=[f"dqk{i}a", f"dqk{i}c", "identb"],
                              W=[f"pq0d{i}"])
                    mk.op("act", "activation", out=KTs[i][:].rearrange("p c n -> p (c n)"),
                          in_=PQ[0][:, i * 512 + 256:i * 512 + 512], func=AF.Copy, R=[f"pq0d{i}"], W=[f"dKT{i}"])
                    for c in range(2):
                        mk.op("dve", "tensor_tensor", out=QM[i][c][:].rearrange("p x s n -> p (x s) n"),
                              in0=PQ[0][:, i * 512 + c * 128:i * 512 + (c + 1) * 128].unsqueeze(1).to_broadcast([128, 10, 128]),
                              in1=qmask.rearrange("p x s n -> p (x s) n"), op=ALU.mult, R=[f"pq0d{i}", "cst"],
                              W=[f"dQM{i}{c}"])
                yield
                if full:
                    for h in range(4):
                        c, hh = h // 2, h % 2
                        mk.op("pe", "matmul", PS[0][:, h * 128:(h + 1) * 128], lhsT=KTs[i][:, c, :], rhs=QM[i][c][:, hh, 4, :],
                              start=True, stop=True, R=[f"dKT{i}", f"dQM{i}{c}"], W=["ps0"])
                    mk.op("dve", "tensor_tensor", out=PT[i][:], in0=PS[0][:, :], in1=msk4, op=ALU.mult, R=["ps0", "cst"],
                          W=[f"dPT{i}"])
                    for h in range(4):
                        mk.op("pe", "matmul", PS[1][:, h * 64:(h + 1) * 64], lhsT=PT[i][:, h * 128:(h + 1) * 128],
                              rhs=Vb[i][:, h, :], start=True, stop=True, R=[f"dPT{i}", f"dVb{i}"], W=["ps1i"])
                par = it % 2
                KV4 = PS[4][:, :].rearrange("p (k c e) -> p k c e", k=DNS, c=2)
                for k_, s_ in enumerate(subs):
                    for h in range(4):
                        c, hh = h // 2, h % 2
                        mk.op("pe", "matmul", KV4[hh * 64:(hh + 1) * 64, k_, c, :], lhsT=KH[i][:, s_, h * 64:(h + 1) * 64],
                              rhs=Vb[i][:, h, :], start=True, stop=True, R=[f"dKH{i}s{s_}", f"dVb{i}"], W=["ps4kv"])
                for k_, s_ in enumerate(subs):
                    for c in range(2):
                        src = Sf[par][c][:, k_, :]
                        if k_ < DNS - 1:
                            dst, dk, db, dbk = Sf[par][c][:, k_ + 1, :], f"dSf{par}{c}", Sb[par][c][:, k_ + 1, :], f"dSb{par}{c}"
                        else:
                            dst, dk, db, dbk = (Sf[1 - par][c][:, 0, :], f"dSf{1 - par}{c}", Sb[1 - par][c][:, 0, :],
                                                f"dSb{1 - par}{c}")
                        mk.op("dve", "scalar_tensor_tensor", out=dst, in0=src, scalar=Gc[i][:, c, s_:s_ + 1],
                              in1=KV4[:, k_, c, :], op0=ALU.mult, op1=ALU.add,
                              R=[f"dSf{par}{c}", "ps4kv", f"dGc{i}"], W=[dk])
                        mk.op("act", "activation", out=db, in_=dst, func=AF.Copy, R=[dk], W=[dbk])
                for k_, s_ in enumerate(subs):
                    bank = PS[2 + s_ // 2]
                    for h in (range(4) if full else ()):
                        c, hh = h // 2, h % 2
                        col = ((s_ % 2) * 4 + h) * 64
                        mk.op("pe", "matmul", bank[:, col:col + 64], lhsT=QM[i][c][:, hh, s_, :], rhs=Sb[par][c][:, k_, :],
                              start=True, stop=True, R=[f"dQM{i}{c}", f"dSb{par}{c}"], W=[f"ps{2 + s_ // 2}"])
                if not full:
                    return
                for b_ in range(2):
                    mk.op("dve", "tensor_reduce", out=red[i][:, b_, :],
                          in_=PS[2 + b_][:, :].rearrange("p (s x) -> p x s", s=2), axis=AX.X, op=ALU.add,
                          R=[f"ps{2 + b_}"], W=[f"dred{i}{b_}"])
                mk.op("dve", "tensor_tensor", out=tot[i][:], in0=PS[1][:, 0:256], in1=red[i][:, 0, :], op=ALU.add,
                      R=["ps1i", f"dred{i}0"], W=[f"dtot{i}"])
                if dr == 0:
                    mk.op("pool", "tensor_tensor", out=OF[:, t, :], in0=tot[i][:], in1=red[i][:, 1, :], op=ALU.add,
                          R=[f"dtot{i}", f"dred{i}1"], W=[f"dOF{t}"])
                else:
                    mk.op("pool", "tensor_tensor", out=tot[i][:], in0=tot[i][:], in1=red[i][:, 1, :], op=ALU.add,
                          R=[f"dtot{i}", f"dred{i}1"], W=[f"dtot{i}"])
                    mk.op("dve", "tensor_tensor", out=tot[i][:], in0=tot[i][:], in1=OF[:, t, :], op=ALU.add,
                          R=[f"dtot{i}", f"dOF{t}"], W=[f"dtot{i}"])
                    mk.op("act", "activation", out=gt[i][:], in_=X[i][:, 1024:1280], func=AF.Silu, R=[f"dX{i}"],
                          W=[f"dgt{i}"])
                    fin(tot[i][:], f"dtot{i}", False, gt[i][:], f"dgt{i}", t)
            gens = []
            for t, full in plan(dr, last):
                gens.append(body(t, full, it))
                it += 1
            run_pipelined(gens, pipelined=not last)
        while wconv_step():
            pass
        for c in range(2):
            mk.dma("pool", YT[3, c], yTb[:, c, :], R=["yTb"], W=[f"YT3{c}"])
        mk.barrier()
        P.close()

    NSMAX = (T + 4 * (SLOT - 1)) // SLOT
    H2U = dscr("H2U", [T, D], BF16)
    HS = dscr("HS", [NSMAX * SLOT, D], BF16)
    GWS = dscr("GWS", [NSMAX * SLOT, 4])
    OS = dscr("OS", [NSMAX * SLOT, D])
    WALL = dscr("WALL", [16, 128, 3 * 4096], BF16)

    def merge_moe_phase(l, last, tiles=None):
        if tiles is None:
            tiles = list(OUT_T) if last else list(range(NT))
        PO = Pool(nc, f"mo{l}")
        GOH = PO.sb([128, NT, 4], F32, "GOH")
        GW = PO.sb([128, NT, 4], F32, "GW")
        merge_part(l, tiles, GOH, GW)
        if DBG.get("moe", True):
            moe_sparse(l, tiles, GOH, GW)
        PO.close()

    def merge_part(l, tiles, GOH, GW):
        P = Pool(nc, f"mm{l}")
        wbr = P.sb([128, 8, D], BF16, "wbr")
        wo = P.sb([128, 8, D], BF16, "wo")
        wstage = P.sb([128, 8, 512], F32, "wstage")
        for half in range(2):
            mk.dma("sp", wstage[:], w_branch[l].rearrange("n (c p) f -> p (n c) f", p=128)[:, :, half * 512:(half + 1) * 512],
                   W=["wstage"])
            mk.ev(wbr[:, :, half * 512:(half + 1) * 512], wstage[:], R=["wstage"], W=["wbr"])
        for half in range(2):
            mk.dma("sp", wstage[:], w_out[l].rearrange("(c p) f -> p c f", p=128)[:, :, half * 512:(half + 1) * 512],
                   W=["wstage"])
            mk.ev(wo[:, :, half * 512:(half + 1) * 512], wstage[:], R=["wstage"], W=["wo"])
        wgr = P.sb([128, 8, 20], F32, "wgr")
        bgr = P.sb([1, 20], F32, "bgr")
        mk.dma("sp", wgr[:], moe_wgr[l].rearrange("(c p) f -> p c f", p=128), W=["wgr"])
        mk.dma("sp", bgr[:], moe_bgr[l], W=["bgr"])
        identf = C("ident")
        ones = C("ones")
        norm = make_norm(P, 2)
        xnew = [P.sb([128, D], F32, "xnew") for _ in range(2)]
        h2Tt = [P.sb([128, 8, 128], BF16, "h2Tt") for _ in range(2)]
        h2tok = [P.sb([128, D], BF16, "h2tok") for _ in range(2)]
        yt = [P.sb([128, 8, 128], BF16, "yt") for _ in range(2)]
        mg = [P.sb([128, 4096], BF16, "mg") for _ in range(2)]
        xt = [P.sb([128, D], F32, "xt") for _ in range(2)]
        zz = [P.sb([128, D], F32, "zz") for _ in range(2)]
        zt = [P.sb([128, D], F32, "zt") for _ in range(2)]
        zb = [P.sb([128, D], BF16, "zb") for _ in range(2)]
        zT = [P.sb([128, 8, 128], BF16, "zT") for _ in range(2)]
        rt = {"gs": P.sb([128, 1], F32, "rtgs")}
        LG = P.sb([128, NT, 20], F32, "LG")
        nit = 0
        def tile_body(t, i):
                j = cond_of(t)
                cols = slice(t * 128, (t + 1) * 128)
                h2f = zz[i]
                h2fT = zt[i][:, :].rearrange("p (c n) -> p c n", c=8)
                mk.dma("sp", yt[i][:], YT[:, :, :, cols].rearrange("n c p t -> p (n c) t"),
                       R=[f"YT{n}{c}" for n in range(4) for c in range(2)], W=[f"yt{i}"])
                mk.dma("sp", mg[i][:], MG[cols, :], R=["MG"], W=[f"mg{i}"])
                mk.dma("sp", xt[i][:], XR[cols, :], R=[f"XR{t}"], W=[f"mxt{i}"])
                for n in range(4):
                    for cb in range(2):
                        pb = (n * 2 + cb) % 2
                        for c in range(2):
                            mk.op("pe", "matmul", PS[pb][:, :], lhsT=yt[i][:, n * 2 + c, :],
                                  rhs=wbr[:, n * 2 + c, cb * 512:(cb + 1) * 512], start=(c == 0), stop=(c == 1),
                                  R=[f"yt{i}", "wbr"], W=[f"ps{pb}"])
                        dst = zz[i] if n == 0 else zt[i]
                        dkey = f"zz{i}" if n == 0 else f"zt{i}"
                        mk.op("dve", "tensor_tensor", out=dst[:, cb * 512:(cb + 1) * 512], in0=PS[pb][:, :],
                              in1=mg[i][:, n * 1024 + cb * 512:n * 1024 + (cb + 1) * 512], op=ALU.mult,
                              R=[f"ps{pb}", f"mg{i}"], W=[dkey])
                        if n > 0:
                            mk.op("pool", "tensor_tensor", out=zz[i][:, cb * 512:(cb + 1) * 512],
                                  in0=zz[i][:, cb * 512:(cb + 1) * 512], in1=zt[i][:, cb * 512:(cb + 1) * 512], op=ALU.add,
                                  R=[f"zz{i}", f"zt{i}"], W=[f"zz{i}"])
                mk.op("act", "activation", out=zb[i][:], in_=zz[i][:], func=AF.Copy, R=[f"zz{i}"], W=[f"zb{i}"])
                for c in range(8):
                    mk.op("pe", "transpose", out=PQ[1][:, c * 128:(c + 1) * 128], in_=zb[i][:, c * 128:(c + 1) * 128],
                          identity=identb[:], R=[f"zb{i}", "identb"], W=["pq1"])
                mk.ev(zT[i][:].rearrange("p c n -> p (c n)"), PQ[1][:, :], R=["pq1"], W=[f"zT{i}"])
                for cb in range(2):
                    pb = 2 + cb
                    for c in range(8):
                        mk.op("pe", "matmul", PS[pb][:, :], lhsT=zT[i][:, c, :], rhs=wo[:, c, cb * 512:(cb + 1) * 512],
                              start=(c == 0), stop=(c == 7), R=[f"zT{i}", "wo"], W=[f"ps{pb}"])
                    mk.op("dve", "tensor_tensor", out=zt[i][:, cb * 512:(cb + 1) * 512], in0=PS[pb][:, :],
                          in1=GB[:, j, 0, cb * 512:(cb + 1) * 512], op=ALU.mult, R=[f"ps{pb}", "GB"], W=[f"zt{i}"])
                    mk.op("pool", "tensor_tensor", out=xnew[i][:, cb * 512:(cb + 1) * 512], in0=zt[i][:, cb * 512:(cb + 1) * 512],
                          in1=xt[i][:, cb * 512:(cb + 1) * 512], op=ALU.add, R=[f"zt{i}", f"mxt{i}"], W=[f"xnew{i}"])
                mk.dma("pool", XR[cols, :], xnew[i][:], R=[f"xnew{i}"], W=[f"XR{t}"])
                yield
                norm(xnew[i][:], f"xnew{i}", j, 3, 2, h2Tt[i][:], f"h2Tt{i}")
                for c in range(8):
                    mk.op("pe", "transpose", out=PQ[1][:, c * 128:(c + 1) * 128], in_=h2Tt[i][:, c, :], identity=identb[:],
                          R=[f"h2Tt{i}", "identb"], W=["pq1"])
                mk.ev(h2tok[i][:], PQ[1][:, :], R=["pq1"], W=[f"h2tok{i}"])
                mk.dma("pool", H2U[cols, :], h2tok[i][:], R=[f"h2tok{i}"], W=[f"H2U{t}"])
                ssr = rt["gs"]
                mk.op("act", "activation", out=h2f[:], in_=xnew[i][:], func=AF.Square, accum_out=ssr[:],
                      R=[f"xnew{i}"], W=[f"zz{i}", "rgs"])
                mk.op("act", "activation", out=ssr[:], in_=ssr[:], func=AF.Sqrt, scale=1.0 / D, bias=EPS, R=["rgs"], W=["rgs"])
                mk.op("dve", "reciprocal", out=ssr[:], in_=ssr[:], R=["rgs"], W=["rgs"])
                mk.op("dve", "tensor_scalar", out=h2f[:], in0=xnew[i][:], scalar1=ssr[:, 0:1], scalar2=None,
                      op0=ALU.mult, R=[f"xnew{i}", "rgs", f"zz{i}"], W=[f"zz{i}"])
                for half in range(2):
                    for c4 in range(4):
                        c = half * 4 + c4
                        mk.op("pe", "transpose", out=PS[5][:, c4 * 128:(c4 + 1) * 128], in_=h2f[:, c * 128:(c + 1) * 128],
                              identity=identf, R=[f"zz{i}", "cst"], W=["ps5"])
                    mk.op("dve", "tensor_tensor", out=h2fT[:, half * 4:(half + 1) * 4, :],
                          in0=PS[5][:, :].rearrange("p (c n) -> p c n", c=4),
                          in1=MODC[:, 3, half * 4:(half + 1) * 4, j:j + 1].to_broadcast([128, 4, 128]), op=ALU.mult,
                          R=["ps5", "MODC"], W=[f"zt{i}"])
                    mk.op("pool", "tensor_tensor", out=h2fT[:, half * 4:(half + 1) * 4, :],
                          in0=h2fT[:, half * 4:(half + 1) * 4, :],
                          in1=MODC[:, 2, half * 4:(half + 1) * 4, j:j + 1].to_broadcast([128, 4, 128]), op=ALU.add,
                          R=[f"zt{i}", "MODC"], W=[f"zt{i}"])
                for c in range(8):
                    mk.op("pe", "matmul", PS[4][:, 0:20], lhsT=h2fT[:, c, :], rhs=wgr[:, c, :], start=(c == 0), stop=False,
                          R=[f"zt{i}", "wgr"], W=["ps4r"])
                mk.op("pe", "matmul", PS[4][:, 0:20], lhsT=ones[0:1, :], rhs=bgr[0:1, :], start=False, stop=True,
                      R=["cst", "bgr"], W=["ps4r"])
                mk.op("act", "activation", out=LG[:, t, :], in_=PS[4][:, 0:20], func=AF.Copy, R=["ps4r"], W=[f"LG{t}"])
        gens = []
        for t in tiles:
            gens.append(tile_body(t, nit % 2))
            nit += 1
        run_pipelined(gens)
        nt, t0 = len(tiles), tiles[0]
        lk = [f"LG{t}" for t in tiles]
        L3 = LG[:, t0:t0 + nt, :]
        gohv = GOH[:, t0:t0 + nt, :]
        gwv = GW[:, t0:t0 + nt, :]
        r1 = {k: P.sb([128, nt], F32, "r1" + k) for k in ("gm", "gs", "m1", "m2", "w1", "w2")}
        r4 = {k: P.sb([128, nt, 4], F32, "r4" + k) for k in ("ge", "el", "tmp", "oh1", "e2", "oh2")}

        def bc(a):
            return a[:].unsqueeze(2).to_broadcast([128, nt, 4])
        mk.op("dve", "tensor_reduce", out=r1["gm"][:], in_=L3[:, :, 0:4], axis=AX.X, op=ALU.max, R=lk, W=["rgm"])
        mk.op("dve", "tensor_tensor", out=gohv, in0=L3[:, :, 0:4], in1=bc(r1["gm"]), op=ALU.is_ge, R=lk + ["rgm"], W=["GOHall"])
        mk.op("dve", "tensor_tensor", out=r4["ge"][:], in0=L3[:, :, 0:4], in1=bc(r1["gm"]), op=ALU.subtract, R=lk + ["rgm"],
              W=["rge"])
        mk.op("act", "activation", out=r4["ge"][:], in_=r4["ge"][:], func=AF.Exp, R=["rge"], W=["rge"])
        mk.op("dve", "tensor_reduce", out=r1["gs"][:], in_=r4["ge"][:], axis=AX.X, op=ALU.add, R=["rge"], W=["rgs2"])
        mk.op("dve", "reciprocal", out=r1["gs"][:], in_=r1["gs"][:], R=["rgs2"], W=["rgs2"])
        mk.op("dve", "tensor_tensor", out=r4["el"][:], in0=L3[:, :, 4:8], in1=gohv[:, :, 0:1].to_broadcast([128, nt, 4]),
              op=ALU.mult, R=lk + ["GOHall"], W=["rel"])
        for g in range(1, 4):
            mk.op("dve", "tensor_tensor", out=r4["tmp"][:], in0=L3[:, :, 4 + 4 * g:8 + 4 * g],
                  in1=gohv[:, :, g:g + 1].to_broadcast([128, nt, 4]), op=ALU.mult, R=lk + ["GOHall"], W=["rtmp"])
            mk.op("dve", "tensor_tensor", out=r4["el"][:], in0=r4["el"][:], in1=r4["tmp"][:], op=ALU.add, R=["rel", "rtmp"],
                  W=["rel"])
        mk.op("dve", "tensor_reduce", out=r1["m1"][:], in_=r4["el"][:], axis=AX.X, op=ALU.max, R=["rel"], W=["rm1"])
        mk.op("dve", "tensor_tensor", out=r4["oh1"][:], in0=r4["el"][:], in1=bc(r1["m1"]), op=ALU.is_ge, R=["rel", "rm1"],
              W=["roh1"])
        mk.op("dve", "scalar_tensor_tensor", out=r4["e2"][:], in0=r4["oh1"][:], scalar=-1e30, in1=r4["el"][:], op0=ALU.mult,
              op1=ALU.add, R=["roh1", "rel"], W=["re2"])
        mk.op("dve", "tensor_reduce", out=r1["m2"][:], in_=r4["e2"][:], axis=AX.X, op=ALU.max, R=["re2"], W=["rm2"])
        mk.op("dve", "tensor_tensor", out=r4["oh2"][:], in0=r4["e2"][:], in1=bc(r1["m2"]), op=ALU.is_ge, R=["re2", "rm2"],
              W=["roh2"])
        mk.op("dve", "tensor_tensor", out=r1["w1"][:], in0=r1["m2"][:], in1=r1["m1"][:], op=ALU.subtract, R=["rm1", "rm2"],
              W=["rw1"])
        mk.op("act", "activation", out=r1["w1"][:], in_=r1["w1"][:], func=AF.Exp, R=["rw1"], W=["rw1"])
        mk.op("dve", "tensor_scalar_add", out=r1["w1"][:], in0=r1["w1"][:], scalar1=1.0, R=["rw1"], W=["rw1"])
        mk.op("dve", "reciprocal", out=r1["w1"][:], in_=r1["w1"][:], R=["rw1"], W=["rw1"])
        mk.op("dve", "tensor_tensor", out=r1["w1"][:], in0=r1["w1"][:], in1=r1["gs"][:], op=ALU.mult, R=["rw1", "rgs2"],
              W=["rw1"])
        mk.op("dve", "tensor_tensor", out=r1["w2"][:], in0=r1["gs"][:], in1=r1["w1"][:], op=ALU.subtract, R=["rw1", "rgs2"],
              W=["rw2"])
        mk.op("dve", "tensor_tensor", out=gwv, in0=r4["oh1"][:], in1=bc(r1["w1"]), op=ALU.mult, R=["roh1", "rw1"],
              W=["GWall"])
        mk.op("dve", "tensor_tensor", out=r4["tmp"][:], in0=r4["oh2"][:], in1=bc(r1["w2"]), op=ALU.mult, R=["roh2", "rw2"],
              W=["rtmp"])
        mk.op("dve", "tensor_tensor", out=gwv, in0=gwv, in1=r4["tmp"][:], op=ALU.add, R=["GWall", "rtmp"], W=["GWall"])
        mk.barrier()
        P.close()

    def moe_sparse(l, tiles, GOH, GW):
        P = Pool(nc, f"ms{l}")
        I32 = mybir.dt.int32
        nt, t0 = len(tiles), tiles[0]
        N = nt * 128
        NS = (N + 4 * (SLOT - 1)) // SLOT
        TPS = SLOT // 128
        Gv = GOH[:, t0:t0 + nt, :]
        gkeys = ["GOHall"]
        cnt = P.sb([128, nt, 4], F32, "cnt")
        inc = P.sb([128, nt, 4], F32, "inc")
        A = P.sb([128, nt, 4], F32, "A")
        tot = P.sb([128, 4], F32, "tot")
        cmp = P.sb([128, 16], F32, "cmp")
        nsl = P.sb([128, 4], F32, "nsl")
        base = P.sb([128, 4], F32, "base")
        posf = P.sb([128, nt], F32, "posf")
        POSI = P.sb([128, nt], I32, "POSI")
        gkb = P.sb([128, NS], F32, "gkb")
        gtmp = P.sb([128, NS], F32, "gtmp")
        widf = P.sb([128, NS, 4], F32, "widf")
        WIDX = P.sb([128, NS, 4], I32, "WIDX")
        Gf = Gv.rearrange("p t g -> p (t g)")
        mk.op("pe", "matmul", PS[0][:, 0:nt * 4], lhsT=C("ltS"), rhs=Gf, start=True, stop=True, R=gkeys + ["cst"], W=["ps0"])
        mk.op("pe", "matmul", PS[1][:, 0:nt * 4], lhsT=C("ones"), rhs=Gf, start=True, stop=True, R=gkeys + ["cst"], W=["ps1"])
        mk.op("act", "activation", out=cnt[:].rearrange("p t g -> p (t g)"), in_=PS[1][:, 0:nt * 4], func=AF.Copy,
              R=["ps1"], W=["scnt"])
        for g in range(4):
            mk.op("dve", "tensor_tensor_scan", out=inc[:, :, g], data0=C("ones")[:, 0:nt], data1=cnt[:, :, g], initial=0.0,
                  op0=ALU.mult, op1=ALU.add, R=["scnt", "cst"], W=[f"sinc{g}"])
        ik = [f"sinc{g}" for g in range(4)]
        mk.op("dve", "tensor_copy", out=tot[:], in_=inc[:, nt - 1, :], R=ik, W=["stot"])
        for g in range(4):
            mk.op("dve", "tensor_scalar", out=cmp[:], in0=C("thrS"), scalar1=tot[:, g:g + 1], scalar2=None, op0=ALU.is_lt,
                  R=["stot", "cst"], W=["scmp"])
            mk.op("dve", "tensor_reduce", out=nsl[:, g:g + 1], in_=cmp[:], axis=AX.X, op=ALU.add, R=["scmp"], W=["snsl"])
        mk.op("dve", "tensor_scalar", out=nsl[:], in0=nsl[:], scalar1=float(SLOT), scalar2=None, op0=ALU.mult,
              R=["snsl"], W=["snsl"])
        mk.op("pool", "memset", base[:], 0.0, W=["sbase"])
        for g in range(1, 4):
            mk.op("dve", "tensor_tensor", out=base[:, g:g + 1], in0=base[:, g - 1:g], in1=nsl[:, g - 1:g], op=ALU.add,
                  R=["sbase", "snsl"], W=["sbase"])
        mk.op("dve", "tensor_tensor", out=A[:], in0=inc[:], in1=cnt[:], op=ALU.subtract, R=ik + ["scnt"], W=["sA"])
        mk.op("dve", "tensor_tensor", out=A[:].rearrange("p t g -> p (t g)"), in0=A[:].rearrange("p t g -> p (t g)"),
              in1=PS[0][:, 0:nt * 4], op=ALU.add, R=["sA", "ps0"], W=["sA"])
        mk.op("dve", "tensor_tensor", out=A[:], in0=A[:], in1=base[:].unsqueeze(1).to_broadcast([128, nt, 4]), op=ALU.add,
              R=["sA", "sbase"], W=["sA"])
        mk.op("dve", "tensor_tensor", out=A[:], in0=A[:], in1=Gv, op=ALU.mult, R=["sA"] + gkeys, W=["sA"])
        mk.op("dve", "tensor_reduce", out=posf[:], in_=A[:], axis=AX.X, op=ALU.add, R=["sA"], W=["sposf"])
        mk.op("dve", "tensor_copy", out=POSI[:], in_=posf[:], R=["sposf"], W=["POSI"])
        mk.op("pool", "memset", gkb[:], 0.0, W=["sgkb"])
        for g in range(1, 4):
            mk.op("dve", "tensor_scalar", out=gtmp[:], in0=C("kS")[:, 0:NS], scalar1=base[:, g:g + 1], scalar2=None,
                  op0=ALU.is_ge, R=["sbase", "cst"], W=["sgtmp"])
            mk.op("dve", "tensor_tensor", out=gkb[:], in0=gkb[:], in1=gtmp[:], op=ALU.add, R=["sgkb", "sgtmp"], W=["sgkb"])
        mk.op("dve", "tensor_scalar", out=gkb[:], in0=gkb[:], scalar1=512.0, scalar2=None, op0=ALU.mult, R=["sgkb"],
              W=["sgkb"])
        for j in range(4):
            mk.op("dve", "tensor_scalar", out=widf[:, :, j], in0=gkb[:], scalar1=C("jp")[:, j:j + 1], scalar2=None,
                  op0=ALU.add, R=["sgkb", "cst"], W=["swidf"])
        mk.op("dve", "tensor_copy", out=WIDX[:], in_=widf[:], R=["swidf"], W=["WIDX"])
        hb = [P.sb([128, D], BF16, "hb") for _ in range(2)]
        for ti, t in enumerate(tiles):
            i = ti % 2
            mk.dma("sp", hb[i][:], H2U[t * 128:(t + 1) * 128, :], R=[f"H2U{t}"], W=[f"hb{i}"])
            mk.idma(HS[:, :], bass.IndirectOffsetOnAxis(ap=POSI[:, ti:ti + 1], axis=0), hb[i][:, :], None,
                    R=[f"hb{i}", "POSI"], W=[f"HSs{ti}"])
            mk.idma(GWS[:, :], bass.IndirectOffsetOnAxis(ap=POSI[:, ti:ti + 1], axis=0), GW[:, t, :], None,
                    R=["GWall", "POSI"], W=[f"GWs{ti}"])
        hsk = [f"HSs{ti}" for ti in range(nt)]
        gwk = [f"GWs{ti}" for ti in range(nt)]
        hs = [P.sb([128, TPS, D], BF16, "hs") for _ in range(2)]
        hTs = [P.sb([128, 8, SLOT], BF16, "hTs") for _ in range(2)]
        gws = [P.sb([128, TPS, 4], F32, "gws") for _ in range(2)]
        acc = [P.sb([128, TPS, D], F32, "acc") for _ in range(2)]
        wall = [P.sb([128, 3 * 4096], BF16, "wall") for _ in range(2)]
        w1b = [w[:, 0:4096].rearrange("p (c f) -> p c f", c=8) for w in wall]
        w3b = [w[:, 4096:8192].rearrange("p (c f) -> p c f", c=8) for w in wall]
        w2b = [w[:, 8192:12288].rearrange("p (c f) -> p c f", c=4) for w in wall]
        sl = [P.sb([128, SLOT], F32, "sl") for _ in range(2)]
        actT = [P.sb([128, 4, SLOT], BF16, "actT") for _ in range(2)]
        Wt = WALL.rearrange("e p f -> (e p) f")
        wkeys = [f"W{a}B{e}" for a in (1, 2, 3) for e in range(16)]
        nw = 0
        for k in range(NS):
            si = k % 2
            mk.dma("sp", hs[si][:], HS[k * SLOT:(k + 1) * SLOT, :].rearrange("(q p) f -> p q f", p=128), R=hsk,
                   W=[f"hs{si}"])
            mk.dma("sp", gws[si][:], GWS[k * SLOT:(k + 1) * SLOT, :].rearrange("(q p) f -> p q f", p=128), R=gwk,
                   W=[f"gws{si}"])
            for q in range(TPS):
                pq = q % 2
                for c in range(8):
                    mk.op("pe", "transpose", out=PQ[pq][:, c * 128:(c + 1) * 128], in_=hs[si][:, q, c * 128:(c + 1) * 128],
                          identity=identb[:], R=[f"hs{si}", "identb"], W=[f"pq{pq}"])
                mk.ev(hTs[si][:, :, q * 128:(q + 1) * 128], PQ[pq][:, :].rearrange("p (c n) -> p c n", c=8), R=[f"pq{pq}"],
                      W=[f"hTs{si}"])
            for j in range(4):
                wi = nw % 2
                nw += 1
                ioff = bass.IndirectOffsetOnAxis(ap=WIDX[:, k, j:j + 1], axis=0)
                mk.idma(wall[wi][:, :], None, Wt, ioff, R=["WIDX"] + wkeys, W=[f"w1b{wi}", f"w3b{wi}", f"w2b{wi}"])
                for fcn in range(4):
                    for kk_ in range(8):
                        mk.op("pe", "matmul", PS[0][:, 0:SLOT], lhsT=w1b[wi][:, kk_, fcn * 128:(fcn + 1) * 128],
                              rhs=hTs[si][:, kk_, :], start=(kk_ == 0), stop=(kk_ == 7), R=[f"w1b{wi}", f"hTs{si}"], W=["ps0"])
                    for kk_ in range(8):
                        mk.op("pe", "matmul", PS[1][:, 0:SLOT], lhsT=w3b[wi][:, kk_, fcn * 128:(fcn + 1) * 128],
                              rhs=hTs[si][:, kk_, :], start=(kk_ == 0), stop=(kk_ == 7), R=[f"w3b{wi}", f"hTs{si}"], W=["ps1"])
                    s2 = fcn % 2
                    mk.op("act", "activation", out=sl[s2][:], in_=PS[0][:, 0:SLOT], func=AF.Silu, R=["ps0"], W=[f"sl{s2}"])
                    mk.op("dve", "tensor_tensor", out=actT[wi][:, fcn, :], in0=sl[s2][:], in1=PS[1][:, 0:SLOT], op=ALU.mult,
                          R=[f"sl{s2}", "ps1"], W=[f"actT{wi}"])
                for q in range(TPS):
                    for cb in range(2):
                        pb = 2 + cb
                        for fcn in range(4):
                            mk.op("pe", "matmul", PS[pb][:, :], lhsT=actT[wi][:, fcn, q * 128:(q + 1) * 128],
                                  rhs=w2b[wi][:, fcn, cb * 512:(cb + 1) * 512], start=(fcn == 0), stop=(fcn == 3),
                                  R=[f"actT{wi}", f"w2b{wi}"], W=[f"ps{pb}"])
                        if j == 0:
                            mk.op("dve", "tensor_scalar", out=acc[si][:, q, cb * 512:(cb + 1) * 512], in0=PS[pb][:, :],
                                  scalar1=gws[si][:, q, j:j + 1], scalar2=None, op0=ALU.mult,
                                  R=[f"ps{pb}", f"gws{si}"], W=[f"sacc{si}"])
                        else:
                            mk.op("dve", "scalar_tensor_tensor", out=acc[si][:, q, cb * 512:(cb + 1) * 512], in0=PS[pb][:, :],
                                  scalar=gws[si][:, q, j:j + 1], in1=acc[si][:, q, cb * 512:(cb + 1) * 512], op0=ALU.mult,
                                  op1=ALU.add, R=[f"ps{pb}", f"gws{si}", f"sacc{si}"], W=[f"sacc{si}"])
            mk.dma("sp", OS[k * SLOT:(k + 1) * SLOT, :].rearrange("(q p) f -> p q f", p=128), acc[si][:], R=[f"sacc{si}"],
                   W=[f"OS{k}"])
        osk = [f"OS{k}" for k in range(NS)]
        og = [P.sb([128, D], F32, "og") for _ in range(2)]
        xt = [P.sb([128, D], F32, "xt") for _ in range(2)]
        for ti, t in enumerate(tiles):
            i = ti % 2
            j = cond_of(t)
            mk.idma(og[i][:, :], None, OS[:, :], bass.IndirectOffsetOnAxis(ap=POSI[:, ti:ti + 1], axis=0), R=osk + ["POSI"],
                    W=[f"og{i}"])
            mk.dma("sp", xt[i][:], XR[t * 128:(t + 1) * 128, :], R=[f"XR{t}"], W=[f"ext{i}"])
            mk.op("pool", "tensor_tensor", out=og[i][:], in0=og[i][:], in1=GB[:, j, 1, :], op=ALU.mult, R=[f"og{i}", "GB"],
                  W=[f"og{i}"])
            mk.op("dve", "tensor_tensor", out=og[i][:], in0=og[i][:], in1=xt[i][:], op=ALU.add, R=[f"og{i}", f"ext{i}"],
                  W=[f"og{i}"])
            mk.dma("sp", XR[t * 128:(t + 1) * 128, :], og[i][:], R=[f"og{i}"], W=[f"XR{t}"])
        mk.barrier()
        P.close()

    def final_phase():
        P = Pool(nc, "fin")
        fw = P.sb([128, D], F32, "fw")
        mk.dma("sp", fw[:], fnw, W=["fw"])
        xt = [P.sb([128, D], F32, "xt") for _ in range(2)]
        ot = [P.sb([128, D], F32, "ot") for _ in range(2)]
        junk = P.sb([128, D], BF16, "junk")
        ss = [P.sb([128, 1], F32, "ss") for _ in range(2)]
        for t in OUT_T:
            i = t % 2
            mk.dma("sp", xt[i][:], XR[t * 128:(t + 1) * 128, :], R=[f"XR{t}"], W=[f"fxt{i}"])
            mk.op("act", "activation", out=junk[:], in_=xt[i][:], func=AF.Square, accum_out=ss[i][:], R=[f"fxt{i}"],
                  W=["fjunk", f"fss{i}"])
            mk.op("act", "activation", out=ss[i][:], in_=ss[i][:], func=AF.Sqrt, scale=1.0 / D, bias=EPS, R=[f"fss{i}"],
                  W=[f"fss{i}"])
            mk.op("dve", "reciprocal", out=ss[i][:], in_=ss[i][:], R=[f"fss{i}"], W=[f"fss{i}"])
            mk.op("dve", "scalar_tensor_tensor", out=ot[i][:], in0=xt[i][:], scalar=ss[i][:, 0:1], in1=fw[:], op0=ALU.mult,
                  op1=ALU.mult, R=[f"fxt{i}", f"fss{i}", "fw"], W=[f"fot{i}"])
            mk.dma("pool", yout[(t - 2) * 128:(t - 1) * 128, :], ot[i][:], R=[f"fot{i}"], W=["yout"])
        mk.barrier()
        P.close()

    stages = dict(mod=mod_phase, inproj=inproj_phase, a=mixer_a, b=mixer_b, c=mixer_c, d=mixer_d)
    return dict(nc=nc, mk=mk, stages=stages, merge=merge_moe_phase, final=final_phase, dbg=dbg,
                scr=dict(XR=XR, FM=FM, TM=TM, MG=MG, YT=YT))


def emit_all(prog, layers=NL, upto=None, skip=()):
    mk = prog["mk"]
    mk.barrier()
    done = False
    for l in range(layers):
        for s in ("mod", "inproj", "a", "b", "c", "d"):
            if s in skip:
                continue
            if s in ("inproj", "b", "c", "d"):
                prog["stages"][s](l, l == NL - 1)
            else:
                prog["stages"][s](l)
            if upto == (l, s):
                done = True
                break
        if done:
            break
        prog["merge"](l, l == NL - 1)
        if upto == (l, "merge"):
            done = True
            break
    if not done:
        prog["final"]()
    mk.barrier(engines=("sp",))


def _consts():
    j = np.arange(128)[:, None]
    i = np.arange(128)[None, :]
    same = (j // DL) == (i // DL)
    m = {}
    m["ident"] = (j == i)
    m["triF"] = (j <= i)
    m["triB"] = (j >= i)
    m["blkF"] = same & (j <= i)
    m["blkB"] = same & (j >= i)
    m["aftF"] = same & (j > i)
    m["befB"] = same & (j < i)
    m["diffF"] = np.maximum(i - j, 0)
    m["diffB"] = np.maximum(j - i, 0)
    m["maskF"] = (i >= j)
    m["maskB"] = (j > i)
    m["posF"] = np.broadcast_to(i + 1, (128, 128))
    m["posB"] = np.broadcast_to(128 - i, (128, 128))
    m["negF4"] = np.tile(np.where(j <= i, 0.0, -30000.0), (1, 4))
    m["negB4"] = np.tile(np.where(j >= i, 0.0, -30000.0), (1, 4))
    m["mblkF4"] = np.tile(same & (j <= i), (1, 4))
    m["mblkB4"] = np.tile(same & (j >= i), (1, 4))
    m["kpos"] = np.concatenate([127 - j, j], 1)
    m["ones"] = np.ones((128, 128))
    sel = np.zeros((128, 256))
    sel[0, 0:128] = 1.0
    sel[1, 128:256] = 1.0
    m["sel"] = sel
    m["subm"] = np.concatenate([(j // DL) == s_ for s_ in range(128 // DL)], 1)
    qm = np.zeros((128, 2, 5, 128), np.float32)
    for hh_ in range(2):
        for s_ in range(5):
            colsel = np.ones(128, bool) if s_ == 4 else (np.arange(128) // DL == s_)
            qm[hh_ * 64:(hh_ + 1) * 64, hh_, s_, :] = colsel[None, :]
    m["qmask"] = qm.reshape(128, 1280)
    m["ltS"] = (j < i)
    m["thrS"] = np.broadcast_to(np.arange(16)[None, :] * SLOT, (128, 16))
    m["kS"] = np.broadcast_to(np.arange(16)[None, :] * SLOT, (128, 16))
    m["jp"] = np.arange(4)[None, :] * 128 + np.arange(128)[:, None]
    out = np.zeros((128, NCST), np.float32)
    for k, (o, w) in CST.items():
        out[:, o:o + w] = np.asarray(m[k], np.float32)
    return out


def _rope_tables(flip=False):
    n = 16
    inv = np.power(np.float32(10000.0), -np.arange(n, dtype=np.float32) / n).astype(np.float32)
    t = np.arange(4096)
    row = (t // 64).astype(np.float32)
    col = (t % 64).astype(np.float32)
    ang = np.concatenate([row[:, None] * inv, col[:, None] * inv], -1)
    cos = np.cos(ang).astype(np.float32).T
    sin = np.sin(ang).astype(np.float32).T
    if flip:
        cos, sin = cos[:, ::-1], sin[:, ::-1]
    Cc = np.ones((128, T), np.float32)
    Ss = np.zeros((128, T), np.float32)
    for hh in range(2):
        Cc[hh * 64:hh * 64 + 32, 256:] = cos
        Cc[hh * 64 + 32:hh * 64 + 64, 256:] = cos
        Ss[hh * 64:hh * 64 + 32, 256:] = -sin
        Ss[hh * 64 + 32:hh * 64 + 64, 256:] = sin
    return Cc, Ss


def prep_shared(inp, flip=False):
    f = np.float32
    w_in = np.asarray(inp["w_in"], f)
    offs = {}
    o = 0
    for name, w in (("a_x", 256), ("a_g", 256), ("b_q", 256), ("b_k", 256), ("b_v", 256), ("b_g", 256), ("c_q", 256),
                    ("c_k", 256), ("c_v", 256), ("c_o", 256), ("c_gates", 16), ("d_q", 256), ("d_ff", 256),
                    ("d_fb", 256), ("d_i", 256), ("d_g", 256), ("merge", 4096)):
        offs[name] = (o, w)
        o += w

    def cols(n):
        a, w = offs[n]
        return w_in[:, :, a:a + w]

    perm = np.concatenate([np.arange(h * 64 + 32, h * 64 + 64).tolist() + np.arange(h * 64, h * 64 + 32).tolist()
                           for h in range(4)]).astype(np.int64)
    w_fm = np.concatenate([cols("a_x"), cols("a_g"), cols("b_q"), cols("b_q")[:, :, perm], cols("b_k"),
                           cols("b_k")[:, :, perm], cols("c_q"), cols("c_k")], -1)
    gperm = np.array([8, 9, 10, 11, 12, 13, 14, 15, 0, 1, 2, 3, 4, 5, 6, 7]) if flip else np.arange(16)
    dfa, dfb = ("d_fb", "d_ff") if flip else ("d_ff", "d_fb")
    w_tm = np.concatenate([cols("b_v"), cols("b_g"), cols("c_v"), cols("c_o"), cols("d_q"), cols(dfa), cols(dfb),
                           cols("d_i"), cols("d_g"), cols("c_gates")[:, :, gperm], cols("merge")], -1)
    sh = {}
    sh["w_mod"] = np.ascontiguousarray(inp["w_mod"], f)
    bm = np.asarray(inp["b_mod"], f)
    sh["bmod_c"] = np.ascontiguousarray(bm.reshape(NL, 48, 128).transpose(0, 2, 1))
    sh["bmod_r"] = np.ascontiguousarray(bm.reshape(NL, 1, 6144))
    sh["w_fm"] = np.ascontiguousarray(w_fm)
    sh["w_tm"] = np.ascontiguousarray(w_tm)
    acw = np.asarray(inp["a_conv_w"], f)
    zt_ = np.zeros_like(acw[:, :1])
    acw = np.concatenate([zt_, acw[:, ::-1]], 1) if flip else np.concatenate([acw, zt_], 1)
    sh["a_cw"] = np.ascontiguousarray(acw.reshape(NL, 5, 2, 128).transpose(0, 3, 2, 1))
    sh["a_cb"] = np.ascontiguousarray(np.asarray(inp["a_conv_b"], f).reshape(NL, 2, 128).transpose(0, 2, 1))
    gw = np.asarray(inp["a_gate_w"], f)
    agw = np.zeros((NL, 128, 2, 2, 2, 128), f)
    for c in range(2):
        for hh in range(2):
            agw[:, hh * 64:(hh + 1) * 64, :, :, c, hh * 64:(hh + 1) * 64] = gw[:, :, :, 2 * c + hh].transpose(0, 3, 1, 2, 4)
    sh["a_gw"] = np.ascontiguousarray(agw[:, :, ::-1]) if flip else agw
    gb = np.asarray(inp["a_gate_b"], f)
    if flip:
        gb = gb[:, ::-1]
    sh["a_gb"] = np.ascontiguousarray(gb.reshape(NL, 2, 2, 2, 128).transpose(0, 4, 1, 2, 3))
    lam = np.asarray(inp["a_lambda"], f)
    if flip:
        lam = lam[:, ::-1]
    sh["a_lam"] = np.ascontiguousarray(lam.reshape(NL, 2, 2, 128).transpose(0, 3, 1, 2))
    th = np.asarray(inp["b_theta"], f)
    if flip:
        th = th[:, ::-1]
    thp = np.zeros((NL, 128, 2, 2), f)
    for c in range(2):
        for hh in range(2):
            thp[:, hh * 64:(hh + 1) * 64, :, c] = th[:, None, :, 2 * c + hh]
    sh["b_thp"] = thp
    sh["b_thh"] = np.ascontiguousarray(np.broadcast_to(th[:, None], (NL, 128, 2, 4)))
    ccw = np.asarray(inp["c_conv_w"], f)
    zt_ = np.zeros_like(ccw[:, :1])
    ccw = np.concatenate([zt_, ccw[:, ::-1]], 1) if flip else np.concatenate([ccw, zt_], 1)
    sh["c_cw"] = np.ascontiguousarray(ccw.reshape(NL, 5, 4, 128).transpose(0, 3, 2, 1))
    sh["c_cb"] = np.ascontiguousarray(np.asarray(inp["c_conv_b"], f).reshape(NL, 4, 128).transpose(0, 2, 1))
    sh["c_gb"] = np.ascontiguousarray(np.broadcast_to(np.asarray(inp["c_gate_b"], f).reshape(NL, 1, 16)[:, :, gperm], (NL, 128, 16)))
    sh["d_lbr"] = np.ascontiguousarray(np.broadcast_to(np.asarray(inp["d_lb"], f)[None], (128, 2, 256)))
    sh["w_branch"] = np.ascontiguousarray(inp["w_branch"], f)
    sh["w_out"] = np.ascontiguousarray(inp["w_out"], f)
    sh["moe_wgr"] = np.ascontiguousarray(np.concatenate([np.asarray(inp["moe_w_group"], f), np.asarray(inp["moe_w_router"], f)], -1))
    sh["moe_bgr"] = np.ascontiguousarray(np.concatenate([np.asarray(inp["moe_b_group"], f), np.asarray(inp["moe_b_router"], f)], -1).reshape(NL, 1, 20))
    sh["moe_w1"] = np.ascontiguousarray(inp["moe_w1"], f)
    sh["moe_w3"] = np.ascontiguousarray(inp["moe_w3"], f)
    sh["moe_w2"] = np.ascontiguousarray(inp["moe_w2"], f)
    sh["fnw"] = np.ascontiguousarray(np.broadcast_to(np.asarray(inp["final_norm_w"], f)[None], (128, D)))
    sh["cst"] = _consts()
    sh["ropeC"], sh["ropeS"] = _rope_tables(flip)
    return sh


def prep_core(inp, b, flip=False):
    f = np.float32
    d = {}
    cx, xx = np.asarray(inp["ctx"][b], f), np.asarray(inp["x"][b], f)
    if flip:
        cx, xx = cx[::-1], xx[::-1]
    d["xin"] = np.ascontiguousarray(np.concatenate([cx, xx], 0))
    cv = np.stack([np.asarray(inp["c_ctx"], f), np.asarray(inp["c"][b], f)], -1)
    d["cvec"] = np.ascontiguousarray(cv.reshape(8, 128, 2).transpose(1, 0, 2))
    return d


_PROG = None


def kernel(**inputs):
    global _PROG
    if _PROG is None:
        _PROG = build_program()
        emit_all(_PROG)
    nc = _PROG["nc"]
    shs = [prep_shared(inputs, False), prep_shared(inputs, True)]
    in_maps = []
    for core in range(8):
        fl = core >= 4
        m = dict(shs[1 if fl else 0])
        m.update(prep_core(inputs, core % 4, fl))
        in_maps.append(m)
    res = run_bass_kernel_spmd(nc, in_maps, core_ids=list(range(8)))
    out = np.empty((4, 4096, D), np.float32)
    for b in range(4):
        out[b, :HALF_OUT] = np.asarray(res.results[b]["yout"], np.float32)[:HALF_OUT]
        out[b, HALF_OUT:] = np.asarray(res.results[b + 4]["yout"], np.float32)[:4096 - HALF_OUT][::-1]
    return out
```

```python
import contextlib
import numpy as np
import ml_dtypes
import concourse.bass as bass
import concourse.mybir as mybir
from concourse.bass_utils import run_bass_kernel_spmd

F32 = mybir.dt.float32
BF16 = mybir.dt.bfloat16
AF = mybir.ActivationFunctionType
ALU = mybir.AluOpType
AX = mybir.AxisListType

T = 4352
NT = 34
D = 1024
EPS = 1e-6
NL = 2
PIPE_LAST = False
SLOT = 512
HALF_OUT = 2048
DL = 32
DNS = 128 // DL
DBG = dict(maxit=None, core=True, fin=True, heads=(0, 1, 2, 3))
TMW = 2320
TM_OFF = dict(b_v=0, b_g=256, c_v=512, c_o=768, d_q=1024, d_ff=1280, d_fb=1536, d_i=1792, d_g=2048, c_gates=2304)
FM_OFF = dict(a_x=0, a_g=2, b_q=4, b_qp=6, b_k=8, b_kp=10, c_q=12, c_k=14)

CST = {}
_off = 0
for _n, _w in (("ident", 128), ("triF", 128), ("triB", 128), ("blkF", 128), ("blkB", 128), ("aftF", 128),
               ("befB", 128), ("diffF", 128), ("diffB", 128), ("maskF", 128), ("maskB", 128), ("posF", 128),
               ("posB", 128), ("negF4", 512), ("negB4", 512), ("mblkF4", 512), ("mblkB4", 512), ("kpos", 2),
               ("ones", 128), ("sel", 256), ("subm", 4), ("qmask", 1280), ("ltS", 128), ("thrS", 16), ("kS", 16), ("jp", 4)):
    CST[_n] = (_off, _w)
    _off += _w
NCST = _off


class MK:
    SEM_ROT = 30000

    def __init__(self, nc, ndma=8):
        self.nc = nc
        self.engs = {"pe": nc.tensor, "act": nc.scalar, "dve": nc.vector, "pool": nc.gpsimd, "sp": nc.sync}
        self._ctxs = []
        self.nsem = 0
        self.sem = {}
        self.cnt = {}
        for e in ("pe", "act", "dve", "pool"):
            self.sem[e] = self._newsem("s_" + e)
            self.cnt[e] = 0
        self.seen = {e: {} for e in self.engs}
        self.dq = {}
        for q in ("sp", "pool"):
            self.dq[q] = {"i": 0, "slots": [[self._newsem(f"d_{q}{i}"), 0] for i in range(ndma)]}
        self.res = {}
        self.ninst = 0
        self.flip = 0

    def _newsem(self, name):
        self.nsem += 1
        cm = self.nc.semaphore(f"{name}_{self.nsem}")
        s = cm.__enter__()
        self._ctxs.append(cm)
        return s

    def _wait(self, eng, tok):
        sem, val = tok
        key = id(sem)
        if self.seen[eng].get(key, 0) >= val:
            return
        self.engs[eng].wait_ge(sem, val)
        self.seen[eng][key] = val

    def _deps(self, R, W):
        deps = []
        for k in R:
            st = self.res.get(k)
            if st and st[0] is not None:
                deps.append(st[0])
        for k in W:
            st = self.res.get(k)
            if st:
                if st[0] is not None:
                    deps.append(st[0])
                deps.extend(st[1])
        return deps

    def _record(self, tok, R, W):
        for k in R:
            st = self.res.setdefault(k, [None, []])
            st[1] = [t for t in st[1] if t[0] is not tok[0]] + [tok]
        for k in W:
            self.res[k] = [tok, []]

    def op(self, eng, method, *args, R=(), W=(), **kw):
        for tok in self._deps(R, W):
            if eng == "pe" and tok[0] is self.sem["pe"]:
                continue
            self._wait(eng, tok)
        ins = getattr(self.engs[eng], method)(*args, **kw)
        if self.cnt[eng] >= self.SEM_ROT:
            self.sem[eng] = self._newsem("s_" + eng)
            self.cnt[eng] = 0
        self.cnt[eng] += 1
        ins.then_inc(self.sem[eng], 1)
        tok = (self.sem[eng], self.cnt[eng])
        self._record(tok, R, W)
        self.ninst += 1
        return tok

    def dma(self, q, out, in_, R=(), W=(), **kw):
        d = self.dq[q]
        slot = d["slots"][d["i"] % len(d["slots"])]
        d["i"] += 1
        if slot[1] > 0:
            self._wait(q, (slot[0], slot[1]))
        if slot[1] >= self.SEM_ROT:
            slot[0] = self._newsem("d_" + q)
            slot[1] = 0
        for tok in self._deps(R, W):
            self._wait(q, tok)
        ins = self.engs[q].dma_start(out=out, in_=in_, **kw)
        slot[1] += 16
        ins.then_inc(slot[0], 16)
        tok = (slot[0], slot[1])
        self._record(tok, R, W)
        self.ninst += 1
        return tok

    def idma(self, out, out_offset, in_, in_offset, R=(), W=()):
        q = "pool"
        d = self.dq[q]
        slot = d["slots"][d["i"] % len(d["slots"])]
        d["i"] += 1
        if slot[1] > 0:
            self._wait(q, (slot[0], slot[1]))
        if slot[1] >= self.SEM_ROT:
            slot[0] = self._newsem("d_" + q)
            slot[1] = 0
        for tok in self._deps(R, W):
            self._wait(q, tok)
        ins = self.nc.gpsimd.indirect_dma_start(out=out, out_offset=out_offset, in_=in_, in_offset=in_offset)
        slot[1] += 16
        ins.then_inc(slot[0], 16)
        tok = (slot[0], slot[1])
        self._record(tok, R, W)
        self.ninst += 1
        return tok

    def barrier(self, engines=("pe", "act", "dve", "pool", "sp")):
        toks = []
        for q, d in self.dq.items():
            for slot in d["slots"]:
                if slot[1] > 0:
                    toks.append((slot[0], slot[1]))
        for e in ("pe", "act", "dve", "pool"):
            if self.cnt[e] > 0:
                toks.append((self.sem[e], self.cnt[e]))
        for e in engines:
            for tok in toks:
                if e in self.sem and tok[0] is self.sem[e]:
                    continue
                self._wait(e, tok)

    def ev(self, out, in_, R=(), W=(), func=None, **kw):
        if func is not None:
            return self.op("act", "activation", out=out, in_=in_, func=func, R=R, W=W, **kw)
        self.flip ^= 1
        if self.flip:
            return self.op("act", "activation", out=out, in_=in_, func=AF.Copy, R=R, W=W)
        return self.op("dve", "tensor_copy", out=out, in_=in_, R=R, W=W)


class Pool:
    def __init__(self, nc, tag):
        self.nc = nc
        self.tag = tag
        self.stack = contextlib.ExitStack()
        self.n = 0

    def sb(self, shape, dt=F32, name=None):
        self.n += 1
        return self.stack.enter_context(self.nc.sbuf_tensor(f"{self.tag}_{name or 't'}{self.n}", list(shape), dt))

    def close(self):
        self.stack.close()


def build_program(debug=()):
    nc = bass.Bass("TRN2", target_bir_lowering=False)

    def din(name, shape, dt=F32):
        return nc.dram_tensor(name, list(shape), dt, kind="ExternalInput").ap()

    def dscr(name, shape, dt=F32):
        return nc.dram_tensor(name, list(shape), dt, kind="Internal").ap()

    xin = din("xin", [T, D])
    cvec = din("cvec", [128, 8, 2])
    w_mod = din("w_mod", [NL, D, 6144])
    bmod_c = din("bmod_c", [NL, 128, 48])
    bmod_r = din("bmod_r", [NL, 1, 6144])
    w_fm = din("w_fm", [NL, D, 2048])
    w_tm = din("w_tm", [NL, D, TMW + 4096])
    a_cw = din("a_cw", [NL, 128, 2, 5])
    a_cb = din("a_cb", [NL, 128, 2])
    a_gw = din("a_gw", [NL, 128, 2, 2, 2, 128])
    a_gb = din("a_gb", [NL, 128, 2, 2, 2])
    a_lam = din("a_lam", [NL, 128, 2, 2])
    b_thp = din("b_thp", [NL, 128, 2, 2])
    b_thh = din("b_thh", [NL, 128, 2, 4])
    c_cw = din("c_cw", [NL, 128, 4, 5])
    c_cb = din("c_cb", [NL, 128, 4])
    c_gb = din("c_gb", [NL, 128, 16])
    d_lbr = din("d_lbr", [128, 2, 256])
    w_branch = din("w_branch", [NL, 4, 256, D])
    w_out = din("w_out", [NL, D, D])
    moe_wgr = din("moe_wgr", [NL, D, 20])
    moe_bgr = din("moe_bgr", [NL, 1, 20])
    moe_w1 = din("moe_w1", [NL, 16, D, 512])
    moe_w3 = din("moe_w3", [NL, 16, D, 512])
    moe_w2 = din("moe_w2", [NL, 16, 512, D])
    fnw = din("fnw", [128, D])
    cst_d = din("cst", [128, NCST])
    ropeC_d = din("ropeC", [128, T])
    ropeS_d = din("ropeS", [128, T])
    yout = nc.dram_tensor("yout", [HALF_OUT, D], F32, kind="ExternalOutput").ap()

    XR = dscr("XR", [T, D])
    FM = dscr("FM", [16, 128, T])
    TM = dscr("TM", [T, TMW])
    MG = dscr("MG", [T, 4096], BF16)
    YT = dscr("YT", [4, 2, 128, T], BF16)
    dbg = {}
    for name, shape, dt in debug:
        dbg[name] = nc.dram_tensor("dbg_" + name, list(shape), dt, kind="ExternalOutput").ap()

    mk = MK(nc)
    G = Pool(nc, "g")

    PS = [nc.psum_tensor(f"ps{i}", [128, 512], F32).__enter__() for i in range(6)]
    PQ = [nc.psum_tensor(f"pq{i}", [128, 1024], BF16).__enter__() for i in range(2)]

    cst = G.sb([128, NCST], F32, "cst")
    identb = G.sb([128, 128], BF16, "identb")
    sT = G.sb([128, 8, 2], F32, "sT")
    MODC = G.sb([128, 4, 8, 2], F32, "MODC")
    GB = G.sb([128, 2, 2, D], F32, "GB")
    mk.dma("sp", cst[:], cst_d, W=["cst"])
    mk.dma("sp", sT[:], cvec, W=["sT"])

    def C(name, rows=slice(0, 128)):
        o, w = CST[name]
        return cst[rows, o:o + w]

    mk.op("dve", "tensor_copy", out=identb[:], in_=C("ident"), R=["cst"], W=["identb"])
    mk.op("act", "activation", out=sT[:], in_=sT[:], func=AF.Silu, R=["sT"], W=["sT"])
    for t in range(NT):
        mk.dma("sp", XR[t * 128:(t + 1) * 128, :], xin[t * 128:(t + 1) * 128, :], W=[f"XR{t}"])

    def cond_of(t):
        return 0 if t < 2 else 1

    def mod_phase(l):
        P = Pool(nc, f"mod{l}")
        wblk = P.sb([128, 8, 1024], F32, "wblk")
        bmc = P.sb([128, 48], F32, "bmc")
        bmr = P.sb([1, 6144], F32, "bmr")
        GR = P.sb([2, 2, D], F32, "GR")
        mk.dma("sp", bmc[:], bmod_c[l], W=["bmc"])
        mk.dma("sp", bmr[:], bmod_r[l], W=["bmr"])
        sel = C("sel", slice(0, 2))
        for m in range(6):
            mk.dma("sp", wblk[:], w_mod[l].rearrange("(c p) f -> p c f", p=128)[:, :, m * 1024:(m + 1) * 1024],
                   W=["wblk"])
            if m in (0, 1, 3, 4):
                m4 = {0: 0, 1: 1, 3: 2, 4: 3}[m]
                for c in range(8):
                    for k in range(8):
                        mk.op("pe", "matmul", PS[0][:, c * 2:(c + 1) * 2], lhsT=wblk[:, k, c * 128:(c + 1) * 128],
                              rhs=sT[:, k, :], start=(k == 0), stop=(k == 7), R=["wblk", "sT"], W=["ps0"])
                mk.op("dve", "tensor_tensor", out=MODC[:, m4, :, :],
                      in0=PS[0][:, 0:16].rearrange("p (c j) -> p c j", j=2),
                      in1=bmc[:, m * 8:(m + 1) * 8].unsqueeze(2).to_broadcast([128, 8, 2]), op=ALU.add,
                      R=["ps0", "bmc"], W=["MODC"])
                if m in (1, 4):
                    mk.op("dve", "tensor_scalar_add", out=MODC[:, m4, :, :], in0=MODC[:, m4, :, :], scalar1=1.0,
                          R=["MODC"], W=["MODC"])
            else:
                mi = 0 if m == 2 else 1
                for cb in range(2):
                    for k in range(8):
                        mk.op("pe", "matmul", PS[1][0:2, :], lhsT=sT[:, k, :], rhs=wblk[:, k, cb * 512:(cb + 1) * 512],
                              start=(k == 0), stop=False, R=["wblk", "sT"], W=["ps1"])
                    mk.op("pe", "matmul", PS[1][0:2, :], lhsT=sel[0:1, 0:2],
                          rhs=bmr[0:1, m * 1024 + cb * 512: m * 1024 + (cb + 1) * 512], start=False, stop=True,
                          R=["bmr", "cst"], W=["ps1"])
                    mk.op("dve", "tensor_copy", out=GR[0:2, mi, cb * 512:(cb + 1) * 512], in_=PS[1][0:2, :],
                          R=["ps1"], W=["GR"])
        for j in range(2):
            for mi in range(2):
                for cb in range(2):
                    mk.op("pe", "matmul", PS[1][:, :], lhsT=sel[0:2, j * 128:(j + 1) * 128],
                          rhs=GR[0:2, mi, cb * 512:(cb + 1) * 512], start=True, stop=True, R=["GR", "cst"], W=["ps1"])
                    mk.op("act", "activation", out=GB[:, j, mi, cb * 512:(cb + 1) * 512], in_=PS[1][:, :], func=AF.Copy,
                          R=["ps1"], W=["GB"])
        mk.barrier()
        P.close()

    def make_norm(P, nbuf=2):
        st = dict(junk=P.sb([128, D], BF16, "junk"), ss=[P.sb([128, 1], F32, "ss") for _ in range(nbuf)],
                  xn=[P.sb([128, D], BF16, "xn") for _ in range(nbuf)],
                  tmp=[P.sb([128, 8, 128], F32, "tmp") for _ in range(nbuf)], i=0, nbuf=nbuf)

        def norm(xt_ap, xt_key, j, msc, msh, h_out, h_key):
            i = st["i"] % st["nbuf"]
            st["i"] += 1
            ss, xn, tmp = st["ss"][i], st["xn"][i], st["tmp"][i]
            mk.op("act", "activation", out=st["junk"][:], in_=xt_ap, func=AF.Square, accum_out=ss[:],
                  R=[xt_key], W=["junk", f"ss{i}"])
            mk.op("act", "activation", out=ss[:], in_=ss[:], func=AF.Sqrt, scale=1.0 / D, bias=EPS,
                  R=[f"ss{i}"], W=[f"ss{i}"])
            mk.op("dve", "reciprocal", out=ss[:], in_=ss[:], R=[f"ss{i}"], W=[f"ss{i}"])
            mk.op("dve", "tensor_scalar", out=xn[:], in0=xt_ap, scalar1=ss[:, 0:1], scalar2=None, op0=ALU.mult,
                  R=[xt_key, f"ss{i}"], W=[f"xn{i}"])
            for c in range(8):
                mk.op("pe", "transpose", out=PQ[i][:, c * 128:(c + 1) * 128], in_=xn[:, c * 128:(c + 1) * 128],
                      identity=identb[:], R=[f"xn{i}", "identb"], W=[f"pq{i}"])
            mk.op("dve", "tensor_tensor", out=tmp[:], in0=PQ[i][:, :].rearrange("p (c n) -> p c n", c=8),
                  in1=MODC[:, msc, :, j:j + 1].to_broadcast([128, 8, 128]), op=ALU.mult,
                  R=[f"pq{i}", "MODC"], W=[f"ntmp{i}"])
            mk.op("pool", "tensor_tensor", out=h_out, in0=tmp[:],
                  in1=MODC[:, msh, :, j:j + 1].to_broadcast([128, 8, 128]), op=ALU.add,
                  R=[f"ntmp{i}", "MODC"], W=[h_key])
        return norm

    def inproj_phase(l, last=False):
        P = Pool(nc, f"ip{l}")
        hT = P.sb([128, 8, T], BF16, "hT")
        xt = [P.sb([128, D], F32, "xt") for _ in range(2)]
        norm = make_norm(P)
        for t in range(NT):
            i = t % 2
            mk.dma("sp", xt[i][:], XR[t * 128:(t + 1) * 128, :], R=[f"XR{t}"], W=[f"xt{i}"])
            norm(xt[i][:], f"xt{i}", cond_of(t), 1, 0, hT[:, :, t * 128:(t + 1) * 128], f"hT{t}")
        hkeys = [f"hT{t}" for t in range(NT)]
        wf = [P.sb([128, 8, 512], F32, "wf")] * 2
        wb = [P.sb([128, 8, 512], BF16, "wb") for _ in range(2)]
        stg = [P.sb([128, T], F32, "stg")] * 2
        nblk = 0
        tblocks = [(i * 512, min(512, T - i * 512)) for i in range(9)]
        for cb in range(4):
            i = nblk % 2
            nblk += 1
            mk.dma("sp", wf[i][:], w_fm[l].rearrange("(c p) f -> p c f", p=128)[:, :, cb * 512:(cb + 1) * 512],
                   W=["wf"])
            mk.ev(wb[i][:], wf[i][:], R=["wf"], W=[f"wb{i}"])
            for sub in range(4):
                fc = cb * 4 + sub
                si = fc % 2
                for bi, (t0, tw) in enumerate(tblocks):
                    pb = bi % 2
                    for k in range(8):
                        mk.op("pe", "matmul", PS[pb][:, 0:tw], lhsT=wb[i][:, k, sub * 128:(sub + 1) * 128],
                              rhs=hT[:, k, t0:t0 + tw], start=(k == 0), stop=(k == 7),
                              R=[f"wb{i}"] + hkeys[t0 // 128:(t0 + tw) // 128], W=[f"ps{pb}"])
                    mk.ev(stg[si][:, t0:t0 + tw], PS[pb][:, 0:tw], R=[f"ps{pb}"], W=["stg"])
                mk.dma("pool", FM[fc], stg[si][:], R=["stg"], W=[f"FM{fc}"])
        cblocks = [(i * 512, 512) for i in range(4)] + [(2048, TMW - 2048)] + [(TMW + i * 512, 512) for i in range(8)]
        stt = [P.sb([128, 4, 512], F32, "stt") for _ in range(2)]
        stb = [P.sb([128, 4, 512], BF16, "stb") for _ in range(2)]
        tgroups = [(g * 4, min(4, NT - g * 4)) for g in range(9)]
        ns = 0
        for (c0, cw) in cblocks:
            i = nblk % 2
            nblk += 1
            mk.dma("sp", wf[i][:, :, 0:cw], w_tm[l].rearrange("(c p) f -> p c f", p=128)[:, :, c0:c0 + cw], W=["wf"])
            mk.ev(wb[i][:, :, 0:cw], wf[i][:, :, 0:cw], R=["wf"], W=[f"wb{i}"])
            is_mg = c0 >= TMW
            for (g0, gn) in tgroups:
                if is_mg and last and not any((g0 + q) in OUT_T for q in range(gn)):
                    continue
                si = ns % 2
                ns += 1
                for tt in range(gn):
                    t = g0 + tt
                    pb = 2 + (t % 2)
                    for k in range(8):
                        mk.op("pe", "matmul", PS[pb][:, 0:cw], lhsT=hT[:, k, t * 128:(t + 1) * 128],
                              rhs=wb[i][:, k, 0:cw], start=(k == 0), stop=(k == 7), R=[f"wb{i}", f"hT{t}"], W=[f"ps{pb}"])
                    if is_mg:
                        mk.ev(stb[si][:, tt, 0:cw], PS[pb][:, 0:cw], R=[f"ps{pb}"], W=[f"stb{si}"], func=AF.Sigmoid)
                    else:
                        mk.ev(stt[si][:, tt, 0:cw], PS[pb][:, 0:cw], R=[f"ps{pb}"], W=[f"stt{si}"])
                if is_mg:
                    mk.dma("pool", MG[g0 * 128:(g0 + gn) * 128, c0 - TMW:c0 - TMW + cw].rearrange("(t p) c -> p t c", p=128),
                           stb[si][:, 0:gn, 0:cw], R=[f"stb{si}"], W=["MG"])
                else:
                    mk.dma("pool", TM[g0 * 128:(g0 + gn) * 128, c0:c0 + cw].rearrange("(t p) c -> p t c", p=128),
                           stt[si][:, 0:gn, 0:cw], R=[f"stt{si}"], W=["TM"])
        mk.barrier()
        P.close()

    SEGS = [(0, 256), (256, T)]

    def conv_fm(u, src, w4, bcol, keyu, keysrc, wkeys):
        for (s0, e) in SEGS:
            mk.op("act", "activation", out=u[:, s0:e], in_=src[:, s0:e], func=AF.Identity, scale=w4[:, 2:3], bias=bcol,
                  R=[keysrc] + wkeys, W=[keyu])
            for k, sh in ((0, -2), (1, -1), (3, 1), (4, 2)):
                if sh < 0:
                    o, i_ = u[:, s0 - sh:e], src[:, s0:e + sh]
                else:
                    o, i_ = u[:, s0:e - sh], src[:, s0 + sh:e]
                mk.op("dve", "scalar_tensor_tensor", out=o, in0=i_, scalar=w4[:, k:k + 1], in1=o, op0=ALU.mult,
                      op1=ALU.add, R=[keysrc, keyu] + wkeys, W=[keyu])

    def mixer_a(l):
        P = Pool(nc, f"ma{l}")
        cw = P.sb([128, 2, 5], F32, "cw")
        cb = P.sb([128, 2], F32, "cb")
        gwf = P.sb([128, 2, 2, 2, 128], F32, "gwf")
        gwb = P.sb([128, 2, 2, 2, 128], BF16, "gwb")
        gb = P.sb([128, 2, 2, 2], F32, "gb")
        lam = P.sb([128, 2, 2], F32, "lam")
        c1 = P.sb([128, 2, 2], F32, "c1")
        mk.dma("sp", cw[:], a_cw[l], W=["a_cw"])
        mk.dma("sp", cb[:], a_cb[l], W=["a_cb"])
        mk.dma("sp", gwf[:], a_gw[l], W=["a_gwf"])
        mk.dma("sp", gb[:], a_gb[l], W=["a_gb"])
        mk.dma("sp", lam[:], a_lam[l], W=["a_lam"])
        mk.op("dve", "tensor_copy", out=gwb[:], in_=gwf[:], R=["a_gwf"], W=["a_gwb"])
        mk.op("act", "activation", out=c1[:], in_=lam[:], func=AF.Exp, scale=-1.0, R=["a_lam"], W=["a_c1"])
        mk.op("act", "activation", out=c1[:], in_=c1[:], func=AF.Ln, bias=1.0, R=["a_c1"], W=["a_c1"])
        mk.op("dve", "tensor_scalar", out=c1[:], in0=c1[:], scalar1=-8.0, scalar2=None, op0=ALU.mult, R=["a_c1"], W=["a_c1"])
        ax = P.sb([128, T], F32, "ax")
        ag = P.sb([128, T], F32, "ag")
        u = P.sb([128, T], F32, "u")
        ub = P.sb([128, T], BF16, "ub")
        aa = P.sb([128, T], F32, "aa")
        bt = P.sb([128, T], F32, "bt")
        hf = P.sb([128, T], F32, "hf")
        hb = P.sb([128, T], F32, "hb")
        r = [P.sb([128, 512], F32, "r") for _ in range(2)]
        gi = [P.sb([128, 512], F32, "gi") for _ in range(2)]
        yb = P.sb([128, T], BF16, "yb")
        tblocks = [(i * 512, min(512, T - i * 512)) for i in range(9)]
        for c in range(2):
            mk.dma("sp", ax[:], FM[FM_OFF["a_x"] + c], R=[f"FM{FM_OFF['a_x'] + c}"], W=["ax"])
            mk.dma("sp", ag[:], FM[FM_OFF["a_g"] + c], R=[f"FM{FM_OFF['a_g'] + c}"], W=["ag"])
            conv_fm(u, ax, cw[:, c, :], cb[:, c:c + 1], "u", "ax", ["a_cw", "a_cb"])
            mk.op("pool", "tensor_copy", out=ub[:], in_=u[:], R=["u"], W=["ub"])
            for d in range(2):
                for bi, (t0, tw) in enumerate(tblocks):
                    i = bi % 2
                    mk.op("pe", "matmul", PS[i][:, 0:tw], lhsT=gwb[:, d, 0, c, :], rhs=ub[:, t0:t0 + tw], start=True,
                          stop=True, R=["a_gwb", "ub"], W=[f"ps{i}"])
                    mk.op("pe", "matmul", PS[2 + i][:, 0:tw], lhsT=gwb[:, d, 1, c, :], rhs=ub[:, t0:t0 + tw], start=True,
                          stop=True, R=["a_gwb", "ub"], W=[f"ps{2 + i}"])
                    mk.op("act", "activation", out=r[i][:, 0:tw], in_=PS[i][:, 0:tw], func=AF.Sigmoid,
                          bias=gb[:, d, 0, c:c + 1], R=[f"ps{i}", "a_gb"], W=[f"r{i}"])
                    mk.op("act", "activation", out=gi[i][:, 0:tw], in_=PS[2 + i][:, 0:tw], func=AF.Sigmoid,
                          bias=gb[:, d, 1, c:c + 1], R=[f"ps{2 + i}", "a_gb"], W=[f"gi{i}"])
                    mk.op("act", "activation", out=aa[:, t0:t0 + tw], in_=r[i][:, 0:tw], func=AF.Exp,
                          scale=c1[:, d, c:c + 1], R=[f"r{i}", "a_c1"], W=["aa"])
                    mk.op("dve", "tensor_tensor", out=r[i][:, 0:tw], in0=aa[:, t0:t0 + tw], in1=aa[:, t0:t0 + tw],
                          op=ALU.mult, R=["aa", f"r{i}"], W=[f"r{i}"])
                    mk.op("dve", "tensor_scalar", out=r[i][:, 0:tw], in0=r[i][:, 0:tw], scalar1=-1.0, scalar2=1.0,
                          op0=ALU.mult, op1=ALU.add, R=[f"r{i}"], W=[f"r{i}"])
                    mk.op("act", "activation", out=r[i][:, 0:tw], in_=r[i][:, 0:tw], func=AF.Sqrt, R=[f"r{i}"], W=[f"r{i}"])
                    mk.op("dve", "tensor_tensor", out=gi[i][:, 0:tw], in0=gi[i][:, 0:tw], in1=r[i][:, 0:tw], op=ALU.mult,
                          R=[f"gi{i}", f"r{i}"], W=[f"gi{i}"])
                    mk.op("pool", "tensor_tensor", out=bt[:, t0:t0 + tw], in0=gi[i][:, 0:tw], in1=u[:, t0:t0 + tw],
                          op=ALU.mult, R=[f"gi{i}", "u"], W=["bt"])
                if d == 0:
                    mk.op("dve", "tensor_tensor_scan", out=hf[:, :], data0=aa[:, :], data1=bt[:, :], initial=0.0,
                          op0=ALU.mult, op1=ALU.add, R=["aa", "bt"], W=["hf"])
                else:
                    mk.op("dve", "tensor_tensor_scan", out=hb[:, 0:256][:, ::-1], data0=aa[:, 0:256][:, ::-1],
                          data1=bt[:, 0:256][:, ::-1], initial=0.0, op0=ALU.mult, op1=ALU.add, R=["aa", "bt"], W=["hb"])
                    mk.op("dve", "tensor_tensor_scan", out=hb[:, 256:T][:, ::-1], data0=aa[:, 256:T][:, ::-1],
                          data1=bt[:, 256:T][:, ::-1], initial=hb[:, 0:1], op0=ALU.mult, op1=ALU.add,
                          R=["aa", "bt", "hb"], W=["hb"])
            mk.op("act", "activation", out=ag[:], in_=ag[:], func=AF.Gelu, R=["ag"], W=["ag"])
            mk.op("dve", "tensor_tensor", out=hf[:], in0=hf[:], in1=hb[:], op=ALU.add, R=["hf", "hb"], W=["hf"])
            mk.op("dve", "tensor_tensor", out=yb[:], in0=hf[:], in1=ag[:], op=ALU.mult, R=["hf", "ag"], W=["yb"])
            mk.dma("pool", YT[0, c], yb[:], R=["yb"], W=[f"YT0{c}"])
        mk.barrier()
        P.close()

    def order_of(dr):
        return list(range(NT)) if dr == 0 else [1, 0] + list(range(NT - 1, 1, -1))

    def run_pipelined(gens, pipelined=True):
        if not pipelined:
            for g in gens:
                for _ in g:
                    pass
            return
        prev = None
        for g in gens:
            next(g, None)
            if prev is not None:
                for _ in prev:
                    pass
            prev = g
        if prev is not None:
            for _ in prev:
                pass

    OUT_T = list(range(2, 2 + HALF_OUT // 128))

    def plan(dr, last):
        if not last:
            return [(t, True) for t in order_of(dr)]
        if dr == 0:
            return [(0, False), (1, False)] + [(t, True) for t in OUT_T]
        return [(t, (t in OUT_T)) for t in order_of(dr)]

    def chunk_core(it, nsub, dr, QT, KT, QIT, KHs, V, vw, Gc, S, Sbf, maskD, maskkey, rkeys, PT, sk, full=True):
        assert nsub == 1
        pi = it % 2
        par = it % 2
        okeys = ["ps2", "ps3"]
        Oh = [PS[2 + hh][:, 0:2 * vw].rearrange("p (c e) -> p c e", c=2) for hh in range(2)]
        KVp = PS[4][:, 0:2 * vw].rearrange("p (c e) -> p c e", c=2)
        for h in range(4):
            c, hh = h // 2, h % 2
            mk.op("pe", "matmul", KVp[hh * 64:(hh + 1) * 64, c, :], lhsT=KHs(0)[:, h * 64:(h + 1) * 64],
                  rhs=V[:, h, :], start=True, stop=True, R=rkeys, W=["ps4kv"])
        for c in range(2):
            mk.op("dve", "scalar_tensor_tensor", out=S[1 - par][c][:], in0=S[par][c][:], scalar=Gc[:, c, 0:1],
                  in1=KVp[:, c, :], op0=ALU.mult, op1=ALU.add, R=[f"{sk}S{par}{c}", "ps4kv"] + rkeys,
                  W=[f"{sk}S{1 - par}{c}"])
            mk.op("act", "activation", out=Sbf[1 - par][c][:], in_=S[1 - par][c][:], func=AF.Copy,
                  R=[f"{sk}S{1 - par}{c}"], W=[f"{sk}Sbf{1 - par}{c}"])
        if not full:
            return okeys, Oh
        for h in range(4):
            c, hh = h // 2, h % 2
            rs = slice(hh * 64, (hh + 1) * 64)
            mk.op("pe", "matmul", PS[hh][:, c * 128:(c + 1) * 128], lhsT=KT[c][rs, :], rhs=QT[c][rs, :], start=True,
                  stop=True, R=rkeys, W=[f"ps{hh}"])
        PTv = PT[pi][:].rearrange("p (c x n) -> p c x n", c=2, x=2)
        Mv = maskD.rearrange("p (c x n) -> p c x n", c=2, x=2)
        for hh in range(2):
            mk.op("dve", "tensor_tensor", out=PTv[:, :, hh, :], in0=PS[hh][:, 0:256].rearrange("p (c n) -> p c n", c=2),
                  in1=Mv[:, :, hh, :], op=ALU.mult, R=[f"ps{hh}", maskkey], W=[f"PT{pi}h{hh}"])
        ptk = [f"PT{pi}h0", f"PT{pi}h1"]
        for h in range(4):
            c, hh = h // 2, h % 2
            rs = slice(hh * 64, (hh + 1) * 64)
            mk.op("pe", "matmul", Oh[hh][:, c, :], lhsT=PT[pi][:, h * 128:(h + 1) * 128], rhs=V[:, h, :], start=True,
                  stop=False, R=[ptk[hh]] + rkeys, W=[okeys[hh]])
            mk.op("pe", "matmul", Oh[hh][:, c, :], lhsT=QIT[c][rs, :], rhs=Sbf[par][c][rs, :], start=False, stop=True,
                  R=rkeys + [f"{sk}Sbf{par}{c}"], W=[okeys[hh]])
        return okeys, Oh

    def hview(ap256, hh):
        return ap256.rearrange("p (c x e) -> p c x e", c=2, x=2)[:, :, hh, :]

    def make_finalize(P, n, yTb):
        st = dict(i=0)
        cent = [P.sb([128, 4, 64], F32, "cent") for _ in range(2)]
        sq = P.sb([128, 4, 64], F32, "sq")
        mm = [P.sb([128, 4], F32, "mm") for _ in range(2)]
        vv = [P.sb([128, 4], F32, "vv") for _ in range(2)]
        yy = [P.sb([128, 256], BF16, "yy") for _ in range(2)]

        def fin(tot, totkey, center, gate, gatekey, t):
            i = st["i"] % 2
            st["i"] += 1
            tv = tot.rearrange("p (h e) -> p h e", h=4)
            tk = list(totkey) if isinstance(totkey, (list, tuple)) else [totkey]
            src, skeys = tv, tk
            if center:
                mk.op("dve", "tensor_reduce", out=mm[i][:], in_=tv, axis=AX.X, op=ALU.add, R=tk, W=[f"fmm{i}"])
                mk.op("dve", "tensor_scalar", out=mm[i][:], in0=mm[i][:], scalar1=-1.0 / 64, scalar2=None, op0=ALU.mult,
                      R=[f"fmm{i}"], W=[f"fmm{i}"])
                mk.op("dve", "tensor_tensor", out=cent[i][:], in0=tv, in1=mm[i][:].unsqueeze(2).to_broadcast([128, 4, 64]),
                      op=ALU.add, R=tk + [f"fmm{i}"], W=[f"fcent{i}"])
                src, skeys = cent[i][:], [f"fcent{i}"]
            mk.op("pool", "tensor_tensor", out=sq[:], in0=src, in1=src, op=ALU.mult, R=skeys, W=["fsq"])
            mk.op("dve", "tensor_reduce", out=vv[i][:], in_=sq[:], axis=AX.X, op=ALU.add, R=["fsq"], W=[f"fvv{i}"])
            mk.op("act", "activation", out=vv[i][:], in_=vv[i][:], func=AF.Sqrt, scale=1.0 / 64, bias=EPS,
                  R=[f"fvv{i}"], W=[f"fvv{i}"])
            mk.op("dve", "reciprocal", out=vv[i][:], in_=vv[i][:], R=[f"fvv{i}"], W=[f"fvv{i}"])
            mk.op("dve", "tensor_tensor", out=cent[i][:], in0=src, in1=vv[i][:].unsqueeze(2).to_broadcast([128, 4, 64]),
                  op=ALU.mult, R=skeys + [f"fvv{i}"], W=[f"fcent{i}"])
            mk.op("dve", "tensor_tensor", out=yy[i][:], in0=cent[i][:].rearrange("p h e -> p (h e)"), in1=gate,
                  op=ALU.mult, R=[f"fcent{i}", gatekey], W=[f"fyy{i}"])
            for c in range(2):
                mk.op("pe", "transpose", out=PQ[1][:, (i * 2 + c) * 128:(i * 2 + c + 1) * 128],
                      in_=yy[i][:, c * 128:(c + 1) * 128], identity=identb[:], R=[f"fyy{i}", "identb"], W=[f"pq1f{i}"])
            mk.op("act", "activation", out=yTb[:, :, t * 128:(t + 1) * 128],
                  in_=PQ[1][:, i * 256:(i + 1) * 256].rearrange("p (c n) -> p c n", c=2), func=AF.Copy,
                  R=[f"pq1f{i}"], W=["yTb"])
        return fin

    def mixer_b(l, last=False):
        P = Pool(nc, f"mb{l}")
        QR = [P.sb([128, T], BF16, "QR") for _ in range(2)]
        KR = [P.sb([128, T], BF16, "KR") for _ in range(2)]
        thp = P.sb([128, 2, 2], F32, "thp")
        thh = P.sb([128, 2, 4], F32, "thh")
        mk.dma("sp", thp[:], b_thp[l], W=["thp"])
        mk.dma("sp", thh[:], b_thh[l], W=["thh"])
        for tt, key in ((thp, "thp"), (thh, "thh")):
            mk.op("act", "activation", out=tt[:], in_=tt[:], func=AF.Exp, scale=-1.0, R=[key], W=[key])
            mk.op("act", "activation", out=tt[:], in_=tt[:], func=AF.Ln, bias=1.0, R=[key], W=[key])
            mk.op("dve", "tensor_scalar", out=tt[:], in0=tt[:], scalar1=-1.0, scalar2=None, op0=ALU.mult, R=[key], W=[key])
        DM = P.sb([128, 2, 512], F32, "DM")
        QW = P.sb([128, 2, 2, 128], F32, "QW")
        KW = P.sb([128, 2, 4], F32, "KW")
        Gc = P.sb([128, 2, 2, 1], F32, "Gc")
        for dr in range(2):
            diff, msk, pos = (C("diffF"), C("maskF"), C("posF")) if dr == 0 else (C("diffB"), C("maskB"), C("posB"))
            for h in range(4):
                mk.op("act", "activation", out=DM[:, dr, h * 128:(h + 1) * 128], in_=diff, func=AF.Exp,
                      scale=thh[:, dr, h:h + 1], R=["cst", "thh"], W=["DM"])
                mk.op("dve", "tensor_tensor", out=DM[:, dr, h * 128:(h + 1) * 128], in0=DM[:, dr, h * 128:(h + 1) * 128],
                      in1=msk, op=ALU.mult, R=["DM", "cst"], W=["DM"])
                mk.op("act", "activation", out=KW[:, dr, h:h + 1], in_=C("kpos")[:, dr:dr + 1], func=AF.Exp,
                      scale=thh[:, dr, h:h + 1], R=["cst", "thh"], W=["KW"])
            for c in range(2):
                mk.op("act", "activation", out=QW[:, dr, c, :], in_=pos, func=AF.Exp, scale=thp[:, dr, c:c + 1],
                      R=["cst", "thp"], W=["QW"])
                mk.op("act", "activation", out=Gc[:, dr, c, :], in_=thp[:, dr, c:c + 1], func=AF.Exp, scale=128.0,
                      R=["thp"], W=["Gc"])
        segw = 1088
        f1 = [P.sb([128, segw], F32, "f1") for _ in range(2)]
        f2 = [P.sb([128, segw], F32, "f2") for _ in range(2)]
        rc = P.sb([128, T], F32, "rc")
        rsn = P.sb([128, T], F32, "rsn")
        mk.dma("sp", rc[:], ropeC_d, W=["rc"])
        mk.dma("sp", rsn[:], ropeS_d, W=["rsn"])
        n = 0
        for (dst, base, pbase, scale) in ((QR, "b_q", "b_qp", 1.0), (KR, "b_k", "b_kp", 0.125)):
            for c in range(2):
                for sg in range(4):
                    i = n % 2
                    n += 1
                    cs = slice(sg * segw, (sg + 1) * segw)
                    mk.dma("sp", f1[i][:], FM[FM_OFF[base] + c][:, cs], R=[f"FM{FM_OFF[base] + c}"], W=[f"f1{i}"])
                    mk.dma("sp", f2[i][:], FM[FM_OFF[pbase] + c][:, cs], R=[f"FM{FM_OFF[pbase] + c}"], W=[f"f2{i}"])
                    mk.op("dve", "tensor_tensor", out=f1[i][:], in0=f1[i][:], in1=rc[:, cs], op=ALU.mult,
                          R=[f"f1{i}", "rc"], W=[f"f1{i}"])
                    mk.op("pool", "tensor_tensor", out=f2[i][:], in0=f2[i][:], in1=rsn[:, cs], op=ALU.mult,
                          R=[f"f2{i}", "rsn"], W=[f"f2{i}"])
                    mk.op("dve", "tensor_tensor", out=f1[i][:], in0=f1[i][:], in1=f2[i][:], op=ALU.add,
                          R=[f"f1{i}", f"f2{i}"], W=[f"f1{i}"])
                    mk.op("act", "activation", out=dst[c][:, cs], in_=f1[i][:], func=AF.Copy, scale=scale,
                          R=[f"f1{i}"], W=[f"b{base}{c}"])
        rkeys = ["bb_q0", "bb_q1", "bb_k0", "bb_k1"]
        OF = P.sb([128, NT, 256], F32, "OF")
        yTb = P.sb([128, 2, T], BF16, "yTb")
        fin = make_finalize(P, 1, yTb)
        PT = [P.sb([128, 512], BF16, "PT") for _ in range(2)]
        QIT = [[P.sb([128, 128], BF16, "QIT") for _ in range(2)] for _ in range(2)]
        KH = [P.sb([128, 256], BF16, "KH") for _ in range(2)]
        Vf = [P.sb([128, 512], F32, "Vf") for _ in range(2)]
        Vb = [P.sb([128, 4, 64], BF16, "Vb") for _ in range(2)]
        gt = [P.sb([128, 256], F32, "gt") for _ in range(2)]
        tot = [P.sb([128, 256], F32, "tot") for _ in range(2)]
        S = [[P.sb([128, 64], F32, "S") for _ in range(2)] for _ in range(2)]
        Sbf = [[P.sb([128, 64], BF16, "Sbf") for _ in range(2)] for _ in range(2)]
        it = 0
        for dr in range(2):
            for c in range(2):
                mk.op("pool", "memset", S[it % 2][c][:], 0.0, W=[f"bS{it % 2}{c}"])
                mk.op("pool", "memset", Sbf[it % 2][c][:], 0.0, W=[f"bSbf{it % 2}{c}"])
            def body(t, full, it):
                i = it % 2
                cols = slice(t * 128, (t + 1) * 128)
                mk.dma("sp", Vf[i][:], TM[t * 128:(t + 1) * 128, 0:512], R=["TM"], W=[f"bVf{i}"])
                mk.op("pool", "tensor_copy", out=Vb[i][:], in_=Vf[i][:, 0:256].rearrange("p (h e) -> p h e", h=4),
                      R=[f"bVf{i}"], W=[f"bVb{i}"])
                for c in range(2):
                    if full:
                        mk.op("pool", "tensor_tensor", out=QIT[i][c][:], in0=QR[c][:, cols], in1=QW[:, dr, c, :],
                              op=ALU.mult, R=[f"bb_q{c}", "QW"], W=[f"bQIT{i}"])
                    mk.op("pe", "transpose", out=PQ[0][:, (i * 2 + c) * 128:(i * 2 + c + 1) * 128], in_=KR[c][:, cols],
                          identity=identb[:], R=[f"bb_k{c}", "identb"], W=[f"pq0k{i}"])
                for h in range(4):
                    mk.op("act", "activation", out=KH[i][:, h * 64:(h + 1) * 64],
                          in_=PQ[0][:, i * 256 + h * 64:i * 256 + (h + 1) * 64], func=AF.Identity, scale=KW[:, dr, h:h + 1],
                          R=[f"pq0k{i}", "KW"], W=[f"bKH{i}"])
                yield
                okeys, Oh = chunk_core(it, 1, dr, [QR[0][:, cols], QR[1][:, cols]], [KR[0][:, cols], KR[1][:, cols]],
                                       [QIT[i][0], QIT[i][1]], (lambda s_, kh=KH[i]: kh), Vb[i], 64, Gc[:, dr], S, Sbf,
                                       DM[:, dr, :], "DM", rkeys + [f"bQIT{i}", f"bKH{i}", f"bVb{i}"], PT, "b", full=full)
                if not full:
                    pass
                elif dr == 0:
                    for hh in range(2):
                        mk.op("act", "activation", out=hview(OF[:, t, :], hh), in_=Oh[hh], func=AF.Copy, R=[okeys[hh]],
                              W=[f"bOF{t}h{hh}"])
                else:
                    for hh in range(2):
                        mk.op("dve", "tensor_tensor", out=hview(tot[i][:], hh), in0=Oh[hh], in1=hview(OF[:, t, :], hh),
                              op=ALU.add, R=[okeys[hh], f"bOF{t}h{hh}"], W=[f"btot{i}h{hh}"])
                    mk.op("act", "activation", out=gt[i][:], in_=Vf[i][:, 256:512], func=AF.Silu, R=[f"bVf{i}"],
                          W=[f"bgt{i}"])
                    fin(tot[i][:], [f"btot{i}h0", f"btot{i}h1"], True, gt[i][:], f"bgt{i}", t)
            gens = []
            for t, full in plan(dr, last):
                gens.append(body(t, full, it))
                it += 1
            run_pipelined(gens, pipelined=PIPE_LAST or not last)
        for c in range(2):
            mk.dma("pool", YT[1, c], yTb[:, c, :], R=["yTb"], W=[f"YT1{c}"])
        mk.barrier()
        P.close()

    def mixer_c(l, last=False):
        P = Pool(nc, f"mc{l}")
        QC = [P.sb([128, T], BF16, "QC") for _ in range(2)]
        KC = [P.sb([128, T], BF16, "KC") for _ in range(2)]
        cw = P.sb([128, 4, 5], F32, "cw")
        cb = P.sb([128, 4], F32, "cb")
        gbias = P.sb([128, 16], F32, "gbias")
        mk.dma("sp", cw[:], c_cw[l], W=["c_cw"])
        mk.dma("sp", cb[:], c_cb[l], W=["c_cb"])
        mk.dma("sp", gbias[:], c_gb[l], W=["c_gb"])
        src = P.sb([128, T], F32, "src")
        u = P.sb([128, T], F32, "u")
        for ch in range(4):
            fc = FM_OFF["c_q"] + ch
            mk.dma("sp", src[:], FM[fc], R=[f"FM{fc}"], W=["csrc"])
            conv_fm(u, src, cw[:, ch, :], cb[:, ch:ch + 1], "cu", "csrc", ["c_cw", "c_cb"])
            dst = QC[ch] if ch < 2 else KC[ch - 2]
            mk.op("act", "activation", out=u[:], in_=u[:], func=AF.Silu, R=["cu"], W=["cu"])
            mk.op("dve", "tensor_scalar", out=dst[:], in0=u[:], scalar1=(1.0 if ch < 2 else 0.125), scalar2=None,
                  op0=ALU.mult, R=["cu"], W=[f"cqk{ch}"])
        Z = P.sb([128, NT, 16], F32, "Z")
        LFN = P.sb([128, NT, 16], F32, "LFN")
        mk.dma("sp", Z[:], TM[:, TM_OFF["c_gates"]:TM_OFF["c_gates"] + 16].rearrange("(t p) g -> p t g", p=128),
               R=["TM"], W=["cZ"])
        mk.op("dve", "tensor_tensor", out=Z[:], in0=Z[:], in1=gbias[:].unsqueeze(1).to_broadcast([128, NT, 16]),
              op=ALU.add, R=["cZ", "c_gb"], W=["cZ"])
        mk.op("act", "activation", out=LFN[:], in_=Z[:], func=AF.Exp, scale=-1.0, R=["cZ"], W=["cLFN"])
        mk.op("act", "activation", out=LFN[:], in_=LFN[:], func=AF.Ln, bias=1.0, R=["cLFN"], W=["cLFN"])
        mk.op("dve", "tensor_scalar", out=LFN[:], in0=LFN[:], scalar1=-1.0, scalar2=None, op0=ALU.mult, R=["cLFN"],
              W=["cLFN"])
        rkeys = ["cqk0", "cqk1", "cqk2", "cqk3"]
        OF = P.sb([128, NT, 256], F32, "OF")
        yTb = P.sb([128, 2, T], BF16, "yTb")
        fin = make_finalize(P, 2, yTb)
        PT = [P.sb([128, 512], BF16, "PT") for _ in range(2)]
        QIT = [[P.sb([128, 128], BF16, "QIT") for _ in range(2)] for _ in range(2)]
        KH = [P.sb([128, 256], BF16, "KH") for _ in range(2)]
        Vf = [P.sb([128, 512], F32, "Vf") for _ in range(2)]
        Vb = [P.sb([128, 4, 65], BF16, "Vb") for _ in range(2)]
        gt = [P.sb([128, 256], F32, "gt") for _ in range(2)]
        tot = [P.sb([128, 256], F32, "tot") for _ in range(2)]
        S = [[P.sb([128, 65], F32, "S") for _ in range(2)] for _ in range(2)]
        Sbf = [[P.sb([128, 65], BF16, "Sbf") for _ in range(2)] for _ in range(2)]
        Bm4 = [P.sb([128, 4, 128], F32, "Bm4") for _ in range(2)]
        tmp4 = [P.sb([128, 512], F32, "tmp4") for _ in range(2)]
        Dm4 = [P.sb([128, 512], F32, "Dm4") for _ in range(2)]
        EB4 = [P.sb([128, 512], F32, "EB4") for _ in range(2)]
        lmb = [P.sb([128, 4], F32, "lmb") for _ in range(2)]
        kw = [P.sb([128, 4], F32, "kw") for _ in range(2)]
        Gc = [P.sb([128, 2, 1], F32, "Gc") for _ in range(2)]
        rden = [P.sb([128, 4], F32, "rden") for _ in range(2)]
        ebe = [P.sb([128, 4], F32, "ebe") for _ in range(2)]
        hid = [P.sb([128, 4, 64], F32, "hid") for _ in range(2)]
        for i in range(2):
            mk.op("pool", "memset", Vb[i][:], 1.0, W=[f"cVb{i}"])
        ones = C("ones")
        it = 0
        for dr in range(2):
            tri = C("triF") if dr == 0 else C("triB")
            neg4 = C("negF4") if dr == 0 else C("negB4")
            e = 127 if dr == 0 else 0
            for c in range(2):
                mk.op("pool", "memset", S[it % 2][c][:], 0.0, W=[f"cS{it % 2}{c}"])
                mk.op("pool", "memset", Sbf[it % 2][c][:], 0.0, W=[f"cSbf{it % 2}{c}"])
            def body(t, full, it):
                i = it % 2
                cols = slice(t * 128, (t + 1) * 128)
                li = Z[:, t, dr * 8:dr * 8 + 4]
                lf = LFN[:, t, dr * 8 + 4:dr * 8 + 8]
                mk.dma("sp", Vf[i][:], TM[t * 128:(t + 1) * 128, 512:1024], R=["TM"], W=[f"cVf{i}"])
                mk.op("pool", "tensor_copy", out=Vb[i][:, :, 0:64], in_=Vf[i][:, 0:256].rearrange("p (h e) -> p h e", h=4),
                      R=[f"cVf{i}"], W=[f"cVb{i}"])
                mk.op("dve", "tensor_tensor", out=Bm4[i][:], in0=tri.unsqueeze(1).to_broadcast([128, 4, 128]),
                      in1=lf.unsqueeze(2).to_broadcast([128, 4, 128]), op=ALU.mult, R=["cst", "cLFN"], W=[f"cBm{i}"])
                mk.op("pe", "matmul", PS[5][:, :], lhsT=ones, rhs=Bm4[i][:].rearrange("p h n -> p (h n)"), start=True,
                      stop=True, R=["cst", f"cBm{i}"], W=["ps5"])
                mk.op("pe", "matmul", PS[4][:, 256:260], lhsT=tri, rhs=lf, start=True, stop=True, R=["cst", "cLFN"],
                      W=["ps4b"])
                mk.op("dve", "tensor_tensor", out=lmb[i][:], in0=li, in1=PS[4][:, 256:260], op=ALU.subtract,
                      R=["cZ", "ps4b"], W=[f"clmb{i}"])
                if full:
                    mk.op("dve", "tensor_tensor", out=tmp4[i][:], in0=PS[5][:, :], in1=neg4, op=ALU.add, R=["ps5", "cst"],
                          W=[f"ctmp{i}"])
                    for h in range(4):
                        mk.op("act", "activation", out=Dm4[i][:, h * 128:(h + 1) * 128],
                              in_=tmp4[i][:, h * 128:(h + 1) * 128], func=AF.Exp, bias=lmb[i][:, h:h + 1],
                              R=[f"ctmp{i}", f"clmb{i}"], W=[f"cDm{i}"])
                    mk.op("act", "activation", out=EB4[i][:], in_=PS[5][:, :], func=AF.Exp, R=["ps5"], W=[f"cEB{i}"])
                bend = PS[5][:, :].rearrange("p (h n) -> p h n", h=4)[:, :, e]
                mk.op("dve", "tensor_tensor", out=kw[i][:], in0=lmb[i][:], in1=bend, op=ALU.add, R=[f"clmb{i}", "ps5"],
                      W=[f"ckw{i}"])
                mk.op("act", "activation", out=kw[i][:], in_=kw[i][:], func=AF.Exp, R=[f"ckw{i}"], W=[f"ckw{i}"])
                mk.op("act", "activation", out=ebe[i][:], in_=bend, func=AF.Exp, R=["ps5"], W=[f"cebe{i}"])
                for h in range(4):
                    c, hh = h // 2, h % 2
                    rs = slice(hh * 64, (hh + 1) * 64)
                    mk.op("pool", "tensor_copy", out=Gc[i][rs, c, :], in_=ebe[i][rs, h:h + 1],
                          R=[f"cebe{i}"], W=[f"cGc{i}"])
                    if full:
                        mk.op("pool", "tensor_tensor", out=QIT[i][c][rs, :], in0=QC[c][rs, cols],
                              in1=EB4[i][rs, h * 128:(h + 1) * 128], op=ALU.mult, R=[f"cqk{c}", f"cEB{i}"],
                              W=[f"cQIT{i}"])
                for c in range(2):
                    mk.op("pe", "transpose", out=PQ[0][:, (i * 2 + c) * 128:(i * 2 + c + 1) * 128], in_=KC[c][:, cols],
                          identity=identb[:], R=[f"cqk{2 + c}", "identb"], W=[f"pq0k{i}"])
                for h in range(4):
                    mk.op("act", "activation", out=KH[i][:, h * 64:(h + 1) * 64],
                          in_=PQ[0][:, i * 256 + h * 64:i * 256 + (h + 1) * 64], func=AF.Identity, scale=kw[i][:, h:h + 1],
                          R=[f"pq0k{i}", f"ckw{i}"], W=[f"cKH{i}"])
                yield
                okeys, Oh = chunk_core(it, 1, dr, [QC[0][:, cols], QC[1][:, cols]], [KC[0][:, cols], KC[1][:, cols]],
                                       [QIT[i][0], QIT[i][1]], (lambda s_, kh=KH[i]: kh), Vb[i], 65, Gc[i], S, Sbf,
                                       Dm4[i][:], f"cDm{i}",
                                       rkeys + [f"cQIT{i}", f"cKH{i}", f"cVb{i}", f"cGc{i}"], PT, "c", full=full)
                if not full:
                    return
                rdv = rden[i][:].rearrange("p (c x) -> p c x", c=2)
                for hh in range(2):
                    mk.op("act", "activation", out=rdv[:, :, hh], in_=Oh[hh][:, :, 64], func=AF.Abs, R=[okeys[hh]],
                          W=[f"crden{i}"])
                mk.op("dve", "tensor_scalar_max", out=rden[i][:], in0=rden[i][:], scalar1=1.0, R=[f"crden{i}"],
                      W=[f"crden{i}"])
                mk.op("dve", "reciprocal", out=rden[i][:], in_=rden[i][:], R=[f"crden{i}"], W=[f"crden{i}"])
                if dr == 0:
                    for hh in range(2):
                        mk.op("dve", "tensor_tensor", out=hview(OF[:, t, :], hh), in0=Oh[hh][:, :, 0:64],
                              in1=rdv[:, :, hh:hh + 1].to_broadcast([128, 2, 64]), op=ALU.mult,
                              R=[okeys[hh], f"crden{i}"], W=[f"cOF{t}h{hh}"])
                else:
                    for hh in range(2):
                        mk.op("dve", "tensor_tensor", out=hview(hid[i][:].rearrange("p h e -> p (h e)"), hh),
                              in0=Oh[hh][:, :, 0:64], in1=rdv[:, :, hh:hh + 1].to_broadcast([128, 2, 64]), op=ALU.mult,
                              R=[okeys[hh], f"crden{i}"], W=[f"chid{i}h{hh}"])
                    mk.op("pool", "tensor_tensor", out=tot[i][:], in0=hid[i][:].rearrange("p h e -> p (h e)"),
                          in1=OF[:, t, :], op=ALU.add, R=[f"chid{i}h0", f"chid{i}h1", f"cOF{t}h0", f"cOF{t}h1"],
                          W=[f"ctot{i}"])
                    mk.op("act", "activation", out=gt[i][:], in_=Vf[i][:, 256:512], func=AF.Sigmoid, R=[f"cVf{i}"],
                          W=[f"cgt{i}"])
                    fin(tot[i][:], f"ctot{i}", True, gt[i][:], f"cgt{i}", t)
            gens = []
            for t, full in plan(dr, last):
                gens.append(body(t, full, it))
                it += 1
            run_pipelined(gens, pipelined=PIPE_LAST or not last)
        for c in range(2):
            mk.dma("pool", YT[2, c], yTb[:, c, :], R=["yTb"], W=[f"YT2{c}"])
        mk.barrier()
        P.close()

    def make_wconv(P, l):
        wstage = P.sb([128, 8, 512], F32, "wstage")
        wcb = P.sb([128, 4096], BF16, "wcb")
        tasks = []
        for e in range(16):
            tasks.append((moe_w1[l, e].rearrange("(c p) f -> p c f", p=128), wstage[:], WALL[e][:, 0:4096], f"W1B{e}"))
            tasks.append((moe_w3[l, e].rearrange("(c p) f -> p c f", p=128), wstage[:], WALL[e][:, 4096:8192], f"W3B{e}"))
            tasks.append((moe_w2[l, e].rearrange("(c p) f -> p c f", p=128),
                          wstage[:].rearrange("p c f -> p (c f)").rearrange("p (c f) -> p c f", c=4),
                          WALL[e][:, 8192:12288], f"W2B{e}"))
        st = dict(k=0)

        def step():
            if st["k"] >= len(tasks):
                return False
            src, stg, dst, key = tasks[st["k"]]
            st["k"] += 1
            mk.dma("sp", stg, src, W=["wstage"])
            mk.ev(wcb[:], wstage[:].rearrange("p c f -> p (c f)"), R=["wstage"], W=["wcb"])
            mk.dma("pool", dst, wcb[:], R=["wcb"], W=[key])
            return True
        return step

    def mixer_d(l, last=False):
        P = Pool(nc, f"md{l}")
        LB = P.sb([128, 256], F32, "LB")
        OML = P.sb([128, 256], F32, "OML")
        if l == 0:
            use_lb = False
        else:
            use_lb = True
            dl = P.sb([128, 2, 256], F32, "dl")
            mk.dma("sp", dl[:], d_lbr, W=["dl"])
            mk.op("dve", "tensor_tensor", out=LB[:], in0=dl[:, 1, :], in1=dl[:, 0, :], op=ALU.subtract, R=["dl"], W=["LB"])
            mk.op("act", "activation", out=LB[:], in_=LB[:], func=AF.Sigmoid, R=["LB"], W=["LB"])
            mk.op("dve", "tensor_scalar", out=OML[:], in0=LB[:], scalar1=-1.0, scalar2=1.0, op0=ALU.mult, op1=ALU.add,
                  R=["LB"], W=["OML"])
        OF = P.sb([128, NT, 256], F32, "OF")
        yTb = P.sb([128, 2, T], BF16, "yTb")
        fin = make_finalize(P, 3, yTb)
        assert DNS == 4
        wconv_step = make_wconv(P, l)
        PT = [P.sb([128, 512], BF16, "PT") for _ in range(2)]
        X = [P.sb([128, 1280], F32, "X") for _ in range(2)]
        ff = [P.sb([128, 256], F32, "ff") for _ in range(2)]
        lf = [P.sb([128, 256], F32, "lf") for _ in range(2)]
        kk = [P.sb([128, 256], F32, "kk") for _ in range(2)]
        qs = [P.sb([128, 256], F32, "qs") for _ in range(2)]
        ee = [P.sb([128, 512], F32, "ee") for _ in range(2)]
        ek = [P.sb([128, 256], F32, "ek") for _ in range(2)]
        qk = [P.sb([128, 512], BF16, "qk") for _ in range(2)]
        KTs = [P.sb([128, 2, 128], BF16, "KTs") for _ in range(2)]
        QM = [[P.sb([128, 2, 5, 128], BF16, "QM") for _ in range(2)] for _ in range(2)]
        KH = [P.sb([128, DNS, 256], BF16, "KH") for _ in range(2)]
        Vb = [P.sb([128, 4, 64], BF16, "Vb") for _ in range(2)]
        gt = [P.sb([128, 256], F32, "gt") for _ in range(2)]
        tot = [P.sb([128, 256], F32, "tot") for _ in range(2)]
        red = [P.sb([128, 2, 256], F32, "red") for _ in range(2)]
        Gc = [P.sb([128, 2, DNS], F32, "Gc") for _ in range(2)]
        Sf = [[P.sb([128, DNS, 64], F32, "Sf") for _ in range(2)] for _ in range(2)]
        Sb = [[P.sb([128, DNS, 64], BF16, "Sb") for _ in range(2)] for _ in range(2)]
        qmask = C("qmask").rearrange("p (x s n) -> p x s n", x=2, s=5)
        it = 0
        for dr in range(2):
            blk = C("blkF") if dr == 0 else C("blkB")
            rem = C("aftF") if dr == 0 else C("befB")
            msk4 = C("mblkF4") if dr == 0 else C("mblkB4")
            zoff = 256 if dr == 0 else 512
            subs = list(range(DNS)) if dr == 0 else list(range(DNS - 1, -1, -1))
            for c in range(2):
                mk.op("pool", "memset", Sf[it % 2][c][:, 0, :], 0.0, W=[f"dSf{it % 2}{c}"])
                mk.op("pool", "memset", Sb[it % 2][c][:, 0, :], 0.0, W=[f"dSb{it % 2}{c}"])
            def body(t, full, it):
                i = it % 2
                mk.dma("sp", X[i][:], TM[t * 128:(t + 1) * 128, 1024:2304], R=["TM"], W=[f"dX{i}"])
                wconv_step()
                mk.op("act", "activation", out=ff[i][:], in_=X[i][:, zoff:zoff + 256], func=AF.Sigmoid, R=[f"dX{i}"],
                      W=[f"dff{i}"])
                if use_lb:
                    mk.op("dve", "tensor_tensor", out=ff[i][:], in0=ff[i][:], in1=OML[:], op=ALU.mult, R=[f"dff{i}", "OML"],
                          W=[f"dff{i}"])
                    mk.op("dve", "tensor_tensor", out=ff[i][:], in0=ff[i][:], in1=LB[:], op=ALU.add, R=[f"dff{i}", "LB"],
                          W=[f"dff{i}"])
                mk.op("act", "activation", out=lf[i][:], in_=ff[i][:], func=AF.Ln, R=[f"dff{i}"], W=[f"dlf{i}"])
                mk.op("pool", "tensor_scalar", out=kk[i][:], in0=ff[i][:], scalar1=-1.0, scalar2=1.0, op0=ALU.mult,
                      op1=ALU.add, R=[f"dff{i}"], W=[f"dkk{i}"])
                if full:
                    mk.op("pe", "matmul", PS[5][:, 0:256], lhsT=blk, rhs=lf[i][:], start=True, stop=True,
                          R=["cst", f"dlf{i}"], W=["ps5"])
                mk.op("pe", "matmul", PS[5][:, 256:512], lhsT=rem, rhs=lf[i][:], start=True, stop=True,
                      R=["cst", f"dlf{i}"], W=["ps5"])
                for c in range(2):
                    mk.op("pe", "matmul", PS[1][:, 384 + c * DNS:384 + (c + 1) * DNS], lhsT=lf[i][:, c * 128:(c + 1) * 128],
                          rhs=C("subm"), start=True, stop=True, R=["cst", f"dlf{i}"], W=["ps1g"])
                mk.op("act", "activation", out=Gc[i][:].rearrange("p c s -> p (c s)"), in_=PS[1][:, 384:384 + 2 * DNS],
                      func=AF.Exp, R=["ps1g"], W=[f"dGc{i}"])
                if full:
                    mk.op("act", "activation", out=ee[i][:], in_=PS[5][:, :], func=AF.Exp, R=["ps5"], W=[f"dee{i}"])
                    mk.op("act", "activation", out=ek[i][:], in_=PS[5][:, 0:256], func=AF.Exp, scale=-1.0, R=["ps5"],
                          W=[f"dek{i}"])
                    mk.op("act", "activation", out=qs[i][:], in_=X[i][:, 0:256], func=AF.Silu, R=[f"dX{i}"],
                          W=[f"dqs{i}"])
                    mk.op("dve", "tensor_tensor", out=qk[i][:, 0:256], in0=qs[i][:], in1=ee[i][:, 0:256], op=ALU.mult,
                          R=[f"dqs{i}", f"dee{i}"], W=[f"dqk{i}a"])
                    mk.op("dve", "tensor_tensor", out=qk[i][:, 256:512], in0=kk[i][:], in1=ek[i][:], op=ALU.mult,
                          R=[f"dkk{i}", f"dek{i}"], W=[f"dqk{i}c"])
                else:
                    mk.op("act", "activation", out=ee[i][:, 256:512], in_=PS[5][:, 256:512], func=AF.Exp, R=["ps5"],
                          W=[f"dee{i}"])
                for s_ in range(DNS):
                    mk.op("dve", "scalar_tensor_tensor", out=KH[i][:, s_, :], in0=kk[i][:], scalar=C("subm")[:, s_:s_ + 1],
                          in1=ee[i][:, 256:512], op0=ALU.mult, op1=ALU.mult, R=[f"dkk{i}", f"dee{i}", "cst"],
                          W=[f"dKH{i}s{s_}"])
                mk.op("pool", "tensor_copy", out=Vb[i][:], in_=X[i][:, 768:1024].rearrange("p (h e) -> p h e", h=4),
                      R=[f"dX{i}"], W=[f"dVb{i}"])
                if full:
                    for j in range(4):
                        mk.op("pe", "transpose", out=PQ[0][:, (i * 4 + j) * 128:(i * 4 + j + 1) * 128],
                              in_=qk[i][:, j * 128:(j + 1) * 128], identity=identb[:], R=[f"dqk{i}a", f"dqk{i}c", "identb"],
                              W=[f"pq0d{i}"])
                    mk.op("act", "activation", out=KTs[i][:].rearrange("p c n -> p (c n)"),
                          in_=PQ[0][:, i * 512 + 256:i * 512 + 512], func=AF.Copy, R=[f"pq0d{i}"], W=[f"dKT{i}"])
                    for c in range(2):
                        mk.op("dve", "tensor_tensor", out=QM[i][c][:].rearrange("p x s n -> p (x s) n"),
                              in0=PQ[0][:, i * 512 + c * 128:i * 512 + (c + 1) * 128].unsqueeze(1).to_broadcast([128, 10, 128]),
                              in1=qmask.rearrange("p x s n -> p (x s) n"), op=ALU.mult, R=[f"pq0d{i}", "cst"],
                              W=[f"dQM{i}{c}"])
                yield
                par = it % 2
                KV4 = PS[4][:, :].rearrange("p (k c e) -> p k c e", k=DNS, c=2)
                for k_, s_ in enumerate(subs):
                    for h in range(4):
                        c, hh = h // 2, h % 2
                        mk.op("pe", "matmul", KV4[hh * 64:(hh + 1) * 64, k_, c, :], lhsT=KH[i][:, s_, h * 64:(h + 1) * 64],
                              rhs=Vb[i][:, h, :], start=True, stop=True, R=[f"dKH{i}s{s_}", f"dVb{i}"], W=["ps4kv"])
                for k_, s_ in enumerate(subs):
                    for c in range(2):
                        src = Sf[par][c][:, k_, :]
                        if k_ < DNS - 1:
                            dst, dk, db, dbk = Sf[par][c][:, k_ + 1, :], f"dSf{par}{c}", Sb[par][c][:, k_ + 1, :], f"dSb{par}{c}"
                        else:
                            dst, dk, db, dbk = (Sf[1 - par][c][:, 0, :], f"dSf{1 - par}{c}", Sb[1 - par][c][:, 0, :],
                                                f"dSb{1 - par}{c}")
                        mk.op("dve", "scalar_tensor_tensor", out=dst, in0=src, scalar=Gc[i][:, c, s_:s_ + 1],
                              in1=KV4[:, k_, c, :], op0=ALU.mult, op1=ALU.add,
                              R=[f"dSf{par}{c}", "ps4kv", f"dGc{i}"], W=[dk])
                        mk.op("act", "activation", out=db, in_=dst, func=AF.Copy, R=[dk], W=[dbk])
                if full:
                    for h in range(4):
                        c, hh = h // 2, h % 2
                        mk.op("pe", "matmul", PS[0][:, h * 128:(h + 1) * 128], lhsT=KTs[i][:, c, :], rhs=QM[i][c][:, hh, 4, :],
                              start=True, stop=True, R=[f"dKT{i}", f"dQM{i}{c}"], W=["ps0"])
                    mk.op("dve", "tensor_tensor", out=PT[i][:], in0=PS[0][:, :], in1=msk4, op=ALU.mult, R=["ps0", "cst"],
                          W=[f"dPT{i}"])
                    for h in range(4):
                        mk.op("pe", "matmul", PS[1][:, h * 64:(h + 1) * 64], lhsT=PT[i][:, h * 128:(h + 1) * 128],
                              rhs=Vb[i][:, h, :], start=True, stop=True, R=[f"dPT{i}", f"dVb{i}"], W=["ps1i"])
                for k_, s_ in enumerate(subs):
                    bank = PS[2 + s_ // 2]
                    for h in (range(4) if full else ()):
                        c, hh = h // 2, h % 2
                        col = ((s_ % 2) * 4 + h) * 64
                        mk.op("pe", "matmul", bank[:, col:col + 64], lhsT=QM[i][c][:, hh, s_, :], rhs=Sb[par][c][:, k_, :],
                              start=True, stop=True, R=[f"dQM{i}{c}", f"dSb{par}{c}"], W=[f"ps{2 + s_ // 2}"])
                if not full:
                    return
                for b_ in range(2):
                    mk.op("dve", "tensor_reduce", out=red[i][:, b_, :],
                          in_=PS[2 + b_][:, :].rearrange("p (s x) -> p x s", s=2), axis=AX.X, op=ALU.add,
                          R=[f"ps{2 + b_}"], W=[f"dred{i}{b_}"])
                mk.op("dve", "tensor_tensor", out=tot[i][:], in0=PS[1][:, 0:256], in1=red[i][:, 0, :], op=ALU.add,
                      R=["ps1i", f"dred{i}0"], W=[f"dtot{i}"])
                if dr == 0:
                    mk.op("pool", "tensor_tensor", out=OF[:, t, :], in0=tot[i][:], in1=red[i][:, 1, :], op=ALU.add,
                          R=[f"dtot{i}", f"dred{i}1"], W=[f"dOF{t}"])
                else:
                    mk.op("pool", "tensor_tensor", out=tot[i][:], in0=tot[i][:], in1=red[i][:, 1, :], op=ALU.add,
                          R=[f"dtot{i}", f"dred{i}1"], W=[f"dtot{i}"])
                    mk.op("dve", "tensor_tensor", out=tot[i][:], in0=tot[i][:], in1=OF[:, t, :], op=ALU.add,
                          R=[f"dtot{i}", f"dOF{t}"], W=[f"dtot{i}"])
                    mk.op("act", "activation", out=gt[i][:], in_=X[i][:, 1024:1280], func=AF.Silu, R=[f"dX{i}"],
                          W=[f"dgt{i}"])
                    fin(tot[i][:], f"dtot{i}", False, gt[i][:], f"dgt{i}", t)
            gens = []
            for t, full in plan(dr, last):
                gens.append(body(t, full, it))
                it += 1
            run_pipelined(gens, pipelined=PIPE_LAST or not last)
        while wconv_step():
            pass
        for c in range(2):
            mk.dma("pool", YT[3, c], yTb[:, c, :], R=["yTb"], W=[f"YT3{c}"])
        mk.barrier()
        P.close()

    NSMAX = (T + 4 * (SLOT - 1)) // SLOT
    H2U = dscr("H2U", [T, D], BF16)
    HS = dscr("HS", [NSMAX * SLOT, D], BF16)
    GWS = dscr("GWS", [NSMAX * SLOT, 4])
    OS = dscr("OS", [NSMAX * SLOT, D])
    WALL = dscr("WALL", [16, 128, 3 * 4096], BF16)

    def merge_moe_phase(l, last, tiles=None):
        if tiles is None:
            tiles = list(OUT_T) if last else list(range(NT))
        PO = Pool(nc, f"mo{l}")
        GOH = PO.sb([128, NT, 4], F32, "GOH")
        GW = PO.sb([128, NT, 4], F32, "GW")
        merge_part(l, tiles, GOH, GW)
        if DBG.get("moe", True):
            moe_sparse(l, tiles, GOH, GW)
        PO.close()

    def merge_part(l, tiles, GOH, GW):
        P = Pool(nc, f"mm{l}")
        wbr = P.sb([128, 8, D], BF16, "wbr")
        wo = P.sb([128, 8, D], BF16, "wo")
        wstage = P.sb([128, 8, 512], F32, "wstage")
        for half in range(2):
            mk.dma("sp", wstage[:], w_branch[l].rearrange("n (c p) f -> p (n c) f", p=128)[:, :, half * 512:(half + 1) * 512],
                   W=["wstage"])
            mk.ev(wbr[:, :, half * 512:(half + 1) * 512], wstage[:], R=["wstage"], W=["wbr"])
        for half in range(2):
            mk.dma("sp", wstage[:], w_out[l].rearrange("(c p) f -> p c f", p=128)[:, :, half * 512:(half + 1) * 512],
                   W=["wstage"])
            mk.ev(wo[:, :, half * 512:(half + 1) * 512], wstage[:], R=["wstage"], W=["wo"])
        wgr = P.sb([128, 8, 20], F32, "wgr")
        bgr = P.sb([1, 20], F32, "bgr")
        mk.dma("sp", wgr[:], moe_wgr[l].rearrange("(c p) f -> p c f", p=128), W=["wgr"])
        mk.dma("sp", bgr[:], moe_bgr[l], W=["bgr"])
        identf = C("ident")
        ones = C("ones")
        norm = make_norm(P, 2)
        xnew = [P.sb([128, D], F32, "xnew") for _ in range(2)]
        h2Tt = [P.sb([128, 8, 128], BF16, "h2Tt") for _ in range(2)]
        h2tok = [P.sb([128, D], BF16, "h2tok") for _ in range(2)]
        yt = [P.sb([128, 8, 128], BF16, "yt") for _ in range(2)]
        mg = [P.sb([128, 4096], BF16, "mg") for _ in range(2)]
        xt = [P.sb([128, D], F32, "xt") for _ in range(2)]
        zz = [P.sb([128, D], F32, "zz") for _ in range(2)]
        zt = [P.sb([128, D], F32, "zt") for _ in range(2)]
        zb = [P.sb([128, D], BF16, "zb") for _ in range(2)]
        zT = [P.sb([128, 8, 128], BF16, "zT") for _ in range(2)]
        rt = {"gs": P.sb([128, 1], F32, "rtgs")}
        LG = P.sb([128, NT, 20], F32, "LG")
        nit = 0
        def tile_body(t, i):
                j = cond_of(t)
                cols = slice(t * 128, (t + 1) * 128)
                h2f = zz[i]
                h2fT = zt[i][:, :].rearrange("p (c n) -> p c n", c=8)
                mk.dma("sp", yt[i][:], YT[:, :, :, cols].rearrange("n c p t -> p (n c) t"),
                       R=[f"YT{n}{c}" for n in range(4) for c in range(2)], W=[f"yt{i}"])
                mk.dma("sp", mg[i][:], MG[cols, :], R=["MG"], W=[f"mg{i}"])
                mk.dma("sp", xt[i][:], XR[cols, :], R=[f"XR{t}"], W=[f"mxt{i}"])
                for n in range(4):
                    for cb in range(2):
                        pb = (n * 2 + cb) % 2
                        for c in range(2):
                            mk.op("pe", "matmul", PS[pb][:, :], lhsT=yt[i][:, n * 2 + c, :],
                                  rhs=wbr[:, n * 2 + c, cb * 512:(cb + 1) * 512], start=(c == 0), stop=(c == 1),
                                  R=[f"yt{i}", "wbr"], W=[f"ps{pb}"])
                        dst = zz[i] if n == 0 else zt[i]
                        dkey = f"zz{i}" if n == 0 else f"zt{i}"
                        mk.op("dve", "tensor_tensor", out=dst[:, cb * 512:(cb + 1) * 512], in0=PS[pb][:, :],
                              in1=mg[i][:, n * 1024 + cb * 512:n * 1024 + (cb + 1) * 512], op=ALU.mult,
                              R=[f"ps{pb}", f"mg{i}"], W=[dkey])
                        if n > 0:
                            mk.op("pool", "tensor_tensor", out=zz[i][:, cb * 512:(cb + 1) * 512],
                                  in0=zz[i][:, cb * 512:(cb + 1) * 512], in1=zt[i][:, cb * 512:(cb + 1) * 512], op=ALU.add,
                                  R=[f"zz{i}", f"zt{i}"], W=[f"zz{i}"])
                mk.op("act", "activation", out=zb[i][:], in_=zz[i][:], func=AF.Copy, R=[f"zz{i}"], W=[f"zb{i}"])
                for c in range(8):
                    mk.op("pe", "transpose", out=PQ[1][:, c * 128:(c + 1) * 128], in_=zb[i][:, c * 128:(c + 1) * 128],
                          identity=identb[:], R=[f"zb{i}", "identb"], W=["pq1"])
                mk.ev(zT[i][:].rearrange("p c n -> p (c n)"), PQ[1][:, :], R=["pq1"], W=[f"zT{i}"])
                for cb in range(2):
                    pb = 2 + cb
                    for c in range(8):
                        mk.op("pe", "matmul", PS[pb][:, :], lhsT=zT[i][:, c, :], rhs=wo[:, c, cb * 512:(cb + 1) * 512],
                              start=(c == 0), stop=(c == 7), R=[f"zT{i}", "wo"], W=[f"ps{pb}"])
                    mk.op("dve", "tensor_tensor", out=zt[i][:, cb * 512:(cb + 1) * 512], in0=PS[pb][:, :],
                          in1=GB[:, j, 0, cb * 512:(cb + 1) * 512], op=ALU.mult, R=[f"ps{pb}", "GB"], W=[f"zt{i}"])
                    mk.op("pool", "tensor_tensor", out=xnew[i][:, cb * 512:(cb + 1) * 512], in0=zt[i][:, cb * 512:(cb + 1) * 512],
                          in1=xt[i][:, cb * 512:(cb + 1) * 512], op=ALU.add, R=[f"zt{i}", f"mxt{i}"], W=[f"xnew{i}"])
                mk.dma("pool", XR[cols, :], xnew[i][:], R=[f"xnew{i}"], W=[f"XR{t}"])
                yield
                norm(xnew[i][:], f"xnew{i}", j, 3, 2, h2Tt[i][:], f"h2Tt{i}")
                for c in range(8):
                    mk.op("pe", "transpose", out=PQ[1][:, c * 128:(c + 1) * 128], in_=h2Tt[i][:, c, :], identity=identb[:],
                          R=[f"h2Tt{i}", "identb"], W=["pq1"])
                mk.ev(h2tok[i][:], PQ[1][:, :], R=["pq1"], W=[f"h2tok{i}"])
                mk.dma("pool", H2U[cols, :], h2tok[i][:], R=[f"h2tok{i}"], W=[f"H2U{t}"])
                ssr = rt["gs"]
                mk.op("act", "activation", out=h2f[:], in_=xnew[i][:], func=AF.Square, accum_out=ssr[:],
                      R=[f"xnew{i}"], W=[f"zz{i}", "rgs"])
                mk.op("act", "activation", out=ssr[:], in_=ssr[:], func=AF.Sqrt, scale=1.0 / D, bias=EPS, R=["rgs"], W=["rgs"])
                mk.op("dve", "reciprocal", out=ssr[:], in_=ssr[:], R=["rgs"], W=["rgs"])
                mk.op("dve", "tensor_scalar", out=h2f[:], in0=xnew[i][:], scalar1=ssr[:, 0:1], scalar2=None,
                      op0=ALU.mult, R=[f"xnew{i}", "rgs", f"zz{i}"], W=[f"zz{i}"])
                for half in range(2):
                    for c4 in range(4):
                        c = half * 4 + c4
                        mk.op("pe", "transpose", out=PS[5][:, c4 * 128:(c4 + 1) * 128], in_=h2f[:, c * 128:(c + 1) * 128],
                              identity=identf, R=[f"zz{i}", "cst"], W=["ps5"])
                    mk.op("dve", "tensor_tensor", out=h2fT[:, half * 4:(half + 1) * 4, :],
                          in0=PS[5][:, :].rearrange("p (c n) -> p c n", c=4),
                          in1=MODC[:, 3, half * 4:(half + 1) * 4, j:j + 1].to_broadcast([128, 4, 128]), op=ALU.mult,
                          R=["ps5", "MODC"], W=[f"zt{i}"])
                    mk.op("pool", "tensor_tensor", out=h2fT[:, half * 4:(half + 1) * 4, :],
                          in0=h2fT[:, half * 4:(half + 1) * 4, :],
                          in1=MODC[:, 2, half * 4:(half + 1) * 4, j:j + 1].to_broadcast([128, 4, 128]), op=ALU.add,
                          R=[f"zt{i}", "MODC"], W=[f"zt{i}"])
                for c in range(8):
                    mk.op("pe", "matmul", PS[4][:, 0:20], lhsT=h2fT[:, c, :], rhs=wgr[:, c, :], start=(c == 0), stop=False,
                          R=[f"zt{i}", "wgr"], W=["ps4r"])
                mk.op("pe", "matmul", PS[4][:, 0:20], lhsT=ones[0:1, :], rhs=bgr[0:1, :], start=False, stop=True,
                      R=["cst", "bgr"], W=["ps4r"])
                mk.op("act", "activation", out=LG[:, t, :], in_=PS[4][:, 0:20], func=AF.Copy, R=["ps4r"], W=[f"LG{t}"])
        gens = []
        for t in tiles:
            gens.append(tile_body(t, nit % 2))
            nit += 1
        run_pipelined(gens)
        nt, t0 = len(tiles), tiles[0]
        lk = [f"LG{t}" for t in tiles]
        L3 = LG[:, t0:t0 + nt, :]
        gohv = GOH[:, t0:t0 + nt, :]
        gwv = GW[:, t0:t0 + nt, :]
        r1 = {k: P.sb([128, nt], F32, "r1" + k) for k in ("gm", "gs", "m1", "m2", "w1", "w2")}
        r4 = {k: P.sb([128, nt, 4], F32, "r4" + k) for k in ("ge", "el", "tmp", "oh1", "e2", "oh2")}

        def bc(a):
            return a[:].unsqueeze(2).to_broadcast([128, nt, 4])
        mk.op("dve", "tensor_reduce", out=r1["gm"][:], in_=L3[:, :, 0:4], axis=AX.X, op=ALU.max, R=lk, W=["rgm"])
        mk.op("dve", "tensor_tensor", out=gohv, in0=L3[:, :, 0:4], in1=bc(r1["gm"]), op=ALU.is_ge, R=lk + ["rgm"], W=["GOHall"])
        mk.op("dve", "tensor_tensor", out=r4["ge"][:], in0=L3[:, :, 0:4], in1=bc(r1["gm"]), op=ALU.subtract, R=lk + ["rgm"],
              W=["rge"])
        mk.op("act", "activation", out=r4["ge"][:], in_=r4["ge"][:], func=AF.Exp, R=["rge"], W=["rge"])
        mk.op("dve", "tensor_reduce", out=r1["gs"][:], in_=r4["ge"][:], axis=AX.X, op=ALU.add, R=["rge"], W=["rgs2"])
        mk.op("dve", "reciprocal", out=r1["gs"][:], in_=r1["gs"][:], R=["rgs2"], W=["rgs2"])
        mk.op("dve", "tensor_tensor", out=r4["el"][:], in0=L3[:, :, 4:8], in1=gohv[:, :, 0:1].to_broadcast([128, nt, 4]),
              op=ALU.mult, R=lk + ["GOHall"], W=["rel"])
        for g in range(1, 4):
            mk.op("dve", "tensor_tensor", out=r4["tmp"][:], in0=L3[:, :, 4 + 4 * g:8 + 4 * g],
                  in1=gohv[:, :, g:g + 1].to_broadcast([128, nt, 4]), op=ALU.mult, R=lk + ["GOHall"], W=["rtmp"])
            mk.op("dve", "tensor_tensor", out=r4["el"][:], in0=r4["el"][:], in1=r4["tmp"][:], op=ALU.add, R=["rel", "rtmp"],
                  W=["rel"])
        mk.op("dve", "tensor_reduce", out=r1["m1"][:], in_=r4["el"][:], axis=AX.X, op=ALU.max, R=["rel"], W=["rm1"])
        mk.op("dve", "tensor_tensor", out=r4["oh1"][:], in0=r4["el"][:], in1=bc(r1["m1"]), op=ALU.is_ge, R=["rel", "rm1"],
              W=["roh1"])
        mk.op("dve", "scalar_tensor_tensor", out=r4["e2"][:], in0=r4["oh1"][:], scalar=-1e30, in1=r4["el"][:], op0=ALU.mult,
              op1=ALU.add, R=["roh1", "rel"], W=["re2"])
        mk.op("dve", "tensor_reduce", out=r1["m2"][:], in_=r4["e2"][:], axis=AX.X, op=ALU.max, R=["re2"], W=["rm2"])
        mk.op("dve", "tensor_tensor", out=r4["oh2"][:], in0=r4["e2"][:], in1=bc(r1["m2"]), op=ALU.is_ge, R=["re2", "rm2"],
              W=["roh2"])
        mk.op("dve", "tensor_tensor", out=r1["w1"][:], in0=r1["m2"][:], in1=r1["m1"][:], op=ALU.subtract, R=["rm1", "rm2"],
              W=["rw1"])
        mk.op("act", "activation", out=r1["w1"][:], in_=r1["w1"][:], func=AF.Exp, R=["rw1"], W=["rw1"])
        mk.op("dve", "tensor_scalar_add", out=r1["w1"][:], in0=r1["w1"][:], scalar1=1.0, R=["rw1"], W=["rw1"])
        mk.op("dve", "reciprocal", out=r1["w1"][:], in_=r1["w1"][:], R=["rw1"], W=["rw1"])
        mk.op("dve", "tensor_tensor", out=r1["w1"][:], in0=r1["w1"][:], in1=r1["gs"][:], op=ALU.mult, R=["rw1", "rgs2"],
              W=["rw1"])
        mk.op("dve", "tensor_tensor", out=r1["w2"][:], in0=r1["gs"][:], in1=r1["w1"][:], op=ALU.subtract, R=["rw1", "rgs2"],
              W=["rw2"])
        mk.op("dve", "tensor_tensor", out=gwv, in0=r4["oh1"][:], in1=bc(r1["w1"]), op=ALU.mult, R=["roh1", "rw1"],
              W=["GWall"])
        mk.op("dve", "tensor_tensor", out=r4["tmp"][:], in0=r4["oh2"][:], in1=bc(r1["w2"]), op=ALU.mult, R=["roh2", "rw2"],
              W=["rtmp"])
        mk.op("dve", "tensor_tensor", out=gwv, in0=gwv, in1=r4["tmp"][:], op=ALU.add, R=["GWall", "rtmp"], W=["GWall"])
        mk.barrier()
        P.close()

    def moe_sparse(l, tiles, GOH, GW):
        P = Pool(nc, f"ms{l}")
        I32 = mybir.dt.int32
        nt, t0 = len(tiles), tiles[0]
        N = nt * 128
        NS = (N + 4 * (SLOT - 1)) // SLOT
        TPS = SLOT // 128
        Gv = GOH[:, t0:t0 + nt, :]
        gkeys = ["GOHall"]
        cnt = P.sb([128, nt, 4], F32, "cnt")
        inc = P.sb([128, nt, 4], F32, "inc")
        A = P.sb([128, nt, 4], F32, "A")
        tot = P.sb([128, 4], F32, "tot")
        cmp = P.sb([128, 16], F32, "cmp")
        nsl = P.sb([128, 4], F32, "nsl")
        base = P.sb([128, 4], F32, "base")
        posf = P.sb([128, nt], F32, "posf")
        POSI = P.sb([128, nt], I32, "POSI")
        gkb = P.sb([128, NS], F32, "gkb")
        gtmp = P.sb([128, NS], F32, "gtmp")
        widf = P.sb([128, NS, 4], F32, "widf")
        WIDX = P.sb([128, NS, 4], I32, "WIDX")
        Gf = Gv.rearrange("p t g -> p (t g)")
        mk.op("pe", "matmul", PS[0][:, 0:nt * 4], lhsT=C("ltS"), rhs=Gf, start=True, stop=True, R=gkeys + ["cst"], W=["ps0"])
        mk.op("pe", "matmul", PS[1][:, 0:nt * 4], lhsT=C("ones"), rhs=Gf, start=True, stop=True, R=gkeys + ["cst"], W=["ps1"])
        mk.op("act", "activation", out=cnt[:].rearrange("p t g -> p (t g)"), in_=PS[1][:, 0:nt * 4], func=AF.Copy,
              R=["ps1"], W=["scnt"])
        for g in range(4):
            mk.op("dve", "tensor_tensor_scan", out=inc[:, :, g], data0=C("ones")[:, 0:nt], data1=cnt[:, :, g], initial=0.0,
                  op0=ALU.mult, op1=ALU.add, R=["scnt", "cst"], W=[f"sinc{g}"])
        ik = [f"sinc{g}" for g in range(4)]
        mk.op("dve", "tensor_copy", out=tot[:], in_=inc[:, nt - 1, :], R=ik, W=["stot"])
        for g in range(4):
            mk.op("dve", "tensor_scalar", out=cmp[:], in0=C("thrS"), scalar1=tot[:, g:g + 1], scalar2=None, op0=ALU.is_lt,
                  R=["stot", "cst"], W=["scmp"])
            mk.op("dve", "tensor_reduce", out=nsl[:, g:g + 1], in_=cmp[:], axis=AX.X, op=ALU.add, R=["scmp"], W=["snsl"])
        mk.op("dve", "tensor_scalar", out=nsl[:], in0=nsl[:], scalar1=float(SLOT), scalar2=None, op0=ALU.mult,
              R=["snsl"], W=["snsl"])
        mk.op("pool", "memset", base[:], 0.0, W=["sbase"])
        for g in range(1, 4):
            mk.op("dve", "tensor_tensor", out=base[:, g:g + 1], in0=base[:, g - 1:g], in1=nsl[:, g - 1:g], op=ALU.add,
                  R=["sbase", "snsl"], W=["sbase"])
        mk.op("dve", "tensor_tensor", out=A[:], in0=inc[:], in1=cnt[:], op=ALU.subtract, R=ik + ["scnt"], W=["sA"])
        mk.op("dve", "tensor_tensor", out=A[:].rearrange("p t g -> p (t g)"), in0=A[:].rearrange("p t g -> p (t g)"),
              in1=PS[0][:, 0:nt * 4], op=ALU.add, R=["sA", "ps0"], W=["sA"])
        mk.op("dve", "tensor_tensor", out=A[:], in0=A[:], in1=base[:].unsqueeze(1).to_broadcast([128, nt, 4]), op=ALU.add,
              R=["sA", "sbase"], W=["sA"])
        mk.op("dve", "tensor_tensor", out=A[:], in0=A[:], in1=Gv, op=ALU.mult, R=["sA"] + gkeys, W=["sA"])
        mk.op("dve", "tensor_reduce", out=posf[:], in_=A[:], axis=AX.X, op=ALU.add, R=["sA"], W=["sposf"])
        mk.op("dve", "tensor_copy", out=POSI[:], in_=posf[:], R=["sposf"], W=["POSI"])
        mk.op("pool", "memset", gkb[:], 0.0, W=["sgkb"])
        for g in range(1, 4):
            mk.op("dve", "tensor_scalar", out=gtmp[:], in0=C("kS")[:, 0:NS], scalar1=base[:, g:g + 1], scalar2=None,
                  op0=ALU.is_ge, R=["sbase", "cst"], W=["sgtmp"])
            mk.op("dve", "tensor_tensor", out=gkb[:], in0=gkb[:], in1=gtmp[:], op=ALU.add, R=["sgkb", "sgtmp"], W=["sgkb"])
        mk.op("dve", "tensor_scalar", out=gkb[:], in0=gkb[:], scalar1=512.0, scalar2=None, op0=ALU.mult, R=["sgkb"],
              W=["sgkb"])
        for j in range(4):
            mk.op("dve", "tensor_scalar", out=widf[:, :, j], in0=gkb[:], scalar1=C("jp")[:, j:j + 1], scalar2=None,
                  op0=ALU.add, R=["sgkb", "cst"], W=["swidf"])
        mk.op("dve", "tensor_copy", out=WIDX[:], in_=widf[:], R=["swidf"], W=["WIDX"])
        hb = [P.sb([128, D], BF16, "hb") for _ in range(2)]
        for ti, t in enumerate(tiles):
            i = ti % 2
            mk.dma("sp", hb[i][:], H2U[t * 128:(t + 1) * 128, :], R=[f"H2U{t}"], W=[f"hb{i}"])
            mk.idma(HS[:, :], bass.IndirectOffsetOnAxis(ap=POSI[:, ti:ti + 1], axis=0), hb[i][:, :], None,
                    R=[f"hb{i}", "POSI"], W=[f"HSs{ti}"])
            mk.idma(GWS[:, :], bass.IndirectOffsetOnAxis(ap=POSI[:, ti:ti + 1], axis=0), GW[:, t, :], None,
                    R=["GWall", "POSI"], W=[f"GWs{ti}"])
        hsk = [f"HSs{ti}" for ti in range(nt)]
        gwk = [f"GWs{ti}" for ti in range(nt)]
        hs = [P.sb([128, TPS, D], BF16, "hs") for _ in range(2)]
        hTs = [P.sb([128, 8, SLOT], BF16, "hTs") for _ in range(2)]
        gws = [P.sb([128, TPS, 4], F32, "gws") for _ in range(2)]
        acc = [P.sb([128, TPS, D], F32, "acc") for _ in range(2)]
        wall = [P.sb([128, 3 * 4096], BF16, "wall") for _ in range(2)]
        w1b = [w[:, 0:4096].rearrange("p (c f) -> p c f", c=8) for w in wall]
        w3b = [w[:, 4096:8192].rearrange("p (c f) -> p c f", c=8) for w in wall]
        w2b = [w[:, 8192:12288].rearrange("p (c f) -> p c f", c=4) for w in wall]
        sl = [P.sb([128, SLOT], F32, "sl") for _ in range(2)]
        actT = [P.sb([128, 4, SLOT], BF16, "actT") for _ in range(2)]
        Wt = WALL.rearrange("e p f -> (e p) f")
        wkeys = [f"W{a}B{e}" for a in (1, 2, 3) for e in range(16)]
        nw = 0
        for k in range(NS):
            si = k % 2
            mk.dma("sp", hs[si][:], HS[k * SLOT:(k + 1) * SLOT, :].rearrange("(q p) f -> p q f", p=128), R=hsk,
                   W=[f"hs{si}"])
            mk.dma("sp", gws[si][:], GWS[k * SLOT:(k + 1) * SLOT, :].rearrange("(q p) f -> p q f", p=128), R=gwk,
                   W=[f"gws{si}"])
            for q in range(TPS):
                pq = q % 2
                for c in range(8):
                    mk.op("pe", "transpose", out=PQ[pq][:, c * 128:(c + 1) * 128], in_=hs[si][:, q, c * 128:(c + 1) * 128],
                          identity=identb[:], R=[f"hs{si}", "identb"], W=[f"pq{pq}"])
                mk.ev(hTs[si][:, :, q * 128:(q + 1) * 128], PQ[pq][:, :].rearrange("p (c n) -> p c n", c=8), R=[f"pq{pq}"],
                      W=[f"hTs{si}"])
            for j in range(4):
                wi = nw % 2
                nw += 1
                ioff = bass.IndirectOffsetOnAxis(ap=WIDX[:, k, j:j + 1], axis=0)
                mk.idma(wall[wi][:, :], None, Wt, ioff, R=["WIDX"] + wkeys, W=[f"w1b{wi}", f"w3b{wi}", f"w2b{wi}"])
                for fcn in range(4):
                    for kk_ in range(8):
                        mk.op("pe", "matmul", PS[0][:, 0:SLOT], lhsT=w1b[wi][:, kk_, fcn * 128:(fcn + 1) * 128],
                              rhs=hTs[si][:, kk_, :], start=(kk_ == 0), stop=(kk_ == 7), R=[f"w1b{wi}", f"hTs{si}"], W=["ps0"])
                    for kk_ in range(8):
                        mk.op("pe", "matmul", PS[1][:, 0:SLOT], lhsT=w3b[wi][:, kk_, fcn * 128:(fcn + 1) * 128],
                              rhs=hTs[si][:, kk_, :], start=(kk_ == 0), stop=(kk_ == 7), R=[f"w3b{wi}", f"hTs{si}"], W=["ps1"])
                    s2 = fcn % 2
                    mk.op("act", "activation", out=sl[s2][:], in_=PS[0][:, 0:SLOT], func=AF.Silu, R=["ps0"], W=[f"sl{s2}"])
                    mk.op("dve", "tensor_tensor", out=actT[wi][:, fcn, :], in0=sl[s2][:], in1=PS[1][:, 0:SLOT], op=ALU.mult,
                          R=[f"sl{s2}", "ps1"], W=[f"actT{wi}"])
                for q in range(TPS):
                    for cb in range(2):
                        pb = 2 + cb
                        for fcn in range(4):
                            mk.op("pe", "matmul", PS[pb][:, :], lhsT=actT[wi][:, fcn, q * 128:(q + 1) * 128],
                                  rhs=w2b[wi][:, fcn, cb * 512:(cb + 1) * 512], start=(fcn == 0), stop=(fcn == 3),
                                  R=[f"actT{wi}", f"w2b{wi}"], W=[f"ps{pb}"])
                        if j == 0:
                            mk.op("dve", "tensor_scalar", out=acc[si][:, q, cb * 512:(cb + 1) * 512], in0=PS[pb][:, :],
                                  scalar1=gws[si][:, q, j:j + 1], scalar2=None, op0=ALU.mult,
                                  R=[f"ps{pb}", f"gws{si}"], W=[f"sacc{si}"])
                        else:
                            mk.op("dve", "scalar_tensor_tensor", out=acc[si][:, q, cb * 512:(cb + 1) * 512], in0=PS[pb][:, :],
                                  scalar=gws[si][:, q, j:j + 1], in1=acc[si][:, q, cb * 512:(cb + 1) * 512], op0=ALU.mult,
                                  op1=ALU.add, R=[f"ps{pb}", f"gws{si}", f"sacc{si}"], W=[f"sacc{si}"])
            mk.dma("sp", OS[k * SLOT:(k + 1) * SLOT, :].rearrange("(q p) f -> p q f", p=128), acc[si][:], R=[f"sacc{si}"],
                   W=[f"OS{k}"])
        osk = [f"OS{k}" for k in range(NS)]
        og = [P.sb([128, D], F32, "og") for _ in range(2)]
        xt = [P.sb([128, D], F32, "xt") for _ in range(2)]
        for ti, t in enumerate(tiles):
            i = ti % 2
            j = cond_of(t)
            mk.idma(og[i][:, :], None, OS[:, :], bass.IndirectOffsetOnAxis(ap=POSI[:, ti:ti + 1], axis=0), R=osk + ["POSI"],
                    W=[f"og{i}"])
            mk.dma("sp", xt[i][:], XR[t * 128:(t + 1) * 128, :], R=[f"XR{t}"], W=[f"ext{i}"])
            mk.op("pool", "tensor_tensor", out=og[i][:], in0=og[i][:], in1=GB[:, j, 1, :], op=ALU.mult, R=[f"og{i}", "GB"],
                  W=[f"og{i}"])
            mk.op("dve", "tensor_tensor", out=og[i][:], in0=og[i][:], in1=xt[i][:], op=ALU.add, R=[f"og{i}", f"ext{i}"],
                  W=[f"og{i}"])
            mk.dma("sp", XR[t * 128:(t + 1) * 128, :], og[i][:], R=[f"og{i}"], W=[f"XR{t}"])
        mk.barrier()
        P.close()

    def final_phase():
        P = Pool(nc, "fin")
        fw = P.sb([128, D], F32, "fw")
        mk.dma("sp", fw[:], fnw, W=["fw"])
        xt = [P.sb([128, D], F32, "xt") for _ in range(2)]
        ot = [P.sb([128, D], F32, "ot") for _ in range(2)]
        junk = P.sb([128, D], BF16, "junk")
        ss = [P.sb([128, 1], F32, "ss") for _ in range(2)]
        for t in OUT_T:
            i = t % 2
            mk.dma("sp", xt[i][:], XR[t * 128:(t + 1) * 128, :], R=[f"XR{t}"], W=[f"fxt{i}"])
            mk.op("act", "activation", out=junk[:], in_=xt[i][:], func=AF.Square, accum_out=ss[i][:], R=[f"fxt{i}"],
                  W=["fjunk", f"fss{i}"])
            mk.op("act", "activation", out=ss[i][:], in_=ss[i][:], func=AF.Sqrt, scale=1.0 / D, bias=EPS, R=[f"fss{i}"],
                  W=[f"fss{i}"])
            mk.op("dve", "reciprocal", out=ss[i][:], in_=ss[i][:], R=[f"fss{i}"], W=[f"fss{i}"])
            mk.op("dve", "scalar_tensor_tensor", out=ot[i][:], in0=xt[i][:], scalar=ss[i][:, 0:1], in1=fw[:], op0=ALU.mult,
                  op1=ALU.mult, R=[f"fxt{i}", f"fss{i}", "fw"], W=[f"fot{i}"])
            mk.dma("pool", yout[(t - 2) * 128:(t - 1) * 128, :], ot[i][:], R=[f"fot{i}"], W=["yout"])
        mk.barrier()
        P.close()

    stages = dict(mod=mod_phase, inproj=inproj_phase, a=mixer_a, b=mixer_b, c=mixer_c, d=mixer_d)
    return dict(nc=nc, mk=mk, stages=stages, merge=merge_moe_phase, final=final_phase, dbg=dbg,
                scr=dict(XR=XR, FM=FM, TM=TM, MG=MG, YT=YT))


def emit_all(prog, layers=NL, upto=None, skip=()):
    mk = prog["mk"]
    mk.barrier()
    done = False
    for l in range(layers):
        for s in ("mod", "inproj", "a", "b", "c", "d"):
            if s in skip:
                continue
            if s in ("inproj", "b", "c", "d"):
                prog["stages"][s](l, l == NL - 1)
            else:
                prog["stages"][s](l)
            if upto == (l, s):
                done = True
                break
        if done:
            break
        prog["merge"](l, l == NL - 1)
        if upto == (l, "merge"):
            done = True
            break
    if not done:
        prog["final"]()
    mk.barrier(engines=("sp",))


def _consts():
    j = np.arange(128)[:, None]
    i = np.arange(128)[None, :]
    same = (j // DL) == (i // DL)
    m = {}
    m["ident"] = (j == i)
    m["triF"] = (j <= i)
    m["triB"] = (j >= i)
    m["blkF"] = same & (j <= i)
    m["blkB"] = same & (j >= i)
    m["aftF"] = same & (j > i)
    m["befB"] = same & (j < i)
    m["diffF"] = np.maximum(i - j, 0)
    m["diffB"] = np.maximum(j - i, 0)
    m["maskF"] = (i >= j)
    m["maskB"] = (j > i)
    m["posF"] = np.broadcast_to(i + 1, (128, 128))
    m["posB"] = np.broadcast_to(128 - i, (128, 128))
    m["negF4"] = np.tile(np.where(j <= i, 0.0, -30000.0), (1, 4))
    m["negB4"] = np.tile(np.where(j >= i, 0.0, -30000.0), (1, 4))
    m["mblkF4"] = np.tile(same & (j <= i), (1, 4))
    m["mblkB4"] = np.tile(same & (j >= i), (1, 4))
    m["kpos"] = np.concatenate([127 - j, j], 1)
    m["ones"] = np.ones((128, 128))
    sel = np.zeros((128, 256))
    sel[0, 0:128] = 1.0
    sel[1, 128:256] = 1.0
    m["sel"] = sel
    m["subm"] = np.concatenate([(j // DL) == s_ for s_ in range(128 // DL)], 1)
    qm = np.zeros((128, 2, 5, 128), np.float32)
    for hh_ in range(2):
        for s_ in range(5):
            colsel = np.ones(128, bool) if s_ == 4 else (np.arange(128) // DL == s_)
            qm[hh_ * 64:(hh_ + 1) * 64, hh_, s_, :] = colsel[None, :]
    m["qmask"] = qm.reshape(128, 1280)
    m["ltS"] = (j < i)
    m["thrS"] = np.broadcast_to(np.arange(16)[None, :] * SLOT, (128, 16))
    m["kS"] = np.broadcast_to(np.arange(16)[None, :] * SLOT, (128, 16))
    m["jp"] = np.arange(4)[None, :] * 128 + np.arange(128)[:, None]
    out = np.zeros((128, NCST), np.float32)
    for k, (o, w) in CST.items():
        out[:, o:o + w] = np.asarray(m[k], np.float32)
    return out


def _rope_tables(flip=False):
    n = 16
    inv = np.power(np.float32(10000.0), -np.arange(n, dtype=np.float32) / n).astype(np.float32)
    t = np.arange(4096)
    row = (t // 64).astype(np.float32)
    col = (t % 64).astype(np.float32)
    ang = np.concatenate([row[:, None] * inv, col[:, None] * inv], -1)
    cos = np.cos(ang).astype(np.float32).T
    sin = np.sin(ang).astype(np.float32).T
    if flip:
        cos, sin = cos[:, ::-1], sin[:, ::-1]
    Cc = np.ones((128, T), np.float32)
    Ss = np.zeros((128, T), np.float32)
    for hh in range(2):
        Cc[hh * 64:hh * 64 + 32, 256:] = cos
        Cc[hh * 64 + 32:hh * 64 + 64, 256:] = cos
        Ss[hh * 64:hh * 64 + 32, 256:] = -sin
        Ss[hh * 64 + 32:hh * 64 + 64, 256:] = sin
    return Cc, Ss


def prep_shared(inp, flip=False):
    f = np.float32
    w_in = np.asarray(inp["w_in"], f)
    offs = {}
    o = 0
    for name, w in (("a_x", 256), ("a_g", 256), ("b_q", 256), ("b_k", 256), ("b_v", 256), ("b_g", 256), ("c_q", 256),
                    ("c_k", 256), ("c_v", 256), ("c_o", 256), ("c_gates", 16), ("d_q", 256), ("d_ff", 256),
                    ("d_fb", 256), ("d_i", 256), ("d_g", 256), ("merge", 4096)):
        offs[name] = (o, w)
        o += w

    def cols(n):
        a, w = offs[n]
        return w_in[:, :, a:a + w]

    perm = np.concatenate([np.arange(h * 64 + 32, h * 64 + 64).tolist() + np.arange(h * 64, h * 64 + 32).tolist()
                           for h in range(4)]).astype(np.int64)
    w_fm = np.concatenate([cols("a_x"), cols("a_g"), cols("b_q"), cols("b_q")[:, :, perm], cols("b_k"),
                           cols("b_k")[:, :, perm], cols("c_q"), cols("c_k")], -1)
    gperm = np.array([8, 9, 10, 11, 12, 13, 14, 15, 0, 1, 2, 3, 4, 5, 6, 7]) if flip else np.arange(16)
    dfa, dfb = ("d_fb", "d_ff") if flip else ("d_ff", "d_fb")
    w_tm = np.concatenate([cols("b_v"), cols("b_g"), cols("c_v"), cols("c_o"), cols("d_q"), cols(dfa), cols(dfb),
                           cols("d_i"), cols("d_g"), cols("c_gates")[:, :, gperm], cols("merge")], -1)
    sh = {}
    sh["w_mod"] = np.ascontiguousarray(inp["w_mod"], f)
    bm = np.asarray(inp["b_mod"], f)
    sh["bmod_c"] = np.ascontiguousarray(bm.reshape(NL, 48, 128).transpose(0, 2, 1))
    sh["bmod_r"] = np.ascontiguousarray(bm.reshape(NL, 1, 6144))
    sh["w_fm"] = np.ascontiguousarray(w_fm)
    sh["w_tm"] = np.ascontiguousarray(w_tm)
    acw = np.asarray(inp["a_conv_w"], f)
    zt_ = np.zeros_like(acw[:, :1])
    acw = np.concatenate([zt_, acw[:, ::-1]], 1) if flip else np.concatenate([acw, zt_], 1)
    sh["a_cw"] = np.ascontiguousarray(acw.reshape(NL, 5, 2, 128).transpose(0, 3, 2, 1))
    sh["a_cb"] = np.ascontiguousarray(np.asarray(inp["a_conv_b"], f).reshape(NL, 2, 128).transpose(0, 2, 1))
    gw = np.asarray(inp["a_gate_w"], f)
    agw = np.zeros((NL, 128, 2, 2, 2, 128), f)
    for c in range(2):
        for hh in range(2):
            agw[:, hh * 64:(hh + 1) * 64, :, :, c, hh * 64:(hh + 1) * 64] = gw[:, :, :, 2 * c + hh].transpose(0, 3, 1, 2, 4)
    sh["a_gw"] = np.ascontiguousarray(agw[:, :, ::-1]) if flip else agw
    gb = np.asarray(inp["a_gate_b"], f)
    if flip:
        gb = gb[:, ::-1]
    sh["a_gb"] = np.ascontiguousarray(gb.reshape(NL, 2, 2, 2, 128).transpose(0, 4, 1, 2, 3))
    lam = np.asarray(inp["a_lambda"], f)
    if flip:
        lam = lam[:, ::-1]
    sh["a_lam"] = np.ascontiguousarray(lam.reshape(NL, 2, 2, 128).transpose(0, 3, 1, 2))
    th = np.asarray(inp["b_theta"], f)
    if flip:
        th = th[:, ::-1]
    thp = np.zeros((NL, 128, 2, 2), f)
    for c in range(2):
        for hh in range(2):
            thp[:, hh * 64:(hh + 1) * 64, :, c] = th[:, None, :, 2 * c + hh]
    sh["b_thp"] = thp
    sh["b_thh"] = np.ascontiguousarray(np.broadcast_to(th[:, None], (NL, 128, 2, 4)))
    ccw = np.asarray(inp["c_conv_w"], f)
    zt_ = np.zeros_like(ccw[:, :1])
    ccw = np.concatenate([zt_, ccw[:, ::-1]], 1) if flip else np.concatenate([ccw, zt_], 1)
    sh["c_cw"] = np.ascontiguousarray(ccw.reshape(NL, 5, 4, 128).transpose(0, 3, 2, 1))
    sh["c_cb"] = np.ascontiguousarray(np.asarray(inp["c_conv_b"], f).reshape(NL, 4, 128).transpose(0, 2, 1))
    sh["c_gb"] = np.ascontiguousarray(np.broadcast_to(np.asarray(inp["c_gate_b"], f).reshape(NL, 1, 16)[:, :, gperm], (NL, 128, 16)))
    sh["d_lbr"] = np.ascontiguousarray(np.broadcast_to(np.asarray(inp["d_lb"], f)[None], (128, 2, 256)))
    sh["w_branch"] = np.ascontiguousarray(inp["w_branch"], f)
    sh["w_out"] = np.ascontiguousarray(inp["w_out"], f)
    sh["moe_wgr"] = np.ascontiguousarray(np.concatenate([np.asarray(inp["moe_w_group"], f), np.asarray(inp["moe_w_router"], f)], -1))
    sh["moe_bgr"] = np.ascontiguousarray(np.concatenate([np.asarray(inp["moe_b_group"], f), np.asarray(inp["moe_b_router"], f)], -1).reshape(NL, 1, 20))
    sh["moe_w1"] = np.ascontiguousarray(inp["moe_w1"], f)
    sh["moe_w3"] = np.ascontiguousarray(inp["moe_w3"], f)
    sh["moe_w2"] = np.ascontiguousarray(inp["moe_w2"], f)
    sh["fnw"] = np.ascontiguousarray(np.broadcast_to(np.asarray(inp["final_norm_w"], f)[None], (128, D)))
    sh["cst"] = _consts()
    sh["ropeC"], sh["ropeS"] = _rope_tables(flip)
    return sh


def prep_core(inp, b, flip=False):
    f = np.float32
    d = {}
    cx, xx = np.asarray(inp["ctx"][b], f), np.asarray(inp["x"][b], f)
    if flip:
        cx, xx = cx[::-1], xx[::-1]
    d["xin"] = np.ascontiguousarray(np.concatenate([cx, xx], 0))
    cv = np.stack([np.asarray(inp["c_ctx"], f), np.asarray(inp["c"][b], f)], -1)
    d["cvec"] = np.ascontiguousarray(cv.reshape(8, 128, 2).transpose(1, 0, 2))
    return d


_PROG = None


def kernel(**inputs):
    global _PROG
    if _PROG is None:
        _PROG = build_program()
        emit_all(_PROG)
    nc = _PROG["nc"]
    shs = [prep_shared(inputs, False), prep_shared(inputs, True)]
    in_maps = []
    for core in range(8):
        fl = core >= 4
        m = dict(shs[1 if fl else 0])
        m.update(prep_core(inputs, core % 4, fl))
        in_maps.append(m)
    res = run_bass_kernel_spmd(nc, in_maps, core_ids=list(range(8)))
    out = np.empty((4, 4096, D), np.float32)
    for b in range(4):
        out[b, :HALF_OUT] = np.asarray(res.results[b]["yout"], np.float32)[:HALF_OUT]
        out[b, HALF_OUT:] = np.asarray(res.results[b + 4]["yout"], np.float32)[:4096 - HALF_OUT][::-1]
    return out
```
